# Optimizing a Trainium2 kernel written in Bass

```python
import jax
import jax.numpy as jnp
from jax import lax
import numpy as np

D_MODEL = 1024
BATCH = 16
SEQ = 4096
DEPTH = 1

N_MEM = 256
RMS_EPS = 1e-6

RW_HEADS = 8
RW_HEAD_DIM = 64
RW_WIDTH = RW_HEADS * RW_HEAD_DIM
RW_DECAY_LORA = 64
RW_AAA_LORA = 64
RW_GATE_LORA = 128
RW_GN_EPS = 64e-5

DSA_HEADS = 8
DSA_HEAD_DIM = 64
DSA_WIDTH = DSA_HEADS * DSA_HEAD_DIM
DSA_KV_RANK = 128
IDX_HEADS = 4
IDX_DIM = 64
IDX_TOPK_MAX = 256
Q_BLOCK = 128

X_HEADS = 4
X_HEAD_DIM = 256
X_WIDTH = X_HEADS * X_HEAD_DIM

N_GROUPS = 4
EXPERTS_PER_GROUP = 8
N_EXPERTS = N_GROUPS * EXPERTS_PER_GROUP
TOP_K_INNER = 2
D_EXPERT = 512
MOE_BLOCK = 256

RW_SIZES = (RW_WIDTH, RW_WIDTH, RW_WIDTH, RW_DECAY_LORA, RW_AAA_LORA, RW_GATE_LORA)
DSA_SIZES = (DSA_WIDTH, DSA_KV_RANK, IDX_HEADS * IDX_DIM, IDX_DIM, IDX_HEADS)
RW_COLS = sum(RW_SIZES)
DSA_COLS = sum(DSA_SIZES)
GATE_COLS = 2 * D_MODEL
IN_COLS = RW_COLS + DSA_COLS + GATE_COLS

kernel_name = 'hybrid_rwkv7_dsa_hmoe_block'


def _split(t, sizes):
    cuts = [int(c) for c in np.cumsum(sizes)[:-1]]
    return jnp.split(t, cuts, axis=-1)


def rmsnorm(x, g):
    xf = x.astype(jnp.float32)
    y = xf * lax.rsqrt(jnp.mean(xf * xf, axis=-1, keepdims=True) + RMS_EPS)
    return (y * g.astype(jnp.float32)).astype(x.dtype)


def rwkv7_branch(z, mu, w0, w2, a0, a2, g2, k_k, k_a, r_k, ln_w, ln_b):
    B, S, _ = z.shape
    H, N = RW_HEADS, RW_HEAD_DIM
    f32 = jnp.float32
    z_prev = jnp.pad(z, ((0, 0), (1, 0), (0, 0)))[:, :-1]
    z = z + (z_prev - z) * mu
    r, k, v, xw, xa, xg = _split(z, RW_SIZES)
    w = -jax.nn.softplus(-(w0 + jnp.tanh(xw) @ w2).astype(f32)) - 0.5
    decay = jnp.exp(-jnp.exp(w))
    a = jax.nn.sigmoid(a0 + xa @ a2)
    g = jax.nn.sigmoid(xg) @ g2
    heads = lambda t: t.astype(f32).reshape(B, S, H, N)
    kk = heads(k * k_k)
    kk = kk / jnp.maximum(jnp.sqrt(jnp.sum(kk * kk, -1, keepdims=True)), 1e-12)
    k = k * (1.0 + (a - 1.0) * k_a)
    r_h, k_h, v_h, a_h = heads(r), heads(k), heads(v), heads(a)
    seq_major = lambda t: jnp.moveaxis(t, 1, 0)
    xs = (seq_major(r_h), seq_major(heads(decay)), seq_major(k_h),
          seq_major(v_h), seq_major(kk), seq_major(a_h))

    def step(state, inp):
        r_t, w_t, k_t, v_t, kk_t, a_t = inp
        sa = jnp.einsum('bhij,bhj->bhi', state, -kk_t)
        state = (state * w_t[:, :, None, :]
                 + sa[..., None] * (kk_t * a_t)[:, :, None, :]
                 + v_t[..., None] * k_t[:, :, None, :])
        return state, jnp.einsum('bhij,bhj->bhi', state, r_t)

    _, y = lax.scan(step, jnp.zeros((B, H, N, N), f32), xs)
    y = jnp.moveaxis(y, 0, 1)
    mean = jnp.mean(y, -1, keepdims=True)
    var = jnp.mean(jnp.square(y - mean), -1, keepdims=True)
    y = ((y - mean) * lax.rsqrt(var + RW_GN_EPS)).reshape(B, S, RW_WIDTH)
    y = y * ln_w.astype(f32) + ln_b.astype(f32)
    bonus = jnp.sum(r_h * k_h * r_k.astype(f32), -1, keepdims=True) * v_h
    y = y + bonus.reshape(B, S, RW_WIDTH)
    return (y * g.astype(f32)).astype(z.dtype)


def dsa_branch(z, kv_norm, w_uk, w_uv):
    B, S, _ = z.shape
    f32 = jnp.float32
    k_sel = min(IDX_TOPK_MAX, S // 4)
    q, c_kv, q_idx, k_idx, w_idx = _split(z, DSA_SIZES)
    q = q.reshape(B, S, DSA_HEADS, DSA_HEAD_DIM)
    c_kv = rmsnorm(c_kv, kv_norm)
    q_lat = jnp.einsum('bshd,rhd->bshr', q, w_uk) * (DSA_HEAD_DIM ** -0.5)
    q_idx = q_idx.reshape(B, S, IDX_HEADS, IDX_DIM)
    w_idx = w_idx * ((IDX_HEADS * IDX_DIM) ** -0.5)
    nb = S // Q_BLOCK
    to_blocks = lambda t: jnp.moveaxis(t.reshape(B, nb, Q_BLOCK, *t.shape[2:]), 1, 0)
    key_pos = jnp.arange(S)

    def block(args):
        b, ql, qi, wi = args
        t_pos = b * Q_BLOCK + jnp.arange(Q_BLOCK)
        rel = jax.nn.relu(jnp.einsum('bqhd,bsd->bqhs', qi, k_idx).astype(f32))
        iscore = jnp.einsum('bqh,bqhs->bqs', wi.astype(f32), rel)
        causal = key_pos[None, :] <= t_pos[:, None]
        iscore = jnp.where(causal[None], iscore, -jnp.inf)
        _, idx = lax.top_k(iscore, k_sel)
        valid = idx <= t_pos[None, :, None]
        c_sel = jax.vmap(lambda c, i: c[i])(c_kv, idx)
        s = jnp.einsum('bqhr,bqkr->bhqk', ql, c_sel).astype(f32)
        s = jnp.where(valid[:, None], s, -jnp.inf)
        p = jax.nn.softmax(s, axis=-1).astype(c_sel.dtype)
        o_lat = jnp.einsum('bhqk,bqkr->bqhr', p, c_sel)
        return jnp.einsum('bqhr,rhd->bqhd', o_lat, w_uv)

    o = lax.map(block, (jnp.arange(nb), to_blocks(q_lat), to_blocks(q_idx), to_blocks(w_idx)))
    return jnp.moveaxis(o, 0, 1).reshape(B, S, DSA_WIDTH)


def cross_attention(hn, mem_n, w_cq, w_ckv, w_co):
    B, S, _ = hn.shape
    M = mem_n.shape[1]
    q = (hn @ w_cq).reshape(B, S, X_HEADS, X_HEAD_DIM)
    k, v = jnp.split((mem_n @ w_ckv).reshape(B, M, 2, X_HEADS, X_HEAD_DIM), 2, axis=2)
    k, v = k[:, :, 0], v[:, :, 0]
    s = jnp.einsum('bshd,bmhd->bhsm', q, k).astype(jnp.float32) * (X_HEAD_DIM ** -0.5)
    p = jax.nn.softmax(s, axis=-1).astype(v.dtype)
    o = jnp.einsum('bhsm,bmhd->bshd', p, v).reshape(B, S, X_WIDTH)
    return o @ w_co


def hier_moe(hn, w_rg, b_rg, w_re, b_re, w_gate, w_up, w_down):
    B, S, D = hn.shape
    T = B * S
    xt = hn.reshape(T, D)
    g_prob = jax.nn.softmax((xt @ w_rg + b_rg).astype(jnp.float32), axis=-1)
    p_grp, grp = lax.top_k(g_prob, 1)
    e_logits = (xt @ w_re + b_re).astype(jnp.float32).reshape(T, N_GROUPS, EXPERTS_PER_GROUP)
    e_logits = jnp.take_along_axis(e_logits, grp[:, :, None], axis=1)[:, 0]
    p_e, e_loc = lax.top_k(jax.nn.softmax(e_logits, axis=-1), TOP_K_INNER)
    gate = p_grp * p_e / jnp.sum(p_e, -1, keepdims=True)
    expert = grp * EXPERTS_PER_GROUP + e_loc
    A = T * TOP_K_INNER
    e_flat = expert.reshape(A)
    tok_flat = jnp.repeat(jnp.arange(T, dtype=jnp.int32), TOP_K_INNER)
    g_flat = gate.reshape(A)
    order = jnp.argsort(e_flat)
    e_sorted = e_flat[order]
    counts = jnp.bincount(e_flat, length=N_EXPERTS)
    start = jnp.cumsum(counts) - counts
    padded = (counts + MOE_BLOCK - 1) // MOE_BLOCK * MOE_BLOCK
    pend = jnp.cumsum(padded)
    pstart = pend - padded
    dest = pstart[e_sorted] + (jnp.arange(A) - start[e_sorted])
    P = A + N_EXPERTS * MOE_BLOCK
    buf_tok = jnp.full((P,), T, jnp.int32).at[dest].set(tok_flat[order])
    buf_gate = jnp.zeros((P,), jnp.float32).at[dest].set(g_flat[order])
    n_blocks = P // MOE_BLOCK
    blk_expert = jnp.minimum(
        jnp.searchsorted(pend, jnp.arange(n_blocks) * MOE_BLOCK, side='right'), N_EXPERTS - 1)
    x_pad = jnp.concatenate([xt, jnp.zeros((1, D), xt.dtype)], axis=0)

    def run_block(args):
        e, toks, gts = args
        xb = x_pad[toks]
        hid = jax.nn.silu(xb @ w_gate[e]) * (xb @ w_up[e])
        return (hid @ w_down[e]) * gts[:, None].astype(xb.dtype)

    yb = lax.map(run_block, (blk_expert, buf_tok.reshape(n_blocks, MOE_BLOCK),
                             buf_gate.reshape(n_blocks, MOE_BLOCK)))
    y = jnp.zeros((T + 1, D), hn.dtype).at[buf_tok].add(yb.reshape(P, D))[:T]
    return y.reshape(B, S, D)


def hybrid_layer(h, mem, norm_mix, w_in, shift_mu, rw_w0, rw_w2, rw_a0, rw_a2, rw_g2,
                 rw_k_k, rw_k_a, rw_r_k, rw_ln_w, rw_ln_b, kv_norm, w_uk, w_uv,
                 w_proj_a, w_proj_b, b_gate, w_out, norm_cross, norm_mem, w_cq, w_ckv, w_co,
                 norm_ffn, w_router_g, b_router_g, w_router_e, b_router_e,
                 w_e_gate, w_e_up, w_e_down):
    z = rmsnorm(h, norm_mix) @ w_in
    z_rw, z_dsa, z_gate = _split(z, (RW_COLS, DSA_COLS, GATE_COLS))
    y_a = rwkv7_branch(z_rw, shift_mu, rw_w0, rw_w2, rw_a0, rw_a2, rw_g2,
                       rw_k_k, rw_k_a, rw_r_k, rw_ln_w, rw_ln_b)
    y_b = dsa_branch(z_dsa, kv_norm, w_uk, w_uv)
    g_a, g_b = jnp.split(jax.nn.sigmoid(z_gate + b_gate), 2, axis=-1)
    h = h + (g_a * (y_a @ w_proj_a) + g_b * (y_b @ w_proj_b)) @ w_out
    h = h + cross_attention(rmsnorm(h, norm_cross), rmsnorm(mem, norm_mem), w_cq, w_ckv, w_co)
    h = h + hier_moe(rmsnorm(h, norm_ffn), w_router_g, b_router_g, w_router_e, b_router_e,
                     w_e_gate, w_e_up, w_e_down)
    return h


def setup_inputs(seed: int = 0) -> dict:
    key = jax.random.key(seed)
    ks = iter(jax.random.split(key, 48))
    f32 = jnp.float32
    L = DEPTH

    def nrm(shape, scale):
        return jax.random.normal(next(ks), shape, f32) * scale

    def gain(shape):
        return 1.0 + nrm(shape, 0.02)

    return {
        'x': nrm((BATCH, SEQ, D_MODEL), 1.0),
        'mem': nrm((BATCH, N_MEM, D_MODEL), 1.0),
        'norm_mix': gain((L, D_MODEL)),
        'w_in': nrm((L, D_MODEL, IN_COLS), D_MODEL ** -0.5),
        'shift_mu': jax.random.uniform(next(ks), (L, RW_COLS), f32),
        'rw_w0': jax.random.uniform(next(ks), (L, RW_WIDTH), f32, -5.0, 1.0),
        'rw_w2': nrm((L, RW_DECAY_LORA, RW_WIDTH), 0.1 * RW_DECAY_LORA ** -0.5),
        'rw_a0': nrm((L, RW_WIDTH), 0.1),
        'rw_a2': nrm((L, RW_AAA_LORA, RW_WIDTH), 0.1 * RW_AAA_LORA ** -0.5),
        'rw_g2': nrm((L, RW_GATE_LORA, RW_WIDTH), RW_GATE_LORA ** -0.5),
        'rw_k_k': 0.85 + nrm((L, RW_WIDTH), 0.02),
        'rw_k_a': gain((L, RW_WIDTH)),
        'rw_r_k': nrm((L, RW_HEADS, RW_HEAD_DIM), 0.1),
        'rw_ln_w': gain((L, RW_WIDTH)),
        'rw_ln_b': nrm((L, RW_WIDTH), 0.01),
        'kv_norm': gain((L, DSA_KV_RANK)),
        'w_uk': nrm((L, DSA_KV_RANK, DSA_HEADS, DSA_HEAD_DIM), DSA_KV_RANK ** -0.5),
        'w_uv': nrm((L, DSA_KV_RANK, DSA_HEADS, DSA_HEAD_DIM), DSA_KV_RANK ** -0.5),
        'w_proj_a': nrm((L, RW_WIDTH, D_MODEL), RW_WIDTH ** -0.5),
        'w_proj_b': nrm((L, DSA_WIDTH, D_MODEL), DSA_WIDTH ** -0.5),
        'b_gate': nrm((L, GATE_COLS), 0.01),
        'w_out': nrm((L, D_MODEL, D_MODEL), D_MODEL ** -0.5),
        'norm_cross': gain((L, D_MODEL)),
        'norm_mem': gain((L, D_MODEL)),
        'w_cq': nrm((L, D_MODEL, X_WIDTH), D_MODEL ** -0.5),
        'w_ckv': nrm((L, D_MODEL, 2 * X_WIDTH), D_MODEL ** -0.5),
        'w_co': nrm((L, X_WIDTH, D_MODEL), X_WIDTH ** -0.5),
        'norm_ffn': gain((L, D_MODEL)),
        'w_router_g': nrm((L, D_MODEL, N_GROUPS), D_MODEL ** -0.5),
        'b_router_g': nrm((L, N_GROUPS), 0.01),
        'w_router_e': nrm((L, D_MODEL, N_EXPERTS), D_MODEL ** -0.5),
        'b_router_e': nrm((L, N_EXPERTS), 0.01),
        'w_e_gate': nrm((L, N_EXPERTS, D_MODEL, D_EXPERT), D_MODEL ** -0.5),
        'w_e_up': nrm((L, N_EXPERTS, D_MODEL, D_EXPERT), D_MODEL ** -0.5),
        'w_e_down': nrm((L, N_EXPERTS, D_EXPERT, D_MODEL), D_EXPERT ** -0.5),
        'norm_final': gain((D_MODEL,)),
    }


def reference(x, mem, norm_mix, w_in, shift_mu, rw_w0, rw_w2, rw_a0, rw_a2, rw_g2,
              rw_k_k, rw_k_a, rw_r_k, rw_ln_w, rw_ln_b, kv_norm, w_uk, w_uv,
              w_proj_a, w_proj_b, b_gate, w_out, norm_cross, norm_mem, w_cq, w_ckv, w_co,
              norm_ffn, w_router_g, b_router_g, w_router_e, b_router_e,
              w_e_gate, w_e_up, w_e_down, norm_final):
    h = x
    for l in range(DEPTH):
        h = hybrid_layer(h, mem, norm_mix[l], w_in[l], shift_mu[l], rw_w0[l], rw_w2[l],
                         rw_a0[l], rw_a2[l], rw_g2[l], rw_k_k[l], rw_k_a[l], rw_r_k[l],
                         rw_ln_w[l], rw_ln_b[l], kv_norm[l], w_uk[l], w_uv[l],
                         w_proj_a[l], w_proj_b[l], b_gate[l], w_out[l],
                         norm_cross[l], norm_mem[l], w_cq[l], w_ckv[l], w_co[l],
                         norm_ffn[l], w_router_g[l], b_router_g[l], w_router_e[l], b_router_e[l],
                         w_e_gate[l], w_e_up[l], w_e_down[l])
    return rmsnorm(h, norm_final)
```

```python
from contextlib import ExitStack
import os
import numpy as np
import ml_dtypes
import concourse.bass as bass
import concourse.mybir as mybir
from concourse.bass_utils import run_bass_kernel_spmd

F32 = mybir.dt.float32
BF16 = mybir.dt.bfloat16
AF = mybir.ActivationFunctionType
ALU = mybir.AluOpType
AX = mybir.AxisListType

D = 1024
NCORES = 8


class Buf:
    __slots__ = ("name", "w", "r")

    def __init__(self, name=""):
        self.name = name
        self.w = None
        self.r = {}


class Sched:
    ENG = ("pe", "act", "dve", "pool", "sp")

    def __init__(self, nc, stack, n_dma_sems=10):
        self.nc = nc
        self.streams = {e: [] for e in self.ENG}
        self.sems = {}
        self.count = {}
        for e in self.ENG:
            self.sems[e] = stack.enter_context(nc.semaphore("s_" + e))
            self.count[e] = 0
        self.dma_sems = {}
        self.dma_rr = {}
        for q in ("sp", "act", "pool"):
            lst = []
            for i in range(n_dma_sems if q != "pool" else 28):
                k = "d_%s_%d" % (q, i)
                self.sems[k] = stack.enter_context(nc.semaphore(k))
                self.count[k] = 0
                lst.append(k)
            self.dma_sems[q] = lst
            self.dma_rr[q] = 0
        self.waited = {}
        self.nwaits = 0
        self.nops = 0

    def _wait(self, eng, key, val):
        if val <= 0 or self.waited.get((eng, key), 0) >= val:
            return
        self.waited[(eng, key)] = val
        self.streams[eng].append(("w", key, val))
        self.nwaits += 1

    def _deps(self, eng, reads, writes, own_key):
        for b in reads:
            if b.w is not None:
                self._dep(eng, b.w, own_key)
        for b in writes:
            if b.w is not None:
                self._dep(eng, b.w, own_key)
            for k, v in b.r.items():
                self._dep(eng, (k, v), own_key)

    def _dep(self, eng, ev, own_key):
        k, v = ev
        if k == "pe" and own_key == "pe":
            return
        self._wait(eng, k, v)

    muted = False

    def op(self, eng, fn, reads=(), writes=()):
        if self.muted:
            return
        self._deps(eng, reads, writes, eng)
        self.count[eng] += 1
        v = self.count[eng]
        self.streams[eng].append(("o", fn, eng, 1))
        for b in writes:
            b.w = (eng, v)
            b.r = {}
        for b in reads:
            if b.r.get(eng, 0) < v:
                b.r[eng] = v
        self.nops += 1

    def dma(self, q, out, in_, reads=(), writes=(), fn=None, **kw):
        if self.muted:
            return
        lst = self.dma_sems[q]
        key = lst[self.dma_rr[q] % len(lst)]
        self.dma_rr[q] += 1
        self._wait(q, key, self.count[key])
        self._deps(q, reads, writes, key)
        self.count[key] += 16
        v = self.count[key]
        if fn is None:
            fn = lambda e, out=out, in_=in_, kw=kw: e.dma_start(out=out, in_=in_, **kw)
        self.streams[q].append(("o", fn, key, 16))
        for b in writes:
            b.w = (key, v)
            b.r = {}
        for b in reads:
            if b.r.get(key, 0) < v:
                b.r[key] = v
        self.nops += 1

    def barrier(self):
        for e in self.ENG:
            for k in self.sems:
                if k != e or True:
                    self._wait(e, k, self.count[k])

    def finish(self, bufs, eng="sp"):
        for b in bufs:
            if b.w is not None:
                self._wait(eng, b.w[0], b.w[1])

    def emit(self):
        nc = self.nc
        sems = self.sems
        streams = self.streams
        with nc.Block() as block:
            def run(engobj, lst):
                for it in lst:
                    if it[0] == "w":
                        engobj.wait_ge(sems[it[1]], it[2])
                    else:
                        it[1](engobj).then_inc(sems[it[2]], it[3])

            @block.tensor
            def _(e):
                run(e, streams["pe"])

            @block.scalar
            def _(e):
                run(e, streams["act"])

            @block.vector
            def _(e):
                run(e, streams["dve"])

            @block.gpsimd
            def _(e):
                run(e, streams["pool"])

            @block.sync
            def _(e):
                run(e, streams["sp"])


class Ctx:
    def __init__(self, nc, S):
        self.nc = nc
        self.S = S
        self.B = {}
        self.rr = 0
        self.uid = 0

    def buf(self, name):
        if name not in self.B:
            self.B[name] = Buf(name)
        return self.B[name]

    def _bl(self, lst):
        return [self.buf(x) if isinstance(x, str) else x for x in lst]

    def sb(self, st, name, shape, dt):
        self.uid += 1
        t = st.enter_context(self.nc.sbuf_tensor("%s_u%d" % (name, self.uid), list(shape), dt))
        self.buf(name)
        return t

    def ps(self, st, name, shape, dt):
        self.uid += 1
        t = st.enter_context(self.nc.psum_tensor("%s_u%d" % (name, self.uid), list(shape), dt))
        self.buf(name)
        return t

    def op(self, eng, method, reads, writes, **kw):
        self.S.op(eng, lambda e, m=method, kw=kw: getattr(e, m)(**kw), self._bl(reads), self._bl(writes))

    def mm(self, out, lhsT, rhs, reads, writes, start=True, stop=True, **kw):
        self.S.op("pe", lambda e: e.matmul(out, lhsT, rhs, start=start, stop=stop, **kw),
                  self._bl(reads), self._bl(writes))

    def tr(self, out, in_, ident, reads, writes):
        self.S.op("pe", lambda e: e.transpose(out, in_, ident), self._bl(reads), self._bl(writes))

    def dma(self, q, out, in_, reads, writes, **kw):
        self.S.dma(q, out, in_, self._bl(reads), self._bl(writes), **kw)

    def q(self):
        self.rr += 1
        return ("sp", "act", "pool")[self.rr % 3]


C_RW = 0
C_Q = 1792
C_CKV = 2304
C_QI = 2432
C_KI = 2688
C_WI = 2752
C_G = 2756
R_RW = 0
R_Q = 1792
R_QI = 2304
R_KI = 2560
R_G = 2624
R_WI = 4672
R_TOT = 4676


def load_cast(K, st, tag, w_ap, kin, n, scale_col=None, dt=BF16, engs=("dve", "pool")):
    nc = K.nc
    kc = kin // 128
    wt = K.sb(st, tag, [128, kc, n], dt)
    src = w_ap.rearrange("(c p) n -> p c n", p=128)
    if True:
        stg = [K.sb(st, "%s_stg%d" % (tag, i), [128, n], F32) for i in range(2)]
        for c in range(kc):
            sg = stg[c % 2]
            nm = "%s_stg%d" % (tag, c % 2)
            K.dma(K.q(), sg[:], src[:, c, :], [], [nm])
            eng = engs[c % len(engs)]
            if scale_col is None:
                K.op(eng, "tensor_copy", [nm], [tag], out=wt[:, c, :], in_=sg[:])
            else:
                K.op(eng, "tensor_scalar", [nm, scale_col[1]], [tag], out=wt[:, c, :], in0=sg[:],
                     scalar1=scale_col[0][:, c:c + 1], scalar2=None, op0=ALU.mult)
    return wt


def norm_rows(K, tag, xt, xt_name, ss, junk, eps_scale=1.0 / D):
    K.op("act", "activation", [xt_name], [tag + "_junk", tag + "_ss"], out=junk[:], in_=xt[:], func=AF.Square,
         accum_out=ss[:])
    K.op("act", "activation", [tag + "_ss"], [tag + "_ss"], out=ss[:], in_=ss[:], func=AF.Sqrt,
         scale=eps_scale, bias=1e-6)
    K.op("dve", "reciprocal", [tag + "_ss"], [tag + "_ss"], out=ss[:], in_=ss[:])


def phase1(K, s, T, X, Wd, SC, CONST):
    nc = K.nc
    NT = T // 128
    NB = T // 512
    with ExitStack() as st:
        xnT = K.sb(st, "p1_xnT", [128, 8, T], BF16)
        gm = K.sb(st, "p1_gm", [128, 8], F32)
        K.dma("sp", gm[:], Wd["norm_mix"].rearrange("o (c p) -> p (o c)", p=128), [], ["p1_gm"], allow_slow_non_contiguous=True)
        bg = K.sb(st, "p1_bg", [128, 16], F32)
        K.dma("sp", bg[:], Wd["b_gate"].rearrange("o (c p) -> p (o c)", p=128), [], ["p1_bg"], allow_slow_non_contiguous=True)
        identb = CONST["identb"]
        pst = K.ps(st, "p1_pst", [128, 8, 128], BF16)
        xts = [K.sb(st, "p1_xt%d" % i, [128, D], F32) for i in range(2)]
        xnb = [K.sb(st, "p1_xn%d" % i, [128, D], BF16) for i in range(2)]
        junk = K.sb(st, "p1_junk", [128, D], F32)
        sss = [K.sb(st, "p1_ss%d" % i, [128, 1], F32) for i in range(2)]
        for tt in range(NT):
            i = tt % 2
            xt, xn, ss = xts[i], xnb[i], sss[i]
            K.dma("sp" if i == 0 else "act", xt[:], X[s * T + tt * 128: s * T + (tt + 1) * 128, :], [], ["p1_xt%d" % i])
            K.op("act", "activation", ["p1_xt%d" % i], ["p1_junk", "p1_ss%d" % i], out=junk[:], in_=xt[:],
                 func=AF.Square, accum_out=ss[:])
            K.op("act", "activation", ["p1_ss%d" % i, "eps6"], ["p1_ss%d" % i], out=ss[:], in_=ss[:], func=AF.Sqrt,
                 scale=1.0 / D, bias=CONST["eps6"][:])
            K.op("dve", "reciprocal", ["p1_ss%d" % i], ["p1_ss%d" % i], out=ss[:], in_=ss[:])
            K.op("dve", "tensor_scalar", ["p1_xt%d" % i, "p1_ss%d" % i], ["p1_xn%d" % i], out=xn[:], in0=xt[:],
                 scalar1=ss[:], scalar2=None, op0=ALU.mult)
            for c in range(8):
                K.tr(pst[:, c, :], xn[:, c * 128:(c + 1) * 128], identb[:], ["p1_xn%d" % i, "identb"], ["p1_pst"])
            K.op("pool" if False else "act", "activation", ["p1_pst"], ["p1_xnT"], out=xnT[:, :, tt * 128:(tt + 1) * 128],
                 in_=pst[:], func=AF.Copy)
        import os
        STOP = int(os.environ.get("STOP", "99"))
        if STOP <= 1:
            return
        chunks = []
        for i in range(14):
            chunks.append((C_RW + i * 128, 128, R_RW + i * 128, "fm", None))
        for i in range(4):
            chunks.append((C_Q + i * 128, 128, R_Q + i * 128, "fm", None))
        for i in range(2):
            chunks.append((C_QI + i * 128, 128, R_QI + i * 128, "fm", None))
        chunks.append((C_KI, 64, R_KI, "fm", None))
        chunks.append((C_WI, 4, R_WI, "fm", None))
        for i in range(16):
            chunks.append((C_G + i * 128, 128, R_G + i * 128, "gate", i))
        wsrc = Wd["w_in"].rearrange("o (c p) n -> p (o c) n", p=128)
        wst = [K.sb(st, "p1_wst%d" % i, [128, 8, 132], F32) for i in range(2)]
        wbf = [K.sb(st, "p1_wbf%d" % i, [128, 8, 132], BF16) for i in range(2)]
        stage = [K.sb(st, "p1_stage%d" % i, [128, T], F32) for i in range(2)]
        pss = [K.ps(st, "p1_ps%d" % i, [128, 512], F32) for i in range(4)]
        gmb = gm[:].unsqueeze(2).to_broadcast([128, 8, 128])
        ZF = SC["ZF"]
        for ci, (c0, ncol, r0, kind, gi) in enumerate(chunks):
            i = ci % 2
            K.dma("sp" if i == 0 else "pool", wst[i][:, :, 0:ncol], wsrc[:, :, c0:c0 + ncol], [], ["p1_wst%d" % i])
            K.op("dve", "tensor_tensor", ["p1_wst%d" % i, "p1_gm"], ["p1_wbf%d" % i], out=wbf[i][:, :, 0:ncol],
                 in0=wst[i][:, :, 0:ncol], in1=gm[:].unsqueeze(2).to_broadcast([128, 8, ncol]), op=ALU.mult)
            for tb in range(NB):
                pj = (ci * NB + tb) % 4
                ps = pss[pj]
                for dc in range(8):
                    K.mm(ps[0:ncol, :], wbf[i][:, dc, 0:ncol], xnT[:, dc, tb * 512:(tb + 1) * 512],
                         ["p1_wbf%d" % i, "p1_xnT"], ["p1_ps%d" % pj], start=(dc == 0), stop=(dc == 7))
                if kind == "gate":
                    K.op("act", "activation", ["p1_ps%d" % pj, "p1_bg"], ["p1_stage%d" % i],
                         out=stage[i][0:ncol, tb * 512:(tb + 1) * 512], in_=ps[0:ncol, :], func=AF.Sigmoid,
                         bias=bg[:, gi:gi + 1])
                else:
                    eng = "dve" if tb % 2 == 0 else "act"
                    if eng == "dve":
                        K.op("dve", "tensor_copy", ["p1_ps%d" % pj], ["p1_stage%d" % i],
                             out=stage[i][0:ncol, tb * 512:(tb + 1) * 512], in_=ps[0:ncol, :])
                    else:
                        K.op("act", "activation", ["p1_ps%d" % pj], ["p1_stage%d" % i],
                             out=stage[i][0:ncol, tb * 512:(tb + 1) * 512], in_=ps[0:ncol, :], func=AF.Copy)
            K.dma("act" if i == 0 else "sp", ZF[s, r0:r0 + ncol, :], stage[i][0:ncol, :], ["p1_stage%d" % i], ["ZF"])
        if STOP <= 2:
            return
        i = len(chunks) % 2
        K.dma("sp", wst[i][:, :, 0:128], wsrc[:, :, C_CKV:C_CKV + 128], [], ["p1_wst%d" % i])
        K.op("dve", "tensor_tensor", ["p1_wst%d" % i, "p1_gm"], ["p1_wbf%d" % i], out=wbf[i][:, :, 0:128],
             in0=wst[i][:, :, 0:128], in1=gm[:].unsqueeze(2).to_broadcast([128, 8, 128]), op=ALU.mult)
        ck = [K.sb(st, "p1_ck%d" % j, [128, 128], F32) for j in range(2)]
        ckb = [K.sb(st, "p1_ckb%d" % j, [128, 128], BF16) for j in range(2)]
        ckT = K.sb(st, "p1_ckT", [128, T], BF16)
        for tt in range(NT):
            j = tt % 2
            pj = tt % 4
            ps = pss[pj]
            for dc in range(8):
                K.mm(ps[:, 0:128], xnT[:, dc, tt * 128:(tt + 1) * 128], wbf[i][:, dc, 0:128],
                     ["p1_wbf%d" % i, "p1_xnT"], ["p1_ps%d" % pj], start=(dc == 0), stop=(dc == 7))
            K.op("dve", "tensor_copy", ["p1_ps%d" % pj], ["p1_ck%d" % j], out=ck[j][:], in_=ps[:, 0:128])
            K.op("act", "activation", ["p1_ck%d" % j], ["p1_junk", "p1_ss%d" % j], out=junk[:, 0:128], in_=ck[j][:],
                 func=AF.Square, accum_out=sss[j][:])
            K.op("act", "activation", ["p1_ss%d" % j, "eps6"], ["p1_ss%d" % j], out=sss[j][:], in_=sss[j][:], func=AF.Sqrt,
                 scale=1.0 / 128, bias=CONST["eps6"][:])
            K.op("dve", "reciprocal", ["p1_ss%d" % j], ["p1_ss%d" % j], out=sss[j][:], in_=sss[j][:])
            K.op("dve", "tensor_scalar", ["p1_ck%d" % j, "p1_ss%d" % j], ["p1_ckb%d" % j], out=ckb[j][:], in0=ck[j][:],
                 scalar1=sss[j][:], scalar2=None, op0=ALU.mult)
            K.tr(pst[:, 0, :], ckb[j][:], identb[:], ["p1_ckb%d" % j, "identb"], ["p1_pst"])
            K.op("act", "activation", ["p1_pst"], ["p1_ckT"], out=ckT[:, tt * 128:(tt + 1) * 128], in_=pst[:, 0, :],
                 func=AF.Copy)
            K.dma("sp", SC["CK"][s, tt * 128:(tt + 1) * 128, :], ckb[j][:], ["p1_ckb%d" % j], ["CK"])
        K.dma("sp", SC["CKT"][s, :, :], ckT[:], ["p1_ckT"], ["CKT"])


NIT = 14


def phase_dsa(K, s, T, Wd, SC, CONST):
    nc = K.nc
    NT = T // 128
    ZF = SC["ZF"]
    identb, identf = CONST["identb"], CONST["identf"]
    with ExitStack() as st:
        dps = [K.ps(st, "ds_ps%d" % i, [128, 512], F32) for i in range(4)]
        Ob = [K.ps(st, "ds_o%d" % i, [128, 3, 130], F32) for i in range(3)]
        MT = K.ps(st, "ds_mt", [128, 8, 128], BF16)
        wuk = K.sb(st, "ds_wuk", [128, 512], F32)
        K.dma("sp", wuk[:], Wd["w_uk"].rearrange("o r h d -> r (o h d)"), [], ["ds_wuk"])
        wukT = K.sb(st, "ds_wukT", [64, 8, 128], BF16)
        for h in range(8):
            K.tr(dps[3][0:64, 0:128], wuk[:, h * 64:(h + 1) * 64], identf[:], ["ds_wuk", "identf"], ["ds_ps3"])
            K.op("dve", "tensor_copy", ["ds_ps3"], ["ds_wukT"], out=wukT[:, h, :], in_=dps[3][0:64, 0:128])
        kvn = K.sb(st, "ds_kvn", [128, 1], F32)
        K.dma("sp", kvn[:], Wd["kv_norm"].rearrange("o r -> r o"), [], ["ds_kvn"], allow_slow_non_contiguous=True)
        kvn8 = K.sb(st, "ds_kvn8", [128, 1], F32)
        K.op("dve", "tensor_scalar", ["ds_kvn"], ["ds_kvn8"], out=kvn8[:], in0=kvn[:], scalar1=0.125, scalar2=None,
             op0=ALU.mult)
        wuv = K.sb(st, "ds_wuv", [128, 512], F32)
        K.dma("sp", wuv[:], Wd["w_uv"].rearrange("o r h d -> r (o h d)"), [], ["ds_wuv"])
        wuvb = K.sb(st, "ds_wuvb", [128, 512], BF16)
        K.op("dve", "tensor_scalar", ["ds_wuv", "ds_kvn"], ["ds_wuvb"], out=wuvb[:], in0=wuv[:], scalar1=kvn[:],
             scalar2=None, op0=ALU.mult)
        CKT = K.sb(st, "ds_ckt", [128, T], BF16)
        K.dma("sp", CKT[:], SC["CKT"][s, :, :], ["CKT"], ["ds_ckt"])
        CKA = K.sb(st, "ds_cka", [128, NT, 130], BF16)
        K.op("pool", "memset", [], ["ds_cka"], ap=CKA[:], constant=1.0)
        K.dma("sp", CKA[:, :, 0:128], SC["CK"][s, :, :].rearrange("(k p) r -> p k r", p=128), ["CK"], ["ds_cka"])
        kif = K.sb(st, "ds_kif", [64, T], F32)
        K.dma("act", kif[:], ZF[s, R_KI:R_KI + 64, :], ["ZF"], ["ds_kif"])
        kib = K.sb(st, "ds_kib", [64, T], BF16)
        K.op("dve", "tensor_copy", ["ds_kif"], ["ds_kib"], out=kib[:], in_=kif[:])
        qib = K.sb(st, "ds_qib", [64, 4, 128], BF16)
        zl = K.sb(st, "ds_zl", [128, 128], BF16)
        zb = K.sb(st, "ds_zb", [128, 390], BF16)
        K.op("pool", "memset", [], ["ds_zl"], ap=zl[:], constant=0.0)
        K.op("pool", "memset", [], ["ds_zb"], ap=zb[:], constant=0.0)
        tri01, negtri, pw = CONST["tri01"], CONST["negtri"], CONST["pw"]
        qf = K.sb(st, "ds_qf", [64, 8, 128], F32)
        qb = K.sb(st, "ds_qb", [64, 8, 128], BF16)
        qif = K.sb(st, "ds_qif", [64, 4, 128], F32)
        wif = K.sb(st, "ds_wif", [4, 128], F32)
        wit = K.sb(st, "ds_wit", [128, 4], F32)
        qlat = K.sb(st, "ds_qlat", [128, 1024], BF16)
        isc = K.sb(st, "ds_isc", [128, T], F32)
        junk = K.sb(st, "ds_junk", [128, T], F32)
        rl = [K.sb(st, "ds_rl%d" % i, [128, 512], F32) for i in range(3)]
        maskb = K.sb(st, "ds_mask", [128, T], BF16)
        col = K.sb(st, "ds_col", [128, 8], F32)
        hk = K.sb(st, "ds_hk", [128, NIT], F32)
        junk2 = K.sb(st, "ds_junk2", [128, T], BF16)
        cola = K.sb(st, "ds_cola", [128, 1], F32)
        mts = [K.sb(st, "ds_mts%d" % i, [128, 128], BF16) for i in range(2)]
        ee = [K.sb(st, "ds_e%d" % i, [128, 4, 128], BF16) for i in range(4)]
        pp = [K.sb(st, "ds_p%d" % i, [128, 4, 128], BF16) for i in range(4)]
        rd = K.sb(st, "ds_rd", [128, 8, 1], F32)
        onb = K.sb(st, "ds_onb", [128, 8, 128], BF16)
        onT = K.sb(st, "ds_onT", [128, 8, 128], BF16)
        ybs = K.sb(st, "ds_ybs", [128, 4, 128], BF16)
        maskbs = [maskb, K.sb(st, "ds_mask1", [128, T], BF16)]
        MN = ["ds_mask", "ds_mask1"]

        def select(qt):
            t0 = qt * 128
            nk = qt + 1
            nkeys = nk * 128
            mb = maskbs[qt % 2]
            mn = MN[qt % 2]
            if qt >= 2:
                K.dma("act", qif[:], ZF[s, R_QI:R_QI + 256, t0:t0 + 128].rearrange("(h p) t -> p h t", p=64), ["ZF"], ["ds_qif"])
                K.op("act", "activation", ["ds_qif"], ["ds_qib"], out=qib[:], in_=qif[:], func=AF.Copy)
                K.dma("act", wif[:], ZF[s, R_WI:R_WI + 4, t0:t0 + 128], ["ZF"], ["ds_wif"])
                K.tr(dps[3][:, 0:4], wif[:], identf[0:4, 0:4], ["ds_wif", "identf"], ["ds_ps3"])
                K.op("dve", "tensor_scalar", ["ds_ps3"], ["ds_wit"], out=wit[:], in0=dps[3][:, 0:4], scalar1=1.0 / 16,
                     scalar2=None, op0=ALU.mult)
                yield
                for kb in range((nkeys + 511) // 512):
                    w = min(512, nkeys - kb * 512)
                    ks = slice(kb * 512, kb * 512 + w)
                    for h in range(4):
                        pb = 2 + (h % 2)
                        K.mm(dps[pb][:, 0:w], qib[:, h, :], kib[:, ks], ["ds_qib", "ds_kib"], ["ds_ps%d" % pb])
                        if h == 0:
                            K.op("dve", "tensor_scalar", ["ds_ps%d" % pb, "ds_wit"], ["ds_isc"], out=isc[:, ks], in0=dps[pb][:, 0:w],
                                 scalar1=0.0, scalar2=wit[:, 0:1], op0=ALU.max, op1=ALU.mult)
                        else:
                            K.op("act", "activation", ["ds_ps%d" % pb], ["ds_rl%d" % (h - 1)], out=rl[h - 1][:, 0:w],
                                 in_=dps[pb][:, 0:w], func=AF.Relu)
                            K.op("dve", "scalar_tensor_tensor", ["ds_rl%d" % (h - 1), "ds_wit", "ds_isc"], ["ds_isc"],
                                 out=isc[:, ks], in0=rl[h - 1][:, 0:w], scalar=wit[:, h:h + 1], in1=isc[:, ks],
                                 op0=ALU.mult, op1=ALU.add)
                    yield
                K.op("dve", "tensor_reduce", ["ds_isc"], ["ds_col"], out=col[:, 0:1], in_=isc[:, 0:nkeys], axis=AX.X, op=ALU.max)
                K.op("dve", "tensor_reduce", ["ds_isc"], ["ds_col"], out=col[:, 1:2], in_=isc[:, 0:nkeys], axis=AX.X, op=ALU.min)
                K.op("dve", "tensor_scalar", ["ds_col"], ["ds_col"], out=col[:, 2:3], in0=col[:, 0:1], scalar1=col[:, 1:2],
                     scalar2=2e-6, op0=ALU.subtract, op1=ALU.add)
                K.op("dve", "tensor_scalar", ["ds_col"], ["ds_col"], out=col[:, 3:4], in0=col[:, 1:2], scalar1=-1e-6,
                     scalar2=None, op0=ALU.add)
                K.op("dve", "tensor_scalar", ["pw", "ds_col"], ["ds_hk"], out=hk[:], in0=pw[:], scalar1=col[:, 2:3],
                     scalar2=None, op0=ALU.mult)
                K.op("dve", "tensor_tensor", ["ds_isc", "negtri"], ["ds_isc"], out=isc[:, t0:t0 + 128], in0=isc[:, t0:t0 + 128],
                     in1=negtri[:], op=ALU.add)
                K.op("dve", "tensor_tensor", ["ds_col", "ds_hk"], ["ds_col", "ds_colm"], out=col[:, 4:5], in0=col[:, 3:4], in1=hk[:, 0:1], op=ALU.add)
                yield
                nd = nkeys
                if nkeys >= 1024 and not os.environ.get("NO_ACTCNT"):
                    nd = ((nkeys * 5 // 8) // 128) * 128
                na = nkeys - nd
                for k in range(NIT):
                    K.op("dve", "tensor_scalar", ["ds_isc", "ds_colm"], ["ds_junk", "ds_col"], out=junk[:, 0:nd],
                         in0=isc[:, 0:nd], scalar1=col[:, 4:5], scalar2=None, op0=ALU.is_ge, op1=ALU.add,
                         accum_out=col[:, 5:6])
                    if na > 0:
                        K.op("act", "activation", ["ds_isc", "ds_colm"], ["ds_junk2", "ds_cola"], out=junk2[:, 0:na], in_=isc[:, nd:nkeys],
                             func=AF.Sign, scale=-1.0, bias=col[:, 4:5], accum_out=cola[:, 0:1])
                        K.op("dve", "scalar_tensor_tensor", ["ds_cola", "ds_col"], ["ds_col"], out=col[:, 5:6], in0=cola[:, 0:1], scalar=-0.5,
                             in1=col[:, 5:6], op0=ALU.mult, op1=ALU.add)
                    K.op("dve", "tensor_scalar", ["ds_col", "ds_hk"], ["ds_col"], out=col[:, 6:7], in0=col[:, 5:6],
                         scalar1=255.5 - 0.5 * na, scalar2=hk[:, k:k + 1], op0=ALU.is_ge, op1=ALU.mult)
                    kn = min(k + 1, NIT - 1)
                    dst = col[:, 4:5] if k < NIT - 1 else col[:, 3:4]
                    K.op("dve", "scalar_tensor_tensor", ["ds_col", "ds_colm", "ds_hk"], ["ds_col", "ds_colm"], out=dst, in0=col[:, 6:7], scalar=col[:, 4:5],
                         in1=hk[:, kn:kn + 1], op0=ALU.add, op1=ALU.subtract)
                    yield
                K.op("dve", "tensor_scalar", ["ds_isc", "ds_col"], [mn], out=mb[:, 0:nkeys], in0=isc[:, 0:nkeys],
                     scalar1=col[:, 3:4], scalar2=None, op0=ALU.is_ge)
            else:
                if qt > 0:
                    K.op("pool", "memset", [], [mn], ap=mb[:, 0:t0], constant=1.0)
                K.op("pool", "tensor_copy", ["tri01"], [mn], out=mb[:, t0:t0 + 128], in_=tri01[:])
            yield

        def attend(qt):
            t0 = qt * 128
            nk = qt + 1
            mb = maskbs[qt % 2]
            mn = MN[qt % 2]
            K.dma("sp", qf[:], ZF[s, R_Q:R_Q + 512, t0:t0 + 128].rearrange("(h p) t -> p h t", p=64), ["ZF"], ["ds_qf"])
            K.op("pool", "tensor_copy", ["ds_qf"], ["ds_qb"], out=qb[:], in_=qf[:])
            for h in range(8):
                K.mm(dps[h // 4][:, (h % 4) * 128:(h % 4 + 1) * 128], wukT[:, h, :], qb[:, h, :], ["ds_wukT", "ds_qb"],
                     ["ds_ps%d" % (h // 4)])
            for j in range(2):
                K.op("act", "activation", ["ds_ps%d" % j, "ds_kvn8"], ["ds_qlat"], out=qlat[:, j * 512:(j + 1) * 512],
                     in_=dps[j][:], func=AF.Copy, scale=kvn8[:, 0:1])
            for bq in range(3):
                K.mm(Ob[bq][:].rearrange("p a b -> p (a b)"), zl[:], zb[:], ["ds_zl", "ds_zb"], ["ds_o%d" % bq], start=True,
                     stop=False, skip_group_check=True)
            yield
            def front(kt):
                par = kt % 2
                K.tr(MT[:, 0, :], mb[:, kt * 128:(kt + 1) * 128], identb[:], [mn, "identb"], ["ds_mt0", "ds_mt1", "ds_mtall"])
                K.op("act", "activation", ["ds_mt0", "ds_mt1", "ds_mtall"], ["ds_mts%d" % par], out=mts[par][:], in_=MT[:, 0, :], func=AF.Copy)
                for j in range(2):
                    ej = 2 * par + j
                    K.mm(dps[j][:], CKT[:, kt * 128:(kt + 1) * 128], qlat[:, j * 512:(j + 1) * 512], ["ds_ckt", "ds_qlat"],
                         ["ds_ps%d" % j])
                    K.op("act", "activation", ["ds_ps%d" % j], ["ds_e%d" % ej], out=ee[ej][:],
                         in_=dps[j][:].rearrange("p (a b) -> p a b", a=4), func=AF.Exp)
            front(0)
            for kt in range(nk):
                par = kt % 2
                mtb = "ds_mt%d" % par
                if kt + 1 < nk:
                    front(kt + 1)
                for j in range(2):
                    ej = 2 * par + j
                    K.op("dve", "tensor_tensor", ["ds_e%d" % ej, "ds_mts%d" % par], ["ds_p%d" % ej], out=pp[ej][:], in0=ee[ej][:],
                         in1=mts[par][:].unsqueeze(1).to_broadcast([128, 4, 128]), op=ALU.mult)
                for j in range(2):
                    ej = 2 * par + j
                    for hh in range(4):
                        h = 4 * j + hh
                        K.mm(Ob[h // 3][:, h % 3, 0:129], pp[ej][:, hh, :], CKA[:, kt, 0:129], ["ds_p%d" % ej, "ds_cka"],
                             ["ds_o%d" % (h // 3)], start=False, stop=(kt == nk - 1), skip_group_check=True)
                yield
            for bq in range(3):
                nh = 3 if bq < 2 else 2
                K.op("dve", "reciprocal", ["ds_o%d" % bq], ["ds_rd"], out=rd[:, 3 * bq:3 * bq + nh, :], in_=Ob[bq][:, 0:nh, 128:129])
                K.op("dve", "tensor_tensor", ["ds_o%d" % bq, "ds_rd"], ["ds_onb"], out=onb[:, 3 * bq:3 * bq + nh, :],
                     in0=Ob[bq][:, 0:nh, 0:128], in1=rd[:, 3 * bq:3 * bq + nh, :].to_broadcast([128, nh, 128]), op=ALU.mult)
            for h in range(8):
                K.tr(MT[:, h, :], onb[:, h, :], identb[:], ["ds_onb", "identb"], ["ds_mt0", "ds_mt1", "ds_mtall"])
            K.op("act", "activation", ["ds_mt0", "ds_mt1", "ds_mtall"], ["ds_onT"], out=onT[:], in_=MT[:], func=AF.Copy)
            for h in range(8):
                K.mm(dps[0][(h % 2) * 64:(h % 2 + 1) * 64, (h // 2) * 128:(h // 2 + 1) * 128], wuvb[:, h * 64:(h + 1) * 64],
                     onT[:, h, :], ["ds_wuvb", "ds_onT"], ["ds_ps0"])
            K.op("dve", "tensor_copy", ["ds_ps0"], ["ds_ybs"], out=ybs[:], in_=dps[0][:].rearrange("p (a b) -> p a b", a=4))
            K.dma("sp", SC["YB"][s, :, t0:t0 + 128].rearrange("(c p) t -> p c t", p=128), ybs[:], ["ds_ybs"], ["YB"])
            yield

        for step in range(NT + 1):
            gens = []
            if step >= 1:
                gens.append(attend(step - 1))
            if step < NT:
                gens.append(select(step))
            while gens:
                for g in list(gens):
                    try:
                        next(g)
                    except StopIteration:
                        gens.remove(g)


class _Stop(Exception):
    pass


def phase_rwkv(K, s, T, Wd, SC, CONST):
    _phase_rwkv(K, s, T, Wd, SC, CONST)
    K.S.muted = False


def _phase_rwkv(K, s, T, Wd, SC, CONST):
    nc = K.nc
    TBK = 256
    NCH = TBK // 64
    NBK = T // TBK
    ZF = SC["ZF"]
    identf = CONST["identf"]
    bo, bo64, maskq, lowm, resetm = CONST["bo"], CONST["bo64"], CONST["maskq"], CONST["lowm"], CONST["resetm"]
    with ExitStack() as st:
        rp = [K.ps(st, "rk_p%d" % i, [128, 512], F32) for i in range(8)]
        RP = ["rk_p%d" % i for i in range(8)]

        def colload(tag, ap512, n=4):
            t = K.sb(st, tag, [128, n], F32)
            K.dma("sp", t[:], ap512.rearrange("o (c p) -> p (o c)", p=128), [], [tag], allow_slow_non_contiguous=True)
            return t
        mu = colload("rk_mu", Wd["shift_mu"], 14)
        w0c = colload("rk_w0c", Wd["rw_w0"])
        a0c = colload("rk_a0c", Wd["rw_a0"])
        kkc = colload("rk_kkc", Wd["rw_k_k"])
        kac = colload("rk_kac", Wd["rw_k_a"])
        rkc = colload("rk_rkc", Wd["rw_r_k"].rearrange("o h d -> o (h d)"))
        lnw = colload("rk_lnw", Wd["rw_ln_w"])
        lnb = colload("rk_lnb", Wd["rw_ln_b"])
        w2a2 = K.sb(st, "rk_w2a2", [128, 512], F32)
        K.dma("sp", w2a2[0:64, :], Wd["rw_w2"][0], [], ["rk_w2a2"])
        K.dma("sp", w2a2[64:128, :], Wd["rw_a2"][0], [], ["rk_w2a2"])
        g2 = K.sb(st, "rk_g2", [128, 512], F32)
        K.dma("sp", g2[:], Wd["rw_g2"][0], [], ["rk_g2"])
        epsg = K.sb(st, "rk_epsg", [128, 1], F32)
        K.op("dve", "memset", [], ["rk_epsg"], ap=epsg[:], constant=64e-5)
        zin = K.sb(st, "rk_zin", [128, 14, TBK + 1], F32)
        zs = K.sb(st, "rk_zs", [128, 14, TBK], F32)
        tw = K.sb(st, "rk_tw", [128, TBK], F32)
        sg = K.sb(st, "rk_sg", [128, TBK], F32)

        def t4(tag):
            return K.sb(st, tag, [128, 4, TBK], F32)
        lw, aa, gg, LL, eL, enL, eLm, kk, t1, kp, bb, bon, Yb = [t4("rk_" + n) for n in
            ("lw", "aa", "gg", "LL", "eL", "enL", "eLm", "kk", "t1", "kp", "bb", "bon", "Yb")]
        QR = K.sb(st, "rk_QR", [128, 4, NCH, 2, 64], F32)
        KB = K.sb(st, "rk_KB", [128, 4, NCH, 2, 64], F32)
        gC = K.sb(st, "rk_gC", [128, 4, NCH], F32)
        M = K.sb(st, "rk_M", [128, 4, 64], F32)
        K.op("dve", "memset", [], ["rk_M"], ap=M[:], constant=0.0)
        KBTs = [K.sb(st, "rk_KBT%d" % i, [128, 4, 128], F32) for i in range(2)]
        VTs = [K.sb(st, "rk_VT%d" % i, [64, 4, 128], F32) for i in range(2)]
        ATs = [K.sb(st, "rk_AT%d" % i, [128, 8, 128], F32) for i in range(2)]
        DDT = BF16 if os.environ.get("RW_BF16", "1") == "1" else F32
        Am = [K.sb(st, "rk_Am%d" % i, [128, 8, 64], DDT) for i in range(2)]
        Bm = [K.sb(st, "rk_Bm%d" % i, [128, 8, 64], DDT) for i in range(2)]
        Pm = [K.sb(st, "rk_Pm%d" % i, [128, 8, 64], DDT) for i in range(2)]
        PmFs = [K.sb(st, "rk_PmF%d" % i, [128, 8, 64], F32) for i in range(2)]
        Rs = K.sb(st, "rk_Rs", [128, 512], F32)
        Us = K.sb(st, "rk_Us", [128, 512], F32)
        yab = K.sb(st, "rk_yab", [128, 4, TBK], BF16)
        H = slice(64, 128)

        def v4(t):
            return t[:].rearrange("p c (n t) -> p c n t", t=64)

        def bc(colt, n=4, w=TBK):
            return colt[:].unsqueeze(2).to_broadcast([128, n, w])

        RS = float(os.environ.get("RSTOP", "99"))

        def chk(k):
            if RS <= k:
                K.S.muted = True

        for tb in range(NBK):
            t0 = tb * TBK
            if tb == 0:
                K.op("dve", "memset", [], ["rk_zin"], ap=zin[:, :, 0:1], constant=0.0)
                K.dma("sp", zin[:, :, 1:TBK + 1], ZF[s, 0:1792, 0:TBK].rearrange("(c p) t -> p c t", p=128), ["ZF"], ["rk_zin"])
            else:
                K.dma("sp", zin[:, :, :], ZF[s, 0:1792, t0 - 1:t0 + TBK].rearrange("(c p) t -> p c t", p=128), ["ZF"], ["rk_zin"])
            K.op("dve", "tensor_tensor", ["rk_zin"], ["rk_zs"], out=zs[:], in0=zin[:, :, 0:TBK], in1=zin[:, :, 1:TBK + 1], op=ALU.subtract)
            for c14 in range(14):
                K.op("dve", "scalar_tensor_tensor", ["rk_zs", "rk_mu", "rk_zin"], ["rk_zs"], out=zs[:, c14, :], in0=zs[:, c14, :], scalar=mu[:, c14:c14 + 1],
                     in1=zin[:, c14, 1:TBK + 1], op0=ALU.mult, op1=ALU.add)
            chk(1)
            r_, k_, v_ = zs[:, 0:4, :], zs[:, 4:8, :], zs[:, 8:12, :]
            K.op("act", "activation", ["rk_zs"], ["rk_tw"], out=tw[0:64, :], in_=zs[0:64, 12, :], func=AF.Tanh)
            K.op("act", "activation", ["rk_zs"], ["rk_sg"], out=sg[:], in_=zs[:, 13, :], func=AF.Sigmoid)
            for cc in range(4):
                cs = slice(cc * 128, (cc + 1) * 128)
                K.mm(rp[0][:, 0:TBK], w2a2[0:64, cs], tw[0:64, :], ["rk_w2a2", "rk_tw"], [RP[0]])
                K.op("act", "activation", [RP[0], "rk_w0c"], ["rk_lw"], out=lw[:, cc, :], in_=rp[0][:, 0:TBK], func=AF.Sigmoid, bias=w0c[:, cc:cc + 1])
                K.mm(rp[1][:, 0:TBK], w2a2[H, cs], zs[H, 12, :], ["rk_w2a2", "rk_zs"], [RP[1]])
                K.op("act", "activation", [RP[1], "rk_a0c"], ["rk_aa"], out=aa[:, cc, :], in_=rp[1][:, 0:TBK], func=AF.Sigmoid, bias=a0c[:, cc:cc + 1])
                K.mm(rp[2][:, 0:TBK], g2[:, cs], sg[:], ["rk_g2", "rk_sg"], [RP[2]])
                K.op("dve", "tensor_copy", [RP[2]], ["rk_gg"], out=gg[:, cc, :], in_=rp[2][:, 0:TBK])
            chk(2)
            K.op("dve", "tensor_scalar", ["rk_lw"], ["rk_lw"], out=lw[:], in0=lw[:], scalar1=-0.6065306597126334, scalar2=None, op0=ALU.mult)
            for cc in range(4):
                K.op("dve", "tensor_tensor_scan", ["rk_lw", "resetm"], ["rk_LL"], out=LL[:, cc, :], data0=resetm[:], data1=lw[:, cc, :],
                     initial=0.0, op0=ALU.mult, op1=ALU.add)
            K.op("act", "activation", ["rk_LL"], ["rk_eL"], out=eL[:], in_=LL[:], func=AF.Exp)
            K.op("act", "activation", ["rk_LL"], ["rk_enL"], out=enL[:], in_=LL[:], func=AF.Exp, scale=-1.0)
            K.op("pool", "tensor_tensor", ["rk_LL", "rk_lw"], ["rk_t1"], out=t1[:], in0=LL[:], in1=lw[:], op=ALU.subtract)
            K.op("act", "activation", ["rk_t1"], ["rk_eLm"], out=eLm[:], in_=t1[:], func=AF.Exp)
            K.op("dve", "tensor_tensor", ["rk_zs", "rk_kkc"], ["rk_kk"], out=kk[:], in0=k_, in1=bc(kkc), op=ALU.mult)
            K.op("pool", "tensor_tensor", ["rk_kk"], ["rk_t1"], out=t1[:], in0=kk[:], in1=kk[:], op=ALU.mult)
            for cc in range(4):
                K.mm(rp[cc % 4][:, 0:TBK], bo[:], t1[:, cc, :], ["bo", "rk_t1"], [RP[cc % 4]])
                K.op("act", "activation", [RP[cc % 4]], ["rk_kp"], out=kp[:, cc, :], in_=rp[cc % 4][:, 0:TBK], func=AF.Sqrt)
            K.op("dve", "tensor_scalar", ["rk_kp"], ["rk_kp"], out=kp[:], in0=kp[:], scalar1=1e-12, scalar2=None, op0=ALU.max)
            K.op("dve", "reciprocal", ["rk_kp"], ["rk_kp"], out=kp[:], in_=kp[:])
            K.op("dve", "tensor_tensor", ["rk_kk", "rk_kp"], ["rk_kk"], out=kk[:], in0=kk[:], in1=kp[:], op=ALU.mult)
            for cc in range(4):
                K.op("dve", "tensor_scalar", ["rk_aa", "rk_kac"], ["rk_t1"], out=t1[:, cc, :], in0=aa[:, cc, :], scalar1=-1.0, scalar2=kac[:, cc:cc + 1],
                     op0=ALU.add, op1=ALU.mult)
            K.op("dve", "scalar_tensor_tensor", ["rk_t1", "rk_zs"], ["rk_kp"], out=kp[:], in0=t1[:], scalar=1.0, in1=k_, op0=ALU.add, op1=ALU.mult)
            K.op("pool", "tensor_tensor", ["rk_kk", "rk_aa"], ["rk_bb"], out=bb[:], in0=kk[:], in1=aa[:], op=ALU.mult)
            K.op("dve", "tensor_tensor", ["rk_zs", "rk_eL"], ["rk_QR"], out=QR[:, :, :, 1, :], in0=r_.rearrange("p c (n t) -> p c n t", t=64), in1=v4(eL), op=ALU.mult)
            K.op("pool", "tensor_tensor", ["rk_kk", "rk_eLm"], ["rk_QR"], out=QR[:, :, :, 0, :], in0=v4(kk), in1=v4(eLm), op=ALU.mult)
            K.op("dve", "tensor_tensor", ["rk_kp", "rk_enL"], ["rk_KB"], out=KB[:, :, :, 0, :], in0=v4(kp), in1=v4(enL), op=ALU.mult)
            K.op("pool", "tensor_tensor", ["rk_bb", "rk_enL"], ["rk_KB"], out=KB[:, :, :, 1, :], in0=v4(bb), in1=v4(enL), op=ALU.mult)
            K.op("dve", "tensor_copy", ["rk_eL"], ["rk_gC"], out=gC[:], in_=v4(eL)[:, :, :, 63])
            K.op("pool", "tensor_tensor", ["rk_zs", "rk_kp"], ["rk_t1"], out=t1[:], in0=r_, in1=kp[:], op=ALU.mult)
            K.op("pool", "tensor_tensor", ["rk_t1", "rk_rkc"], ["rk_t1"], out=t1[:], in0=t1[:], in1=bc(rkc), op=ALU.mult)
            for cc in range(4):
                K.mm(rp[cc % 4][:, 0:TBK], bo[:], t1[:, cc, :], ["bo", "rk_t1"], [RP[cc % 4]])
                K.op("dve", "tensor_tensor", [RP[cc % 4], "rk_zs"], ["rk_bon"], out=bon[:, cc, :], in0=rp[cc % 4][:, 0:TBK], in1=zs[:, 8 + cc, :], op=ALU.mult)
            chk(3)
            def ev(t, par):
                return t.rearrange("p (a two) b -> p a two b", two=2)[:, :, par, :]

            def pre(c):
                q = c % 2
                KBT, VT, AT, PmF = KBTs[q], VTs[q], ATs[q], PmFs[q]
                nKBT, nVT, nAT, nPmF = "rk_KBT%d" % q, "rk_VT%d" % q, "rk_AT%d" % q, "rk_PmF%d" % q
                for cc in range(4):
                    K.tr(rp[0][:, cc * 128:(cc + 1) * 128], KB[:, cc, c, :, :].rearrange("p a b -> p (a b)"), identf[:], ["rk_KB", "identf"], [RP[0]])
                    K.tr(rp[1][0:64, cc * 128:(cc + 1) * 128], zs[:, 8 + cc, c * 64:(c + 1) * 64], identf[:], ["rk_zs", "identf"], [RP[1]])
                K.op("act", "activation", [RP[0]], [nKBT], out=KBT[:].rearrange("p a b -> p (a b)"), in_=rp[0][:], func=AF.Copy)
                K.op("dve", "tensor_copy", [RP[1]], [nVT], out=VT[:].rearrange("p a b -> p (a b)"), in_=rp[1][0:64, :])
                yield
                for h in range(8):
                    cc, h2 = h // 2, h % 2
                    rows = slice(h2 * 64, (h2 + 1) * 64)
                    K.mm(rp[2 + h2][:, cc * 128:(cc + 1) * 128], KB[rows, cc, c, :, :].rearrange("p a b -> p (a b)"),
                         QR[rows, cc, c, :, :].rearrange("p a b -> p (a b)"), ["rk_KB", "rk_QR"], [RP[2 + h2]])
                    K.mm(rp[h2][H, cc * 64:(cc + 1) * 64], QR[rows, cc, c, 0, :], KB[rows, cc, c, 1, :], ["rk_QR", "rk_KB"], [RP[h2]])
                for h2 in range(2):
                    K.op("dve", "tensor_tensor", [RP[2 + h2], "maskq"], [nAT], out=ev(AT[:], h2),
                         in0=rp[2 + h2][:].rearrange("p (a b) -> p a b", a=4), in1=maskq[:].unsqueeze(1).to_broadcast([128, 4, 128]), op=ALU.mult)
                    K.op("dve", "tensor_tensor", [RP[h2], "lowm"], ["rk_Bm0"], out=ev(Bm[0][H, :, :], h2),
                         in0=rp[h2][H, 0:256].rearrange("p (a b) -> p a b", a=4), in1=lowm[H, :].unsqueeze(1).to_broadcast([64, 4, 64]), op=ALU.mult)
                K.op("act", "activation", [nAT], ["rk_Am0"], out=Am[0][H, :, :], in_=AT[H, :, 0:64], func=AF.Copy)
                K.op("dve", "tensor_tensor", ["identf", nAT], ["rk_Pm0"], out=Pm[0][H, :, :],
                     in0=identf[H, 64:128].unsqueeze(1).to_broadcast([64, 8, 64]), in1=AT[H, :, 0:64], op=ALU.subtract)
                yield
                for lvl in range(5):
                    ci, ni = lvl % 2, (lvl + 1) % 2
                    An, Bn, Pn = "rk_Am%d" % ni, "rk_Bm%d" % ni, "rk_Pm%d" % ni
                    Ac, Bc, Pc = "rk_Am%d" % ci, "rk_Bm%d" % ci, "rk_Pm%d" % ci
                    for h in range(8):
                        hs = slice(h * 64, (h + 1) * 64)
                        if lvl < 4:
                            K.mm(rp[2][H, hs], Bm[ci][H, h, :], Am[ci][H, h, :], [Ac, Bc], [RP[2]])
                        K.mm(rp[3][H, hs], Am[ci][H, h, :], Bm[ci][H, h, :], [Ac, Bc], [RP[3]])
                    if lvl < 4:
                        K.op("act", "activation", [RP[2]], [An], out=Am[ni][H, :, :], in_=rp[2][H, :].rearrange("p (a b) -> p a b", a=8), func=AF.Copy)
                    K.op("dve", "tensor_copy", [RP[3]], [Bn], out=Bm[ni][H, :, :], in_=rp[3][H, :].rearrange("p (a b) -> p a b", a=8))
                    yield
                    for h in range(8):
                        hs = slice(h * 64, (h + 1) * 64)
                        K.mm(rp[0][H, hs], Bm[ni][H, h, :], Pm[ci][H, h, :], [Bn, Pc], [RP[0]])
                    if lvl < 4:
                        K.op("dve", "tensor_tensor", [RP[0], Pc], [Pn], out=Pm[ni][H, :, :], in0=rp[0][H, :].rearrange("p (a b) -> p a b", a=8),
                             in1=Pm[ci][H, :, :], op=ALU.add)
                    else:
                        K.op("dve", "tensor_tensor", [RP[0], Pc], [nPmF], out=PmF[H, :, :], in0=rp[0][H, :].rearrange("p (a b) -> p a b", a=8),
                             in1=Pm[ci][H, :, :], op=ALU.add)
                    yield

            def post(c):
                q = c % 2
                KBT, VT, AT, PF = KBTs[q], VTs[q], ATs[q], PmFs[q]
                nKBT, nVT, nAT, PFn = "rk_KBT%d" % q, "rk_VT%d" % q, "rk_AT%d" % q, "rk_PmF%d" % q
                Rs3 = Rs[H, :].rearrange("p (a b) -> p a b", a=8)
                for h in range(8):
                    cc, h2 = h // 2, h % 2
                    rows = slice(h2 * 64, (h2 + 1) * 64)
                    hs = slice(h * 64, (h + 1) * 64)
                    K.mm(rp[6 + h2][H, cc * 64:(cc + 1) * 64], QR[rows, cc, c, 0, :], M[rows, cc, :], ["rk_QR", "rk_M"], [RP[6 + h2]])
                    K.mm(rp[4][H, hs], AT[0:64, h, 0:64], VT[0:64, cc, h2 * 64:(h2 + 1) * 64], [nAT, nVT], [RP[4]])
                for h2 in range(2):
                    K.op("act", "activation", [RP[6 + h2]], ["rk_Rs"], out=ev(Rs3, h2), in_=rp[6 + h2][H, 0:256].rearrange("p (a b) -> p a b", a=4), func=AF.Copy)
                K.op("dve", "tensor_tensor", [RP[4], "rk_Rs"], ["rk_Rs"], out=Rs[H, :], in0=rp[4][H, :], in1=Rs[H, :], op=ALU.add)
                yield
                for h in range(8):
                    hs = slice(h * 64, (h + 1) * 64)
                    K.mm(rp[5][H, hs], PF[H, h, :], Rs[H, hs], [PFn, "rk_Rs"], [RP[5]])
                K.op("act", "activation", [RP[5]], ["rk_Us"], out=Us[H, :], in_=rp[5][H, :], func=AF.Copy, scale=-1.0)
                yield
                for h in range(8):
                    cc, h2 = h // 2, h % 2
                    rows = slice(h2 * 64, (h2 + 1) * 64)
                    hs = slice(h * 64, (h + 1) * 64)
                    ys = slice(cc * 64, (cc + 1) * 64)
                    K.mm(rp[6 + h2][rows, ys], M[rows, cc, :], QR[rows, cc, c, 1, :], ["rk_M", "rk_QR"], [RP[6 + h2]])
                    K.mm(rp[4][rows, ys], VT[0:64, cc, h2 * 64:(h2 + 1) * 64], AT[0:64, h, 64:128], [nVT, nAT], [RP[4]])
                    K.mm(rp[5][rows, ys], Us[H, hs], AT[H, h, 64:128], ["rk_Us", nAT], [RP[5]])
                for h2 in range(2):
                    rows = slice(h2 * 64, (h2 + 1) * 64)
                    K.op("act", "activation", [RP[6 + h2]], ["rk_Yb"], out=Yb[rows, :, c * 64:(c + 1) * 64],
                         in_=rp[6 + h2][rows, 0:256].rearrange("p (a b) -> p a b", a=4), func=AF.Copy)
                yv = Yb[:, :, c * 64:(c + 1) * 64]
                K.op("dve", "tensor_tensor", [RP[4], "rk_Yb"], ["rk_Yb"], out=yv, in0=rp[4][:, 0:256].rearrange("p (a b) -> p a b", a=4), in1=yv, op=ALU.add)
                K.op("dve", "tensor_tensor", [RP[5], "rk_Yb"], ["rk_Yb"], out=yv, in0=rp[5][:, 0:256].rearrange("p (a b) -> p a b", a=4), in1=yv, op=ALU.add)
                yield
                for cc in range(4):
                    for h2 in range(2):
                        h = 2 * cc + h2
                        rows = slice(h2 * 64, (h2 + 1) * 64)
                        hs = slice(h * 64, (h + 1) * 64)
                        K.mm(rp[4][rows, cc * 64:(cc + 1) * 64], KBT[0:64, cc, rows], VT[0:64, cc, rows], [nKBT, nVT], [RP[4]])
                        K.mm(rp[5][rows, cc * 64:(cc + 1) * 64], KBT[H, cc, rows], Us[H, hs], [nKBT, "rk_Us"], [RP[5]])
                K.op("dve", "tensor_tensor", [RP[4], "rk_M"], ["rk_M"], out=M[:], in0=rp[4][:, 0:256].rearrange("p (a b) -> p a b", a=4), in1=M[:], op=ALU.add)
                K.op("dve", "tensor_tensor", [RP[5], "rk_M"], ["rk_M"], out=M[:], in0=rp[5][:, 0:256].rearrange("p (a b) -> p a b", a=4), in1=M[:], op=ALU.add)
                K.op("dve", "tensor_tensor", ["rk_M", "rk_gC"], ["rk_M"], out=M[:], in0=M[:],
                     in1=gC[:, :, c:c + 1].to_broadcast([128, 4, 64]), op=ALU.mult)
                yield

            for step in range(NCH + 1):
                gens = []
                if step >= 1:
                    gens.append(post(step - 1))
                if step < NCH:
                    gens.append(pre(step))
                while gens:
                    for g in list(gens):
                        try:
                            next(g)
                        except StopIteration:
                            gens.remove(g)
            chk(7)
            for cc in range(4):
                K.mm(rp[0][:, 0:TBK], bo64[:], Yb[:, cc, :], ["bo64", "rk_Yb"], [RP[0]])
                K.op("dve", "tensor_tensor", ["rk_Yb", RP[0]], ["rk_t1"], out=t1[:, cc, :], in0=Yb[:, cc, :], in1=rp[0][:, 0:TBK], op=ALU.subtract)
                K.op("pool", "tensor_tensor", ["rk_t1"], ["rk_kk"], out=kk[:, cc, :], in0=t1[:, cc, :], in1=t1[:, cc, :], op=ALU.mult)
                K.mm(rp[1][:, 0:TBK], bo64[:], kk[:, cc, :], ["bo64", "rk_kk"], [RP[1]])
                K.op("act", "activation", [RP[1], "rk_epsg"], ["rk_kp"], out=kp[:, cc, :], in_=rp[1][:, 0:TBK], func=AF.Sqrt, bias=epsg[:])
            K.op("dve", "reciprocal", ["rk_kp"], ["rk_kp"], out=kp[:], in_=kp[:])
            K.op("dve", "tensor_tensor", ["rk_t1", "rk_kp"], ["rk_t1"], out=t1[:], in0=t1[:], in1=kp[:], op=ALU.mult)
            K.op("pool", "tensor_tensor", ["rk_t1", "rk_lnw"], ["rk_t1"], out=t1[:], in0=t1[:], in1=bc(lnw), op=ALU.mult)
            K.op("pool", "tensor_tensor", ["rk_t1", "rk_lnb"], ["rk_t1"], out=t1[:], in0=t1[:], in1=bc(lnb), op=ALU.add)
            K.op("dve", "tensor_tensor", ["rk_t1", "rk_bon"], ["rk_t1"], out=t1[:], in0=t1[:], in1=bon[:], op=ALU.add)
            K.op("dve", "tensor_tensor", ["rk_t1", "rk_gg"], ["rk_yab"], out=yab[:], in0=t1[:], in1=gg[:], op=ALU.mult)
            K.dma("sp", SC["YA"][s, :, t0:t0 + TBK].rearrange("(c p) t -> p c t", p=128), yab[:], ["rk_yab"], ["YA"])


def norm_T(K, tag, src_tile, src_name, xn, ss, junk, pst, dstT, col0, identb, eps):
    K.op("act", "activation", [src_name], [tag + "junk", tag + "ss"], out=junk[:], in_=src_tile, func=AF.Square, accum_out=ss[:])
    K.op("act", "activation", [tag + "ss", "eps6"], [tag + "ss"], out=ss[:], in_=ss[:], func=AF.Sqrt, scale=1.0 / D, bias=eps[:])
    K.op("dve", "reciprocal", [tag + "ss"], [tag + "ss"], out=ss[:], in_=ss[:])
    K.op("dve", "tensor_scalar", [src_name, tag + "ss"], [tag + "xn"], out=xn[:], in0=src_tile, scalar1=ss[:], scalar2=None, op0=ALU.mult)
    for c in range(8):
        K.tr(pst[:, c, :], xn[:, c * 128:(c + 1) * 128], identb[:], [tag + "xn", "identb"], [tag + "pst"])
    K.op("act", "activation", [tag + "pst"], [dstT[1]], out=dstT[0][:, :, col0:col0 + 128], in_=pst[:], func=AF.Copy)


def phase_mix(K, s, T, X, Wd, SC, CONST):
    ZF = SC["ZF"]
    with ExitStack() as st:
        wpa = load_cast(K, st, "mx_wpa", Wd["w_proj_a"][0], 512, 1024)
        wpb = load_cast(K, st, "mx_wpb", Wd["w_proj_b"][0], 512, 1024)
        wout = load_cast(K, st, "mx_wout", Wd["w_out"][0], 1024, 1024)
        ps = [K.ps(st, "mx_ps%d" % i, [128, 512], F32) for i in range(4)]
        ya = K.sb(st, "mx_ya", [128, 4, 512], BF16)
        yb = K.sb(st, "mx_yb", [128, 4, 512], BF16)
        G = K.sb(st, "mx_G", [128, 16, 512], F32)
        ta = K.sb(st, "mx_ta", [128, 512], F32)
        tb_ = K.sb(st, "mx_tb", [128, 512], F32)
        mixT = K.sb(st, "mx_mixT", [128, 8, 512], BF16)
        xt = [K.sb(st, "mx_xt%d" % i, [128, D], F32) for i in range(2)]
        for tb in range(T // 512):
            t0 = tb * 512
            K.dma("sp", ya[:], SC["YA"][s, :, t0:t0 + 512].rearrange("(c p) t -> p c t", p=128), ["YA"], ["mx_ya"])
            K.dma("act", yb[:], SC["YB"][s, :, t0:t0 + 512].rearrange("(c p) t -> p c t", p=128), ["YB"], ["mx_yb"])
            K.dma("sp", G[:], ZF[s, R_G:R_G + 2048, t0:t0 + 512].rearrange("(c p) t -> p c t", p=128), ["ZF"], ["mx_G"])
            for cc in range(8):
                cs = slice(cc * 128, (cc + 1) * 128)
                for k in range(4):
                    K.mm(ps[0][:], wpa[:, k, cs], ya[:, k, :], ["mx_wpa", "mx_ya"], ["mx_ps0"], start=(k == 0), stop=(k == 3))
                for k in range(4):
                    K.mm(ps[1][:], wpb[:, k, cs], yb[:, k, :], ["mx_wpb", "mx_yb"], ["mx_ps1"], start=(k == 0), stop=(k == 3))
                K.op("dve", "tensor_tensor", ["mx_ps0", "mx_G"], ["mx_ta"], out=ta[:], in0=ps[0][:], in1=G[:, cc, :], op=ALU.mult)
                K.op("dve", "tensor_tensor", ["mx_ps1", "mx_G"], ["mx_tb"], out=tb_[:], in0=ps[1][:], in1=G[:, 8 + cc, :], op=ALU.mult)
                K.op("pool", "tensor_tensor", ["mx_ta", "mx_tb"], ["mx_mixT"], out=mixT[:, cc, :], in0=ta[:], in1=tb_[:], op=ALU.add)
            for tt in range(4):
                i = tt % 2
                r0 = s * T + t0 + tt * 128
                K.dma("act", xt[i][:], X[r0:r0 + 128, :], [], ["mx_xt%d" % i])
                for half in range(2):
                    pj = 2 + half
                    for k in range(8):
                        K.mm(ps[pj][:], mixT[:, k, tt * 128:(tt + 1) * 128], wout[:, k, half * 512:(half + 1) * 512], ["mx_mixT", "mx_wout"],
                             ["mx_ps%d" % pj], start=(k == 0), stop=(k == 7))
                    K.op("dve", "tensor_tensor", ["mx_ps%d" % pj, "mx_xt%d" % i], ["mx_xt%d" % i], out=xt[i][:, half * 512:(half + 1) * 512],
                         in0=ps[pj][:], in1=xt[i][:, half * 512:(half + 1) * 512], op=ALU.add)
                K.dma("sp", SC["H1"][r0:r0 + 128, :], xt[i][:], ["mx_xt%d" % i], ["H1"])


def colvec(K, st, tag, ap, n=8):
    t = K.sb(st, tag, [128, n], F32)
    K.dma("sp", t[:], ap.rearrange("o (c p) -> p (o c)", p=128), [], [tag], allow_slow_non_contiguous=True)
    return t


def phase_cross(K, s, T, MEM, Wd, SC, CONST):
    identb = CONST["identb"]
    with ExitStack() as st:
        nrc = colvec(K, st, "cx_nrc", Wd["norm_cross"])
        nrm = colvec(K, st, "cx_nrm", Wd["norm_mem"])
        wcq = load_cast(K, st, "cx_wcq", Wd["w_cq"][0], 1024, 1024, scale_col=(nrc, "cx_nrc"))
        wckv = load_cast(K, st, "cx_wckv", Wd["w_ckv"][0], 1024, 2048, scale_col=(nrm, "cx_nrm"))
        wco = load_cast(K, st, "cx_wco", Wd["w_co"][0], 1024, 1024)
        ps = [K.ps(st, "cx_ps%d" % i, [128, 512], F32) for i in range(6)]
        pst = K.ps(st, "cx_pst", [128, 8, 128], BF16)
        ht = K.sb(st, "cx_ht", [128, 4, D], F32)
        xn = K.sb(st, "cx_xn", [128, D], BF16)
        ss = K.sb(st, "cx_ss", [128, 1], F32)
        junk = K.sb(st, "cx_junk", [128, D], F32)
        memT = K.sb(st, "cx_memT", [128, 8, 256], BF16)
        ones = K.sb(st, "cx_ones", [128, 128], BF16)
        K.op("pool", "memset", [], ["cx_ones"], ap=ones[:], constant=1.0)
        for mt in range(2):
            K.dma("sp", ht[:, 0, :], MEM[s * 256 + mt * 128: s * 256 + (mt + 1) * 128, :], [], ["cx_ht0"])
            norm_T(K, "cx_", ht[:, 0, :], "cx_ht0", xn, ss, junk, pst, (memT, "cx_memT"), mt * 128, identb, CONST["eps6"])
        kTs = K.sb(st, "cx_kTs", [128, 8, 256], BF16)
        vS = K.sb(st, "cx_vS", [128, 2, 1024], BF16)
        for j in range(8):
            for k in range(8):
                K.mm(ps[0][:, 0:256], wckv[:, k, j * 128:(j + 1) * 128], memT[:, k, :], ["cx_wckv", "cx_memT"], ["cx_ps0"], start=(k == 0), stop=(k == 7))
            K.op("dve", "tensor_copy", ["cx_ps0"], ["cx_kTs"], out=kTs[:, j, :], in_=ps[0][:, 0:256])
        for mt in range(2):
            for half in range(2):
                for k in range(8):
                    K.mm(ps[1][:], memT[:, k, mt * 128:(mt + 1) * 128], wckv[:, k, 1024 + half * 512:1024 + (half + 1) * 512], ["cx_wckv", "cx_memT"],
                         ["cx_ps1"], start=(k == 0), stop=(k == 7))
                K.op("dve", "tensor_copy", ["cx_ps1"], ["cx_vS"], out=vS[:, mt, half * 512:(half + 1) * 512], in_=ps[1][:])
        hnT = K.sb(st, "cx_hnT", [128, 8, 512], BF16)
        qTs = K.sb(st, "cx_qTs", [128, 8, 512], BF16)
        pT = [K.sb(st, "cx_pT%d" % i, [128, 512], BF16) for i in range(2)]
        rden = K.sb(st, "cx_rden", [128, 512], F32)
        oT = K.sb(st, "cx_oT", [128, 8, 512], BF16)
        for tb in range(T // 512):
            t0 = tb * 512
            for tt in range(4):
                r0 = s * T + t0 + tt * 128
                K.dma("sp" if tt % 2 == 0 else "act", ht[:, tt, :], SC["H1"][r0:r0 + 128, :], ["H1"], ["cx_ht%d" % tt])
                norm_T(K, "cx_", ht[:, tt, :], "cx_ht%d" % tt, xn, ss, junk, pst, (hnT, "cx_hnT"), tt * 128, identb, CONST["eps6"])
            for j in range(8):
                pj = j % 2
                for k in range(8):
                    K.mm(ps[pj][:], wcq[:, k, j * 128:(j + 1) * 128], hnT[:, k, :], ["cx_wcq", "cx_hnT"], ["cx_ps%d" % pj], start=(k == 0), stop=(k == 7))
                if pj == 0:
                    K.op("dve", "tensor_copy", ["cx_ps0"], ["cx_qTs"], out=qTs[:, j, :], in_=ps[0][:])
                else:
                    K.op("act", "activation", ["cx_ps1"], ["cx_qTs"], out=qTs[:, j, :], in_=ps[1][:], func=AF.Copy)
            for h in range(4):
                for mt in range(2):
                    for dc in range(2):
                        K.mm(ps[2 + mt][:], kTs[:, 2 * h + dc, mt * 128:(mt + 1) * 128], qTs[:, 2 * h + dc, :], ["cx_kTs", "cx_qTs"],
                             ["cx_ps%d" % (2 + mt)], start=(dc == 0), stop=(dc == 1))
                    K.op("act", "activation", ["cx_ps%d" % (2 + mt)], ["cx_pT%d" % mt], out=pT[mt][:], in_=ps[2 + mt][:], func=AF.Exp, scale=1.0 / 16)
                for mt in range(2):
                    K.mm(ps[4][:], ones[:], pT[mt][:], ["cx_ones", "cx_pT%d" % mt], ["cx_ps4"], start=(mt == 0), stop=(mt == 1))
                K.op("dve", "reciprocal", ["cx_ps4"], ["cx_rden"], out=rden[:], in_=ps[4][:])
                for dc in range(2):
                    for mt in range(2):
                        K.mm(ps[5][:], vS[:, mt, h * 256 + dc * 128:h * 256 + (dc + 1) * 128], pT[mt][:], ["cx_vS", "cx_pT%d" % mt], ["cx_ps5"],
                             start=(mt == 0), stop=(mt == 1))
                    K.op("dve", "tensor_tensor", ["cx_ps5", "cx_rden"], ["cx_oT"], out=oT[:, 2 * h + dc, :], in0=ps[5][:], in1=rden[:], op=ALU.mult)
            for tt in range(4):
                r0 = s * T + t0 + tt * 128
                for half in range(2):
                    pj = half
                    for k in range(8):
                        K.mm(ps[pj][:], oT[:, k, tt * 128:(tt + 1) * 128], wco[:, k, half * 512:(half + 1) * 512], ["cx_oT", "cx_wco"],
                             ["cx_ps%d" % pj], start=(k == 0), stop=(k == 7))
                    K.op("dve", "tensor_tensor", ["cx_ps%d" % pj, "cx_ht%d" % tt], ["cx_ht%d" % tt], out=ht[:, tt, half * 512:(half + 1) * 512],
                         in0=ps[pj][:], in1=ht[:, tt, half * 512:(half + 1) * 512], op=ALU.add)
                K.dma("sp", SC["H1"][r0:r0 + 128, :], ht[:, tt, :], ["cx_ht%d" % tt], ["H1"])


def phase_moe(K, s, T, Wd, SC, CONST, OUT):
    identb = CONST["identb"]
    HT = min(T, 1024)
    NTL = HT // 128
    with ExitStack() as st:
        nrf = colvec(K, st, "mo_nrf", Wd["norm_ffn"])
        wrf = K.sb(st, "mo_wrf", [128, 8, 36], F32)
        K.dma("sp", wrf[:, :, 0:4], Wd["w_router_g"][0].rearrange("(c p) n -> p c n", p=128), [], ["mo_wrf"])
        K.dma("sp", wrf[:, :, 4:36], Wd["w_router_e"][0].rearrange("(c p) n -> p c n", p=128), [], ["mo_wrf"])
        wr = K.sb(st, "mo_wr", [128, 8, 36], BF16)
        K.op("dve", "tensor_tensor", ["mo_wrf", "mo_nrf"], ["mo_wr"], out=wr[:], in0=wrf[:], in1=nrf[:].unsqueeze(2).to_broadcast([128, 8, 36]), op=ALU.mult)
        brb = K.sb(st, "mo_brb", [128, 36], F32)
        K.dma("sp", brb[:, 0:4], Wd["b_router_g"].partition_broadcast(128), [], ["mo_brb"])
        K.dma("sp", brb[:, 4:36], Wd["b_router_e"].partition_broadcast(128), [], ["mo_brb"])
        nfb = K.sb(st, "mo_nfb", [128, D], F32)
        K.dma("sp", nfb[:], Wd["norm_final"].partition_broadcast(128), [], ["mo_nfb"])
        ps = [K.ps(st, "mo_ps%d" % i, [128, 512], F32) for i in range(7)]
        pst = K.ps(st, "mo_pst", [128, 8, 128], BF16)
        ht = K.sb(st, "mo_ht", [128, D], F32)
        xn = K.sb(st, "mo_xn", [128, D], BF16)
        ss = K.sb(st, "mo_ss", [128, 1], F32)
        junk = K.sb(st, "mo_junk", [128, D], F32)
        xT = K.sb(st, "mo_xT", [128, 8, HT], BF16)
        G = K.sb(st, "mo_G", [128, NTL, 32], F32)
        acc = K.sb(st, "mo_acc", [128, NTL, D], F32)
        lg = K.sb(st, "mo_lg", [128, 36], F32)
        cl = K.sb(st, "mo_cl", [128, 12], F32)
        lem = K.sb(st, "mo_lem", [128, 4, 8], F32)
        m8 = K.sb(st, "mo_m8", [128, 8], F32)
        sel = K.sb(st, "mo_sel", [128, 32], F32)
        ex = K.sb(st, "mo_ex", [128, 32], F32)
        stg = [K.sb(st, "mo_stg%d" % i, [128, 4096], F32) for i in range(2)]
        wg = [K.sb(st, "mo_wg%d" % i, [128, 8, 512], BF16) for i in range(2)]
        wu = [K.sb(st, "mo_wu%d" % i, [128, 8, 512], BF16) for i in range(2)]
        wd = [K.sb(st, "mo_wd%d" % i, [128, 4, 1024], BF16) for i in range(2)]
        sgts = [K.sb(st, "mo_sgt%d" % i, [128, 512], F32) for i in range(2)]
        hT = K.sb(st, "mo_hT", [128, 4, 512], BF16)
        tmp = [K.sb(st, "mo_tmp%d" % i, [128, 512], F32) for i in range(3)]
        for hf in range(T // HT):
            base = s * T + hf * HT
            for tl in range(NTL):
                r0 = base + tl * 128
                K.dma("sp", ht[:], SC["H1"][r0:r0 + 128, :], ["H1"], ["mo_ht"])
                norm_T(K, "mo_", ht[:], "mo_ht", xn, ss, junk, pst, (xT, "mo_xT"), tl * 128, identb, CONST["eps6"])
                for k in range(8):
                    K.mm(ps[0][:, 0:36], xT[:, k, tl * 128:(tl + 1) * 128], wr[:, k, :], ["mo_xT", "mo_wr"], ["mo_ps0"], start=(k == 0), stop=(k == 7))
                K.op("dve", "tensor_tensor", ["mo_ps0", "mo_brb"], ["mo_lg"], out=lg[:], in0=ps[0][:, 0:36], in1=brb[:], op=ALU.add)
                K.op("dve", "tensor_reduce", ["mo_lg"], ["mo_cl"], out=cl[:, 0:1], in_=lg[:, 0:4], axis=AX.X, op=ALU.max)
                K.op("dve", "tensor_scalar", ["mo_cl"], ["mo_cl"], out=cl[:, 1:2], in0=cl[:, 0:1], scalar1=-1.0, scalar2=None, op0=ALU.mult)
                K.op("act", "activation", ["mo_lg", "mo_cl"], ["mo_ex", "mo_cl"], out=ex[:, 0:4], in_=lg[:, 0:4], func=AF.Exp, bias=cl[:, 1:2], accum_out=cl[:, 2:3])
                K.op("dve", "reciprocal", ["mo_cl"], ["mo_cl"], out=cl[:, 3:4], in_=cl[:, 2:3])
                K.op("dve", "tensor_scalar", ["mo_lg", "mo_cl"], ["mo_sel"], out=sel[:, 0:4], in0=lg[:, 0:4], scalar1=cl[:, 0:1], scalar2=None, op0=ALU.is_ge)
                K.op("dve", "tensor_scalar", ["mo_sel"], ["mo_sel"], out=sel[:, 0:4], in0=sel[:, 0:4], scalar1=-1.0, scalar2=1e30, op0=ALU.add, op1=ALU.mult)
                K.op("dve", "tensor_tensor", ["mo_lg", "mo_sel"], ["mo_lem"], out=lem[:], in0=lg[:, 4:36].rearrange("p (a b) -> p a b", a=4),
                     in1=sel[:, 0:4].unsqueeze(2).to_broadcast([128, 4, 8]), op=ALU.add)
                lemf = lem[:].rearrange("p a b -> p (a b)")
                K.op("dve", "max", ["mo_lem"], ["mo_m8"], out=m8[:], in_=lemf)
                K.op("dve", "tensor_scalar", ["mo_lem", "mo_m8"], ["mo_sel"], out=sel[:], in0=lemf, scalar1=m8[:, 1:2], scalar2=None, op0=ALU.is_ge)
                K.op("dve", "tensor_scalar", ["mo_m8"], ["mo_cl"], out=cl[:, 4:5], in0=m8[:, 0:1], scalar1=-1.0, scalar2=None, op0=ALU.mult)
                K.op("act", "activation", ["mo_lem", "mo_cl"], ["mo_ex"], out=ex[:], in_=lemf, func=AF.Exp, bias=cl[:, 4:5])
                K.op("dve", "tensor_tensor", ["mo_ex", "mo_sel"], ["mo_ex"], out=ex[:], in0=ex[:], in1=sel[:], op=ALU.mult)
                K.op("dve", "tensor_reduce", ["mo_ex"], ["mo_cl"], out=cl[:, 5:6], in_=ex[:], axis=AX.X, op=ALU.add)
                K.op("dve", "reciprocal", ["mo_cl"], ["mo_cl"], out=cl[:, 6:7], in_=cl[:, 5:6])
                K.op("dve", "tensor_tensor", ["mo_cl"], ["mo_cl"], out=cl[:, 7:8], in0=cl[:, 6:7], in1=cl[:, 3:4], op=ALU.mult)
                K.op("dve", "tensor_scalar", ["mo_ex", "mo_cl"], ["mo_G"], out=G[:, tl, :], in0=ex[:], scalar1=cl[:, 7:8], scalar2=None, op0=ALU.mult)
            for e in range(32):
                i = e % 2
                nfb8 = nrf[:].unsqueeze(2).to_broadcast([128, 8, 512])
                K.dma("sp", stg[0][:].rearrange("p (c n) -> p c n", c=8), Wd["w_e_gate"][0, e].rearrange("(c p) n -> p c n", p=128), [], ["mo_stg0"])
                K.op("pool", "tensor_tensor", ["mo_stg0", "mo_nrf"], ["mo_wg%d" % i], out=wg[i][:], in0=stg[0][:].rearrange("p (c n) -> p c n", c=8), in1=nfb8, op=ALU.mult)
                K.dma("act", stg[1][:].rearrange("p (c n) -> p c n", c=8), Wd["w_e_up"][0, e].rearrange("(c p) n -> p c n", p=128), [], ["mo_stg1"])
                K.op("pool", "tensor_tensor", ["mo_stg1", "mo_nrf"], ["mo_wu%d" % i], out=wu[i][:], in0=stg[1][:].rearrange("p (c n) -> p c n", c=8), in1=nfb8, op=ALU.mult)
                K.dma("sp", stg[0][:].rearrange("p (c n) -> p c n", c=4), Wd["w_e_down"][0, e].rearrange("(c p) n -> p c n", p=128), [], ["mo_stg0"])
                K.op("pool", "tensor_copy", ["mo_stg0"], ["mo_wd%d" % i], out=wd[i][:], in_=stg[0][:].rearrange("p (c n) -> p c n", c=4))
                for bk in range(HT // 512):
                    bs = slice(bk * 512, (bk + 1) * 512)
                    for fc in range(4):
                        fs = slice(fc * 128, (fc + 1) * 128)
                        pg, pu = (0, 1) if fc % 2 == 0 else (4, 5)
                        sg_ = sgts[fc % 2]
                        sgn = "mo_sgt%d" % (fc % 2)
                        for k in range(8):
                            K.mm(ps[pg][:], wg[i][:, k, fs], xT[:, k, bs], ["mo_wg%d" % i, "mo_xT"], ["mo_ps%d" % pg], start=(k == 0), stop=(k == 7))
                        for k in range(8):
                            K.mm(ps[pu][:], wu[i][:, k, fs], xT[:, k, bs], ["mo_wu%d" % i, "mo_xT"], ["mo_ps%d" % pu], start=(k == 0), stop=(k == 7))
                        K.op("act", "activation", ["mo_ps%d" % pg], [sgn], out=sg_[:], in_=ps[pg][:], func=AF.Silu)
                        K.op("dve", "tensor_tensor", ["mo_ps%d" % pu, sgn], ["mo_hT%d" % fc], out=hT[:, fc, :], in0=ps[pu][:], in1=sg_[:], op=ALU.mult)
                    for tt in range(4):
                        tl = bk * 4 + tt
                        for half in range(2):
                            pj = (2, 3, 6)[(2 * tt + half) % 3]
                            for fc in range(4):
                                K.mm(ps[pj][:], hT[:, fc, tt * 128:(tt + 1) * 128], wd[i][:, fc, half * 512:(half + 1) * 512], ["mo_hT%d" % fc, "mo_wd%d" % i],
                                     ["mo_ps%d" % pj], start=(fc == 0), stop=(fc == 3))
                            hs = slice(half * 512, (half + 1) * 512)
                            if e == 0:
                                K.op("act", "activation", ["mo_ps%d" % pj, "mo_G"], ["mo_acc%d_%d" % (tl, half)], out=acc[:, tl, hs], in_=ps[pj][:], func=AF.Copy, scale=G[:, tl, e:e + 1])
                            else:
                                ti = (2 * tt + half) % 3
                                accn = "mo_acc%d_%d" % (tl, half)
                                K.op("act", "activation", ["mo_ps%d" % pj, "mo_G"], ["mo_tmp%d" % ti], out=tmp[ti][:], in_=ps[pj][:], func=AF.Copy, scale=G[:, tl, e:e + 1])
                                K.op("pool" if half == 0 else "dve", "tensor_tensor", ["mo_tmp%d" % ti, accn], [accn], out=acc[:, tl, hs], in0=acc[:, tl, hs], in1=tmp[ti][:], op=ALU.add)
            for tl in range(NTL):
                r0 = base + tl * 128
                K.dma("sp", ht[:], SC["H1"][r0:r0 + 128, :], ["H1"], ["mo_ht"])
                K.op("dve", "tensor_tensor", ["mo_ht", "mo_acc%d_0" % tl, "mo_acc%d_1" % tl], ["mo_ht"], out=ht[:], in0=ht[:], in1=acc[:, tl, :], op=ALU.add)
                K.op("act", "activation", ["mo_ht"], ["mo_junk", "mo_ss"], out=junk[:], in_=ht[:], func=AF.Square, accum_out=ss[:])
                K.op("act", "activation", ["mo_ss", "eps6"], ["mo_ss"], out=ss[:], in_=ss[:], func=AF.Sqrt, scale=1.0 / D, bias=CONST["eps6"][:])
                K.op("dve", "reciprocal", ["mo_ss"], ["mo_ss"], out=ss[:], in_=ss[:])
                K.op("dve", "scalar_tensor_tensor", ["mo_ht", "mo_ss", "mo_nfb"], ["mo_junk"], out=junk[:], in0=ht[:], scalar=ss[:], in1=nfb[:], op0=ALU.mult, op1=ALU.mult)
                K.dma("sp", OUT[r0:r0 + 128, :], junk[:], ["mo_junk"], ["OUT"])


I32 = mybir.dt.int32


def phase_prepack(K, Wd, SC):
    WGU, WDS = SC["WGU"], SC["WDS"]
    with ExitStack() as st:
        nrf = colvec(K, st, "pk_nrf", Wd["norm_ffn"])
        sg = [K.sb(st, "pk_sg%d" % i, [128, 8, 512], F32) for i in range(2)]
        su = [K.sb(st, "pk_su%d" % i, [128, 8, 512], F32) for i in range(2)]
        sd = [K.sb(st, "pk_sd%d" % i, [128, 4, 1024], F32) for i in range(2)]
        og = [K.sb(st, "pk_og%d" % i, [128, 8, 1024], BF16) for i in range(2)]
        od = [K.sb(st, "pk_od%d" % i, [128, 4, 1024], BF16) for i in range(2)]
        nf8 = nrf[:].unsqueeze(2).to_broadcast([128, 8, 512])
        for e in range(32):
            i = e % 2
            K.dma("sp", sg[i][:], Wd["w_e_gate"][0, e].rearrange("(c p) n -> p c n", p=128), [], ["pk_sg%d" % i])
            K.dma("act", su[i][:], Wd["w_e_up"][0, e].rearrange("(c p) n -> p c n", p=128), [], ["pk_su%d" % i])
            K.dma("sp", sd[i][:], Wd["w_e_down"][0, e].rearrange("(c p) n -> p c n", p=128), [], ["pk_sd%d" % i])
            K.op("dve", "tensor_tensor", ["pk_sg%d" % i, "pk_nrf"], ["pk_og%d" % i], out=og[i][:, :, 0:512], in0=sg[i][:], in1=nf8, op=ALU.mult)
            K.op("pool", "tensor_tensor", ["pk_su%d" % i, "pk_nrf"], ["pk_og%d" % i], out=og[i][:, :, 512:1024], in0=su[i][:], in1=nf8, op=ALU.mult)
            K.op("act", "activation", ["pk_sd%d" % i], ["pk_od%d" % i], out=od[i][:], in_=sd[i][:], func=AF.Copy)
            K.dma("act", WGU[e * 1024:(e + 1) * 1024, :].rearrange("(c p) n -> p c n", p=128), og[i][:], ["pk_og%d" % i], ["WGU%d" % i])
            K.dma("sp", WDS[e * 512:(e + 1) * 512, :].rearrange("(c p) n -> p c n", p=128), od[i][:], ["pk_od%d" % i], ["WDS%d" % i])


def phase_moe_sparse(K, s, T, Wd, SC, CONST, OUT):
    nc = K.nc
    S = K.S
    identb = CONST["identb"]
    NTL = T // 128
    SB = 256
    NBLK = (2 * T) // SB + 32
    XS, YS = SC["XS"], SC["YS"]
    WG = Wd["w_e_gate"].rearrange("o e d f -> (o e d) f")
    WU = Wd["w_e_up"].rearrange("o e d f -> (o e d) f")
    WDN = Wd["w_e_down"].rearrange("o e f d -> (o e f) d")
    base = s * T
    with ExitStack() as st0:
        nrf = colvec(K, st0, "ms_nrf", Wd["norm_ffn"])
        GG = K.sb(st0, "ms_GG", [128, NTL, 2], F32)
        DST = K.sb(st0, "ms_DST", [128, NTL, 2], I32)
        IDXG = K.sb(st0, "ms_IDXG", [128, NBLK, 8], I32)
        IDXD = K.sb(st0, "ms_IDXD", [128, NBLK, 4], I32)
        with ExitStack() as st:
            wrf = K.sb(st, "ms_wrf", [128, 8, 36], F32)
            K.dma("sp", wrf[:, :, 0:4], Wd["w_router_g"][0].rearrange("(c p) n -> p c n", p=128), [], ["ms_wrf"])
            K.dma("sp", wrf[:, :, 4:36], Wd["w_router_e"][0].rearrange("(c p) n -> p c n", p=128), [], ["ms_wrf"])
            wr = K.sb(st, "ms_wr", [128, 8, 36], BF16)
            K.op("dve", "tensor_tensor", ["ms_wrf", "ms_nrf"], ["ms_wr"], out=wr[:], in0=wrf[:], in1=nrf[:].unsqueeze(2).to_broadcast([128, 8, 36]), op=ALU.mult)
            brb = K.sb(st, "ms_brb", [128, 36], F32)
            K.dma("sp", brb[:, 0:4], Wd["b_router_g"].partition_broadcast(128), [], ["ms_brb"])
            K.dma("sp", brb[:, 4:36], Wd["b_router_e"].partition_broadcast(128), [], ["ms_brb"])
            ps = [K.ps(st, "ms_ps%d" % i, [128, 512], F32) for i in range(2)]
            pst = K.ps(st, "ms_pst", [128, 8, 128], BF16)
            ht = K.sb(st, "ms_ht", [128, D], F32)
            ss = K.sb(st, "ms_ss", [128, 1], F32)
            junk = K.sb(st, "ms_junk", [128, D], F32)
            XN = K.sb(st, "ms_XN", [128, NTL, D], BF16)
            xT = K.sb(st, "ms_xT", [128, 8, 128], BF16)
            SEL = K.sb(st, "ms_SEL", [128, NTL, 2, 32], F32)
            RNK = K.sb(st, "ms_RNK", [128, NTL, 2], F32)
            carry = K.sb(st, "ms_carry", [128, 32], F32)
            K.op("dve", "memset", [], ["ms_carry"], ap=carry[:], constant=0.0)
            lg = K.sb(st, "ms_lg", [128, 36], F32)
            cl = K.sb(st, "ms_cl", [128, 12], F32)
            lem = K.sb(st, "ms_lem", [128, 32], F32)
            m8 = K.sb(st, "ms_m8", [128, 8], F32)
            s12 = K.sb(st, "ms_s12", [128, 32], F32)
            ex = K.sb(st, "ms_ex", [128, 32], F32)
            t32 = K.sb(st, "ms_t32", [128, 32], F32)
            utri, ones128, bstart, iotap = CONST["utri"], CONST["ones128"], CONST["bstart"], CONST["iotap"]
            GB = 8
            LG = K.sb(st, "ms_LG", [128, GB, 36], F32)
            LM = K.sb(st, "ms_LM", [128, GB, 32], F32)
            L2 = K.sb(st, "ms_L2", [128, GB, 32], F32)
            EX = K.sb(st, "ms_EX", [128, GB, 32], F32)
            S12 = K.sb(st, "ms_S12", [128, GB, 32], F32)
            RKt = K.sb(st, "ms_RKt", [128, GB, 32], F32)
            T4 = K.sb(st, "ms_T4", [128, GB, 4], F32)
            E4 = K.sb(st, "ms_E4", [128, GB, 4], F32)
            CG = K.sb(st, "ms_CG", [128, 8, GB], F32)
            hts = [ht, K.sb(st, "ms_ht1", [128, D], F32)]

            def b3(colv, n):
                return colv.unsqueeze(2).to_broadcast([128, GB, n])

            for g0 in range(0, NTL, GB):
                for gi in range(GB):
                    tl = g0 + gi
                    r0 = base + tl * 128
                    hh_ = hts[tl % 2]
                    hn = "ms_ht" if tl % 2 == 0 else "ms_ht1"
                    K.dma("sp" if tl % 2 == 0 else "act", hh_[:], SC["H1"][r0:r0 + 128, :], ["H1"], [hn])
                    K.op("act", "activation", [hn], ["ms_junk", "ms_ss"], out=junk[:], in_=hh_[:], func=AF.Square, accum_out=ss[:])
                    K.op("act", "activation", ["ms_ss", "eps6"], ["ms_ss"], out=ss[:], in_=ss[:], func=AF.Sqrt, scale=1.0 / D, bias=CONST["eps6"][:])
                    K.op("dve", "reciprocal", ["ms_ss"], ["ms_ss"], out=ss[:], in_=ss[:])
                    K.op("dve", "tensor_scalar", [hn, "ms_ss"], ["ms_XN%d" % tl], out=XN[:, tl, :], in0=hh_[:], scalar1=ss[:], scalar2=None, op0=ALU.mult)
                    for c in range(8):
                        K.tr(pst[:, c, :], XN[:, tl, c * 128:(c + 1) * 128], identb[:], ["ms_XN%d" % tl, "identb"], ["ms_pst"])
                    K.op("act", "activation", ["ms_pst"], ["ms_xT"], out=xT[:], in_=pst[:], func=AF.Copy)
                    for k in range(8):
                        K.mm(ps[0][:, 0:36], xT[:, k, :], wr[:, k, :], ["ms_xT", "ms_wr"], ["ms_ps0"], start=(k == 0), stop=(k == 7))
                    K.op("dve", "tensor_tensor", ["ms_ps0", "ms_brb"], ["ms_LG"], out=LG[:, gi, :], in0=ps[0][:, 0:36], in1=brb[:], op=ALU.add)
                K.op("dve", "tensor_reduce", ["ms_LG"], ["ms_CG"], out=CG[:, 0, :], in_=LG[:, :, 0:4], axis=AX.X, op=ALU.max)
                K.op("dve", "tensor_tensor", ["ms_LG", "ms_CG"], ["ms_T4"], out=T4[:], in0=LG[:, :, 0:4], in1=b3(CG[:, 0, :], 4), op=ALU.subtract)
                K.op("act", "activation", ["ms_T4"], ["ms_E4"], out=E4[:], in_=T4[:], func=AF.Exp)
                K.op("dve", "tensor_reduce", ["ms_E4"], ["ms_CG"], out=CG[:, 1, :], in_=E4[:], axis=AX.X, op=ALU.add)
                K.op("dve", "reciprocal", ["ms_CG"], ["ms_CG"], out=CG[:, 2, :], in_=CG[:, 1, :])
                K.op("dve", "tensor_scalar", ["ms_T4"], ["ms_T4"], out=T4[:], in0=T4[:], scalar1=0.0, scalar2=None, op0=ALU.is_ge)
                K.op("dve", "tensor_scalar", ["ms_T4"], ["ms_T4"], out=T4[:], in0=T4[:], scalar1=-1.0, scalar2=1e30, op0=ALU.add, op1=ALU.mult)
                K.op("dve", "tensor_tensor", ["ms_LG", "ms_T4"], ["ms_LM"], out=LM[:].rearrange("p g (a b) -> p g a b", a=4),
                     in0=LG[:, :, 4:36].rearrange("p g (a b) -> p g a b", a=4), in1=T4[:].unsqueeze(3).to_broadcast([128, GB, 4, 8]), op=ALU.add)
                K.op("dve", "tensor_reduce", ["ms_LM"], ["ms_CG"], out=CG[:, 3, :], in_=LM[:], axis=AX.X, op=ALU.max)
                sel1 = SEL[:, g0:g0 + GB, 0, :]
                sel2 = SEL[:, g0:g0 + GB, 1, :]
                K.op("dve", "tensor_tensor", ["ms_LM", "ms_CG"], ["ms_SEL"], out=sel1, in0=LM[:], in1=b3(CG[:, 3, :], 32), op=ALU.is_ge)
                K.op("dve", "scalar_tensor_tensor", ["ms_SEL", "ms_LM"], ["ms_L2"], out=L2[:], in0=sel1, scalar=-1e30, in1=LM[:], op0=ALU.mult, op1=ALU.add)
                K.op("dve", "tensor_reduce", ["ms_L2"], ["ms_CG"], out=CG[:, 4, :], in_=L2[:], axis=AX.X, op=ALU.max)
                K.op("dve", "tensor_tensor", ["ms_LM", "ms_CG"], ["ms_S12"], out=S12[:], in0=LM[:], in1=b3(CG[:, 4, :], 32), op=ALU.is_ge)
                K.op("dve", "tensor_tensor", ["ms_S12", "ms_SEL"], ["ms_SEL"], out=sel2, in0=S12[:], in1=sel1, op=ALU.subtract)
                K.op("dve", "tensor_tensor", ["ms_LM", "ms_CG"], ["ms_L2"], out=L2[:], in0=LM[:], in1=b3(CG[:, 3, :], 32), op=ALU.subtract)
                K.op("dve", "tensor_scalar", ["ms_L2"], ["ms_L2"], out=L2[:], in0=L2[:], scalar1=-80.0, scalar2=None, op0=ALU.max)
                K.op("act", "activation", ["ms_L2"], ["ms_EX"], out=EX[:], in_=L2[:], func=AF.Exp)
                K.op("dve", "tensor_tensor", ["ms_EX", "ms_S12"], ["ms_EX"], out=EX[:], in0=EX[:], in1=S12[:], op=ALU.mult)
                K.op("dve", "tensor_reduce", ["ms_EX"], ["ms_CG"], out=CG[:, 5, :], in_=EX[:], axis=AX.X, op=ALU.add)
                K.op("dve", "reciprocal", ["ms_CG"], ["ms_CG"], out=CG[:, 6, :], in_=CG[:, 5, :])
                K.op("dve", "tensor_tensor", ["ms_CG"], ["ms_CG"], out=CG[:, 6, :], in0=CG[:, 6, :], in1=CG[:, 2, :], op=ALU.mult)
                for kk_ in range(2):
                    K.op("dve", "tensor_tensor", ["ms_EX", "ms_SEL"], ["ms_L2"], out=L2[:], in0=EX[:], in1=SEL[:, g0:g0 + GB, kk_, :], op=ALU.mult)
                    K.op("dve", "tensor_reduce", ["ms_L2"], ["ms_CG"], out=CG[:, 7, :], in_=L2[:], axis=AX.X, op=ALU.add)
                    K.op("dve", "tensor_tensor", ["ms_CG"], ["ms_GG"], out=GG[:, g0:g0 + GB, kk_], in0=CG[:, 7, :], in1=CG[:, 6, :], op=ALU.mult)
                for gi in range(GB):
                    K.mm(ps[1][:, gi * 64:gi * 64 + 32], utri[:], S12[:, gi, :], ["utri", "ms_S12"], ["ms_ps1"])
                    K.mm(ps[1][:, gi * 64 + 32:gi * 64 + 64], ones128[:], S12[:, gi, :], ["ones128", "ms_S12"], ["ms_ps1"])
                for gi in range(GB):
                    K.op("dve", "tensor_tensor", ["ms_ps1", "ms_carry"], ["ms_RKt"], out=RKt[:, gi, :], in0=ps[1][:, gi * 64:gi * 64 + 32], in1=carry[:], op=ALU.add)
                    K.op("dve", "tensor_tensor", ["ms_ps1", "ms_carry"], ["ms_carry"], out=carry[:], in0=ps[1][:, gi * 64 + 32:gi * 64 + 64], in1=carry[:], op=ALU.add)
                for kk_ in range(2):
                    K.op("dve", "tensor_tensor", ["ms_RKt", "ms_SEL"], ["ms_L2"], out=L2[:], in0=RKt[:], in1=SEL[:, g0:g0 + GB, kk_, :], op=ALU.mult)
                    K.op("dve", "tensor_reduce", ["ms_L2"], ["ms_RNK"], out=RNK[:, g0:g0 + GB, kk_], in_=L2[:], axis=AX.X, op=ALU.add)
            ci = K.sb(st, "ms_ci", [128, 32], I32)
            pad = K.sb(st, "ms_pad", [128, 32], F32)
            pend = K.sb(st, "ms_pend", [128, 32], F32)
            pstart = K.sb(st, "ms_pstart", [128, 32], F32)
            ones32 = K.sb(st, "ms_ones32", [128, 32], F32)
            K.op("dve", "memset", [], ["ms_ones32"], ap=ones32[:], constant=1.0)
            K.op("dve", "tensor_scalar", ["ms_carry"], ["ms_ci"], out=ci[:], in0=carry[:], scalar1=float(SB - 1), scalar2=None, op0=ALU.add)
            K.op("dve", "tensor_scalar", ["ms_ci"], ["ms_ci"], out=ci[:], in0=ci[:], scalar1=8, scalar2=None, op0=ALU.arith_shift_right)
            K.op("dve", "tensor_scalar", ["ms_ci"], ["ms_ci"], out=ci[:], in0=ci[:], scalar1=8, scalar2=None, op0=ALU.logical_shift_left)
            K.op("dve", "tensor_copy", ["ms_ci"], ["ms_pad"], out=pad[:], in_=ci[:])
            K.op("dve", "tensor_tensor_scan", ["ms_pad", "ms_ones32"], ["ms_pend"], out=pend[:], data0=ones32[:], data1=pad[:], initial=0.0, op0=ALU.mult, op1=ALU.add)
            K.op("dve", "tensor_tensor", ["ms_pend", "ms_pad"], ["ms_pstart"], out=pstart[:], in0=pend[:], in1=pad[:], op=ALU.subtract)
            bst = K.sb(st, "ms_bst", [128, NBLK], F32)
            K.op("dve", "tensor_scalar", ["bstart"], ["ms_bst"], out=bst[:], in0=bstart[:, 0:NBLK], scalar1=float(SB // 128), scalar2=None, op0=ALU.mult)
            be = K.sb(st, "ms_be", [128, NBLK], F32)
            K.op("dve", "tensor_scalar", ["ms_bst", "ms_pend"], ["ms_be"], out=be[:], in0=bst[:], scalar1=pend[:, 0:1], scalar2=None, op0=ALU.is_ge)
            for e in range(1, 32):
                K.op("dve", "scalar_tensor_tensor", ["ms_bst", "ms_pend", "ms_be"], ["ms_be"], out=be[:], in0=bst[:], scalar=pend[:, e:e + 1], in1=be[:],
                     op0=ALU.is_ge, op1=ALU.add)
            K.op("dve", "tensor_scalar", ["ms_be"], ["ms_be"], out=be[:], in0=be[:], scalar1=31.0, scalar2=None, op0=ALU.min)
            bg = K.sb(st, "ms_bg", [128, NBLK], F32)
            bd = K.sb(st, "ms_bd", [128, NBLK], F32)
            K.op("dve", "tensor_scalar", ["ms_be", "iotap"], ["ms_bg"], out=bg[:], in0=be[:], scalar1=1024.0, scalar2=iotap[:, 0:1], op0=ALU.mult, op1=ALU.add)
            K.op("dve", "tensor_scalar", ["ms_be", "iotap"], ["ms_bd"], out=bd[:], in0=be[:], scalar1=512.0, scalar2=iotap[:, 0:1], op0=ALU.mult, op1=ALU.add)
            for c in range(8):
                K.op("dve", "tensor_scalar", ["ms_bg"], ["ms_IDXG"], out=IDXG[:, :, c], in0=bg[:], scalar1=float(c * 128), scalar2=None, op0=ALU.add)
            for c in range(4):
                K.op("dve", "tensor_scalar", ["ms_bd"], ["ms_IDXD"], out=IDXD[:, :, c], in0=bd[:], scalar1=float(c * 128), scalar2=None, op0=ALU.add)
            zt = K.sb(st, "ms_zt", [128, 4, D], BF16)
            K.op("pool", "memset", [], ["ms_zt"], ap=zt[:], constant=0.0)
            XSv = XS.rearrange("(b p) d -> p b d", p=128)
            for b0 in range(0, NBLK * SB // 128, 4):
                K.dma("sp" if (b0 // 4) % 2 == 0 else "act", XSv[:, b0:b0 + 4, :], zt[:], ["ms_zt"], ["XS"])
            for tl in range(NTL):
                for kk_ in range(2):
                    K.op("dve", "tensor_tensor", ["ms_pstart", "ms_SEL"], ["ms_t32"], out=t32[:], in0=pstart[:], in1=SEL[:, tl, kk_, :], op=ALU.mult)
                    K.op("dve", "tensor_reduce", ["ms_t32"], ["ms_cl"], out=cl[:, 9:10], in_=t32[:], axis=AX.X, op=ALU.add)
                    K.op("dve", "tensor_tensor", ["ms_cl", "ms_RNK"], ["ms_DST"], out=DST[:, tl, kk_:kk_ + 1], in0=cl[:, 9:10], in1=RNK[:, tl, kk_:kk_ + 1], op=ALU.add)
                for kk_ in range(2):
                    S.dma("pool", None, None, K._bl(["ms_DST", "ms_XN%d" % tl, "XS"]), K._bl(["XSs_%d_%d" % (tl, kk_)]),
                          fn=lambda e, tl=tl, kk_=kk_: e.indirect_dma_start(out=XS, out_offset=bass.IndirectOffsetOnAxis(ap=DST[:, tl, kk_:kk_ + 1], axis=0),
                                                                        in_=XN[:, tl, :], in_offset=None))
        S.barrier()
        with ExitStack() as st:
            ps = [K.ps(st, "mb_ps%d" % i, [128, 512], F32) for i in range(6)]
            pst = K.ps(st, "mb_pst", [128, 8, 128], BF16)
            wgu = [K.sb(st, "mb_wgu%d" % i, [128, 8, 1024], BF16) for i in range(2)]
            wd = [K.sb(st, "mb_wd%d" % i, [128, 4, 1024], BF16) for i in range(2)]
            xb = [K.sb(st, "mb_xb%d" % i, [128, D], BF16) for i in range(2)]
            xT = K.sb(st, "mb_xT", [128, 8, 128], BF16)
            sgt = K.sb(st, "mb_sgt", [128, 512], F32)
            hb = K.sb(st, "mb_hb", [128, 512], BF16)
            hT = K.sb(st, "mb_hT", [128, 4, 128], BF16)
            ysb = [K.sb(st, "mb_ysb%d" % i, [128, D], F32) for i in range(2)]
            WGU, WDS = SC["WGU"], SC["WDS"]
            for b in range(NBLK):
                i = b % 2
                for c in range(8):
                    S.dma("pool", None, None, K._bl(["ms_IDXG"]), K._bl(["mb_wgu%d_%d" % (i, c)]),
                          fn=lambda e, b=b, c=c, i=i: e.indirect_dma_start(out=wgu[i][:, c, :], out_offset=None, in_=WGU,
                                                                         in_offset=bass.IndirectOffsetOnAxis(ap=IDXG[:, b, c:c + 1], axis=0)))
                for c in range(4):
                    S.dma("pool", None, None, K._bl(["ms_IDXD"]), K._bl(["mb_wd%d_%d" % (i, c)]),
                          fn=lambda e, b=b, c=c, i=i: e.indirect_dma_start(out=wd[i][:, c, :], out_offset=None, in_=WDS,
                                                                         in_offset=bass.IndirectOffsetOnAxis(ap=IDXD[:, b, c:c + 1], axis=0)))
                for sub in range(SB // 128):
                    j = sub % 2
                    r0 = b * SB + sub * 128
                    K.dma("sp", xb[j][:], XS[r0:r0 + 128, :], ["XS"], ["mb_xb%d" % j])
                    for c in range(8):
                        K.tr(pst[:, c, :], xb[j][:, c * 128:(c + 1) * 128], identb[:], ["mb_xb%d" % j, "identb"], ["mb_pst"])
                    K.op("act", "activation", ["mb_pst"], ["mb_xT"], out=xT[:], in_=pst[:], func=AF.Copy)
                    for k in range(8):
                        K.mm(ps[0][:], xT[:, k, :], wgu[i][:, k, 0:512], ["mb_xT"] + ["mb_wgu%d_%d" % (i, c) for c in range(8)], ["mb_ps0"], start=(k == 0), stop=(k == 7))
                    for k in range(8):
                        K.mm(ps[1][:], xT[:, k, :], wgu[i][:, k, 512:1024], ["mb_xT"] + ["mb_wgu%d_%d" % (i, c) for c in range(8)], ["mb_ps1"], start=(k == 0), stop=(k == 7))
                    K.op("act", "activation", ["mb_ps0"], ["mb_sgt"], out=sgt[:], in_=ps[0][:], func=AF.Silu)
                    K.op("dve", "tensor_tensor", ["mb_ps1", "mb_sgt"], ["mb_hb"], out=hb[:], in0=ps[1][:], in1=sgt[:], op=ALU.mult)
                    for fc in range(4):
                        K.tr(pst[:, fc, :], hb[:, fc * 128:(fc + 1) * 128], identb[:], ["mb_hb", "identb"], ["mb_pst"])
                    K.op("dve", "tensor_copy", ["mb_pst"], ["mb_hT"], out=hT[:], in_=pst[:, 0:4, :])
                    for half in range(2):
                        pj = 2 + 2 * j + half
                        for fc in range(4):
                            K.mm(ps[pj][:], hT[:, fc, :], wd[i][:, fc, half * 512:(half + 1) * 512], ["mb_hT"] + ["mb_wd%d_%d" % (i, c) for c in range(4)], ["mb_ps%d" % pj], start=(fc == 0), stop=(fc == 3))
                        if half == 0:
                            K.op("act", "activation", ["mb_ps%d" % pj], ["mb_ysb%d" % j], out=ysb[j][:, 0:512], in_=ps[pj][:], func=AF.Copy)
                        else:
                            K.op("dve", "tensor_copy", ["mb_ps%d" % pj], ["mb_ysb%d" % j], out=ysb[j][:, 512:1024], in_=ps[pj][:])
                    K.dma("act", YS[r0:r0 + 128, :], ysb[j][:], ["mb_ysb%d" % j], ["YS"])
        S.barrier()
        with ExitStack() as st:
            nfb = K.sb(st, "mc_nfb", [128, D], F32)
            K.dma("sp", nfb[:], Wd["norm_final"].partition_broadcast(128), [], ["mc_nfb"])
            hts = [K.sb(st, "mc_ht%d" % i, [128, D], F32) for i in range(2)]
            y1 = [K.sb(st, "mc_y1%d" % i, [128, D], F32) for i in range(2)]
            y2 = [K.sb(st, "mc_y2%d" % i, [128, D], F32) for i in range(2)]
            ob = [K.sb(st, "mc_ob%d" % i, [128, D], F32) for i in range(2)]
            junk = K.sb(st, "mc_junk", [128, D], F32)
            sss = [K.sb(st, "mc_ss%d" % i, [128, 1], F32) for i in range(2)]
            for tl in range(NTL):
                i = tl % 2
                r0 = base + tl * 128
                K.dma("sp", hts[i][:], SC["H1"][r0:r0 + 128, :], ["H1"], ["mc_ht%d" % i])
                S.dma("pool", None, None, K._bl(["ms_DST", "YS"]), K._bl(["mc_y1%d" % i]),
                      fn=lambda e, tl=tl, i=i: e.indirect_dma_start(out=y1[i][:], out_offset=None, in_=YS, in_offset=bass.IndirectOffsetOnAxis(ap=DST[:, tl, 0:1], axis=0)))
                S.dma("pool", None, None, K._bl(["ms_DST", "YS"]), K._bl(["mc_y2%d" % i]),
                      fn=lambda e, tl=tl, i=i: e.indirect_dma_start(out=y2[i][:], out_offset=None, in_=YS, in_offset=bass.IndirectOffsetOnAxis(ap=DST[:, tl, 1:2], axis=0)))
                K.op("dve", "scalar_tensor_tensor", ["mc_y1%d" % i, "ms_GG", "mc_ht%d" % i], ["mc_ht%d" % i], out=hts[i][:], in0=y1[i][:], scalar=GG[:, tl, 0:1], in1=hts[i][:],
                     op0=ALU.mult, op1=ALU.add)
                K.op("dve", "scalar_tensor_tensor", ["mc_y2%d" % i, "ms_GG", "mc_ht%d" % i], ["mc_ht%d" % i], out=hts[i][:], in0=y2[i][:], scalar=GG[:, tl, 1:2], in1=hts[i][:],
                     op0=ALU.mult, op1=ALU.add)
                K.op("act", "activation", ["mc_ht%d" % i], ["mc_junk", "mc_ss%d" % i], out=junk[:], in_=hts[i][:], func=AF.Square, accum_out=sss[i][:])
                K.op("act", "activation", ["mc_ss%d" % i, "eps6"], ["mc_ss%d" % i], out=sss[i][:], in_=sss[i][:], func=AF.Sqrt, scale=1.0 / D, bias=CONST["eps6"][:])
                K.op("dve", "reciprocal", ["mc_ss%d" % i], ["mc_ss%d" % i], out=sss[i][:], in_=sss[i][:])
                K.op("dve", "scalar_tensor_tensor", ["mc_ht%d" % i, "mc_ss%d" % i, "mc_nfb"], ["mc_ob%d" % i], out=ob[i][:], in0=hts[i][:], scalar=sss[i][:], in1=nfb[:],
                     op0=ALU.mult, op1=ALU.mult)
                K.dma("act", OUT[r0:r0 + 128, :], ob[i][:], ["mc_ob%d" % i], ["OUT"])


def build(T, NSEQ, stop_after=99, debug=False):
    nc = bass.Bass("TRN2", target_bir_lowering=False)
    NTOK = NSEQ * T

    def din(name, shape, dt=F32):
        return nc.dram_tensor(name, list(shape), dt, kind="ExternalInput").ap()

    X = din("x", [NTOK, D])
    MEM = din("mem", [NSEQ * 256, D])
    Wd = {}
    for name, shape in WSHAPES.items():
        Wd[name] = din(name, shape)
    identb_d = din("c_identb", [128, 128], BF16)
    identf_d = din("c_identf", [128, 128], F32)
    OUT = nc.dram_tensor("out", [NTOK, D], F32, kind="ExternalOutput").ap()
    SC = {}
    SC["ZF"] = nc.dram_tensor("sc_zf", [NSEQ, R_TOT, T], F32, kind="Internal").ap() if not debug else \
        nc.dram_tensor("sc_zf", [NSEQ, R_TOT, T], F32, kind="ExternalOutput").ap()
    kindd = "ExternalOutput" if debug else "Internal"
    SC["CK"] = nc.dram_tensor("sc_ck", [NSEQ, T, 128], BF16, kind=kindd).ap()
    SC["CKT"] = nc.dram_tensor("sc_ckt", [NSEQ, 128, T], BF16, kind=kindd).ap()
    SC["H1"] = nc.dram_tensor("sc_h1", [NTOK, D], F32, kind=kindd).ap()
    NSLOT = ((2 * T) // 256 + 32) * 256
    SC["WGU"] = nc.dram_tensor("sc_wgu", [32 * 1024, 1024], BF16, kind="Internal").ap()
    SC["WDS"] = nc.dram_tensor("sc_wds", [32 * 512, 1024], BF16, kind="Internal").ap()
    SC["XS"] = nc.dram_tensor("sc_xs", [NSLOT, D], BF16, kind="Internal").ap()
    SC["YS"] = nc.dram_tensor("sc_ys", [NSLOT, D], F32, kind="Internal").ap()
    SC["YB"] = nc.dram_tensor("sc_yb", [NSEQ, 512, T], BF16, kind=kindd).ap()
    SC["YA"] = nc.dram_tensor("sc_ya", [NSEQ, 512, T], BF16, kind=kindd).ap()
    cdram = {}
    for nm, arr in consts().items():
        if nm not in ("c_identb", "c_identf"):
            cdram[nm] = din(nm, arr.shape, BF16 if arr.dtype == ml_dtypes.bfloat16 else F32)
    with ExitStack() as st:
        S = Sched(nc, st)
        K = Ctx(nc, S)
        CONST = {}
        CONST["identb"] = K.sb(st, "identb", [128, 128], BF16)
        CONST["identf"] = K.sb(st, "identf", [128, 128], F32)
        CONST["eps6"] = K.sb(st, "eps6", [128, 1], F32)
        K.dma("sp", CONST["identb"][:], identb_d, [], ["identb"])
        K.dma("sp", CONST["identf"][:], identf_d, [], ["identf"])
        K.op("dve", "memset", [], ["eps6"], ap=CONST["eps6"][:], constant=1e-6)
        for nm, ap in cdram.items():
            sh = list(ap.shape)
            CONST[nm[2:]] = K.sb(st, nm[2:], sh, ap.dtype)
            K.dma("sp", CONST[nm[2:]][:], ap, [], [nm[2:]])
        if stop_after >= 6 and not os.environ.get("MOE_DENSE"):
            phase_prepack(K, Wd, SC)
            S.barrier()
        for s in range(NSEQ):
            phase1(K, s, T, X, Wd, SC, CONST)
            S.barrier()
            if stop_after >= 2 and not os.environ.get("SKIP_DSA"):
                phase_dsa(K, s, T, Wd, SC, CONST)
                S.barrier()
            if stop_after >= 3:
                phase_rwkv(K, s, T, Wd, SC, CONST)
                S.barrier()
            if stop_after >= 4:
                phase_mix(K, s, T, X, Wd, SC, CONST)
                S.barrier()
            if stop_after >= 5:
                phase_cross(K, s, T, MEM, Wd, SC, CONST)
                S.barrier()
            if stop_after >= 6:
                if os.environ.get("MOE_DENSE"):
                    phase_moe(K, s, T, Wd, SC, CONST, OUT)
                else:
                    phase_moe_sparse(K, s, T, Wd, SC, CONST, OUT)
                S.barrier()
        S.finish(list(K.B.values()))
        print("ops", S.nops, "waits", S.nwaits)
        S.emit()
    return nc


WSHAPES = {
    "norm_mix": [1, 1024], "w_in": [1, 1024, 4804], "shift_mu": [1, 1792], "rw_w0": [1, 512],
    "rw_w2": [1, 64, 512], "rw_a0": [1, 512], "rw_a2": [1, 64, 512], "rw_g2": [1, 128, 512],
    "rw_k_k": [1, 512], "rw_k_a": [1, 512], "rw_r_k": [1, 8, 64], "rw_ln_w": [1, 512], "rw_ln_b": [1, 512],
    "kv_norm": [1, 128], "w_uk": [1, 128, 8, 64], "w_uv": [1, 128, 8, 64], "w_proj_a": [1, 512, 1024],
    "w_proj_b": [1, 512, 1024], "b_gate": [1, 2048], "w_out": [1, 1024, 1024], "norm_cross": [1, 1024],
    "norm_mem": [1, 1024], "w_cq": [1, 1024, 1024], "w_ckv": [1, 1024, 2048], "w_co": [1, 1024, 1024],
    "norm_ffn": [1, 1024], "w_router_g": [1, 1024, 4], "b_router_g": [1, 4], "w_router_e": [1, 1024, 32],
    "b_router_e": [1, 32], "w_e_gate": [1, 32, 1024, 512], "w_e_up": [1, 32, 1024, 512],
    "w_e_down": [1, 32, 512, 1024], "norm_final": [1024],
}


def consts():
    return {
        "c_identb": np.eye(128, dtype=np.float32).astype(ml_dtypes.bfloat16),
        "c_identf": np.eye(128, dtype=np.float32),
        "c_tri01": (np.arange(128)[None, :] <= np.arange(128)[:, None]).astype(np.float32).astype(ml_dtypes.bfloat16),
        "c_negtri": np.where(np.arange(128)[None, :] <= np.arange(128)[:, None], 0.0, -1e30).astype(np.float32),
        "c_bo": np.kron(np.eye(2), np.ones((64, 64))).astype(np.float32),
        "c_bo64": (np.kron(np.eye(2), np.ones((64, 64))) / 64.0).astype(np.float32),
        "c_maskq": np.block([[np.triu(np.ones((64, 64)), 1), np.triu(np.ones((64, 64)), 0)],
                             [np.triu(np.ones((64, 64)), 1), np.triu(np.ones((64, 64)), 0)]]).astype(np.float32),
        "c_lowm": np.concatenate([np.zeros((64, 64)), np.tril(np.ones((64, 64)), -1)], 0).astype(np.float32),
        "c_resetm": np.tile((np.arange(256) % 64 != 0).astype(np.float32)[None, :], (128, 1)),
        "c_utri": (np.arange(128)[:, None] < np.arange(128)[None, :]).astype(np.float32),
        "c_ones128": np.ones((128, 128), np.float32),
        "c_bstart": np.tile((np.arange(320) * 128.0)[None, :], (128, 1)).astype(np.float32),
        "c_iotap": np.arange(128, dtype=np.float32)[:, None].copy(),
        "c_pw": np.tile((0.5 ** (np.arange(NIT) + 1))[None, :], (128, 1)).astype(np.float32),
    }


def kernel(**inputs):
    x = np.asarray(inputs["x"], dtype=np.float32)
    mem = np.asarray(inputs["mem"], dtype=np.float32)
    B, T, _ = x.shape
    nseq = B // NCORES
    nc = build(T, nseq)
    cs = consts()
    in_maps = []
    for c in range(NCORES):
        m = {"x": np.ascontiguousarray(x[c * nseq:(c + 1) * nseq].reshape(nseq * T, D)),
             "mem": np.ascontiguousarray(mem[c * nseq:(c + 1) * nseq].reshape(nseq * 256, D))}
        for name in WSHAPES:
            m[name] = np.ascontiguousarray(np.asarray(inputs[name], dtype=np.float32))
        m.update(cs)
        in_maps.append(m)
    res = run_bass_kernel_spmd(nc, in_maps, core_ids=list(range(NCORES)))
    out = np.concatenate([r["out"].reshape(nseq, T, D) for r in res.results], axis=0)
    return out.astype(np.float32)
```

```python
from contextlib import ExitStack
import os
import numpy as np
import ml_dtypes
import concourse.bass as bass
import concourse.mybir as mybir
from concourse.bass_utils import run_bass_kernel_spmd

F32 = mybir.dt.float32
BF16 = mybir.dt.bfloat16
AF = mybir.ActivationFunctionType
ALU = mybir.AluOpType
AX = mybir.AxisListType

D = 1024
NCORES = 8


class Buf:
    __slots__ = ("name", "w", "r")

    def __init__(self, name=""):
        self.name = name
        self.w = None
        self.r = {}


class Sched:
    ENG = ("pe", "act", "dve", "pool", "sp")

    def __init__(self, nc, stack, n_dma_sems=10):
        self.nc = nc
        self.streams = {e: [] for e in self.ENG}
        self.sems = {}
        self.count = {}
        for e in self.ENG:
            self.sems[e] = stack.enter_context(nc.semaphore("s_" + e))
            self.count[e] = 0
        self.dma_sems = {}
        self.dma_rr = {}
        for q in ("sp", "act", "pool"):
            lst = []
            for i in range(n_dma_sems if q != "pool" else 28):
                k = "d_%s_%d" % (q, i)
                self.sems[k] = stack.enter_context(nc.semaphore(k))
                self.count[k] = 0
                lst.append(k)
            self.dma_sems[q] = lst
            self.dma_rr[q] = 0
        self.waited = {}
        self.nwaits = 0
        self.nops = 0

    def _wait(self, eng, key, val):
        if val <= 0 or self.waited.get((eng, key), 0) >= val:
            return
        self.waited[(eng, key)] = val
        self.streams[eng].append(("w", key, val))
        self.nwaits += 1

    def _deps(self, eng, reads, writes, own_key):
        for b in reads:
            if b.w is not None:
                self._dep(eng, b.w, own_key)
        for b in writes:
            if b.w is not None:
                self._dep(eng, b.w, own_key)
            for k, v in b.r.items():
                self._dep(eng, (k, v), own_key)

    def _dep(self, eng, ev, own_key):
        k, v = ev
        if k == "pe" and own_key == "pe":
            return
        self._wait(eng, k, v)

    muted = False

    def op(self, eng, fn, reads=(), writes=()):
        if self.muted:
            return
        self._deps(eng, reads, writes, eng)
        self.count[eng] += 1
        v = self.count[eng]
        self.streams[eng].append(("o", fn, eng, 1))
        for b in writes:
            b.w = (eng, v)
            b.r = {}
        for b in reads:
            if b.r.get(eng, 0) < v:
                b.r[eng] = v
        self.nops += 1

    def dma(self, q, out, in_, reads=(), writes=(), fn=None, **kw):
        if self.muted:
            return
        lst = self.dma_sems[q]
        key = lst[self.dma_rr[q] % len(lst)]
        self.dma_rr[q] += 1
        self._wait(q, key, self.count[key])
        self._deps(q, reads, writes, key)
        self.count[key] += 16
        v = self.count[key]
        if fn is None:
            fn = lambda e, out=out, in_=in_, kw=kw: e.dma_start(out=out, in_=in_, **kw)
        self.streams[q].append(("o", fn, key, 16))
        for b in writes:
            b.w = (key, v)
            b.r = {}
        for b in reads:
            if b.r.get(key, 0) < v:
                b.r[key] = v
        self.nops += 1

    def barrier(self):
        for e in self.ENG:
            for k in self.sems:
                if k != e or True:
                    self._wait(e, k, self.count[k])

    def finish(self, bufs, eng="sp"):
        for b in bufs:
            if b.w is not None:
                self._wait(eng, b.w[0], b.w[1])

    def emit(self):
        nc = self.nc
        sems = self.sems
        streams = self.streams
        with nc.Block() as block:
            def run(engobj, lst):
                for it in lst:
                    if it[0] == "w":
                        engobj.wait_ge(sems[it[1]], it[2])
                    else:
                        it[1](engobj).then_inc(sems[it[2]], it[3])

            @block.tensor
            def _(e):
                run(e, streams["pe"])

            @block.scalar
            def _(e):
                run(e, streams["act"])

            @block.vector
            def _(e):
                run(e, streams["dve"])

            @block.gpsimd
            def _(e):
                run(e, streams["pool"])

            @block.sync
            def _(e):
                run(e, streams["sp"])


class Ctx:
    def __init__(self, nc, S):
        self.nc = nc
        self.S = S
        self.B = {}
        self.rr = 0
        self.uid = 0

    def buf(self, name):
        if name not in self.B:
            self.B[name] = Buf(name)
        return self.B[name]

    def _bl(self, lst):
        return [self.buf(x) if isinstance(x, str) else x for x in lst]

    def sb(self, st, name, shape, dt):
        self.uid += 1
        t = st.enter_context(self.nc.sbuf_tensor("%s_u%d" % (name, self.uid), list(shape), dt))
        self.buf(name)
        return t

    def ps(self, st, name, shape, dt):
        self.uid += 1
        t = st.enter_context(self.nc.psum_tensor("%s_u%d" % (name, self.uid), list(shape), dt))
        self.buf(name)
        return t

    def op(self, eng, method, reads, writes, **kw):
        self.S.op(eng, lambda e, m=method, kw=kw: getattr(e, m)(**kw), self._bl(reads), self._bl(writes))

    def mm(self, out, lhsT, rhs, reads, writes, start=True, stop=True, **kw):
        self.S.op("pe", lambda e: e.matmul(out, lhsT, rhs, start=start, stop=stop, **kw),
                  self._bl(reads), self._bl(writes))

    def tr(self, out, in_, ident, reads, writes):
        self.S.op("pe", lambda e: e.transpose(out, in_, ident), self._bl(reads), self._bl(writes))

    def dma(self, q, out, in_, reads, writes, **kw):
        self.S.dma(q, out, in_, self._bl(reads), self._bl(writes), **kw)

    def q(self):
        self.rr += 1
        return ("sp", "act", "pool")[self.rr % 3]


C_RW = 0
C_Q = 1792
C_CKV = 2304
C_QI = 2432
C_KI = 2688
C_WI = 2752
C_G = 2756
R_RW = 0
R_Q = 1792
R_QI = 2304
R_KI = 2560
R_G = 2624
R_WI = 4672
R_TOT = 4676


def load_cast(K, st, tag, w_ap, kin, n, scale_col=None, dt=BF16, engs=("dve", "pool")):
    nc = K.nc
    kc = kin // 128
    wt = K.sb(st, tag, [128, kc, n], dt)
    src = w_ap.rearrange("(c p) n -> p c n", p=128)
    if True:
        stg = [K.sb(st, "%s_stg%d" % (tag, i), [128, n], F32) for i in range(2)]
        for c in range(kc):
            sg = stg[c % 2]
            nm = "%s_stg%d" % (tag, c % 2)
            K.dma(K.q(), sg[:], src[:, c, :], [], [nm])
            eng = engs[c % len(engs)]
            if scale_col is None:
                K.op(eng, "tensor_copy", [nm], [tag], out=wt[:, c, :], in_=sg[:])
            else:
                K.op(eng, "tensor_scalar", [nm, scale_col[1]], [tag], out=wt[:, c, :], in0=sg[:],
                     scalar1=scale_col[0][:, c:c + 1], scalar2=None, op0=ALU.mult)
    return wt


def norm_rows(K, tag, xt, xt_name, ss, junk, eps_scale=1.0 / D):
    K.op("act", "activation", [xt_name], [tag + "_junk", tag + "_ss"], out=junk[:], in_=xt[:], func=AF.Square,
         accum_out=ss[:])
    K.op("act", "activation", [tag + "_ss"], [tag + "_ss"], out=ss[:], in_=ss[:], func=AF.Sqrt,
         scale=eps_scale, bias=1e-6)
    K.op("dve", "reciprocal", [tag + "_ss"], [tag + "_ss"], out=ss[:], in_=ss[:])


def phase1(K, s, T, X, Wd, SC, CONST):
    nc = K.nc
    NT = T // 128
    NB = T // 512
    with ExitStack() as st:
        xnT = K.sb(st, "p1_xnT", [128, 8, T], BF16)
        gm = K.sb(st, "p1_gm", [128, 8], F32)
        K.dma("sp", gm[:], Wd["norm_mix"].rearrange("o (c p) -> p (o c)", p=128), [], ["p1_gm"], allow_slow_non_contiguous=True)
        bg = K.sb(st, "p1_bg", [128, 16], F32)
        K.dma("sp", bg[:], Wd["b_gate"].rearrange("o (c p) -> p (o c)", p=128), [], ["p1_bg"], allow_slow_non_contiguous=True)
        identb = CONST["identb"]
        pst = K.ps(st, "p1_pst", [128, 8, 128], BF16)
        xts = [K.sb(st, "p1_xt%d" % i, [128, D], F32) for i in range(2)]
        xnb = [K.sb(st, "p1_xn%d" % i, [128, D], BF16) for i in range(2)]
        junk = K.sb(st, "p1_junk", [128, D], F32)
        sss = [K.sb(st, "p1_ss%d" % i, [128, 1], F32) for i in range(2)]
        for tt in range(NT):
            i = tt % 2
            xt, xn, ss = xts[i], xnb[i], sss[i]
            K.dma("sp" if i == 0 else "act", xt[:], X[s * T + tt * 128: s * T + (tt + 1) * 128, :], [], ["p1_xt%d" % i])
            K.op("act", "activation", ["p1_xt%d" % i], ["p1_junk", "p1_ss%d" % i], out=junk[:], in_=xt[:],
                 func=AF.Square, accum_out=ss[:])
            K.op("act", "activation", ["p1_ss%d" % i, "eps6"], ["p1_ss%d" % i], out=ss[:], in_=ss[:], func=AF.Sqrt,
                 scale=1.0 / D, bias=CONST["eps6"][:])
            K.op("dve", "reciprocal", ["p1_ss%d" % i], ["p1_ss%d" % i], out=ss[:], in_=ss[:])
            K.op("dve", "tensor_scalar", ["p1_xt%d" % i, "p1_ss%d" % i], ["p1_xn%d" % i], out=xn[:], in0=xt[:],
                 scalar1=ss[:], scalar2=None, op0=ALU.mult)
            for c in range(8):
                K.tr(pst[:, c, :], xn[:, c * 128:(c + 1) * 128], identb[:], ["p1_xn%d" % i, "identb"], ["p1_pst"])
            K.op("pool" if False else "act", "activation", ["p1_pst"], ["p1_xnT"], out=xnT[:, :, tt * 128:(tt + 1) * 128],
                 in_=pst[:], func=AF.Copy)
        import os
        STOP = int(os.environ.get("STOP", "99"))
        if STOP <= 1:
            return
        chunks = []
        for i in range(14):
            chunks.append((C_RW + i * 128, 128, R_RW + i * 128, "fm", None))
        for i in range(4):
            chunks.append((C_Q + i * 128, 128, R_Q + i * 128, "fm", None))
        for i in range(2):
            chunks.append((C_QI + i * 128, 128, R_QI + i * 128, "fm", None))
        chunks.append((C_KI, 64, R_KI, "fm", None))
        chunks.append((C_WI, 4, R_WI, "fm", None))
        for i in range(16):
            chunks.append((C_G + i * 128, 128, R_G + i * 128, "gate", i))
        wsrc = Wd["w_in"].rearrange("o (c p) n -> p (o c) n", p=128)
        wst = [K.sb(st, "p1_wst%d" % i, [128, 8, 132], F32) for i in range(2)]
        wbf = [K.sb(st, "p1_wbf%d" % i, [128, 8, 132], BF16) for i in range(2)]
        stage = [K.sb(st, "p1_stage%d" % i, [128, T], F32) for i in range(2)]
        pss = [K.ps(st, "p1_ps%d" % i, [128, 512], F32) for i in range(4)]
        gmb = gm[:].unsqueeze(2).to_broadcast([128, 8, 128])
        ZF = SC["ZF"]
        for ci, (c0, ncol, r0, kind, gi) in enumerate(chunks):
            i = ci % 2
            K.dma("sp" if i == 0 else "pool", wst[i][:, :, 0:ncol], wsrc[:, :, c0:c0 + ncol], [], ["p1_wst%d" % i])
            K.op("dve", "tensor_tensor", ["p1_wst%d" % i, "p1_gm"], ["p1_wbf%d" % i], out=wbf[i][:, :, 0:ncol],
                 in0=wst[i][:, :, 0:ncol], in1=gm[:].unsqueeze(2).to_broadcast([128, 8, ncol]), op=ALU.mult)
            for tb in range(NB):
                pj = (ci * NB + tb) % 4
                ps = pss[pj]
                for dc in range(8):
                    K.mm(ps[0:ncol, :], wbf[i][:, dc, 0:ncol], xnT[:, dc, tb * 512:(tb + 1) * 512],
                         ["p1_wbf%d" % i, "p1_xnT"], ["p1_ps%d" % pj], start=(dc == 0), stop=(dc == 7))
                if kind == "gate":
                    K.op("act", "activation", ["p1_ps%d" % pj, "p1_bg"], ["p1_stage%d" % i],
                         out=stage[i][0:ncol, tb * 512:(tb + 1) * 512], in_=ps[0:ncol, :], func=AF.Sigmoid,
                         bias=bg[:, gi:gi + 1])
                else:
                    eng = "dve" if tb % 2 == 0 else "act"
                    if eng == "dve":
                        K.op("dve", "tensor_copy", ["p1_ps%d" % pj], ["p1_stage%d" % i],
                             out=stage[i][0:ncol, tb * 512:(tb + 1) * 512], in_=ps[0:ncol, :])
                    else:
                        K.op("act", "activation", ["p1_ps%d" % pj], ["p1_stage%d" % i],
                             out=stage[i][0:ncol, tb * 512:(tb + 1) * 512], in_=ps[0:ncol, :], func=AF.Copy)
            K.dma("act" if i == 0 else "sp", ZF[s, r0:r0 + ncol, :], stage[i][0:ncol, :], ["p1_stage%d" % i], ["ZF"])
        if STOP <= 2:
            return
        i = len(chunks) % 2
        K.dma("sp", wst[i][:, :, 0:128], wsrc[:, :, C_CKV:C_CKV + 128], [], ["p1_wst%d" % i])
        K.op("dve", "tensor_tensor", ["p1_wst%d" % i, "p1_gm"], ["p1_wbf%d" % i], out=wbf[i][:, :, 0:128],
             in0=wst[i][:, :, 0:128], in1=gm[:].unsqueeze(2).to_broadcast([128, 8, 128]), op=ALU.mult)
        ck = [K.sb(st, "p1_ck%d" % j, [128, 128], F32) for j in range(2)]
        ckb = [K.sb(st, "p1_ckb%d" % j, [128, 128], BF16) for j in range(2)]
        ckT = K.sb(st, "p1_ckT", [128, T], BF16)
        for tt in range(NT):
            j = tt % 2
            pj = tt % 4
            ps = pss[pj]
            for dc in range(8):
                K.mm(ps[:, 0:128], xnT[:, dc, tt * 128:(tt + 1) * 128], wbf[i][:, dc, 0:128],
                     ["p1_wbf%d" % i, "p1_xnT"], ["p1_ps%d" % pj], start=(dc == 0), stop=(dc == 7))
            K.op("dve", "tensor_copy", ["p1_ps%d" % pj], ["p1_ck%d" % j], out=ck[j][:], in_=ps[:, 0:128])
            K.op("act", "activation", ["p1_ck%d" % j], ["p1_junk", "p1_ss%d" % j], out=junk[:, 0:128], in_=ck[j][:],
                 func=AF.Square, accum_out=sss[j][:])
            K.op("act", "activation", ["p1_ss%d" % j, "eps6"], ["p1_ss%d" % j], out=sss[j][:], in_=sss[j][:], func=AF.Sqrt,
                 scale=1.0 / 128, bias=CONST["eps6"][:])
            K.op("dve", "reciprocal", ["p1_ss%d" % j], ["p1_ss%d" % j], out=sss[j][:], in_=sss[j][:])
            K.op("dve", "tensor_scalar", ["p1_ck%d" % j, "p1_ss%d" % j], ["p1_ckb%d" % j], out=ckb[j][:], in0=ck[j][:],
                 scalar1=sss[j][:], scalar2=None, op0=ALU.mult)
            K.tr(pst[:, 0, :], ckb[j][:], identb[:], ["p1_ckb%d" % j, "identb"], ["p1_pst"])
            K.op("act", "activation", ["p1_pst"], ["p1_ckT"], out=ckT[:, tt * 128:(tt + 1) * 128], in_=pst[:, 0, :],
                 func=AF.Copy)
            K.dma("sp", SC["CK"][s, tt * 128:(tt + 1) * 128, :], ckb[j][:], ["p1_ckb%d" % j], ["CK"])
        K.dma("sp", SC["CKT"][s, :, :], ckT[:], ["p1_ckT"], ["CKT"])


NIT = 14


def phase_dsa(K, s, T, Wd, SC, CONST, extra=None):
    nc = K.nc
    NT = T // 128
    ZF = SC["ZF"]
    identb, identf = CONST["identb"], CONST["identf"]
    with ExitStack() as st:
        dps = [K.ps(st, "ds_ps%d" % i, [128, 512], F32) for i in range(4)]
        Ob = [K.ps(st, "ds_o%d" % i, [128, 3, 130], F32) for i in range(3)]
        MT = K.ps(st, "ds_mt", [128, 8, 128], BF16)
        wuk = K.sb(st, "ds_wuk", [128, 512], F32)
        K.dma("sp", wuk[:], Wd["w_uk"].rearrange("o r h d -> r (o h d)"), [], ["ds_wuk"])
        wukT = K.sb(st, "ds_wukT", [64, 8, 128], BF16)
        for h in range(8):
            K.tr(dps[3][0:64, 0:128], wuk[:, h * 64:(h + 1) * 64], identf[:], ["ds_wuk", "identf"], ["ds_ps3"])
            K.op("dve", "tensor_copy", ["ds_ps3"], ["ds_wukT"], out=wukT[:, h, :], in_=dps[3][0:64, 0:128])
        kvn = K.sb(st, "ds_kvn", [128, 1], F32)
        K.dma("sp", kvn[:], Wd["kv_norm"].rearrange("o r -> r o"), [], ["ds_kvn"], allow_slow_non_contiguous=True)
        kvn8 = K.sb(st, "ds_kvn8", [128, 1], F32)
        K.op("dve", "tensor_scalar", ["ds_kvn"], ["ds_kvn8"], out=kvn8[:], in0=kvn[:], scalar1=0.125, scalar2=None,
             op0=ALU.mult)
        wuv = K.sb(st, "ds_wuv", [128, 512], F32)
        K.dma("sp", wuv[:], Wd["w_uv"].rearrange("o r h d -> r (o h d)"), [], ["ds_wuv"])
        wuvb = K.sb(st, "ds_wuvb", [128, 512], BF16)
        K.op("dve", "tensor_scalar", ["ds_wuv", "ds_kvn"], ["ds_wuvb"], out=wuvb[:], in0=wuv[:], scalar1=kvn[:],
             scalar2=None, op0=ALU.mult)
        CKT = K.sb(st, "ds_ckt", [128, T], BF16)
        K.dma("sp", CKT[:], SC["CKT"][s, :, :], ["CKT"], ["ds_ckt"])
        CKA = K.sb(st, "ds_cka", [128, NT, 130], BF16)
        K.op("pool", "memset", [], ["ds_cka"], ap=CKA[:], constant=1.0)
        K.dma("sp", CKA[:, :, 0:128], SC["CK"][s, :, :].rearrange("(k p) r -> p k r", p=128), ["CK"], ["ds_cka"])
        kif = K.sb(st, "ds_kif", [64, T], F32)
        K.dma("act", kif[:], ZF[s, R_KI:R_KI + 64, :], ["ZF"], ["ds_kif"])
        kib = K.sb(st, "ds_kib", [64, T], BF16)
        K.op("dve", "tensor_copy", ["ds_kif"], ["ds_kib"], out=kib[:], in_=kif[:])
        qib = K.sb(st, "ds_qib", [64, 4, 128], BF16)
        zl = K.sb(st, "ds_zl", [128, 128], BF16)
        zb = K.sb(st, "ds_zb", [128, 390], BF16)
        K.op("pool", "memset", [], ["ds_zl"], ap=zl[:], constant=0.0)
        K.op("pool", "memset", [], ["ds_zb"], ap=zb[:], constant=0.0)
        tri01, negtri, pw = CONST["tri01"], CONST["negtri"], CONST["pw"]
        qf = K.sb(st, "ds_qf", [64, 8, 128], F32)
        qb = K.sb(st, "ds_qb", [64, 8, 128], BF16)
        qif = K.sb(st, "ds_qif", [64, 4, 128], F32)
        wif = K.sb(st, "ds_wif", [4, 128], F32)
        wit = K.sb(st, "ds_wit", [128, 4], F32)
        qlat = K.sb(st, "ds_qlat", [128, 1024], BF16)
        isc = K.sb(st, "ds_isc", [128, T], F32)
        junk = K.sb(st, "ds_junk", [128, T], BF16)
        rl = [K.sb(st, "ds_rl%d" % i, [128, 512], F32) for i in range(3)]
        maskb = K.sb(st, "ds_mask", [128, T], BF16)
        col = K.sb(st, "ds_col", [128, 8], F32)
        hk = K.sb(st, "ds_hk", [128, NIT], F32)
        junk2 = K.sb(st, "ds_junk2", [128, T], BF16)
        cola = K.sb(st, "ds_cola", [128, 1], F32)
        mts = [K.sb(st, "ds_mts%d" % i, [128, 128], BF16) for i in range(2)]
        ee = [K.sb(st, "ds_e%d" % i, [128, 4, 128], BF16) for i in range(4)]
        pp = [K.sb(st, "ds_p%d" % i, [128, 4, 128], BF16) for i in range(4)]
        rd = K.sb(st, "ds_rd", [128, 8, 1], F32)
        onb = K.sb(st, "ds_onb", [128, 8, 128], BF16)
        onT = K.sb(st, "ds_onT", [128, 8, 128], BF16)
        ybs = K.sb(st, "ds_ybs", [128, 4, 128], BF16)
        maskbs = [maskb, K.sb(st, "ds_mask1", [128, T], BF16)]
        MN = ["ds_mask", "ds_mask1"]

        def select(qt):
            t0 = qt * 128
            nk = qt + 1
            nkeys = nk * 128
            mb = maskbs[qt % 2]
            mn = MN[qt % 2]
            if qt >= 2:
                K.dma("act", qif[:], ZF[s, R_QI:R_QI + 256, t0:t0 + 128].rearrange("(h p) t -> p h t", p=64), ["ZF"], ["ds_qif"])
                K.op("act", "activation", ["ds_qif"], ["ds_qib"], out=qib[:], in_=qif[:], func=AF.Copy)
                K.dma("act", wif[:], ZF[s, R_WI:R_WI + 4, t0:t0 + 128], ["ZF"], ["ds_wif"])
                K.tr(dps[3][:, 0:4], wif[:], identf[0:4, 0:4], ["ds_wif", "identf"], ["ds_ps3"])
                K.op("dve", "tensor_scalar", ["ds_ps3"], ["ds_wit"], out=wit[:], in0=dps[3][:, 0:4], scalar1=1.0 / 16,
                     scalar2=None, op0=ALU.mult)
                yield
                for kb in range((nkeys + 511) // 512):
                    w = min(512, nkeys - kb * 512)
                    ks = slice(kb * 512, kb * 512 + w)
                    for h in range(4):
                        pb = 2 + (h % 2)
                        K.mm(dps[pb][:, 0:w], qib[:, h, :], kib[:, ks], ["ds_qib", "ds_kib"], ["ds_ps%d" % pb])
                        if h == 0:
                            K.op("dve", "tensor_scalar", ["ds_ps%d" % pb, "ds_wit"], ["ds_isc"], out=isc[:, ks], in0=dps[pb][:, 0:w],
                                 scalar1=0.0, scalar2=wit[:, 0:1], op0=ALU.max, op1=ALU.mult)
                        else:
                            K.op("act", "activation", ["ds_ps%d" % pb], ["ds_rl%d" % (h - 1)], out=rl[h - 1][:, 0:w],
                                 in_=dps[pb][:, 0:w], func=AF.Relu)
                            K.op("dve", "scalar_tensor_tensor", ["ds_rl%d" % (h - 1), "ds_wit", "ds_isc"], ["ds_isc"],
                                 out=isc[:, ks], in0=rl[h - 1][:, 0:w], scalar=wit[:, h:h + 1], in1=isc[:, ks],
                                 op0=ALU.mult, op1=ALU.add)
                    yield
                K.op("dve", "tensor_reduce", ["ds_isc"], ["ds_col"], out=col[:, 0:1], in_=isc[:, 0:nkeys], axis=AX.X, op=ALU.max)
                K.op("dve", "tensor_reduce", ["ds_isc"], ["ds_col"], out=col[:, 1:2], in_=isc[:, 0:nkeys], axis=AX.X, op=ALU.min)
                K.op("dve", "tensor_scalar", ["ds_col"], ["ds_col"], out=col[:, 2:3], in0=col[:, 0:1], scalar1=col[:, 1:2],
                     scalar2=2e-6, op0=ALU.subtract, op1=ALU.add)
                K.op("dve", "tensor_scalar", ["ds_col"], ["ds_col"], out=col[:, 3:4], in0=col[:, 1:2], scalar1=-1e-6,
                     scalar2=None, op0=ALU.add)
                K.op("dve", "tensor_scalar", ["pw", "ds_col"], ["ds_hk"], out=hk[:], in0=pw[:], scalar1=col[:, 2:3],
                     scalar2=None, op0=ALU.mult)
                K.op("dve", "tensor_tensor", ["ds_isc", "negtri"], ["ds_isc"], out=isc[:, t0:t0 + 128], in0=isc[:, t0:t0 + 128],
                     in1=negtri[:], op=ALU.add)
                K.op("dve", "tensor_tensor", ["ds_col", "ds_hk"], ["ds_col", "ds_colm"], out=col[:, 4:5], in0=col[:, 3:4], in1=hk[:, 0:1], op=ALU.add)
                yield
                nd = nkeys
                if nkeys >= 1024 and not os.environ.get("NO_ACTCNT"):
                    nd = ((nkeys * 5 // 8) // 128) * 128
                na = nkeys - nd
                for k in range(NIT):
                    K.op("dve", "tensor_scalar", ["ds_isc", "ds_colm"], ["ds_junk", "ds_col"], out=junk[:, 0:nd],
                         in0=isc[:, 0:nd], scalar1=col[:, 4:5], scalar2=None, op0=ALU.is_ge, op1=ALU.add,
                         accum_out=col[:, 5:6])
                    if na > 0:
                        K.op("act", "activation", ["ds_isc", "ds_colm"], ["ds_junk2", "ds_cola"], out=junk2[:, 0:na], in_=isc[:, nd:nkeys],
                             func=AF.Sign, scale=-1.0, bias=col[:, 4:5], accum_out=cola[:, 0:1])
                        K.op("dve", "scalar_tensor_tensor", ["ds_cola", "ds_col"], ["ds_col"], out=col[:, 5:6], in0=cola[:, 0:1], scalar=-0.5,
                             in1=col[:, 5:6], op0=ALU.mult, op1=ALU.add)
                    K.op("dve", "tensor_scalar", ["ds_col", "ds_hk"], ["ds_col"], out=col[:, 6:7], in0=col[:, 5:6],
                         scalar1=255.5 - 0.5 * na, scalar2=hk[:, k:k + 1], op0=ALU.is_ge, op1=ALU.mult)
                    kn = min(k + 1, NIT - 1)
                    dst = col[:, 4:5] if k < NIT - 1 else col[:, 3:4]
                    K.op("dve", "scalar_tensor_tensor", ["ds_col", "ds_colm", "ds_hk"], ["ds_col", "ds_colm"], out=dst, in0=col[:, 6:7], scalar=col[:, 4:5],
                         in1=hk[:, kn:kn + 1], op0=ALU.add, op1=ALU.subtract)
                    yield
                K.op("dve", "tensor_scalar", ["ds_isc", "ds_col"], [mn], out=mb[:, 0:nkeys], in0=isc[:, 0:nkeys],
                     scalar1=col[:, 3:4], scalar2=None, op0=ALU.is_ge)
            else:
                if qt > 0:
                    K.op("pool", "memset", [], [mn], ap=mb[:, 0:t0], constant=1.0)
                K.op("pool", "tensor_copy", ["tri01"], [mn], out=mb[:, t0:t0 + 128], in_=tri01[:])
            yield

        def attend(qt):
            t0 = qt * 128
            nk = qt + 1
            mb = maskbs[qt % 2]
            mn = MN[qt % 2]
            K.dma("sp", qf[:], ZF[s, R_Q:R_Q + 512, t0:t0 + 128].rearrange("(h p) t -> p h t", p=64), ["ZF"], ["ds_qf"])
            K.op("pool", "tensor_copy", ["ds_qf"], ["ds_qb"], out=qb[:], in_=qf[:])
            for h in range(8):
                K.mm(dps[h // 4][:, (h % 4) * 128:(h % 4 + 1) * 128], wukT[:, h, :], qb[:, h, :], ["ds_wukT", "ds_qb"],
                     ["ds_ps%d" % (h // 4)])
            for j in range(2):
                K.op("act", "activation", ["ds_ps%d" % j, "ds_kvn8"], ["ds_qlat"], out=qlat[:, j * 512:(j + 1) * 512],
                     in_=dps[j][:], func=AF.Copy, scale=kvn8[:, 0:1])
            for bq in range(3):
                K.mm(Ob[bq][:].rearrange("p a b -> p (a b)"), zl[:], zb[:], ["ds_zl", "ds_zb"], ["ds_o%d" % bq], start=True,
                     stop=False, skip_group_check=True)
            yield
            def front(kt):
                par = kt % 2
                K.tr(MT[:, 0, :], mb[:, kt * 128:(kt + 1) * 128], identb[:], [mn, "identb"], ["ds_mt0", "ds_mt1", "ds_mtall"])
                K.op("act", "activation", ["ds_mt0", "ds_mt1", "ds_mtall"], ["ds_mts%d" % par], out=mts[par][:], in_=MT[:, 0, :], func=AF.Copy)
                for j in range(2):
                    ej = 2 * par + j
                    K.mm(dps[j][:], CKT[:, kt * 128:(kt + 1) * 128], qlat[:, j * 512:(j + 1) * 512], ["ds_ckt", "ds_qlat"],
                         ["ds_ps%d" % j])
                    K.op("act", "activation", ["ds_ps%d" % j], ["ds_e%d" % ej], out=ee[ej][:],
                         in_=dps[j][:].rearrange("p (a b) -> p a b", a=4), func=AF.Exp)
            front(0)
            for kt in range(nk):
                par = kt % 2
                mtb = "ds_mt%d" % par
                if kt + 1 < nk:
                    front(kt + 1)
                for j in range(2):
                    ej = 2 * par + j
                    K.op("dve", "tensor_tensor", ["ds_e%d" % ej, "ds_mts%d" % par], ["ds_p%d" % ej], out=pp[ej][:], in0=ee[ej][:],
                         in1=mts[par][:].unsqueeze(1).to_broadcast([128, 4, 128]), op=ALU.mult)
                for j in range(2):
                    ej = 2 * par + j
                    for hh in range(4):
                        h = 4 * j + hh
                        K.mm(Ob[h // 3][:, h % 3, 0:129], pp[ej][:, hh, :], CKA[:, kt, 0:129], ["ds_p%d" % ej, "ds_cka"],
                             ["ds_o%d" % (h // 3)], start=False, stop=(kt == nk - 1), skip_group_check=True)
                yield
            for bq in range(3):
                nh = 3 if bq < 2 else 2
                K.op("dve", "reciprocal", ["ds_o%d" % bq], ["ds_rd"], out=rd[:, 3 * bq:3 * bq + nh, :], in_=Ob[bq][:, 0:nh, 128:129])
                K.op("dve", "tensor_tensor", ["ds_o%d" % bq, "ds_rd"], ["ds_onb"], out=onb[:, 3 * bq:3 * bq + nh, :],
                     in0=Ob[bq][:, 0:nh, 0:128], in1=rd[:, 3 * bq:3 * bq + nh, :].to_broadcast([128, nh, 128]), op=ALU.mult)
            for h in range(8):
                K.tr(MT[:, h, :], onb[:, h, :], identb[:], ["ds_onb", "identb"], ["ds_mt0", "ds_mt1", "ds_mtall"])
            K.op("act", "activation", ["ds_mt0", "ds_mt1", "ds_mtall"], ["ds_onT"], out=onT[:], in_=MT[:], func=AF.Copy)
            for h in range(8):
                K.mm(dps[0][(h % 2) * 64:(h % 2 + 1) * 64, (h // 2) * 128:(h // 2 + 1) * 128], wuvb[:, h * 64:(h + 1) * 64],
                     onT[:, h, :], ["ds_wuvb", "ds_onT"], ["ds_ps0"])
            K.op("dve", "tensor_copy", ["ds_ps0"], ["ds_ybs"], out=ybs[:], in_=dps[0][:].rearrange("p (a b) -> p a b", a=4))
            K.dma("sp", SC["YB"][s, :, t0:t0 + 128].rearrange("(c p) t -> p c t", p=128), ybs[:], ["ds_ybs"], ["YB"])
            yield

        xg = extra(st) if extra is not None else None
        for step in range(NT + 1):
            gens = []
            if step >= 1:
                gens.append(attend(step - 1))
            if step < NT:
                gens.append(select(step))
            if xg is not None:
                try:
                    next(xg)
                except StopIteration:
                    xg = None
            while gens:
                for g in list(gens):
                    try:
                        next(g)
                    except StopIteration:
                        gens.remove(g)
        if xg is not None:
            for _ in xg:
                pass


class _Stop(Exception):
    pass


def phase_rwkv(K, s, T, Wd, SC, CONST):
    _phase_rwkv(K, s, T, Wd, SC, CONST)
    K.S.muted = False


def _phase_rwkv(K, s, T, Wd, SC, CONST):
    nc = K.nc
    TBK = 256
    NCH = TBK // 64
    NBK = T // TBK
    ZF = SC["ZF"]
    identf = CONST["identf"]
    bo, bo64, maskq, lowm, resetm = CONST["bo"], CONST["bo64"], CONST["maskq"], CONST["lowm"], CONST["resetm"]
    with ExitStack() as st:
        rp = [K.ps(st, "rk_p%d" % i, [128, 512], F32) for i in range(8)]
        RP = ["rk_p%d" % i for i in range(8)]

        def colload(tag, ap512, n=4):
            t = K.sb(st, tag, [128, n], F32)
            K.dma("sp", t[:], ap512.rearrange("o (c p) -> p (o c)", p=128), [], [tag], allow_slow_non_contiguous=True)
            return t
        mu = colload("rk_mu", Wd["shift_mu"], 14)
        w0c = colload("rk_w0c", Wd["rw_w0"])
        a0c = colload("rk_a0c", Wd["rw_a0"])
        kkc = colload("rk_kkc", Wd["rw_k_k"])
        kac = colload("rk_kac", Wd["rw_k_a"])
        rkc = colload("rk_rkc", Wd["rw_r_k"].rearrange("o h d -> o (h d)"))
        lnw = colload("rk_lnw", Wd["rw_ln_w"])
        lnb = colload("rk_lnb", Wd["rw_ln_b"])
        w2a2 = K.sb(st, "rk_w2a2", [128, 512], F32)
        K.dma("sp", w2a2[0:64, :], Wd["rw_w2"][0], [], ["rk_w2a2"])
        K.dma("sp", w2a2[64:128, :], Wd["rw_a2"][0], [], ["rk_w2a2"])
        g2 = K.sb(st, "rk_g2", [128, 512], F32)
        K.dma("sp", g2[:], Wd["rw_g2"][0], [], ["rk_g2"])
        epsg = K.sb(st, "rk_epsg", [128, 1], F32)
        K.op("dve", "memset", [], ["rk_epsg"], ap=epsg[:], constant=64e-5)
        zin = K.sb(st, "rk_zin", [128, 14, TBK + 1], F32)
        zs = K.sb(st, "rk_zs", [128, 14, TBK], F32)
        tw = K.sb(st, "rk_tw", [128, TBK], F32)
        sg = K.sb(st, "rk_sg", [128, TBK], F32)

        def t4(tag):
            return K.sb(st, tag, [128, 4, TBK], F32)
        lw, aa, gg, LL, eL, enL, eLm, kk, t1, kp, bb, bon, Yb = [t4("rk_" + n) for n in
            ("lw", "aa", "gg", "LL", "eL", "enL", "eLm", "kk", "t1", "kp", "bb", "bon", "Yb")]
        QR = K.sb(st, "rk_QR", [128, 4, NCH, 2, 64], F32)
        KB = K.sb(st, "rk_KB", [128, 4, NCH, 2, 64], F32)
        gC = K.sb(st, "rk_gC", [128, 4, NCH], F32)
        M = K.sb(st, "rk_M", [128, 4, 64], F32)
        K.op("dve", "memset", [], ["rk_M"], ap=M[:], constant=0.0)
        KBTs = [K.sb(st, "rk_KBT%d" % i, [128, 4, 128], F32) for i in range(2)]
        VTs = [K.sb(st, "rk_VT%d" % i, [64, 4, 128], F32) for i in range(2)]
        ATs = [K.sb(st, "rk_AT%d" % i, [128, 8, 128], F32) for i in range(2)]
        DDT = BF16 if os.environ.get("RW_BF16", "1") == "1" else F32
        Am = [K.sb(st, "rk_Am%d" % i, [128, 8, 64], DDT) for i in range(2)]
        Bm = [K.sb(st, "rk_Bm%d" % i, [128, 8, 64], DDT) for i in range(2)]
        Pm = [K.sb(st, "rk_Pm%d" % i, [128, 8, 64], DDT) for i in range(2)]
        PmFs = [K.sb(st, "rk_PmF%d" % i, [128, 8, 64], F32) for i in range(2)]
        Rs = K.sb(st, "rk_Rs", [128, 512], F32)
        Us = K.sb(st, "rk_Us", [128, 512], F32)
        yab = K.sb(st, "rk_yab", [128, 4, TBK], BF16)
        H = slice(64, 128)

        def v4(t):
            return t[:].rearrange("p c (n t) -> p c n t", t=64)

        def bc(colt, n=4, w=TBK):
            return colt[:].unsqueeze(2).to_broadcast([128, n, w])

        RS = float(os.environ.get("RSTOP", "99"))

        def chk(k):
            if RS <= k:
                K.S.muted = True

        for tb in range(NBK):
            t0 = tb * TBK
            if tb == 0:
                K.op("dve", "memset", [], ["rk_zin"], ap=zin[:, :, 0:1], constant=0.0)
                K.dma("sp", zin[:, :, 1:TBK + 1], ZF[s, 0:1792, 0:TBK].rearrange("(c p) t -> p c t", p=128), ["ZF"], ["rk_zin"])
            else:
                K.dma("sp", zin[:, :, :], ZF[s, 0:1792, t0 - 1:t0 + TBK].rearrange("(c p) t -> p c t", p=128), ["ZF"], ["rk_zin"])
            K.op("dve", "tensor_tensor", ["rk_zin"], ["rk_zs"], out=zs[:], in0=zin[:, :, 0:TBK], in1=zin[:, :, 1:TBK + 1], op=ALU.subtract)
            for c14 in range(14):
                K.op("dve", "scalar_tensor_tensor", ["rk_zs", "rk_mu", "rk_zin"], ["rk_zs"], out=zs[:, c14, :], in0=zs[:, c14, :], scalar=mu[:, c14:c14 + 1],
                     in1=zin[:, c14, 1:TBK + 1], op0=ALU.mult, op1=ALU.add)
            chk(1)
            r_, k_, v_ = zs[:, 0:4, :], zs[:, 4:8, :], zs[:, 8:12, :]
            K.op("act", "activation", ["rk_zs"], ["rk_tw"], out=tw[0:64, :], in_=zs[0:64, 12, :], func=AF.Tanh)
            K.op("act", "activation", ["rk_zs"], ["rk_sg"], out=sg[:], in_=zs[:, 13, :], func=AF.Sigmoid)
            for cc in range(4):
                cs = slice(cc * 128, (cc + 1) * 128)
                K.mm(rp[0][:, 0:TBK], w2a2[0:64, cs], tw[0:64, :], ["rk_w2a2", "rk_tw"], [RP[0]])
                K.op("act", "activation", [RP[0], "rk_w0c"], ["rk_lw"], out=lw[:, cc, :], in_=rp[0][:, 0:TBK], func=AF.Sigmoid, bias=w0c[:, cc:cc + 1])
                K.mm(rp[1][:, 0:TBK], w2a2[H, cs], zs[H, 12, :], ["rk_w2a2", "rk_zs"], [RP[1]])
                K.op("act", "activation", [RP[1], "rk_a0c"], ["rk_aa"], out=aa[:, cc, :], in_=rp[1][:, 0:TBK], func=AF.Sigmoid, bias=a0c[:, cc:cc + 1])
                K.mm(rp[2][:, 0:TBK], g2[:, cs], sg[:], ["rk_g2", "rk_sg"], [RP[2]])
                K.op("dve", "tensor_copy", [RP[2]], ["rk_gg"], out=gg[:, cc, :], in_=rp[2][:, 0:TBK])
            chk(2)
            K.op("dve", "tensor_scalar", ["rk_lw"], ["rk_lw"], out=lw[:], in0=lw[:], scalar1=-0.6065306597126334, scalar2=None, op0=ALU.mult)
            for cc in range(4):
                K.op("dve", "tensor_tensor_scan", ["rk_lw", "resetm"], ["rk_LL"], out=LL[:, cc, :], data0=resetm[:], data1=lw[:, cc, :],
                     initial=0.0, op0=ALU.mult, op1=ALU.add)
            K.op("act", "activation", ["rk_LL"], ["rk_eL"], out=eL[:], in_=LL[:], func=AF.Exp)
            K.op("act", "activation", ["rk_LL"], ["rk_enL"], out=enL[:], in_=LL[:], func=AF.Exp, scale=-1.0)
            K.op("pool", "tensor_tensor", ["rk_LL", "rk_lw"], ["rk_t1"], out=t1[:], in0=LL[:], in1=lw[:], op=ALU.subtract)
            K.op("act", "activation", ["rk_t1"], ["rk_eLm"], out=eLm[:], in_=t1[:], func=AF.Exp)
            K.op("dve", "tensor_tensor", ["rk_zs", "rk_kkc"], ["rk_kk"], out=kk[:], in0=k_, in1=bc(kkc), op=ALU.mult)
            K.op("pool", "tensor_tensor", ["rk_kk"], ["rk_t1"], out=t1[:], in0=kk[:], in1=kk[:], op=ALU.mult)
            for cc in range(4):
                K.mm(rp[cc % 4][:, 0:TBK], bo[:], t1[:, cc, :], ["bo", "rk_t1"], [RP[cc % 4]])
                K.op("act", "activation", [RP[cc % 4]], ["rk_kp"], out=kp[:, cc, :], in_=rp[cc % 4][:, 0:TBK], func=AF.Sqrt)
            K.op("dve", "tensor_scalar", ["rk_kp"], ["rk_kp"], out=kp[:], in0=kp[:], scalar1=1e-12, scalar2=None, op0=ALU.max)
            K.op("dve", "reciprocal", ["rk_kp"], ["rk_kp"], out=kp[:], in_=kp[:])
            K.op("dve", "tensor_tensor", ["rk_kk", "rk_kp"], ["rk_kk"], out=kk[:], in0=kk[:], in1=kp[:], op=ALU.mult)
            for cc in range(4):
                K.op("dve", "tensor_scalar", ["rk_aa", "rk_kac"], ["rk_t1"], out=t1[:, cc, :], in0=aa[:, cc, :], scalar1=-1.0, scalar2=kac[:, cc:cc + 1],
                     op0=ALU.add, op1=ALU.mult)
            K.op("dve", "scalar_tensor_tensor", ["rk_t1", "rk_zs"], ["rk_kp"], out=kp[:], in0=t1[:], scalar=1.0, in1=k_, op0=ALU.add, op1=ALU.mult)
            K.op("pool", "tensor_tensor", ["rk_kk", "rk_aa"], ["rk_bb"], out=bb[:], in0=kk[:], in1=aa[:], op=ALU.mult)
            K.op("dve", "tensor_tensor", ["rk_zs", "rk_eL"], ["rk_QR"], out=QR[:, :, :, 1, :], in0=r_.rearrange("p c (n t) -> p c n t", t=64), in1=v4(eL), op=ALU.mult)
            K.op("pool", "tensor_tensor", ["rk_kk", "rk_eLm"], ["rk_QR"], out=QR[:, :, :, 0, :], in0=v4(kk), in1=v4(eLm), op=ALU.mult)
            K.op("dve", "tensor_tensor", ["rk_kp", "rk_enL"], ["rk_KB"], out=KB[:, :, :, 0, :], in0=v4(kp), in1=v4(enL), op=ALU.mult)
            K.op("pool", "tensor_tensor", ["rk_bb", "rk_enL"], ["rk_KB"], out=KB[:, :, :, 1, :], in0=v4(bb), in1=v4(enL), op=ALU.mult)
            K.op("dve", "tensor_copy", ["rk_eL"], ["rk_gC"], out=gC[:], in_=v4(eL)[:, :, :, 63])
            K.op("pool", "tensor_tensor", ["rk_zs", "rk_kp"], ["rk_t1"], out=t1[:], in0=r_, in1=kp[:], op=ALU.mult)
            K.op("pool", "tensor_tensor", ["rk_t1", "rk_rkc"], ["rk_t1"], out=t1[:], in0=t1[:], in1=bc(rkc), op=ALU.mult)
            for cc in range(4):
                K.mm(rp[cc % 4][:, 0:TBK], bo[:], t1[:, cc, :], ["bo", "rk_t1"], [RP[cc % 4]])
                K.op("dve", "tensor_tensor", [RP[cc % 4], "rk_zs"], ["rk_bon"], out=bon[:, cc, :], in0=rp[cc % 4][:, 0:TBK], in1=zs[:, 8 + cc, :], op=ALU.mult)
            chk(3)
            def ev(t, par):
                return t.rearrange("p (a two) b -> p a two b", two=2)[:, :, par, :]

            def pre(c):
                q = c % 2
                KBT, VT, AT, PmF = KBTs[q], VTs[q], ATs[q], PmFs[q]
                nKBT, nVT, nAT, nPmF = "rk_KBT%d" % q, "rk_VT%d" % q, "rk_AT%d" % q, "rk_PmF%d" % q
                for cc in range(4):
                    K.tr(rp[0][:, cc * 128:(cc + 1) * 128], KB[:, cc, c, :, :].rearrange("p a b -> p (a b)"), identf[:], ["rk_KB", "identf"], [RP[0]])
                    K.tr(rp[1][0:64, cc * 128:(cc + 1) * 128], zs[:, 8 + cc, c * 64:(c + 1) * 64], identf[:], ["rk_zs", "identf"], [RP[1]])
                K.op("act", "activation", [RP[0]], [nKBT], out=KBT[:].rearrange("p a b -> p (a b)"), in_=rp[0][:], func=AF.Copy)
                K.op("dve", "tensor_copy", [RP[1]], [nVT], out=VT[:].rearrange("p a b -> p (a b)"), in_=rp[1][0:64, :])
                yield
                for h in range(8):
                    cc, h2 = h // 2, h % 2
                    rows = slice(h2 * 64, (h2 + 1) * 64)
                    K.mm(rp[2 + h2][:, cc * 128:(cc + 1) * 128], KB[rows, cc, c, :, :].rearrange("p a b -> p (a b)"),
                         QR[rows, cc, c, :, :].rearrange("p a b -> p (a b)"), ["rk_KB", "rk_QR"], [RP[2 + h2]])
                    K.mm(rp[h2][H, cc * 64:(cc + 1) * 64], QR[rows, cc, c, 0, :], KB[rows, cc, c, 1, :], ["rk_QR", "rk_KB"], [RP[h2]])
                for h2 in range(2):
                    K.op("dve", "tensor_tensor", [RP[2 + h2], "maskq"], [nAT], out=ev(AT[:], h2),
                         in0=rp[2 + h2][:].rearrange("p (a b) -> p a b", a=4), in1=maskq[:].unsqueeze(1).to_broadcast([128, 4, 128]), op=ALU.mult)
                    K.op("dve", "tensor_tensor", [RP[h2], "lowm"], ["rk_Bm0"], out=ev(Bm[0][H, :, :], h2),
                         in0=rp[h2][H, 0:256].rearrange("p (a b) -> p a b", a=4), in1=lowm[H, :].unsqueeze(1).to_broadcast([64, 4, 64]), op=ALU.mult)
                K.op("act", "activation", [nAT], ["rk_Am0"], out=Am[0][H, :, :], in_=AT[H, :, 0:64], func=AF.Copy)
                K.op("dve", "tensor_tensor", ["identf", nAT], ["rk_Pm0"], out=Pm[0][H, :, :],
                     in0=identf[H, 64:128].unsqueeze(1).to_broadcast([64, 8, 64]), in1=AT[H, :, 0:64], op=ALU.subtract)
                yield
                for lvl in range(5):
                    ci, ni = lvl % 2, (lvl + 1) % 2
                    An, Bn, Pn = "rk_Am%d" % ni, "rk_Bm%d" % ni, "rk_Pm%d" % ni
                    Ac, Bc, Pc = "rk_Am%d" % ci, "rk_Bm%d" % ci, "rk_Pm%d" % ci
                    for h in range(8):
                        hs = slice(h * 64, (h + 1) * 64)
                        if lvl < 4:
                            K.mm(rp[2][H, hs], Bm[ci][H, h, :], Am[ci][H, h, :], [Ac, Bc], [RP[2]])
                        K.mm(rp[3][H, hs], Am[ci][H, h, :], Bm[ci][H, h, :], [Ac, Bc], [RP[3]])
                    if lvl < 4:
                        K.op("act", "activation", [RP[2]], [An], out=Am[ni][H, :, :], in_=rp[2][H, :].rearrange("p (a b) -> p a b", a=8), func=AF.Copy)
                    K.op("dve", "tensor_copy", [RP[3]], [Bn], out=Bm[ni][H, :, :], in_=rp[3][H, :].rearrange("p (a b) -> p a b", a=8))
                    yield
                    for h in range(8):
                        hs = slice(h * 64, (h + 1) * 64)
                        K.mm(rp[0][H, hs], Bm[ni][H, h, :], Pm[ci][H, h, :], [Bn, Pc], [RP[0]])
                    if lvl < 4:
                        K.op("dve", "tensor_tensor", [RP[0], Pc], [Pn], out=Pm[ni][H, :, :], in0=rp[0][H, :].rearrange("p (a b) -> p a b", a=8),
                             in1=Pm[ci][H, :, :], op=ALU.add)
                    else:
                        K.op("dve", "tensor_tensor", [RP[0], Pc], [nPmF], out=PmF[H, :, :], in0=rp[0][H, :].rearrange("p (a b) -> p a b", a=8),
                             in1=Pm[ci][H, :, :], op=ALU.add)
                    yield

            def post(c):
                q = c % 2
                KBT, VT, AT, PF = KBTs[q], VTs[q], ATs[q], PmFs[q]
                nKBT, nVT, nAT, PFn = "rk_KBT%d" % q, "rk_VT%d" % q, "rk_AT%d" % q, "rk_PmF%d" % q
                Rs3 = Rs[H, :].rearrange("p (a b) -> p a b", a=8)
                for h in range(8):
                    cc, h2 = h // 2, h % 2
                    rows = slice(h2 * 64, (h2 + 1) * 64)
                    hs = slice(h * 64, (h + 1) * 64)
                    K.mm(rp[6 + h2][H, cc * 64:(cc + 1) * 64], QR[rows, cc, c, 0, :], M[rows, cc, :], ["rk_QR", "rk_M"], [RP[6 + h2]])
                    K.mm(rp[4][H, hs], AT[0:64, h, 0:64], VT[0:64, cc, h2 * 64:(h2 + 1) * 64], [nAT, nVT], [RP[4]])
                for h2 in range(2):
                    K.op("act", "activation", [RP[6 + h2]], ["rk_Rs"], out=ev(Rs3, h2), in_=rp[6 + h2][H, 0:256].rearrange("p (a b) -> p a b", a=4), func=AF.Copy)
                K.op("dve", "tensor_tensor", [RP[4], "rk_Rs"], ["rk_Rs"], out=Rs[H, :], in0=rp[4][H, :], in1=Rs[H, :], op=ALU.add)
                yield
                for h in range(8):
                    hs = slice(h * 64, (h + 1) * 64)
                    K.mm(rp[5][H, hs], PF[H, h, :], Rs[H, hs], [PFn, "rk_Rs"], [RP[5]])
                K.op("act", "activation", [RP[5]], ["rk_Us"], out=Us[H, :], in_=rp[5][H, :], func=AF.Copy, scale=-1.0)
                yield
                for h in range(8):
                    cc, h2 = h // 2, h % 2
                    rows = slice(h2 * 64, (h2 + 1) * 64)
                    hs = slice(h * 64, (h + 1) * 64)
                    ys = slice(cc * 64, (cc + 1) * 64)
                    K.mm(rp[6 + h2][rows, ys], M[rows, cc, :], QR[rows, cc, c, 1, :], ["rk_M", "rk_QR"], [RP[6 + h2]])
                    K.mm(rp[4][rows, ys], VT[0:64, cc, h2 * 64:(h2 + 1) * 64], AT[0:64, h, 64:128], [nVT, nAT], [RP[4]])
                    K.mm(rp[5][rows, ys], Us[H, hs], AT[H, h, 64:128], ["rk_Us", nAT], [RP[5]])
                for h2 in range(2):
                    rows = slice(h2 * 64, (h2 + 1) * 64)
                    K.op("act", "activation", [RP[6 + h2]], ["rk_Yb"], out=Yb[rows, :, c * 64:(c + 1) * 64],
                         in_=rp[6 + h2][rows, 0:256].rearrange("p (a b) -> p a b", a=4), func=AF.Copy)
                yv = Yb[:, :, c * 64:(c + 1) * 64]
                K.op("dve", "tensor_tensor", [RP[4], "rk_Yb"], ["rk_Yb"], out=yv, in0=rp[4][:, 0:256].rearrange("p (a b) -> p a b", a=4), in1=yv, op=ALU.add)
                K.op("dve", "tensor_tensor", [RP[5], "rk_Yb"], ["rk_Yb"], out=yv, in0=rp[5][:, 0:256].rearrange("p (a b) -> p a b", a=4), in1=yv, op=ALU.add)
                yield
                for cc in range(4):
                    for h2 in range(2):
                        h = 2 * cc + h2
                        rows = slice(h2 * 64, (h2 + 1) * 64)
                        hs = slice(h * 64, (h + 1) * 64)
                        K.mm(rp[4][rows, cc * 64:(cc + 1) * 64], KBT[0:64, cc, rows], VT[0:64, cc, rows], [nKBT, nVT], [RP[4]])
                        K.mm(rp[5][rows, cc * 64:(cc + 1) * 64], KBT[H, cc, rows], Us[H, hs], [nKBT, "rk_Us"], [RP[5]])
                K.op("dve", "tensor_tensor", [RP[4], "rk_M"], ["rk_M"], out=M[:], in0=rp[4][:, 0:256].rearrange("p (a b) -> p a b", a=4), in1=M[:], op=ALU.add)
                K.op("dve", "tensor_tensor", [RP[5], "rk_M"], ["rk_M"], out=M[:], in0=rp[5][:, 0:256].rearrange("p (a b) -> p a b", a=4), in1=M[:], op=ALU.add)
                K.op("dve", "tensor_tensor", ["rk_M", "rk_gC"], ["rk_M"], out=M[:], in0=M[:],
                     in1=gC[:, :, c:c + 1].to_broadcast([128, 4, 64]), op=ALU.mult)
                yield

            for step in range(NCH + 1):
                gens = []
                if step >= 1:
                    gens.append(post(step - 1))
                if step < NCH:
                    gens.append(pre(step))
                while gens:
                    for g in list(gens):
                        try:
                            next(g)
                        except StopIteration:
                            gens.remove(g)
            chk(7)
            for cc in range(4):
                K.mm(rp[0][:, 0:TBK], bo64[:], Yb[:, cc, :], ["bo64", "rk_Yb"], [RP[0]])
                K.op("dve", "tensor_tensor", ["rk_Yb", RP[0]], ["rk_t1"], out=t1[:, cc, :], in0=Yb[:, cc, :], in1=rp[0][:, 0:TBK], op=ALU.subtract)
                K.op("pool", "tensor_tensor", ["rk_t1"], ["rk_kk"], out=kk[:, cc, :], in0=t1[:, cc, :], in1=t1[:, cc, :], op=ALU.mult)
                K.mm(rp[1][:, 0:TBK], bo64[:], kk[:, cc, :], ["bo64", "rk_kk"], [RP[1]])
                K.op("act", "activation", [RP[1], "rk_epsg"], ["rk_kp"], out=kp[:, cc, :], in_=rp[1][:, 0:TBK], func=AF.Sqrt, bias=epsg[:])
            K.op("dve", "reciprocal", ["rk_kp"], ["rk_kp"], out=kp[:], in_=kp[:])
            K.op("dve", "tensor_tensor", ["rk_t1", "rk_kp"], ["rk_t1"], out=t1[:], in0=t1[:], in1=kp[:], op=ALU.mult)
            K.op("pool", "tensor_tensor", ["rk_t1", "rk_lnw"], ["rk_t1"], out=t1[:], in0=t1[:], in1=bc(lnw), op=ALU.mult)
            K.op("pool", "tensor_tensor", ["rk_t1", "rk_lnb"], ["rk_t1"], out=t1[:], in0=t1[:], in1=bc(lnb), op=ALU.add)
            K.op("dve", "tensor_tensor", ["rk_t1", "rk_bon"], ["rk_t1"], out=t1[:], in0=t1[:], in1=bon[:], op=ALU.add)
            K.op("dve", "tensor_tensor", ["rk_t1", "rk_gg"], ["rk_yab"], out=yab[:], in0=t1[:], in1=gg[:], op=ALU.mult)
            K.dma("sp", SC["YA"][s, :, t0:t0 + TBK].rearrange("(c p) t -> p c t", p=128), yab[:], ["rk_yab"], ["YA"])


def norm_T(K, tag, src_tile, src_name, xn, ss, junk, pst, dstT, col0, identb, eps):
    K.op("act", "activation", [src_name], [tag + "junk", tag + "ss"], out=junk[:], in_=src_tile, func=AF.Square, accum_out=ss[:])
    K.op("act", "activation", [tag + "ss", "eps6"], [tag + "ss"], out=ss[:], in_=ss[:], func=AF.Sqrt, scale=1.0 / D, bias=eps[:])
    K.op("dve", "reciprocal", [tag + "ss"], [tag + "ss"], out=ss[:], in_=ss[:])
    K.op("dve", "tensor_scalar", [src_name, tag + "ss"], [tag + "xn"], out=xn[:], in0=src_tile, scalar1=ss[:], scalar2=None, op0=ALU.mult)
    for c in range(8):
        K.tr(pst[:, c, :], xn[:, c * 128:(c + 1) * 128], identb[:], [tag + "xn", "identb"], [tag + "pst"])
    K.op("act", "activation", [tag + "pst"], [dstT[1]], out=dstT[0][:, :, col0:col0 + 128], in_=pst[:], func=AF.Copy)


def phase_mix(K, s, T, X, Wd, SC, CONST):
    ZF = SC["ZF"]
    with ExitStack() as st:
        wpa = load_cast(K, st, "mx_wpa", Wd["w_proj_a"][0], 512, 1024)
        wpb = load_cast(K, st, "mx_wpb", Wd["w_proj_b"][0], 512, 1024)
        wout = load_cast(K, st, "mx_wout", Wd["w_out"][0], 1024, 1024)
        ps = [K.ps(st, "mx_ps%d" % i, [128, 512], F32) for i in range(4)]
        ya = K.sb(st, "mx_ya", [128, 4, 512], BF16)
        yb = K.sb(st, "mx_yb", [128, 4, 512], BF16)
        G = K.sb(st, "mx_G", [128, 16, 512], F32)
        ta = K.sb(st, "mx_ta", [128, 512], F32)
        tb_ = K.sb(st, "mx_tb", [128, 512], F32)
        mixT = K.sb(st, "mx_mixT", [128, 8, 512], BF16)
        xt = [K.sb(st, "mx_xt%d" % i, [128, D], F32) for i in range(2)]
        for tb in range(T // 512):
            t0 = tb * 512
            K.dma("sp", ya[:], SC["YA"][s, :, t0:t0 + 512].rearrange("(c p) t -> p c t", p=128), ["YA"], ["mx_ya"])
            K.dma("act", yb[:], SC["YB"][s, :, t0:t0 + 512].rearrange("(c p) t -> p c t", p=128), ["YB"], ["mx_yb"])
            K.dma("sp", G[:], ZF[s, R_G:R_G + 2048, t0:t0 + 512].rearrange("(c p) t -> p c t", p=128), ["ZF"], ["mx_G"])
            for cc in range(8):
                cs = slice(cc * 128, (cc + 1) * 128)
                for k in range(4):
                    K.mm(ps[0][:], wpa[:, k, cs], ya[:, k, :], ["mx_wpa", "mx_ya"], ["mx_ps0"], start=(k == 0), stop=(k == 3))
                for k in range(4):
                    K.mm(ps[1][:], wpb[:, k, cs], yb[:, k, :], ["mx_wpb", "mx_yb"], ["mx_ps1"], start=(k == 0), stop=(k == 3))
                K.op("dve", "tensor_tensor", ["mx_ps0", "mx_G"], ["mx_ta"], out=ta[:], in0=ps[0][:], in1=G[:, cc, :], op=ALU.mult)
                K.op("dve", "tensor_tensor", ["mx_ps1", "mx_G"], ["mx_tb"], out=tb_[:], in0=ps[1][:], in1=G[:, 8 + cc, :], op=ALU.mult)
                K.op("pool", "tensor_tensor", ["mx_ta", "mx_tb"], ["mx_mixT"], out=mixT[:, cc, :], in0=ta[:], in1=tb_[:], op=ALU.add)
            for tt in range(4):
                i = tt % 2
                r0 = s * T + t0 + tt * 128
                K.dma("act", xt[i][:], X[r0:r0 + 128, :], [], ["mx_xt%d" % i])
                for half in range(2):
                    pj = 2 + half
                    for k in range(8):
                        K.mm(ps[pj][:], mixT[:, k, tt * 128:(tt + 1) * 128], wout[:, k, half * 512:(half + 1) * 512], ["mx_mixT", "mx_wout"],
                             ["mx_ps%d" % pj], start=(k == 0), stop=(k == 7))
                    K.op("dve", "tensor_tensor", ["mx_ps%d" % pj, "mx_xt%d" % i], ["mx_xt%d" % i], out=xt[i][:, half * 512:(half + 1) * 512],
                         in0=ps[pj][:], in1=xt[i][:, half * 512:(half + 1) * 512], op=ALU.add)
                K.dma("sp", SC["H1"][r0:r0 + 128, :], xt[i][:], ["mx_xt%d" % i], ["H1"])


def colvec(K, st, tag, ap, n=8):
    t = K.sb(st, tag, [128, n], F32)
    K.dma("sp", t[:], ap.rearrange("o (c p) -> p (o c)", p=128), [], [tag], allow_slow_non_contiguous=True)
    return t


def phase_cross(K, s, T, MEM, Wd, SC, CONST):
    identb = CONST["identb"]
    with ExitStack() as st:
        nrc = colvec(K, st, "cx_nrc", Wd["norm_cross"])
        nrm = colvec(K, st, "cx_nrm", Wd["norm_mem"])
        wcq = load_cast(K, st, "cx_wcq", Wd["w_cq"][0], 1024, 1024, scale_col=(nrc, "cx_nrc"))
        wckv = load_cast(K, st, "cx_wckv", Wd["w_ckv"][0], 1024, 2048, scale_col=(nrm, "cx_nrm"))
        wco = load_cast(K, st, "cx_wco", Wd["w_co"][0], 1024, 1024)
        ps = [K.ps(st, "cx_ps%d" % i, [128, 512], F32) for i in range(6)]
        pst = K.ps(st, "cx_pst", [128, 8, 128], BF16)
        ht = K.sb(st, "cx_ht", [128, 4, D], F32)
        xn = K.sb(st, "cx_xn", [128, D], BF16)
        ss = K.sb(st, "cx_ss", [128, 1], F32)
        junk = K.sb(st, "cx_junk", [128, D], F32)
        memT = K.sb(st, "cx_memT", [128, 8, 256], BF16)
        ones = K.sb(st, "cx_ones", [128, 128], BF16)
        K.op("pool", "memset", [], ["cx_ones"], ap=ones[:], constant=1.0)
        for mt in range(2):
            K.dma("sp", ht[:, 0, :], MEM[s * 256 + mt * 128: s * 256 + (mt + 1) * 128, :], [], ["cx_ht0"])
            norm_T(K, "cx_", ht[:, 0, :], "cx_ht0", xn, ss, junk, pst, (memT, "cx_memT"), mt * 128, identb, CONST["eps6"])
        kTs = K.sb(st, "cx_kTs", [128, 8, 256], BF16)
        vS = K.sb(st, "cx_vS", [128, 2, 1024], BF16)
        for j in range(8):
            for k in range(8):
                K.mm(ps[0][:, 0:256], wckv[:, k, j * 128:(j + 1) * 128], memT[:, k, :], ["cx_wckv", "cx_memT"], ["cx_ps0"], start=(k == 0), stop=(k == 7))
            K.op("dve", "tensor_copy", ["cx_ps0"], ["cx_kTs"], out=kTs[:, j, :], in_=ps[0][:, 0:256])
        for mt in range(2):
            for half in range(2):
                for k in range(8):
                    K.mm(ps[1][:], memT[:, k, mt * 128:(mt + 1) * 128], wckv[:, k, 1024 + half * 512:1024 + (half + 1) * 512], ["cx_wckv", "cx_memT"],
                         ["cx_ps1"], start=(k == 0), stop=(k == 7))
                K.op("dve", "tensor_copy", ["cx_ps1"], ["cx_vS"], out=vS[:, mt, half * 512:(half + 1) * 512], in_=ps[1][:])
        hnT = K.sb(st, "cx_hnT", [128, 8, 512], BF16)
        qTs = K.sb(st, "cx_qTs", [128, 8, 512], BF16)
        pT = [K.sb(st, "cx_pT%d" % i, [128, 512], BF16) for i in range(2)]
        rden = K.sb(st, "cx_rden", [128, 512], F32)
        oT = K.sb(st, "cx_oT", [128, 8, 512], BF16)
        for tb in range(T // 512):
            t0 = tb * 512
            for tt in range(4):
                r0 = s * T + t0 + tt * 128
                K.dma("sp" if tt % 2 == 0 else "act", ht[:, tt, :], SC["H1"][r0:r0 + 128, :], ["H1"], ["cx_ht%d" % tt])
                norm_T(K, "cx_", ht[:, tt, :], "cx_ht%d" % tt, xn, ss, junk, pst, (hnT, "cx_hnT"), tt * 128, identb, CONST["eps6"])
            for j in range(8):
                pj = j % 2
                for k in range(8):
                    K.mm(ps[pj][:], wcq[:, k, j * 128:(j + 1) * 128], hnT[:, k, :], ["cx_wcq", "cx_hnT"], ["cx_ps%d" % pj], start=(k == 0), stop=(k == 7))
                if pj == 0:
                    K.op("dve", "tensor_copy", ["cx_ps0"], ["cx_qTs"], out=qTs[:, j, :], in_=ps[0][:])
                else:
                    K.op("act", "activation", ["cx_ps1"], ["cx_qTs"], out=qTs[:, j, :], in_=ps[1][:], func=AF.Copy)
            for h in range(4):
                for mt in range(2):
                    for dc in range(2):
                        K.mm(ps[2 + mt][:], kTs[:, 2 * h + dc, mt * 128:(mt + 1) * 128], qTs[:, 2 * h + dc, :], ["cx_kTs", "cx_qTs"],
                             ["cx_ps%d" % (2 + mt)], start=(dc == 0), stop=(dc == 1))
                    K.op("act", "activation", ["cx_ps%d" % (2 + mt)], ["cx_pT%d" % mt], out=pT[mt][:], in_=ps[2 + mt][:], func=AF.Exp, scale=1.0 / 16)
                for mt in range(2):
                    K.mm(ps[4][:], ones[:], pT[mt][:], ["cx_ones", "cx_pT%d" % mt], ["cx_ps4"], start=(mt == 0), stop=(mt == 1))
                K.op("dve", "reciprocal", ["cx_ps4"], ["cx_rden"], out=rden[:], in_=ps[4][:])
                for dc in range(2):
                    for mt in range(2):
                        K.mm(ps[5][:], vS[:, mt, h * 256 + dc * 128:h * 256 + (dc + 1) * 128], pT[mt][:], ["cx_vS", "cx_pT%d" % mt], ["cx_ps5"],
                             start=(mt == 0), stop=(mt == 1))
                    K.op("dve", "tensor_tensor", ["cx_ps5", "cx_rden"], ["cx_oT"], out=oT[:, 2 * h + dc, :], in0=ps[5][:], in1=rden[:], op=ALU.mult)
            for tt in range(4):
                r0 = s * T + t0 + tt * 128
                for half in range(2):
                    pj = half
                    for k in range(8):
                        K.mm(ps[pj][:], oT[:, k, tt * 128:(tt + 1) * 128], wco[:, k, half * 512:(half + 1) * 512], ["cx_oT", "cx_wco"],
                             ["cx_ps%d" % pj], start=(k == 0), stop=(k == 7))
                    K.op("dve", "tensor_tensor", ["cx_ps%d" % pj, "cx_ht%d" % tt], ["cx_ht%d" % tt], out=ht[:, tt, half * 512:(half + 1) * 512],
                         in0=ps[pj][:], in1=ht[:, tt, half * 512:(half + 1) * 512], op=ALU.add)
                K.dma("sp", SC["H1"][r0:r0 + 128, :], ht[:, tt, :], ["cx_ht%d" % tt], ["H1"])


def phase_moe(K, s, T, Wd, SC, CONST, OUT):
    identb = CONST["identb"]
    HT = min(T, 1024)
    NTL = HT // 128
    with ExitStack() as st:
        nrf = colvec(K, st, "mo_nrf", Wd["norm_ffn"])
        wrf = K.sb(st, "mo_wrf", [128, 8, 36], F32)
        K.dma("sp", wrf[:, :, 0:4], Wd["w_router_g"][0].rearrange("(c p) n -> p c n", p=128), [], ["mo_wrf"])
        K.dma("sp", wrf[:, :, 4:36], Wd["w_router_e"][0].rearrange("(c p) n -> p c n", p=128), [], ["mo_wrf"])
        wr = K.sb(st, "mo_wr", [128, 8, 36], BF16)
        K.op("dve", "tensor_tensor", ["mo_wrf", "mo_nrf"], ["mo_wr"], out=wr[:], in0=wrf[:], in1=nrf[:].unsqueeze(2).to_broadcast([128, 8, 36]), op=ALU.mult)
        brb = K.sb(st, "mo_brb", [128, 36], F32)
        K.dma("sp", brb[:, 0:4], Wd["b_router_g"].partition_broadcast(128), [], ["mo_brb"])
        K.dma("sp", brb[:, 4:36], Wd["b_router_e"].partition_broadcast(128), [], ["mo_brb"])
        nfb = K.sb(st, "mo_nfb", [128, D], F32)
        K.dma("sp", nfb[:], Wd["norm_final"].partition_broadcast(128), [], ["mo_nfb"])
        ps = [K.ps(st, "mo_ps%d" % i, [128, 512], F32) for i in range(7)]
        pst = K.ps(st, "mo_pst", [128, 8, 128], BF16)
        ht = K.sb(st, "mo_ht", [128, D], F32)
        xn = K.sb(st, "mo_xn", [128, D], BF16)
        ss = K.sb(st, "mo_ss", [128, 1], F32)
        junk = K.sb(st, "mo_junk", [128, D], F32)
        xT = K.sb(st, "mo_xT", [128, 8, HT], BF16)
        G = K.sb(st, "mo_G", [128, NTL, 32], F32)
        acc = K.sb(st, "mo_acc", [128, NTL, D], F32)
        lg = K.sb(st, "mo_lg", [128, 36], F32)
        cl = K.sb(st, "mo_cl", [128, 12], F32)
        lem = K.sb(st, "mo_lem", [128, 4, 8], F32)
        m8 = K.sb(st, "mo_m8", [128, 8], F32)
        sel = K.sb(st, "mo_sel", [128, 32], F32)
        ex = K.sb(st, "mo_ex", [128, 32], F32)
        stg = [K.sb(st, "mo_stg%d" % i, [128, 4096], F32) for i in range(2)]
        wg = [K.sb(st, "mo_wg%d" % i, [128, 8, 512], BF16) for i in range(2)]
        wu = [K.sb(st, "mo_wu%d" % i, [128, 8, 512], BF16) for i in range(2)]
        wd = [K.sb(st, "mo_wd%d" % i, [128, 4, 1024], BF16) for i in range(2)]
        sgts = [K.sb(st, "mo_sgt%d" % i, [128, 512], F32) for i in range(2)]
        hT = K.sb(st, "mo_hT", [128, 4, 512], BF16)
        tmp = [K.sb(st, "mo_tmp%d" % i, [128, 512], F32) for i in range(3)]
        for hf in range(T // HT):
            base = s * T + hf * HT
            for tl in range(NTL):
                r0 = base + tl * 128
                K.dma("sp", ht[:], SC["H1"][r0:r0 + 128, :], ["H1"], ["mo_ht"])
                norm_T(K, "mo_", ht[:], "mo_ht", xn, ss, junk, pst, (xT, "mo_xT"), tl * 128, identb, CONST["eps6"])
                for k in range(8):
                    K.mm(ps[0][:, 0:36], xT[:, k, tl * 128:(tl + 1) * 128], wr[:, k, :], ["mo_xT", "mo_wr"], ["mo_ps0"], start=(k == 0), stop=(k == 7))
                K.op("dve", "tensor_tensor", ["mo_ps0", "mo_brb"], ["mo_lg"], out=lg[:], in0=ps[0][:, 0:36], in1=brb[:], op=ALU.add)
                K.op("dve", "tensor_reduce", ["mo_lg"], ["mo_cl"], out=cl[:, 0:1], in_=lg[:, 0:4], axis=AX.X, op=ALU.max)
                K.op("dve", "tensor_scalar", ["mo_cl"], ["mo_cl"], out=cl[:, 1:2], in0=cl[:, 0:1], scalar1=-1.0, scalar2=None, op0=ALU.mult)
                K.op("act", "activation", ["mo_lg", "mo_cl"], ["mo_ex", "mo_cl"], out=ex[:, 0:4], in_=lg[:, 0:4], func=AF.Exp, bias=cl[:, 1:2], accum_out=cl[:, 2:3])
                K.op("dve", "reciprocal", ["mo_cl"], ["mo_cl"], out=cl[:, 3:4], in_=cl[:, 2:3])
                K.op("dve", "tensor_scalar", ["mo_lg", "mo_cl"], ["mo_sel"], out=sel[:, 0:4], in0=lg[:, 0:4], scalar1=cl[:, 0:1], scalar2=None, op0=ALU.is_ge)
                K.op("dve", "tensor_scalar", ["mo_sel"], ["mo_sel"], out=sel[:, 0:4], in0=sel[:, 0:4], scalar1=-1.0, scalar2=1e30, op0=ALU.add, op1=ALU.mult)
                K.op("dve", "tensor_tensor", ["mo_lg", "mo_sel"], ["mo_lem"], out=lem[:], in0=lg[:, 4:36].rearrange("p (a b) -> p a b", a=4),
                     in1=sel[:, 0:4].unsqueeze(2).to_broadcast([128, 4, 8]), op=ALU.add)
                lemf = lem[:].rearrange("p a b -> p (a b)")
                K.op("dve", "max", ["mo_lem"], ["mo_m8"], out=m8[:], in_=lemf)
                K.op("dve", "tensor_scalar", ["mo_lem", "mo_m8"], ["mo_sel"], out=sel[:], in0=lemf, scalar1=m8[:, 1:2], scalar2=None, op0=ALU.is_ge)
                K.op("dve", "tensor_scalar", ["mo_m8"], ["mo_cl"], out=cl[:, 4:5], in0=m8[:, 0:1], scalar1=-1.0, scalar2=None, op0=ALU.mult)
                K.op("act", "activation", ["mo_lem", "mo_cl"], ["mo_ex"], out=ex[:], in_=lemf, func=AF.Exp, bias=cl[:, 4:5])
                K.op("dve", "tensor_tensor", ["mo_ex", "mo_sel"], ["mo_ex"], out=ex[:], in0=ex[:], in1=sel[:], op=ALU.mult)
                K.op("dve", "tensor_reduce", ["mo_ex"], ["mo_cl"], out=cl[:, 5:6], in_=ex[:], axis=AX.X, op=ALU.add)
                K.op("dve", "reciprocal", ["mo_cl"], ["mo_cl"], out=cl[:, 6:7], in_=cl[:, 5:6])
                K.op("dve", "tensor_tensor", ["mo_cl"], ["mo_cl"], out=cl[:, 7:8], in0=cl[:, 6:7], in1=cl[:, 3:4], op=ALU.mult)
                K.op("dve", "tensor_scalar", ["mo_ex", "mo_cl"], ["mo_G"], out=G[:, tl, :], in0=ex[:], scalar1=cl[:, 7:8], scalar2=None, op0=ALU.mult)
            for e in range(32):
                i = e % 2
                nfb8 = nrf[:].unsqueeze(2).to_broadcast([128, 8, 512])
                K.dma("sp", stg[0][:].rearrange("p (c n) -> p c n", c=8), Wd["w_e_gate"][0, e].rearrange("(c p) n -> p c n", p=128), [], ["mo_stg0"])
                K.op("pool", "tensor_tensor", ["mo_stg0", "mo_nrf"], ["mo_wg%d" % i], out=wg[i][:], in0=stg[0][:].rearrange("p (c n) -> p c n", c=8), in1=nfb8, op=ALU.mult)
                K.dma("act", stg[1][:].rearrange("p (c n) -> p c n", c=8), Wd["w_e_up"][0, e].rearrange("(c p) n -> p c n", p=128), [], ["mo_stg1"])
                K.op("pool", "tensor_tensor", ["mo_stg1", "mo_nrf"], ["mo_wu%d" % i], out=wu[i][:], in0=stg[1][:].rearrange("p (c n) -> p c n", c=8), in1=nfb8, op=ALU.mult)
                K.dma("sp", stg[0][:].rearrange("p (c n) -> p c n", c=4), Wd["w_e_down"][0, e].rearrange("(c p) n -> p c n", p=128), [], ["mo_stg0"])
                K.op("pool", "tensor_copy", ["mo_stg0"], ["mo_wd%d" % i], out=wd[i][:], in_=stg[0][:].rearrange("p (c n) -> p c n", c=4))
                for bk in range(HT // 512):
                    bs = slice(bk * 512, (bk + 1) * 512)
                    for fc in range(4):
                        fs = slice(fc * 128, (fc + 1) * 128)
                        pg, pu = (0, 1) if fc % 2 == 0 else (4, 5)
                        sg_ = sgts[fc % 2]
                        sgn = "mo_sgt%d" % (fc % 2)
                        for k in range(8):
                            K.mm(ps[pg][:], wg[i][:, k, fs], xT[:, k, bs], ["mo_wg%d" % i, "mo_xT"], ["mo_ps%d" % pg], start=(k == 0), stop=(k == 7))
                        for k in range(8):
                            K.mm(ps[pu][:], wu[i][:, k, fs], xT[:, k, bs], ["mo_wu%d" % i, "mo_xT"], ["mo_ps%d" % pu], start=(k == 0), stop=(k == 7))
                        K.op("act", "activation", ["mo_ps%d" % pg], [sgn], out=sg_[:], in_=ps[pg][:], func=AF.Silu)
                        K.op("dve", "tensor_tensor", ["mo_ps%d" % pu, sgn], ["mo_hT%d" % fc], out=hT[:, fc, :], in0=ps[pu][:], in1=sg_[:], op=ALU.mult)
                    for tt in range(4):
                        tl = bk * 4 + tt
                        for half in range(2):
                            pj = (2, 3, 6)[(2 * tt + half) % 3]
                            for fc in range(4):
                                K.mm(ps[pj][:], hT[:, fc, tt * 128:(tt + 1) * 128], wd[i][:, fc, half * 512:(half + 1) * 512], ["mo_hT%d" % fc, "mo_wd%d" % i],
                                     ["mo_ps%d" % pj], start=(fc == 0), stop=(fc == 3))
                            hs = slice(half * 512, (half + 1) * 512)
                            if e == 0:
                                K.op("act", "activation", ["mo_ps%d" % pj, "mo_G"], ["mo_acc%d_%d" % (tl, half)], out=acc[:, tl, hs], in_=ps[pj][:], func=AF.Copy, scale=G[:, tl, e:e + 1])
                            else:
                                ti = (2 * tt + half) % 3
                                accn = "mo_acc%d_%d" % (tl, half)
                                K.op("act", "activation", ["mo_ps%d" % pj, "mo_G"], ["mo_tmp%d" % ti], out=tmp[ti][:], in_=ps[pj][:], func=AF.Copy, scale=G[:, tl, e:e + 1])
                                K.op("pool" if half == 0 else "dve", "tensor_tensor", ["mo_tmp%d" % ti, accn], [accn], out=acc[:, tl, hs], in0=acc[:, tl, hs], in1=tmp[ti][:], op=ALU.add)
            for tl in range(NTL):
                r0 = base + tl * 128
                K.dma("sp", ht[:], SC["H1"][r0:r0 + 128, :], ["H1"], ["mo_ht"])
                K.op("dve", "tensor_tensor", ["mo_ht", "mo_acc%d_0" % tl, "mo_acc%d_1" % tl], ["mo_ht"], out=ht[:], in0=ht[:], in1=acc[:, tl, :], op=ALU.add)
                K.op("act", "activation", ["mo_ht"], ["mo_junk", "mo_ss"], out=junk[:], in_=ht[:], func=AF.Square, accum_out=ss[:])
                K.op("act", "activation", ["mo_ss", "eps6"], ["mo_ss"], out=ss[:], in_=ss[:], func=AF.Sqrt, scale=1.0 / D, bias=CONST["eps6"][:])
                K.op("dve", "reciprocal", ["mo_ss"], ["mo_ss"], out=ss[:], in_=ss[:])
                K.op("dve", "scalar_tensor_tensor", ["mo_ht", "mo_ss", "mo_nfb"], ["mo_junk"], out=junk[:], in0=ht[:], scalar=ss[:], in1=nfb[:], op0=ALU.mult, op1=ALU.mult)
                K.dma("sp", OUT[r0:r0 + 128, :], junk[:], ["mo_junk"], ["OUT"])


I32 = mybir.dt.int32


def prepack_gen(K, st, Wd, SC):
    WGU, WDS = SC["WGU"], SC["WDS"]
    nrf = colvec(K, st, "pk_nrf", Wd["norm_ffn"])
    sg = K.sb(st, "pk_sg", [128, 8, 512], F32)
    su = K.sb(st, "pk_su", [128, 8, 512], F32)
    sd = K.sb(st, "pk_sd", [128, 4, 1024], F32)
    og = K.sb(st, "pk_og", [128, 8, 1024], BF16)
    od = K.sb(st, "pk_od", [128, 4, 1024], BF16)
    nf8 = nrf[:].unsqueeze(2).to_broadcast([128, 8, 512])
    for e in range(32):
        K.dma("pool", sg[:], Wd["w_e_gate"][0, e].rearrange("(c p) n -> p c n", p=128), [], ["pk_sg"])
        K.dma("pool", su[:], Wd["w_e_up"][0, e].rearrange("(c p) n -> p c n", p=128), [], ["pk_su"])
        K.dma("pool", sd[:], Wd["w_e_down"][0, e].rearrange("(c p) n -> p c n", p=128), [], ["pk_sd"])
        K.op("dve", "tensor_tensor", ["pk_sg", "pk_nrf"], ["pk_og0"], out=og[:, :, 0:512], in0=sg[:], in1=nf8, op=ALU.mult)
        K.op("pool", "tensor_tensor", ["pk_su", "pk_nrf"], ["pk_og1"], out=og[:, :, 512:1024], in0=su[:], in1=nf8, op=ALU.mult)
        K.op("act", "activation", ["pk_sd"], ["pk_od"], out=od[:], in_=sd[:], func=AF.Copy)
        K.dma("pool", WGU[e * 1024:(e + 1) * 1024, :].rearrange("(c p) n -> p c n", p=128), og[:], ["pk_og0", "pk_og1"], ["WGU"])
        K.dma("pool", WDS[e * 512:(e + 1) * 512, :].rearrange("(c p) n -> p c n", p=128), od[:], ["pk_od"], ["WDS"])
        yield


def phase_moe_sparse(K, s, T, Wd, SC, CONST, OUT):
    nc = K.nc
    S = K.S
    identb = CONST["identb"]
    NTL = T // 128
    SB = 256
    NBLK = (2 * T) // SB + 32
    XS, YS = SC["XS"], SC["YS"]
    WG = Wd["w_e_gate"].rearrange("o e d f -> (o e d) f")
    WU = Wd["w_e_up"].rearrange("o e d f -> (o e d) f")
    WDN = Wd["w_e_down"].rearrange("o e f d -> (o e f) d")
    base = s * T
    with ExitStack() as st0:
        nrf = colvec(K, st0, "ms_nrf", Wd["norm_ffn"])
        GG = K.sb(st0, "ms_GG", [128, NTL, 2], F32)
        DST = K.sb(st0, "ms_DST", [128, NTL, 2], I32)
        IDXG = K.sb(st0, "ms_IDXG", [128, NBLK, 8], I32)
        IDXD = K.sb(st0, "ms_IDXD", [128, NBLK, 4], I32)
        with ExitStack() as st:
            wrf = K.sb(st, "ms_wrf", [128, 8, 36], F32)
            K.dma("sp", wrf[:, :, 0:4], Wd["w_router_g"][0].rearrange("(c p) n -> p c n", p=128), [], ["ms_wrf"])
            K.dma("sp", wrf[:, :, 4:36], Wd["w_router_e"][0].rearrange("(c p) n -> p c n", p=128), [], ["ms_wrf"])
            wr = K.sb(st, "ms_wr", [128, 8, 36], BF16)
            K.op("dve", "tensor_tensor", ["ms_wrf", "ms_nrf"], ["ms_wr"], out=wr[:], in0=wrf[:], in1=nrf[:].unsqueeze(2).to_broadcast([128, 8, 36]), op=ALU.mult)
            brb = K.sb(st, "ms_brb", [128, 36], F32)
            K.dma("sp", brb[:, 0:4], Wd["b_router_g"].partition_broadcast(128), [], ["ms_brb"])
            K.dma("sp", brb[:, 4:36], Wd["b_router_e"].partition_broadcast(128), [], ["ms_brb"])
            ps = [K.ps(st, "ms_ps%d" % i, [128, 512], F32) for i in range(2)]
            pst = K.ps(st, "ms_pst", [128, 8, 128], BF16)
            ht = K.sb(st, "ms_ht", [128, D], F32)
            ss = K.sb(st, "ms_ss", [128, 1], F32)
            junk = K.sb(st, "ms_junk", [128, D], F32)
            XN = K.sb(st, "ms_XN", [128, NTL, D], BF16)
            xT = K.sb(st, "ms_xT", [128, 8, 128], BF16)
            SEL = K.sb(st, "ms_SEL", [128, NTL, 2, 32], F32)
            RNK = K.sb(st, "ms_RNK", [128, NTL, 2], F32)
            carry = K.sb(st, "ms_carry", [128, 32], F32)
            K.op("dve", "memset", [], ["ms_carry"], ap=carry[:], constant=0.0)
            lg = K.sb(st, "ms_lg", [128, 36], F32)
            cl = K.sb(st, "ms_cl", [128, 12], F32)
            lem = K.sb(st, "ms_lem", [128, 32], F32)
            m8 = K.sb(st, "ms_m8", [128, 8], F32)
            s12 = K.sb(st, "ms_s12", [128, 32], F32)
            ex = K.sb(st, "ms_ex", [128, 32], F32)
            t32 = K.sb(st, "ms_t32", [128, 32], F32)
            utri, ones128, bstart, iotap = CONST["utri"], CONST["ones128"], CONST["bstart"], CONST["iotap"]
            GB = 8
            LG = K.sb(st, "ms_LG", [128, GB, 36], F32)
            LM = K.sb(st, "ms_LM", [128, GB, 32], F32)
            L2 = K.sb(st, "ms_L2", [128, GB, 32], F32)
            EX = K.sb(st, "ms_EX", [128, GB, 32], F32)
            S12 = K.sb(st, "ms_S12", [128, GB, 32], F32)
            RKt = K.sb(st, "ms_RKt", [128, GB, 32], F32)
            T4 = K.sb(st, "ms_T4", [128, GB, 4], F32)
            E4 = K.sb(st, "ms_E4", [128, GB, 4], F32)
            CG = K.sb(st, "ms_CG", [128, 8, GB], F32)
            hts = [ht, K.sb(st, "ms_ht1", [128, D], F32)]

            def b3(colv, n):
                return colv.unsqueeze(2).to_broadcast([128, GB, n])

            for g0 in range(0, NTL, GB):
                for gi in range(GB):
                    tl = g0 + gi
                    r0 = base + tl * 128
                    hh_ = hts[tl % 2]
                    hn = "ms_ht" if tl % 2 == 0 else "ms_ht1"
                    K.dma("sp" if tl % 2 == 0 else "act", hh_[:], SC["H1"][r0:r0 + 128, :], ["H1"], [hn])
                    K.op("act", "activation", [hn], ["ms_junk", "ms_ss"], out=junk[:], in_=hh_[:], func=AF.Square, accum_out=ss[:])
                    K.op("act", "activation", ["ms_ss", "eps6"], ["ms_ss"], out=ss[:], in_=ss[:], func=AF.Sqrt, scale=1.0 / D, bias=CONST["eps6"][:])
                    K.op("dve", "reciprocal", ["ms_ss"], ["ms_ss"], out=ss[:], in_=ss[:])
                    K.op("dve", "tensor_scalar", [hn, "ms_ss"], ["ms_XN%d" % tl], out=XN[:, tl, :], in0=hh_[:], scalar1=ss[:], scalar2=None, op0=ALU.mult)
                    for c in range(8):
                        K.tr(pst[:, c, :], XN[:, tl, c * 128:(c + 1) * 128], identb[:], ["ms_XN%d" % tl, "identb"], ["ms_pst"])
                    K.op("act", "activation", ["ms_pst"], ["ms_xT"], out=xT[:], in_=pst[:], func=AF.Copy)
                    for k in range(8):
                        K.mm(ps[0][:, 0:36], xT[:, k, :], wr[:, k, :], ["ms_xT", "ms_wr"], ["ms_ps0"], start=(k == 0), stop=(k == 7))
                    K.op("dve", "tensor_tensor", ["ms_ps0", "ms_brb"], ["ms_LG"], out=LG[:, gi, :], in0=ps[0][:, 0:36], in1=brb[:], op=ALU.add)
                K.op("dve", "tensor_reduce", ["ms_LG"], ["ms_CG"], out=CG[:, 0, :], in_=LG[:, :, 0:4], axis=AX.X, op=ALU.max)
                K.op("dve", "tensor_tensor", ["ms_LG", "ms_CG"], ["ms_T4"], out=T4[:], in0=LG[:, :, 0:4], in1=b3(CG[:, 0, :], 4), op=ALU.subtract)
                K.op("act", "activation", ["ms_T4"], ["ms_E4"], out=E4[:], in_=T4[:], func=AF.Exp)
                K.op("dve", "tensor_reduce", ["ms_E4"], ["ms_CG"], out=CG[:, 1, :], in_=E4[:], axis=AX.X, op=ALU.add)
                K.op("dve", "reciprocal", ["ms_CG"], ["ms_CG"], out=CG[:, 2, :], in_=CG[:, 1, :])
                K.op("dve", "tensor_scalar", ["ms_T4"], ["ms_T4"], out=T4[:], in0=T4[:], scalar1=0.0, scalar2=None, op0=ALU.is_ge)
                K.op("dve", "tensor_scalar", ["ms_T4"], ["ms_T4"], out=T4[:], in0=T4[:], scalar1=-1.0, scalar2=1e30, op0=ALU.add, op1=ALU.mult)
                K.op("dve", "tensor_tensor", ["ms_LG", "ms_T4"], ["ms_LM"], out=LM[:].rearrange("p g (a b) -> p g a b", a=4),
                     in0=LG[:, :, 4:36].rearrange("p g (a b) -> p g a b", a=4), in1=T4[:].unsqueeze(3).to_broadcast([128, GB, 4, 8]), op=ALU.add)
                K.op("dve", "tensor_reduce", ["ms_LM"], ["ms_CG"], out=CG[:, 3, :], in_=LM[:], axis=AX.X, op=ALU.max)
                sel1 = SEL[:, g0:g0 + GB, 0, :]
                sel2 = SEL[:, g0:g0 + GB, 1, :]
                K.op("dve", "tensor_tensor", ["ms_LM", "ms_CG"], ["ms_SEL"], out=sel1, in0=LM[:], in1=b3(CG[:, 3, :], 32), op=ALU.is_ge)
                K.op("dve", "scalar_tensor_tensor", ["ms_SEL", "ms_LM"], ["ms_L2"], out=L2[:], in0=sel1, scalar=-1e30, in1=LM[:], op0=ALU.mult, op1=ALU.add)
                K.op("dve", "tensor_reduce", ["ms_L2"], ["ms_CG"], out=CG[:, 4, :], in_=L2[:], axis=AX.X, op=ALU.max)
                K.op("dve", "tensor_tensor", ["ms_LM", "ms_CG"], ["ms_S12"], out=S12[:], in0=LM[:], in1=b3(CG[:, 4, :], 32), op=ALU.is_ge)
                K.op("dve", "tensor_tensor", ["ms_S12", "ms_SEL"], ["ms_SEL"], out=sel2, in0=S12[:], in1=sel1, op=ALU.subtract)
                K.op("dve", "tensor_tensor", ["ms_LM", "ms_CG"], ["ms_L2"], out=L2[:], in0=LM[:], in1=b3(CG[:, 3, :], 32), op=ALU.subtract)
                K.op("dve", "tensor_scalar", ["ms_L2"], ["ms_L2"], out=L2[:], in0=L2[:], scalar1=-80.0, scalar2=None, op0=ALU.max)
                K.op("act", "activation", ["ms_L2"], ["ms_EX"], out=EX[:], in_=L2[:], func=AF.Exp)
                K.op("dve", "tensor_tensor", ["ms_EX", "ms_S12"], ["ms_EX"], out=EX[:], in0=EX[:], in1=S12[:], op=ALU.mult)
                K.op("dve", "tensor_reduce", ["ms_EX"], ["ms_CG"], out=CG[:, 5, :], in_=EX[:], axis=AX.X, op=ALU.add)
                K.op("dve", "reciprocal", ["ms_CG"], ["ms_CG"], out=CG[:, 6, :], in_=CG[:, 5, :])
                K.op("dve", "tensor_tensor", ["ms_CG"], ["ms_CG"], out=CG[:, 6, :], in0=CG[:, 6, :], in1=CG[:, 2, :], op=ALU.mult)
                for kk_ in range(2):
                    K.op("dve", "tensor_tensor", ["ms_EX", "ms_SEL"], ["ms_L2"], out=L2[:], in0=EX[:], in1=SEL[:, g0:g0 + GB, kk_, :], op=ALU.mult)
                    K.op("dve", "tensor_reduce", ["ms_L2"], ["ms_CG"], out=CG[:, 7, :], in_=L2[:], axis=AX.X, op=ALU.add)
                    K.op("dve", "tensor_tensor", ["ms_CG"], ["ms_GG"], out=GG[:, g0:g0 + GB, kk_], in0=CG[:, 7, :], in1=CG[:, 6, :], op=ALU.mult)
                for gi in range(GB):
                    K.mm(ps[1][:, gi * 64:gi * 64 + 32], utri[:], S12[:, gi, :], ["utri", "ms_S12"], ["ms_ps1"])
                    K.mm(ps[1][:, gi * 64 + 32:gi * 64 + 64], ones128[:], S12[:, gi, :], ["ones128", "ms_S12"], ["ms_ps1"])
                for gi in range(GB):
                    K.op("dve", "tensor_tensor", ["ms_ps1", "ms_carry"], ["ms_RKt"], out=RKt[:, gi, :], in0=ps[1][:, gi * 64:gi * 64 + 32], in1=carry[:], op=ALU.add)
                    K.op("dve", "tensor_tensor", ["ms_ps1", "ms_carry"], ["ms_carry"], out=carry[:], in0=ps[1][:, gi * 64 + 32:gi * 64 + 64], in1=carry[:], op=ALU.add)
                for kk_ in range(2):
                    K.op("dve", "tensor_tensor", ["ms_RKt", "ms_SEL"], ["ms_L2"], out=L2[:], in0=RKt[:], in1=SEL[:, g0:g0 + GB, kk_, :], op=ALU.mult)
                    K.op("dve", "tensor_reduce", ["ms_L2"], ["ms_RNK"], out=RNK[:, g0:g0 + GB, kk_], in_=L2[:], axis=AX.X, op=ALU.add)
            ci = K.sb(st, "ms_ci", [128, 32], I32)
            pad = K.sb(st, "ms_pad", [128, 32], F32)
            pend = K.sb(st, "ms_pend", [128, 32], F32)
            pstart = K.sb(st, "ms_pstart", [128, 32], F32)
            ones32 = K.sb(st, "ms_ones32", [128, 32], F32)
            K.op("dve", "memset", [], ["ms_ones32"], ap=ones32[:], constant=1.0)
            K.op("dve", "tensor_scalar", ["ms_carry"], ["ms_ci"], out=ci[:], in0=carry[:], scalar1=float(SB - 1), scalar2=None, op0=ALU.add)
            K.op("dve", "tensor_scalar", ["ms_ci"], ["ms_ci"], out=ci[:], in0=ci[:], scalar1=8, scalar2=None, op0=ALU.arith_shift_right)
            K.op("dve", "tensor_scalar", ["ms_ci"], ["ms_ci"], out=ci[:], in0=ci[:], scalar1=8, scalar2=None, op0=ALU.logical_shift_left)
            K.op("dve", "tensor_copy", ["ms_ci"], ["ms_pad"], out=pad[:], in_=ci[:])
            K.op("dve", "tensor_tensor_scan", ["ms_pad", "ms_ones32"], ["ms_pend"], out=pend[:], data0=ones32[:], data1=pad[:], initial=0.0, op0=ALU.mult, op1=ALU.add)
            K.op("dve", "tensor_tensor", ["ms_pend", "ms_pad"], ["ms_pstart"], out=pstart[:], in0=pend[:], in1=pad[:], op=ALU.subtract)
            bst = K.sb(st, "ms_bst", [128, NBLK], F32)
            K.op("dve", "tensor_scalar", ["bstart"], ["ms_bst"], out=bst[:], in0=bstart[:, 0:NBLK], scalar1=float(SB // 128), scalar2=None, op0=ALU.mult)
            be = K.sb(st, "ms_be", [128, NBLK], F32)
            K.op("dve", "tensor_scalar", ["ms_bst", "ms_pend"], ["ms_be"], out=be[:], in0=bst[:], scalar1=pend[:, 0:1], scalar2=None, op0=ALU.is_ge)
            for e in range(1, 32):
                K.op("dve", "scalar_tensor_tensor", ["ms_bst", "ms_pend", "ms_be"], ["ms_be"], out=be[:], in0=bst[:], scalar=pend[:, e:e + 1], in1=be[:],
                     op0=ALU.is_ge, op1=ALU.add)
            K.op("dve", "tensor_scalar", ["ms_be"], ["ms_be"], out=be[:], in0=be[:], scalar1=31.0, scalar2=None, op0=ALU.min)
            bg = K.sb(st, "ms_bg", [128, NBLK], F32)
            bd = K.sb(st, "ms_bd", [128, NBLK], F32)
            K.op("dve", "tensor_scalar", ["ms_be", "iotap"], ["ms_bg"], out=bg[:], in0=be[:], scalar1=1024.0, scalar2=iotap[:, 0:1], op0=ALU.mult, op1=ALU.add)
            K.op("dve", "tensor_scalar", ["ms_be", "iotap"], ["ms_bd"], out=bd[:], in0=be[:], scalar1=512.0, scalar2=iotap[:, 0:1], op0=ALU.mult, op1=ALU.add)
            for c in range(8):
                K.op("dve", "tensor_scalar", ["ms_bg"], ["ms_IDXG"], out=IDXG[:, :, c], in0=bg[:], scalar1=float(c * 128), scalar2=None, op0=ALU.add)
            for c in range(4):
                K.op("dve", "tensor_scalar", ["ms_bd"], ["ms_IDXD"], out=IDXD[:, :, c], in0=bd[:], scalar1=float(c * 128), scalar2=None, op0=ALU.add)
            zt = K.sb(st, "ms_zt", [128, 4, D], BF16)
            K.op("pool", "memset", [], ["ms_zt"], ap=zt[:], constant=0.0)
            XSv = XS.rearrange("(b p) d -> p b d", p=128)
            for b0 in range(0, NBLK * SB // 128, 4):
                K.dma("sp" if (b0 // 4) % 2 == 0 else "act", XSv[:, b0:b0 + 4, :], zt[:], ["ms_zt"], ["XS"])
            for tl in range(NTL):
                for kk_ in range(2):
                    K.op("dve", "tensor_tensor", ["ms_pstart", "ms_SEL"], ["ms_t32"], out=t32[:], in0=pstart[:], in1=SEL[:, tl, kk_, :], op=ALU.mult)
                    K.op("dve", "tensor_reduce", ["ms_t32"], ["ms_cl"], out=cl[:, 9:10], in_=t32[:], axis=AX.X, op=ALU.add)
                    K.op("dve", "tensor_tensor", ["ms_cl", "ms_RNK"], ["ms_DST"], out=DST[:, tl, kk_:kk_ + 1], in0=cl[:, 9:10], in1=RNK[:, tl, kk_:kk_ + 1], op=ALU.add)
                for kk_ in range(2):
                    S.dma("pool", None, None, K._bl(["ms_DST", "ms_XN%d" % tl, "XS"]), K._bl(["XSs_%d_%d" % (tl, kk_)]),
                          fn=lambda e, tl=tl, kk_=kk_: e.indirect_dma_start(out=XS, out_offset=bass.IndirectOffsetOnAxis(ap=DST[:, tl, kk_:kk_ + 1], axis=0),
                                                                        in_=XN[:, tl, :], in_offset=None))
        S.barrier()
        with ExitStack() as st:
            ps = [K.ps(st, "mb_ps%d" % i, [128, 512], F32) for i in range(6)]
            pst = K.ps(st, "mb_pst", [128, 8, 128], BF16)
            wgu = [K.sb(st, "mb_wgu%d" % i, [128, 8, 1024], BF16) for i in range(2)]
            wd = [K.sb(st, "mb_wd%d" % i, [128, 4, 1024], BF16) for i in range(2)]
            xb = [K.sb(st, "mb_xb%d" % i, [128, D], BF16) for i in range(2)]
            xT = K.sb(st, "mb_xT", [128, 8, 128], BF16)
            sgt = K.sb(st, "mb_sgt", [128, 512], F32)
            hb = K.sb(st, "mb_hb", [128, 512], BF16)
            hT = K.sb(st, "mb_hT", [128, 4, 128], BF16)
            ysb = [K.sb(st, "mb_ysb%d" % i, [128, D], F32) for i in range(2)]
            WGU, WDS = SC["WGU"], SC["WDS"]
            for b in range(NBLK):
                i = b % 2
                for c in range(8):
                    S.dma("pool", None, None, K._bl(["ms_IDXG"]), K._bl(["mb_wgu%d_%d" % (i, c)]),
                          fn=lambda e, b=b, c=c, i=i: e.indirect_dma_start(out=wgu[i][:, c, :], out_offset=None, in_=WGU,
                                                                         in_offset=bass.IndirectOffsetOnAxis(ap=IDXG[:, b, c:c + 1], axis=0)))
                for c in range(4):
                    S.dma("pool", None, None, K._bl(["ms_IDXD"]), K._bl(["mb_wd%d_%d" % (i, c)]),
                          fn=lambda e, b=b, c=c, i=i: e.indirect_dma_start(out=wd[i][:, c, :], out_offset=None, in_=WDS,
                                                                         in_offset=bass.IndirectOffsetOnAxis(ap=IDXD[:, b, c:c + 1], axis=0)))
                for sub in range(SB // 128):
                    j = sub % 2
                    r0 = b * SB + sub * 128
                    K.dma("sp", xb[j][:], XS[r0:r0 + 128, :], ["XS"], ["mb_xb%d" % j])
                    for c in range(8):
                        K.tr(pst[:, c, :], xb[j][:, c * 128:(c + 1) * 128], identb[:], ["mb_xb%d" % j, "identb"], ["mb_pst"])
                    K.op("act", "activation", ["mb_pst"], ["mb_xT"], out=xT[:], in_=pst[:], func=AF.Copy)
                    for k in range(8):
                        K.mm(ps[0][:], xT[:, k, :], wgu[i][:, k, 0:512], ["mb_xT"] + ["mb_wgu%d_%d" % (i, c) for c in range(8)], ["mb_ps0"], start=(k == 0), stop=(k == 7))
                    for k in range(8):
                        K.mm(ps[1][:], xT[:, k, :], wgu[i][:, k, 512:1024], ["mb_xT"] + ["mb_wgu%d_%d" % (i, c) for c in range(8)], ["mb_ps1"], start=(k == 0), stop=(k == 7))
                    K.op("act", "activation", ["mb_ps0"], ["mb_sgt"], out=sgt[:], in_=ps[0][:], func=AF.Silu)
                    K.op("dve", "tensor_tensor", ["mb_ps1", "mb_sgt"], ["mb_hb"], out=hb[:], in0=ps[1][:], in1=sgt[:], op=ALU.mult)
                    for fc in range(4):
                        K.tr(pst[:, fc, :], hb[:, fc * 128:(fc + 1) * 128], identb[:], ["mb_hb", "identb"], ["mb_pst"])
                    K.op("dve", "tensor_copy", ["mb_pst"], ["mb_hT"], out=hT[:], in_=pst[:, 0:4, :])
                    for half in range(2):
                        pj = 2 + 2 * j + half
                        for fc in range(4):
                            K.mm(ps[pj][:], hT[:, fc, :], wd[i][:, fc, half * 512:(half + 1) * 512], ["mb_hT"] + ["mb_wd%d_%d" % (i, c) for c in range(4)], ["mb_ps%d" % pj], start=(fc == 0), stop=(fc == 3))
                        if half == 0:
                            K.op("act", "activation", ["mb_ps%d" % pj], ["mb_ysb%d" % j], out=ysb[j][:, 0:512], in_=ps[pj][:], func=AF.Copy)
                        else:
                            K.op("dve", "tensor_copy", ["mb_ps%d" % pj], ["mb_ysb%d" % j], out=ysb[j][:, 512:1024], in_=ps[pj][:])
                    K.dma("act", YS[r0:r0 + 128, :], ysb[j][:], ["mb_ysb%d" % j], ["YS"])
        S.barrier()
        with ExitStack() as st:
            nfb = K.sb(st, "mc_nfb", [128, D], F32)
            K.dma("sp", nfb[:], Wd["norm_final"].partition_broadcast(128), [], ["mc_nfb"])
            hts = [K.sb(st, "mc_ht%d" % i, [128, D], F32) for i in range(2)]
            y1 = [K.sb(st, "mc_y1%d" % i, [128, D], F32) for i in range(2)]
            y2 = [K.sb(st, "mc_y2%d" % i, [128, D], F32) for i in range(2)]
            ob = [K.sb(st, "mc_ob%d" % i, [128, D], F32) for i in range(2)]
            junk = K.sb(st, "mc_junk", [128, D], F32)
            sss = [K.sb(st, "mc_ss%d" % i, [128, 1], F32) for i in range(2)]
            for tl in range(NTL):
                i = tl % 2
                r0 = base + tl * 128
                K.dma("sp", hts[i][:], SC["H1"][r0:r0 + 128, :], ["H1"], ["mc_ht%d" % i])
                S.dma("pool", None, None, K._bl(["ms_DST", "YS"]), K._bl(["mc_y1%d" % i]),
                      fn=lambda e, tl=tl, i=i: e.indirect_dma_start(out=y1[i][:], out_offset=None, in_=YS, in_offset=bass.IndirectOffsetOnAxis(ap=DST[:, tl, 0:1], axis=0)))
                S.dma("pool", None, None, K._bl(["ms_DST", "YS"]), K._bl(["mc_y2%d" % i]),
                      fn=lambda e, tl=tl, i=i: e.indirect_dma_start(out=y2[i][:], out_offset=None, in_=YS, in_offset=bass.IndirectOffsetOnAxis(ap=DST[:, tl, 1:2], axis=0)))
                K.op("dve", "scalar_tensor_tensor", ["mc_y1%d" % i, "ms_GG", "mc_ht%d" % i], ["mc_ht%d" % i], out=hts[i][:], in0=y1[i][:], scalar=GG[:, tl, 0:1], in1=hts[i][:],
                     op0=ALU.mult, op1=ALU.add)
                K.op("dve", "scalar_tensor_tensor", ["mc_y2%d" % i, "ms_GG", "mc_ht%d" % i], ["mc_ht%d" % i], out=hts[i][:], in0=y2[i][:], scalar=GG[:, tl, 1:2], in1=hts[i][:],
                     op0=ALU.mult, op1=ALU.add)
                K.op("act", "activation", ["mc_ht%d" % i], ["mc_junk", "mc_ss%d" % i], out=junk[:], in_=hts[i][:], func=AF.Square, accum_out=sss[i][:])
                K.op("act", "activation", ["mc_ss%d" % i, "eps6"], ["mc_ss%d" % i], out=sss[i][:], in_=sss[i][:], func=AF.Sqrt, scale=1.0 / D, bias=CONST["eps6"][:])
                K.op("dve", "reciprocal", ["mc_ss%d" % i], ["mc_ss%d" % i], out=sss[i][:], in_=sss[i][:])
                K.op("dve", "scalar_tensor_tensor", ["mc_ht%d" % i, "mc_ss%d" % i, "mc_nfb"], ["mc_ob%d" % i], out=ob[i][:], in0=hts[i][:], scalar=sss[i][:], in1=nfb[:],
                     op0=ALU.mult, op1=ALU.mult)
                K.dma("act", OUT[r0:r0 + 128, :], ob[i][:], ["mc_ob%d" % i], ["OUT"])


def build(T, NSEQ, stop_after=99, debug=False):
    nc = bass.Bass("TRN2", target_bir_lowering=False)
    NTOK = NSEQ * T

    def din(name, shape, dt=F32):
        return nc.dram_tensor(name, list(shape), dt, kind="ExternalInput").ap()

    X = din("x", [NTOK, D])
    MEM = din("mem", [NSEQ * 256, D])
    Wd = {}
    for name, shape in WSHAPES.items():
        Wd[name] = din(name, shape)
    identb_d = din("c_identb", [128, 128], BF16)
    identf_d = din("c_identf", [128, 128], F32)
    OUT = nc.dram_tensor("out", [NTOK, D], F32, kind="ExternalOutput").ap()
    SC = {}
    SC["ZF"] = nc.dram_tensor("sc_zf", [NSEQ, R_TOT, T], F32, kind="Internal").ap() if not debug else \
        nc.dram_tensor("sc_zf", [NSEQ, R_TOT, T], F32, kind="ExternalOutput").ap()
    kindd = "ExternalOutput" if debug else "Internal"
    SC["CK"] = nc.dram_tensor("sc_ck", [NSEQ, T, 128], BF16, kind=kindd).ap()
    SC["CKT"] = nc.dram_tensor("sc_ckt", [NSEQ, 128, T], BF16, kind=kindd).ap()
    SC["H1"] = nc.dram_tensor("sc_h1", [NTOK, D], F32, kind=kindd).ap()
    NSLOT = ((2 * T) // 256 + 32) * 256
    SC["WGU"] = nc.dram_tensor("sc_wgu", [32 * 1024, 1024], BF16, kind="Internal").ap()
    SC["WDS"] = nc.dram_tensor("sc_wds", [32 * 512, 1024], BF16, kind="Internal").ap()
    SC["XS"] = nc.dram_tensor("sc_xs", [NSLOT, D], BF16, kind="Internal").ap()
    SC["YS"] = nc.dram_tensor("sc_ys", [NSLOT, D], F32, kind="Internal").ap()
    SC["YB"] = nc.dram_tensor("sc_yb", [NSEQ, 512, T], BF16, kind=kindd).ap()
    SC["YA"] = nc.dram_tensor("sc_ya", [NSEQ, 512, T], BF16, kind=kindd).ap()
    cdram = {}
    for nm, arr in consts().items():
        if nm not in ("c_identb", "c_identf"):
            cdram[nm] = din(nm, arr.shape, BF16 if arr.dtype == ml_dtypes.bfloat16 else F32)
    with ExitStack() as st:
        S = Sched(nc, st)
        K = Ctx(nc, S)
        CONST = {}
        CONST["identb"] = K.sb(st, "identb", [128, 128], BF16)
        CONST["identf"] = K.sb(st, "identf", [128, 128], F32)
        CONST["eps6"] = K.sb(st, "eps6", [128, 1], F32)
        K.dma("sp", CONST["identb"][:], identb_d, [], ["identb"])
        K.dma("sp", CONST["identf"][:], identf_d, [], ["identf"])
        K.op("dve", "memset", [], ["eps6"], ap=CONST["eps6"][:], constant=1e-6)
        for nm, ap in cdram.items():
            sh = list(ap.shape)
            CONST[nm[2:]] = K.sb(st, nm[2:], sh, ap.dtype)
            K.dma("sp", CONST[nm[2:]][:], ap, [], [nm[2:]])
        for s in range(NSEQ):
            phase1(K, s, T, X, Wd, SC, CONST)
            S.barrier()
            if stop_after >= 2 and not os.environ.get("SKIP_DSA"):
                ex_ = (lambda st_: prepack_gen(K, st_, Wd, SC)) if (s == 0 and stop_after >= 6 and not os.environ.get("MOE_DENSE")) else None
                phase_dsa(K, s, T, Wd, SC, CONST, extra=ex_)
                S.barrier()
            if stop_after >= 3:
                phase_rwkv(K, s, T, Wd, SC, CONST)
                S.barrier()
            if stop_after >= 4:
                phase_mix(K, s, T, X, Wd, SC, CONST)
                S.barrier()
            if stop_after >= 5:
                phase_cross(K, s, T, MEM, Wd, SC, CONST)
                S.barrier()
            if stop_after >= 6:
                if os.environ.get("MOE_DENSE"):
                    phase_moe(K, s, T, Wd, SC, CONST, OUT)
                else:
                    phase_moe_sparse(K, s, T, Wd, SC, CONST, OUT)
                S.barrier()
        S.finish(list(K.B.values()))
        print("ops", S.nops, "waits", S.nwaits)
        S.emit()
    return nc


WSHAPES = {
    "norm_mix": [1, 1024], "w_in": [1, 1024, 4804], "shift_mu": [1, 1792], "rw_w0": [1, 512],
    "rw_w2": [1, 64, 512], "rw_a0": [1, 512], "rw_a2": [1, 64, 512], "rw_g2": [1, 128, 512],
    "rw_k_k": [1, 512], "rw_k_a": [1, 512], "rw_r_k": [1, 8, 64], "rw_ln_w": [1, 512], "rw_ln_b": [1, 512],
    "kv_norm": [1, 128], "w_uk": [1, 128, 8, 64], "w_uv": [1, 128, 8, 64], "w_proj_a": [1, 512, 1024],
    "w_proj_b": [1, 512, 1024], "b_gate": [1, 2048], "w_out": [1, 1024, 1024], "norm_cross": [1, 1024],
    "norm_mem": [1, 1024], "w_cq": [1, 1024, 1024], "w_ckv": [1, 1024, 2048], "w_co": [1, 1024, 1024],
    "norm_ffn": [1, 1024], "w_router_g": [1, 1024, 4], "b_router_g": [1, 4], "w_router_e": [1, 1024, 32],
    "b_router_e": [1, 32], "w_e_gate": [1, 32, 1024, 512], "w_e_up": [1, 32, 1024, 512],
    "w_e_down": [1, 32, 512, 1024], "norm_final": [1024],
}


def consts():
    return {
        "c_identb": np.eye(128, dtype=np.float32).astype(ml_dtypes.bfloat16),
        "c_identf": np.eye(128, dtype=np.float32),
        "c_tri01": (np.arange(128)[None, :] <= np.arange(128)[:, None]).astype(np.float32).astype(ml_dtypes.bfloat16),
        "c_negtri": np.where(np.arange(128)[None, :] <= np.arange(128)[:, None], 0.0, -1e30).astype(np.float32),
        "c_bo": np.kron(np.eye(2), np.ones((64, 64))).astype(np.float32),
        "c_bo64": (np.kron(np.eye(2), np.ones((64, 64))) / 64.0).astype(np.float32),
        "c_maskq": np.block([[np.triu(np.ones((64, 64)), 1), np.triu(np.ones((64, 64)), 0)],
                             [np.triu(np.ones((64, 64)), 1), np.triu(np.ones((64, 64)), 0)]]).astype(np.float32),
        "c_lowm": np.concatenate([np.zeros((64, 64)), np.tril(np.ones((64, 64)), -1)], 0).astype(np.float32),
        "c_resetm": np.tile((np.arange(256) % 64 != 0).astype(np.float32)[None, :], (128, 1)),
        "c_utri": (np.arange(128)[:, None] < np.arange(128)[None, :]).astype(np.float32),
        "c_ones128": np.ones((128, 128), np.float32),
        "c_bstart": np.tile((np.arange(320) * 128.0)[None, :], (128, 1)).astype(np.float32),
        "c_iotap": np.arange(128, dtype=np.float32)[:, None].copy(),
        "c_pw": np.tile((0.5 ** (np.arange(NIT) + 1))[None, :], (128, 1)).astype(np.float32),
    }


def kernel(**inputs):
    x = np.asarray(inputs["x"], dtype=np.float32)
    mem = np.asarray(inputs["mem"], dtype=np.float32)
    B, T, _ = x.shape
    nseq = B // NCORES
    nc = build(T, nseq)
    cs = consts()
    in_maps = []
    for c in range(NCORES):
        m = {"x": np.ascontiguousarray(x[c * nseq:(c + 1) * nseq].reshape(nseq * T, D)),
             "mem": np.ascontiguousarray(mem[c * nseq:(c + 1) * nseq].reshape(nseq * 256, D))}
        for name in WSHAPES:
            m[name] = np.ascontiguousarray(np.asarray(inputs[name], dtype=np.float32))
        m.update(cs)
        in_maps.append(m)
    res = run_bass_kernel_spmd(nc, in_maps, core_ids=list(range(NCORES)))
    out = np.concatenate([r["out"].reshape(nseq, T, D) for r in res.results], axis=0)
    return out.astype(np.float32)
```

```python
from contextlib import ExitStack
import os
import numpy as np
import ml_dtypes
import concourse.bass as bass
import concourse.mybir as mybir
from concourse.bass_utils import run_bass_kernel_spmd

F32 = mybir.dt.float32
BF16 = mybir.dt.bfloat16
AF = mybir.ActivationFunctionType
ALU = mybir.AluOpType
AX = mybir.AxisListType

D = 1024
NCORES = 8


class Buf:
    __slots__ = ("name", "w", "r")

    def __init__(self, name=""):
        self.name = name
        self.w = None
        self.r = {}


class Sched:
    ENG = ("pe", "act", "dve", "pool", "sp")

    def __init__(self, nc, stack, n_dma_sems=10):
        self.nc = nc
        self.streams = {e: [] for e in self.ENG}
        self.sems = {}
        self.count = {}
        for e in self.ENG:
            self.sems[e] = stack.enter_context(nc.semaphore("s_" + e))
            self.count[e] = 0
        self.dma_sems = {}
        self.dma_rr = {}
        for q in ("sp", "act", "pool"):
            lst = []
            for i in range(n_dma_sems if q != "pool" else 28):
                k = "d_%s_%d" % (q, i)
                self.sems[k] = stack.enter_context(nc.semaphore(k))
                self.count[k] = 0
                lst.append(k)
            self.dma_sems[q] = lst
            self.dma_rr[q] = 0
        self.waited = {}
        self.nwaits = 0
        self.nops = 0

    def _wait(self, eng, key, val):
        if val <= 0 or self.waited.get((eng, key), 0) >= val:
            return
        self.waited[(eng, key)] = val
        self.streams[eng].append(("w", key, val))
        self.nwaits += 1

    def _deps(self, eng, reads, writes, own_key):
        for b in reads:
            if b.w is not None:
                self._dep(eng, b.w, own_key)
        for b in writes:
            if b.w is not None:
                self._dep(eng, b.w, own_key)
            for k, v in b.r.items():
                self._dep(eng, (k, v), own_key)

    def _dep(self, eng, ev, own_key):
        k, v = ev
        if k == "pe" and own_key == "pe":
            return
        self._wait(eng, k, v)

    muted = False

    def op(self, eng, fn, reads=(), writes=()):
        if self.muted:
            return
        self._deps(eng, reads, writes, eng)
        self.count[eng] += 1
        v = self.count[eng]
        self.streams[eng].append(("o", fn, eng, 1))
        for b in writes:
            b.w = (eng, v)
            b.r = {}
        for b in reads:
            if b.r.get(eng, 0) < v:
                b.r[eng] = v
        self.nops += 1

    def dma(self, q, out, in_, reads=(), writes=(), fn=None, **kw):
        if self.muted:
            return
        lst = self.dma_sems[q]
        key = lst[self.dma_rr[q] % len(lst)]
        self.dma_rr[q] += 1
        self._wait(q, key, self.count[key])
        self._deps(q, reads, writes, key)
        self.count[key] += 16
        v = self.count[key]
        if fn is None:
            fn = lambda e, out=out, in_=in_, kw=kw: e.dma_start(out=out, in_=in_, **kw)
        self.streams[q].append(("o", fn, key, 16))
        for b in writes:
            b.w = (key, v)
            b.r = {}
        for b in reads:
            if b.r.get(key, 0) < v:
                b.r[key] = v
        self.nops += 1

    def barrier(self):
        for e in self.ENG:
            for k in self.sems:
                if k != e or True:
                    self._wait(e, k, self.count[k])

    def finish(self, bufs, eng="sp"):
        for b in bufs:
            if b.w is not None:
                self._wait(eng, b.w[0], b.w[1])

    def emit(self):
        nc = self.nc
        sems = self.sems
        streams = self.streams
        with nc.Block() as block:
            def run(engobj, lst):
                for it in lst:
                    if it[0] == "w":
                        engobj.wait_ge(sems[it[1]], it[2])
                    else:
                        it[1](engobj).then_inc(sems[it[2]], it[3])

            @block.tensor
            def _(e):
                run(e, streams["pe"])

            @block.scalar
            def _(e):
                run(e, streams["act"])

            @block.vector
            def _(e):
                run(e, streams["dve"])

            @block.gpsimd
            def _(e):
                run(e, streams["pool"])

            @block.sync
            def _(e):
                run(e, streams["sp"])


class Ctx:
    def __init__(self, nc, S):
        self.nc = nc
        self.S = S
        self.B = {}
        self.rr = 0
        self.uid = 0

    def buf(self, name):
        if name not in self.B:
            self.B[name] = Buf(name)
        return self.B[name]

    def _bl(self, lst):
        return [self.buf(x) if isinstance(x, str) else x for x in lst]

    def sb(self, st, name, shape, dt):
        self.uid += 1
        t = st.enter_context(self.nc.sbuf_tensor("%s_u%d" % (name, self.uid), list(shape), dt))
        self.buf(name)
        return t

    def ps(self, st, name, shape, dt):
        self.uid += 1
        t = st.enter_context(self.nc.psum_tensor("%s_u%d" % (name, self.uid), list(shape), dt))
        self.buf(name)
        return t

    def op(self, eng, method, reads, writes, **kw):
        self.S.op(eng, lambda e, m=method, kw=kw: getattr(e, m)(**kw), self._bl(reads), self._bl(writes))

    def mm(self, out, lhsT, rhs, reads, writes, start=True, stop=True, **kw):
        self.S.op("pe", lambda e: e.matmul(out, lhsT, rhs, start=start, stop=stop, **kw),
                  self._bl(reads), self._bl(writes))

    def tr(self, out, in_, ident, reads, writes):
        self.S.op("pe", lambda e: e.transpose(out, in_, ident), self._bl(reads), self._bl(writes))

    def dma(self, q, out, in_, reads, writes, **kw):
        self.S.dma(q, out, in_, self._bl(reads), self._bl(writes), **kw)

    def q(self):
        self.rr += 1
        return ("sp", "act", "pool")[self.rr % 3]


C_RW = 0
C_Q = 1792
C_CKV = 2304
C_QI = 2432
C_KI = 2688
C_WI = 2752
C_G = 2756
R_RW = 0
R_Q = 1792
R_QI = 2304
R_KI = 2560
R_G = 2624
R_WI = 4672
R_TOT = 4676


def load_cast(K, st, tag, w_ap, kin, n, scale_col=None, dt=BF16, engs=("dve", "pool")):
    nc = K.nc
    kc = kin // 128
    wt = K.sb(st, tag, [128, kc, n], dt)
    src = w_ap.rearrange("(c p) n -> p c n", p=128)
    if True:
        stg = [K.sb(st, "%s_stg%d" % (tag, i), [128, n], F32) for i in range(2)]
        for c in range(kc):
            sg = stg[c % 2]
            nm = "%s_stg%d" % (tag, c % 2)
            K.dma(K.q(), sg[:], src[:, c, :], [], [nm])
            eng = engs[c % len(engs)]
            if scale_col is None:
                K.op(eng, "tensor_copy", [nm], [tag], out=wt[:, c, :], in_=sg[:])
            else:
                K.op(eng, "tensor_scalar", [nm, scale_col[1]], [tag], out=wt[:, c, :], in0=sg[:],
                     scalar1=scale_col[0][:, c:c + 1], scalar2=None, op0=ALU.mult)
    return wt


def norm_rows(K, tag, xt, xt_name, ss, junk, eps_scale=1.0 / D):
    K.op("act", "activation", [xt_name], [tag + "_junk", tag + "_ss"], out=junk[:], in_=xt[:], func=AF.Square,
         accum_out=ss[:])
    K.op("act", "activation", [tag + "_ss"], [tag + "_ss"], out=ss[:], in_=ss[:], func=AF.Sqrt,
         scale=eps_scale, bias=1e-6)
    K.op("dve", "reciprocal", [tag + "_ss"], [tag + "_ss"], out=ss[:], in_=ss[:])


def phase1(K, s, T, X, Wd, SC, CONST):
    nc = K.nc
    NT = T // 128
    NB = T // 512
    with ExitStack() as st:
        xnT = K.sb(st, "p1_xnT", [128, 8, T], BF16)
        gm = K.sb(st, "p1_gm", [128, 8], F32)
        K.dma("sp", gm[:], Wd["norm_mix"].rearrange("o (c p) -> p (o c)", p=128), [], ["p1_gm"], allow_slow_non_contiguous=True)
        bg = K.sb(st, "p1_bg", [128, 16], F32)
        K.dma("sp", bg[:], Wd["b_gate"].rearrange("o (c p) -> p (o c)", p=128), [], ["p1_bg"], allow_slow_non_contiguous=True)
        identb = CONST["identb"]
        pst = K.ps(st, "p1_pst", [128, 8, 128], BF16)
        xts = [K.sb(st, "p1_xt%d" % i, [128, D], F32) for i in range(2)]
        xnb = [K.sb(st, "p1_xn%d" % i, [128, D], BF16) for i in range(2)]
        junk = K.sb(st, "p1_junk", [128, D], F32)
        sss = [K.sb(st, "p1_ss%d" % i, [128, 1], F32) for i in range(2)]
        for tt in range(NT):
            i = tt % 2
            xt, xn, ss = xts[i], xnb[i], sss[i]
            K.dma("sp" if i == 0 else "act", xt[:], X[s * T + tt * 128: s * T + (tt + 1) * 128, :], [], ["p1_xt%d" % i])
            K.op("act", "activation", ["p1_xt%d" % i], ["p1_junk", "p1_ss%d" % i], out=junk[:], in_=xt[:],
                 func=AF.Square, accum_out=ss[:])
            K.op("act", "activation", ["p1_ss%d" % i, "eps6"], ["p1_ss%d" % i], out=ss[:], in_=ss[:], func=AF.Sqrt,
                 scale=1.0 / D, bias=CONST["eps6"][:])
            K.op("dve", "reciprocal", ["p1_ss%d" % i], ["p1_ss%d" % i], out=ss[:], in_=ss[:])
            K.op("dve", "tensor_scalar", ["p1_xt%d" % i, "p1_ss%d" % i], ["p1_xn%d" % i], out=xn[:], in0=xt[:],
                 scalar1=ss[:], scalar2=None, op0=ALU.mult)
            for c in range(8):
                K.tr(pst[:, c, :], xn[:, c * 128:(c + 1) * 128], identb[:], ["p1_xn%d" % i, "identb"], ["p1_pst"])
            K.op("pool" if False else "act", "activation", ["p1_pst"], ["p1_xnT"], out=xnT[:, :, tt * 128:(tt + 1) * 128],
                 in_=pst[:], func=AF.Copy)
        import os
        STOP = int(os.environ.get("STOP", "99"))
        if STOP <= 1:
            return
        chunks = []
        for i in range(14):
            chunks.append((C_RW + i * 128, 128, R_RW + i * 128, "fm", None))
        for i in range(4):
            chunks.append((C_Q + i * 128, 128, R_Q + i * 128, "fm", None))
        for i in range(2):
            chunks.append((C_QI + i * 128, 128, R_QI + i * 128, "fm", None))
        chunks.append((C_KI, 64, R_KI, "fm", None))
        chunks.append((C_WI, 4, R_WI, "fm", None))
        for i in range(16):
            chunks.append((C_G + i * 128, 128, R_G + i * 128, "gate", i))
        wsrc = Wd["w_in"].rearrange("o (c p) n -> p (o c) n", p=128)
        wst = [K.sb(st, "p1_wst%d" % i, [128, 8, 132], F32) for i in range(2)]
        wbf = [K.sb(st, "p1_wbf%d" % i, [128, 8, 132], BF16) for i in range(2)]
        stage = [K.sb(st, "p1_stage%d" % i, [128, T], F32) for i in range(2)]
        pss = [K.ps(st, "p1_ps%d" % i, [128, 512], F32) for i in range(4)]
        gmb = gm[:].unsqueeze(2).to_broadcast([128, 8, 128])
        ZF = SC["ZF"]
        for ci, (c0, ncol, r0, kind, gi) in enumerate(chunks):
            i = ci % 2
            K.dma("sp" if i == 0 else "pool", wst[i][:, :, 0:ncol], wsrc[:, :, c0:c0 + ncol], [], ["p1_wst%d" % i])
            K.op("dve", "tensor_tensor", ["p1_wst%d" % i, "p1_gm"], ["p1_wbf%d" % i], out=wbf[i][:, :, 0:ncol],
                 in0=wst[i][:, :, 0:ncol], in1=gm[:].unsqueeze(2).to_broadcast([128, 8, ncol]), op=ALU.mult)
            for tb in range(NB):
                pj = (ci * NB + tb) % 4
                ps = pss[pj]
                for dc in range(8):
                    K.mm(ps[0:ncol, :], wbf[i][:, dc, 0:ncol], xnT[:, dc, tb * 512:(tb + 1) * 512],
                         ["p1_wbf%d" % i, "p1_xnT"], ["p1_ps%d" % pj], start=(dc == 0), stop=(dc == 7))
                if kind == "gate":
                    K.op("act", "activation", ["p1_ps%d" % pj, "p1_bg"], ["p1_stage%d" % i],
                         out=stage[i][0:ncol, tb * 512:(tb + 1) * 512], in_=ps[0:ncol, :], func=AF.Sigmoid,
                         bias=bg[:, gi:gi + 1])
                else:
                    eng = "dve" if tb % 2 == 0 else "act"
                    if eng == "dve":
                        K.op("dve", "tensor_copy", ["p1_ps%d" % pj], ["p1_stage%d" % i],
                             out=stage[i][0:ncol, tb * 512:(tb + 1) * 512], in_=ps[0:ncol, :])
                    else:
                        K.op("act", "activation", ["p1_ps%d" % pj], ["p1_stage%d" % i],
                             out=stage[i][0:ncol, tb * 512:(tb + 1) * 512], in_=ps[0:ncol, :], func=AF.Copy)
            K.dma("act" if i == 0 else "sp", ZF[s, r0:r0 + ncol, :], stage[i][0:ncol, :], ["p1_stage%d" % i], ["ZF"])
        if STOP <= 2:
            return
        i = len(chunks) % 2
        K.dma("sp", wst[i][:, :, 0:128], wsrc[:, :, C_CKV:C_CKV + 128], [], ["p1_wst%d" % i])
        K.op("dve", "tensor_tensor", ["p1_wst%d" % i, "p1_gm"], ["p1_wbf%d" % i], out=wbf[i][:, :, 0:128],
             in0=wst[i][:, :, 0:128], in1=gm[:].unsqueeze(2).to_broadcast([128, 8, 128]), op=ALU.mult)
        ck = [K.sb(st, "p1_ck%d" % j, [128, 128], F32) for j in range(2)]
        ckb = [K.sb(st, "p1_ckb%d" % j, [128, 128], BF16) for j in range(2)]
        ckT = K.sb(st, "p1_ckT", [128, T], BF16)
        for tt in range(NT):
            j = tt % 2
            pj = tt % 4
            ps = pss[pj]
            for dc in range(8):
                K.mm(ps[:, 0:128], xnT[:, dc, tt * 128:(tt + 1) * 128], wbf[i][:, dc, 0:128],
                     ["p1_wbf%d" % i, "p1_xnT"], ["p1_ps%d" % pj], start=(dc == 0), stop=(dc == 7))
            K.op("dve", "tensor_copy", ["p1_ps%d" % pj], ["p1_ck%d" % j], out=ck[j][:], in_=ps[:, 0:128])
            K.op("act", "activation", ["p1_ck%d" % j], ["p1_junk", "p1_ss%d" % j], out=junk[:, 0:128], in_=ck[j][:],
                 func=AF.Square, accum_out=sss[j][:])
            K.op("act", "activation", ["p1_ss%d" % j, "eps6"], ["p1_ss%d" % j], out=sss[j][:], in_=sss[j][:], func=AF.Sqrt,
                 scale=1.0 / 128, bias=CONST["eps6"][:])
            K.op("dve", "reciprocal", ["p1_ss%d" % j], ["p1_ss%d" % j], out=sss[j][:], in_=sss[j][:])
            K.op("dve", "tensor_scalar", ["p1_ck%d" % j, "p1_ss%d" % j], ["p1_ckb%d" % j], out=ckb[j][:], in0=ck[j][:],
                 scalar1=sss[j][:], scalar2=None, op0=ALU.mult)
            K.tr(pst[:, 0, :], ckb[j][:], identb[:], ["p1_ckb%d" % j, "identb"], ["p1_pst"])
            K.op("act", "activation", ["p1_pst"], ["p1_ckT"], out=ckT[:, tt * 128:(tt + 1) * 128], in_=pst[:, 0, :],
                 func=AF.Copy)
            K.dma("sp", SC["CK"][s, tt * 128:(tt + 1) * 128, :], ckb[j][:], ["p1_ckb%d" % j], ["CK"])
        K.dma("sp", SC["CKT"][s, :, :], ckT[:], ["p1_ckT"], ["CKT"])


NIT = 14


def phase_dsa(K, s, T, Wd, SC, CONST, extra=None):
    nc = K.nc
    NT = T // 128
    ZF = SC["ZF"]
    identb, identf = CONST["identb"], CONST["identf"]
    with ExitStack() as st:
        dps = [K.ps(st, "ds_ps%d" % i, [128, 512], F32) for i in range(4)]
        Ob = [K.ps(st, "ds_o%d" % i, [128, 3, 130], F32) for i in range(3)]
        MT = K.ps(st, "ds_mt", [128, 8, 128], BF16)
        wuk = K.sb(st, "ds_wuk", [128, 512], F32)
        K.dma("sp", wuk[:], Wd["w_uk"].rearrange("o r h d -> r (o h d)"), [], ["ds_wuk"])
        wukT = K.sb(st, "ds_wukT", [64, 8, 128], BF16)
        for h in range(8):
            K.tr(dps[3][0:64, 0:128], wuk[:, h * 64:(h + 1) * 64], identf[:], ["ds_wuk", "identf"], ["ds_ps3"])
            K.op("dve", "tensor_copy", ["ds_ps3"], ["ds_wukT"], out=wukT[:, h, :], in_=dps[3][0:64, 0:128])
        kvn = K.sb(st, "ds_kvn", [128, 1], F32)
        K.dma("sp", kvn[:], Wd["kv_norm"].rearrange("o r -> r o"), [], ["ds_kvn"], allow_slow_non_contiguous=True)
        kvn8 = K.sb(st, "ds_kvn8", [128, 1], F32)
        K.op("dve", "tensor_scalar", ["ds_kvn"], ["ds_kvn8"], out=kvn8[:], in0=kvn[:], scalar1=0.125, scalar2=None,
             op0=ALU.mult)
        wuv = K.sb(st, "ds_wuv", [128, 512], F32)
        K.dma("sp", wuv[:], Wd["w_uv"].rearrange("o r h d -> r (o h d)"), [], ["ds_wuv"])
        wuvb = K.sb(st, "ds_wuvb", [128, 512], BF16)
        K.op("dve", "tensor_scalar", ["ds_wuv", "ds_kvn"], ["ds_wuvb"], out=wuvb[:], in0=wuv[:], scalar1=kvn[:],
             scalar2=None, op0=ALU.mult)
        CKT = K.sb(st, "ds_ckt", [128, T], BF16)
        K.dma("sp", CKT[:], SC["CKT"][s, :, :], ["CKT"], ["ds_ckt"])
        CKA = K.sb(st, "ds_cka", [128, NT, 130], BF16)
        K.op("pool", "memset", [], ["ds_cka"], ap=CKA[:], constant=1.0)
        K.dma("sp", CKA[:, :, 0:128], SC["CK"][s, :, :].rearrange("(k p) r -> p k r", p=128), ["CK"], ["ds_cka"])
        kif = K.sb(st, "ds_kif", [64, T], F32)
        K.dma("act", kif[:], ZF[s, R_KI:R_KI + 64, :], ["ZF"], ["ds_kif"])
        kib = K.sb(st, "ds_kib", [64, T], BF16)
        K.op("dve", "tensor_copy", ["ds_kif"], ["ds_kib"], out=kib[:], in_=kif[:])
        qib = K.sb(st, "ds_qib", [64, 4, 128], BF16)
        zl = K.sb(st, "ds_zl", [128, 128], BF16)
        zb = K.sb(st, "ds_zb", [128, 390], BF16)
        K.op("pool", "memset", [], ["ds_zl"], ap=zl[:], constant=0.0)
        K.op("pool", "memset", [], ["ds_zb"], ap=zb[:], constant=0.0)
        tri01, negtri, pw = CONST["tri01"], CONST["negtri"], CONST["pw"]
        qf = K.sb(st, "ds_qf", [64, 8, 128], F32)
        qb = K.sb(st, "ds_qb", [64, 8, 128], BF16)
        qif = K.sb(st, "ds_qif", [64, 4, 128], F32)
        wif = K.sb(st, "ds_wif", [4, 128], F32)
        wit = K.sb(st, "ds_wit", [128, 4], F32)
        qlat = K.sb(st, "ds_qlat", [128, 1024], BF16)
        isc = K.sb(st, "ds_isc", [128, T], F32)
        junk = K.sb(st, "ds_junk", [128, T], BF16)
        rl = [K.sb(st, "ds_rl%d" % i, [128, 512], F32) for i in range(3)]
        maskb = K.sb(st, "ds_mask", [128, T], BF16)
        col = K.sb(st, "ds_col", [128, 8], F32)
        hk = K.sb(st, "ds_hk", [128, NIT], F32)
        junk2 = K.sb(st, "ds_junk2", [128, T], BF16)
        cola = K.sb(st, "ds_cola", [128, 1], F32)
        mts = [K.sb(st, "ds_mts%d" % i, [128, 128], BF16) for i in range(2)]
        ee = [K.sb(st, "ds_e%d" % i, [128, 4, 128], BF16) for i in range(4)]
        pp = [K.sb(st, "ds_p%d" % i, [128, 4, 128], BF16) for i in range(4)]
        rd = K.sb(st, "ds_rd", [128, 8, 1], F32)
        onb = K.sb(st, "ds_onb", [128, 8, 128], BF16)
        onT = K.sb(st, "ds_onT", [128, 8, 128], BF16)
        ybs = K.sb(st, "ds_ybs", [128, 4, 128], BF16)
        maskbs = [maskb, K.sb(st, "ds_mask1", [128, T], BF16)]
        MN = ["ds_mask", "ds_mask1"]

        def select(qt):
            t0 = qt * 128
            nk = qt + 1
            nkeys = nk * 128
            mb = maskbs[qt % 2]
            mn = MN[qt % 2]
            if qt >= 2:
                K.dma("act", qif[:], ZF[s, R_QI:R_QI + 256, t0:t0 + 128].rearrange("(h p) t -> p h t", p=64), ["ZF"], ["ds_qif"])
                K.op("act", "activation", ["ds_qif"], ["ds_qib"], out=qib[:], in_=qif[:], func=AF.Copy)
                K.dma("act", wif[:], ZF[s, R_WI:R_WI + 4, t0:t0 + 128], ["ZF"], ["ds_wif"])
                K.tr(dps[3][:, 0:4], wif[:], identf[0:4, 0:4], ["ds_wif", "identf"], ["ds_ps3"])
                K.op("dve", "tensor_scalar", ["ds_ps3"], ["ds_wit"], out=wit[:], in0=dps[3][:, 0:4], scalar1=1.0 / 16,
                     scalar2=None, op0=ALU.mult)
                yield
                for kb in range((nkeys + 511) // 512):
                    w = min(512, nkeys - kb * 512)
                    ks = slice(kb * 512, kb * 512 + w)
                    for h in range(4):
                        pb = 2 + (h % 2)
                        K.mm(dps[pb][:, 0:w], qib[:, h, :], kib[:, ks], ["ds_qib", "ds_kib"], ["ds_ps%d" % pb])
                        if h == 0:
                            K.op("dve", "tensor_scalar", ["ds_ps%d" % pb, "ds_wit"], ["ds_isc"], out=isc[:, ks], in0=dps[pb][:, 0:w],
                                 scalar1=0.0, scalar2=wit[:, 0:1], op0=ALU.max, op1=ALU.mult)
                        else:
                            K.op("act", "activation", ["ds_ps%d" % pb], ["ds_rl%d" % (h - 1)], out=rl[h - 1][:, 0:w],
                                 in_=dps[pb][:, 0:w], func=AF.Relu)
                            K.op("dve", "scalar_tensor_tensor", ["ds_rl%d" % (h - 1), "ds_wit", "ds_isc"], ["ds_isc"],
                                 out=isc[:, ks], in0=rl[h - 1][:, 0:w], scalar=wit[:, h:h + 1], in1=isc[:, ks],
                                 op0=ALU.mult, op1=ALU.add)
                    yield
                K.op("dve", "tensor_reduce", ["ds_isc"], ["ds_col"], out=col[:, 0:1], in_=isc[:, 0:nkeys], axis=AX.X, op=ALU.max)
                K.op("dve", "tensor_reduce", ["ds_isc"], ["ds_col"], out=col[:, 1:2], in_=isc[:, 0:nkeys], axis=AX.X, op=ALU.min)
                K.op("dve", "tensor_scalar", ["ds_col"], ["ds_col"], out=col[:, 2:3], in0=col[:, 0:1], scalar1=col[:, 1:2],
                     scalar2=2e-6, op0=ALU.subtract, op1=ALU.add)
                K.op("dve", "tensor_scalar", ["ds_col"], ["ds_col"], out=col[:, 3:4], in0=col[:, 1:2], scalar1=-1e-6,
                     scalar2=None, op0=ALU.add)
                K.op("dve", "tensor_scalar", ["pw", "ds_col"], ["ds_hk"], out=hk[:], in0=pw[:], scalar1=col[:, 2:3],
                     scalar2=None, op0=ALU.mult)
                K.op("dve", "tensor_tensor", ["ds_isc", "negtri"], ["ds_isc"], out=isc[:, t0:t0 + 128], in0=isc[:, t0:t0 + 128],
                     in1=negtri[:], op=ALU.add)
                K.op("dve", "tensor_tensor", ["ds_col", "ds_hk"], ["ds_col", "ds_colm"], out=col[:, 4:5], in0=col[:, 3:4], in1=hk[:, 0:1], op=ALU.add)
                yield
                nd = nkeys
                if nkeys >= 1024 and not os.environ.get("NO_ACTCNT"):
                    nd = ((nkeys * 5 // 8) // 128) * 128
                na = nkeys - nd
                for k in range(NIT):
                    K.op("dve", "tensor_scalar", ["ds_isc", "ds_colm"], ["ds_junk", "ds_col"], out=junk[:, 0:nd],
                         in0=isc[:, 0:nd], scalar1=col[:, 4:5], scalar2=None, op0=ALU.is_ge, op1=ALU.add,
                         accum_out=col[:, 5:6])
                    if na > 0:
                        K.op("act", "activation", ["ds_isc", "ds_colm"], ["ds_junk2", "ds_cola"], out=junk2[:, 0:na], in_=isc[:, nd:nkeys],
                             func=AF.Sign, scale=-1.0, bias=col[:, 4:5], accum_out=cola[:, 0:1])
                        K.op("dve", "scalar_tensor_tensor", ["ds_cola", "ds_col"], ["ds_col"], out=col[:, 5:6], in0=cola[:, 0:1], scalar=-0.5,
                             in1=col[:, 5:6], op0=ALU.mult, op1=ALU.add)
                    K.op("dve", "tensor_scalar", ["ds_col", "ds_hk"], ["ds_col"], out=col[:, 6:7], in0=col[:, 5:6],
                         scalar1=255.5 - 0.5 * na, scalar2=hk[:, k:k + 1], op0=ALU.is_ge, op1=ALU.mult)
                    kn = min(k + 1, NIT - 1)
                    dst = col[:, 4:5] if k < NIT - 1 else col[:, 3:4]
                    K.op("dve", "scalar_tensor_tensor", ["ds_col", "ds_colm", "ds_hk"], ["ds_col", "ds_colm"], out=dst, in0=col[:, 6:7], scalar=col[:, 4:5],
                         in1=hk[:, kn:kn + 1], op0=ALU.add, op1=ALU.subtract)
                    yield
                K.op("dve", "tensor_scalar", ["ds_isc", "ds_col"], [mn], out=mb[:, 0:nkeys], in0=isc[:, 0:nkeys],
                     scalar1=col[:, 3:4], scalar2=None, op0=ALU.is_ge)
            else:
                if qt > 0:
                    K.op("pool", "memset", [], [mn], ap=mb[:, 0:t0], constant=1.0)
                K.op("pool", "tensor_copy", ["tri01"], [mn], out=mb[:, t0:t0 + 128], in_=tri01[:])
            yield

        def attend(qt):
            t0 = qt * 128
            nk = qt + 1
            mb = maskbs[qt % 2]
            mn = MN[qt % 2]
            K.dma("sp", qf[:], ZF[s, R_Q:R_Q + 512, t0:t0 + 128].rearrange("(h p) t -> p h t", p=64), ["ZF"], ["ds_qf"])
            K.op("pool", "tensor_copy", ["ds_qf"], ["ds_qb"], out=qb[:], in_=qf[:])
            for h in range(8):
                K.mm(dps[h // 4][:, (h % 4) * 128:(h % 4 + 1) * 128], wukT[:, h, :], qb[:, h, :], ["ds_wukT", "ds_qb"],
                     ["ds_ps%d" % (h // 4)])
            for j in range(2):
                K.op("act", "activation", ["ds_ps%d" % j, "ds_kvn8"], ["ds_qlat"], out=qlat[:, j * 512:(j + 1) * 512],
                     in_=dps[j][:], func=AF.Copy, scale=kvn8[:, 0:1])
            for bq in range(3):
                K.mm(Ob[bq][:].rearrange("p a b -> p (a b)"), zl[:], zb[:], ["ds_zl", "ds_zb"], ["ds_o%d" % bq], start=True,
                     stop=False, skip_group_check=True)
            yield
            def front(kt):
                par = kt % 2
                K.tr(MT[:, 0, :], mb[:, kt * 128:(kt + 1) * 128], identb[:], [mn, "identb"], ["ds_mt0", "ds_mt1", "ds_mtall"])
                K.op("act", "activation", ["ds_mt0", "ds_mt1", "ds_mtall"], ["ds_mts%d" % par], out=mts[par][:], in_=MT[:, 0, :], func=AF.Copy)
                for j in range(2):
                    ej = 2 * par + j
                    K.mm(dps[j][:], CKT[:, kt * 128:(kt + 1) * 128], qlat[:, j * 512:(j + 1) * 512], ["ds_ckt", "ds_qlat"],
                         ["ds_ps%d" % j])
                    K.op("act", "activation", ["ds_ps%d" % j], ["ds_e%d" % ej], out=ee[ej][:],
                         in_=dps[j][:].rearrange("p (a b) -> p a b", a=4), func=AF.Exp)
            front(0)
            for kt in range(nk):
                par = kt % 2
                mtb = "ds_mt%d" % par
                if kt + 1 < nk:
                    front(kt + 1)
                for j in range(2):
                    ej = 2 * par + j
                    K.op("dve", "tensor_tensor", ["ds_e%d" % ej, "ds_mts%d" % par], ["ds_p%d" % ej], out=pp[ej][:], in0=ee[ej][:],
                         in1=mts[par][:].unsqueeze(1).to_broadcast([128, 4, 128]), op=ALU.mult)
                for j in range(2):
                    ej = 2 * par + j
                    for hh in range(4):
                        h = 4 * j + hh
                        K.mm(Ob[h // 3][:, h % 3, 0:129], pp[ej][:, hh, :], CKA[:, kt, 0:129], ["ds_p%d" % ej, "ds_cka"],
                             ["ds_o%d" % (h // 3)], start=False, stop=(kt == nk - 1), skip_group_check=True)
                yield
            for bq in range(3):
                nh = 3 if bq < 2 else 2
                K.op("dve", "reciprocal", ["ds_o%d" % bq], ["ds_rd"], out=rd[:, 3 * bq:3 * bq + nh, :], in_=Ob[bq][:, 0:nh, 128:129])
                K.op("dve", "tensor_tensor", ["ds_o%d" % bq, "ds_rd"], ["ds_onb"], out=onb[:, 3 * bq:3 * bq + nh, :],
                     in0=Ob[bq][:, 0:nh, 0:128], in1=rd[:, 3 * bq:3 * bq + nh, :].to_broadcast([128, nh, 128]), op=ALU.mult)
            for h in range(8):
                K.tr(MT[:, h, :], onb[:, h, :], identb[:], ["ds_onb", "identb"], ["ds_mt0", "ds_mt1", "ds_mtall"])
            K.op("act", "activation", ["ds_mt0", "ds_mt1", "ds_mtall"], ["ds_onT"], out=onT[:], in_=MT[:], func=AF.Copy)
            for h in range(8):
                K.mm(dps[0][(h % 2) * 64:(h % 2 + 1) * 64, (h // 2) * 128:(h // 2 + 1) * 128], wuvb[:, h * 64:(h + 1) * 64],
                     onT[:, h, :], ["ds_wuvb", "ds_onT"], ["ds_ps0"])
            K.op("dve", "tensor_copy", ["ds_ps0"], ["ds_ybs"], out=ybs[:], in_=dps[0][:].rearrange("p (a b) -> p a b", a=4))
            K.dma("sp", SC["YB"][s, :, t0:t0 + 128].rearrange("(c p) t -> p c t", p=128), ybs[:], ["ds_ybs"], ["YB"])
            yield

        xg = extra(st) if extra is not None else None
        for step in range(NT + 1):
            gens = []
            if step >= 1:
                gens.append(attend(step - 1))
            if step < NT:
                gens.append(select(step))
            if xg is not None:
                try:
                    next(xg)
                except StopIteration:
                    xg = None
            while gens:
                for g in list(gens):
                    try:
                        next(g)
                    except StopIteration:
                        gens.remove(g)
        if xg is not None:
            for _ in xg:
                pass


class _Stop(Exception):
    pass


def phase_rwkv(K, s, T, Wd, SC, CONST):
    _phase_rwkv(K, s, T, Wd, SC, CONST)
    K.S.muted = False


def _phase_rwkv(K, s, T, Wd, SC, CONST):
    nc = K.nc
    TBK = 256
    NCH = TBK // 64
    NBK = T // TBK
    ZF = SC["ZF"]
    identf = CONST["identf"]
    bo, bo64, maskq, lowm, resetm = CONST["bo"], CONST["bo64"], CONST["maskq"], CONST["lowm"], CONST["resetm"]
    with ExitStack() as st:
        rp = [K.ps(st, "rk_p%d" % i, [128, 512], F32) for i in range(8)]
        RP = ["rk_p%d" % i for i in range(8)]

        def colload(tag, ap512, n=4):
            t = K.sb(st, tag, [128, n], F32)
            K.dma("sp", t[:], ap512.rearrange("o (c p) -> p (o c)", p=128), [], [tag], allow_slow_non_contiguous=True)
            return t
        mu = colload("rk_mu", Wd["shift_mu"], 14)
        w0c = colload("rk_w0c", Wd["rw_w0"])
        a0c = colload("rk_a0c", Wd["rw_a0"])
        kkc = colload("rk_kkc", Wd["rw_k_k"])
        kac = colload("rk_kac", Wd["rw_k_a"])
        rkc = colload("rk_rkc", Wd["rw_r_k"].rearrange("o h d -> o (h d)"))
        lnw = colload("rk_lnw", Wd["rw_ln_w"])
        lnb = colload("rk_lnb", Wd["rw_ln_b"])
        w2a2 = K.sb(st, "rk_w2a2", [128, 512], F32)
        K.dma("sp", w2a2[0:64, :], Wd["rw_w2"][0], [], ["rk_w2a2"])
        K.dma("sp", w2a2[64:128, :], Wd["rw_a2"][0], [], ["rk_w2a2"])
        g2 = K.sb(st, "rk_g2", [128, 512], F32)
        K.dma("sp", g2[:], Wd["rw_g2"][0], [], ["rk_g2"])
        epsg = K.sb(st, "rk_epsg", [128, 1], F32)
        K.op("dve", "memset", [], ["rk_epsg"], ap=epsg[:], constant=64e-5)
        zin = K.sb(st, "rk_zin", [128, 14, TBK + 1], F32)
        zs = K.sb(st, "rk_zs", [128, 14, TBK], F32)
        tw = K.sb(st, "rk_tw", [128, TBK], F32)
        sg = K.sb(st, "rk_sg", [128, TBK], F32)

        def t4(tag):
            return K.sb(st, tag, [128, 4, TBK], F32)
        lw, aa, gg, LL, eL, enL, eLm, kk, t1, kp, bb, bon, Yb = [t4("rk_" + n) for n in
            ("lw", "aa", "gg", "LL", "eL", "enL", "eLm", "kk", "t1", "kp", "bb", "bon", "Yb")]
        QR = K.sb(st, "rk_QR", [128, 4, NCH, 2, 64], F32)
        KB = K.sb(st, "rk_KB", [128, 4, NCH, 2, 64], F32)
        gC = K.sb(st, "rk_gC", [128, 4, NCH], F32)
        M = K.sb(st, "rk_M", [128, 4, 64], F32)
        K.op("dve", "memset", [], ["rk_M"], ap=M[:], constant=0.0)
        KBTs = [K.sb(st, "rk_KBT%d" % i, [128, 4, 128], F32) for i in range(2)]
        VTs = [K.sb(st, "rk_VT%d" % i, [64, 4, 128], F32) for i in range(2)]
        ATs = [K.sb(st, "rk_AT%d" % i, [128, 8, 128], F32) for i in range(2)]
        DDT = BF16 if os.environ.get("RW_BF16", "1") == "1" else F32
        Am = [K.sb(st, "rk_Am%d" % i, [128, 8, 64], DDT) for i in range(2)]
        Bm = [K.sb(st, "rk_Bm%d" % i, [128, 8, 64], DDT) for i in range(2)]
        Pm = [K.sb(st, "rk_Pm%d" % i, [128, 8, 64], DDT) for i in range(2)]
        PmFs = [K.sb(st, "rk_PmF%d" % i, [128, 8, 64], F32) for i in range(2)]
        Rs = K.sb(st, "rk_Rs", [128, 512], F32)
        Us = K.sb(st, "rk_Us", [128, 512], F32)
        yab = K.sb(st, "rk_yab", [128, 4, TBK], BF16)
        H = slice(64, 128)

        def v4(t):
            return t[:].rearrange("p c (n t) -> p c n t", t=64)

        def bc(colt, n=4, w=TBK):
            return colt[:].unsqueeze(2).to_broadcast([128, n, w])

        RS = float(os.environ.get("RSTOP", "99"))

        def chk(k):
            if RS <= k:
                K.S.muted = True

        for tb in range(NBK):
            t0 = tb * TBK
            if tb == 0:
                K.op("dve", "memset", [], ["rk_zin"], ap=zin[:, :, 0:1], constant=0.0)
                K.dma("sp", zin[:, :, 1:TBK + 1], ZF[s, 0:1792, 0:TBK].rearrange("(c p) t -> p c t", p=128), ["ZF"], ["rk_zin"])
            else:
                K.dma("sp", zin[:, :, :], ZF[s, 0:1792, t0 - 1:t0 + TBK].rearrange("(c p) t -> p c t", p=128), ["ZF"], ["rk_zin"])
            K.op("dve", "tensor_tensor", ["rk_zin"], ["rk_zs"], out=zs[:], in0=zin[:, :, 0:TBK], in1=zin[:, :, 1:TBK + 1], op=ALU.subtract)
            for c14 in range(14):
                K.op("dve", "scalar_tensor_tensor", ["rk_zs", "rk_mu", "rk_zin"], ["rk_zs"], out=zs[:, c14, :], in0=zs[:, c14, :], scalar=mu[:, c14:c14 + 1],
                     in1=zin[:, c14, 1:TBK + 1], op0=ALU.mult, op1=ALU.add)
            chk(1)
            r_, k_, v_ = zs[:, 0:4, :], zs[:, 4:8, :], zs[:, 8:12, :]
            K.op("act", "activation", ["rk_zs"], ["rk_tw"], out=tw[0:64, :], in_=zs[0:64, 12, :], func=AF.Tanh)
            K.op("act", "activation", ["rk_zs"], ["rk_sg"], out=sg[:], in_=zs[:, 13, :], func=AF.Sigmoid)
            for cc in range(4):
                cs = slice(cc * 128, (cc + 1) * 128)
                K.mm(rp[0][:, 0:TBK], w2a2[0:64, cs], tw[0:64, :], ["rk_w2a2", "rk_tw"], [RP[0]])
                K.op("act", "activation", [RP[0], "rk_w0c"], ["rk_lw"], out=lw[:, cc, :], in_=rp[0][:, 0:TBK], func=AF.Sigmoid, bias=w0c[:, cc:cc + 1])
                K.mm(rp[1][:, 0:TBK], w2a2[H, cs], zs[H, 12, :], ["rk_w2a2", "rk_zs"], [RP[1]])
                K.op("act", "activation", [RP[1], "rk_a0c"], ["rk_aa"], out=aa[:, cc, :], in_=rp[1][:, 0:TBK], func=AF.Sigmoid, bias=a0c[:, cc:cc + 1])
                K.mm(rp[2][:, 0:TBK], g2[:, cs], sg[:], ["rk_g2", "rk_sg"], [RP[2]])
                K.op("dve", "tensor_copy", [RP[2]], ["rk_gg"], out=gg[:, cc, :], in_=rp[2][:, 0:TBK])
            chk(2)
            K.op("dve", "tensor_scalar", ["rk_lw"], ["rk_lw"], out=lw[:], in0=lw[:], scalar1=-0.6065306597126334, scalar2=None, op0=ALU.mult)
            for cc in range(4):
                K.op("dve", "tensor_tensor_scan", ["rk_lw", "resetm"], ["rk_LL"], out=LL[:, cc, :], data0=resetm[:], data1=lw[:, cc, :],
                     initial=0.0, op0=ALU.mult, op1=ALU.add)
            K.op("act", "activation", ["rk_LL"], ["rk_eL"], out=eL[:], in_=LL[:], func=AF.Exp)
            K.op("act", "activation", ["rk_LL"], ["rk_enL"], out=enL[:], in_=LL[:], func=AF.Exp, scale=-1.0)
            K.op("pool", "tensor_tensor", ["rk_LL", "rk_lw"], ["rk_t1"], out=t1[:], in0=LL[:], in1=lw[:], op=ALU.subtract)
            K.op("act", "activation", ["rk_t1"], ["rk_eLm"], out=eLm[:], in_=t1[:], func=AF.Exp)
            K.op("dve", "tensor_tensor", ["rk_zs", "rk_kkc"], ["rk_kk"], out=kk[:], in0=k_, in1=bc(kkc), op=ALU.mult)
            K.op("pool", "tensor_tensor", ["rk_kk"], ["rk_t1"], out=t1[:], in0=kk[:], in1=kk[:], op=ALU.mult)
            for cc in range(4):
                K.mm(rp[cc % 4][:, 0:TBK], bo[:], t1[:, cc, :], ["bo", "rk_t1"], [RP[cc % 4]])
                K.op("act", "activation", [RP[cc % 4]], ["rk_kp"], out=kp[:, cc, :], in_=rp[cc % 4][:, 0:TBK], func=AF.Sqrt)
            K.op("dve", "tensor_scalar", ["rk_kp"], ["rk_kp"], out=kp[:], in0=kp[:], scalar1=1e-12, scalar2=None, op0=ALU.max)
            K.op("dve", "reciprocal", ["rk_kp"], ["rk_kp"], out=kp[:], in_=kp[:])
            K.op("dve", "tensor_tensor", ["rk_kk", "rk_kp"], ["rk_kk"], out=kk[:], in0=kk[:], in1=kp[:], op=ALU.mult)
            for cc in range(4):
                K.op("dve", "tensor_scalar", ["rk_aa", "rk_kac"], ["rk_t1"], out=t1[:, cc, :], in0=aa[:, cc, :], scalar1=-1.0, scalar2=kac[:, cc:cc + 1],
                     op0=ALU.add, op1=ALU.mult)
            K.op("dve", "scalar_tensor_tensor", ["rk_t1", "rk_zs"], ["rk_kp"], out=kp[:], in0=t1[:], scalar=1.0, in1=k_, op0=ALU.add, op1=ALU.mult)
            K.op("pool", "tensor_tensor", ["rk_kk", "rk_aa"], ["rk_bb"], out=bb[:], in0=kk[:], in1=aa[:], op=ALU.mult)
            K.op("dve", "tensor_tensor", ["rk_zs", "rk_eL"], ["rk_QR"], out=QR[:, :, :, 1, :], in0=r_.rearrange("p c (n t) -> p c n t", t=64), in1=v4(eL), op=ALU.mult)
            K.op("pool", "tensor_tensor", ["rk_kk", "rk_eLm"], ["rk_QR"], out=QR[:, :, :, 0, :], in0=v4(kk), in1=v4(eLm), op=ALU.mult)
            K.op("dve", "tensor_tensor", ["rk_kp", "rk_enL"], ["rk_KB"], out=KB[:, :, :, 0, :], in0=v4(kp), in1=v4(enL), op=ALU.mult)
            K.op("pool", "tensor_tensor", ["rk_bb", "rk_enL"], ["rk_KB"], out=KB[:, :, :, 1, :], in0=v4(bb), in1=v4(enL), op=ALU.mult)
            K.op("dve", "tensor_copy", ["rk_eL"], ["rk_gC"], out=gC[:], in_=v4(eL)[:, :, :, 63])
            K.op("pool", "tensor_tensor", ["rk_zs", "rk_kp"], ["rk_t1"], out=t1[:], in0=r_, in1=kp[:], op=ALU.mult)
            K.op("pool", "tensor_tensor", ["rk_t1", "rk_rkc"], ["rk_t1"], out=t1[:], in0=t1[:], in1=bc(rkc), op=ALU.mult)
            for cc in range(4):
                K.mm(rp[cc % 4][:, 0:TBK], bo[:], t1[:, cc, :], ["bo", "rk_t1"], [RP[cc % 4]])
                K.op("dve", "tensor_tensor", [RP[cc % 4], "rk_zs"], ["rk_bon"], out=bon[:, cc, :], in0=rp[cc % 4][:, 0:TBK], in1=zs[:, 8 + cc, :], op=ALU.mult)
            chk(3)
            def ev(t, par):
                return t.rearrange("p (a two) b -> p a two b", two=2)[:, :, par, :]

            def pre(c):
                q = c % 2
                KBT, VT, AT, PmF = KBTs[q], VTs[q], ATs[q], PmFs[q]
                nKBT, nVT, nAT, nPmF = "rk_KBT%d" % q, "rk_VT%d" % q, "rk_AT%d" % q, "rk_PmF%d" % q
                for cc in range(4):
                    K.tr(rp[0][:, cc * 128:(cc + 1) * 128], KB[:, cc, c, :, :].rearrange("p a b -> p (a b)"), identf[:], ["rk_KB", "identf"], [RP[0]])
                    K.tr(rp[1][0:64, cc * 128:(cc + 1) * 128], zs[:, 8 + cc, c * 64:(c + 1) * 64], identf[:], ["rk_zs", "identf"], [RP[1]])
                K.op("act", "activation", [RP[0]], [nKBT], out=KBT[:].rearrange("p a b -> p (a b)"), in_=rp[0][:], func=AF.Copy)
                K.op("dve", "tensor_copy", [RP[1]], [nVT], out=VT[:].rearrange("p a b -> p (a b)"), in_=rp[1][0:64, :])
                yield
                for h in range(8):
                    cc, h2 = h // 2, h % 2
                    rows = slice(h2 * 64, (h2 + 1) * 64)
                    K.mm(rp[2 + h2][:, cc * 128:(cc + 1) * 128], KB[rows, cc, c, :, :].rearrange("p a b -> p (a b)"),
                         QR[rows, cc, c, :, :].rearrange("p a b -> p (a b)"), ["rk_KB", "rk_QR"], [RP[2 + h2]])
                    K.mm(rp[h2][H, cc * 64:(cc + 1) * 64], QR[rows, cc, c, 0, :], KB[rows, cc, c, 1, :], ["rk_QR", "rk_KB"], [RP[h2]])
                for h2 in range(2):
                    K.op("dve", "tensor_tensor", [RP[2 + h2], "maskq"], [nAT], out=ev(AT[:], h2),
                         in0=rp[2 + h2][:].rearrange("p (a b) -> p a b", a=4), in1=maskq[:].unsqueeze(1).to_broadcast([128, 4, 128]), op=ALU.mult)
                    K.op("dve", "tensor_tensor", [RP[h2], "lowm"], ["rk_Bm0"], out=ev(Bm[0][H, :, :], h2),
                         in0=rp[h2][H, 0:256].rearrange("p (a b) -> p a b", a=4), in1=lowm[H, :].unsqueeze(1).to_broadcast([64, 4, 64]), op=ALU.mult)
                K.op("act", "activation", [nAT], ["rk_Am0"], out=Am[0][H, :, :], in_=AT[H, :, 0:64], func=AF.Copy)
                K.op("dve", "tensor_tensor", ["identf", nAT], ["rk_Pm0"], out=Pm[0][H, :, :],
                     in0=identf[H, 64:128].unsqueeze(1).to_broadcast([64, 8, 64]), in1=AT[H, :, 0:64], op=ALU.subtract)
                yield
                for lvl in range(5):
                    ci, ni = lvl % 2, (lvl + 1) % 2
                    An, Bn, Pn = "rk_Am%d" % ni, "rk_Bm%d" % ni, "rk_Pm%d" % ni
                    Ac, Bc, Pc = "rk_Am%d" % ci, "rk_Bm%d" % ci, "rk_Pm%d" % ci
                    for h in range(8):
                        hs = slice(h * 64, (h + 1) * 64)
                        if lvl < 4:
                            K.mm(rp[2][H, hs], Bm[ci][H, h, :], Am[ci][H, h, :], [Ac, Bc], [RP[2]])
                        K.mm(rp[3][H, hs], Am[ci][H, h, :], Bm[ci][H, h, :], [Ac, Bc], [RP[3]])
                    if lvl < 4:
                        K.op("act", "activation", [RP[2]], [An], out=Am[ni][H, :, :], in_=rp[2][H, :].rearrange("p (a b) -> p a b", a=8), func=AF.Copy)
                    K.op("dve", "tensor_copy", [RP[3]], [Bn], out=Bm[ni][H, :, :], in_=rp[3][H, :].rearrange("p (a b) -> p a b", a=8))
                    yield
                    for h in range(8):
                        hs = slice(h * 64, (h + 1) * 64)
                        K.mm(rp[0][H, hs], Bm[ni][H, h, :], Pm[ci][H, h, :], [Bn, Pc], [RP[0]])
                    if lvl < 4:
                        K.op("dve", "tensor_tensor", [RP[0], Pc], [Pn], out=Pm[ni][H, :, :], in0=rp[0][H, :].rearrange("p (a b) -> p a b", a=8),
                             in1=Pm[ci][H, :, :], op=ALU.add)
                    else:
                        K.op("dve", "tensor_tensor", [RP[0], Pc], [nPmF], out=PmF[H, :, :], in0=rp[0][H, :].rearrange("p (a b) -> p a b", a=8),
                             in1=Pm[ci][H, :, :], op=ALU.add)
                    yield

            def post(c):
                q = c % 2
                KBT, VT, AT, PF = KBTs[q], VTs[q], ATs[q], PmFs[q]
                nKBT, nVT, nAT, PFn = "rk_KBT%d" % q, "rk_VT%d" % q, "rk_AT%d" % q, "rk_PmF%d" % q
                Rs3 = Rs[H, :].rearrange("p (a b) -> p a b", a=8)
                for h in range(8):
                    cc, h2 = h // 2, h % 2
                    rows = slice(h2 * 64, (h2 + 1) * 64)
                    hs = slice(h * 64, (h + 1) * 64)
                    K.mm(rp[6 + h2][H, cc * 64:(cc + 1) * 64], QR[rows, cc, c, 0, :], M[rows, cc, :], ["rk_QR", "rk_M"], [RP[6 + h2]])
                    K.mm(rp[4][H, hs], AT[0:64, h, 0:64], VT[0:64, cc, h2 * 64:(h2 + 1) * 64], [nAT, nVT], [RP[4]])
                for h2 in range(2):
                    K.op("act", "activation", [RP[6 + h2]], ["rk_Rs"], out=ev(Rs3, h2), in_=rp[6 + h2][H, 0:256].rearrange("p (a b) -> p a b", a=4), func=AF.Copy)
                K.op("dve", "tensor_tensor", [RP[4], "rk_Rs"], ["rk_Rs"], out=Rs[H, :], in0=rp[4][H, :], in1=Rs[H, :], op=ALU.add)
                yield
                for h in range(8):
                    hs = slice(h * 64, (h + 1) * 64)
                    K.mm(rp[5][H, hs], PF[H, h, :], Rs[H, hs], [PFn, "rk_Rs"], [RP[5]])
                K.op("act", "activation", [RP[5]], ["rk_Us"], out=Us[H, :], in_=rp[5][H, :], func=AF.Copy, scale=-1.0)
                yield
                for h in range(8):
                    cc, h2 = h // 2, h % 2
                    rows = slice(h2 * 64, (h2 + 1) * 64)
                    hs = slice(h * 64, (h + 1) * 64)
                    ys = slice(cc * 64, (cc + 1) * 64)
                    K.mm(rp[6 + h2][rows, ys], M[rows, cc, :], QR[rows, cc, c, 1, :], ["rk_M", "rk_QR"], [RP[6 + h2]])
                    K.mm(rp[4][rows, ys], VT[0:64, cc, h2 * 64:(h2 + 1) * 64], AT[0:64, h, 64:128], [nVT, nAT], [RP[4]])
                    K.mm(rp[5][rows, ys], Us[H, hs], AT[H, h, 64:128], ["rk_Us", nAT], [RP[5]])
                for h2 in range(2):
                    rows = slice(h2 * 64, (h2 + 1) * 64)
                    K.op("act", "activation", [RP[6 + h2]], ["rk_Yb"], out=Yb[rows, :, c * 64:(c + 1) * 64],
                         in_=rp[6 + h2][rows, 0:256].rearrange("p (a b) -> p a b", a=4), func=AF.Copy)
                yv = Yb[:, :, c * 64:(c + 1) * 64]
                K.op("dve", "tensor_tensor", [RP[4], "rk_Yb"], ["rk_Yb"], out=yv, in0=rp[4][:, 0:256].rearrange("p (a b) -> p a b", a=4), in1=yv, op=ALU.add)
                K.op("dve", "tensor_tensor", [RP[5], "rk_Yb"], ["rk_Yb"], out=yv, in0=rp[5][:, 0:256].rearrange("p (a b) -> p a b", a=4), in1=yv, op=ALU.add)
                yield
                for cc in range(4):
                    for h2 in range(2):
                        h = 2 * cc + h2
                        rows = slice(h2 * 64, (h2 + 1) * 64)
                        hs = slice(h * 64, (h + 1) * 64)
                        K.mm(rp[4][rows, cc * 64:(cc + 1) * 64], KBT[0:64, cc, rows], VT[0:64, cc, rows], [nKBT, nVT], [RP[4]])
                        K.mm(rp[5][rows, cc * 64:(cc + 1) * 64], KBT[H, cc, rows], Us[H, hs], [nKBT, "rk_Us"], [RP[5]])
                K.op("dve", "tensor_tensor", [RP[4], "rk_M"], ["rk_M"], out=M[:], in0=rp[4][:, 0:256].rearrange("p (a b) -> p a b", a=4), in1=M[:], op=ALU.add)
                K.op("dve", "tensor_tensor", [RP[5], "rk_M"], ["rk_M"], out=M[:], in0=rp[5][:, 0:256].rearrange("p (a b) -> p a b", a=4), in1=M[:], op=ALU.add)
                K.op("dve", "tensor_tensor", ["rk_M", "rk_gC"], ["rk_M"], out=M[:], in0=M[:],
                     in1=gC[:, :, c:c + 1].to_broadcast([128, 4, 64]), op=ALU.mult)
                yield

            for step in range(NCH + 1):
                gens = []
                if step >= 1:
                    gens.append(post(step - 1))
                if step < NCH:
                    gens.append(pre(step))
                while gens:
                    for g in list(gens):
                        try:
                            next(g)
                        except StopIteration:
                            gens.remove(g)
            chk(7)
            for cc in range(4):
                K.mm(rp[0][:, 0:TBK], bo64[:], Yb[:, cc, :], ["bo64", "rk_Yb"], [RP[0]])
                K.op("dve", "tensor_tensor", ["rk_Yb", RP[0]], ["rk_t1"], out=t1[:, cc, :], in0=Yb[:, cc, :], in1=rp[0][:, 0:TBK], op=ALU.subtract)
                K.op("pool", "tensor_tensor", ["rk_t1"], ["rk_kk"], out=kk[:, cc, :], in0=t1[:, cc, :], in1=t1[:, cc, :], op=ALU.mult)
                K.mm(rp[1][:, 0:TBK], bo64[:], kk[:, cc, :], ["bo64", "rk_kk"], [RP[1]])
                K.op("act", "activation", [RP[1], "rk_epsg"], ["rk_kp"], out=kp[:, cc, :], in_=rp[1][:, 0:TBK], func=AF.Sqrt, bias=epsg[:])
            K.op("dve", "reciprocal", ["rk_kp"], ["rk_kp"], out=kp[:], in_=kp[:])
            K.op("dve", "tensor_tensor", ["rk_t1", "rk_kp"], ["rk_t1"], out=t1[:], in0=t1[:], in1=kp[:], op=ALU.mult)
            K.op("pool", "tensor_tensor", ["rk_t1", "rk_lnw"], ["rk_t1"], out=t1[:], in0=t1[:], in1=bc(lnw), op=ALU.mult)
            K.op("pool", "tensor_tensor", ["rk_t1", "rk_lnb"], ["rk_t1"], out=t1[:], in0=t1[:], in1=bc(lnb), op=ALU.add)
            K.op("dve", "tensor_tensor", ["rk_t1", "rk_bon"], ["rk_t1"], out=t1[:], in0=t1[:], in1=bon[:], op=ALU.add)
            K.op("dve", "tensor_tensor", ["rk_t1", "rk_gg"], ["rk_yab"], out=yab[:], in0=t1[:], in1=gg[:], op=ALU.mult)
            K.dma("sp", SC["YA"][s, :, t0:t0 + TBK].rearrange("(c p) t -> p c t", p=128), yab[:], ["rk_yab"], ["YA"])


def norm_T(K, tag, src_tile, src_name, xn, ss, junk, pst, dstT, col0, identb, eps):
    K.op("act", "activation", [src_name], [tag + "junk", tag + "ss"], out=junk[:], in_=src_tile, func=AF.Square, accum_out=ss[:])
    K.op("act", "activation", [tag + "ss", "eps6"], [tag + "ss"], out=ss[:], in_=ss[:], func=AF.Sqrt, scale=1.0 / D, bias=eps[:])
    K.op("dve", "reciprocal", [tag + "ss"], [tag + "ss"], out=ss[:], in_=ss[:])
    K.op("dve", "tensor_scalar", [src_name, tag + "ss"], [tag + "xn"], out=xn[:], in0=src_tile, scalar1=ss[:], scalar2=None, op0=ALU.mult)
    for c in range(8):
        K.tr(pst[:, c, :], xn[:, c * 128:(c + 1) * 128], identb[:], [tag + "xn", "identb"], [tag + "pst"])
    K.op("act", "activation", [tag + "pst"], [dstT[1]], out=dstT[0][:, :, col0:col0 + 128], in_=pst[:], func=AF.Copy)


def phase_mix(K, s, T, X, Wd, SC, CONST):
    ZF = SC["ZF"]
    with ExitStack() as st:
        wpa = load_cast(K, st, "mx_wpa", Wd["w_proj_a"][0], 512, 1024)
        wpb = load_cast(K, st, "mx_wpb", Wd["w_proj_b"][0], 512, 1024)
        wout = load_cast(K, st, "mx_wout", Wd["w_out"][0], 1024, 1024)
        ps = [K.ps(st, "mx_ps%d" % i, [128, 512], F32) for i in range(4)]
        ya = K.sb(st, "mx_ya", [128, 4, 512], BF16)
        yb = K.sb(st, "mx_yb", [128, 4, 512], BF16)
        G = K.sb(st, "mx_G", [128, 16, 512], F32)
        ta = K.sb(st, "mx_ta", [128, 512], F32)
        tb_ = K.sb(st, "mx_tb", [128, 512], F32)
        mixT = K.sb(st, "mx_mixT", [128, 8, 512], BF16)
        xt = [K.sb(st, "mx_xt%d" % i, [128, D], F32) for i in range(2)]
        for tb in range(T // 512):
            t0 = tb * 512
            K.dma("sp", ya[:], SC["YA"][s, :, t0:t0 + 512].rearrange("(c p) t -> p c t", p=128), ["YA"], ["mx_ya"])
            K.dma("act", yb[:], SC["YB"][s, :, t0:t0 + 512].rearrange("(c p) t -> p c t", p=128), ["YB"], ["mx_yb"])
            K.dma("sp", G[:], ZF[s, R_G:R_G + 2048, t0:t0 + 512].rearrange("(c p) t -> p c t", p=128), ["ZF"], ["mx_G"])
            for cc in range(8):
                cs = slice(cc * 128, (cc + 1) * 128)
                for k in range(4):
                    K.mm(ps[0][:], wpa[:, k, cs], ya[:, k, :], ["mx_wpa", "mx_ya"], ["mx_ps0"], start=(k == 0), stop=(k == 3))
                for k in range(4):
                    K.mm(ps[1][:], wpb[:, k, cs], yb[:, k, :], ["mx_wpb", "mx_yb"], ["mx_ps1"], start=(k == 0), stop=(k == 3))
                K.op("dve", "tensor_tensor", ["mx_ps0", "mx_G"], ["mx_ta"], out=ta[:], in0=ps[0][:], in1=G[:, cc, :], op=ALU.mult)
                K.op("dve", "tensor_tensor", ["mx_ps1", "mx_G"], ["mx_tb"], out=tb_[:], in0=ps[1][:], in1=G[:, 8 + cc, :], op=ALU.mult)
                K.op("pool", "tensor_tensor", ["mx_ta", "mx_tb"], ["mx_mixT"], out=mixT[:, cc, :], in0=ta[:], in1=tb_[:], op=ALU.add)
            for tt in range(4):
                i = tt % 2
                r0 = s * T + t0 + tt * 128
                K.dma("act", xt[i][:], X[r0:r0 + 128, :], [], ["mx_xt%d" % i])
                for half in range(2):
                    pj = 2 + half
                    for k in range(8):
                        K.mm(ps[pj][:], mixT[:, k, tt * 128:(tt + 1) * 128], wout[:, k, half * 512:(half + 1) * 512], ["mx_mixT", "mx_wout"],
                             ["mx_ps%d" % pj], start=(k == 0), stop=(k == 7))
                    K.op("dve", "tensor_tensor", ["mx_ps%d" % pj, "mx_xt%d" % i], ["mx_xt%d" % i], out=xt[i][:, half * 512:(half + 1) * 512],
                         in0=ps[pj][:], in1=xt[i][:, half * 512:(half + 1) * 512], op=ALU.add)
                K.dma("sp", SC["H1"][r0:r0 + 128, :], xt[i][:], ["mx_xt%d" % i], ["H1"])


def colvec(K, st, tag, ap, n=8):
    t = K.sb(st, tag, [128, n], F32)
    K.dma("sp", t[:], ap.rearrange("o (c p) -> p (o c)", p=128), [], [tag], allow_slow_non_contiguous=True)
    return t


def phase_cross(K, s, T, MEM, Wd, SC, CONST):
    identb = CONST["identb"]
    with ExitStack() as st:
        nrc = colvec(K, st, "cx_nrc", Wd["norm_cross"])
        nrm = colvec(K, st, "cx_nrm", Wd["norm_mem"])
        wcq = load_cast(K, st, "cx_wcq", Wd["w_cq"][0], 1024, 1024, scale_col=(nrc, "cx_nrc"))
        wckv = load_cast(K, st, "cx_wckv", Wd["w_ckv"][0], 1024, 2048, scale_col=(nrm, "cx_nrm"))
        wco = load_cast(K, st, "cx_wco", Wd["w_co"][0], 1024, 1024)
        ps = [K.ps(st, "cx_ps%d" % i, [128, 512], F32) for i in range(6)]
        pst = K.ps(st, "cx_pst", [128, 8, 128], BF16)
        ht = K.sb(st, "cx_ht", [128, 4, D], F32)
        xn = K.sb(st, "cx_xn", [128, D], BF16)
        ss = K.sb(st, "cx_ss", [128, 1], F32)
        junk = K.sb(st, "cx_junk", [128, D], F32)
        memT = K.sb(st, "cx_memT", [128, 8, 256], BF16)
        ones = K.sb(st, "cx_ones", [128, 128], BF16)
        K.op("pool", "memset", [], ["cx_ones"], ap=ones[:], constant=1.0)
        for mt in range(2):
            K.dma("sp", ht[:, 0, :], MEM[s * 256 + mt * 128: s * 256 + (mt + 1) * 128, :], [], ["cx_ht0"])
            norm_T(K, "cx_", ht[:, 0, :], "cx_ht0", xn, ss, junk, pst, (memT, "cx_memT"), mt * 128, identb, CONST["eps6"])
        kTs = K.sb(st, "cx_kTs", [128, 8, 256], BF16)
        vS = K.sb(st, "cx_vS", [128, 2, 1024], BF16)
        for j in range(8):
            for k in range(8):
                K.mm(ps[0][:, 0:256], wckv[:, k, j * 128:(j + 1) * 128], memT[:, k, :], ["cx_wckv", "cx_memT"], ["cx_ps0"], start=(k == 0), stop=(k == 7))
            K.op("dve", "tensor_copy", ["cx_ps0"], ["cx_kTs"], out=kTs[:, j, :], in_=ps[0][:, 0:256])
        for mt in range(2):
            for half in range(2):
                for k in range(8):
                    K.mm(ps[1][:], memT[:, k, mt * 128:(mt + 1) * 128], wckv[:, k, 1024 + half * 512:1024 + (half + 1) * 512], ["cx_wckv", "cx_memT"],
                         ["cx_ps1"], start=(k == 0), stop=(k == 7))
                K.op("dve", "tensor_copy", ["cx_ps1"], ["cx_vS"], out=vS[:, mt, half * 512:(half + 1) * 512], in_=ps[1][:])
        hnT = K.sb(st, "cx_hnT", [128, 8, 512], BF16)
        qTs = K.sb(st, "cx_qTs", [128, 8, 512], BF16)
        pT = [K.sb(st, "cx_pT%d" % i, [128, 512], BF16) for i in range(2)]
        rden = K.sb(st, "cx_rden", [128, 512], F32)
        oT = K.sb(st, "cx_oT", [128, 8, 512], BF16)
        for tb in range(T // 512):
            t0 = tb * 512
            for tt in range(4):
                r0 = s * T + t0 + tt * 128
                K.dma("sp" if tt % 2 == 0 else "act", ht[:, tt, :], SC["H1"][r0:r0 + 128, :], ["H1"], ["cx_ht%d" % tt])
                norm_T(K, "cx_", ht[:, tt, :], "cx_ht%d" % tt, xn, ss, junk, pst, (hnT, "cx_hnT"), tt * 128, identb, CONST["eps6"])
            for j in range(8):
                pj = j % 2
                for k in range(8):
                    K.mm(ps[pj][:], wcq[:, k, j * 128:(j + 1) * 128], hnT[:, k, :], ["cx_wcq", "cx_hnT"], ["cx_ps%d" % pj], start=(k == 0), stop=(k == 7))
                if pj == 0:
                    K.op("dve", "tensor_copy", ["cx_ps0"], ["cx_qTs"], out=qTs[:, j, :], in_=ps[0][:])
                else:
                    K.op("act", "activation", ["cx_ps1"], ["cx_qTs"], out=qTs[:, j, :], in_=ps[1][:], func=AF.Copy)
            for h in range(4):
                for mt in range(2):
                    for dc in range(2):
                        K.mm(ps[2 + mt][:], kTs[:, 2 * h + dc, mt * 128:(mt + 1) * 128], qTs[:, 2 * h + dc, :], ["cx_kTs", "cx_qTs"],
                             ["cx_ps%d" % (2 + mt)], start=(dc == 0), stop=(dc == 1))
                    K.op("act", "activation", ["cx_ps%d" % (2 + mt)], ["cx_pT%d" % mt], out=pT[mt][:], in_=ps[2 + mt][:], func=AF.Exp, scale=1.0 / 16)
                for mt in range(2):
                    K.mm(ps[4][:], ones[:], pT[mt][:], ["cx_ones", "cx_pT%d" % mt], ["cx_ps4"], start=(mt == 0), stop=(mt == 1))
                K.op("dve", "reciprocal", ["cx_ps4"], ["cx_rden"], out=rden[:], in_=ps[4][:])
                for dc in range(2):
                    for mt in range(2):
                        K.mm(ps[5][:], vS[:, mt, h * 256 + dc * 128:h * 256 + (dc + 1) * 128], pT[mt][:], ["cx_vS", "cx_pT%d" % mt], ["cx_ps5"],
                             start=(mt == 0), stop=(mt == 1))
                    K.op("dve", "tensor_tensor", ["cx_ps5", "cx_rden"], ["cx_oT"], out=oT[:, 2 * h + dc, :], in0=ps[5][:], in1=rden[:], op=ALU.mult)
            for tt in range(4):
                r0 = s * T + t0 + tt * 128
                for half in range(2):
                    pj = half
                    for k in range(8):
                        K.mm(ps[pj][:], oT[:, k, tt * 128:(tt + 1) * 128], wco[:, k, half * 512:(half + 1) * 512], ["cx_oT", "cx_wco"],
                             ["cx_ps%d" % pj], start=(k == 0), stop=(k == 7))
                    K.op("dve", "tensor_tensor", ["cx_ps%d" % pj, "cx_ht%d" % tt], ["cx_ht%d" % tt], out=ht[:, tt, half * 512:(half + 1) * 512],
                         in0=ps[pj][:], in1=ht[:, tt, half * 512:(half + 1) * 512], op=ALU.add)
                K.dma("sp", SC["H1"][r0:r0 + 128, :], ht[:, tt, :], ["cx_ht%d" % tt], ["H1"])


def phase_moe(K, s, T, Wd, SC, CONST, OUT):
    identb = CONST["identb"]
    HT = min(T, 1024)
    NTL = HT // 128
    with ExitStack() as st:
        nrf = colvec(K, st, "mo_nrf", Wd["norm_ffn"])
        wrf = K.sb(st, "mo_wrf", [128, 8, 36], F32)
        K.dma("sp", wrf[:, :, 0:4], Wd["w_router_g"][0].rearrange("(c p) n -> p c n", p=128), [], ["mo_wrf"])
        K.dma("sp", wrf[:, :, 4:36], Wd["w_router_e"][0].rearrange("(c p) n -> p c n", p=128), [], ["mo_wrf"])
        wr = K.sb(st, "mo_wr", [128, 8, 36], BF16)
        K.op("dve", "tensor_tensor", ["mo_wrf", "mo_nrf"], ["mo_wr"], out=wr[:], in0=wrf[:], in1=nrf[:].unsqueeze(2).to_broadcast([128, 8, 36]), op=ALU.mult)
        brb = K.sb(st, "mo_brb", [128, 36], F32)
        K.dma("sp", brb[:, 0:4], Wd["b_router_g"].partition_broadcast(128), [], ["mo_brb"])
        K.dma("sp", brb[:, 4:36], Wd["b_router_e"].partition_broadcast(128), [], ["mo_brb"])
        nfb = K.sb(st, "mo_nfb", [128, D], F32)
        K.dma("sp", nfb[:], Wd["norm_final"].partition_broadcast(128), [], ["mo_nfb"])
        ps = [K.ps(st, "mo_ps%d" % i, [128, 512], F32) for i in range(7)]
        pst = K.ps(st, "mo_pst", [128, 8, 128], BF16)
        ht = K.sb(st, "mo_ht", [128, D], F32)
        xn = K.sb(st, "mo_xn", [128, D], BF16)
        ss = K.sb(st, "mo_ss", [128, 1], F32)
        junk = K.sb(st, "mo_junk", [128, D], F32)
        xT = K.sb(st, "mo_xT", [128, 8, HT], BF16)
        G = K.sb(st, "mo_G", [128, NTL, 32], F32)
        acc = K.sb(st, "mo_acc", [128, NTL, D], F32)
        lg = K.sb(st, "mo_lg", [128, 36], F32)
        cl = K.sb(st, "mo_cl", [128, 12], F32)
        lem = K.sb(st, "mo_lem", [128, 4, 8], F32)
        m8 = K.sb(st, "mo_m8", [128, 8], F32)
        sel = K.sb(st, "mo_sel", [128, 32], F32)
        ex = K.sb(st, "mo_ex", [128, 32], F32)
        stg = [K.sb(st, "mo_stg%d" % i, [128, 4096], F32) for i in range(2)]
        wg = [K.sb(st, "mo_wg%d" % i, [128, 8, 512], BF16) for i in range(2)]
        wu = [K.sb(st, "mo_wu%d" % i, [128, 8, 512], BF16) for i in range(2)]
        wd = [K.sb(st, "mo_wd%d" % i, [128, 4, 1024], BF16) for i in range(2)]
        sgts = [K.sb(st, "mo_sgt%d" % i, [128, 512], F32) for i in range(2)]
        hT = K.sb(st, "mo_hT", [128, 4, 512], BF16)
        tmp = [K.sb(st, "mo_tmp%d" % i, [128, 512], F32) for i in range(3)]
        for hf in range(T // HT):
            base = s * T + hf * HT
            for tl in range(NTL):
                r0 = base + tl * 128
                K.dma("sp", ht[:], SC["H1"][r0:r0 + 128, :], ["H1"], ["mo_ht"])
                norm_T(K, "mo_", ht[:], "mo_ht", xn, ss, junk, pst, (xT, "mo_xT"), tl * 128, identb, CONST["eps6"])
                for k in range(8):
                    K.mm(ps[0][:, 0:36], xT[:, k, tl * 128:(tl + 1) * 128], wr[:, k, :], ["mo_xT", "mo_wr"], ["mo_ps0"], start=(k == 0), stop=(k == 7))
                K.op("dve", "tensor_tensor", ["mo_ps0", "mo_brb"], ["mo_lg"], out=lg[:], in0=ps[0][:, 0:36], in1=brb[:], op=ALU.add)
                K.op("dve", "tensor_reduce", ["mo_lg"], ["mo_cl"], out=cl[:, 0:1], in_=lg[:, 0:4], axis=AX.X, op=ALU.max)
                K.op("dve", "tensor_scalar", ["mo_cl"], ["mo_cl"], out=cl[:, 1:2], in0=cl[:, 0:1], scalar1=-1.0, scalar2=None, op0=ALU.mult)
                K.op("act", "activation", ["mo_lg", "mo_cl"], ["mo_ex", "mo_cl"], out=ex[:, 0:4], in_=lg[:, 0:4], func=AF.Exp, bias=cl[:, 1:2], accum_out=cl[:, 2:3])
                K.op("dve", "reciprocal", ["mo_cl"], ["mo_cl"], out=cl[:, 3:4], in_=cl[:, 2:3])
                K.op("dve", "tensor_scalar", ["mo_lg", "mo_cl"], ["mo_sel"], out=sel[:, 0:4], in0=lg[:, 0:4], scalar1=cl[:, 0:1], scalar2=None, op0=ALU.is_ge)
                K.op("dve", "tensor_scalar", ["mo_sel"], ["mo_sel"], out=sel[:, 0:4], in0=sel[:, 0:4], scalar1=-1.0, scalar2=1e30, op0=ALU.add, op1=ALU.mult)
                K.op("dve", "tensor_tensor", ["mo_lg", "mo_sel"], ["mo_lem"], out=lem[:], in0=lg[:, 4:36].rearrange("p (a b) -> p a b", a=4),
                     in1=sel[:, 0:4].unsqueeze(2).to_broadcast([128, 4, 8]), op=ALU.add)
                lemf = lem[:].rearrange("p a b -> p (a b)")
                K.op("dve", "max", ["mo_lem"], ["mo_m8"], out=m8[:], in_=lemf)
                K.op("dve", "tensor_scalar", ["mo_lem", "mo_m8"], ["mo_sel"], out=sel[:], in0=lemf, scalar1=m8[:, 1:2], scalar2=None, op0=ALU.is_ge)
                K.op("dve", "tensor_scalar", ["mo_m8"], ["mo_cl"], out=cl[:, 4:5], in0=m8[:, 0:1], scalar1=-1.0, scalar2=None, op0=ALU.mult)
                K.op("act", "activation", ["mo_lem", "mo_cl"], ["mo_ex"], out=ex[:], in_=lemf, func=AF.Exp, bias=cl[:, 4:5])
                K.op("dve", "tensor_tensor", ["mo_ex", "mo_sel"], ["mo_ex"], out=ex[:], in0=ex[:], in1=sel[:], op=ALU.mult)
                K.op("dve", "tensor_reduce", ["mo_ex"], ["mo_cl"], out=cl[:, 5:6], in_=ex[:], axis=AX.X, op=ALU.add)
                K.op("dve", "reciprocal", ["mo_cl"], ["mo_cl"], out=cl[:, 6:7], in_=cl[:, 5:6])
                K.op("dve", "tensor_tensor", ["mo_cl"], ["mo_cl"], out=cl[:, 7:8], in0=cl[:, 6:7], in1=cl[:, 3:4], op=ALU.mult)
                K.op("dve", "tensor_scalar", ["mo_ex", "mo_cl"], ["mo_G"], out=G[:, tl, :], in0=ex[:], scalar1=cl[:, 7:8], scalar2=None, op0=ALU.mult)
            for e in range(32):
                i = e % 2
                nfb8 = nrf[:].unsqueeze(2).to_broadcast([128, 8, 512])
                K.dma("sp", stg[0][:].rearrange("p (c n) -> p c n", c=8), Wd["w_e_gate"][0, e].rearrange("(c p) n -> p c n", p=128), [], ["mo_stg0"])
                K.op("pool", "tensor_tensor", ["mo_stg0", "mo_nrf"], ["mo_wg%d" % i], out=wg[i][:], in0=stg[0][:].rearrange("p (c n) -> p c n", c=8), in1=nfb8, op=ALU.mult)
                K.dma("act", stg[1][:].rearrange("p (c n) -> p c n", c=8), Wd["w_e_up"][0, e].rearrange("(c p) n -> p c n", p=128), [], ["mo_stg1"])
                K.op("pool", "tensor_tensor", ["mo_stg1", "mo_nrf"], ["mo_wu%d" % i], out=wu[i][:], in0=stg[1][:].rearrange("p (c n) -> p c n", c=8), in1=nfb8, op=ALU.mult)
                K.dma("sp", stg[0][:].rearrange("p (c n) -> p c n", c=4), Wd["w_e_down"][0, e].rearrange("(c p) n -> p c n", p=128), [], ["mo_stg0"])
                K.op("pool", "tensor_copy", ["mo_stg0"], ["mo_wd%d" % i], out=wd[i][:], in_=stg[0][:].rearrange("p (c n) -> p c n", c=4))
                for bk in range(HT // 512):
                    bs = slice(bk * 512, (bk + 1) * 512)
                    for fc in range(4):
                        fs = slice(fc * 128, (fc + 1) * 128)
                        pg, pu = (0, 1) if fc % 2 == 0 else (4, 5)
                        sg_ = sgts[fc % 2]
                        sgn = "mo_sgt%d" % (fc % 2)
                        for k in range(8):
                            K.mm(ps[pg][:], wg[i][:, k, fs], xT[:, k, bs], ["mo_wg%d" % i, "mo_xT"], ["mo_ps%d" % pg], start=(k == 0), stop=(k == 7))
                        for k in range(8):
                            K.mm(ps[pu][:], wu[i][:, k, fs], xT[:, k, bs], ["mo_wu%d" % i, "mo_xT"], ["mo_ps%d" % pu], start=(k == 0), stop=(k == 7))
                        K.op("act", "activation", ["mo_ps%d" % pg], [sgn], out=sg_[:], in_=ps[pg][:], func=AF.Silu)
                        K.op("dve", "tensor_tensor", ["mo_ps%d" % pu, sgn], ["mo_hT%d" % fc], out=hT[:, fc, :], in0=ps[pu][:], in1=sg_[:], op=ALU.mult)
                    for tt in range(4):
                        tl = bk * 4 + tt
                        for half in range(2):
                            pj = (2, 3, 6)[(2 * tt + half) % 3]
                            for fc in range(4):
                                K.mm(ps[pj][:], hT[:, fc, tt * 128:(tt + 1) * 128], wd[i][:, fc, half * 512:(half + 1) * 512], ["mo_hT%d" % fc, "mo_wd%d" % i],
                                     ["mo_ps%d" % pj], start=(fc == 0), stop=(fc == 3))
                            hs = slice(half * 512, (half + 1) * 512)
                            if e == 0:
                                K.op("act", "activation", ["mo_ps%d" % pj, "mo_G"], ["mo_acc%d_%d" % (tl, half)], out=acc[:, tl, hs], in_=ps[pj][:], func=AF.Copy, scale=G[:, tl, e:e + 1])
                            else:
                                ti = (2 * tt + half) % 3
                                accn = "mo_acc%d_%d" % (tl, half)
                                K.op("act", "activation", ["mo_ps%d" % pj, "mo_G"], ["mo_tmp%d" % ti], out=tmp[ti][:], in_=ps[pj][:], func=AF.Copy, scale=G[:, tl, e:e + 1])
                                K.op("pool" if half == 0 else "dve", "tensor_tensor", ["mo_tmp%d" % ti, accn], [accn], out=acc[:, tl, hs], in0=acc[:, tl, hs], in1=tmp[ti][:], op=ALU.add)
            for tl in range(NTL):
                r0 = base + tl * 128
                K.dma("sp", ht[:], SC["H1"][r0:r0 + 128, :], ["H1"], ["mo_ht"])
                K.op("dve", "tensor_tensor", ["mo_ht", "mo_acc%d_0" % tl, "mo_acc%d_1" % tl], ["mo_ht"], out=ht[:], in0=ht[:], in1=acc[:, tl, :], op=ALU.add)
                K.op("act", "activation", ["mo_ht"], ["mo_junk", "mo_ss"], out=junk[:], in_=ht[:], func=AF.Square, accum_out=ss[:])
                K.op("act", "activation", ["mo_ss", "eps6"], ["mo_ss"], out=ss[:], in_=ss[:], func=AF.Sqrt, scale=1.0 / D, bias=CONST["eps6"][:])
                K.op("dve", "reciprocal", ["mo_ss"], ["mo_ss"], out=ss[:], in_=ss[:])
                K.op("dve", "scalar_tensor_tensor", ["mo_ht", "mo_ss", "mo_nfb"], ["mo_junk"], out=junk[:], in0=ht[:], scalar=ss[:], in1=nfb[:], op0=ALU.mult, op1=ALU.mult)
                K.dma("sp", OUT[r0:r0 + 128, :], junk[:], ["mo_junk"], ["OUT"])


I32 = mybir.dt.int32


def prepack_gen(K, st, Wd, SC):
    WGU, WDS = SC["WGU"], SC["WDS"]
    nrf = colvec(K, st, "pk_nrf", Wd["norm_ffn"])
    sg = K.sb(st, "pk_sg", [128, 8, 512], F32)
    su = K.sb(st, "pk_su", [128, 8, 512], F32)
    sd = K.sb(st, "pk_sd", [128, 4, 1024], F32)
    og = K.sb(st, "pk_og", [128, 8, 1024], BF16)
    od = K.sb(st, "pk_od", [128, 4, 1024], BF16)
    nf8 = nrf[:].unsqueeze(2).to_broadcast([128, 8, 512])
    def loads(e):
        K.dma("pool", sg[:], Wd["w_e_gate"][0, e].rearrange("(c p) n -> p c n", p=128), [], ["pk_sg"])
        K.dma("pool", su[:], Wd["w_e_up"][0, e].rearrange("(c p) n -> p c n", p=128), [], ["pk_su"])
        K.dma("pool", sd[:], Wd["w_e_down"][0, e].rearrange("(c p) n -> p c n", p=128), [], ["pk_sd"])

    loads(0)
    yield
    for e in range(32):
        K.op("dve", "tensor_tensor", ["pk_sg", "pk_nrf"], ["pk_og0"], out=og[:, :, 0:512], in0=sg[:], in1=nf8, op=ALU.mult)
        for c in range(8):
            K.op("act", "activation", ["pk_su", "pk_nrf"], ["pk_og1"], out=og[:, c, 512:1024], in_=su[:, c, :], func=AF.Copy, scale=nrf[:, c:c + 1])
        K.op("act", "activation", ["pk_sd"], ["pk_od"], out=od[:], in_=sd[:], func=AF.Copy)
        K.dma("pool", WGU[e * 1024:(e + 1) * 1024, :].rearrange("(c p) n -> p c n", p=128), og[:], ["pk_og0", "pk_og1"], ["WGU"])
        K.dma("pool", WDS[e * 512:(e + 1) * 512, :].rearrange("(c p) n -> p c n", p=128), od[:], ["pk_od"], ["WDS"])
        if e + 1 < 32:
            loads(e + 1)
        yield


def phase_moe_sparse(K, s, T, Wd, SC, CONST, OUT):
    nc = K.nc
    S = K.S
    identb = CONST["identb"]
    NTL = T // 128
    SB = 256
    NBLK = (2 * T) // SB + 32
    XS, YS = SC["XS"], SC["YS"]
    WG = Wd["w_e_gate"].rearrange("o e d f -> (o e d) f")
    WU = Wd["w_e_up"].rearrange("o e d f -> (o e d) f")
    WDN = Wd["w_e_down"].rearrange("o e f d -> (o e f) d")
    base = s * T
    with ExitStack() as st0:
        nrf = colvec(K, st0, "ms_nrf", Wd["norm_ffn"])
        GG = K.sb(st0, "ms_GG", [128, NTL, 2], F32)
        DST = K.sb(st0, "ms_DST", [128, NTL, 2], I32)
        IDXG = K.sb(st0, "ms_IDXG", [128, NBLK, 8], I32)
        IDXD = K.sb(st0, "ms_IDXD", [128, NBLK, 4], I32)
        with ExitStack() as st:
            wrf = K.sb(st, "ms_wrf", [128, 8, 36], F32)
            K.dma("sp", wrf[:, :, 0:4], Wd["w_router_g"][0].rearrange("(c p) n -> p c n", p=128), [], ["ms_wrf"])
            K.dma("sp", wrf[:, :, 4:36], Wd["w_router_e"][0].rearrange("(c p) n -> p c n", p=128), [], ["ms_wrf"])
            wr = K.sb(st, "ms_wr", [128, 8, 36], BF16)
            K.op("dve", "tensor_tensor", ["ms_wrf", "ms_nrf"], ["ms_wr"], out=wr[:], in0=wrf[:], in1=nrf[:].unsqueeze(2).to_broadcast([128, 8, 36]), op=ALU.mult)
            brb = K.sb(st, "ms_brb", [128, 36], F32)
            K.dma("sp", brb[:, 0:4], Wd["b_router_g"].partition_broadcast(128), [], ["ms_brb"])
            K.dma("sp", brb[:, 4:36], Wd["b_router_e"].partition_broadcast(128), [], ["ms_brb"])
            ps = [K.ps(st, "ms_ps%d" % i, [128, 512], F32) for i in range(2)]
            pst = K.ps(st, "ms_pst", [128, 8, 128], BF16)
            ht = K.sb(st, "ms_ht", [128, D], F32)
            ss = K.sb(st, "ms_ss", [128, 1], F32)
            junk = K.sb(st, "ms_junk", [128, D], F32)
            XN = K.sb(st, "ms_XN", [128, NTL, D], BF16)
            xT = K.sb(st, "ms_xT", [128, 8, 128], BF16)
            SEL = K.sb(st, "ms_SEL", [128, NTL, 2, 32], F32)
            RNK = K.sb(st, "ms_RNK", [128, NTL, 2], F32)
            carry = K.sb(st, "ms_carry", [128, 32], F32)
            K.op("dve", "memset", [], ["ms_carry"], ap=carry[:], constant=0.0)
            lg = K.sb(st, "ms_lg", [128, 36], F32)
            cl = K.sb(st, "ms_cl", [128, 12], F32)
            lem = K.sb(st, "ms_lem", [128, 32], F32)
            m8 = K.sb(st, "ms_m8", [128, 8], F32)
            s12 = K.sb(st, "ms_s12", [128, 32], F32)
            ex = K.sb(st, "ms_ex", [128, 32], F32)
            t32 = K.sb(st, "ms_t32", [128, 32], F32)
            utri, ones128, bstart, iotap = CONST["utri"], CONST["ones128"], CONST["bstart"], CONST["iotap"]
            GB = 8
            LG = K.sb(st, "ms_LG", [128, GB, 36], F32)
            LM = K.sb(st, "ms_LM", [128, GB, 32], F32)
            L2 = K.sb(st, "ms_L2", [128, GB, 32], F32)
            EX = K.sb(st, "ms_EX", [128, GB, 32], F32)
            S12 = K.sb(st, "ms_S12", [128, GB, 32], F32)
            RKt = K.sb(st, "ms_RKt", [128, GB, 32], F32)
            T4 = K.sb(st, "ms_T4", [128, GB, 4], F32)
            E4 = K.sb(st, "ms_E4", [128, GB, 4], F32)
            CG = K.sb(st, "ms_CG", [128, 8, GB], F32)
            hts = [ht, K.sb(st, "ms_ht1", [128, D], F32)]

            def b3(colv, n):
                return colv.unsqueeze(2).to_broadcast([128, GB, n])

            for g0 in range(0, NTL, GB):
                for gi in range(GB):
                    tl = g0 + gi
                    r0 = base + tl * 128
                    hh_ = hts[tl % 2]
                    hn = "ms_ht" if tl % 2 == 0 else "ms_ht1"
                    K.dma("sp" if tl % 2 == 0 else "act", hh_[:], SC["H1"][r0:r0 + 128, :], ["H1"], [hn])
                    K.op("act", "activation", [hn], ["ms_junk", "ms_ss"], out=junk[:], in_=hh_[:], func=AF.Square, accum_out=ss[:])
                    K.op("act", "activation", ["ms_ss", "eps6"], ["ms_ss"], out=ss[:], in_=ss[:], func=AF.Sqrt, scale=1.0 / D, bias=CONST["eps6"][:])
                    K.op("dve", "reciprocal", ["ms_ss"], ["ms_ss"], out=ss[:], in_=ss[:])
                    K.op("dve", "tensor_scalar", [hn, "ms_ss"], ["ms_XN%d" % tl], out=XN[:, tl, :], in0=hh_[:], scalar1=ss[:], scalar2=None, op0=ALU.mult)
                    for c in range(8):
                        K.tr(pst[:, c, :], XN[:, tl, c * 128:(c + 1) * 128], identb[:], ["ms_XN%d" % tl, "identb"], ["ms_pst"])
                    K.op("act", "activation", ["ms_pst"], ["ms_xT"], out=xT[:], in_=pst[:], func=AF.Copy)
                    for k in range(8):
                        K.mm(ps[0][:, 0:36], xT[:, k, :], wr[:, k, :], ["ms_xT", "ms_wr"], ["ms_ps0"], start=(k == 0), stop=(k == 7))
                    K.op("dve", "tensor_tensor", ["ms_ps0", "ms_brb"], ["ms_LG"], out=LG[:, gi, :], in0=ps[0][:, 0:36], in1=brb[:], op=ALU.add)
                K.op("dve", "tensor_reduce", ["ms_LG"], ["ms_CG"], out=CG[:, 0, :], in_=LG[:, :, 0:4], axis=AX.X, op=ALU.max)
                K.op("dve", "tensor_tensor", ["ms_LG", "ms_CG"], ["ms_T4"], out=T4[:], in0=LG[:, :, 0:4], in1=b3(CG[:, 0, :], 4), op=ALU.subtract)
                K.op("act", "activation", ["ms_T4"], ["ms_E4"], out=E4[:], in_=T4[:], func=AF.Exp)
                K.op("dve", "tensor_reduce", ["ms_E4"], ["ms_CG"], out=CG[:, 1, :], in_=E4[:], axis=AX.X, op=ALU.add)
                K.op("dve", "reciprocal", ["ms_CG"], ["ms_CG"], out=CG[:, 2, :], in_=CG[:, 1, :])
                K.op("dve", "tensor_scalar", ["ms_T4"], ["ms_T4"], out=T4[:], in0=T4[:], scalar1=0.0, scalar2=None, op0=ALU.is_ge)
                K.op("dve", "tensor_scalar", ["ms_T4"], ["ms_T4"], out=T4[:], in0=T4[:], scalar1=-1.0, scalar2=1e30, op0=ALU.add, op1=ALU.mult)
                K.op("dve", "tensor_tensor", ["ms_LG", "ms_T4"], ["ms_LM"], out=LM[:].rearrange("p g (a b) -> p g a b", a=4),
                     in0=LG[:, :, 4:36].rearrange("p g (a b) -> p g a b", a=4), in1=T4[:].unsqueeze(3).to_broadcast([128, GB, 4, 8]), op=ALU.add)
                K.op("dve", "tensor_reduce", ["ms_LM"], ["ms_CG"], out=CG[:, 3, :], in_=LM[:], axis=AX.X, op=ALU.max)
                sel1 = SEL[:, g0:g0 + GB, 0, :]
                sel2 = SEL[:, g0:g0 + GB, 1, :]
                K.op("dve", "tensor_tensor", ["ms_LM", "ms_CG"], ["ms_SEL"], out=sel1, in0=LM[:], in1=b3(CG[:, 3, :], 32), op=ALU.is_ge)
                K.op("dve", "scalar_tensor_tensor", ["ms_SEL", "ms_LM"], ["ms_L2"], out=L2[:], in0=sel1, scalar=-1e30, in1=LM[:], op0=ALU.mult, op1=ALU.add)
                K.op("dve", "tensor_reduce", ["ms_L2"], ["ms_CG"], out=CG[:, 4, :], in_=L2[:], axis=AX.X, op=ALU.max)
                K.op("dve", "tensor_tensor", ["ms_LM", "ms_CG"], ["ms_S12"], out=S12[:], in0=LM[:], in1=b3(CG[:, 4, :], 32), op=ALU.is_ge)
                K.op("dve", "tensor_tensor", ["ms_S12", "ms_SEL"], ["ms_SEL"], out=sel2, in0=S12[:], in1=sel1, op=ALU.subtract)
                K.op("dve", "tensor_tensor", ["ms_LM", "ms_CG"], ["ms_L2"], out=L2[:], in0=LM[:], in1=b3(CG[:, 3, :], 32), op=ALU.subtract)
                K.op("dve", "tensor_scalar", ["ms_L2"], ["ms_L2"], out=L2[:], in0=L2[:], scalar1=-80.0, scalar2=None, op0=ALU.max)
                K.op("act", "activation", ["ms_L2"], ["ms_EX"], out=EX[:], in_=L2[:], func=AF.Exp)
                K.op("dve", "tensor_tensor", ["ms_EX", "ms_S12"], ["ms_EX"], out=EX[:], in0=EX[:], in1=S12[:], op=ALU.mult)
                K.op("dve", "tensor_reduce", ["ms_EX"], ["ms_CG"], out=CG[:, 5, :], in_=EX[:], axis=AX.X, op=ALU.add)
                K.op("dve", "reciprocal", ["ms_CG"], ["ms_CG"], out=CG[:, 6, :], in_=CG[:, 5, :])
                K.op("dve", "tensor_tensor", ["ms_CG"], ["ms_CG"], out=CG[:, 6, :], in0=CG[:, 6, :], in1=CG[:, 2, :], op=ALU.mult)
                for kk_ in range(2):
                    K.op("dve", "tensor_tensor", ["ms_EX", "ms_SEL"], ["ms_L2"], out=L2[:], in0=EX[:], in1=SEL[:, g0:g0 + GB, kk_, :], op=ALU.mult)
                    K.op("dve", "tensor_reduce", ["ms_L2"], ["ms_CG"], out=CG[:, 7, :], in_=L2[:], axis=AX.X, op=ALU.add)
                    K.op("dve", "tensor_tensor", ["ms_CG"], ["ms_GG"], out=GG[:, g0:g0 + GB, kk_], in0=CG[:, 7, :], in1=CG[:, 6, :], op=ALU.mult)
                for gi in range(GB):
                    K.mm(ps[1][:, gi * 64:gi * 64 + 32], utri[:], S12[:, gi, :], ["utri", "ms_S12"], ["ms_ps1"])
                    K.mm(ps[1][:, gi * 64 + 32:gi * 64 + 64], ones128[:], S12[:, gi, :], ["ones128", "ms_S12"], ["ms_ps1"])
                for gi in range(GB):
                    K.op("dve", "tensor_tensor", ["ms_ps1", "ms_carry"], ["ms_RKt"], out=RKt[:, gi, :], in0=ps[1][:, gi * 64:gi * 64 + 32], in1=carry[:], op=ALU.add)
                    K.op("dve", "tensor_tensor", ["ms_ps1", "ms_carry"], ["ms_carry"], out=carry[:], in0=ps[1][:, gi * 64 + 32:gi * 64 + 64], in1=carry[:], op=ALU.add)
                for kk_ in range(2):
                    K.op("dve", "tensor_tensor", ["ms_RKt", "ms_SEL"], ["ms_L2"], out=L2[:], in0=RKt[:], in1=SEL[:, g0:g0 + GB, kk_, :], op=ALU.mult)
                    K.op("dve", "tensor_reduce", ["ms_L2"], ["ms_RNK"], out=RNK[:, g0:g0 + GB, kk_], in_=L2[:], axis=AX.X, op=ALU.add)
            ci = K.sb(st, "ms_ci", [128, 32], I32)
            pad = K.sb(st, "ms_pad", [128, 32], F32)
            pend = K.sb(st, "ms_pend", [128, 32], F32)
            pstart = K.sb(st, "ms_pstart", [128, 32], F32)
            ones32 = K.sb(st, "ms_ones32", [128, 32], F32)
            K.op("dve", "memset", [], ["ms_ones32"], ap=ones32[:], constant=1.0)
            K.op("dve", "tensor_scalar", ["ms_carry"], ["ms_ci"], out=ci[:], in0=carry[:], scalar1=float(SB - 1), scalar2=None, op0=ALU.add)
            K.op("dve", "tensor_scalar", ["ms_ci"], ["ms_ci"], out=ci[:], in0=ci[:], scalar1=8, scalar2=None, op0=ALU.arith_shift_right)
            K.op("dve", "tensor_scalar", ["ms_ci"], ["ms_ci"], out=ci[:], in0=ci[:], scalar1=8, scalar2=None, op0=ALU.logical_shift_left)
            K.op("dve", "tensor_copy", ["ms_ci"], ["ms_pad"], out=pad[:], in_=ci[:])
            K.op("dve", "tensor_tensor_scan", ["ms_pad", "ms_ones32"], ["ms_pend"], out=pend[:], data0=ones32[:], data1=pad[:], initial=0.0, op0=ALU.mult, op1=ALU.add)
            K.op("dve", "tensor_tensor", ["ms_pend", "ms_pad"], ["ms_pstart"], out=pstart[:], in0=pend[:], in1=pad[:], op=ALU.subtract)
            bst = K.sb(st, "ms_bst", [128, NBLK], F32)
            K.op("dve", "tensor_scalar", ["bstart"], ["ms_bst"], out=bst[:], in0=bstart[:, 0:NBLK], scalar1=float(SB // 128), scalar2=None, op0=ALU.mult)
            be = K.sb(st, "ms_be", [128, NBLK], F32)
            K.op("dve", "tensor_scalar", ["ms_bst", "ms_pend"], ["ms_be"], out=be[:], in0=bst[:], scalar1=pend[:, 0:1], scalar2=None, op0=ALU.is_ge)
            for e in range(1, 32):
                K.op("dve", "scalar_tensor_tensor", ["ms_bst", "ms_pend", "ms_be"], ["ms_be"], out=be[:], in0=bst[:], scalar=pend[:, e:e + 1], in1=be[:],
                     op0=ALU.is_ge, op1=ALU.add)
            K.op("dve", "tensor_scalar", ["ms_be"], ["ms_be"], out=be[:], in0=be[:], scalar1=31.0, scalar2=None, op0=ALU.min)
            bg = K.sb(st, "ms_bg", [128, NBLK], F32)
            bd = K.sb(st, "ms_bd", [128, NBLK], F32)
            K.op("dve", "tensor_scalar", ["ms_be", "iotap"], ["ms_bg"], out=bg[:], in0=be[:], scalar1=1024.0, scalar2=iotap[:, 0:1], op0=ALU.mult, op1=ALU.add)
            K.op("dve", "tensor_scalar", ["ms_be", "iotap"], ["ms_bd"], out=bd[:], in0=be[:], scalar1=512.0, scalar2=iotap[:, 0:1], op0=ALU.mult, op1=ALU.add)
            for c in range(8):
                K.op("dve", "tensor_scalar", ["ms_bg"], ["ms_IDXG"], out=IDXG[:, :, c], in0=bg[:], scalar1=float(c * 128), scalar2=None, op0=ALU.add)
            for c in range(4):
                K.op("dve", "tensor_scalar", ["ms_bd"], ["ms_IDXD"], out=IDXD[:, :, c], in0=bd[:], scalar1=float(c * 128), scalar2=None, op0=ALU.add)
            zt = K.sb(st, "ms_zt", [128, 4, D], BF16)
            K.op("pool", "memset", [], ["ms_zt"], ap=zt[:], constant=0.0)
            XSv = XS.rearrange("(b p) d -> p b d", p=128)
            for b0 in range(0, NBLK * SB // 128, 4):
                K.dma("sp" if (b0 // 4) % 2 == 0 else "act", XSv[:, b0:b0 + 4, :], zt[:], ["ms_zt"], ["XS"])
            for tl in range(NTL):
                for kk_ in range(2):
                    K.op("dve", "tensor_tensor", ["ms_pstart", "ms_SEL"], ["ms_t32"], out=t32[:], in0=pstart[:], in1=SEL[:, tl, kk_, :], op=ALU.mult)
                    K.op("dve", "tensor_reduce", ["ms_t32"], ["ms_cl"], out=cl[:, 9:10], in_=t32[:], axis=AX.X, op=ALU.add)
                    K.op("dve", "tensor_tensor", ["ms_cl", "ms_RNK"], ["ms_DST"], out=DST[:, tl, kk_:kk_ + 1], in0=cl[:, 9:10], in1=RNK[:, tl, kk_:kk_ + 1], op=ALU.add)
                for kk_ in range(2):
                    S.dma("pool", None, None, K._bl(["ms_DST", "ms_XN%d" % tl, "XS"]), K._bl(["XSs_%d_%d" % (tl, kk_)]),
                          fn=lambda e, tl=tl, kk_=kk_: e.indirect_dma_start(out=XS, out_offset=bass.IndirectOffsetOnAxis(ap=DST[:, tl, kk_:kk_ + 1], axis=0),
                                                                        in_=XN[:, tl, :], in_offset=None))
        S.barrier()
        with ExitStack() as st:
            ps = [K.ps(st, "mb_ps%d" % i, [128, 512], F32) for i in range(6)]
            pst = K.ps(st, "mb_pst", [128, 8, 128], BF16)
            wgu = [K.sb(st, "mb_wgu%d" % i, [128, 8, 1024], BF16) for i in range(2)]
            wd = [K.sb(st, "mb_wd%d" % i, [128, 4, 1024], BF16) for i in range(2)]
            xb = [K.sb(st, "mb_xb%d" % i, [128, D], BF16) for i in range(2)]
            xT = K.sb(st, "mb_xT", [128, 8, 128], BF16)
            sgt = K.sb(st, "mb_sgt", [128, 512], F32)
            hb = K.sb(st, "mb_hb", [128, 512], BF16)
            hT = K.sb(st, "mb_hT", [128, 4, 128], BF16)
            ysb = [K.sb(st, "mb_ysb%d" % i, [128, D], F32) for i in range(2)]
            WGU, WDS = SC["WGU"], SC["WDS"]
            for b in range(NBLK):
                i = b % 2
                for c in range(8):
                    S.dma("pool", None, None, K._bl(["ms_IDXG"]), K._bl(["mb_wgu%d_%d" % (i, c)]),
                          fn=lambda e, b=b, c=c, i=i: e.indirect_dma_start(out=wgu[i][:, c, :], out_offset=None, in_=WGU,
                                                                         in_offset=bass.IndirectOffsetOnAxis(ap=IDXG[:, b, c:c + 1], axis=0)))
                for c in range(4):
                    S.dma("pool", None, None, K._bl(["ms_IDXD"]), K._bl(["mb_wd%d_%d" % (i, c)]),
                          fn=lambda e, b=b, c=c, i=i: e.indirect_dma_start(out=wd[i][:, c, :], out_offset=None, in_=WDS,
                                                                         in_offset=bass.IndirectOffsetOnAxis(ap=IDXD[:, b, c:c + 1], axis=0)))
                for sub in range(SB // 128):
                    j = sub % 2
                    r0 = b * SB + sub * 128
                    K.dma("sp", xb[j][:], XS[r0:r0 + 128, :], ["XS"], ["mb_xb%d" % j])
                    for c in range(8):
                        K.tr(pst[:, c, :], xb[j][:, c * 128:(c + 1) * 128], identb[:], ["mb_xb%d" % j, "identb"], ["mb_pst"])
                    K.op("act", "activation", ["mb_pst"], ["mb_xT"], out=xT[:], in_=pst[:], func=AF.Copy)
                    for k in range(8):
                        K.mm(ps[0][:], xT[:, k, :], wgu[i][:, k, 0:512], ["mb_xT"] + ["mb_wgu%d_%d" % (i, c) for c in range(8)], ["mb_ps0"], start=(k == 0), stop=(k == 7))
                    for k in range(8):
                        K.mm(ps[1][:], xT[:, k, :], wgu[i][:, k, 512:1024], ["mb_xT"] + ["mb_wgu%d_%d" % (i, c) for c in range(8)], ["mb_ps1"], start=(k == 0), stop=(k == 7))
                    K.op("act", "activation", ["mb_ps0"], ["mb_sgt"], out=sgt[:], in_=ps[0][:], func=AF.Silu)
                    K.op("dve", "tensor_tensor", ["mb_ps1", "mb_sgt"], ["mb_hb"], out=hb[:], in0=ps[1][:], in1=sgt[:], op=ALU.mult)
                    for fc in range(4):
                        K.tr(pst[:, fc, :], hb[:, fc * 128:(fc + 1) * 128], identb[:], ["mb_hb", "identb"], ["mb_pst"])
                    K.op("dve", "tensor_copy", ["mb_pst"], ["mb_hT"], out=hT[:], in_=pst[:, 0:4, :])
                    for half in range(2):
                        pj = 2 + 2 * j + half
                        for fc in range(4):
                            K.mm(ps[pj][:], hT[:, fc, :], wd[i][:, fc, half * 512:(half + 1) * 512], ["mb_hT"] + ["mb_wd%d_%d" % (i, c) for c in range(4)], ["mb_ps%d" % pj], start=(fc == 0), stop=(fc == 3))
                        if half == 0:
                            K.op("act", "activation", ["mb_ps%d" % pj], ["mb_ysb%d" % j], out=ysb[j][:, 0:512], in_=ps[pj][:], func=AF.Copy)
                        else:
                            K.op("dve", "tensor_copy", ["mb_ps%d" % pj], ["mb_ysb%d" % j], out=ysb[j][:, 512:1024], in_=ps[pj][:])
                    K.dma("act", YS[r0:r0 + 128, :], ysb[j][:], ["mb_ysb%d" % j], ["YS"])
        S.barrier()
        with ExitStack() as st:
            nfb = K.sb(st, "mc_nfb", [128, D], F32)
            K.dma("sp", nfb[:], Wd["norm_final"].partition_broadcast(128), [], ["mc_nfb"])
            hts = [K.sb(st, "mc_ht%d" % i, [128, D], F32) for i in range(2)]
            y1 = [K.sb(st, "mc_y1%d" % i, [128, D], F32) for i in range(2)]
            y2 = [K.sb(st, "mc_y2%d" % i, [128, D], F32) for i in range(2)]
            ob = [K.sb(st, "mc_ob%d" % i, [128, D], F32) for i in range(2)]
            junk = K.sb(st, "mc_junk", [128, D], F32)
            sss = [K.sb(st, "mc_ss%d" % i, [128, 1], F32) for i in range(2)]
            for tl in range(NTL):
                i = tl % 2
                r0 = base + tl * 128
                K.dma("sp", hts[i][:], SC["H1"][r0:r0 + 128, :], ["H1"], ["mc_ht%d" % i])
                S.dma("pool", None, None, K._bl(["ms_DST", "YS"]), K._bl(["mc_y1%d" % i]),
                      fn=lambda e, tl=tl, i=i: e.indirect_dma_start(out=y1[i][:], out_offset=None, in_=YS, in_offset=bass.IndirectOffsetOnAxis(ap=DST[:, tl, 0:1], axis=0)))
                S.dma("pool", None, None, K._bl(["ms_DST", "YS"]), K._bl(["mc_y2%d" % i]),
                      fn=lambda e, tl=tl, i=i: e.indirect_dma_start(out=y2[i][:], out_offset=None, in_=YS, in_offset=bass.IndirectOffsetOnAxis(ap=DST[:, tl, 1:2], axis=0)))
                K.op("dve", "scalar_tensor_tensor", ["mc_y1%d" % i, "ms_GG", "mc_ht%d" % i], ["mc_ht%d" % i], out=hts[i][:], in0=y1[i][:], scalar=GG[:, tl, 0:1], in1=hts[i][:],
                     op0=ALU.mult, op1=ALU.add)
                K.op("dve", "scalar_tensor_tensor", ["mc_y2%d" % i, "ms_GG", "mc_ht%d" % i], ["mc_ht%d" % i], out=hts[i][:], in0=y2[i][:], scalar=GG[:, tl, 1:2], in1=hts[i][:],
                     op0=ALU.mult, op1=ALU.add)
                K.op("act", "activation", ["mc_ht%d" % i], ["mc_junk", "mc_ss%d" % i], out=junk[:], in_=hts[i][:], func=AF.Square, accum_out=sss[i][:])
                K.op("act", "activation", ["mc_ss%d" % i, "eps6"], ["mc_ss%d" % i], out=sss[i][:], in_=sss[i][:], func=AF.Sqrt, scale=1.0 / D, bias=CONST["eps6"][:])
                K.op("dve", "reciprocal", ["mc_ss%d" % i], ["mc_ss%d" % i], out=sss[i][:], in_=sss[i][:])
                K.op("dve", "scalar_tensor_tensor", ["mc_ht%d" % i, "mc_ss%d" % i, "mc_nfb"], ["mc_ob%d" % i], out=ob[i][:], in0=hts[i][:], scalar=sss[i][:], in1=nfb[:],
                     op0=ALU.mult, op1=ALU.mult)
                K.dma("act", OUT[r0:r0 + 128, :], ob[i][:], ["mc_ob%d" % i], ["OUT"])


def build(T, NSEQ, stop_after=99, debug=False):
    nc = bass.Bass("TRN2", target_bir_lowering=False)
    NTOK = NSEQ * T

    def din(name, shape, dt=F32):
        return nc.dram_tensor(name, list(shape), dt, kind="ExternalInput").ap()

    X = din("x", [NTOK, D])
    MEM = din("mem", [NSEQ * 256, D])
    Wd = {}
    for name, shape in WSHAPES.items():
        Wd[name] = din(name, shape)
    identb_d = din("c_identb", [128, 128], BF16)
    identf_d = din("c_identf", [128, 128], F32)
    OUT = nc.dram_tensor("out", [NTOK, D], F32, kind="ExternalOutput").ap()
    SC = {}
    SC["ZF"] = nc.dram_tensor("sc_zf", [NSEQ, R_TOT, T], F32, kind="Internal").ap() if not debug else \
        nc.dram_tensor("sc_zf", [NSEQ, R_TOT, T], F32, kind="ExternalOutput").ap()
    kindd = "ExternalOutput" if debug else "Internal"
    SC["CK"] = nc.dram_tensor("sc_ck", [NSEQ, T, 128], BF16, kind=kindd).ap()
    SC["CKT"] = nc.dram_tensor("sc_ckt", [NSEQ, 128, T], BF16, kind=kindd).ap()
    SC["H1"] = nc.dram_tensor("sc_h1", [NTOK, D], F32, kind=kindd).ap()
    NSLOT = ((2 * T) // 256 + 32) * 256
    SC["WGU"] = nc.dram_tensor("sc_wgu", [32 * 1024, 1024], BF16, kind="Internal").ap()
    SC["WDS"] = nc.dram_tensor("sc_wds", [32 * 512, 1024], BF16, kind="Internal").ap()
    SC["XS"] = nc.dram_tensor("sc_xs", [NSLOT, D], BF16, kind="Internal").ap()
    SC["YS"] = nc.dram_tensor("sc_ys", [NSLOT, D], F32, kind="Internal").ap()
    SC["YB"] = nc.dram_tensor("sc_yb", [NSEQ, 512, T], BF16, kind=kindd).ap()
    SC["YA"] = nc.dram_tensor("sc_ya", [NSEQ, 512, T], BF16, kind=kindd).ap()
    cdram = {}
    for nm, arr in consts().items():
        if nm not in ("c_identb", "c_identf"):
            cdram[nm] = din(nm, arr.shape, BF16 if arr.dtype == ml_dtypes.bfloat16 else F32)
    with ExitStack() as st:
        S = Sched(nc, st)
        K = Ctx(nc, S)
        CONST = {}
        CONST["identb"] = K.sb(st, "identb", [128, 128], BF16)
        CONST["identf"] = K.sb(st, "identf", [128, 128], F32)
        CONST["eps6"] = K.sb(st, "eps6", [128, 1], F32)
        K.dma("sp", CONST["identb"][:], identb_d, [], ["identb"])
        K.dma("sp", CONST["identf"][:], identf_d, [], ["identf"])
        K.op("dve", "memset", [], ["eps6"], ap=CONST["eps6"][:], constant=1e-6)
        for nm, ap in cdram.items():
            sh = list(ap.shape)
            CONST[nm[2:]] = K.sb(st, nm[2:], sh, ap.dtype)
            K.dma("sp", CONST[nm[2:]][:], ap, [], [nm[2:]])
        for s in range(NSEQ):
            phase1(K, s, T, X, Wd, SC, CONST)
            S.barrier()
            if stop_after >= 2 and not os.environ.get("SKIP_DSA"):
                ex_ = (lambda st_: prepack_gen(K, st_, Wd, SC)) if (s == 0 and stop_after >= 6 and not os.environ.get("MOE_DENSE")) else None
                phase_dsa(K, s, T, Wd, SC, CONST, extra=ex_)
                S.barrier()
            if stop_after >= 3:
                phase_rwkv(K, s, T, Wd, SC, CONST)
                S.barrier()
            if stop_after >= 4:
                phase_mix(K, s, T, X, Wd, SC, CONST)
                S.barrier()
            if stop_after >= 5:
                phase_cross(K, s, T, MEM, Wd, SC, CONST)
                S.barrier()
            if stop_after >= 6:
                if os.environ.get("MOE_DENSE"):
                    phase_moe(K, s, T, Wd, SC, CONST, OUT)
                else:
                    phase_moe_sparse(K, s, T, Wd, SC, CONST, OUT)
                S.barrier()
        S.finish(list(K.B.values()))
        print("ops", S.nops, "waits", S.nwaits)
        S.emit()
    return nc


WSHAPES = {
    "norm_mix": [1, 1024], "w_in": [1, 1024, 4804], "shift_mu": [1, 1792], "rw_w0": [1, 512],
    "rw_w2": [1, 64, 512], "rw_a0": [1, 512], "rw_a2": [1, 64, 512], "rw_g2": [1, 128, 512],
    "rw_k_k": [1, 512], "rw_k_a": [1, 512], "rw_r_k": [1, 8, 64], "rw_ln_w": [1, 512], "rw_ln_b": [1, 512],
    "kv_norm": [1, 128], "w_uk": [1, 128, 8, 64], "w_uv": [1, 128, 8, 64], "w_proj_a": [1, 512, 1024],
    "w_proj_b": [1, 512, 1024], "b_gate": [1, 2048], "w_out": [1, 1024, 1024], "norm_cross": [1, 1024],
    "norm_mem": [1, 1024], "w_cq": [1, 1024, 1024], "w_ckv": [1, 1024, 2048], "w_co": [1, 1024, 1024],
    "norm_ffn": [1, 1024], "w_router_g": [1, 1024, 4], "b_router_g": [1, 4], "w_router_e": [1, 1024, 32],
    "b_router_e": [1, 32], "w_e_gate": [1, 32, 1024, 512], "w_e_up": [1, 32, 1024, 512],
    "w_e_down": [1, 32, 512, 1024], "norm_final": [1024],
}


def consts():
    return {
        "c_identb": np.eye(128, dtype=np.float32).astype(ml_dtypes.bfloat16),
        "c_identf": np.eye(128, dtype=np.float32),
        "c_tri01": (np.arange(128)[None, :] <= np.arange(128)[:, None]).astype(np.float32).astype(ml_dtypes.bfloat16),
        "c_negtri": np.where(np.arange(128)[None, :] <= np.arange(128)[:, None], 0.0, -1e30).astype(np.float32),
        "c_bo": np.kron(np.eye(2), np.ones((64, 64))).astype(np.float32),
        "c_bo64": (np.kron(np.eye(2), np.ones((64, 64))) / 64.0).astype(np.float32),
        "c_maskq": np.block([[np.triu(np.ones((64, 64)), 1), np.triu(np.ones((64, 64)), 0)],
                             [np.triu(np.ones((64, 64)), 1), np.triu(np.ones((64, 64)), 0)]]).astype(np.float32),
        "c_lowm": np.concatenate([np.zeros((64, 64)), np.tril(np.ones((64, 64)), -1)], 0).astype(np.float32),
        "c_resetm": np.tile((np.arange(256) % 64 != 0).astype(np.float32)[None, :], (128, 1)),
        "c_utri": (np.arange(128)[:, None] < np.arange(128)[None, :]).astype(np.float32),
        "c_ones128": np.ones((128, 128), np.float32),
        "c_bstart": np.tile((np.arange(320) * 128.0)[None, :], (128, 1)).astype(np.float32),
        "c_iotap": np.arange(128, dtype=np.float32)[:, None].copy(),
        "c_pw": np.tile((0.5 ** (np.arange(NIT) + 1))[None, :], (128, 1)).astype(np.float32),
    }


def kernel(**inputs):
    x = np.asarray(inputs["x"], dtype=np.float32)
    mem = np.asarray(inputs["mem"], dtype=np.float32)
    B, T, _ = x.shape
    nseq = B // NCORES
    nc = build(T, nseq)
    cs = consts()
    in_maps = []
    for c in range(NCORES):
        m = {"x": np.ascontiguousarray(x[c * nseq:(c + 1) * nseq].reshape(nseq * T, D)),
             "mem": np.ascontiguousarray(mem[c * nseq:(c + 1) * nseq].reshape(nseq * 256, D))}
        for name in WSHAPES:
            m[name] = np.ascontiguousarray(np.asarray(inputs[name], dtype=np.float32))
        m.update(cs)
        in_maps.append(m)
    res = run_bass_kernel_spmd(nc, in_maps, core_ids=list(range(NCORES)))
    out = np.concatenate([r["out"].reshape(nseq, T, D) for r in res.results], axis=0)
    return out.astype(np.float32)
```

```python
from contextlib import ExitStack
import os
import numpy as np
import ml_dtypes
import concourse.bass as bass
import concourse.mybir as mybir
from concourse.bass_utils import run_bass_kernel_spmd

F32 = mybir.dt.float32
BF16 = mybir.dt.bfloat16
AF = mybir.ActivationFunctionType
ALU = mybir.AluOpType
AX = mybir.AxisListType

D = 1024
NCORES = 8


class Buf:
    __slots__ = ("name", "w", "r")

    def __init__(self, name=""):
        self.name = name
        self.w = None
        self.r = {}


class Sched:
    ENG = ("pe", "act", "dve", "pool", "sp")

    def __init__(self, nc, stack, n_dma_sems=10):
        self.nc = nc
        self.streams = {e: [] for e in self.ENG}
        self.sems = {}
        self.count = {}
        for e in self.ENG:
            self.sems[e] = stack.enter_context(nc.semaphore("s_" + e))
            self.count[e] = 0
        self.dma_sems = {}
        self.dma_rr = {}
        for q in ("sp", "act", "pool"):
            lst = []
            for i in range(n_dma_sems if q != "pool" else 28):
                k = "d_%s_%d" % (q, i)
                self.sems[k] = stack.enter_context(nc.semaphore(k))
                self.count[k] = 0
                lst.append(k)
            self.dma_sems[q] = lst
            self.dma_rr[q] = 0
        self.waited = {}
        self.nwaits = 0
        self.nops = 0

    def _wait(self, eng, key, val):
        if val <= 0 or self.waited.get((eng, key), 0) >= val:
            return
        self.waited[(eng, key)] = val
        self.streams[eng].append(("w", key, val))
        self.nwaits += 1

    def _deps(self, eng, reads, writes, own_key):
        for b in reads:
            if b.w is not None:
                self._dep(eng, b.w, own_key)
        for b in writes:
            if b.w is not None:
                self._dep(eng, b.w, own_key)
            for k, v in b.r.items():
                self._dep(eng, (k, v), own_key)

    def _dep(self, eng, ev, own_key):
        k, v = ev
        if k == "pe" and own_key == "pe":
            return
        self._wait(eng, k, v)

    muted = False

    def op(self, eng, fn, reads=(), writes=()):
        if self.muted:
            return
        self._deps(eng, reads, writes, eng)
        self.count[eng] += 1
        v = self.count[eng]
        self.streams[eng].append(("o", fn, eng, 1))
        for b in writes:
            b.w = (eng, v)
            b.r = {}
        for b in reads:
            if b.r.get(eng, 0) < v:
                b.r[eng] = v
        self.nops += 1

    def dma(self, q, out, in_, reads=(), writes=(), fn=None, **kw):
        if self.muted:
            return
        lst = self.dma_sems[q]
        key = lst[self.dma_rr[q] % len(lst)]
        self.dma_rr[q] += 1
        self._wait(q, key, self.count[key])
        self._deps(q, reads, writes, key)
        self.count[key] += 16
        v = self.count[key]
        if fn is None:
            fn = lambda e, out=out, in_=in_, kw=kw: e.dma_start(out=out, in_=in_, **kw)
        self.streams[q].append(("o", fn, key, 16))
        for b in writes:
            b.w = (key, v)
            b.r = {}
        for b in reads:
            if b.r.get(key, 0) < v:
                b.r[key] = v
        self.nops += 1

    def barrier(self):
        for e in self.ENG:
            for k in self.sems:
                if k != e or True:
                    self._wait(e, k, self.count[k])

    def finish(self, bufs, eng="sp"):
        for b in bufs:
            if b.w is not None:
                self._wait(eng, b.w[0], b.w[1])

    def emit(self):
        nc = self.nc
        sems = self.sems
        streams = self.streams
        with nc.Block() as block:
            def run(engobj, lst):
                for it in lst:
                    if it[0] == "w":
                        engobj.wait_ge(sems[it[1]], it[2])
                    else:
                        it[1](engobj).then_inc(sems[it[2]], it[3])

            @block.tensor
            def _(e):
                run(e, streams["pe"])

            @block.scalar
            def _(e):
                run(e, streams["act"])

            @block.vector
            def _(e):
                run(e, streams["dve"])

            @block.gpsimd
            def _(e):
                run(e, streams["pool"])

            @block.sync
            def _(e):
                run(e, streams["sp"])


class Ctx:
    def __init__(self, nc, S):
        self.nc = nc
        self.S = S
        self.B = {}
        self.rr = 0
        self.uid = 0

    def buf(self, name):
        if name not in self.B:
            self.B[name] = Buf(name)
        return self.B[name]

    def _bl(self, lst):
        return [self.buf(x) if isinstance(x, str) else x for x in lst]

    def sb(self, st, name, shape, dt):
        self.uid += 1
        t = st.enter_context(self.nc.sbuf_tensor("%s_u%d" % (name, self.uid), list(shape), dt))
        self.buf(name)
        return t

    def ps(self, st, name, shape, dt):
        self.uid += 1
        t = st.enter_context(self.nc.psum_tensor("%s_u%d" % (name, self.uid), list(shape), dt))
        self.buf(name)
        return t

    def op(self, eng, method, reads, writes, **kw):
        self.S.op(eng, lambda e, m=method, kw=kw: getattr(e, m)(**kw), self._bl(reads), self._bl(writes))

    def mm(self, out, lhsT, rhs, reads, writes, start=True, stop=True, **kw):
        self.S.op("pe", lambda e: e.matmul(out, lhsT, rhs, start=start, stop=stop, **kw),
                  self._bl(reads), self._bl(writes))

    def tr(self, out, in_, ident, reads, writes):
        self.S.op("pe", lambda e: e.transpose(out, in_, ident), self._bl(reads), self._bl(writes))

    def dma(self, q, out, in_, reads, writes, **kw):
        self.S.dma(q, out, in_, self._bl(reads), self._bl(writes), **kw)

    def q(self):
        self.rr += 1
        return ("sp", "act", "pool")[self.rr % 3]


C_RW = 0
C_Q = 1792
C_CKV = 2304
C_QI = 2432
C_KI = 2688
C_WI = 2752
C_G = 2756
R_RW = 0
R_Q = 1792
R_QI = 2304
R_KI = 2560
R_G = 2624
R_WI = 4672
R_TOT = 4676


def load_cast(K, st, tag, w_ap, kin, n, scale_col=None, dt=BF16, engs=("dve", "pool")):
    nc = K.nc
    kc = kin // 128
    wt = K.sb(st, tag, [128, kc, n], dt)
    src = w_ap.rearrange("(c p) n -> p c n", p=128)
    if True:
        stg = [K.sb(st, "%s_stg%d" % (tag, i), [128, n], F32) for i in range(2)]
        for c in range(kc):
            sg = stg[c % 2]
            nm = "%s_stg%d" % (tag, c % 2)
            K.dma(K.q(), sg[:], src[:, c, :], [], [nm])
            eng = engs[c % len(engs)]
            if scale_col is None:
                K.op(eng, "tensor_copy", [nm], [tag], out=wt[:, c, :], in_=sg[:])
            else:
                K.op(eng, "tensor_scalar", [nm, scale_col[1]], [tag], out=wt[:, c, :], in0=sg[:],
                     scalar1=scale_col[0][:, c:c + 1], scalar2=None, op0=ALU.mult)
    return wt


def norm_rows(K, tag, xt, xt_name, ss, junk, eps_scale=1.0 / D):
    K.op("act", "activation", [xt_name], [tag + "_junk", tag + "_ss"], out=junk[:], in_=xt[:], func=AF.Square,
         accum_out=ss[:])
    K.op("act", "activation", [tag + "_ss"], [tag + "_ss"], out=ss[:], in_=ss[:], func=AF.Sqrt,
         scale=eps_scale, bias=1e-6)
    K.op("dve", "reciprocal", [tag + "_ss"], [tag + "_ss"], out=ss[:], in_=ss[:])


def phase1(K, s, T, X, Wd, SC, CONST):
    nc = K.nc
    NT = T // 128
    NB = T // 512
    with ExitStack() as st:
        xnT = K.sb(st, "p1_xnT", [128, 8, T], BF16)
        gm = K.sb(st, "p1_gm", [128, 8], F32)
        K.dma("sp", gm[:], Wd["norm_mix"].rearrange("o (c p) -> p (o c)", p=128), [], ["p1_gm"], allow_slow_non_contiguous=True)
        bg = K.sb(st, "p1_bg", [128, 16], F32)
        K.dma("sp", bg[:], Wd["b_gate"].rearrange("o (c p) -> p (o c)", p=128), [], ["p1_bg"], allow_slow_non_contiguous=True)
        identb = CONST["identb"]
        pst = K.ps(st, "p1_pst", [128, 8, 128], BF16)
        xts = [K.sb(st, "p1_xt%d" % i, [128, D], F32) for i in range(2)]
        xnb = [K.sb(st, "p1_xn%d" % i, [128, D], BF16) for i in range(2)]
        junk = K.sb(st, "p1_junk", [128, D], F32)
        sss = [K.sb(st, "p1_ss%d" % i, [128, 1], F32) for i in range(2)]
        for tt in range(NT):
            i = tt % 2
            xt, xn, ss = xts[i], xnb[i], sss[i]
            K.dma("sp" if i == 0 else "act", xt[:], X[s * T + tt * 128: s * T + (tt + 1) * 128, :], [], ["p1_xt%d" % i])
            K.op("act", "activation", ["p1_xt%d" % i], ["p1_junk", "p1_ss%d" % i], out=junk[:], in_=xt[:],
                 func=AF.Square, accum_out=ss[:])
            K.op("act", "activation", ["p1_ss%d" % i, "eps6"], ["p1_ss%d" % i], out=ss[:], in_=ss[:], func=AF.Sqrt,
                 scale=1.0 / D, bias=CONST["eps6"][:])
            K.op("dve", "reciprocal", ["p1_ss%d" % i], ["p1_ss%d" % i], out=ss[:], in_=ss[:])
            K.op("dve", "tensor_scalar", ["p1_xt%d" % i, "p1_ss%d" % i], ["p1_xn%d" % i], out=xn[:], in0=xt[:],
                 scalar1=ss[:], scalar2=None, op0=ALU.mult)
            for c in range(8):
                K.tr(pst[:, c, :], xn[:, c * 128:(c + 1) * 128], identb[:], ["p1_xn%d" % i, "identb"], ["p1_pst"])
            K.op("pool" if False else "act", "activation", ["p1_pst"], ["p1_xnT"], out=xnT[:, :, tt * 128:(tt + 1) * 128],
                 in_=pst[:], func=AF.Copy)
        import os
        STOP = int(os.environ.get("STOP", "99"))
        if STOP <= 1:
            return
        chunks = []
        for i in range(14):
            chunks.append((C_RW + i * 128, 128, R_RW + i * 128, "fm", None))
        for i in range(4):
            chunks.append((C_Q + i * 128, 128, R_Q + i * 128, "fm", None))
        for i in range(2):
            chunks.append((C_QI + i * 128, 128, R_QI + i * 128, "fm", None))
        chunks.append((C_KI, 64, R_KI, "fm", None))
        chunks.append((C_WI, 4, R_WI, "fm", None))
        for i in range(16):
            chunks.append((C_G + i * 128, 128, R_G + i * 128, "gate", i))
        wsrc = Wd["w_in"].rearrange("o (c p) n -> p (o c) n", p=128)
        wst = [K.sb(st, "p1_wst%d" % i, [128, 8, 132], F32) for i in range(2)]
        wbf = [K.sb(st, "p1_wbf%d" % i, [128, 8, 132], BF16) for i in range(2)]
        stage = [K.sb(st, "p1_stage%d" % i, [128, T], F32) for i in range(2)]
        pss = [K.ps(st, "p1_ps%d" % i, [128, 512], F32) for i in range(4)]
        gmb = gm[:].unsqueeze(2).to_broadcast([128, 8, 128])
        ZF = SC["ZF"]
        for ci, (c0, ncol, r0, kind, gi) in enumerate(chunks):
            i = ci % 2
            K.dma("sp" if i == 0 else "pool", wst[i][:, :, 0:ncol], wsrc[:, :, c0:c0 + ncol], [], ["p1_wst%d" % i])
            K.op("dve", "tensor_tensor", ["p1_wst%d" % i, "p1_gm"], ["p1_wbf%d" % i], out=wbf[i][:, :, 0:ncol],
                 in0=wst[i][:, :, 0:ncol], in1=gm[:].unsqueeze(2).to_broadcast([128, 8, ncol]), op=ALU.mult)
            for tb in range(NB):
                pj = (ci * NB + tb) % 4
                ps = pss[pj]
                for dc in range(8):
                    K.mm(ps[0:ncol, :], wbf[i][:, dc, 0:ncol], xnT[:, dc, tb * 512:(tb + 1) * 512],
                         ["p1_wbf%d" % i, "p1_xnT"], ["p1_ps%d" % pj], start=(dc == 0), stop=(dc == 7))
                if kind == "gate":
                    K.op("act", "activation", ["p1_ps%d" % pj, "p1_bg"], ["p1_stage%d" % i],
                         out=stage[i][0:ncol, tb * 512:(tb + 1) * 512], in_=ps[0:ncol, :], func=AF.Sigmoid,
                         bias=bg[:, gi:gi + 1])
                else:
                    eng = "dve" if tb % 2 == 0 else "act"
                    if eng == "dve":
                        K.op("dve", "tensor_copy", ["p1_ps%d" % pj], ["p1_stage%d" % i],
                             out=stage[i][0:ncol, tb * 512:(tb + 1) * 512], in_=ps[0:ncol, :])
                    else:
                        K.op("act", "activation", ["p1_ps%d" % pj], ["p1_stage%d" % i],
                             out=stage[i][0:ncol, tb * 512:(tb + 1) * 512], in_=ps[0:ncol, :], func=AF.Copy)
            K.dma("act" if i == 0 else "sp", ZF[s, r0:r0 + ncol, :], stage[i][0:ncol, :], ["p1_stage%d" % i], ["ZF"])
        if STOP <= 2:
            return
        i = len(chunks) % 2
        K.dma("sp", wst[i][:, :, 0:128], wsrc[:, :, C_CKV:C_CKV + 128], [], ["p1_wst%d" % i])
        K.op("dve", "tensor_tensor", ["p1_wst%d" % i, "p1_gm"], ["p1_wbf%d" % i], out=wbf[i][:, :, 0:128],
             in0=wst[i][:, :, 0:128], in1=gm[:].unsqueeze(2).to_broadcast([128, 8, 128]), op=ALU.mult)
        ck = [K.sb(st, "p1_ck%d" % j, [128, 128], F32) for j in range(2)]
        ckb = [K.sb(st, "p1_ckb%d" % j, [128, 128], BF16) for j in range(2)]
        ckT = K.sb(st, "p1_ckT", [128, T], BF16)
        for tt in range(NT):
            j = tt % 2
            pj = tt % 4
            ps = pss[pj]
            for dc in range(8):
                K.mm(ps[:, 0:128], xnT[:, dc, tt * 128:(tt + 1) * 128], wbf[i][:, dc, 0:128],
                     ["p1_wbf%d" % i, "p1_xnT"], ["p1_ps%d" % pj], start=(dc == 0), stop=(dc == 7))
            K.op("dve", "tensor_copy", ["p1_ps%d" % pj], ["p1_ck%d" % j], out=ck[j][:], in_=ps[:, 0:128])
            K.op("act", "activation", ["p1_ck%d" % j], ["p1_junk", "p1_ss%d" % j], out=junk[:, 0:128], in_=ck[j][:],
                 func=AF.Square, accum_out=sss[j][:])
            K.op("act", "activation", ["p1_ss%d" % j, "eps6"], ["p1_ss%d" % j], out=sss[j][:], in_=sss[j][:], func=AF.Sqrt,
                 scale=1.0 / 128, bias=CONST["eps6"][:])
            K.op("dve", "reciprocal", ["p1_ss%d" % j], ["p1_ss%d" % j], out=sss[j][:], in_=sss[j][:])
            K.op("dve", "tensor_scalar", ["p1_ck%d" % j, "p1_ss%d" % j], ["p1_ckb%d" % j], out=ckb[j][:], in0=ck[j][:],
                 scalar1=sss[j][:], scalar2=None, op0=ALU.mult)
            K.tr(pst[:, 0, :], ckb[j][:], identb[:], ["p1_ckb%d" % j, "identb"], ["p1_pst"])
            K.op("act", "activation", ["p1_pst"], ["p1_ckT"], out=ckT[:, tt * 128:(tt + 1) * 128], in_=pst[:, 0, :],
                 func=AF.Copy)
            K.dma("sp", SC["CK"][s, tt * 128:(tt + 1) * 128, :], ckb[j][:], ["p1_ckb%d" % j], ["CK"])
        K.dma("sp", SC["CKT"][s, :, :], ckT[:], ["p1_ckT"], ["CKT"])


NIT = 14


def phase_dsa(K, s, T, Wd, SC, CONST, extra=None):
    nc = K.nc
    NT = T // 128
    ZF = SC["ZF"]
    identb, identf = CONST["identb"], CONST["identf"]
    with ExitStack() as st:
        dps = [K.ps(st, "ds_ps%d" % i, [128, 512], F32) for i in range(4)]
        Ob = [K.ps(st, "ds_o%d" % i, [128, 3, 130], F32) for i in range(3)]
        MT = K.ps(st, "ds_mt", [128, 8, 128], BF16)
        wuk = K.sb(st, "ds_wuk", [128, 512], F32)
        K.dma("sp", wuk[:], Wd["w_uk"].rearrange("o r h d -> r (o h d)"), [], ["ds_wuk"])
        wukT = K.sb(st, "ds_wukT", [64, 8, 128], BF16)
        for h in range(8):
            K.tr(dps[3][0:64, 0:128], wuk[:, h * 64:(h + 1) * 64], identf[:], ["ds_wuk", "identf"], ["ds_ps3"])
            K.op("dve", "tensor_copy", ["ds_ps3"], ["ds_wukT"], out=wukT[:, h, :], in_=dps[3][0:64, 0:128])
        kvn = K.sb(st, "ds_kvn", [128, 1], F32)
        K.dma("sp", kvn[:], Wd["kv_norm"].rearrange("o r -> r o"), [], ["ds_kvn"], allow_slow_non_contiguous=True)
        kvn8 = K.sb(st, "ds_kvn8", [128, 1], F32)
        K.op("dve", "tensor_scalar", ["ds_kvn"], ["ds_kvn8"], out=kvn8[:], in0=kvn[:], scalar1=0.125, scalar2=None,
             op0=ALU.mult)
        wuv = K.sb(st, "ds_wuv", [128, 512], F32)
        K.dma("sp", wuv[:], Wd["w_uv"].rearrange("o r h d -> r (o h d)"), [], ["ds_wuv"])
        wuvb = K.sb(st, "ds_wuvb", [128, 512], BF16)
        K.op("dve", "tensor_scalar", ["ds_wuv", "ds_kvn"], ["ds_wuvb"], out=wuvb[:], in0=wuv[:], scalar1=kvn[:],
             scalar2=None, op0=ALU.mult)
        CKT = K.sb(st, "ds_ckt", [128, T], BF16)
        K.dma("sp", CKT[:], SC["CKT"][s, :, :], ["CKT"], ["ds_ckt"])
        CKA = K.sb(st, "ds_cka", [128, NT, 130], BF16)
        K.op("pool", "memset", [], ["ds_cka"], ap=CKA[:], constant=1.0)
        K.dma("sp", CKA[:, :, 0:128], SC["CK"][s, :, :].rearrange("(k p) r -> p k r", p=128), ["CK"], ["ds_cka"])
        kif = K.sb(st, "ds_kif", [64, T], F32)
        K.dma("act", kif[:], ZF[s, R_KI:R_KI + 64, :], ["ZF"], ["ds_kif"])
        kib = K.sb(st, "ds_kib", [64, T], BF16)
        K.op("dve", "tensor_copy", ["ds_kif"], ["ds_kib"], out=kib[:], in_=kif[:])
        qib = K.sb(st, "ds_qib", [64, 4, 128], BF16)
        zl = K.sb(st, "ds_zl", [128, 128], BF16)
        zb = K.sb(st, "ds_zb", [128, 390], BF16)
        K.op("pool", "memset", [], ["ds_zl"], ap=zl[:], constant=0.0)
        K.op("pool", "memset", [], ["ds_zb"], ap=zb[:], constant=0.0)
        tri01, negtri, pw = CONST["tri01"], CONST["negtri"], CONST["pw"]
        qf = K.sb(st, "ds_qf", [64, 8, 128], F32)
        qb = K.sb(st, "ds_qb", [64, 8, 128], BF16)
        qif = K.sb(st, "ds_qif", [64, 4, 128], F32)
        wif = K.sb(st, "ds_wif", [4, 128], F32)
        wit = K.sb(st, "ds_wit", [128, 4], F32)
        qlat = K.sb(st, "ds_qlat", [128, 1024], BF16)
        isc = K.sb(st, "ds_isc", [128, T], F32)
        junk = K.sb(st, "ds_junk", [128, T], BF16)
        rl = [K.sb(st, "ds_rl%d" % i, [128, 512], F32) for i in range(3)]
        maskb = K.sb(st, "ds_mask", [128, T], BF16)
        col = K.sb(st, "ds_col", [128, 8], F32)
        hk = K.sb(st, "ds_hk", [128, NIT], F32)
        junk2 = K.sb(st, "ds_junk2", [128, T], BF16)
        cola = K.sb(st, "ds_cola", [128, 1], F32)
        mts = [K.sb(st, "ds_mts%d" % i, [128, 128], BF16) for i in range(2)]
        ee = [K.sb(st, "ds_e%d" % i, [128, 4, 128], BF16) for i in range(4)]
        pp = [K.sb(st, "ds_p%d" % i, [128, 4, 128], BF16) for i in range(4)]
        rd = K.sb(st, "ds_rd", [128, 8, 1], F32)
        onb = K.sb(st, "ds_onb", [128, 8, 128], BF16)
        onT = K.sb(st, "ds_onT", [128, 8, 128], BF16)
        ybs = K.sb(st, "ds_ybs", [128, 4, 128], BF16)
        maskbs = [maskb, K.sb(st, "ds_mask1", [128, T], BF16)]
        MN = ["ds_mask", "ds_mask1"]

        def select(qt):
            t0 = qt * 128
            nk = qt + 1
            nkeys = nk * 128
            mb = maskbs[qt % 2]
            mn = MN[qt % 2]
            if qt >= 2:
                K.dma("act", qif[:], ZF[s, R_QI:R_QI + 256, t0:t0 + 128].rearrange("(h p) t -> p h t", p=64), ["ZF"], ["ds_qif"])
                K.op("act", "activation", ["ds_qif"], ["ds_qib"], out=qib[:], in_=qif[:], func=AF.Copy)
                K.dma("act", wif[:], ZF[s, R_WI:R_WI + 4, t0:t0 + 128], ["ZF"], ["ds_wif"])
                K.tr(dps[3][:, 0:4], wif[:], identf[0:4, 0:4], ["ds_wif", "identf"], ["ds_ps3"])
                K.op("dve", "tensor_scalar", ["ds_ps3"], ["ds_wit"], out=wit[:], in0=dps[3][:, 0:4], scalar1=1.0 / 16,
                     scalar2=None, op0=ALU.mult)
                yield
                for kb in range((nkeys + 511) // 512):
                    w = min(512, nkeys - kb * 512)
                    ks = slice(kb * 512, kb * 512 + w)
                    for h in range(4):
                        pb = 2 + (h % 2)
                        K.mm(dps[pb][:, 0:w], qib[:, h, :], kib[:, ks], ["ds_qib", "ds_kib"], ["ds_ps%d" % pb])
                        if h == 0:
                            K.op("dve", "tensor_scalar", ["ds_ps%d" % pb, "ds_wit"], ["ds_isc"], out=isc[:, ks], in0=dps[pb][:, 0:w],
                                 scalar1=0.0, scalar2=wit[:, 0:1], op0=ALU.max, op1=ALU.mult)
                        else:
                            K.op("act", "activation", ["ds_ps%d" % pb], ["ds_rl%d" % (h - 1)], out=rl[h - 1][:, 0:w],
                                 in_=dps[pb][:, 0:w], func=AF.Relu)
                            K.op("dve", "scalar_tensor_tensor", ["ds_rl%d" % (h - 1), "ds_wit", "ds_isc"], ["ds_isc"],
                                 out=isc[:, ks], in0=rl[h - 1][:, 0:w], scalar=wit[:, h:h + 1], in1=isc[:, ks],
                                 op0=ALU.mult, op1=ALU.add)
                    yield
                K.op("dve", "tensor_reduce", ["ds_isc"], ["ds_col"], out=col[:, 0:1], in_=isc[:, 0:nkeys], axis=AX.X, op=ALU.max)
                K.op("dve", "tensor_reduce", ["ds_isc"], ["ds_col"], out=col[:, 1:2], in_=isc[:, 0:nkeys], axis=AX.X, op=ALU.min)
                K.op("dve", "tensor_scalar", ["ds_col"], ["ds_col"], out=col[:, 2:3], in0=col[:, 0:1], scalar1=col[:, 1:2],
                     scalar2=2e-6, op0=ALU.subtract, op1=ALU.add)
                K.op("dve", "tensor_scalar", ["ds_col"], ["ds_col"], out=col[:, 3:4], in0=col[:, 1:2], scalar1=-1e-6,
                     scalar2=None, op0=ALU.add)
                K.op("dve", "tensor_scalar", ["pw", "ds_col"], ["ds_hk"], out=hk[:], in0=pw[:], scalar1=col[:, 2:3],
                     scalar2=None, op0=ALU.mult)
                K.op("dve", "tensor_tensor", ["ds_isc", "negtri"], ["ds_isc"], out=isc[:, t0:t0 + 128], in0=isc[:, t0:t0 + 128],
                     in1=negtri[:], op=ALU.add)
                K.op("dve", "tensor_tensor", ["ds_col", "ds_hk"], ["ds_col", "ds_colm"], out=col[:, 4:5], in0=col[:, 3:4], in1=hk[:, 0:1], op=ALU.add)
                yield
                nd = nkeys
                if nkeys >= 1024 and not os.environ.get("NO_ACTCNT"):
                    nd = ((nkeys * 5 // 8) // 128) * 128
                na = nkeys - nd
                for k in range(NIT):
                    K.op("dve", "tensor_scalar", ["ds_isc", "ds_colm"], ["ds_junk", "ds_col"], out=junk[:, 0:nd],
                         in0=isc[:, 0:nd], scalar1=col[:, 4:5], scalar2=None, op0=ALU.is_ge, op1=ALU.add,
                         accum_out=col[:, 5:6])
                    if na > 0:
                        K.op("act", "activation", ["ds_isc", "ds_colm"], ["ds_junk2", "ds_cola"], out=junk2[:, 0:na], in_=isc[:, nd:nkeys],
                             func=AF.Sign, scale=-1.0, bias=col[:, 4:5], accum_out=cola[:, 0:1])
                        K.op("dve", "scalar_tensor_tensor", ["ds_cola", "ds_col"], ["ds_col"], out=col[:, 5:6], in0=cola[:, 0:1], scalar=-0.5,
                             in1=col[:, 5:6], op0=ALU.mult, op1=ALU.add)
                    K.op("dve", "tensor_scalar", ["ds_col", "ds_hk"], ["ds_col"], out=col[:, 6:7], in0=col[:, 5:6],
                         scalar1=255.5 - 0.5 * na, scalar2=hk[:, k:k + 1], op0=ALU.is_ge, op1=ALU.mult)
                    kn = min(k + 1, NIT - 1)
                    dst = col[:, 4:5] if k < NIT - 1 else col[:, 3:4]
                    K.op("dve", "scalar_tensor_tensor", ["ds_col", "ds_colm", "ds_hk"], ["ds_col", "ds_colm"], out=dst, in0=col[:, 6:7], scalar=col[:, 4:5],
                         in1=hk[:, kn:kn + 1], op0=ALU.add, op1=ALU.subtract)
                    yield
                K.op("dve", "tensor_scalar", ["ds_isc", "ds_col"], [mn], out=mb[:, 0:nkeys], in0=isc[:, 0:nkeys],
                     scalar1=col[:, 3:4], scalar2=None, op0=ALU.is_ge)
            else:
                if qt > 0:
                    K.op("pool", "memset", [], [mn], ap=mb[:, 0:t0], constant=1.0)
                K.op("pool", "tensor_copy", ["tri01"], [mn], out=mb[:, t0:t0 + 128], in_=tri01[:])
            yield

        def attend(qt):
            t0 = qt * 128
            nk = qt + 1
            mb = maskbs[qt % 2]
            mn = MN[qt % 2]
            K.dma("sp", qf[:], ZF[s, R_Q:R_Q + 512, t0:t0 + 128].rearrange("(h p) t -> p h t", p=64), ["ZF"], ["ds_qf"])
            K.op("act", "activation", ["ds_qf"], ["ds_qb"], out=qb[:], in_=qf[:], func=AF.Copy)
            for h in range(8):
                K.mm(dps[h // 4][:, (h % 4) * 128:(h % 4 + 1) * 128], wukT[:, h, :], qb[:, h, :], ["ds_wukT", "ds_qb"],
                     ["ds_ps%d" % (h // 4)])
            for j in range(2):
                K.op("act", "activation", ["ds_ps%d" % j, "ds_kvn8"], ["ds_qlat"], out=qlat[:, j * 512:(j + 1) * 512],
                     in_=dps[j][:], func=AF.Copy, scale=kvn8[:, 0:1])
            for bq in range(3):
                K.mm(Ob[bq][:].rearrange("p a b -> p (a b)"), zl[:], zb[:], ["ds_zl", "ds_zb"], ["ds_o%d" % bq], start=True,
                     stop=False, skip_group_check=True)
            yield
            def front(kt):
                par = kt % 2
                K.tr(MT[:, 0, :], mb[:, kt * 128:(kt + 1) * 128], identb[:], [mn, "identb"], ["ds_mt0", "ds_mt1", "ds_mtall"])
                K.op("act", "activation", ["ds_mt0", "ds_mt1", "ds_mtall"], ["ds_mts%d" % par], out=mts[par][:], in_=MT[:, 0, :], func=AF.Copy)
                for j in range(2):
                    ej = 2 * par + j
                    K.mm(dps[j][:], CKT[:, kt * 128:(kt + 1) * 128], qlat[:, j * 512:(j + 1) * 512], ["ds_ckt", "ds_qlat"],
                         ["ds_ps%d" % j])
                    K.op("act", "activation", ["ds_ps%d" % j], ["ds_e%d" % ej], out=ee[ej][:],
                         in_=dps[j][:].rearrange("p (a b) -> p a b", a=4), func=AF.Exp)
            front(0)
            for kt in range(nk):
                par = kt % 2
                mtb = "ds_mt%d" % par
                if kt + 1 < nk:
                    front(kt + 1)
                for j in range(2):
                    ej = 2 * par + j
                    K.op("dve", "tensor_tensor", ["ds_e%d" % ej, "ds_mts%d" % par], ["ds_p%d" % ej], out=pp[ej][:], in0=ee[ej][:],
                         in1=mts[par][:].unsqueeze(1).to_broadcast([128, 4, 128]), op=ALU.mult)
                for j in range(2):
                    ej = 2 * par + j
                    for hh in range(4):
                        h = 4 * j + hh
                        K.mm(Ob[h // 3][:, h % 3, 0:129], pp[ej][:, hh, :], CKA[:, kt, 0:129], ["ds_p%d" % ej, "ds_cka"],
                             ["ds_o%d" % (h // 3)], start=False, stop=(kt == nk - 1), skip_group_check=True)
                yield
            for bq in range(3):
                nh = 3 if bq < 2 else 2
                K.op("dve", "reciprocal", ["ds_o%d" % bq], ["ds_rd"], out=rd[:, 3 * bq:3 * bq + nh, :], in_=Ob[bq][:, 0:nh, 128:129])
                K.op("dve", "tensor_tensor", ["ds_o%d" % bq, "ds_rd"], ["ds_onb"], out=onb[:, 3 * bq:3 * bq + nh, :],
                     in0=Ob[bq][:, 0:nh, 0:128], in1=rd[:, 3 * bq:3 * bq + nh, :].to_broadcast([128, nh, 128]), op=ALU.mult)
            for h in range(8):
                K.tr(MT[:, h, :], onb[:, h, :], identb[:], ["ds_onb", "identb"], ["ds_mt0", "ds_mt1", "ds_mtall"])
            K.op("act", "activation", ["ds_mt0", "ds_mt1", "ds_mtall"], ["ds_onT"], out=onT[:], in_=MT[:], func=AF.Copy)
            for h in range(8):
                K.mm(dps[0][(h % 2) * 64:(h % 2 + 1) * 64, (h // 2) * 128:(h // 2 + 1) * 128], wuvb[:, h * 64:(h + 1) * 64],
                     onT[:, h, :], ["ds_wuvb", "ds_onT"], ["ds_ps0"])
            K.op("dve", "tensor_copy", ["ds_ps0"], ["ds_ybs"], out=ybs[:], in_=dps[0][:].rearrange("p (a b) -> p a b", a=4))
            K.dma("sp", SC["YB"][s, :, t0:t0 + 128].rearrange("(c p) t -> p c t", p=128), ybs[:], ["ds_ybs"], ["YB"])
            yield

        xg = extra(st) if extra is not None else None
        for step in range(NT + 1):
            gens = []
            if step >= 1:
                gens.append(attend(step - 1))
            if step < NT:
                gens.append(select(step))
            if xg is not None:
                try:
                    next(xg)
                except StopIteration:
                    xg = None
            while gens:
                for g in list(gens):
                    try:
                        next(g)
                    except StopIteration:
                        gens.remove(g)
        if xg is not None:
            for _ in xg:
                pass


class _Stop(Exception):
    pass


def phase_rwkv(K, s, T, Wd, SC, CONST):
    _phase_rwkv(K, s, T, Wd, SC, CONST)
    K.S.muted = False


def _phase_rwkv(K, s, T, Wd, SC, CONST):
    nc = K.nc
    TBK = 256
    NCH = TBK // 64
    NBK = T // TBK
    ZF = SC["ZF"]
    identf = CONST["identf"]
    bo, bo64, maskq, lowm, resetm = CONST["bo"], CONST["bo64"], CONST["maskq"], CONST["lowm"], CONST["resetm"]
    with ExitStack() as st:
        rp = [K.ps(st, "rk_p%d" % i, [128, 512], F32) for i in range(8)]
        RP = ["rk_p%d" % i for i in range(8)]

        def colload(tag, ap512, n=4):
            t = K.sb(st, tag, [128, n], F32)
            K.dma("sp", t[:], ap512.rearrange("o (c p) -> p (o c)", p=128), [], [tag], allow_slow_non_contiguous=True)
            return t
        mu = colload("rk_mu", Wd["shift_mu"], 14)
        w0c = colload("rk_w0c", Wd["rw_w0"])
        a0c = colload("rk_a0c", Wd["rw_a0"])
        kkc = colload("rk_kkc", Wd["rw_k_k"])
        kac = colload("rk_kac", Wd["rw_k_a"])
        rkc = colload("rk_rkc", Wd["rw_r_k"].rearrange("o h d -> o (h d)"))
        lnw = colload("rk_lnw", Wd["rw_ln_w"])
        lnb = colload("rk_lnb", Wd["rw_ln_b"])
        w2a2 = K.sb(st, "rk_w2a2", [128, 512], F32)
        K.dma("sp", w2a2[0:64, :], Wd["rw_w2"][0], [], ["rk_w2a2"])
        K.dma("sp", w2a2[64:128, :], Wd["rw_a2"][0], [], ["rk_w2a2"])
        g2 = K.sb(st, "rk_g2", [128, 512], F32)
        K.dma("sp", g2[:], Wd["rw_g2"][0], [], ["rk_g2"])
        epsg = K.sb(st, "rk_epsg", [128, 1], F32)
        K.op("dve", "memset", [], ["rk_epsg"], ap=epsg[:], constant=64e-5)
        zin = K.sb(st, "rk_zin", [128, 14, TBK + 1], F32)
        zs = K.sb(st, "rk_zs", [128, 14, TBK], F32)
        tw = K.sb(st, "rk_tw", [128, TBK], F32)
        sg = K.sb(st, "rk_sg", [128, TBK], F32)

        def t4(tag):
            return K.sb(st, tag, [128, 4, TBK], F32)
        lw, aa, gg, LL, eL, enL, eLm, kk, t1, kp, bb, bon, Yb = [t4("rk_" + n) for n in
            ("lw", "aa", "gg", "LL", "eL", "enL", "eLm", "kk", "t1", "kp", "bb", "bon", "Yb")]
        QR = K.sb(st, "rk_QR", [128, 4, NCH, 2, 64], F32)
        KB = K.sb(st, "rk_KB", [128, 4, NCH, 2, 64], F32)
        gC = K.sb(st, "rk_gC", [128, 4, NCH], F32)
        M = K.sb(st, "rk_M", [128, 4, 64], F32)
        K.op("dve", "memset", [], ["rk_M"], ap=M[:], constant=0.0)
        KBTs = [K.sb(st, "rk_KBT%d" % i, [128, 4, 128], F32) for i in range(2)]
        VTs = [K.sb(st, "rk_VT%d" % i, [64, 4, 128], F32) for i in range(2)]
        ATs = [K.sb(st, "rk_AT%d" % i, [128, 8, 128], F32) for i in range(2)]
        DDT = BF16 if os.environ.get("RW_BF16", "1") == "1" else F32
        Am = [K.sb(st, "rk_Am%d" % i, [128, 8, 64], DDT) for i in range(2)]
        Bm = [K.sb(st, "rk_Bm%d" % i, [128, 8, 64], DDT) for i in range(2)]
        Pm = [K.sb(st, "rk_Pm%d" % i, [128, 8, 64], DDT) for i in range(2)]
        PmFs = [K.sb(st, "rk_PmF%d" % i, [128, 8, 64], F32) for i in range(2)]
        Rs = K.sb(st, "rk_Rs", [128, 512], F32)
        Us = K.sb(st, "rk_Us", [128, 512], F32)
        yab = K.sb(st, "rk_yab", [128, 4, TBK], BF16)
        H = slice(64, 128)

        def v4(t):
            return t[:].rearrange("p c (n t) -> p c n t", t=64)

        def bc(colt, n=4, w=TBK):
            return colt[:].unsqueeze(2).to_broadcast([128, n, w])

        RS = float(os.environ.get("RSTOP", "99"))

        def chk(k):
            if RS <= k:
                K.S.muted = True

        for tb in range(NBK):
            t0 = tb * TBK
            if tb == 0:
                K.op("dve", "memset", [], ["rk_zin"], ap=zin[:, :, 0:1], constant=0.0)
                K.dma("sp", zin[:, :, 1:TBK + 1], ZF[s, 0:1792, 0:TBK].rearrange("(c p) t -> p c t", p=128), ["ZF"], ["rk_zin"])
            else:
                K.dma("sp", zin[:, :, :], ZF[s, 0:1792, t0 - 1:t0 + TBK].rearrange("(c p) t -> p c t", p=128), ["ZF"], ["rk_zin"])
            K.op("dve", "tensor_tensor", ["rk_zin"], ["rk_zs"], out=zs[:], in0=zin[:, :, 0:TBK], in1=zin[:, :, 1:TBK + 1], op=ALU.subtract)
            for c14 in range(14):
                K.op("dve", "scalar_tensor_tensor", ["rk_zs", "rk_mu", "rk_zin"], ["rk_zs"], out=zs[:, c14, :], in0=zs[:, c14, :], scalar=mu[:, c14:c14 + 1],
                     in1=zin[:, c14, 1:TBK + 1], op0=ALU.mult, op1=ALU.add)
            chk(1)
            r_, k_, v_ = zs[:, 0:4, :], zs[:, 4:8, :], zs[:, 8:12, :]
            K.op("act", "activation", ["rk_zs"], ["rk_tw"], out=tw[0:64, :], in_=zs[0:64, 12, :], func=AF.Tanh)
            K.op("act", "activation", ["rk_zs"], ["rk_sg"], out=sg[:], in_=zs[:, 13, :], func=AF.Sigmoid)
            for cc in range(4):
                cs = slice(cc * 128, (cc + 1) * 128)
                K.mm(rp[0][:, 0:TBK], w2a2[0:64, cs], tw[0:64, :], ["rk_w2a2", "rk_tw"], [RP[0]])
                K.op("act", "activation", [RP[0], "rk_w0c"], ["rk_lw"], out=lw[:, cc, :], in_=rp[0][:, 0:TBK], func=AF.Sigmoid, bias=w0c[:, cc:cc + 1])
                K.mm(rp[1][:, 0:TBK], w2a2[H, cs], zs[H, 12, :], ["rk_w2a2", "rk_zs"], [RP[1]])
                K.op("act", "activation", [RP[1], "rk_a0c"], ["rk_aa"], out=aa[:, cc, :], in_=rp[1][:, 0:TBK], func=AF.Sigmoid, bias=a0c[:, cc:cc + 1])
                K.mm(rp[2][:, 0:TBK], g2[:, cs], sg[:], ["rk_g2", "rk_sg"], [RP[2]])
                K.op("dve", "tensor_copy", [RP[2]], ["rk_gg"], out=gg[:, cc, :], in_=rp[2][:, 0:TBK])
            chk(2)
            K.op("dve", "tensor_scalar", ["rk_lw"], ["rk_lw"], out=lw[:], in0=lw[:], scalar1=-0.6065306597126334, scalar2=None, op0=ALU.mult)
            for cc in range(4):
                K.op("dve", "tensor_tensor_scan", ["rk_lw", "resetm"], ["rk_LL"], out=LL[:, cc, :], data0=resetm[:], data1=lw[:, cc, :],
                     initial=0.0, op0=ALU.mult, op1=ALU.add)
            K.op("act", "activation", ["rk_LL"], ["rk_eL"], out=eL[:], in_=LL[:], func=AF.Exp)
            K.op("act", "activation", ["rk_LL"], ["rk_enL"], out=enL[:], in_=LL[:], func=AF.Exp, scale=-1.0)
            K.op("pool", "tensor_tensor", ["rk_LL", "rk_lw"], ["rk_t1"], out=t1[:], in0=LL[:], in1=lw[:], op=ALU.subtract)
            K.op("act", "activation", ["rk_t1"], ["rk_eLm"], out=eLm[:], in_=t1[:], func=AF.Exp)
            K.op("dve", "tensor_tensor", ["rk_zs", "rk_kkc"], ["rk_kk"], out=kk[:], in0=k_, in1=bc(kkc), op=ALU.mult)
            K.op("pool", "tensor_tensor", ["rk_kk"], ["rk_t1"], out=t1[:], in0=kk[:], in1=kk[:], op=ALU.mult)
            for cc in range(4):
                K.mm(rp[cc % 4][:, 0:TBK], bo[:], t1[:, cc, :], ["bo", "rk_t1"], [RP[cc % 4]])
                K.op("act", "activation", [RP[cc % 4]], ["rk_kp"], out=kp[:, cc, :], in_=rp[cc % 4][:, 0:TBK], func=AF.Sqrt)
            K.op("dve", "tensor_scalar", ["rk_kp"], ["rk_kp"], out=kp[:], in0=kp[:], scalar1=1e-12, scalar2=None, op0=ALU.max)
            K.op("dve", "reciprocal", ["rk_kp"], ["rk_kp"], out=kp[:], in_=kp[:])
            K.op("dve", "tensor_tensor", ["rk_kk", "rk_kp"], ["rk_kk"], out=kk[:], in0=kk[:], in1=kp[:], op=ALU.mult)
            for cc in range(4):
                K.op("dve", "tensor_scalar", ["rk_aa", "rk_kac"], ["rk_t1"], out=t1[:, cc, :], in0=aa[:, cc, :], scalar1=-1.0, scalar2=kac[:, cc:cc + 1],
                     op0=ALU.add, op1=ALU.mult)
            K.op("dve", "scalar_tensor_tensor", ["rk_t1", "rk_zs"], ["rk_kp"], out=kp[:], in0=t1[:], scalar=1.0, in1=k_, op0=ALU.add, op1=ALU.mult)
            K.op("pool", "tensor_tensor", ["rk_kk", "rk_aa"], ["rk_bb"], out=bb[:], in0=kk[:], in1=aa[:], op=ALU.mult)
            K.op("dve", "tensor_tensor", ["rk_zs", "rk_eL"], ["rk_QR"], out=QR[:, :, :, 1, :], in0=r_.rearrange("p c (n t) -> p c n t", t=64), in1=v4(eL), op=ALU.mult)
            K.op("pool", "tensor_tensor", ["rk_kk", "rk_eLm"], ["rk_QR"], out=QR[:, :, :, 0, :], in0=v4(kk), in1=v4(eLm), op=ALU.mult)
            K.op("dve", "tensor_tensor", ["rk_kp", "rk_enL"], ["rk_KB"], out=KB[:, :, :, 0, :], in0=v4(kp), in1=v4(enL), op=ALU.mult)
            K.op("pool", "tensor_tensor", ["rk_bb", "rk_enL"], ["rk_KB"], out=KB[:, :, :, 1, :], in0=v4(bb), in1=v4(enL), op=ALU.mult)
            K.op("dve", "tensor_copy", ["rk_eL"], ["rk_gC"], out=gC[:], in_=v4(eL)[:, :, :, 63])
            K.op("pool", "tensor_tensor", ["rk_zs", "rk_kp"], ["rk_t1"], out=t1[:], in0=r_, in1=kp[:], op=ALU.mult)
            K.op("pool", "tensor_tensor", ["rk_t1", "rk_rkc"], ["rk_t1"], out=t1[:], in0=t1[:], in1=bc(rkc), op=ALU.mult)
            for cc in range(4):
                K.mm(rp[cc % 4][:, 0:TBK], bo[:], t1[:, cc, :], ["bo", "rk_t1"], [RP[cc % 4]])
                K.op("dve", "tensor_tensor", [RP[cc % 4], "rk_zs"], ["rk_bon"], out=bon[:, cc, :], in0=rp[cc % 4][:, 0:TBK], in1=zs[:, 8 + cc, :], op=ALU.mult)
            chk(3)
            def ev(t, par):
                return t.rearrange("p (a two) b -> p a two b", two=2)[:, :, par, :]

            def pre(c):
                q = c % 2
                KBT, VT, AT, PmF = KBTs[q], VTs[q], ATs[q], PmFs[q]
                nKBT, nVT, nAT, nPmF = "rk_KBT%d" % q, "rk_VT%d" % q, "rk_AT%d" % q, "rk_PmF%d" % q
                for cc in range(4):
                    K.tr(rp[0][:, cc * 128:(cc + 1) * 128], KB[:, cc, c, :, :].rearrange("p a b -> p (a b)"), identf[:], ["rk_KB", "identf"], [RP[0]])
                    K.tr(rp[1][0:64, cc * 128:(cc + 1) * 128], zs[:, 8 + cc, c * 64:(c + 1) * 64], identf[:], ["rk_zs", "identf"], [RP[1]])
                K.op("act", "activation", [RP[0]], [nKBT], out=KBT[:].rearrange("p a b -> p (a b)"), in_=rp[0][:], func=AF.Copy)
                K.op("dve", "tensor_copy", [RP[1]], [nVT], out=VT[:].rearrange("p a b -> p (a b)"), in_=rp[1][0:64, :])
                yield
                for h in range(8):
                    cc, h2 = h // 2, h % 2
                    rows = slice(h2 * 64, (h2 + 1) * 64)
                    K.mm(rp[2 + h2][:, cc * 128:(cc + 1) * 128], KB[rows, cc, c, :, :].rearrange("p a b -> p (a b)"),
                         QR[rows, cc, c, :, :].rearrange("p a b -> p (a b)"), ["rk_KB", "rk_QR"], [RP[2 + h2]])
                    K.mm(rp[h2][H, cc * 64:(cc + 1) * 64], QR[rows, cc, c, 0, :], KB[rows, cc, c, 1, :], ["rk_QR", "rk_KB"], [RP[h2]])
                for h2 in range(2):
                    K.op("dve", "tensor_tensor", [RP[2 + h2], "maskq"], [nAT], out=ev(AT[:], h2),
                         in0=rp[2 + h2][:].rearrange("p (a b) -> p a b", a=4), in1=maskq[:].unsqueeze(1).to_broadcast([128, 4, 128]), op=ALU.mult)
                    K.op("dve", "tensor_tensor", [RP[h2], "lowm"], ["rk_Bm0"], out=ev(Bm[0][H, :, :], h2),
                         in0=rp[h2][H, 0:256].rearrange("p (a b) -> p a b", a=4), in1=lowm[H, :].unsqueeze(1).to_broadcast([64, 4, 64]), op=ALU.mult)
                K.op("act", "activation", [nAT], ["rk_Am0"], out=Am[0][H, :, :], in_=AT[H, :, 0:64], func=AF.Copy)
                K.op("dve", "tensor_tensor", ["identf", nAT], ["rk_Pm0"], out=Pm[0][H, :, :],
                     in0=identf[H, 64:128].unsqueeze(1).to_broadcast([64, 8, 64]), in1=AT[H, :, 0:64], op=ALU.subtract)
                yield
                for lvl in range(5):
                    ci, ni = lvl % 2, (lvl + 1) % 2
                    An, Bn, Pn = "rk_Am%d" % ni, "rk_Bm%d" % ni, "rk_Pm%d" % ni
                    Ac, Bc, Pc = "rk_Am%d" % ci, "rk_Bm%d" % ci, "rk_Pm%d" % ci
                    for h in range(8):
                        hs = slice(h * 64, (h + 1) * 64)
                        if lvl < 4:
                            K.mm(rp[2][H, hs], Bm[ci][H, h, :], Am[ci][H, h, :], [Ac, Bc], [RP[2]])
                        K.mm(rp[3][H, hs], Am[ci][H, h, :], Bm[ci][H, h, :], [Ac, Bc], [RP[3]])
                    if lvl < 4:
                        K.op("act", "activation", [RP[2]], [An], out=Am[ni][H, :, :], in_=rp[2][H, :].rearrange("p (a b) -> p a b", a=8), func=AF.Copy)
                    K.op("dve", "tensor_copy", [RP[3]], [Bn], out=Bm[ni][H, :, :], in_=rp[3][H, :].rearrange("p (a b) -> p a b", a=8))
                    yield
                    for h in range(8):
                        hs = slice(h * 64, (h + 1) * 64)
                        K.mm(rp[0][H, hs], Bm[ni][H, h, :], Pm[ci][H, h, :], [Bn, Pc], [RP[0]])
                    if lvl < 4:
                        K.op("dve", "tensor_tensor", [RP[0], Pc], [Pn], out=Pm[ni][H, :, :], in0=rp[0][H, :].rearrange("p (a b) -> p a b", a=8),
                             in1=Pm[ci][H, :, :], op=ALU.add)
                    else:
                        K.op("dve", "tensor_tensor", [RP[0], Pc], [nPmF], out=PmF[H, :, :], in0=rp[0][H, :].rearrange("p (a b) -> p a b", a=8),
                             in1=Pm[ci][H, :, :], op=ALU.add)
                    yield

            def post(c):
                q = c % 2
                KBT, VT, AT, PF = KBTs[q], VTs[q], ATs[q], PmFs[q]
                nKBT, nVT, nAT, PFn = "rk_KBT%d" % q, "rk_VT%d" % q, "rk_AT%d" % q, "rk_PmF%d" % q
                Rs3 = Rs[H, :].rearrange("p (a b) -> p a b", a=8)
                for h in range(8):
                    cc, h2 = h // 2, h % 2
                    rows = slice(h2 * 64, (h2 + 1) * 64)
                    hs = slice(h * 64, (h + 1) * 64)
                    K.mm(rp[6 + h2][H, cc * 64:(cc + 1) * 64], QR[rows, cc, c, 0, :], M[rows, cc, :], ["rk_QR", "rk_M"], [RP[6 + h2]])
                    K.mm(rp[4][H, hs], AT[0:64, h, 0:64], VT[0:64, cc, h2 * 64:(h2 + 1) * 64], [nAT, nVT], [RP[4]])
                for h2 in range(2):
                    K.op("act", "activation", [RP[6 + h2]], ["rk_Rs"], out=ev(Rs3, h2), in_=rp[6 + h2][H, 0:256].rearrange("p (a b) -> p a b", a=4), func=AF.Copy)
                K.op("dve", "tensor_tensor", [RP[4], "rk_Rs"], ["rk_Rs"], out=Rs[H, :], in0=rp[4][H, :], in1=Rs[H, :], op=ALU.add)
                yield
                for h in range(8):
                    hs = slice(h * 64, (h + 1) * 64)
                    K.mm(rp[5][H, hs], PF[H, h, :], Rs[H, hs], [PFn, "rk_Rs"], [RP[5]])
                K.op("act", "activation", [RP[5]], ["rk_Us"], out=Us[H, :], in_=rp[5][H, :], func=AF.Copy, scale=-1.0)
                yield
                for h in range(8):
                    cc, h2 = h // 2, h % 2
                    rows = slice(h2 * 64, (h2 + 1) * 64)
                    hs = slice(h * 64, (h + 1) * 64)
                    ys = slice(cc * 64, (cc + 1) * 64)
                    K.mm(rp[6 + h2][rows, ys], M[rows, cc, :], QR[rows, cc, c, 1, :], ["rk_M", "rk_QR"], [RP[6 + h2]])
                    K.mm(rp[4][rows, ys], VT[0:64, cc, h2 * 64:(h2 + 1) * 64], AT[0:64, h, 64:128], [nVT, nAT], [RP[4]])
                    K.mm(rp[5][rows, ys], Us[H, hs], AT[H, h, 64:128], ["rk_Us", nAT], [RP[5]])
                for h2 in range(2):
                    rows = slice(h2 * 64, (h2 + 1) * 64)
                    K.op("act", "activation", [RP[6 + h2]], ["rk_Yb"], out=Yb[rows, :, c * 64:(c + 1) * 64],
                         in_=rp[6 + h2][rows, 0:256].rearrange("p (a b) -> p a b", a=4), func=AF.Copy)
                yv = Yb[:, :, c * 64:(c + 1) * 64]
                K.op("dve", "tensor_tensor", [RP[4], "rk_Yb"], ["rk_Yb"], out=yv, in0=rp[4][:, 0:256].rearrange("p (a b) -> p a b", a=4), in1=yv, op=ALU.add)
                K.op("dve", "tensor_tensor", [RP[5], "rk_Yb"], ["rk_Yb"], out=yv, in0=rp[5][:, 0:256].rearrange("p (a b) -> p a b", a=4), in1=yv, op=ALU.add)
                yield
                for cc in range(4):
                    for h2 in range(2):
                        h = 2 * cc + h2
                        rows = slice(h2 * 64, (h2 + 1) * 64)
                        hs = slice(h * 64, (h + 1) * 64)
                        K.mm(rp[4][rows, cc * 64:(cc + 1) * 64], KBT[0:64, cc, rows], VT[0:64, cc, rows], [nKBT, nVT], [RP[4]])
                        K.mm(rp[5][rows, cc * 64:(cc + 1) * 64], KBT[H, cc, rows], Us[H, hs], [nKBT, "rk_Us"], [RP[5]])
                K.op("dve", "tensor_tensor", [RP[4], "rk_M"], ["rk_M"], out=M[:], in0=rp[4][:, 0:256].rearrange("p (a b) -> p a b", a=4), in1=M[:], op=ALU.add)
                K.op("dve", "tensor_tensor", [RP[5], "rk_M"], ["rk_M"], out=M[:], in0=rp[5][:, 0:256].rearrange("p (a b) -> p a b", a=4), in1=M[:], op=ALU.add)
                K.op("dve", "tensor_tensor", ["rk_M", "rk_gC"], ["rk_M"], out=M[:], in0=M[:],
                     in1=gC[:, :, c:c + 1].to_broadcast([128, 4, 64]), op=ALU.mult)
                yield

            for step in range(NCH + 1):
                gens = []
                if step >= 1:
                    gens.append(post(step - 1))
                if step < NCH:
                    gens.append(pre(step))
                while gens:
                    for g in list(gens):
                        try:
                            next(g)
                        except StopIteration:
                            gens.remove(g)
            chk(7)
            for cc in range(4):
                K.mm(rp[0][:, 0:TBK], bo64[:], Yb[:, cc, :], ["bo64", "rk_Yb"], [RP[0]])
                K.op("dve", "tensor_tensor", ["rk_Yb", RP[0]], ["rk_t1"], out=t1[:, cc, :], in0=Yb[:, cc, :], in1=rp[0][:, 0:TBK], op=ALU.subtract)
                K.op("pool", "tensor_tensor", ["rk_t1"], ["rk_kk"], out=kk[:, cc, :], in0=t1[:, cc, :], in1=t1[:, cc, :], op=ALU.mult)
                K.mm(rp[1][:, 0:TBK], bo64[:], kk[:, cc, :], ["bo64", "rk_kk"], [RP[1]])
                K.op("act", "activation", [RP[1], "rk_epsg"], ["rk_kp"], out=kp[:, cc, :], in_=rp[1][:, 0:TBK], func=AF.Sqrt, bias=epsg[:])
            K.op("dve", "reciprocal", ["rk_kp"], ["rk_kp"], out=kp[:], in_=kp[:])
            K.op("dve", "tensor_tensor", ["rk_t1", "rk_kp"], ["rk_t1"], out=t1[:], in0=t1[:], in1=kp[:], op=ALU.mult)
            K.op("pool", "tensor_tensor", ["rk_t1", "rk_lnw"], ["rk_t1"], out=t1[:], in0=t1[:], in1=bc(lnw), op=ALU.mult)
            K.op("pool", "tensor_tensor", ["rk_t1", "rk_lnb"], ["rk_t1"], out=t1[:], in0=t1[:], in1=bc(lnb), op=ALU.add)
            K.op("dve", "tensor_tensor", ["rk_t1", "rk_bon"], ["rk_t1"], out=t1[:], in0=t1[:], in1=bon[:], op=ALU.add)
            K.op("dve", "tensor_tensor", ["rk_t1", "rk_gg"], ["rk_yab"], out=yab[:], in0=t1[:], in1=gg[:], op=ALU.mult)
            K.dma("sp", SC["YA"][s, :, t0:t0 + TBK].rearrange("(c p) t -> p c t", p=128), yab[:], ["rk_yab"], ["YA"])


def norm_T(K, tag, src_tile, src_name, xn, ss, junk, pst, dstT, col0, identb, eps):
    K.op("act", "activation", [src_name], [tag + "junk", tag + "ss"], out=junk[:], in_=src_tile, func=AF.Square, accum_out=ss[:])
    K.op("act", "activation", [tag + "ss", "eps6"], [tag + "ss"], out=ss[:], in_=ss[:], func=AF.Sqrt, scale=1.0 / D, bias=eps[:])
    K.op("dve", "reciprocal", [tag + "ss"], [tag + "ss"], out=ss[:], in_=ss[:])
    K.op("dve", "tensor_scalar", [src_name, tag + "ss"], [tag + "xn"], out=xn[:], in0=src_tile, scalar1=ss[:], scalar2=None, op0=ALU.mult)
    for c in range(8):
        K.tr(pst[:, c, :], xn[:, c * 128:(c + 1) * 128], identb[:], [tag + "xn", "identb"], [tag + "pst"])
    K.op("act", "activation", [tag + "pst"], [dstT[1]], out=dstT[0][:, :, col0:col0 + 128], in_=pst[:], func=AF.Copy)


def phase_mix(K, s, T, X, Wd, SC, CONST):
    ZF = SC["ZF"]
    with ExitStack() as st:
        wpa = load_cast(K, st, "mx_wpa", Wd["w_proj_a"][0], 512, 1024)
        wpb = load_cast(K, st, "mx_wpb", Wd["w_proj_b"][0], 512, 1024)
        wout = load_cast(K, st, "mx_wout", Wd["w_out"][0], 1024, 1024)
        ps = [K.ps(st, "mx_ps%d" % i, [128, 512], F32) for i in range(4)]
        ya = K.sb(st, "mx_ya", [128, 4, 512], BF16)
        yb = K.sb(st, "mx_yb", [128, 4, 512], BF16)
        G = K.sb(st, "mx_G", [128, 16, 512], F32)
        ta = K.sb(st, "mx_ta", [128, 512], F32)
        tb_ = K.sb(st, "mx_tb", [128, 512], F32)
        mixT = K.sb(st, "mx_mixT", [128, 8, 512], BF16)
        xt = [K.sb(st, "mx_xt%d" % i, [128, D], F32) for i in range(2)]
        for tb in range(T // 512):
            t0 = tb * 512
            K.dma("sp", ya[:], SC["YA"][s, :, t0:t0 + 512].rearrange("(c p) t -> p c t", p=128), ["YA"], ["mx_ya"])
            K.dma("act", yb[:], SC["YB"][s, :, t0:t0 + 512].rearrange("(c p) t -> p c t", p=128), ["YB"], ["mx_yb"])
            K.dma("sp", G[:], ZF[s, R_G:R_G + 2048, t0:t0 + 512].rearrange("(c p) t -> p c t", p=128), ["ZF"], ["mx_G"])
            for cc in range(8):
                cs = slice(cc * 128, (cc + 1) * 128)
                for k in range(4):
                    K.mm(ps[0][:], wpa[:, k, cs], ya[:, k, :], ["mx_wpa", "mx_ya"], ["mx_ps0"], start=(k == 0), stop=(k == 3))
                for k in range(4):
                    K.mm(ps[1][:], wpb[:, k, cs], yb[:, k, :], ["mx_wpb", "mx_yb"], ["mx_ps1"], start=(k == 0), stop=(k == 3))
                K.op("dve", "tensor_tensor", ["mx_ps0", "mx_G"], ["mx_ta"], out=ta[:], in0=ps[0][:], in1=G[:, cc, :], op=ALU.mult)
                K.op("dve", "tensor_tensor", ["mx_ps1", "mx_G"], ["mx_tb"], out=tb_[:], in0=ps[1][:], in1=G[:, 8 + cc, :], op=ALU.mult)
                K.op("pool", "tensor_tensor", ["mx_ta", "mx_tb"], ["mx_mixT"], out=mixT[:, cc, :], in0=ta[:], in1=tb_[:], op=ALU.add)
            for tt in range(4):
                i = tt % 2
                r0 = s * T + t0 + tt * 128
                K.dma("act", xt[i][:], X[r0:r0 + 128, :], [], ["mx_xt%d" % i])
                for half in range(2):
                    pj = 2 + half
                    for k in range(8):
                        K.mm(ps[pj][:], mixT[:, k, tt * 128:(tt + 1) * 128], wout[:, k, half * 512:(half + 1) * 512], ["mx_mixT", "mx_wout"],
                             ["mx_ps%d" % pj], start=(k == 0), stop=(k == 7))
                    K.op("dve", "tensor_tensor", ["mx_ps%d" % pj, "mx_xt%d" % i], ["mx_xt%d" % i], out=xt[i][:, half * 512:(half + 1) * 512],
                         in0=ps[pj][:], in1=xt[i][:, half * 512:(half + 1) * 512], op=ALU.add)
                K.dma("sp", SC["H1"][r0:r0 + 128, :], xt[i][:], ["mx_xt%d" % i], ["H1"])


def colvec(K, st, tag, ap, n=8):
    t = K.sb(st, tag, [128, n], F32)
    K.dma("sp", t[:], ap.rearrange("o (c p) -> p (o c)", p=128), [], [tag], allow_slow_non_contiguous=True)
    return t


def phase_cross(K, s, T, MEM, Wd, SC, CONST):
    identb = CONST["identb"]
    with ExitStack() as st:
        nrc = colvec(K, st, "cx_nrc", Wd["norm_cross"])
        nrm = colvec(K, st, "cx_nrm", Wd["norm_mem"])
        wcq = load_cast(K, st, "cx_wcq", Wd["w_cq"][0], 1024, 1024, scale_col=(nrc, "cx_nrc"))
        wckv = load_cast(K, st, "cx_wckv", Wd["w_ckv"][0], 1024, 2048, scale_col=(nrm, "cx_nrm"))
        wco = load_cast(K, st, "cx_wco", Wd["w_co"][0], 1024, 1024)
        ps = [K.ps(st, "cx_ps%d" % i, [128, 512], F32) for i in range(6)]
        pst = K.ps(st, "cx_pst", [128, 8, 128], BF16)
        ht = K.sb(st, "cx_ht", [128, 4, D], F32)
        xn = K.sb(st, "cx_xn", [128, D], BF16)
        ss = K.sb(st, "cx_ss", [128, 1], F32)
        junk = K.sb(st, "cx_junk", [128, D], F32)
        memT = K.sb(st, "cx_memT", [128, 8, 256], BF16)
        ones = K.sb(st, "cx_ones", [128, 128], BF16)
        K.op("pool", "memset", [], ["cx_ones"], ap=ones[:], constant=1.0)
        for mt in range(2):
            K.dma("sp", ht[:, 0, :], MEM[s * 256 + mt * 128: s * 256 + (mt + 1) * 128, :], [], ["cx_ht0"])
            norm_T(K, "cx_", ht[:, 0, :], "cx_ht0", xn, ss, junk, pst, (memT, "cx_memT"), mt * 128, identb, CONST["eps6"])
        kTs = K.sb(st, "cx_kTs", [128, 8, 256], BF16)
        vS = K.sb(st, "cx_vS", [128, 2, 1024], BF16)
        for j in range(8):
            for k in range(8):
                K.mm(ps[0][:, 0:256], wckv[:, k, j * 128:(j + 1) * 128], memT[:, k, :], ["cx_wckv", "cx_memT"], ["cx_ps0"], start=(k == 0), stop=(k == 7))
            K.op("dve", "tensor_copy", ["cx_ps0"], ["cx_kTs"], out=kTs[:, j, :], in_=ps[0][:, 0:256])
        for mt in range(2):
            for half in range(2):
                for k in range(8):
                    K.mm(ps[1][:], memT[:, k, mt * 128:(mt + 1) * 128], wckv[:, k, 1024 + half * 512:1024 + (half + 1) * 512], ["cx_wckv", "cx_memT"],
                         ["cx_ps1"], start=(k == 0), stop=(k == 7))
                K.op("dve", "tensor_copy", ["cx_ps1"], ["cx_vS"], out=vS[:, mt, half * 512:(half + 1) * 512], in_=ps[1][:])
        hnT = K.sb(st, "cx_hnT", [128, 8, 512], BF16)
        qTs = K.sb(st, "cx_qTs", [128, 8, 512], BF16)
        pT = [K.sb(st, "cx_pT%d" % i, [128, 512], BF16) for i in range(2)]
        rden = K.sb(st, "cx_rden", [128, 512], F32)
        oT = K.sb(st, "cx_oT", [128, 8, 512], BF16)
        for tb in range(T // 512):
            t0 = tb * 512
            for tt in range(4):
                r0 = s * T + t0 + tt * 128
                K.dma("sp" if tt % 2 == 0 else "act", ht[:, tt, :], SC["H1"][r0:r0 + 128, :], ["H1"], ["cx_ht%d" % tt])
                norm_T(K, "cx_", ht[:, tt, :], "cx_ht%d" % tt, xn, ss, junk, pst, (hnT, "cx_hnT"), tt * 128, identb, CONST["eps6"])
            for j in range(8):
                pj = j % 2
                for k in range(8):
                    K.mm(ps[pj][:], wcq[:, k, j * 128:(j + 1) * 128], hnT[:, k, :], ["cx_wcq", "cx_hnT"], ["cx_ps%d" % pj], start=(k == 0), stop=(k == 7))
                if pj == 0:
                    K.op("dve", "tensor_copy", ["cx_ps0"], ["cx_qTs"], out=qTs[:, j, :], in_=ps[0][:])
                else:
                    K.op("act", "activation", ["cx_ps1"], ["cx_qTs"], out=qTs[:, j, :], in_=ps[1][:], func=AF.Copy)
            for h in range(4):
                for mt in range(2):
                    for dc in range(2):
                        K.mm(ps[2 + mt][:], kTs[:, 2 * h + dc, mt * 128:(mt + 1) * 128], qTs[:, 2 * h + dc, :], ["cx_kTs", "cx_qTs"],
                             ["cx_ps%d" % (2 + mt)], start=(dc == 0), stop=(dc == 1))
                    K.op("act", "activation", ["cx_ps%d" % (2 + mt)], ["cx_pT%d" % mt], out=pT[mt][:], in_=ps[2 + mt][:], func=AF.Exp, scale=1.0 / 16)
                for mt in range(2):
                    K.mm(ps[4][:], ones[:], pT[mt][:], ["cx_ones", "cx_pT%d" % mt], ["cx_ps4"], start=(mt == 0), stop=(mt == 1))
                K.op("dve", "reciprocal", ["cx_ps4"], ["cx_rden"], out=rden[:], in_=ps[4][:])
                for dc in range(2):
                    for mt in range(2):
                        K.mm(ps[5][:], vS[:, mt, h * 256 + dc * 128:h * 256 + (dc + 1) * 128], pT[mt][:], ["cx_vS", "cx_pT%d" % mt], ["cx_ps5"],
                             start=(mt == 0), stop=(mt == 1))
                    K.op("dve", "tensor_tensor", ["cx_ps5", "cx_rden"], ["cx_oT"], out=oT[:, 2 * h + dc, :], in0=ps[5][:], in1=rden[:], op=ALU.mult)
            for tt in range(4):
                r0 = s * T + t0 + tt * 128
                for half in range(2):
                    pj = half
                    for k in range(8):
                        K.mm(ps[pj][:], oT[:, k, tt * 128:(tt + 1) * 128], wco[:, k, half * 512:(half + 1) * 512], ["cx_oT", "cx_wco"],
                             ["cx_ps%d" % pj], start=(k == 0), stop=(k == 7))
                    K.op("dve", "tensor_tensor", ["cx_ps%d" % pj, "cx_ht%d" % tt], ["cx_ht%d" % tt], out=ht[:, tt, half * 512:(half + 1) * 512],
                         in0=ps[pj][:], in1=ht[:, tt, half * 512:(half + 1) * 512], op=ALU.add)
                K.dma("sp", SC["H1"][r0:r0 + 128, :], ht[:, tt, :], ["cx_ht%d" % tt], ["H1"])


def phase_moe(K, s, T, Wd, SC, CONST, OUT):
    identb = CONST["identb"]
    HT = min(T, 1024)
    NTL = HT // 128
    with ExitStack() as st:
        nrf = colvec(K, st, "mo_nrf", Wd["norm_ffn"])
        wrf = K.sb(st, "mo_wrf", [128, 8, 36], F32)
        K.dma("sp", wrf[:, :, 0:4], Wd["w_router_g"][0].rearrange("(c p) n -> p c n", p=128), [], ["mo_wrf"])
        K.dma("sp", wrf[:, :, 4:36], Wd["w_router_e"][0].rearrange("(c p) n -> p c n", p=128), [], ["mo_wrf"])
        wr = K.sb(st, "mo_wr", [128, 8, 36], BF16)
        K.op("dve", "tensor_tensor", ["mo_wrf", "mo_nrf"], ["mo_wr"], out=wr[:], in0=wrf[:], in1=nrf[:].unsqueeze(2).to_broadcast([128, 8, 36]), op=ALU.mult)
        brb = K.sb(st, "mo_brb", [128, 36], F32)
        K.dma("sp", brb[:, 0:4], Wd["b_router_g"].partition_broadcast(128), [], ["mo_brb"])
        K.dma("sp", brb[:, 4:36], Wd["b_router_e"].partition_broadcast(128), [], ["mo_brb"])
        nfb = K.sb(st, "mo_nfb", [128, D], F32)
        K.dma("sp", nfb[:], Wd["norm_final"].partition_broadcast(128), [], ["mo_nfb"])
        ps = [K.ps(st, "mo_ps%d" % i, [128, 512], F32) for i in range(7)]
        pst = K.ps(st, "mo_pst", [128, 8, 128], BF16)
        ht = K.sb(st, "mo_ht", [128, D], F32)
        xn = K.sb(st, "mo_xn", [128, D], BF16)
        ss = K.sb(st, "mo_ss", [128, 1], F32)
        junk = K.sb(st, "mo_junk", [128, D], F32)
        xT = K.sb(st, "mo_xT", [128, 8, HT], BF16)
        G = K.sb(st, "mo_G", [128, NTL, 32], F32)
        acc = K.sb(st, "mo_acc", [128, NTL, D], F32)
        lg = K.sb(st, "mo_lg", [128, 36], F32)
        cl = K.sb(st, "mo_cl", [128, 12], F32)
        lem = K.sb(st, "mo_lem", [128, 4, 8], F32)
        m8 = K.sb(st, "mo_m8", [128, 8], F32)
        sel = K.sb(st, "mo_sel", [128, 32], F32)
        ex = K.sb(st, "mo_ex", [128, 32], F32)
        stg = [K.sb(st, "mo_stg%d" % i, [128, 4096], F32) for i in range(2)]
        wg = [K.sb(st, "mo_wg%d" % i, [128, 8, 512], BF16) for i in range(2)]
        wu = [K.sb(st, "mo_wu%d" % i, [128, 8, 512], BF16) for i in range(2)]
        wd = [K.sb(st, "mo_wd%d" % i, [128, 4, 1024], BF16) for i in range(2)]
        sgts = [K.sb(st, "mo_sgt%d" % i, [128, 512], F32) for i in range(2)]
        hT = K.sb(st, "mo_hT", [128, 4, 512], BF16)
        tmp = [K.sb(st, "mo_tmp%d" % i, [128, 512], F32) for i in range(3)]
        for hf in range(T // HT):
            base = s * T + hf * HT
            for tl in range(NTL):
                r0 = base + tl * 128
                K.dma("sp", ht[:], SC["H1"][r0:r0 + 128, :], ["H1"], ["mo_ht"])
                norm_T(K, "mo_", ht[:], "mo_ht", xn, ss, junk, pst, (xT, "mo_xT"), tl * 128, identb, CONST["eps6"])
                for k in range(8):
                    K.mm(ps[0][:, 0:36], xT[:, k, tl * 128:(tl + 1) * 128], wr[:, k, :], ["mo_xT", "mo_wr"], ["mo_ps0"], start=(k == 0), stop=(k == 7))
                K.op("dve", "tensor_tensor", ["mo_ps0", "mo_brb"], ["mo_lg"], out=lg[:], in0=ps[0][:, 0:36], in1=brb[:], op=ALU.add)
                K.op("dve", "tensor_reduce", ["mo_lg"], ["mo_cl"], out=cl[:, 0:1], in_=lg[:, 0:4], axis=AX.X, op=ALU.max)
                K.op("dve", "tensor_scalar", ["mo_cl"], ["mo_cl"], out=cl[:, 1:2], in0=cl[:, 0:1], scalar1=-1.0, scalar2=None, op0=ALU.mult)
                K.op("act", "activation", ["mo_lg", "mo_cl"], ["mo_ex", "mo_cl"], out=ex[:, 0:4], in_=lg[:, 0:4], func=AF.Exp, bias=cl[:, 1:2], accum_out=cl[:, 2:3])
                K.op("dve", "reciprocal", ["mo_cl"], ["mo_cl"], out=cl[:, 3:4], in_=cl[:, 2:3])
                K.op("dve", "tensor_scalar", ["mo_lg", "mo_cl"], ["mo_sel"], out=sel[:, 0:4], in0=lg[:, 0:4], scalar1=cl[:, 0:1], scalar2=None, op0=ALU.is_ge)
                K.op("dve", "tensor_scalar", ["mo_sel"], ["mo_sel"], out=sel[:, 0:4], in0=sel[:, 0:4], scalar1=-1.0, scalar2=1e30, op0=ALU.add, op1=ALU.mult)
                K.op("dve", "tensor_tensor", ["mo_lg", "mo_sel"], ["mo_lem"], out=lem[:], in0=lg[:, 4:36].rearrange("p (a b) -> p a b", a=4),
                     in1=sel[:, 0:4].unsqueeze(2).to_broadcast([128, 4, 8]), op=ALU.add)
                lemf = lem[:].rearrange("p a b -> p (a b)")
                K.op("dve", "max", ["mo_lem"], ["mo_m8"], out=m8[:], in_=lemf)
                K.op("dve", "tensor_scalar", ["mo_lem", "mo_m8"], ["mo_sel"], out=sel[:], in0=lemf, scalar1=m8[:, 1:2], scalar2=None, op0=ALU.is_ge)
                K.op("dve", "tensor_scalar", ["mo_m8"], ["mo_cl"], out=cl[:, 4:5], in0=m8[:, 0:1], scalar1=-1.0, scalar2=None, op0=ALU.mult)
                K.op("act", "activation", ["mo_lem", "mo_cl"], ["mo_ex"], out=ex[:], in_=lemf, func=AF.Exp, bias=cl[:, 4:5])
                K.op("dve", "tensor_tensor", ["mo_ex", "mo_sel"], ["mo_ex"], out=ex[:], in0=ex[:], in1=sel[:], op=ALU.mult)
                K.op("dve", "tensor_reduce", ["mo_ex"], ["mo_cl"], out=cl[:, 5:6], in_=ex[:], axis=AX.X, op=ALU.add)
                K.op("dve", "reciprocal", ["mo_cl"], ["mo_cl"], out=cl[:, 6:7], in_=cl[:, 5:6])
                K.op("dve", "tensor_tensor", ["mo_cl"], ["mo_cl"], out=cl[:, 7:8], in0=cl[:, 6:7], in1=cl[:, 3:4], op=ALU.mult)
                K.op("dve", "tensor_scalar", ["mo_ex", "mo_cl"], ["mo_G"], out=G[:, tl, :], in0=ex[:], scalar1=cl[:, 7:8], scalar2=None, op0=ALU.mult)
            for e in range(32):
                i = e % 2
                nfb8 = nrf[:].unsqueeze(2).to_broadcast([128, 8, 512])
                K.dma("sp", stg[0][:].rearrange("p (c n) -> p c n", c=8), Wd["w_e_gate"][0, e].rearrange("(c p) n -> p c n", p=128), [], ["mo_stg0"])
                K.op("pool", "tensor_tensor", ["mo_stg0", "mo_nrf"], ["mo_wg%d" % i], out=wg[i][:], in0=stg[0][:].rearrange("p (c n) -> p c n", c=8), in1=nfb8, op=ALU.mult)
                K.dma("act", stg[1][:].rearrange("p (c n) -> p c n", c=8), Wd["w_e_up"][0, e].rearrange("(c p) n -> p c n", p=128), [], ["mo_stg1"])
                K.op("pool", "tensor_tensor", ["mo_stg1", "mo_nrf"], ["mo_wu%d" % i], out=wu[i][:], in0=stg[1][:].rearrange("p (c n) -> p c n", c=8), in1=nfb8, op=ALU.mult)
                K.dma("sp", stg[0][:].rearrange("p (c n) -> p c n", c=4), Wd["w_e_down"][0, e].rearrange("(c p) n -> p c n", p=128), [], ["mo_stg0"])
                K.op("pool", "tensor_copy", ["mo_stg0"], ["mo_wd%d" % i], out=wd[i][:], in_=stg[0][:].rearrange("p (c n) -> p c n", c=4))
                for bk in range(HT // 512):
                    bs = slice(bk * 512, (bk + 1) * 512)
                    for fc in range(4):
                        fs = slice(fc * 128, (fc + 1) * 128)
                        pg, pu = (0, 1) if fc % 2 == 0 else (4, 5)
                        sg_ = sgts[fc % 2]
                        sgn = "mo_sgt%d" % (fc % 2)
                        for k in range(8):
                            K.mm(ps[pg][:], wg[i][:, k, fs], xT[:, k, bs], ["mo_wg%d" % i, "mo_xT"], ["mo_ps%d" % pg], start=(k == 0), stop=(k == 7))
                        for k in range(8):
                            K.mm(ps[pu][:], wu[i][:, k, fs], xT[:, k, bs], ["mo_wu%d" % i, "mo_xT"], ["mo_ps%d" % pu], start=(k == 0), stop=(k == 7))
                        K.op("act", "activation", ["mo_ps%d" % pg], [sgn], out=sg_[:], in_=ps[pg][:], func=AF.Silu)
                        K.op("dve", "tensor_tensor", ["mo_ps%d" % pu, sgn], ["mo_hT%d" % fc], out=hT[:, fc, :], in0=ps[pu][:], in1=sg_[:], op=ALU.mult)
                    for tt in range(4):
                        tl = bk * 4 + tt
                        for half in range(2):
                            pj = (2, 3, 6)[(2 * tt + half) % 3]
                            for fc in range(4):
                                K.mm(ps[pj][:], hT[:, fc, tt * 128:(tt + 1) * 128], wd[i][:, fc, half * 512:(half + 1) * 512], ["mo_hT%d" % fc, "mo_wd%d" % i],
                                     ["mo_ps%d" % pj], start=(fc == 0), stop=(fc == 3))
                            hs = slice(half * 512, (half + 1) * 512)
                            if e == 0:
                                K.op("act", "activation", ["mo_ps%d" % pj, "mo_G"], ["mo_acc%d_%d" % (tl, half)], out=acc[:, tl, hs], in_=ps[pj][:], func=AF.Copy, scale=G[:, tl, e:e + 1])
                            else:
                                ti = (2 * tt + half) % 3
                                accn = "mo_acc%d_%d" % (tl, half)
                                K.op("act", "activation", ["mo_ps%d" % pj, "mo_G"], ["mo_tmp%d" % ti], out=tmp[ti][:], in_=ps[pj][:], func=AF.Copy, scale=G[:, tl, e:e + 1])
                                K.op("pool" if half == 0 else "dve", "tensor_tensor", ["mo_tmp%d" % ti, accn], [accn], out=acc[:, tl, hs], in0=acc[:, tl, hs], in1=tmp[ti][:], op=ALU.add)
            for tl in range(NTL):
                r0 = base + tl * 128
                K.dma("sp", ht[:], SC["H1"][r0:r0 + 128, :], ["H1"], ["mo_ht"])
                K.op("dve", "tensor_tensor", ["mo_ht", "mo_acc%d_0" % tl, "mo_acc%d_1" % tl], ["mo_ht"], out=ht[:], in0=ht[:], in1=acc[:, tl, :], op=ALU.add)
                K.op("act", "activation", ["mo_ht"], ["mo_junk", "mo_ss"], out=junk[:], in_=ht[:], func=AF.Square, accum_out=ss[:])
                K.op("act", "activation", ["mo_ss", "eps6"], ["mo_ss"], out=ss[:], in_=ss[:], func=AF.Sqrt, scale=1.0 / D, bias=CONST["eps6"][:])
                K.op("dve", "reciprocal", ["mo_ss"], ["mo_ss"], out=ss[:], in_=ss[:])
                K.op("dve", "scalar_tensor_tensor", ["mo_ht", "mo_ss", "mo_nfb"], ["mo_junk"], out=junk[:], in0=ht[:], scalar=ss[:], in1=nfb[:], op0=ALU.mult, op1=ALU.mult)
                K.dma("sp", OUT[r0:r0 + 128, :], junk[:], ["mo_junk"], ["OUT"])


I32 = mybir.dt.int32


def prepack_gen(K, st, Wd, SC):
    WGU, WDS = SC["WGU"], SC["WDS"]
    nrf = colvec(K, st, "pk_nrf", Wd["norm_ffn"])
    sg = K.sb(st, "pk_sg", [128, 8, 512], F32)
    su = K.sb(st, "pk_su", [128, 8, 512], F32)
    sd = K.sb(st, "pk_sd", [128, 4, 1024], F32)
    og = K.sb(st, "pk_og", [128, 8, 1024], BF16)
    od = K.sb(st, "pk_od", [128, 4, 1024], BF16)
    nf8 = nrf[:].unsqueeze(2).to_broadcast([128, 8, 512])
    def loads(e):
        K.dma("pool", sg[:], Wd["w_e_gate"][0, e].rearrange("(c p) n -> p c n", p=128), [], ["pk_sg"])
        K.dma("pool", su[:], Wd["w_e_up"][0, e].rearrange("(c p) n -> p c n", p=128), [], ["pk_su"])
        K.dma("pool", sd[:], Wd["w_e_down"][0, e].rearrange("(c p) n -> p c n", p=128), [], ["pk_sd"])

    loads(0)
    yield
    for e in range(32):
        K.op("dve", "tensor_tensor", ["pk_sg", "pk_nrf"], ["pk_og0"], out=og[:, :, 0:512], in0=sg[:], in1=nf8, op=ALU.mult)
        for c in range(8):
            K.op("act", "activation", ["pk_su", "pk_nrf"], ["pk_og1"], out=og[:, c, 512:1024], in_=su[:, c, :], func=AF.Copy, scale=nrf[:, c:c + 1])
        K.op("act", "activation", ["pk_sd"], ["pk_od"], out=od[:], in_=sd[:], func=AF.Copy)
        K.dma("pool", WGU[e * 1024:(e + 1) * 1024, :].rearrange("(c p) n -> p c n", p=128), og[:], ["pk_og0", "pk_og1"], ["WGU"])
        K.dma("pool", WDS[e * 512:(e + 1) * 512, :].rearrange("(c p) n -> p c n", p=128), od[:], ["pk_od"], ["WDS"])
        if e + 1 < 32:
            loads(e + 1)
        yield


def phase_moe_sparse(K, s, T, Wd, SC, CONST, OUT):
    nc = K.nc
    S = K.S
    identb = CONST["identb"]
    NTL = T // 128
    SB = 256
    NBLK = (2 * T) // SB + 32
    XS, YS = SC["XS"], SC["YS"]
    WG = Wd["w_e_gate"].rearrange("o e d f -> (o e d) f")
    WU = Wd["w_e_up"].rearrange("o e d f -> (o e d) f")
    WDN = Wd["w_e_down"].rearrange("o e f d -> (o e f) d")
    base = s * T
    with ExitStack() as st0:
        nrf = colvec(K, st0, "ms_nrf", Wd["norm_ffn"])
        GG = K.sb(st0, "ms_GG", [128, NTL, 2], F32)
        DST = K.sb(st0, "ms_DST", [128, NTL, 2], I32)
        IDXG = K.sb(st0, "ms_IDXG", [128, NBLK, 8], I32)
        IDXD = K.sb(st0, "ms_IDXD", [128, NBLK, 4], I32)
        with ExitStack() as st:
            wrf = K.sb(st, "ms_wrf", [128, 8, 36], F32)
            K.dma("sp", wrf[:, :, 0:4], Wd["w_router_g"][0].rearrange("(c p) n -> p c n", p=128), [], ["ms_wrf"])
            K.dma("sp", wrf[:, :, 4:36], Wd["w_router_e"][0].rearrange("(c p) n -> p c n", p=128), [], ["ms_wrf"])
            wr = K.sb(st, "ms_wr", [128, 8, 36], BF16)
            K.op("dve", "tensor_tensor", ["ms_wrf", "ms_nrf"], ["ms_wr"], out=wr[:], in0=wrf[:], in1=nrf[:].unsqueeze(2).to_broadcast([128, 8, 36]), op=ALU.mult)
            brb = K.sb(st, "ms_brb", [128, 36], F32)
            K.dma("sp", brb[:, 0:4], Wd["b_router_g"].partition_broadcast(128), [], ["ms_brb"])
            K.dma("sp", brb[:, 4:36], Wd["b_router_e"].partition_broadcast(128), [], ["ms_brb"])
            ps = [K.ps(st, "ms_ps%d" % i, [128, 512], F32) for i in range(2)]
            pst = K.ps(st, "ms_pst", [128, 8, 128], BF16)
            ht = K.sb(st, "ms_ht", [128, D], F32)
            ss = K.sb(st, "ms_ss", [128, 1], F32)
            junk = K.sb(st, "ms_junk", [128, D], F32)
            XN = K.sb(st, "ms_XN", [128, NTL, D], BF16)
            xT = K.sb(st, "ms_xT", [128, 8, 128], BF16)
            SEL = K.sb(st, "ms_SEL", [128, NTL, 2, 32], F32)
            RNK = K.sb(st, "ms_RNK", [128, NTL, 2], F32)
            carry = K.sb(st, "ms_carry", [128, 32], F32)
            K.op("dve", "memset", [], ["ms_carry"], ap=carry[:], constant=0.0)
            lg = K.sb(st, "ms_lg", [128, 36], F32)
            cl = K.sb(st, "ms_cl", [128, 12], F32)
            lem = K.sb(st, "ms_lem", [128, 32], F32)
            m8 = K.sb(st, "ms_m8", [128, 8], F32)
            s12 = K.sb(st, "ms_s12", [128, 32], F32)
            ex = K.sb(st, "ms_ex", [128, 32], F32)
            t32 = K.sb(st, "ms_t32", [128, 32], F32)
            utri, ones128, bstart, iotap = CONST["utri"], CONST["ones128"], CONST["bstart"], CONST["iotap"]
            GB = 8
            LG = K.sb(st, "ms_LG", [128, GB, 36], F32)
            LM = K.sb(st, "ms_LM", [128, GB, 32], F32)
            L2 = K.sb(st, "ms_L2", [128, GB, 32], F32)
            EX = K.sb(st, "ms_EX", [128, GB, 32], F32)
            S12 = K.sb(st, "ms_S12", [128, GB, 32], F32)
            RKt = K.sb(st, "ms_RKt", [128, GB, 32], F32)
            T4 = K.sb(st, "ms_T4", [128, GB, 4], F32)
            E4 = K.sb(st, "ms_E4", [128, GB, 4], F32)
            CG = K.sb(st, "ms_CG", [128, 8, GB], F32)
            hts = [ht, K.sb(st, "ms_ht1", [128, D], F32)]

            def b3(colv, n):
                return colv.unsqueeze(2).to_broadcast([128, GB, n])

            for g0 in range(0, NTL, GB):
                for gi in range(GB):
                    tl = g0 + gi
                    r0 = base + tl * 128
                    hh_ = hts[tl % 2]
                    hn = "ms_ht" if tl % 2 == 0 else "ms_ht1"
                    K.dma("sp" if tl % 2 == 0 else "act", hh_[:], SC["H1"][r0:r0 + 128, :], ["H1"], [hn])
                    K.op("act", "activation", [hn], ["ms_junk", "ms_ss"], out=junk[:], in_=hh_[:], func=AF.Square, accum_out=ss[:])
                    K.op("act", "activation", ["ms_ss", "eps6"], ["ms_ss"], out=ss[:], in_=ss[:], func=AF.Sqrt, scale=1.0 / D, bias=CONST["eps6"][:])
                    K.op("dve", "reciprocal", ["ms_ss"], ["ms_ss"], out=ss[:], in_=ss[:])
                    K.op("dve", "tensor_scalar", [hn, "ms_ss"], ["ms_XN%d" % tl], out=XN[:, tl, :], in0=hh_[:], scalar1=ss[:], scalar2=None, op0=ALU.mult)
                    for c in range(8):
                        K.tr(pst[:, c, :], XN[:, tl, c * 128:(c + 1) * 128], identb[:], ["ms_XN%d" % tl, "identb"], ["ms_pst"])
                    K.op("act", "activation", ["ms_pst"], ["ms_xT"], out=xT[:], in_=pst[:], func=AF.Copy)
                    for k in range(8):
                        K.mm(ps[0][:, 0:36], xT[:, k, :], wr[:, k, :], ["ms_xT", "ms_wr"], ["ms_ps0"], start=(k == 0), stop=(k == 7))
                    K.op("dve", "tensor_tensor", ["ms_ps0", "ms_brb"], ["ms_LG"], out=LG[:, gi, :], in0=ps[0][:, 0:36], in1=brb[:], op=ALU.add)
                K.op("dve", "tensor_reduce", ["ms_LG"], ["ms_CG"], out=CG[:, 0, :], in_=LG[:, :, 0:4], axis=AX.X, op=ALU.max)
                K.op("dve", "tensor_tensor", ["ms_LG", "ms_CG"], ["ms_T4"], out=T4[:], in0=LG[:, :, 0:4], in1=b3(CG[:, 0, :], 4), op=ALU.subtract)
                K.op("act", "activation", ["ms_T4"], ["ms_E4"], out=E4[:], in_=T4[:], func=AF.Exp)
                K.op("dve", "tensor_reduce", ["ms_E4"], ["ms_CG"], out=CG[:, 1, :], in_=E4[:], axis=AX.X, op=ALU.add)
                K.op("dve", "reciprocal", ["ms_CG"], ["ms_CG"], out=CG[:, 2, :], in_=CG[:, 1, :])
                K.op("dve", "tensor_scalar", ["ms_T4"], ["ms_T4"], out=T4[:], in0=T4[:], scalar1=0.0, scalar2=None, op0=ALU.is_ge)
                K.op("dve", "tensor_scalar", ["ms_T4"], ["ms_T4"], out=T4[:], in0=T4[:], scalar1=-1.0, scalar2=1e30, op0=ALU.add, op1=ALU.mult)
                K.op("dve", "tensor_tensor", ["ms_LG", "ms_T4"], ["ms_LM"], out=LM[:].rearrange("p g (a b) -> p g a b", a=4),
                     in0=LG[:, :, 4:36].rearrange("p g (a b) -> p g a b", a=4), in1=T4[:].unsqueeze(3).to_broadcast([128, GB, 4, 8]), op=ALU.add)
                K.op("dve", "tensor_reduce", ["ms_LM"], ["ms_CG"], out=CG[:, 3, :], in_=LM[:], axis=AX.X, op=ALU.max)
                sel1 = SEL[:, g0:g0 + GB, 0, :]
                sel2 = SEL[:, g0:g0 + GB, 1, :]
                K.op("dve", "tensor_tensor", ["ms_LM", "ms_CG"], ["ms_SEL"], out=sel1, in0=LM[:], in1=b3(CG[:, 3, :], 32), op=ALU.is_ge)
                K.op("dve", "scalar_tensor_tensor", ["ms_SEL", "ms_LM"], ["ms_L2"], out=L2[:], in0=sel1, scalar=-1e30, in1=LM[:], op0=ALU.mult, op1=ALU.add)
                K.op("dve", "tensor_reduce", ["ms_L2"], ["ms_CG"], out=CG[:, 4, :], in_=L2[:], axis=AX.X, op=ALU.max)
                K.op("dve", "tensor_tensor", ["ms_LM", "ms_CG"], ["ms_S12"], out=S12[:], in0=LM[:], in1=b3(CG[:, 4, :], 32), op=ALU.is_ge)
                K.op("dve", "tensor_tensor", ["ms_S12", "ms_SEL"], ["ms_SEL"], out=sel2, in0=S12[:], in1=sel1, op=ALU.subtract)
                K.op("dve", "tensor_tensor", ["ms_LM", "ms_CG"], ["ms_L2"], out=L2[:], in0=LM[:], in1=b3(CG[:, 3, :], 32), op=ALU.subtract)
                K.op("dve", "tensor_scalar", ["ms_L2"], ["ms_L2"], out=L2[:], in0=L2[:], scalar1=-80.0, scalar2=None, op0=ALU.max)
                K.op("act", "activation", ["ms_L2"], ["ms_EX"], out=EX[:], in_=L2[:], func=AF.Exp)
                K.op("dve", "tensor_tensor", ["ms_EX", "ms_S12"], ["ms_EX"], out=EX[:], in0=EX[:], in1=S12[:], op=ALU.mult)
                K.op("dve", "tensor_reduce", ["ms_EX"], ["ms_CG"], out=CG[:, 5, :], in_=EX[:], axis=AX.X, op=ALU.add)
                K.op("dve", "reciprocal", ["ms_CG"], ["ms_CG"], out=CG[:, 6, :], in_=CG[:, 5, :])
                K.op("dve", "tensor_tensor", ["ms_CG"], ["ms_CG"], out=CG[:, 6, :], in0=CG[:, 6, :], in1=CG[:, 2, :], op=ALU.mult)
                for kk_ in range(2):
                    K.op("dve", "tensor_tensor", ["ms_EX", "ms_SEL"], ["ms_L2"], out=L2[:], in0=EX[:], in1=SEL[:, g0:g0 + GB, kk_, :], op=ALU.mult)
                    K.op("dve", "tensor_reduce", ["ms_L2"], ["ms_CG"], out=CG[:, 7, :], in_=L2[:], axis=AX.X, op=ALU.add)
                    K.op("dve", "tensor_tensor", ["ms_CG"], ["ms_GG"], out=GG[:, g0:g0 + GB, kk_], in0=CG[:, 7, :], in1=CG[:, 6, :], op=ALU.mult)
                for gi in range(GB):
                    K.mm(ps[1][:, gi * 64:gi * 64 + 32], utri[:], S12[:, gi, :], ["utri", "ms_S12"], ["ms_ps1"])
                    K.mm(ps[1][:, gi * 64 + 32:gi * 64 + 64], ones128[:], S12[:, gi, :], ["ones128", "ms_S12"], ["ms_ps1"])
                for gi in range(GB):
                    K.op("dve", "tensor_tensor", ["ms_ps1", "ms_carry"], ["ms_RKt"], out=RKt[:, gi, :], in0=ps[1][:, gi * 64:gi * 64 + 32], in1=carry[:], op=ALU.add)
                    K.op("dve", "tensor_tensor", ["ms_ps1", "ms_carry"], ["ms_carry"], out=carry[:], in0=ps[1][:, gi * 64 + 32:gi * 64 + 64], in1=carry[:], op=ALU.add)
                for kk_ in range(2):
                    K.op("dve", "tensor_tensor", ["ms_RKt", "ms_SEL"], ["ms_L2"], out=L2[:], in0=RKt[:], in1=SEL[:, g0:g0 + GB, kk_, :], op=ALU.mult)
                    K.op("dve", "tensor_reduce", ["ms_L2"], ["ms_RNK"], out=RNK[:, g0:g0 + GB, kk_], in_=L2[:], axis=AX.X, op=ALU.add)
            ci = K.sb(st, "ms_ci", [128, 32], I32)
            pad = K.sb(st, "ms_pad", [128, 32], F32)
            pend = K.sb(st, "ms_pend", [128, 32], F32)
            pstart = K.sb(st, "ms_pstart", [128, 32], F32)
            ones32 = K.sb(st, "ms_ones32", [128, 32], F32)
            K.op("dve", "memset", [], ["ms_ones32"], ap=ones32[:], constant=1.0)
            K.op("dve", "tensor_scalar", ["ms_carry"], ["ms_ci"], out=ci[:], in0=carry[:], scalar1=float(SB - 1), scalar2=None, op0=ALU.add)
            K.op("dve", "tensor_scalar", ["ms_ci"], ["ms_ci"], out=ci[:], in0=ci[:], scalar1=8, scalar2=None, op0=ALU.arith_shift_right)
            K.op("dve", "tensor_scalar", ["ms_ci"], ["ms_ci"], out=ci[:], in0=ci[:], scalar1=8, scalar2=None, op0=ALU.logical_shift_left)
            K.op("dve", "tensor_copy", ["ms_ci"], ["ms_pad"], out=pad[:], in_=ci[:])
            K.op("dve", "tensor_tensor_scan", ["ms_pad", "ms_ones32"], ["ms_pend"], out=pend[:], data0=ones32[:], data1=pad[:], initial=0.0, op0=ALU.mult, op1=ALU.add)
            K.op("dve", "tensor_tensor", ["ms_pend", "ms_pad"], ["ms_pstart"], out=pstart[:], in0=pend[:], in1=pad[:], op=ALU.subtract)
            bst = K.sb(st, "ms_bst", [128, NBLK], F32)
            K.op("dve", "tensor_scalar", ["bstart"], ["ms_bst"], out=bst[:], in0=bstart[:, 0:NBLK], scalar1=float(SB // 128), scalar2=None, op0=ALU.mult)
            be = K.sb(st, "ms_be", [128, NBLK], F32)
            K.op("dve", "tensor_scalar", ["ms_bst", "ms_pend"], ["ms_be"], out=be[:], in0=bst[:], scalar1=pend[:, 0:1], scalar2=None, op0=ALU.is_ge)
            for e in range(1, 32):
                K.op("dve", "scalar_tensor_tensor", ["ms_bst", "ms_pend", "ms_be"], ["ms_be"], out=be[:], in0=bst[:], scalar=pend[:, e:e + 1], in1=be[:],
                     op0=ALU.is_ge, op1=ALU.add)
            K.op("dve", "tensor_scalar", ["ms_be"], ["ms_be"], out=be[:], in0=be[:], scalar1=31.0, scalar2=None, op0=ALU.min)
            bg = K.sb(st, "ms_bg", [128, NBLK], F32)
            bd = K.sb(st, "ms_bd", [128, NBLK], F32)
            K.op("dve", "tensor_scalar", ["ms_be", "iotap"], ["ms_bg"], out=bg[:], in0=be[:], scalar1=1024.0, scalar2=iotap[:, 0:1], op0=ALU.mult, op1=ALU.add)
            K.op("dve", "tensor_scalar", ["ms_be", "iotap"], ["ms_bd"], out=bd[:], in0=be[:], scalar1=512.0, scalar2=iotap[:, 0:1], op0=ALU.mult, op1=ALU.add)
            for c in range(8):
                K.op("dve", "tensor_scalar", ["ms_bg"], ["ms_IDXG"], out=IDXG[:, :, c], in0=bg[:], scalar1=float(c * 128), scalar2=None, op0=ALU.add)
            for c in range(4):
                K.op("dve", "tensor_scalar", ["ms_bd"], ["ms_IDXD"], out=IDXD[:, :, c], in0=bd[:], scalar1=float(c * 128), scalar2=None, op0=ALU.add)
            zt = K.sb(st, "ms_zt", [128, 4, D], BF16)
            K.op("pool", "memset", [], ["ms_zt"], ap=zt[:], constant=0.0)
            XSv = XS.rearrange("(b p) d -> p b d", p=128)
            for b0 in range(0, NBLK * SB // 128, 4):
                K.dma("sp" if (b0 // 4) % 2 == 0 else "act", XSv[:, b0:b0 + 4, :], zt[:], ["ms_zt"], ["XS"])
            TB3 = K.sb(st, "ms_TB3", [128, NTL, 32], F32)
            DF = K.sb(st, "ms_DF", [128, NTL], F32)
            for kk_ in range(2):
                K.op("dve", "tensor_tensor", ["ms_pstart", "ms_SEL"], ["ms_TB3"], out=TB3[:], in0=SEL[:, :, kk_, :],
                     in1=pstart[:].unsqueeze(1).to_broadcast([128, NTL, 32]), op=ALU.mult)
                K.op("dve", "tensor_reduce", ["ms_TB3"], ["ms_DF"], out=DF[:], in_=TB3[:], axis=AX.X, op=ALU.add)
                K.op("dve", "tensor_tensor", ["ms_DF", "ms_RNK"], ["ms_DST"], out=DST[:, :, kk_], in0=DF[:], in1=RNK[:, :, kk_], op=ALU.add)
            for tl in range(NTL):
                for kk_ in range(2):
                    S.dma("pool", None, None, K._bl(["ms_DST", "ms_XN%d" % tl, "XS"]), K._bl(["XSs_%d_%d" % (tl, kk_)]),
                          fn=lambda e, tl=tl, kk_=kk_: e.indirect_dma_start(out=XS, out_offset=bass.IndirectOffsetOnAxis(ap=DST[:, tl, kk_:kk_ + 1], axis=0),
                                                                        in_=XN[:, tl, :], in_offset=None))
        S.barrier()
        with ExitStack() as st:
            ps = [K.ps(st, "mb_ps%d" % i, [128, 512], F32) for i in range(6)]
            pst = K.ps(st, "mb_pst", [128, 8, 128], BF16)
            wgu = [K.sb(st, "mb_wgu%d" % i, [128, 8, 1024], BF16) for i in range(2)]
            wd = [K.sb(st, "mb_wd%d" % i, [128, 4, 1024], BF16) for i in range(2)]
            xb = [K.sb(st, "mb_xb%d" % i, [128, D], BF16) for i in range(2)]
            xT = K.sb(st, "mb_xT", [128, 8, 128], BF16)
            sgt = K.sb(st, "mb_sgt", [128, 512], F32)
            hb = K.sb(st, "mb_hb", [128, 512], BF16)
            hT = K.sb(st, "mb_hT", [128, 4, 128], BF16)
            ysb = [K.sb(st, "mb_ysb%d" % i, [128, D], F32) for i in range(2)]
            WGU, WDS = SC["WGU"], SC["WDS"]
            for b in range(NBLK):
                i = b % 2
                for c in range(8):
                    S.dma("pool", None, None, K._bl(["ms_IDXG"]), K._bl(["mb_wgu%d_%d" % (i, c)]),
                          fn=lambda e, b=b, c=c, i=i: e.indirect_dma_start(out=wgu[i][:, c, :], out_offset=None, in_=WGU,
                                                                         in_offset=bass.IndirectOffsetOnAxis(ap=IDXG[:, b, c:c + 1], axis=0)))
                for c in range(4):
                    S.dma("pool", None, None, K._bl(["ms_IDXD"]), K._bl(["mb_wd%d_%d" % (i, c)]),
                          fn=lambda e, b=b, c=c, i=i: e.indirect_dma_start(out=wd[i][:, c, :], out_offset=None, in_=WDS,
                                                                         in_offset=bass.IndirectOffsetOnAxis(ap=IDXD[:, b, c:c + 1], axis=0)))
                for sub in range(SB // 128):
                    j = sub % 2
                    r0 = b * SB + sub * 128
                    K.dma("sp", xb[j][:], XS[r0:r0 + 128, :], ["XS"], ["mb_xb%d" % j])
                    for c in range(8):
                        K.tr(pst[:, c, :], xb[j][:, c * 128:(c + 1) * 128], identb[:], ["mb_xb%d" % j, "identb"], ["mb_pst"])
                    K.op("act", "activation", ["mb_pst"], ["mb_xT"], out=xT[:], in_=pst[:], func=AF.Copy)
                    for k in range(8):
                        K.mm(ps[0][:], xT[:, k, :], wgu[i][:, k, 0:512], ["mb_xT"] + ["mb_wgu%d_%d" % (i, c) for c in range(8)], ["mb_ps0"], start=(k == 0), stop=(k == 7))
                    for k in range(8):
                        K.mm(ps[1][:], xT[:, k, :], wgu[i][:, k, 512:1024], ["mb_xT"] + ["mb_wgu%d_%d" % (i, c) for c in range(8)], ["mb_ps1"], start=(k == 0), stop=(k == 7))
                    K.op("act", "activation", ["mb_ps0"], ["mb_sgt"], out=sgt[:], in_=ps[0][:], func=AF.Silu)
                    K.op("dve", "tensor_tensor", ["mb_ps1", "mb_sgt"], ["mb_hb"], out=hb[:], in0=ps[1][:], in1=sgt[:], op=ALU.mult)
                    for fc in range(4):
                        K.tr(pst[:, fc, :], hb[:, fc * 128:(fc + 1) * 128], identb[:], ["mb_hb", "identb"], ["mb_pst"])
                    K.op("dve", "tensor_copy", ["mb_pst"], ["mb_hT"], out=hT[:], in_=pst[:, 0:4, :])
                    for half in range(2):
                        pj = 2 + 2 * j + half
                        for fc in range(4):
                            K.mm(ps[pj][:], hT[:, fc, :], wd[i][:, fc, half * 512:(half + 1) * 512], ["mb_hT"] + ["mb_wd%d_%d" % (i, c) for c in range(4)], ["mb_ps%d" % pj], start=(fc == 0), stop=(fc == 3))
                        if half == 0:
                            K.op("act", "activation", ["mb_ps%d" % pj], ["mb_ysb%d" % j], out=ysb[j][:, 0:512], in_=ps[pj][:], func=AF.Copy)
                        else:
                            K.op("dve", "tensor_copy", ["mb_ps%d" % pj], ["mb_ysb%d" % j], out=ysb[j][:, 512:1024], in_=ps[pj][:])
                    K.dma("act", YS[r0:r0 + 128, :], ysb[j][:], ["mb_ysb%d" % j], ["YS"])
        S.barrier()
        with ExitStack() as st:
            nfb = K.sb(st, "mc_nfb", [128, D], F32)
            K.dma("sp", nfb[:], Wd["norm_final"].partition_broadcast(128), [], ["mc_nfb"])
            hts = [K.sb(st, "mc_ht%d" % i, [128, D], F32) for i in range(2)]
            y1 = [K.sb(st, "mc_y1%d" % i, [128, D], F32) for i in range(2)]
            y2 = [K.sb(st, "mc_y2%d" % i, [128, D], F32) for i in range(2)]
            ob = [K.sb(st, "mc_ob%d" % i, [128, D], F32) for i in range(2)]
            junk = K.sb(st, "mc_junk", [128, D], F32)
            sss = [K.sb(st, "mc_ss%d" % i, [128, 1], F32) for i in range(2)]
            for tl in range(NTL):
                i = tl % 2
                r0 = base + tl * 128
                K.dma("sp", hts[i][:], SC["H1"][r0:r0 + 128, :], ["H1"], ["mc_ht%d" % i])
                S.dma("pool", None, None, K._bl(["ms_DST", "YS"]), K._bl(["mc_y1%d" % i]),
                      fn=lambda e, tl=tl, i=i: e.indirect_dma_start(out=y1[i][:], out_offset=None, in_=YS, in_offset=bass.IndirectOffsetOnAxis(ap=DST[:, tl, 0:1], axis=0)))
                S.dma("pool", None, None, K._bl(["ms_DST", "YS"]), K._bl(["mc_y2%d" % i]),
                      fn=lambda e, tl=tl, i=i: e.indirect_dma_start(out=y2[i][:], out_offset=None, in_=YS, in_offset=bass.IndirectOffsetOnAxis(ap=DST[:, tl, 1:2], axis=0)))
                K.op("dve", "scalar_tensor_tensor", ["mc_y1%d" % i, "ms_GG", "mc_ht%d" % i], ["mc_ht%d" % i], out=hts[i][:], in0=y1[i][:], scalar=GG[:, tl, 0:1], in1=hts[i][:],
                     op0=ALU.mult, op1=ALU.add)
                K.op("dve", "scalar_tensor_tensor", ["mc_y2%d" % i, "ms_GG", "mc_ht%d" % i], ["mc_ht%d" % i], out=hts[i][:], in0=y2[i][:], scalar=GG[:, tl, 1:2], in1=hts[i][:],
                     op0=ALU.mult, op1=ALU.add)
                K.op("act", "activation", ["mc_ht%d" % i], ["mc_junk", "mc_ss%d" % i], out=junk[:], in_=hts[i][:], func=AF.Square, accum_out=sss[i][:])
                K.op("act", "activation", ["mc_ss%d" % i, "eps6"], ["mc_ss%d" % i], out=sss[i][:], in_=sss[i][:], func=AF.Sqrt, scale=1.0 / D, bias=CONST["eps6"][:])
                K.op("dve", "reciprocal", ["mc_ss%d" % i], ["mc_ss%d" % i], out=sss[i][:], in_=sss[i][:])
                K.op("dve", "scalar_tensor_tensor", ["mc_ht%d" % i, "mc_ss%d" % i, "mc_nfb"], ["mc_ob%d" % i], out=ob[i][:], in0=hts[i][:], scalar=sss[i][:], in1=nfb[:],
                     op0=ALU.mult, op1=ALU.mult)
                K.dma("act", OUT[r0:r0 + 128, :], ob[i][:], ["mc_ob%d" % i], ["OUT"])


def build(T, NSEQ, stop_after=99, debug=False):
    nc = bass.Bass("TRN2", target_bir_lowering=False)
    NTOK = NSEQ * T

    def din(name, shape, dt=F32):
        return nc.dram_tensor(name, list(shape), dt, kind="ExternalInput").ap()

    X = din("x", [NTOK, D])
    MEM = din("mem", [NSEQ * 256, D])
    Wd = {}
    for name, shape in WSHAPES.items():
        Wd[name] = din(name, shape)
    identb_d = din("c_identb", [128, 128], BF16)
    identf_d = din("c_identf", [128, 128], F32)
    OUT = nc.dram_tensor("out", [NTOK, D], F32, kind="ExternalOutput").ap()
    SC = {}
    SC["ZF"] = nc.dram_tensor("sc_zf", [NSEQ, R_TOT, T], F32, kind="Internal").ap() if not debug else \
        nc.dram_tensor("sc_zf", [NSEQ, R_TOT, T], F32, kind="ExternalOutput").ap()
    kindd = "ExternalOutput" if debug else "Internal"
    SC["CK"] = nc.dram_tensor("sc_ck", [NSEQ, T, 128], BF16, kind=kindd).ap()
    SC["CKT"] = nc.dram_tensor("sc_ckt", [NSEQ, 128, T], BF16, kind=kindd).ap()
    SC["H1"] = nc.dram_tensor("sc_h1", [NTOK, D], F32, kind=kindd).ap()
    NSLOT = ((2 * T) // 256 + 32) * 256
    SC["WGU"] = nc.dram_tensor("sc_wgu", [32 * 1024, 1024], BF16, kind="Internal").ap()
    SC["WDS"] = nc.dram_tensor("sc_wds", [32 * 512, 1024], BF16, kind="Internal").ap()
    SC["XS"] = nc.dram_tensor("sc_xs", [NSLOT, D], BF16, kind="Internal").ap()
    SC["YS"] = nc.dram_tensor("sc_ys", [NSLOT, D], F32, kind="Internal").ap()
    SC["YB"] = nc.dram_tensor("sc_yb", [NSEQ, 512, T], BF16, kind=kindd).ap()
    SC["YA"] = nc.dram_tensor("sc_ya", [NSEQ, 512, T], BF16, kind=kindd).ap()
    cdram = {}
    for nm, arr in consts().items():
        if nm not in ("c_identb", "c_identf"):
            cdram[nm] = din(nm, arr.shape, BF16 if arr.dtype == ml_dtypes.bfloat16 else F32)
    with ExitStack() as st:
        S = Sched(nc, st)
        K = Ctx(nc, S)
        CONST = {}
        CONST["identb"] = K.sb(st, "identb", [128, 128], BF16)
        CONST["identf"] = K.sb(st, "identf", [128, 128], F32)
        CONST["eps6"] = K.sb(st, "eps6", [128, 1], F32)
        K.dma("sp", CONST["identb"][:], identb_d, [], ["identb"])
        K.dma("sp", CONST["identf"][:], identf_d, [], ["identf"])
        K.op("dve", "memset", [], ["eps6"], ap=CONST["eps6"][:], constant=1e-6)
        for nm, ap in cdram.items():
            sh = list(ap.shape)
            CONST[nm[2:]] = K.sb(st, nm[2:], sh, ap.dtype)
            K.dma("sp", CONST[nm[2:]][:], ap, [], [nm[2:]])
        for s in range(NSEQ):
            phase1(K, s, T, X, Wd, SC, CONST)
            S.barrier()
            if stop_after >= 2 and not os.environ.get("SKIP_DSA"):
                ex_ = (lambda st_: prepack_gen(K, st_, Wd, SC)) if (s == 0 and stop_after >= 6 and not os.environ.get("MOE_DENSE")) else None
                phase_dsa(K, s, T, Wd, SC, CONST, extra=ex_)
                S.barrier()
            if stop_after >= 3:
                phase_rwkv(K, s, T, Wd, SC, CONST)
                S.barrier()
            if stop_after >= 4:
                phase_mix(K, s, T, X, Wd, SC, CONST)
                S.barrier()
            if stop_after >= 5:
                phase_cross(K, s, T, MEM, Wd, SC, CONST)
                S.barrier()
            if stop_after >= 6:
                if os.environ.get("MOE_DENSE"):
                    phase_moe(K, s, T, Wd, SC, CONST, OUT)
                else:
                    phase_moe_sparse(K, s, T, Wd, SC, CONST, OUT)
                S.barrier()
        S.finish(list(K.B.values()))
        print("ops", S.nops, "waits", S.nwaits)
        S.emit()
    return nc


WSHAPES = {
    "norm_mix": [1, 1024], "w_in": [1, 1024, 4804], "shift_mu": [1, 1792], "rw_w0": [1, 512],
    "rw_w2": [1, 64, 512], "rw_a0": [1, 512], "rw_a2": [1, 64, 512], "rw_g2": [1, 128, 512],
    "rw_k_k": [1, 512], "rw_k_a": [1, 512], "rw_r_k": [1, 8, 64], "rw_ln_w": [1, 512], "rw_ln_b": [1, 512],
    "kv_norm": [1, 128], "w_uk": [1, 128, 8, 64], "w_uv": [1, 128, 8, 64], "w_proj_a": [1, 512, 1024],
    "w_proj_b": [1, 512, 1024], "b_gate": [1, 2048], "w_out": [1, 1024, 1024], "norm_cross": [1, 1024],
    "norm_mem": [1, 1024], "w_cq": [1, 1024, 1024], "w_ckv": [1, 1024, 2048], "w_co": [1, 1024, 1024],
    "norm_ffn": [1, 1024], "w_router_g": [1, 1024, 4], "b_router_g": [1, 4], "w_router_e": [1, 1024, 32],
    "b_router_e": [1, 32], "w_e_gate": [1, 32, 1024, 512], "w_e_up": [1, 32, 1024, 512],
    "w_e_down": [1, 32, 512, 1024], "norm_final": [1024],
}


def consts():
    return {
        "c_identb": np.eye(128, dtype=np.float32).astype(ml_dtypes.bfloat16),
        "c_identf": np.eye(128, dtype=np.float32),
        "c_tri01": (np.arange(128)[None, :] <= np.arange(128)[:, None]).astype(np.float32).astype(ml_dtypes.bfloat16),
        "c_negtri": np.where(np.arange(128)[None, :] <= np.arange(128)[:, None], 0.0, -1e30).astype(np.float32),
        "c_bo": np.kron(np.eye(2), np.ones((64, 64))).astype(np.float32),
        "c_bo64": (np.kron(np.eye(2), np.ones((64, 64))) / 64.0).astype(np.float32),
        "c_maskq": np.block([[np.triu(np.ones((64, 64)), 1), np.triu(np.ones((64, 64)), 0)],
                             [np.triu(np.ones((64, 64)), 1), np.triu(np.ones((64, 64)), 0)]]).astype(np.float32),
        "c_lowm": np.concatenate([np.zeros((64, 64)), np.tril(np.ones((64, 64)), -1)], 0).astype(np.float32),
        "c_resetm": np.tile((np.arange(256) % 64 != 0).astype(np.float32)[None, :], (128, 1)),
        "c_utri": (np.arange(128)[:, None] < np.arange(128)[None, :]).astype(np.float32),
        "c_ones128": np.ones((128, 128), np.float32),
        "c_bstart": np.tile((np.arange(320) * 128.0)[None, :], (128, 1)).astype(np.float32),
        "c_iotap": np.arange(128, dtype=np.float32)[:, None].copy(),
        "c_pw": np.tile((0.5 ** (np.arange(NIT) + 1))[None, :], (128, 1)).astype(np.float32),
    }


def kernel(**inputs):
    x = np.asarray(inputs["x"], dtype=np.float32)
    mem = np.asarray(inputs["mem"], dtype=np.float32)
    B, T, _ = x.shape
    nseq = B // NCORES
    nc = build(T, nseq)
    cs = consts()
    in_maps = []
    for c in range(NCORES):
        m = {"x": np.ascontiguousarray(x[c * nseq:(c + 1) * nseq].reshape(nseq * T, D)),
             "mem": np.ascontiguousarray(mem[c * nseq:(c + 1) * nseq].reshape(nseq * 256, D))}
        for name in WSHAPES:
            m[name] = np.ascontiguousarray(np.asarray(inputs[name], dtype=np.float32))
        m.update(cs)
        in_maps.append(m)
    res = run_bass_kernel_spmd(nc, in_maps, core_ids=list(range(NCORES)))
    out = np.concatenate([r["out"].reshape(nseq, T, D) for r in res.results], axis=0)
    return out.astype(np.float32)
```

```python
from contextlib import ExitStack
import os
import numpy as np
import ml_dtypes
import concourse.bass as bass
import concourse.mybir as mybir
from concourse.bass_utils import run_bass_kernel_spmd

F32 = mybir.dt.float32
BF16 = mybir.dt.bfloat16
AF = mybir.ActivationFunctionType
ALU = mybir.AluOpType
AX = mybir.AxisListType

D = 1024
NCORES = 8


class Buf:
    __slots__ = ("name", "w", "r")

    def __init__(self, name=""):
        self.name = name
        self.w = None
        self.r = {}


class Sched:
    ENG = ("pe", "act", "dve", "pool", "sp")

    def __init__(self, nc, stack, n_dma_sems=10):
        self.nc = nc
        self.streams = {e: [] for e in self.ENG}
        self.sems = {}
        self.count = {}
        for e in self.ENG:
            self.sems[e] = stack.enter_context(nc.semaphore("s_" + e))
            self.count[e] = 0
        self.dma_sems = {}
        self.dma_rr = {}
        for q in ("sp", "act", "pool"):
            lst = []
            for i in range(n_dma_sems if q != "pool" else 28):
                k = "d_%s_%d" % (q, i)
                self.sems[k] = stack.enter_context(nc.semaphore(k))
                self.count[k] = 0
                lst.append(k)
            self.dma_sems[q] = lst
            self.dma_rr[q] = 0
        self.waited = {}
        self.nwaits = 0
        self.nops = 0

    def _wait(self, eng, key, val):
        if val <= 0 or self.waited.get((eng, key), 0) >= val:
            return
        self.waited[(eng, key)] = val
        self.streams[eng].append(("w", key, val))
        self.nwaits += 1

    def _deps(self, eng, reads, writes, own_key):
        for b in reads:
            if b.w is not None:
                self._dep(eng, b.w, own_key)
        for b in writes:
            if b.w is not None:
                self._dep(eng, b.w, own_key)
            for k, v in b.r.items():
                self._dep(eng, (k, v), own_key)

    def _dep(self, eng, ev, own_key):
        k, v = ev
        if k == "pe" and own_key == "pe":
            return
        self._wait(eng, k, v)

    muted = False

    def op(self, eng, fn, reads=(), writes=()):
        if self.muted:
            return
        self._deps(eng, reads, writes, eng)
        self.count[eng] += 1
        v = self.count[eng]
        self.streams[eng].append(("o", fn, eng, 1))
        for b in writes:
            b.w = (eng, v)
            b.r = {}
        for b in reads:
            if b.r.get(eng, 0) < v:
                b.r[eng] = v
        self.nops += 1

    def dma(self, q, out, in_, reads=(), writes=(), fn=None, **kw):
        if self.muted:
            return
        lst = self.dma_sems[q]
        key = lst[self.dma_rr[q] % len(lst)]
        self.dma_rr[q] += 1
        self._wait(q, key, self.count[key])
        self._deps(q, reads, writes, key)
        self.count[key] += 16
        v = self.count[key]
        if fn is None:
            fn = lambda e, out=out, in_=in_, kw=kw: e.dma_start(out=out, in_=in_, **kw)
        self.streams[q].append(("o", fn, key, 16))
        for b in writes:
            b.w = (key, v)
            b.r = {}
        for b in reads:
            if b.r.get(key, 0) < v:
                b.r[key] = v
        self.nops += 1

    def barrier(self):
        for e in self.ENG:
            for k in self.sems:
                if k != e or True:
                    self._wait(e, k, self.count[k])

    def finish(self, bufs, eng="sp"):
        for b in bufs:
            if b.w is not None:
                self._wait(eng, b.w[0], b.w[1])

    def emit(self):
        nc = self.nc
        sems = self.sems
        streams = self.streams
        with nc.Block() as block:
            def run(engobj, lst):
                for it in lst:
                    if it[0] == "w":
                        engobj.wait_ge(sems[it[1]], it[2])
                    else:
                        it[1](engobj).then_inc(sems[it[2]], it[3])

            @block.tensor
            def _(e):
                run(e, streams["pe"])

            @block.scalar
            def _(e):
                run(e, streams["act"])

            @block.vector
            def _(e):
                run(e, streams["dve"])

            @block.gpsimd
            def _(e):
                run(e, streams["pool"])

            @block.sync
            def _(e):
                run(e, streams["sp"])


class Ctx:
    def __init__(self, nc, S):
        self.nc = nc
        self.S = S
        self.B = {}
        self.rr = 0
        self.uid = 0

    def buf(self, name):
        if name not in self.B:
            self.B[name] = Buf(name)
        return self.B[name]

    def _bl(self, lst):
        return [self.buf(x) if isinstance(x, str) else x for x in lst]

    def sb(self, st, name, shape, dt):
        self.uid += 1
        t = st.enter_context(self.nc.sbuf_tensor("%s_u%d" % (name, self.uid), list(shape), dt))
        self.buf(name)
        return t

    def ps(self, st, name, shape, dt):
        self.uid += 1
        t = st.enter_context(self.nc.psum_tensor("%s_u%d" % (name, self.uid), list(shape), dt))
        self.buf(name)
        return t

    def op(self, eng, method, reads, writes, **kw):
        self.S.op(eng, lambda e, m=method, kw=kw: getattr(e, m)(**kw), self._bl(reads), self._bl(writes))

    def mm(self, out, lhsT, rhs, reads, writes, start=True, stop=True, **kw):
        self.S.op("pe", lambda e: e.matmul(out, lhsT, rhs, start=start, stop=stop, **kw),
                  self._bl(reads), self._bl(writes))

    def tr(self, out, in_, ident, reads, writes):
        self.S.op("pe", lambda e: e.transpose(out, in_, ident), self._bl(reads), self._bl(writes))

    def dma(self, q, out, in_, reads, writes, **kw):
        self.S.dma(q, out, in_, self._bl(reads), self._bl(writes), **kw)

    def q(self):
        self.rr += 1
        return ("sp", "act", "pool")[self.rr % 3]


C_RW = 0
C_Q = 1792
C_CKV = 2304
C_QI = 2432
C_KI = 2688
C_WI = 2752
C_G = 2756
R_RW = 0
R_Q = 1792
R_QI = 2304
R_KI = 2560
R_G = 2624
R_WI = 4672
R_TOT = 4676


def load_cast(K, st, tag, w_ap, kin, n, scale_col=None, dt=BF16, engs=("dve", "pool")):
    nc = K.nc
    kc = kin // 128
    wt = K.sb(st, tag, [128, kc, n], dt)
    src = w_ap.rearrange("(c p) n -> p c n", p=128)
    if True:
        stg = [K.sb(st, "%s_stg%d" % (tag, i), [128, n], F32) for i in range(2)]
        for c in range(kc):
            sg = stg[c % 2]
            nm = "%s_stg%d" % (tag, c % 2)
            K.dma(K.q(), sg[:], src[:, c, :], [], [nm])
            eng = engs[c % len(engs)]
            if scale_col is None:
                K.op(eng, "tensor_copy", [nm], [tag], out=wt[:, c, :], in_=sg[:])
            else:
                K.op(eng, "tensor_scalar", [nm, scale_col[1]], [tag], out=wt[:, c, :], in0=sg[:],
                     scalar1=scale_col[0][:, c:c + 1], scalar2=None, op0=ALU.mult)
    return wt


def norm_rows(K, tag, xt, xt_name, ss, junk, eps_scale=1.0 / D):
    K.op("act", "activation", [xt_name], [tag + "_junk", tag + "_ss"], out=junk[:], in_=xt[:], func=AF.Square,
         accum_out=ss[:])
    K.op("act", "activation", [tag + "_ss"], [tag + "_ss"], out=ss[:], in_=ss[:], func=AF.Sqrt,
         scale=eps_scale, bias=1e-6)
    K.op("dve", "reciprocal", [tag + "_ss"], [tag + "_ss"], out=ss[:], in_=ss[:])


def phase1(K, s, T, X, Wd, SC, CONST):
    nc = K.nc
    NT = T // 128
    NB = T // 512
    with ExitStack() as st:
        xnT = K.sb(st, "p1_xnT", [128, 8, T], BF16)
        gm = K.sb(st, "p1_gm", [128, 8], F32)
        K.dma("sp", gm[:], Wd["norm_mix"].rearrange("o (c p) -> p (o c)", p=128), [], ["p1_gm"], allow_slow_non_contiguous=True)
        bg = K.sb(st, "p1_bg", [128, 16], F32)
        K.dma("sp", bg[:], Wd["b_gate"].rearrange("o (c p) -> p (o c)", p=128), [], ["p1_bg"], allow_slow_non_contiguous=True)
        identb = CONST["identb"]
        pst = K.ps(st, "p1_pst", [128, 8, 128], BF16)
        xts = [K.sb(st, "p1_xt%d" % i, [128, D], F32) for i in range(2)]
        xnb = [K.sb(st, "p1_xn%d" % i, [128, D], BF16) for i in range(2)]
        junk = K.sb(st, "p1_junk", [128, D], F32)
        sss = [K.sb(st, "p1_ss%d" % i, [128, 1], F32) for i in range(2)]
        for tt in range(NT):
            i = tt % 2
            xt, xn, ss = xts[i], xnb[i], sss[i]
            K.dma("sp" if i == 0 else "act", xt[:], X[s * T + tt * 128: s * T + (tt + 1) * 128, :], [], ["p1_xt%d" % i])
            K.op("act", "activation", ["p1_xt%d" % i], ["p1_junk", "p1_ss%d" % i], out=junk[:], in_=xt[:],
                 func=AF.Square, accum_out=ss[:])
            K.op("act", "activation", ["p1_ss%d" % i, "eps6"], ["p1_ss%d" % i], out=ss[:], in_=ss[:], func=AF.Sqrt,
                 scale=1.0 / D, bias=CONST["eps6"][:])
            K.op("dve", "reciprocal", ["p1_ss%d" % i], ["p1_ss%d" % i], out=ss[:], in_=ss[:])
            K.op("dve", "tensor_scalar", ["p1_xt%d" % i, "p1_ss%d" % i], ["p1_xn%d" % i], out=xn[:], in0=xt[:],
                 scalar1=ss[:], scalar2=None, op0=ALU.mult)
            for c in range(8):
                K.tr(pst[:, c, :], xn[:, c * 128:(c + 1) * 128], identb[:], ["p1_xn%d" % i, "identb"], ["p1_pst"])
            K.op("pool" if False else "act", "activation", ["p1_pst"], ["p1_xnT"], out=xnT[:, :, tt * 128:(tt + 1) * 128],
                 in_=pst[:], func=AF.Copy)
        import os
        STOP = int(os.environ.get("STOP", "99"))
        if STOP <= 1:
            return
        chunks = []
        for i in range(14):
            chunks.append((C_RW + i * 128, 128, R_RW + i * 128, "fm", None))
        for i in range(4):
            chunks.append((C_Q + i * 128, 128, R_Q + i * 128, "fm", None))
        for i in range(2):
            chunks.append((C_QI + i * 128, 128, R_QI + i * 128, "fm", None))
        chunks.append((C_KI, 64, R_KI, "fm", None))
        chunks.append((C_WI, 4, R_WI, "fm", None))
        for i in range(16):
            chunks.append((C_G + i * 128, 128, R_G + i * 128, "gate", i))
        wsrc = Wd["w_in"].rearrange("o (c p) n -> p (o c) n", p=128)
        wst = [K.sb(st, "p1_wst%d" % i, [128, 8, 132], F32) for i in range(2)]
        wbf = [K.sb(st, "p1_wbf%d" % i, [128, 8, 132], BF16) for i in range(2)]
        stage = [K.sb(st, "p1_stage%d" % i, [128, T], F32) for i in range(2)]
        pss = [K.ps(st, "p1_ps%d" % i, [128, 512], F32) for i in range(4)]
        gmb = gm[:].unsqueeze(2).to_broadcast([128, 8, 128])
        ZF = SC["ZF"]
        for ci, (c0, ncol, r0, kind, gi) in enumerate(chunks):
            i = ci % 2
            K.dma("sp" if i == 0 else "pool", wst[i][:, :, 0:ncol], wsrc[:, :, c0:c0 + ncol], [], ["p1_wst%d" % i])
            K.op("dve", "tensor_tensor", ["p1_wst%d" % i, "p1_gm"], ["p1_wbf%d" % i], out=wbf[i][:, :, 0:ncol],
                 in0=wst[i][:, :, 0:ncol], in1=gm[:].unsqueeze(2).to_broadcast([128, 8, ncol]), op=ALU.mult)
            for tb in range(NB):
                pj = (ci * NB + tb) % 4
                ps = pss[pj]
                for dc in range(8):
                    K.mm(ps[0:ncol, :], wbf[i][:, dc, 0:ncol], xnT[:, dc, tb * 512:(tb + 1) * 512],
                         ["p1_wbf%d" % i, "p1_xnT"], ["p1_ps%d" % pj], start=(dc == 0), stop=(dc == 7))
                if kind == "gate":
                    K.op("act", "activation", ["p1_ps%d" % pj, "p1_bg"], ["p1_stage%d" % i],
                         out=stage[i][0:ncol, tb * 512:(tb + 1) * 512], in_=ps[0:ncol, :], func=AF.Sigmoid,
                         bias=bg[:, gi:gi + 1])
                else:
                    eng = "dve" if tb % 2 == 0 else "act"
                    if eng == "dve":
                        K.op("dve", "tensor_copy", ["p1_ps%d" % pj], ["p1_stage%d" % i],
                             out=stage[i][0:ncol, tb * 512:(tb + 1) * 512], in_=ps[0:ncol, :])
                    else:
                        K.op("act", "activation", ["p1_ps%d" % pj], ["p1_stage%d" % i],
                             out=stage[i][0:ncol, tb * 512:(tb + 1) * 512], in_=ps[0:ncol, :], func=AF.Copy)
            K.dma("act" if i == 0 else "sp", ZF[s, r0:r0 + ncol, :], stage[i][0:ncol, :], ["p1_stage%d" % i], ["ZF"])
        if STOP <= 2:
            return
        i = len(chunks) % 2
        K.dma("sp", wst[i][:, :, 0:128], wsrc[:, :, C_CKV:C_CKV + 128], [], ["p1_wst%d" % i])
        K.op("dve", "tensor_tensor", ["p1_wst%d" % i, "p1_gm"], ["p1_wbf%d" % i], out=wbf[i][:, :, 0:128],
             in0=wst[i][:, :, 0:128], in1=gm[:].unsqueeze(2).to_broadcast([128, 8, 128]), op=ALU.mult)
        ck = [K.sb(st, "p1_ck%d" % j, [128, 128], F32) for j in range(2)]
        ckb = [K.sb(st, "p1_ckb%d" % j, [128, 128], BF16) for j in range(2)]
        ckT = K.sb(st, "p1_ckT", [128, T], BF16)
        for tt in range(NT):
            j = tt % 2
            pj = tt % 4
            ps = pss[pj]
            for dc in range(8):
                K.mm(ps[:, 0:128], xnT[:, dc, tt * 128:(tt + 1) * 128], wbf[i][:, dc, 0:128],
                     ["p1_wbf%d" % i, "p1_xnT"], ["p1_ps%d" % pj], start=(dc == 0), stop=(dc == 7))
            K.op("dve", "tensor_copy", ["p1_ps%d" % pj], ["p1_ck%d" % j], out=ck[j][:], in_=ps[:, 0:128])
            K.op("act", "activation", ["p1_ck%d" % j], ["p1_junk", "p1_ss%d" % j], out=junk[:, 0:128], in_=ck[j][:],
                 func=AF.Square, accum_out=sss[j][:])
            K.op("act", "activation", ["p1_ss%d" % j, "eps6"], ["p1_ss%d" % j], out=sss[j][:], in_=sss[j][:], func=AF.Sqrt,
                 scale=1.0 / 128, bias=CONST["eps6"][:])
            K.op("dve", "reciprocal", ["p1_ss%d" % j], ["p1_ss%d" % j], out=sss[j][:], in_=sss[j][:])
            K.op("dve", "tensor_scalar", ["p1_ck%d" % j, "p1_ss%d" % j], ["p1_ckb%d" % j], out=ckb[j][:], in0=ck[j][:],
                 scalar1=sss[j][:], scalar2=None, op0=ALU.mult)
            K.tr(pst[:, 0, :], ckb[j][:], identb[:], ["p1_ckb%d" % j, "identb"], ["p1_pst"])
            K.op("act", "activation", ["p1_pst"], ["p1_ckT"], out=ckT[:, tt * 128:(tt + 1) * 128], in_=pst[:, 0, :],
                 func=AF.Copy)
            K.dma("sp", SC["CK"][s, tt * 128:(tt + 1) * 128, :], ckb[j][:], ["p1_ckb%d" % j], ["CK"])
        K.dma("sp", SC["CKT"][s, :, :], ckT[:], ["p1_ckT"], ["CKT"])


NIT = 14


def phase_dsa(K, s, T, Wd, SC, CONST, extra=None):
    nc = K.nc
    NT = T // 128
    ZF = SC["ZF"]
    identb, identf = CONST["identb"], CONST["identf"]
    with ExitStack() as st:
        dps = [K.ps(st, "ds_ps%d" % i, [128, 512], F32) for i in range(4)]
        Ob = [K.ps(st, "ds_o%d" % i, [128, 3, 130], F32) for i in range(3)]
        MT = K.ps(st, "ds_mt", [128, 8, 128], BF16)
        wuk = K.sb(st, "ds_wuk", [128, 512], F32)
        K.dma("sp", wuk[:], Wd["w_uk"].rearrange("o r h d -> r (o h d)"), [], ["ds_wuk"])
        wukT = K.sb(st, "ds_wukT", [64, 8, 128], BF16)
        for h in range(8):
            K.tr(dps[3][0:64, 0:128], wuk[:, h * 64:(h + 1) * 64], identf[:], ["ds_wuk", "identf"], ["ds_ps3"])
            K.op("dve", "tensor_copy", ["ds_ps3"], ["ds_wukT"], out=wukT[:, h, :], in_=dps[3][0:64, 0:128])
        kvn = K.sb(st, "ds_kvn", [128, 1], F32)
        K.dma("sp", kvn[:], Wd["kv_norm"].rearrange("o r -> r o"), [], ["ds_kvn"], allow_slow_non_contiguous=True)
        kvn8 = K.sb(st, "ds_kvn8", [128, 1], F32)
        K.op("dve", "tensor_scalar", ["ds_kvn"], ["ds_kvn8"], out=kvn8[:], in0=kvn[:], scalar1=0.125, scalar2=None,
             op0=ALU.mult)
        wuv = K.sb(st, "ds_wuv", [128, 512], F32)
        K.dma("sp", wuv[:], Wd["w_uv"].rearrange("o r h d -> r (o h d)"), [], ["ds_wuv"])
        wuvb = K.sb(st, "ds_wuvb", [128, 512], BF16)
        K.op("dve", "tensor_scalar", ["ds_wuv", "ds_kvn"], ["ds_wuvb"], out=wuvb[:], in0=wuv[:], scalar1=kvn[:],
             scalar2=None, op0=ALU.mult)
        CKT = K.sb(st, "ds_ckt", [128, T], BF16)
        K.dma("sp", CKT[:], SC["CKT"][s, :, :], ["CKT"], ["ds_ckt"])
        CKA = K.sb(st, "ds_cka", [128, NT, 130], BF16)
        K.op("pool", "memset", [], ["ds_cka"], ap=CKA[:], constant=1.0)
        K.dma("sp", CKA[:, :, 0:128], SC["CK"][s, :, :].rearrange("(k p) r -> p k r", p=128), ["CK"], ["ds_cka"])
        kif = K.sb(st, "ds_kif", [64, T], F32)
        K.dma("act", kif[:], ZF[s, R_KI:R_KI + 64, :], ["ZF"], ["ds_kif"])
        kib = K.sb(st, "ds_kib", [64, T], BF16)
        K.op("dve", "tensor_copy", ["ds_kif"], ["ds_kib"], out=kib[:], in_=kif[:])
        qib = K.sb(st, "ds_qib", [64, 4, 128], BF16)
        zl = K.sb(st, "ds_zl", [128, 128], BF16)
        zb = K.sb(st, "ds_zb", [128, 390], BF16)
        K.op("pool", "memset", [], ["ds_zl"], ap=zl[:], constant=0.0)
        K.op("pool", "memset", [], ["ds_zb"], ap=zb[:], constant=0.0)
        tri01, negtri, pw = CONST["tri01"], CONST["negtri"], CONST["pw"]
        qf = K.sb(st, "ds_qf", [64, 8, 128], F32)
        qb = K.sb(st, "ds_qb", [64, 8, 128], BF16)
        qif = K.sb(st, "ds_qif", [64, 4, 128], F32)
        wif = K.sb(st, "ds_wif", [4, 128], F32)
        wit = K.sb(st, "ds_wit", [128, 4], F32)
        qlat = K.sb(st, "ds_qlat", [128, 1024], BF16)
        isc = K.sb(st, "ds_isc", [128, T], F32)
        junk = K.sb(st, "ds_junk", [128, T], BF16)
        rl = [K.sb(st, "ds_rl%d" % i, [128, 512], F32) for i in range(3)]
        maskb = K.sb(st, "ds_mask", [128, T], BF16)
        col = K.sb(st, "ds_col", [128, 8], F32)
        hk = K.sb(st, "ds_hk", [128, NIT], F32)
        junk2 = K.sb(st, "ds_junk2", [128, T], BF16)
        cola = K.sb(st, "ds_cola", [128, 1], F32)
        mts = [K.sb(st, "ds_mts%d" % i, [128, 128], BF16) for i in range(2)]
        ee = [K.sb(st, "ds_e%d" % i, [128, 4, 128], BF16) for i in range(4)]
        pp = [K.sb(st, "ds_p%d" % i, [128, 4, 128], BF16) for i in range(4)]
        rd = K.sb(st, "ds_rd", [128, 8, 1], F32)
        onb = K.sb(st, "ds_onb", [128, 8, 128], BF16)
        onT = K.sb(st, "ds_onT", [128, 8, 128], BF16)
        ybs = K.sb(st, "ds_ybs", [128, 4, 128], BF16)
        maskbs = [maskb, K.sb(st, "ds_mask1", [128, T], BF16)]
        MN = ["ds_mask", "ds_mask1"]

        def select(qt):
            t0 = qt * 128
            nk = qt + 1
            nkeys = nk * 128
            mb = maskbs[qt % 2]
            mn = MN[qt % 2]
            if qt >= 2:
                K.dma("act", qif[:], ZF[s, R_QI:R_QI + 256, t0:t0 + 128].rearrange("(h p) t -> p h t", p=64), ["ZF"], ["ds_qif"])
                K.op("act", "activation", ["ds_qif"], ["ds_qib"], out=qib[:], in_=qif[:], func=AF.Copy)
                K.dma("act", wif[:], ZF[s, R_WI:R_WI + 4, t0:t0 + 128], ["ZF"], ["ds_wif"])
                K.tr(dps[3][:, 0:4], wif[:], identf[0:4, 0:4], ["ds_wif", "identf"], ["ds_ps3"])
                K.op("dve", "tensor_scalar", ["ds_ps3"], ["ds_wit"], out=wit[:], in0=dps[3][:, 0:4], scalar1=1.0 / 16,
                     scalar2=None, op0=ALU.mult)
                yield
                for kb in range((nkeys + 511) // 512):
                    w = min(512, nkeys - kb * 512)
                    ks = slice(kb * 512, kb * 512 + w)
                    for h in range(4):
                        pb = 2 + (h % 2)
                        K.mm(dps[pb][:, 0:w], qib[:, h, :], kib[:, ks], ["ds_qib", "ds_kib"], ["ds_ps%d" % pb])
                        if h == 0:
                            K.op("dve", "tensor_scalar", ["ds_ps%d" % pb, "ds_wit"], ["ds_isc"], out=isc[:, ks], in0=dps[pb][:, 0:w],
                                 scalar1=0.0, scalar2=wit[:, 0:1], op0=ALU.max, op1=ALU.mult)
                        else:
                            K.op("act", "activation", ["ds_ps%d" % pb], ["ds_rl%d" % (h - 1)], out=rl[h - 1][:, 0:w],
                                 in_=dps[pb][:, 0:w], func=AF.Relu)
                            K.op("dve", "scalar_tensor_tensor", ["ds_rl%d" % (h - 1), "ds_wit", "ds_isc"], ["ds_isc"],
                                 out=isc[:, ks], in0=rl[h - 1][:, 0:w], scalar=wit[:, h:h + 1], in1=isc[:, ks],
                                 op0=ALU.mult, op1=ALU.add)
                    yield
                K.op("dve", "tensor_reduce", ["ds_isc"], ["ds_col"], out=col[:, 0:1], in_=isc[:, 0:nkeys], axis=AX.X, op=ALU.max)
                K.op("dve", "tensor_reduce", ["ds_isc"], ["ds_col"], out=col[:, 1:2], in_=isc[:, 0:nkeys], axis=AX.X, op=ALU.min)
                K.op("dve", "tensor_scalar", ["ds_col"], ["ds_col"], out=col[:, 2:3], in0=col[:, 0:1], scalar1=col[:, 1:2],
                     scalar2=2e-6, op0=ALU.subtract, op1=ALU.add)
                K.op("dve", "tensor_scalar", ["ds_col"], ["ds_col"], out=col[:, 3:4], in0=col[:, 1:2], scalar1=-1e-6,
                     scalar2=None, op0=ALU.add)
                K.op("dve", "tensor_scalar", ["pw", "ds_col"], ["ds_hk"], out=hk[:], in0=pw[:], scalar1=col[:, 2:3],
                     scalar2=None, op0=ALU.mult)
                K.op("dve", "tensor_tensor", ["ds_isc", "negtri"], ["ds_isc"], out=isc[:, t0:t0 + 128], in0=isc[:, t0:t0 + 128],
                     in1=negtri[:], op=ALU.add)
                K.op("dve", "tensor_tensor", ["ds_col", "ds_hk"], ["ds_col", "ds_colm"], out=col[:, 4:5], in0=col[:, 3:4], in1=hk[:, 0:1], op=ALU.add)
                yield
                nd = nkeys
                if nkeys >= 1024 and not os.environ.get("NO_ACTCNT"):
                    nd = ((nkeys * 5 // 8) // 128) * 128
                na = nkeys - nd
                for k in range(NIT):
                    K.op("dve", "tensor_scalar", ["ds_isc", "ds_colm"], ["ds_junk", "ds_col"], out=junk[:, 0:nd],
                         in0=isc[:, 0:nd], scalar1=col[:, 4:5], scalar2=None, op0=ALU.is_ge, op1=ALU.add,
                         accum_out=col[:, 5:6])
                    if na > 0:
                        K.op("act", "activation", ["ds_isc", "ds_colm"], ["ds_junk2", "ds_cola"], out=junk2[:, 0:na], in_=isc[:, nd:nkeys],
                             func=AF.Sign, scale=-1.0, bias=col[:, 4:5], accum_out=cola[:, 0:1])
                        K.op("dve", "scalar_tensor_tensor", ["ds_cola", "ds_col"], ["ds_col"], out=col[:, 5:6], in0=cola[:, 0:1], scalar=-0.5,
                             in1=col[:, 5:6], op0=ALU.mult, op1=ALU.add)
                    K.op("dve", "tensor_scalar", ["ds_col", "ds_hk"], ["ds_col"], out=col[:, 6:7], in0=col[:, 5:6],
                         scalar1=255.5 - 0.5 * na, scalar2=hk[:, k:k + 1], op0=ALU.is_ge, op1=ALU.mult)
                    kn = min(k + 1, NIT - 1)
                    dst = col[:, 4:5] if k < NIT - 1 else col[:, 3:4]
                    K.op("dve", "scalar_tensor_tensor", ["ds_col", "ds_colm", "ds_hk"], ["ds_col", "ds_colm"], out=dst, in0=col[:, 6:7], scalar=col[:, 4:5],
                         in1=hk[:, kn:kn + 1], op0=ALU.add, op1=ALU.subtract)
                    yield
                K.op("dve", "tensor_scalar", ["ds_isc", "ds_col"], [mn], out=mb[:, 0:nkeys], in0=isc[:, 0:nkeys],
                     scalar1=col[:, 3:4], scalar2=None, op0=ALU.is_ge)
            else:
                if qt > 0:
                    K.op("pool", "memset", [], [mn], ap=mb[:, 0:t0], constant=1.0)
                K.op("pool", "tensor_copy", ["tri01"], [mn], out=mb[:, t0:t0 + 128], in_=tri01[:])
            yield

        def attend(qt):
            t0 = qt * 128
            nk = qt + 1
            mb = maskbs[qt % 2]
            mn = MN[qt % 2]
            K.dma("sp", qf[:], ZF[s, R_Q:R_Q + 512, t0:t0 + 128].rearrange("(h p) t -> p h t", p=64), ["ZF"], ["ds_qf"])
            K.op("act", "activation", ["ds_qf"], ["ds_qb"], out=qb[:], in_=qf[:], func=AF.Copy)
            for h in range(8):
                K.mm(dps[h // 4][:, (h % 4) * 128:(h % 4 + 1) * 128], wukT[:, h, :], qb[:, h, :], ["ds_wukT", "ds_qb"],
                     ["ds_ps%d" % (h // 4)])
            for j in range(2):
                K.op("act", "activation", ["ds_ps%d" % j, "ds_kvn8"], ["ds_qlat"], out=qlat[:, j * 512:(j + 1) * 512],
                     in_=dps[j][:], func=AF.Copy, scale=kvn8[:, 0:1])
            for bq in range(3):
                K.mm(Ob[bq][:].rearrange("p a b -> p (a b)"), zl[:], zb[:], ["ds_zl", "ds_zb"], ["ds_o%d" % bq], start=True,
                     stop=False, skip_group_check=True)
            yield
            def front(kt):
                par = kt % 2
                K.tr(MT[:, 0, :], mb[:, kt * 128:(kt + 1) * 128], identb[:], [mn, "identb"], ["ds_mt0", "ds_mt1", "ds_mtall"])
                K.op("act", "activation", ["ds_mt0", "ds_mt1", "ds_mtall"], ["ds_mts%d" % par], out=mts[par][:], in_=MT[:, 0, :], func=AF.Copy)
                for j in range(2):
                    ej = 2 * par + j
                    K.mm(dps[j][:], CKT[:, kt * 128:(kt + 1) * 128], qlat[:, j * 512:(j + 1) * 512], ["ds_ckt", "ds_qlat"],
                         ["ds_ps%d" % j])
                    K.op("act", "activation", ["ds_ps%d" % j], ["ds_e%d" % ej], out=ee[ej][:],
                         in_=dps[j][:].rearrange("p (a b) -> p a b", a=4), func=AF.Exp)
            front(0)
            for kt in range(nk):
                par = kt % 2
                mtb = "ds_mt%d" % par
                if kt + 1 < nk:
                    front(kt + 1)
                for j in range(2):
                    ej = 2 * par + j
                    K.op("dve", "tensor_tensor", ["ds_e%d" % ej, "ds_mts%d" % par], ["ds_p%d" % ej], out=pp[ej][:], in0=ee[ej][:],
                         in1=mts[par][:].unsqueeze(1).to_broadcast([128, 4, 128]), op=ALU.mult)
                for j in range(2):
                    ej = 2 * par + j
                    for hh in range(4):
                        h = 4 * j + hh
                        K.mm(Ob[h // 3][:, h % 3, 0:129], pp[ej][:, hh, :], CKA[:, kt, 0:129], ["ds_p%d" % ej, "ds_cka"],
                             ["ds_o%d" % (h // 3)], start=False, stop=(kt == nk - 1), skip_group_check=True)
                yield
            for bq in range(3):
                nh = 3 if bq < 2 else 2
                K.op("dve", "reciprocal", ["ds_o%d" % bq], ["ds_rd"], out=rd[:, 3 * bq:3 * bq + nh, :], in_=Ob[bq][:, 0:nh, 128:129])
                K.op("dve", "tensor_tensor", ["ds_o%d" % bq, "ds_rd"], ["ds_onb"], out=onb[:, 3 * bq:3 * bq + nh, :],
                     in0=Ob[bq][:, 0:nh, 0:128], in1=rd[:, 3 * bq:3 * bq + nh, :].to_broadcast([128, nh, 128]), op=ALU.mult)
            for h in range(8):
                K.tr(MT[:, h, :], onb[:, h, :], identb[:], ["ds_onb", "identb"], ["ds_mt0", "ds_mt1", "ds_mtall"])
            K.op("act", "activation", ["ds_mt0", "ds_mt1", "ds_mtall"], ["ds_onT"], out=onT[:], in_=MT[:], func=AF.Copy)
            for h in range(8):
                K.mm(dps[0][(h % 2) * 64:(h % 2 + 1) * 64, (h // 2) * 128:(h // 2 + 1) * 128], wuvb[:, h * 64:(h + 1) * 64],
                     onT[:, h, :], ["ds_wuvb", "ds_onT"], ["ds_ps0"])
            K.op("dve", "tensor_copy", ["ds_ps0"], ["ds_ybs"], out=ybs[:], in_=dps[0][:].rearrange("p (a b) -> p a b", a=4))
            K.dma("sp", SC["YB"][s, :, t0:t0 + 128].rearrange("(c p) t -> p c t", p=128), ybs[:], ["ds_ybs"], ["YB"])
            yield

        xg = extra(st) if extra is not None else None
        for step in range(NT + 1):
            gens = []
            if step >= 1:
                gens.append(attend(step - 1))
            if step < NT:
                gens.append(select(step))
            if xg is not None:
                try:
                    next(xg)
                except StopIteration:
                    xg = None
            while gens:
                for g in list(gens):
                    try:
                        next(g)
                    except StopIteration:
                        gens.remove(g)
        if xg is not None:
            for _ in xg:
                pass


class _Stop(Exception):
    pass


def phase_rwkv(K, s, T, Wd, SC, CONST):
    _phase_rwkv(K, s, T, Wd, SC, CONST)
    K.S.muted = False


def _phase_rwkv(K, s, T, Wd, SC, CONST):
    nc = K.nc
    TBK = 256
    NCH = TBK // 64
    NBK = T // TBK
    ZF = SC["ZF"]
    identf = CONST["identf"]
    bo, bo64, maskq, lowm, resetm = CONST["bo"], CONST["bo64"], CONST["maskq"], CONST["lowm"], CONST["resetm"]
    with ExitStack() as st:
        rp = [K.ps(st, "rk_p%d" % i, [128, 512], F32) for i in range(8)]
        RP = ["rk_p%d" % i for i in range(8)]

        def colload(tag, ap512, n=4):
            t = K.sb(st, tag, [128, n], F32)
            K.dma("sp", t[:], ap512.rearrange("o (c p) -> p (o c)", p=128), [], [tag], allow_slow_non_contiguous=True)
            return t
        mu = colload("rk_mu", Wd["shift_mu"], 14)
        w0c = colload("rk_w0c", Wd["rw_w0"])
        a0c = colload("rk_a0c", Wd["rw_a0"])
        kkc = colload("rk_kkc", Wd["rw_k_k"])
        kac = colload("rk_kac", Wd["rw_k_a"])
        rkc = colload("rk_rkc", Wd["rw_r_k"].rearrange("o h d -> o (h d)"))
        lnw = colload("rk_lnw", Wd["rw_ln_w"])
        lnb = colload("rk_lnb", Wd["rw_ln_b"])
        w2a2 = K.sb(st, "rk_w2a2", [128, 512], F32)
        K.dma("sp", w2a2[0:64, :], Wd["rw_w2"][0], [], ["rk_w2a2"])
        K.dma("sp", w2a2[64:128, :], Wd["rw_a2"][0], [], ["rk_w2a2"])
        g2 = K.sb(st, "rk_g2", [128, 512], F32)
        K.dma("sp", g2[:], Wd["rw_g2"][0], [], ["rk_g2"])
        epsg = K.sb(st, "rk_epsg", [128, 1], F32)
        K.op("dve", "memset", [], ["rk_epsg"], ap=epsg[:], constant=64e-5)
        zin = K.sb(st, "rk_zin", [128, 14, TBK + 1], F32)
        zs = K.sb(st, "rk_zs", [128, 14, TBK], F32)
        tw = K.sb(st, "rk_tw", [128, TBK], F32)
        sg = K.sb(st, "rk_sg", [128, TBK], F32)

        def t4(tag):
            return K.sb(st, tag, [128, 4, TBK], F32)
        lw, aa, gg, LL, eL, enL, eLm, kk, t1, kp, bb, bon, Yb = [t4("rk_" + n) for n in
            ("lw", "aa", "gg", "LL", "eL", "enL", "eLm", "kk", "t1", "kp", "bb", "bon", "Yb")]
        QR = K.sb(st, "rk_QR", [128, 4, NCH, 2, 64], F32)
        KB = K.sb(st, "rk_KB", [128, 4, NCH, 2, 64], F32)
        gC = K.sb(st, "rk_gC", [128, 4, NCH], F32)
        M = K.sb(st, "rk_M", [128, 4, 64], F32)
        K.op("dve", "memset", [], ["rk_M"], ap=M[:], constant=0.0)
        KBTs = [K.sb(st, "rk_KBT%d" % i, [128, 4, 128], F32) for i in range(2)]
        VTs = [K.sb(st, "rk_VT%d" % i, [64, 4, 128], F32) for i in range(2)]
        ATs = [K.sb(st, "rk_AT%d" % i, [128, 8, 128], F32) for i in range(2)]
        DDT = BF16 if os.environ.get("RW_BF16", "1") == "1" else F32
        Am = [K.sb(st, "rk_Am%d" % i, [128, 8, 64], DDT) for i in range(2)]
        Bm = [K.sb(st, "rk_Bm%d" % i, [128, 8, 64], DDT) for i in range(2)]
        Pm = [K.sb(st, "rk_Pm%d" % i, [128, 8, 64], DDT) for i in range(2)]
        PmFs = [K.sb(st, "rk_PmF%d" % i, [128, 8, 64], F32) for i in range(2)]
        Rs = K.sb(st, "rk_Rs", [128, 512], F32)
        Us = K.sb(st, "rk_Us", [128, 512], F32)
        yab = K.sb(st, "rk_yab", [128, 4, TBK], BF16)
        H = slice(64, 128)

        def v4(t):
            return t[:].rearrange("p c (n t) -> p c n t", t=64)

        def bc(colt, n=4, w=TBK):
            return colt[:].unsqueeze(2).to_broadcast([128, n, w])

        RS = float(os.environ.get("RSTOP", "99"))

        def chk(k):
            if RS <= k:
                K.S.muted = True

        for tb in range(NBK):
            t0 = tb * TBK
            if tb == 0:
                K.op("dve", "memset", [], ["rk_zin"], ap=zin[:, :, 0:1], constant=0.0)
                K.dma("sp", zin[:, :, 1:TBK + 1], ZF[s, 0:1792, 0:TBK].rearrange("(c p) t -> p c t", p=128), ["ZF"], ["rk_zin"])
            else:
                K.dma("sp", zin[:, :, :], ZF[s, 0:1792, t0 - 1:t0 + TBK].rearrange("(c p) t -> p c t", p=128), ["ZF"], ["rk_zin"])
            K.op("dve", "tensor_tensor", ["rk_zin"], ["rk_zs"], out=zs[:], in0=zin[:, :, 0:TBK], in1=zin[:, :, 1:TBK + 1], op=ALU.subtract)
            for c14 in range(14):
                K.op("dve", "scalar_tensor_tensor", ["rk_zs", "rk_mu", "rk_zin"], ["rk_zs"], out=zs[:, c14, :], in0=zs[:, c14, :], scalar=mu[:, c14:c14 + 1],
                     in1=zin[:, c14, 1:TBK + 1], op0=ALU.mult, op1=ALU.add)
            chk(1)
            r_, k_, v_ = zs[:, 0:4, :], zs[:, 4:8, :], zs[:, 8:12, :]
            K.op("act", "activation", ["rk_zs"], ["rk_tw"], out=tw[0:64, :], in_=zs[0:64, 12, :], func=AF.Tanh)
            K.op("act", "activation", ["rk_zs"], ["rk_sg"], out=sg[:], in_=zs[:, 13, :], func=AF.Sigmoid)
            for cc in range(4):
                cs = slice(cc * 128, (cc + 1) * 128)
                K.mm(rp[0][:, 0:TBK], w2a2[0:64, cs], tw[0:64, :], ["rk_w2a2", "rk_tw"], [RP[0]])
                K.op("act", "activation", [RP[0], "rk_w0c"], ["rk_lw"], out=lw[:, cc, :], in_=rp[0][:, 0:TBK], func=AF.Sigmoid, bias=w0c[:, cc:cc + 1])
                K.mm(rp[1][:, 0:TBK], w2a2[H, cs], zs[H, 12, :], ["rk_w2a2", "rk_zs"], [RP[1]])
                K.op("act", "activation", [RP[1], "rk_a0c"], ["rk_aa"], out=aa[:, cc, :], in_=rp[1][:, 0:TBK], func=AF.Sigmoid, bias=a0c[:, cc:cc + 1])
                K.mm(rp[2][:, 0:TBK], g2[:, cs], sg[:], ["rk_g2", "rk_sg"], [RP[2]])
                K.op("dve", "tensor_copy", [RP[2]], ["rk_gg"], out=gg[:, cc, :], in_=rp[2][:, 0:TBK])
            chk(2)
            K.op("dve", "tensor_scalar", ["rk_lw"], ["rk_lw"], out=lw[:], in0=lw[:], scalar1=-0.6065306597126334, scalar2=None, op0=ALU.mult)
            for cc in range(4):
                K.op("dve", "tensor_tensor_scan", ["rk_lw", "resetm"], ["rk_LL"], out=LL[:, cc, :], data0=resetm[:], data1=lw[:, cc, :],
                     initial=0.0, op0=ALU.mult, op1=ALU.add)
            K.op("act", "activation", ["rk_LL"], ["rk_eL"], out=eL[:], in_=LL[:], func=AF.Exp)
            K.op("act", "activation", ["rk_LL"], ["rk_enL"], out=enL[:], in_=LL[:], func=AF.Exp, scale=-1.0)
            K.op("dve", "tensor_tensor", ["rk_LL", "rk_lw"], ["rk_t1"], out=t1[:], in0=LL[:], in1=lw[:], op=ALU.subtract)
            K.op("act", "activation", ["rk_t1"], ["rk_eLm"], out=eLm[:], in_=t1[:], func=AF.Exp)
            K.op("dve", "tensor_tensor", ["rk_zs", "rk_kkc"], ["rk_kk"], out=kk[:], in0=k_, in1=bc(kkc), op=ALU.mult)
            K.op("pool", "tensor_tensor", ["rk_kk"], ["rk_t1"], out=t1[:], in0=kk[:], in1=kk[:], op=ALU.mult)
            for cc in range(4):
                K.mm(rp[cc % 4][:, 0:TBK], bo[:], t1[:, cc, :], ["bo", "rk_t1"], [RP[cc % 4]])
                K.op("act", "activation", [RP[cc % 4]], ["rk_kp"], out=kp[:, cc, :], in_=rp[cc % 4][:, 0:TBK], func=AF.Sqrt)
            K.op("dve", "tensor_scalar", ["rk_kp"], ["rk_kp"], out=kp[:], in0=kp[:], scalar1=1e-12, scalar2=None, op0=ALU.max)
            K.op("dve", "reciprocal", ["rk_kp"], ["rk_kp"], out=kp[:], in_=kp[:])
            K.op("dve", "tensor_tensor", ["rk_kk", "rk_kp"], ["rk_kk"], out=kk[:], in0=kk[:], in1=kp[:], op=ALU.mult)
            for cc in range(4):
                K.op("dve", "tensor_scalar", ["rk_aa", "rk_kac"], ["rk_t1"], out=t1[:, cc, :], in0=aa[:, cc, :], scalar1=-1.0, scalar2=kac[:, cc:cc + 1],
                     op0=ALU.add, op1=ALU.mult)
            K.op("dve", "scalar_tensor_tensor", ["rk_t1", "rk_zs"], ["rk_kp"], out=kp[:], in0=t1[:], scalar=1.0, in1=k_, op0=ALU.add, op1=ALU.mult)
            K.op("pool", "tensor_tensor", ["rk_kk", "rk_aa"], ["rk_bb"], out=bb[:], in0=kk[:], in1=aa[:], op=ALU.mult)
            K.op("dve", "tensor_tensor", ["rk_zs", "rk_eL"], ["rk_QR"], out=QR[:, :, :, 1, :], in0=r_.rearrange("p c (n t) -> p c n t", t=64), in1=v4(eL), op=ALU.mult)
            K.op("dve", "tensor_tensor", ["rk_kk", "rk_eLm"], ["rk_QR"], out=QR[:, :, :, 0, :], in0=v4(kk), in1=v4(eLm), op=ALU.mult)
            K.op("dve", "tensor_tensor", ["rk_kp", "rk_enL"], ["rk_KB"], out=KB[:, :, :, 0, :], in0=v4(kp), in1=v4(enL), op=ALU.mult)
            K.op("pool", "tensor_tensor", ["rk_bb", "rk_enL"], ["rk_KB"], out=KB[:, :, :, 1, :], in0=v4(bb), in1=v4(enL), op=ALU.mult)
            K.op("dve", "tensor_copy", ["rk_eL"], ["rk_gC"], out=gC[:], in_=v4(eL)[:, :, :, 63])
            K.op("pool", "tensor_tensor", ["rk_zs", "rk_kp"], ["rk_t1"], out=t1[:], in0=r_, in1=kp[:], op=ALU.mult)
            K.op("pool", "tensor_tensor", ["rk_t1", "rk_rkc"], ["rk_t1"], out=t1[:], in0=t1[:], in1=bc(rkc), op=ALU.mult)
            for cc in range(4):
                K.mm(rp[cc % 4][:, 0:TBK], bo[:], t1[:, cc, :], ["bo", "rk_t1"], [RP[cc % 4]])
                K.op("dve", "tensor_tensor", [RP[cc % 4], "rk_zs"], ["rk_bon"], out=bon[:, cc, :], in0=rp[cc % 4][:, 0:TBK], in1=zs[:, 8 + cc, :], op=ALU.mult)
            chk(3)
            def ev(t, par):
                return t.rearrange("p (a two) b -> p a two b", two=2)[:, :, par, :]

            def pre(c):
                q = c % 2
                KBT, VT, AT, PmF = KBTs[q], VTs[q], ATs[q], PmFs[q]
                nKBT, nVT, nAT, nPmF = "rk_KBT%d" % q, "rk_VT%d" % q, "rk_AT%d" % q, "rk_PmF%d" % q
                for cc in range(4):
                    K.tr(rp[0][:, cc * 128:(cc + 1) * 128], KB[:, cc, c, :, :].rearrange("p a b -> p (a b)"), identf[:], ["rk_KB", "identf"], [RP[0]])
                    K.tr(rp[1][0:64, cc * 128:(cc + 1) * 128], zs[:, 8 + cc, c * 64:(c + 1) * 64], identf[:], ["rk_zs", "identf"], [RP[1]])
                K.op("act", "activation", [RP[0]], [nKBT], out=KBT[:].rearrange("p a b -> p (a b)"), in_=rp[0][:], func=AF.Copy)
                K.op("dve", "tensor_copy", [RP[1]], [nVT], out=VT[:].rearrange("p a b -> p (a b)"), in_=rp[1][0:64, :])
                yield
                for h in range(8):
                    cc, h2 = h // 2, h % 2
                    rows = slice(h2 * 64, (h2 + 1) * 64)
                    K.mm(rp[2 + h2][:, cc * 128:(cc + 1) * 128], KB[rows, cc, c, :, :].rearrange("p a b -> p (a b)"),
                         QR[rows, cc, c, :, :].rearrange("p a b -> p (a b)"), ["rk_KB", "rk_QR"], [RP[2 + h2]])
                    K.mm(rp[h2][H, cc * 64:(cc + 1) * 64], QR[rows, cc, c, 0, :], KB[rows, cc, c, 1, :], ["rk_QR", "rk_KB"], [RP[h2]])
                for h2 in range(2):
                    K.op("dve", "tensor_tensor", [RP[2 + h2], "maskq"], [nAT], out=ev(AT[:], h2),
                         in0=rp[2 + h2][:].rearrange("p (a b) -> p a b", a=4), in1=maskq[:].unsqueeze(1).to_broadcast([128, 4, 128]), op=ALU.mult)
                    K.op("dve", "tensor_tensor", [RP[h2], "lowm"], ["rk_Bm0"], out=ev(Bm[0][H, :, :], h2),
                         in0=rp[h2][H, 0:256].rearrange("p (a b) -> p a b", a=4), in1=lowm[H, :].unsqueeze(1).to_broadcast([64, 4, 64]), op=ALU.mult)
                K.op("act", "activation", [nAT], ["rk_Am0"], out=Am[0][H, :, :], in_=AT[H, :, 0:64], func=AF.Copy)
                K.op("dve", "tensor_tensor", ["identf", nAT], ["rk_Pm0"], out=Pm[0][H, :, :],
                     in0=identf[H, 64:128].unsqueeze(1).to_broadcast([64, 8, 64]), in1=AT[H, :, 0:64], op=ALU.subtract)
                yield
                for lvl in range(5):
                    ci, ni = lvl % 2, (lvl + 1) % 2
                    An, Bn, Pn = "rk_Am%d" % ni, "rk_Bm%d" % ni, "rk_Pm%d" % ni
                    Ac, Bc, Pc = "rk_Am%d" % ci, "rk_Bm%d" % ci, "rk_Pm%d" % ci
                    for h in range(8):
                        hs = slice(h * 64, (h + 1) * 64)
                        if lvl < 4:
                            K.mm(rp[2][H, hs], Bm[ci][H, h, :], Am[ci][H, h, :], [Ac, Bc], [RP[2]])
                        K.mm(rp[3][H, hs], Am[ci][H, h, :], Bm[ci][H, h, :], [Ac, Bc], [RP[3]])
                    if lvl < 4:
                        K.op("act", "activation", [RP[2]], [An], out=Am[ni][H, :, :], in_=rp[2][H, :].rearrange("p (a b) -> p a b", a=8), func=AF.Copy)
                    K.op("dve", "tensor_copy", [RP[3]], [Bn], out=Bm[ni][H, :, :], in_=rp[3][H, :].rearrange("p (a b) -> p a b", a=8))
                    yield
                    for h in range(8):
                        hs = slice(h * 64, (h + 1) * 64)
                        K.mm(rp[0][H, hs], Bm[ni][H, h, :], Pm[ci][H, h, :], [Bn, Pc], [RP[0]])
                    if lvl < 4:
                        K.op("dve", "tensor_tensor", [RP[0], Pc], [Pn], out=Pm[ni][H, :, :], in0=rp[0][H, :].rearrange("p (a b) -> p a b", a=8),
                             in1=Pm[ci][H, :, :], op=ALU.add)
                    else:
                        K.op("dve", "tensor_tensor", [RP[0], Pc], [nPmF], out=PmF[H, :, :], in0=rp[0][H, :].rearrange("p (a b) -> p a b", a=8),
                             in1=Pm[ci][H, :, :], op=ALU.add)
                    yield

            def post(c):
                q = c % 2
                KBT, VT, AT, PF = KBTs[q], VTs[q], ATs[q], PmFs[q]
                nKBT, nVT, nAT, PFn = "rk_KBT%d" % q, "rk_VT%d" % q, "rk_AT%d" % q, "rk_PmF%d" % q
                Rs3 = Rs[H, :].rearrange("p (a b) -> p a b", a=8)
                for h in range(8):
                    cc, h2 = h // 2, h % 2
                    rows = slice(h2 * 64, (h2 + 1) * 64)
                    hs = slice(h * 64, (h + 1) * 64)
                    K.mm(rp[6 + h2][H, cc * 64:(cc + 1) * 64], QR[rows, cc, c, 0, :], M[rows, cc, :], ["rk_QR", "rk_M"], [RP[6 + h2]])
                    K.mm(rp[4][H, hs], AT[0:64, h, 0:64], VT[0:64, cc, h2 * 64:(h2 + 1) * 64], [nAT, nVT], [RP[4]])
                for h2 in range(2):
                    K.op("act", "activation", [RP[6 + h2]], ["rk_Rs"], out=ev(Rs3, h2), in_=rp[6 + h2][H, 0:256].rearrange("p (a b) -> p a b", a=4), func=AF.Copy)
                K.op("dve", "tensor_tensor", [RP[4], "rk_Rs"], ["rk_Rs"], out=Rs[H, :], in0=rp[4][H, :], in1=Rs[H, :], op=ALU.add)
                yield
                for h in range(8):
                    hs = slice(h * 64, (h + 1) * 64)
                    K.mm(rp[5][H, hs], PF[H, h, :], Rs[H, hs], [PFn, "rk_Rs"], [RP[5]])
                K.op("act", "activation", [RP[5]], ["rk_Us"], out=Us[H, :], in_=rp[5][H, :], func=AF.Copy, scale=-1.0)
                yield
                for h in range(8):
                    cc, h2 = h // 2, h % 2
                    rows = slice(h2 * 64, (h2 + 1) * 64)
                    hs = slice(h * 64, (h + 1) * 64)
                    ys = slice(cc * 64, (cc + 1) * 64)
                    K.mm(rp[6 + h2][rows, ys], M[rows, cc, :], QR[rows, cc, c, 1, :], ["rk_M", "rk_QR"], [RP[6 + h2]])
                    K.mm(rp[4][rows, ys], VT[0:64, cc, h2 * 64:(h2 + 1) * 64], AT[0:64, h, 64:128], [nVT, nAT], [RP[4]])
                    K.mm(rp[5][rows, ys], Us[H, hs], AT[H, h, 64:128], ["rk_Us", nAT], [RP[5]])
                for h2 in range(2):
                    rows = slice(h2 * 64, (h2 + 1) * 64)
                    K.op("act", "activation", [RP[6 + h2]], ["rk_Yb"], out=Yb[rows, :, c * 64:(c + 1) * 64],
                         in_=rp[6 + h2][rows, 0:256].rearrange("p (a b) -> p a b", a=4), func=AF.Copy)
                yv = Yb[:, :, c * 64:(c + 1) * 64]
                K.op("dve", "tensor_tensor", [RP[4], "rk_Yb"], ["rk_Yb"], out=yv, in0=rp[4][:, 0:256].rearrange("p (a b) -> p a b", a=4), in1=yv, op=ALU.add)
                K.op("dve", "tensor_tensor", [RP[5], "rk_Yb"], ["rk_Yb"], out=yv, in0=rp[5][:, 0:256].rearrange("p (a b) -> p a b", a=4), in1=yv, op=ALU.add)
                yield
                for cc in range(4):
                    for h2 in range(2):
                        h = 2 * cc + h2
                        rows = slice(h2 * 64, (h2 + 1) * 64)
                        hs = slice(h * 64, (h + 1) * 64)
                        K.mm(rp[4][rows, cc * 64:(cc + 1) * 64], KBT[0:64, cc, rows], VT[0:64, cc, rows], [nKBT, nVT], [RP[4]])
                        K.mm(rp[5][rows, cc * 64:(cc + 1) * 64], KBT[H, cc, rows], Us[H, hs], [nKBT, "rk_Us"], [RP[5]])
                K.op("dve", "tensor_tensor", [RP[4], "rk_M"], ["rk_M"], out=M[:], in0=rp[4][:, 0:256].rearrange("p (a b) -> p a b", a=4), in1=M[:], op=ALU.add)
                K.op("dve", "tensor_tensor", [RP[5], "rk_M"], ["rk_M"], out=M[:], in0=rp[5][:, 0:256].rearrange("p (a b) -> p a b", a=4), in1=M[:], op=ALU.add)
                K.op("dve", "tensor_tensor", ["rk_M", "rk_gC"], ["rk_M"], out=M[:], in0=M[:],
                     in1=gC[:, :, c:c + 1].to_broadcast([128, 4, 64]), op=ALU.mult)
                yield

            for step in range(NCH + 1):
                gens = []
                if step >= 1:
                    gens.append(post(step - 1))
                if step < NCH:
                    gens.append(pre(step))
                while gens:
                    for g in list(gens):
                        try:
                            next(g)
                        except StopIteration:
                            gens.remove(g)
            chk(7)
            for cc in range(4):
                K.mm(rp[0][:, 0:TBK], bo64[:], Yb[:, cc, :], ["bo64", "rk_Yb"], [RP[0]])
                K.op("dve", "tensor_tensor", ["rk_Yb", RP[0]], ["rk_t1"], out=t1[:, cc, :], in0=Yb[:, cc, :], in1=rp[0][:, 0:TBK], op=ALU.subtract)
                K.op("pool", "tensor_tensor", ["rk_t1"], ["rk_kk"], out=kk[:, cc, :], in0=t1[:, cc, :], in1=t1[:, cc, :], op=ALU.mult)
                K.mm(rp[1][:, 0:TBK], bo64[:], kk[:, cc, :], ["bo64", "rk_kk"], [RP[1]])
                K.op("act", "activation", [RP[1], "rk_epsg"], ["rk_kp"], out=kp[:, cc, :], in_=rp[1][:, 0:TBK], func=AF.Sqrt, bias=epsg[:])
            K.op("dve", "reciprocal", ["rk_kp"], ["rk_kp"], out=kp[:], in_=kp[:])
            K.op("dve", "tensor_tensor", ["rk_t1", "rk_kp"], ["rk_t1"], out=t1[:], in0=t1[:], in1=kp[:], op=ALU.mult)
            K.op("pool", "tensor_tensor", ["rk_t1", "rk_lnw"], ["rk_t1"], out=t1[:], in0=t1[:], in1=bc(lnw), op=ALU.mult)
            K.op("pool", "tensor_tensor", ["rk_t1", "rk_lnb"], ["rk_t1"], out=t1[:], in0=t1[:], in1=bc(lnb), op=ALU.add)
            K.op("dve", "tensor_tensor", ["rk_t1", "rk_bon"], ["rk_t1"], out=t1[:], in0=t1[:], in1=bon[:], op=ALU.add)
            K.op("dve", "tensor_tensor", ["rk_t1", "rk_gg"], ["rk_yab"], out=yab[:], in0=t1[:], in1=gg[:], op=ALU.mult)
            K.dma("sp", SC["YA"][s, :, t0:t0 + TBK].rearrange("(c p) t -> p c t", p=128), yab[:], ["rk_yab"], ["YA"])


def norm_T(K, tag, src_tile, src_name, xn, ss, junk, pst, dstT, col0, identb, eps):
    K.op("act", "activation", [src_name], [tag + "junk", tag + "ss"], out=junk[:], in_=src_tile, func=AF.Square, accum_out=ss[:])
    K.op("act", "activation", [tag + "ss", "eps6"], [tag + "ss"], out=ss[:], in_=ss[:], func=AF.Sqrt, scale=1.0 / D, bias=eps[:])
    K.op("dve", "reciprocal", [tag + "ss"], [tag + "ss"], out=ss[:], in_=ss[:])
    K.op("dve", "tensor_scalar", [src_name, tag + "ss"], [tag + "xn"], out=xn[:], in0=src_tile, scalar1=ss[:], scalar2=None, op0=ALU.mult)
    for c in range(8):
        K.tr(pst[:, c, :], xn[:, c * 128:(c + 1) * 128], identb[:], [tag + "xn", "identb"], [tag + "pst"])
    K.op("act", "activation", [tag + "pst"], [dstT[1]], out=dstT[0][:, :, col0:col0 + 128], in_=pst[:], func=AF.Copy)


def phase_mix(K, s, T, X, Wd, SC, CONST):
    ZF = SC["ZF"]
    with ExitStack() as st:
        wpa = load_cast(K, st, "mx_wpa", Wd["w_proj_a"][0], 512, 1024)
        wpb = load_cast(K, st, "mx_wpb", Wd["w_proj_b"][0], 512, 1024)
        wout = load_cast(K, st, "mx_wout", Wd["w_out"][0], 1024, 1024)
        ps = [K.ps(st, "mx_ps%d" % i, [128, 512], F32) for i in range(4)]
        ya = K.sb(st, "mx_ya", [128, 4, 512], BF16)
        yb = K.sb(st, "mx_yb", [128, 4, 512], BF16)
        G = K.sb(st, "mx_G", [128, 16, 512], F32)
        ta = K.sb(st, "mx_ta", [128, 512], F32)
        tb_ = K.sb(st, "mx_tb", [128, 512], F32)
        mixT = K.sb(st, "mx_mixT", [128, 8, 512], BF16)
        xt = [K.sb(st, "mx_xt%d" % i, [128, D], F32) for i in range(2)]
        for tb in range(T // 512):
            t0 = tb * 512
            K.dma("sp", ya[:], SC["YA"][s, :, t0:t0 + 512].rearrange("(c p) t -> p c t", p=128), ["YA"], ["mx_ya"])
            K.dma("act", yb[:], SC["YB"][s, :, t0:t0 + 512].rearrange("(c p) t -> p c t", p=128), ["YB"], ["mx_yb"])
            K.dma("sp", G[:], ZF[s, R_G:R_G + 2048, t0:t0 + 512].rearrange("(c p) t -> p c t", p=128), ["ZF"], ["mx_G"])
            for cc in range(8):
                cs = slice(cc * 128, (cc + 1) * 128)
                for k in range(4):
                    K.mm(ps[0][:], wpa[:, k, cs], ya[:, k, :], ["mx_wpa", "mx_ya"], ["mx_ps0"], start=(k == 0), stop=(k == 3))
                for k in range(4):
                    K.mm(ps[1][:], wpb[:, k, cs], yb[:, k, :], ["mx_wpb", "mx_yb"], ["mx_ps1"], start=(k == 0), stop=(k == 3))
                K.op("dve", "tensor_tensor", ["mx_ps0", "mx_G"], ["mx_ta"], out=ta[:], in0=ps[0][:], in1=G[:, cc, :], op=ALU.mult)
                K.op("dve", "tensor_tensor", ["mx_ps1", "mx_G"], ["mx_tb"], out=tb_[:], in0=ps[1][:], in1=G[:, 8 + cc, :], op=ALU.mult)
                K.op("pool", "tensor_tensor", ["mx_ta", "mx_tb"], ["mx_mixT"], out=mixT[:, cc, :], in0=ta[:], in1=tb_[:], op=ALU.add)
            for tt in range(4):
                i = tt % 2
                r0 = s * T + t0 + tt * 128
                K.dma("act", xt[i][:], X[r0:r0 + 128, :], [], ["mx_xt%d" % i])
                for half in range(2):
                    pj = 2 + half
                    for k in range(8):
                        K.mm(ps[pj][:], mixT[:, k, tt * 128:(tt + 1) * 128], wout[:, k, half * 512:(half + 1) * 512], ["mx_mixT", "mx_wout"],
                             ["mx_ps%d" % pj], start=(k == 0), stop=(k == 7))
                    K.op("dve", "tensor_tensor", ["mx_ps%d" % pj, "mx_xt%d" % i], ["mx_xt%d" % i], out=xt[i][:, half * 512:(half + 1) * 512],
                         in0=ps[pj][:], in1=xt[i][:, half * 512:(half + 1) * 512], op=ALU.add)
                K.dma("sp", SC["H1"][r0:r0 + 128, :], xt[i][:], ["mx_xt%d" % i], ["H1"])


def colvec(K, st, tag, ap, n=8):
    t = K.sb(st, tag, [128, n], F32)
    K.dma("sp", t[:], ap.rearrange("o (c p) -> p (o c)", p=128), [], [tag], allow_slow_non_contiguous=True)
    return t


def phase_cross(K, s, T, MEM, Wd, SC, CONST):
    identb = CONST["identb"]
    with ExitStack() as st:
        nrc = colvec(K, st, "cx_nrc", Wd["norm_cross"])
        nrm = colvec(K, st, "cx_nrm", Wd["norm_mem"])
        wcq = load_cast(K, st, "cx_wcq", Wd["w_cq"][0], 1024, 1024, scale_col=(nrc, "cx_nrc"))
        wckv = load_cast(K, st, "cx_wckv", Wd["w_ckv"][0], 1024, 2048, scale_col=(nrm, "cx_nrm"))
        wco = load_cast(K, st, "cx_wco", Wd["w_co"][0], 1024, 1024)
        ps = [K.ps(st, "cx_ps%d" % i, [128, 512], F32) for i in range(6)]
        pst = K.ps(st, "cx_pst", [128, 8, 128], BF16)
        ht = K.sb(st, "cx_ht", [128, 4, D], F32)
        xn = K.sb(st, "cx_xn", [128, D], BF16)
        ss = K.sb(st, "cx_ss", [128, 1], F32)
        junk = K.sb(st, "cx_junk", [128, D], F32)
        memT = K.sb(st, "cx_memT", [128, 8, 256], BF16)
        ones = K.sb(st, "cx_ones", [128, 128], BF16)
        K.op("pool", "memset", [], ["cx_ones"], ap=ones[:], constant=1.0)
        for mt in range(2):
            K.dma("sp", ht[:, 0, :], MEM[s * 256 + mt * 128: s * 256 + (mt + 1) * 128, :], [], ["cx_ht0"])
            norm_T(K, "cx_", ht[:, 0, :], "cx_ht0", xn, ss, junk, pst, (memT, "cx_memT"), mt * 128, identb, CONST["eps6"])
        kTs = K.sb(st, "cx_kTs", [128, 8, 256], BF16)
        vS = K.sb(st, "cx_vS", [128, 2, 1024], BF16)
        for j in range(8):
            for k in range(8):
                K.mm(ps[0][:, 0:256], wckv[:, k, j * 128:(j + 1) * 128], memT[:, k, :], ["cx_wckv", "cx_memT"], ["cx_ps0"], start=(k == 0), stop=(k == 7))
            K.op("dve", "tensor_copy", ["cx_ps0"], ["cx_kTs"], out=kTs[:, j, :], in_=ps[0][:, 0:256])
        for mt in range(2):
            for half in range(2):
                for k in range(8):
                    K.mm(ps[1][:], memT[:, k, mt * 128:(mt + 1) * 128], wckv[:, k, 1024 + half * 512:1024 + (half + 1) * 512], ["cx_wckv", "cx_memT"],
                         ["cx_ps1"], start=(k == 0), stop=(k == 7))
                K.op("dve", "tensor_copy", ["cx_ps1"], ["cx_vS"], out=vS[:, mt, half * 512:(half + 1) * 512], in_=ps[1][:])
        hnT = K.sb(st, "cx_hnT", [128, 8, 512], BF16)
        qTs = K.sb(st, "cx_qTs", [128, 8, 512], BF16)
        pT = [K.sb(st, "cx_pT%d" % i, [128, 512], BF16) for i in range(2)]
        rden = K.sb(st, "cx_rden", [128, 512], F32)
        oT = K.sb(st, "cx_oT", [128, 8, 512], BF16)
        for tb in range(T // 512):
            t0 = tb * 512
            for tt in range(4):
                r0 = s * T + t0 + tt * 128
                K.dma("sp" if tt % 2 == 0 else "act", ht[:, tt, :], SC["H1"][r0:r0 + 128, :], ["H1"], ["cx_ht%d" % tt])
                norm_T(K, "cx_", ht[:, tt, :], "cx_ht%d" % tt, xn, ss, junk, pst, (hnT, "cx_hnT"), tt * 128, identb, CONST["eps6"])
            for j in range(8):
                pj = j % 2
                for k in range(8):
                    K.mm(ps[pj][:], wcq[:, k, j * 128:(j + 1) * 128], hnT[:, k, :], ["cx_wcq", "cx_hnT"], ["cx_ps%d" % pj], start=(k == 0), stop=(k == 7))
                if pj == 0:
                    K.op("dve", "tensor_copy", ["cx_ps0"], ["cx_qTs"], out=qTs[:, j, :], in_=ps[0][:])
                else:
                    K.op("act", "activation", ["cx_ps1"], ["cx_qTs"], out=qTs[:, j, :], in_=ps[1][:], func=AF.Copy)
            for h in range(4):
                for mt in range(2):
                    for dc in range(2):
                        K.mm(ps[2 + mt][:], kTs[:, 2 * h + dc, mt * 128:(mt + 1) * 128], qTs[:, 2 * h + dc, :], ["cx_kTs", "cx_qTs"],
                             ["cx_ps%d" % (2 + mt)], start=(dc == 0), stop=(dc == 1))
                    K.op("act", "activation", ["cx_ps%d" % (2 + mt)], ["cx_pT%d" % mt], out=pT[mt][:], in_=ps[2 + mt][:], func=AF.Exp, scale=1.0 / 16)
                for mt in range(2):
                    K.mm(ps[4][:], ones[:], pT[mt][:], ["cx_ones", "cx_pT%d" % mt], ["cx_ps4"], start=(mt == 0), stop=(mt == 1))
                K.op("dve", "reciprocal", ["cx_ps4"], ["cx_rden"], out=rden[:], in_=ps[4][:])
                for dc in range(2):
                    for mt in range(2):
                        K.mm(ps[5][:], vS[:, mt, h * 256 + dc * 128:h * 256 + (dc + 1) * 128], pT[mt][:], ["cx_vS", "cx_pT%d" % mt], ["cx_ps5"],
                             start=(mt == 0), stop=(mt == 1))
                    K.op("dve", "tensor_tensor", ["cx_ps5", "cx_rden"], ["cx_oT"], out=oT[:, 2 * h + dc, :], in0=ps[5][:], in1=rden[:], op=ALU.mult)
            for tt in range(4):
                r0 = s * T + t0 + tt * 128
                for half in range(2):
                    pj = half
                    for k in range(8):
                        K.mm(ps[pj][:], oT[:, k, tt * 128:(tt + 1) * 128], wco[:, k, half * 512:(half + 1) * 512], ["cx_oT", "cx_wco"],
                             ["cx_ps%d" % pj], start=(k == 0), stop=(k == 7))
                    K.op("dve", "tensor_tensor", ["cx_ps%d" % pj, "cx_ht%d" % tt], ["cx_ht%d" % tt], out=ht[:, tt, half * 512:(half + 1) * 512],
                         in0=ps[pj][:], in1=ht[:, tt, half * 512:(half + 1) * 512], op=ALU.add)
                K.dma("sp", SC["H1"][r0:r0 + 128, :], ht[:, tt, :], ["cx_ht%d" % tt], ["H1"])


def phase_moe(K, s, T, Wd, SC, CONST, OUT):
    identb = CONST["identb"]
    HT = min(T, 1024)
    NTL = HT // 128
    with ExitStack() as st:
        nrf = colvec(K, st, "mo_nrf", Wd["norm_ffn"])
        wrf = K.sb(st, "mo_wrf", [128, 8, 36], F32)
        K.dma("sp", wrf[:, :, 0:4], Wd["w_router_g"][0].rearrange("(c p) n -> p c n", p=128), [], ["mo_wrf"])
        K.dma("sp", wrf[:, :, 4:36], Wd["w_router_e"][0].rearrange("(c p) n -> p c n", p=128), [], ["mo_wrf"])
        wr = K.sb(st, "mo_wr", [128, 8, 36], BF16)
        K.op("dve", "tensor_tensor", ["mo_wrf", "mo_nrf"], ["mo_wr"], out=wr[:], in0=wrf[:], in1=nrf[:].unsqueeze(2).to_broadcast([128, 8, 36]), op=ALU.mult)
        brb = K.sb(st, "mo_brb", [128, 36], F32)
        K.dma("sp", brb[:, 0:4], Wd["b_router_g"].partition_broadcast(128), [], ["mo_brb"])
        K.dma("sp", brb[:, 4:36], Wd["b_router_e"].partition_broadcast(128), [], ["mo_brb"])
        nfb = K.sb(st, "mo_nfb", [128, D], F32)
        K.dma("sp", nfb[:], Wd["norm_final"].partition_broadcast(128), [], ["mo_nfb"])
        ps = [K.ps(st, "mo_ps%d" % i, [128, 512], F32) for i in range(7)]
        pst = K.ps(st, "mo_pst", [128, 8, 128], BF16)
        ht = K.sb(st, "mo_ht", [128, D], F32)
        xn = K.sb(st, "mo_xn", [128, D], BF16)
        ss = K.sb(st, "mo_ss", [128, 1], F32)
        junk = K.sb(st, "mo_junk", [128, D], F32)
        xT = K.sb(st, "mo_xT", [128, 8, HT], BF16)
        G = K.sb(st, "mo_G", [128, NTL, 32], F32)
        acc = K.sb(st, "mo_acc", [128, NTL, D], F32)
        lg = K.sb(st, "mo_lg", [128, 36], F32)
        cl = K.sb(st, "mo_cl", [128, 12], F32)
        lem = K.sb(st, "mo_lem", [128, 4, 8], F32)
        m8 = K.sb(st, "mo_m8", [128, 8], F32)
        sel = K.sb(st, "mo_sel", [128, 32], F32)
        ex = K.sb(st, "mo_ex", [128, 32], F32)
        stg = [K.sb(st, "mo_stg%d" % i, [128, 4096], F32) for i in range(2)]
        wg = [K.sb(st, "mo_wg%d" % i, [128, 8, 512], BF16) for i in range(2)]
        wu = [K.sb(st, "mo_wu%d" % i, [128, 8, 512], BF16) for i in range(2)]
        wd = [K.sb(st, "mo_wd%d" % i, [128, 4, 1024], BF16) for i in range(2)]
        sgts = [K.sb(st, "mo_sgt%d" % i, [128, 512], F32) for i in range(2)]
        hT = K.sb(st, "mo_hT", [128, 4, 512], BF16)
        tmp = [K.sb(st, "mo_tmp%d" % i, [128, 512], F32) for i in range(3)]
        for hf in range(T // HT):
            base = s * T + hf * HT
            for tl in range(NTL):
                r0 = base + tl * 128
                K.dma("sp", ht[:], SC["H1"][r0:r0 + 128, :], ["H1"], ["mo_ht"])
                norm_T(K, "mo_", ht[:], "mo_ht", xn, ss, junk, pst, (xT, "mo_xT"), tl * 128, identb, CONST["eps6"])
                for k in range(8):
                    K.mm(ps[0][:, 0:36], xT[:, k, tl * 128:(tl + 1) * 128], wr[:, k, :], ["mo_xT", "mo_wr"], ["mo_ps0"], start=(k == 0), stop=(k == 7))
                K.op("dve", "tensor_tensor", ["mo_ps0", "mo_brb"], ["mo_lg"], out=lg[:], in0=ps[0][:, 0:36], in1=brb[:], op=ALU.add)
                K.op("dve", "tensor_reduce", ["mo_lg"], ["mo_cl"], out=cl[:, 0:1], in_=lg[:, 0:4], axis=AX.X, op=ALU.max)
                K.op("dve", "tensor_scalar", ["mo_cl"], ["mo_cl"], out=cl[:, 1:2], in0=cl[:, 0:1], scalar1=-1.0, scalar2=None, op0=ALU.mult)
                K.op("act", "activation", ["mo_lg", "mo_cl"], ["mo_ex", "mo_cl"], out=ex[:, 0:4], in_=lg[:, 0:4], func=AF.Exp, bias=cl[:, 1:2], accum_out=cl[:, 2:3])
                K.op("dve", "reciprocal", ["mo_cl"], ["mo_cl"], out=cl[:, 3:4], in_=cl[:, 2:3])
                K.op("dve", "tensor_scalar", ["mo_lg", "mo_cl"], ["mo_sel"], out=sel[:, 0:4], in0=lg[:, 0:4], scalar1=cl[:, 0:1], scalar2=None, op0=ALU.is_ge)
                K.op("dve", "tensor_scalar", ["mo_sel"], ["mo_sel"], out=sel[:, 0:4], in0=sel[:, 0:4], scalar1=-1.0, scalar2=1e30, op0=ALU.add, op1=ALU.mult)
                K.op("dve", "tensor_tensor", ["mo_lg", "mo_sel"], ["mo_lem"], out=lem[:], in0=lg[:, 4:36].rearrange("p (a b) -> p a b", a=4),
                     in1=sel[:, 0:4].unsqueeze(2).to_broadcast([128, 4, 8]), op=ALU.add)
                lemf = lem[:].rearrange("p a b -> p (a b)")
                K.op("dve", "max", ["mo_lem"], ["mo_m8"], out=m8[:], in_=lemf)
                K.op("dve", "tensor_scalar", ["mo_lem", "mo_m8"], ["mo_sel"], out=sel[:], in0=lemf, scalar1=m8[:, 1:2], scalar2=None, op0=ALU.is_ge)
                K.op("dve", "tensor_scalar", ["mo_m8"], ["mo_cl"], out=cl[:, 4:5], in0=m8[:, 0:1], scalar1=-1.0, scalar2=None, op0=ALU.mult)
                K.op("act", "activation", ["mo_lem", "mo_cl"], ["mo_ex"], out=ex[:], in_=lemf, func=AF.Exp, bias=cl[:, 4:5])
                K.op("dve", "tensor_tensor", ["mo_ex", "mo_sel"], ["mo_ex"], out=ex[:], in0=ex[:], in1=sel[:], op=ALU.mult)
                K.op("dve", "tensor_reduce", ["mo_ex"], ["mo_cl"], out=cl[:, 5:6], in_=ex[:], axis=AX.X, op=ALU.add)
                K.op("dve", "reciprocal", ["mo_cl"], ["mo_cl"], out=cl[:, 6:7], in_=cl[:, 5:6])
                K.op("dve", "tensor_tensor", ["mo_cl"], ["mo_cl"], out=cl[:, 7:8], in0=cl[:, 6:7], in1=cl[:, 3:4], op=ALU.mult)
                K.op("dve", "tensor_scalar", ["mo_ex", "mo_cl"], ["mo_G"], out=G[:, tl, :], in0=ex[:], scalar1=cl[:, 7:8], scalar2=None, op0=ALU.mult)
            for e in range(32):
                i = e % 2
                nfb8 = nrf[:].unsqueeze(2).to_broadcast([128, 8, 512])
                K.dma("sp", stg[0][:].rearrange("p (c n) -> p c n", c=8), Wd["w_e_gate"][0, e].rearrange("(c p) n -> p c n", p=128), [], ["mo_stg0"])
                K.op("pool", "tensor_tensor", ["mo_stg0", "mo_nrf"], ["mo_wg%d" % i], out=wg[i][:], in0=stg[0][:].rearrange("p (c n) -> p c n", c=8), in1=nfb8, op=ALU.mult)
                K.dma("act", stg[1][:].rearrange("p (c n) -> p c n", c=8), Wd["w_e_up"][0, e].rearrange("(c p) n -> p c n", p=128), [], ["mo_stg1"])
                K.op("pool", "tensor_tensor", ["mo_stg1", "mo_nrf"], ["mo_wu%d" % i], out=wu[i][:], in0=stg[1][:].rearrange("p (c n) -> p c n", c=8), in1=nfb8, op=ALU.mult)
                K.dma("sp", stg[0][:].rearrange("p (c n) -> p c n", c=4), Wd["w_e_down"][0, e].rearrange("(c p) n -> p c n", p=128), [], ["mo_stg0"])
                K.op("pool", "tensor_copy", ["mo_stg0"], ["mo_wd%d" % i], out=wd[i][:], in_=stg[0][:].rearrange("p (c n) -> p c n", c=4))
                for bk in range(HT // 512):
                    bs = slice(bk * 512, (bk + 1) * 512)
                    for fc in range(4):
                        fs = slice(fc * 128, (fc + 1) * 128)
                        pg, pu = (0, 1) if fc % 2 == 0 else (4, 5)
                        sg_ = sgts[fc % 2]
                        sgn = "mo_sgt%d" % (fc % 2)
                        for k in range(8):
                            K.mm(ps[pg][:], wg[i][:, k, fs], xT[:, k, bs], ["mo_wg%d" % i, "mo_xT"], ["mo_ps%d" % pg], start=(k == 0), stop=(k == 7))
                        for k in range(8):
                            K.mm(ps[pu][:], wu[i][:, k, fs], xT[:, k, bs], ["mo_wu%d" % i, "mo_xT"], ["mo_ps%d" % pu], start=(k == 0), stop=(k == 7))
                        K.op("act", "activation", ["mo_ps%d" % pg], [sgn], out=sg_[:], in_=ps[pg][:], func=AF.Silu)
                        K.op("dve", "tensor_tensor", ["mo_ps%d" % pu, sgn], ["mo_hT%d" % fc], out=hT[:, fc, :], in0=ps[pu][:], in1=sg_[:], op=ALU.mult)
                    for tt in range(4):
                        tl = bk * 4 + tt
                        for half in range(2):
                            pj = (2, 3, 6)[(2 * tt + half) % 3]
                            for fc in range(4):
                                K.mm(ps[pj][:], hT[:, fc, tt * 128:(tt + 1) * 128], wd[i][:, fc, half * 512:(half + 1) * 512], ["mo_hT%d" % fc, "mo_wd%d" % i],
                                     ["mo_ps%d" % pj], start=(fc == 0), stop=(fc == 3))
                            hs = slice(half * 512, (half + 1) * 512)
                            if e == 0:
                                K.op("act", "activation", ["mo_ps%d" % pj, "mo_G"], ["mo_acc%d_%d" % (tl, half)], out=acc[:, tl, hs], in_=ps[pj][:], func=AF.Copy, scale=G[:, tl, e:e + 1])
                            else:
                                ti = (2 * tt + half) % 3
                                accn = "mo_acc%d_%d" % (tl, half)
                                K.op("act", "activation", ["mo_ps%d" % pj, "mo_G"], ["mo_tmp%d" % ti], out=tmp[ti][:], in_=ps[pj][:], func=AF.Copy, scale=G[:, tl, e:e + 1])
                                K.op("pool" if half == 0 else "dve", "tensor_tensor", ["mo_tmp%d" % ti, accn], [accn], out=acc[:, tl, hs], in0=acc[:, tl, hs], in1=tmp[ti][:], op=ALU.add)
            for tl in range(NTL):
                r0 = base + tl * 128
                K.dma("sp", ht[:], SC["H1"][r0:r0 + 128, :], ["H1"], ["mo_ht"])
                K.op("dve", "tensor_tensor", ["mo_ht", "mo_acc%d_0" % tl, "mo_acc%d_1" % tl], ["mo_ht"], out=ht[:], in0=ht[:], in1=acc[:, tl, :], op=ALU.add)
                K.op("act", "activation", ["mo_ht"], ["mo_junk", "mo_ss"], out=junk[:], in_=ht[:], func=AF.Square, accum_out=ss[:])
                K.op("act", "activation", ["mo_ss", "eps6"], ["mo_ss"], out=ss[:], in_=ss[:], func=AF.Sqrt, scale=1.0 / D, bias=CONST["eps6"][:])
                K.op("dve", "reciprocal", ["mo_ss"], ["mo_ss"], out=ss[:], in_=ss[:])
                K.op("dve", "scalar_tensor_tensor", ["mo_ht", "mo_ss", "mo_nfb"], ["mo_junk"], out=junk[:], in0=ht[:], scalar=ss[:], in1=nfb[:], op0=ALU.mult, op1=ALU.mult)
                K.dma("sp", OUT[r0:r0 + 128, :], junk[:], ["mo_junk"], ["OUT"])


I32 = mybir.dt.int32


def prepack_gen(K, st, Wd, SC):
    WGU, WDS = SC["WGU"], SC["WDS"]
    nrf = colvec(K, st, "pk_nrf", Wd["norm_ffn"])
    sg = K.sb(st, "pk_sg", [128, 8, 512], F32)
    su = K.sb(st, "pk_su", [128, 8, 512], F32)
    sd = K.sb(st, "pk_sd", [128, 4, 1024], F32)
    og = K.sb(st, "pk_og", [128, 8, 1024], BF16)
    od = K.sb(st, "pk_od", [128, 4, 1024], BF16)
    nf8 = nrf[:].unsqueeze(2).to_broadcast([128, 8, 512])
    def loads(e):
        K.dma("pool", sg[:], Wd["w_e_gate"][0, e].rearrange("(c p) n -> p c n", p=128), [], ["pk_sg"])
        K.dma("pool", su[:], Wd["w_e_up"][0, e].rearrange("(c p) n -> p c n", p=128), [], ["pk_su"])
        K.dma("pool", sd[:], Wd["w_e_down"][0, e].rearrange("(c p) n -> p c n", p=128), [], ["pk_sd"])

    loads(0)
    yield
    for e in range(32):
        K.op("dve", "tensor_tensor", ["pk_sg", "pk_nrf"], ["pk_og0"], out=og[:, :, 0:512], in0=sg[:], in1=nf8, op=ALU.mult)
        for c in range(8):
            K.op("act", "activation", ["pk_su", "pk_nrf"], ["pk_og1"], out=og[:, c, 512:1024], in_=su[:, c, :], func=AF.Copy, scale=nrf[:, c:c + 1])
        K.op("act", "activation", ["pk_sd"], ["pk_od"], out=od[:], in_=sd[:], func=AF.Copy)
        K.dma("pool", WGU[e * 1024:(e + 1) * 1024, :].rearrange("(c p) n -> p c n", p=128), og[:], ["pk_og0", "pk_og1"], ["WGU"])
        K.dma("pool", WDS[e * 512:(e + 1) * 512, :].rearrange("(c p) n -> p c n", p=128), od[:], ["pk_od"], ["WDS"])
        if e + 1 < 32:
            loads(e + 1)
        yield


def phase_moe_sparse(K, s, T, Wd, SC, CONST, OUT):
    nc = K.nc
    S = K.S
    identb = CONST["identb"]
    NTL = T // 128
    SB = 256
    NBLK = (2 * T) // SB + 32
    XS, YS = SC["XS"], SC["YS"]
    WG = Wd["w_e_gate"].rearrange("o e d f -> (o e d) f")
    WU = Wd["w_e_up"].rearrange("o e d f -> (o e d) f")
    WDN = Wd["w_e_down"].rearrange("o e f d -> (o e f) d")
    base = s * T
    with ExitStack() as st0:
        nrf = colvec(K, st0, "ms_nrf", Wd["norm_ffn"])
        GG = K.sb(st0, "ms_GG", [128, NTL, 2], F32)
        DST = K.sb(st0, "ms_DST", [128, NTL, 2], I32)
        IDXG = K.sb(st0, "ms_IDXG", [128, NBLK, 8], I32)
        IDXD = K.sb(st0, "ms_IDXD", [128, NBLK, 4], I32)
        with ExitStack() as st:
            wrf = K.sb(st, "ms_wrf", [128, 8, 36], F32)
            K.dma("sp", wrf[:, :, 0:4], Wd["w_router_g"][0].rearrange("(c p) n -> p c n", p=128), [], ["ms_wrf"])
            K.dma("sp", wrf[:, :, 4:36], Wd["w_router_e"][0].rearrange("(c p) n -> p c n", p=128), [], ["ms_wrf"])
            wr = K.sb(st, "ms_wr", [128, 8, 36], BF16)
            K.op("dve", "tensor_tensor", ["ms_wrf", "ms_nrf"], ["ms_wr"], out=wr[:], in0=wrf[:], in1=nrf[:].unsqueeze(2).to_broadcast([128, 8, 36]), op=ALU.mult)
            brb = K.sb(st, "ms_brb", [128, 36], F32)
            K.dma("sp", brb[:, 0:4], Wd["b_router_g"].partition_broadcast(128), [], ["ms_brb"])
            K.dma("sp", brb[:, 4:36], Wd["b_router_e"].partition_broadcast(128), [], ["ms_brb"])
            ps = [K.ps(st, "ms_ps%d" % i, [128, 512], F32) for i in range(2)]
            pst = K.ps(st, "ms_pst", [128, 8, 128], BF16)
            ht = K.sb(st, "ms_ht", [128, D], F32)
            ss = K.sb(st, "ms_ss", [128, 1], F32)
            junk = K.sb(st, "ms_junk", [128, D], F32)
            XN = K.sb(st, "ms_XN", [128, NTL, D], BF16)
            xT = K.sb(st, "ms_xT", [128, 8, 128], BF16)
            SEL = K.sb(st, "ms_SEL", [128, NTL, 2, 32], F32)
            RNK = K.sb(st, "ms_RNK", [128, NTL, 2], F32)
            carry = K.sb(st, "ms_carry", [128, 32], F32)
            K.op("dve", "memset", [], ["ms_carry"], ap=carry[:], constant=0.0)
            lg = K.sb(st, "ms_lg", [128, 36], F32)
            cl = K.sb(st, "ms_cl", [128, 12], F32)
            lem = K.sb(st, "ms_lem", [128, 32], F32)
            m8 = K.sb(st, "ms_m8", [128, 8], F32)
            s12 = K.sb(st, "ms_s12", [128, 32], F32)
            ex = K.sb(st, "ms_ex", [128, 32], F32)
            t32 = K.sb(st, "ms_t32", [128, 32], F32)
            utri, ones128, bstart, iotap = CONST["utri"], CONST["ones128"], CONST["bstart"], CONST["iotap"]
            GB = 8
            LG = K.sb(st, "ms_LG", [128, GB, 36], F32)
            LM = K.sb(st, "ms_LM", [128, GB, 32], F32)
            L2 = K.sb(st, "ms_L2", [128, GB, 32], F32)
            EX = K.sb(st, "ms_EX", [128, GB, 32], F32)
            S12 = K.sb(st, "ms_S12", [128, GB, 32], F32)
            RKt = K.sb(st, "ms_RKt", [128, GB, 32], F32)
            T4 = K.sb(st, "ms_T4", [128, GB, 4], F32)
            E4 = K.sb(st, "ms_E4", [128, GB, 4], F32)
            CG = K.sb(st, "ms_CG", [128, 8, GB], F32)
            hts = [ht, K.sb(st, "ms_ht1", [128, D], F32)]

            def b3(colv, n):
                return colv.unsqueeze(2).to_broadcast([128, GB, n])

            for g0 in range(0, NTL, GB):
                for gi in range(GB):
                    tl = g0 + gi
                    r0 = base + tl * 128
                    hh_ = hts[tl % 2]
                    hn = "ms_ht" if tl % 2 == 0 else "ms_ht1"
                    K.dma("sp" if tl % 2 == 0 else "act", hh_[:], SC["H1"][r0:r0 + 128, :], ["H1"], [hn])
                    K.op("act", "activation", [hn], ["ms_junk", "ms_ss"], out=junk[:], in_=hh_[:], func=AF.Square, accum_out=ss[:])
                    K.op("act", "activation", ["ms_ss", "eps6"], ["ms_ss"], out=ss[:], in_=ss[:], func=AF.Sqrt, scale=1.0 / D, bias=CONST["eps6"][:])
                    K.op("dve", "reciprocal", ["ms_ss"], ["ms_ss"], out=ss[:], in_=ss[:])
                    K.op("dve", "tensor_scalar", [hn, "ms_ss"], ["ms_XN%d" % tl], out=XN[:, tl, :], in0=hh_[:], scalar1=ss[:], scalar2=None, op0=ALU.mult)
                    for c in range(8):
                        K.tr(pst[:, c, :], XN[:, tl, c * 128:(c + 1) * 128], identb[:], ["ms_XN%d" % tl, "identb"], ["ms_pst"])
                    K.op("act", "activation", ["ms_pst"], ["ms_xT"], out=xT[:], in_=pst[:], func=AF.Copy)
                    for k in range(8):
                        K.mm(ps[0][:, 0:36], xT[:, k, :], wr[:, k, :], ["ms_xT", "ms_wr"], ["ms_ps0"], start=(k == 0), stop=(k == 7))
                    K.op("dve", "tensor_tensor", ["ms_ps0", "ms_brb"], ["ms_LG"], out=LG[:, gi, :], in0=ps[0][:, 0:36], in1=brb[:], op=ALU.add)
                K.op("dve", "tensor_reduce", ["ms_LG"], ["ms_CG"], out=CG[:, 0, :], in_=LG[:, :, 0:4], axis=AX.X, op=ALU.max)
                K.op("dve", "tensor_tensor", ["ms_LG", "ms_CG"], ["ms_T4"], out=T4[:], in0=LG[:, :, 0:4], in1=b3(CG[:, 0, :], 4), op=ALU.subtract)
                K.op("act", "activation", ["ms_T4"], ["ms_E4"], out=E4[:], in_=T4[:], func=AF.Exp)
                K.op("dve", "tensor_reduce", ["ms_E4"], ["ms_CG"], out=CG[:, 1, :], in_=E4[:], axis=AX.X, op=ALU.add)
                K.op("dve", "reciprocal", ["ms_CG"], ["ms_CG"], out=CG[:, 2, :], in_=CG[:, 1, :])
                K.op("dve", "tensor_scalar", ["ms_T4"], ["ms_T4"], out=T4[:], in0=T4[:], scalar1=0.0, scalar2=None, op0=ALU.is_ge)
                K.op("dve", "tensor_scalar", ["ms_T4"], ["ms_T4"], out=T4[:], in0=T4[:], scalar1=-1.0, scalar2=1e30, op0=ALU.add, op1=ALU.mult)
                K.op("dve", "tensor_tensor", ["ms_LG", "ms_T4"], ["ms_LM"], out=LM[:].rearrange("p g (a b) -> p g a b", a=4),
                     in0=LG[:, :, 4:36].rearrange("p g (a b) -> p g a b", a=4), in1=T4[:].unsqueeze(3).to_broadcast([128, GB, 4, 8]), op=ALU.add)
                K.op("dve", "tensor_reduce", ["ms_LM"], ["ms_CG"], out=CG[:, 3, :], in_=LM[:], axis=AX.X, op=ALU.max)
                sel1 = SEL[:, g0:g0 + GB, 0, :]
                sel2 = SEL[:, g0:g0 + GB, 1, :]
                K.op("dve", "tensor_tensor", ["ms_LM", "ms_CG"], ["ms_SEL"], out=sel1, in0=LM[:], in1=b3(CG[:, 3, :], 32), op=ALU.is_ge)
                K.op("dve", "scalar_tensor_tensor", ["ms_SEL", "ms_LM"], ["ms_L2"], out=L2[:], in0=sel1, scalar=-1e30, in1=LM[:], op0=ALU.mult, op1=ALU.add)
                K.op("dve", "tensor_reduce", ["ms_L2"], ["ms_CG"], out=CG[:, 4, :], in_=L2[:], axis=AX.X, op=ALU.max)
                K.op("dve", "tensor_tensor", ["ms_LM", "ms_CG"], ["ms_S12"], out=S12[:], in0=LM[:], in1=b3(CG[:, 4, :], 32), op=ALU.is_ge)
                K.op("dve", "tensor_tensor", ["ms_S12", "ms_SEL"], ["ms_SEL"], out=sel2, in0=S12[:], in1=sel1, op=ALU.subtract)
                K.op("dve", "tensor_tensor", ["ms_LM", "ms_CG"], ["ms_L2"], out=L2[:], in0=LM[:], in1=b3(CG[:, 3, :], 32), op=ALU.subtract)
                K.op("dve", "tensor_scalar", ["ms_L2"], ["ms_L2"], out=L2[:], in0=L2[:], scalar1=-80.0, scalar2=None, op0=ALU.max)
                K.op("act", "activation", ["ms_L2"], ["ms_EX"], out=EX[:], in_=L2[:], func=AF.Exp)
                K.op("dve", "tensor_tensor", ["ms_EX", "ms_S12"], ["ms_EX"], out=EX[:], in0=EX[:], in1=S12[:], op=ALU.mult)
                K.op("dve", "tensor_reduce", ["ms_EX"], ["ms_CG"], out=CG[:, 5, :], in_=EX[:], axis=AX.X, op=ALU.add)
                K.op("dve", "reciprocal", ["ms_CG"], ["ms_CG"], out=CG[:, 6, :], in_=CG[:, 5, :])
                K.op("dve", "tensor_tensor", ["ms_CG"], ["ms_CG"], out=CG[:, 6, :], in0=CG[:, 6, :], in1=CG[:, 2, :], op=ALU.mult)
                for kk_ in range(2):
                    K.op("dve", "tensor_tensor", ["ms_EX", "ms_SEL"], ["ms_L2"], out=L2[:], in0=EX[:], in1=SEL[:, g0:g0 + GB, kk_, :], op=ALU.mult)
                    K.op("dve", "tensor_reduce", ["ms_L2"], ["ms_CG"], out=CG[:, 7, :], in_=L2[:], axis=AX.X, op=ALU.add)
                    K.op("dve", "tensor_tensor", ["ms_CG"], ["ms_GG"], out=GG[:, g0:g0 + GB, kk_], in0=CG[:, 7, :], in1=CG[:, 6, :], op=ALU.mult)
                for gi in range(GB):
                    K.mm(ps[1][:, gi * 64:gi * 64 + 32], utri[:], S12[:, gi, :], ["utri", "ms_S12"], ["ms_ps1"])
                    K.mm(ps[1][:, gi * 64 + 32:gi * 64 + 64], ones128[:], S12[:, gi, :], ["ones128", "ms_S12"], ["ms_ps1"])
                for gi in range(GB):
                    K.op("dve", "tensor_tensor", ["ms_ps1", "ms_carry"], ["ms_RKt"], out=RKt[:, gi, :], in0=ps[1][:, gi * 64:gi * 64 + 32], in1=carry[:], op=ALU.add)
                    K.op("dve", "tensor_tensor", ["ms_ps1", "ms_carry"], ["ms_carry"], out=carry[:], in0=ps[1][:, gi * 64 + 32:gi * 64 + 64], in1=carry[:], op=ALU.add)
                for kk_ in range(2):
                    K.op("dve", "tensor_tensor", ["ms_RKt", "ms_SEL"], ["ms_L2"], out=L2[:], in0=RKt[:], in1=SEL[:, g0:g0 + GB, kk_, :], op=ALU.mult)
                    K.op("dve", "tensor_reduce", ["ms_L2"], ["ms_RNK"], out=RNK[:, g0:g0 + GB, kk_], in_=L2[:], axis=AX.X, op=ALU.add)
            ci = K.sb(st, "ms_ci", [128, 32], I32)
            pad = K.sb(st, "ms_pad", [128, 32], F32)
            pend = K.sb(st, "ms_pend", [128, 32], F32)
            pstart = K.sb(st, "ms_pstart", [128, 32], F32)
            ones32 = K.sb(st, "ms_ones32", [128, 32], F32)
            K.op("dve", "memset", [], ["ms_ones32"], ap=ones32[:], constant=1.0)
            K.op("dve", "tensor_scalar", ["ms_carry"], ["ms_ci"], out=ci[:], in0=carry[:], scalar1=float(SB - 1), scalar2=None, op0=ALU.add)
            K.op("dve", "tensor_scalar", ["ms_ci"], ["ms_ci"], out=ci[:], in0=ci[:], scalar1=8, scalar2=None, op0=ALU.arith_shift_right)
            K.op("dve", "tensor_scalar", ["ms_ci"], ["ms_ci"], out=ci[:], in0=ci[:], scalar1=8, scalar2=None, op0=ALU.logical_shift_left)
            K.op("dve", "tensor_copy", ["ms_ci"], ["ms_pad"], out=pad[:], in_=ci[:])
            K.op("dve", "tensor_tensor_scan", ["ms_pad", "ms_ones32"], ["ms_pend"], out=pend[:], data0=ones32[:], data1=pad[:], initial=0.0, op0=ALU.mult, op1=ALU.add)
            K.op("dve", "tensor_tensor", ["ms_pend", "ms_pad"], ["ms_pstart"], out=pstart[:], in0=pend[:], in1=pad[:], op=ALU.subtract)
            bst = K.sb(st, "ms_bst", [128, NBLK], F32)
            K.op("dve", "tensor_scalar", ["bstart"], ["ms_bst"], out=bst[:], in0=bstart[:, 0:NBLK], scalar1=float(SB // 128), scalar2=None, op0=ALU.mult)
            be = K.sb(st, "ms_be", [128, NBLK], F32)
            K.op("dve", "tensor_scalar", ["ms_bst", "ms_pend"], ["ms_be"], out=be[:], in0=bst[:], scalar1=pend[:, 0:1], scalar2=None, op0=ALU.is_ge)
            for e in range(1, 32):
                K.op("dve", "scalar_tensor_tensor", ["ms_bst", "ms_pend", "ms_be"], ["ms_be"], out=be[:], in0=bst[:], scalar=pend[:, e:e + 1], in1=be[:],
                     op0=ALU.is_ge, op1=ALU.add)
            K.op("dve", "tensor_scalar", ["ms_be"], ["ms_be"], out=be[:], in0=be[:], scalar1=31.0, scalar2=None, op0=ALU.min)
            bg = K.sb(st, "ms_bg", [128, NBLK], F32)
            bd = K.sb(st, "ms_bd", [128, NBLK], F32)
            K.op("dve", "tensor_scalar", ["ms_be", "iotap"], ["ms_bg"], out=bg[:], in0=be[:], scalar1=1024.0, scalar2=iotap[:, 0:1], op0=ALU.mult, op1=ALU.add)
            K.op("dve", "tensor_scalar", ["ms_be", "iotap"], ["ms_bd"], out=bd[:], in0=be[:], scalar1=512.0, scalar2=iotap[:, 0:1], op0=ALU.mult, op1=ALU.add)
            for c in range(8):
                K.op("dve", "tensor_scalar", ["ms_bg"], ["ms_IDXG"], out=IDXG[:, :, c], in0=bg[:], scalar1=float(c * 128), scalar2=None, op0=ALU.add)
            for c in range(4):
                K.op("dve", "tensor_scalar", ["ms_bd"], ["ms_IDXD"], out=IDXD[:, :, c], in0=bd[:], scalar1=float(c * 128), scalar2=None, op0=ALU.add)
            zt = K.sb(st, "ms_zt", [128, 4, D], BF16)
            K.op("pool", "memset", [], ["ms_zt"], ap=zt[:], constant=0.0)
            XSv = XS.rearrange("(b p) d -> p b d", p=128)
            for b0 in range(0, NBLK * SB // 128, 4):
                K.dma("sp" if (b0 // 4) % 2 == 0 else "act", XSv[:, b0:b0 + 4, :], zt[:], ["ms_zt"], ["XS"])
            TB3 = K.sb(st, "ms_TB3", [128, NTL, 32], F32)
            DF = K.sb(st, "ms_DF", [128, NTL], F32)
            for kk_ in range(2):
                K.op("dve", "tensor_tensor", ["ms_pstart", "ms_SEL"], ["ms_TB3"], out=TB3[:], in0=SEL[:, :, kk_, :],
                     in1=pstart[:].unsqueeze(1).to_broadcast([128, NTL, 32]), op=ALU.mult)
                K.op("dve", "tensor_reduce", ["ms_TB3"], ["ms_DF"], out=DF[:], in_=TB3[:], axis=AX.X, op=ALU.add)
                K.op("dve", "tensor_tensor", ["ms_DF", "ms_RNK"], ["ms_DST"], out=DST[:, :, kk_], in0=DF[:], in1=RNK[:, :, kk_], op=ALU.add)
            for tl in range(NTL):
                for kk_ in range(2):
                    S.dma("pool", None, None, K._bl(["ms_DST", "ms_XN%d" % tl, "XS"]), K._bl(["XSs_%d_%d" % (tl, kk_)]),
                          fn=lambda e, tl=tl, kk_=kk_: e.indirect_dma_start(out=XS, out_offset=bass.IndirectOffsetOnAxis(ap=DST[:, tl, kk_:kk_ + 1], axis=0),
                                                                        in_=XN[:, tl, :], in_offset=None))
        S.barrier()
        with ExitStack() as st:
            ps = [K.ps(st, "mb_ps%d" % i, [128, 512], F32) for i in range(6)]
            pst = K.ps(st, "mb_pst", [128, 8, 128], BF16)
            wgu = [K.sb(st, "mb_wgu%d" % i, [128, 8, 1024], BF16) for i in range(2)]
            wd = [K.sb(st, "mb_wd%d" % i, [128, 4, 1024], BF16) for i in range(2)]
            xb = [K.sb(st, "mb_xb%d" % i, [128, D], BF16) for i in range(2)]
            xT = K.sb(st, "mb_xT", [128, 8, 128], BF16)
            sgt = K.sb(st, "mb_sgt", [128, 512], F32)
            hb = K.sb(st, "mb_hb", [128, 512], BF16)
            hT = K.sb(st, "mb_hT", [128, 4, 128], BF16)
            ysb = [K.sb(st, "mb_ysb%d" % i, [128, D], F32) for i in range(2)]
            WGU, WDS = SC["WGU"], SC["WDS"]
            for b in range(NBLK):
                i = b % 2
                for c in range(8):
                    S.dma("pool", None, None, K._bl(["ms_IDXG"]), K._bl(["mb_wgu%d_%d" % (i, c)]),
                          fn=lambda e, b=b, c=c, i=i: e.indirect_dma_start(out=wgu[i][:, c, :], out_offset=None, in_=WGU,
                                                                         in_offset=bass.IndirectOffsetOnAxis(ap=IDXG[:, b, c:c + 1], axis=0)))
                for c in range(4):
                    S.dma("pool", None, None, K._bl(["ms_IDXD"]), K._bl(["mb_wd%d_%d" % (i, c)]),
                          fn=lambda e, b=b, c=c, i=i: e.indirect_dma_start(out=wd[i][:, c, :], out_offset=None, in_=WDS,
                                                                         in_offset=bass.IndirectOffsetOnAxis(ap=IDXD[:, b, c:c + 1], axis=0)))
                for sub in range(SB // 128):
                    j = sub % 2
                    r0 = b * SB + sub * 128
                    K.dma("sp", xb[j][:], XS[r0:r0 + 128, :], ["XS"], ["mb_xb%d" % j])
                    for c in range(8):
                        K.tr(pst[:, c, :], xb[j][:, c * 128:(c + 1) * 128], identb[:], ["mb_xb%d" % j, "identb"], ["mb_pst"])
                    K.op("act", "activation", ["mb_pst"], ["mb_xT"], out=xT[:], in_=pst[:], func=AF.Copy)
                    for k in range(8):
                        K.mm(ps[0][:], xT[:, k, :], wgu[i][:, k, 0:512], ["mb_xT"] + ["mb_wgu%d_%d" % (i, c) for c in range(8)], ["mb_ps0"], start=(k == 0), stop=(k == 7))
                    for k in range(8):
                        K.mm(ps[1][:], xT[:, k, :], wgu[i][:, k, 512:1024], ["mb_xT"] + ["mb_wgu%d_%d" % (i, c) for c in range(8)], ["mb_ps1"], start=(k == 0), stop=(k == 7))
                    K.op("act", "activation", ["mb_ps0"], ["mb_sgt"], out=sgt[:], in_=ps[0][:], func=AF.Silu)
                    K.op("dve", "tensor_tensor", ["mb_ps1", "mb_sgt"], ["mb_hb"], out=hb[:], in0=ps[1][:], in1=sgt[:], op=ALU.mult)
                    for fc in range(4):
                        K.tr(pst[:, fc, :], hb[:, fc * 128:(fc + 1) * 128], identb[:], ["mb_hb", "identb"], ["mb_pst"])
                    K.op("dve", "tensor_copy", ["mb_pst"], ["mb_hT"], out=hT[:], in_=pst[:, 0:4, :])
                    for half in range(2):
                        pj = 2 + 2 * j + half
                        for fc in range(4):
                            K.mm(ps[pj][:], hT[:, fc, :], wd[i][:, fc, half * 512:(half + 1) * 512], ["mb_hT"] + ["mb_wd%d_%d" % (i, c) for c in range(4)], ["mb_ps%d" % pj], start=(fc == 0), stop=(fc == 3))
                        if half == 0:
                            K.op("act", "activation", ["mb_ps%d" % pj], ["mb_ysb%d" % j], out=ysb[j][:, 0:512], in_=ps[pj][:], func=AF.Copy)
                        else:
                            K.op("dve", "tensor_copy", ["mb_ps%d" % pj], ["mb_ysb%d" % j], out=ysb[j][:, 512:1024], in_=ps[pj][:])
                    K.dma("act", YS[r0:r0 + 128, :], ysb[j][:], ["mb_ysb%d" % j], ["YS"])
        S.barrier()
        with ExitStack() as st:
            nfb = K.sb(st, "mc_nfb", [128, D], F32)
            K.dma("sp", nfb[:], Wd["norm_final"].partition_broadcast(128), [], ["mc_nfb"])
            hts = [K.sb(st, "mc_ht%d" % i, [128, D], F32) for i in range(2)]
            y1 = [K.sb(st, "mc_y1%d" % i, [128, D], F32) for i in range(2)]
            y2 = [K.sb(st, "mc_y2%d" % i, [128, D], F32) for i in range(2)]
            ob = [K.sb(st, "mc_ob%d" % i, [128, D], F32) for i in range(2)]
            junk = K.sb(st, "mc_junk", [128, D], F32)
            sss = [K.sb(st, "mc_ss%d" % i, [128, 1], F32) for i in range(2)]
            for tl in range(NTL):
                i = tl % 2
                r0 = base + tl * 128
                K.dma("sp", hts[i][:], SC["H1"][r0:r0 + 128, :], ["H1"], ["mc_ht%d" % i])
                S.dma("pool", None, None, K._bl(["ms_DST", "YS"]), K._bl(["mc_y1%d" % i]),
                      fn=lambda e, tl=tl, i=i: e.indirect_dma_start(out=y1[i][:], out_offset=None, in_=YS, in_offset=bass.IndirectOffsetOnAxis(ap=DST[:, tl, 0:1], axis=0)))
                S.dma("pool", None, None, K._bl(["ms_DST", "YS"]), K._bl(["mc_y2%d" % i]),
                      fn=lambda e, tl=tl, i=i: e.indirect_dma_start(out=y2[i][:], out_offset=None, in_=YS, in_offset=bass.IndirectOffsetOnAxis(ap=DST[:, tl, 1:2], axis=0)))
                K.op("dve", "scalar_tensor_tensor", ["mc_y1%d" % i, "ms_GG", "mc_ht%d" % i], ["mc_ht%d" % i], out=hts[i][:], in0=y1[i][:], scalar=GG[:, tl, 0:1], in1=hts[i][:],
                     op0=ALU.mult, op1=ALU.add)
                K.op("dve", "scalar_tensor_tensor", ["mc_y2%d" % i, "ms_GG", "mc_ht%d" % i], ["mc_ht%d" % i], out=hts[i][:], in0=y2[i][:], scalar=GG[:, tl, 1:2], in1=hts[i][:],
                     op0=ALU.mult, op1=ALU.add)
                K.op("act", "activation", ["mc_ht%d" % i], ["mc_junk", "mc_ss%d" % i], out=junk[:], in_=hts[i][:], func=AF.Square, accum_out=sss[i][:])
                K.op("act", "activation", ["mc_ss%d" % i, "eps6"], ["mc_ss%d" % i], out=sss[i][:], in_=sss[i][:], func=AF.Sqrt, scale=1.0 / D, bias=CONST["eps6"][:])
                K.op("dve", "reciprocal", ["mc_ss%d" % i], ["mc_ss%d" % i], out=sss[i][:], in_=sss[i][:])
                K.op("dve", "scalar_tensor_tensor", ["mc_ht%d" % i, "mc_ss%d" % i, "mc_nfb"], ["mc_ob%d" % i], out=ob[i][:], in0=hts[i][:], scalar=sss[i][:], in1=nfb[:],
                     op0=ALU.mult, op1=ALU.mult)
                K.dma("act", OUT[r0:r0 + 128, :], ob[i][:], ["mc_ob%d" % i], ["OUT"])


def build(T, NSEQ, stop_after=99, debug=False):
    nc = bass.Bass("TRN2", target_bir_lowering=False)
    NTOK = NSEQ * T

    def din(name, shape, dt=F32):
        return nc.dram_tensor(name, list(shape), dt, kind="ExternalInput").ap()

    X = din("x", [NTOK, D])
    MEM = din("mem", [NSEQ * 256, D])
    Wd = {}
    for name, shape in WSHAPES.items():
        Wd[name] = din(name, shape)
    identb_d = din("c_identb", [128, 128], BF16)
    identf_d = din("c_identf", [128, 128], F32)
    OUT = nc.dram_tensor("out", [NTOK, D], F32, kind="ExternalOutput").ap()
    SC = {}
    SC["ZF"] = nc.dram_tensor("sc_zf", [NSEQ, R_TOT, T], F32, kind="Internal").ap() if not debug else \
        nc.dram_tensor("sc_zf", [NSEQ, R_TOT, T], F32, kind="ExternalOutput").ap()
    kindd = "ExternalOutput" if debug else "Internal"
    SC["CK"] = nc.dram_tensor("sc_ck", [NSEQ, T, 128], BF16, kind=kindd).ap()
    SC["CKT"] = nc.dram_tensor("sc_ckt", [NSEQ, 128, T], BF16, kind=kindd).ap()
    SC["H1"] = nc.dram_tensor("sc_h1", [NTOK, D], F32, kind=kindd).ap()
    NSLOT = ((2 * T) // 256 + 32) * 256
    SC["WGU"] = nc.dram_tensor("sc_wgu", [32 * 1024, 1024], BF16, kind="Internal").ap()
    SC["WDS"] = nc.dram_tensor("sc_wds", [32 * 512, 1024], BF16, kind="Internal").ap()
    SC["XS"] = nc.dram_tensor("sc_xs", [NSLOT, D], BF16, kind="Internal").ap()
    SC["YS"] = nc.dram_tensor("sc_ys", [NSLOT, D], F32, kind="Internal").ap()
    SC["YB"] = nc.dram_tensor("sc_yb", [NSEQ, 512, T], BF16, kind=kindd).ap()
    SC["YA"] = nc.dram_tensor("sc_ya", [NSEQ, 512, T], BF16, kind=kindd).ap()
    cdram = {}
    for nm, arr in consts().items():
        if nm not in ("c_identb", "c_identf"):
            cdram[nm] = din(nm, arr.shape, BF16 if arr.dtype == ml_dtypes.bfloat16 else F32)
    with ExitStack() as st:
        S = Sched(nc, st)
        K = Ctx(nc, S)
        CONST = {}
        CONST["identb"] = K.sb(st, "identb", [128, 128], BF16)
        CONST["identf"] = K.sb(st, "identf", [128, 128], F32)
        CONST["eps6"] = K.sb(st, "eps6", [128, 1], F32)
        K.dma("sp", CONST["identb"][:], identb_d, [], ["identb"])
        K.dma("sp", CONST["identf"][:], identf_d, [], ["identf"])
        K.op("dve", "memset", [], ["eps6"], ap=CONST["eps6"][:], constant=1e-6)
        for nm, ap in cdram.items():
            sh = list(ap.shape)
            CONST[nm[2:]] = K.sb(st, nm[2:], sh, ap.dtype)
            K.dma("sp", CONST[nm[2:]][:], ap, [], [nm[2:]])
        for s in range(NSEQ):
            phase1(K, s, T, X, Wd, SC, CONST)
            S.barrier()
            if stop_after >= 2 and not os.environ.get("SKIP_DSA"):
                ex_ = (lambda st_: prepack_gen(K, st_, Wd, SC)) if (s == 0 and stop_after >= 6 and not os.environ.get("MOE_DENSE")) else None
                phase_dsa(K, s, T, Wd, SC, CONST, extra=ex_)
                S.barrier()
            if stop_after >= 3:
                phase_rwkv(K, s, T, Wd, SC, CONST)
                S.barrier()
            if stop_after >= 4:
                phase_mix(K, s, T, X, Wd, SC, CONST)
                S.barrier()
            if stop_after >= 5:
                phase_cross(K, s, T, MEM, Wd, SC, CONST)
                S.barrier()
            if stop_after >= 6:
                if os.environ.get("MOE_DENSE"):
                    phase_moe(K, s, T, Wd, SC, CONST, OUT)
                else:
                    phase_moe_sparse(K, s, T, Wd, SC, CONST, OUT)
                S.barrier()
        S.finish(list(K.B.values()))
        print("ops", S.nops, "waits", S.nwaits)
        S.emit()
    return nc


WSHAPES = {
    "norm_mix": [1, 1024], "w_in": [1, 1024, 4804], "shift_mu": [1, 1792], "rw_w0": [1, 512],
    "rw_w2": [1, 64, 512], "rw_a0": [1, 512], "rw_a2": [1, 64, 512], "rw_g2": [1, 128, 512],
    "rw_k_k": [1, 512], "rw_k_a": [1, 512], "rw_r_k": [1, 8, 64], "rw_ln_w": [1, 512], "rw_ln_b": [1, 512],
    "kv_norm": [1, 128], "w_uk": [1, 128, 8, 64], "w_uv": [1, 128, 8, 64], "w_proj_a": [1, 512, 1024],
    "w_proj_b": [1, 512, 1024], "b_gate": [1, 2048], "w_out": [1, 1024, 1024], "norm_cross": [1, 1024],
    "norm_mem": [1, 1024], "w_cq": [1, 1024, 1024], "w_ckv": [1, 1024, 2048], "w_co": [1, 1024, 1024],
    "norm_ffn": [1, 1024], "w_router_g": [1, 1024, 4], "b_router_g": [1, 4], "w_router_e": [1, 1024, 32],
    "b_router_e": [1, 32], "w_e_gate": [1, 32, 1024, 512], "w_e_up": [1, 32, 1024, 512],
    "w_e_down": [1, 32, 512, 1024], "norm_final": [1024],
}


def consts():
    return {
        "c_identb": np.eye(128, dtype=np.float32).astype(ml_dtypes.bfloat16),
        "c_identf": np.eye(128, dtype=np.float32),
        "c_tri01": (np.arange(128)[None, :] <= np.arange(128)[:, None]).astype(np.float32).astype(ml_dtypes.bfloat16),
        "c_negtri": np.where(np.arange(128)[None, :] <= np.arange(128)[:, None], 0.0, -1e30).astype(np.float32),
        "c_bo": np.kron(np.eye(2), np.ones((64, 64))).astype(np.float32),
        "c_bo64": (np.kron(np.eye(2), np.ones((64, 64))) / 64.0).astype(np.float32),
        "c_maskq": np.block([[np.triu(np.ones((64, 64)), 1), np.triu(np.ones((64, 64)), 0)],
                             [np.triu(np.ones((64, 64)), 1), np.triu(np.ones((64, 64)), 0)]]).astype(np.float32),
        "c_lowm": np.concatenate([np.zeros((64, 64)), np.tril(np.ones((64, 64)), -1)], 0).astype(np.float32),
        "c_resetm": np.tile((np.arange(256) % 64 != 0).astype(np.float32)[None, :], (128, 1)),
        "c_utri": (np.arange(128)[:, None] < np.arange(128)[None, :]).astype(np.float32),
        "c_ones128": np.ones((128, 128), np.float32),
        "c_bstart": np.tile((np.arange(320) * 128.0)[None, :], (128, 1)).astype(np.float32),
        "c_iotap": np.arange(128, dtype=np.float32)[:, None].copy(),
        "c_pw": np.tile((0.5 ** (np.arange(NIT) + 1))[None, :], (128, 1)).astype(np.float32),
    }


def kernel(**inputs):
    x = np.asarray(inputs["x"], dtype=np.float32)
    mem = np.asarray(inputs["mem"], dtype=np.float32)
    B, T, _ = x.shape
    nseq = B // NCORES
    nc = build(T, nseq)
    cs = consts()
    in_maps = []
    for c in range(NCORES):
        m = {"x": np.ascontiguousarray(x[c * nseq:(c + 1) * nseq].reshape(nseq * T, D)),
             "mem": np.ascontiguousarray(mem[c * nseq:(c + 1) * nseq].reshape(nseq * 256, D))}
        for name in WSHAPES:
            m[name] = np.ascontiguousarray(np.asarray(inputs[name], dtype=np.float32))
        m.update(cs)
        in_maps.append(m)
    res = run_bass_kernel_spmd(nc, in_maps, core_ids=list(range(NCORES)))
    out = np.concatenate([r["out"].reshape(nseq, T, D) for r in res.results], axis=0)
    return out.astype(np.float32)
```

```python
from contextlib import ExitStack
import os
import numpy as np
import ml_dtypes
import concourse.bass as bass
import concourse.mybir as mybir
from concourse.bass_utils import run_bass_kernel_spmd

F32 = mybir.dt.float32
BF16 = mybir.dt.bfloat16
AF = mybir.ActivationFunctionType
ALU = mybir.AluOpType
AX = mybir.AxisListType

D = 1024
NCORES = 8


class Buf:
    __slots__ = ("name", "w", "r")

    def __init__(self, name=""):
        self.name = name
        self.w = None
        self.r = {}


class Sched:
    ENG = ("pe", "act", "dve", "pool", "sp")

    def __init__(self, nc, stack, n_dma_sems=10):
        self.nc = nc
        self.streams = {e: [] for e in self.ENG}
        self.sems = {}
        self.count = {}
        for e in self.ENG:
            self.sems[e] = stack.enter_context(nc.semaphore("s_" + e))
            self.count[e] = 0
        self.dma_sems = {}
        self.dma_rr = {}
        for q in ("sp", "act", "pool"):
            lst = []
            for i in range(n_dma_sems if q != "pool" else 28):
                k = "d_%s_%d" % (q, i)
                self.sems[k] = stack.enter_context(nc.semaphore(k))
                self.count[k] = 0
                lst.append(k)
            self.dma_sems[q] = lst
            self.dma_rr[q] = 0
        self.waited = {}
        self.nwaits = 0
        self.nops = 0

    def _wait(self, eng, key, val):
        if val <= 0 or self.waited.get((eng, key), 0) >= val:
            return
        self.waited[(eng, key)] = val
        self.streams[eng].append(("w", key, val))
        self.nwaits += 1

    def _deps(self, eng, reads, writes, own_key):
        for b in reads:
            if b.w is not None:
                self._dep(eng, b.w, own_key)
        for b in writes:
            if b.w is not None:
                self._dep(eng, b.w, own_key)
            for k, v in b.r.items():
                self._dep(eng, (k, v), own_key)

    def _dep(self, eng, ev, own_key):
        k, v = ev
        if k == "pe" and own_key == "pe":
            return
        self._wait(eng, k, v)

    muted = False

    def op(self, eng, fn, reads=(), writes=()):
        if self.muted:
            return
        self._deps(eng, reads, writes, eng)
        self.count[eng] += 1
        v = self.count[eng]
        self.streams[eng].append(("o", fn, eng, 1))
        for b in writes:
            b.w = (eng, v)
            b.r = {}
        for b in reads:
            if b.r.get(eng, 0) < v:
                b.r[eng] = v
        self.nops += 1

    def dma(self, q, out, in_, reads=(), writes=(), fn=None, **kw):
        if self.muted:
            return
        lst = self.dma_sems[q]
        key = lst[self.dma_rr[q] % len(lst)]
        self.dma_rr[q] += 1
        self._wait(q, key, self.count[key])
        self._deps(q, reads, writes, key)
        self.count[key] += 16
        v = self.count[key]
        if fn is None:
            fn = lambda e, out=out, in_=in_, kw=kw: e.dma_start(out=out, in_=in_, **kw)
        self.streams[q].append(("o", fn, key, 16))
        for b in writes:
            b.w = (key, v)
            b.r = {}
        for b in reads:
            if b.r.get(key, 0) < v:
                b.r[key] = v
        self.nops += 1

    def barrier(self):
        for e in self.ENG:
            for k in self.sems:
                if k != e or True:
                    self._wait(e, k, self.count[k])

    def finish(self, bufs, eng="sp"):
        for b in bufs:
            if b.w is not None:
                self._wait(eng, b.w[0], b.w[1])

    def emit(self):
        nc = self.nc
        sems = self.sems
        streams = self.streams
        with nc.Block() as block:
            def run(engobj, lst):
                for it in lst:
                    if it[0] == "w":
                        engobj.wait_ge(sems[it[1]], it[2])
                    else:
                        it[1](engobj).then_inc(sems[it[2]], it[3])

            @block.tensor
            def _(e):
                run(e, streams["pe"])

            @block.scalar
            def _(e):
                run(e, streams["act"])

            @block.vector
            def _(e):
                run(e, streams["dve"])

            @block.gpsimd
            def _(e):
                run(e, streams["pool"])

            @block.sync
            def _(e):
                run(e, streams["sp"])


class Ctx:
    def __init__(self, nc, S):
        self.nc = nc
        self.S = S
        self.B = {}
        self.rr = 0
        self.uid = 0

    def buf(self, name):
        if name not in self.B:
            self.B[name] = Buf(name)
        return self.B[name]

    def _bl(self, lst):
        return [self.buf(x) if isinstance(x, str) else x for x in lst]

    def sb(self, st, name, shape, dt):
        self.uid += 1
        t = st.enter_context(self.nc.sbuf_tensor("%s_u%d" % (name, self.uid), list(shape), dt))
        self.buf(name)
        return t

    def ps(self, st, name, shape, dt):
        self.uid += 1
        t = st.enter_context(self.nc.psum_tensor("%s_u%d" % (name, self.uid), list(shape), dt))
        self.buf(name)
        return t

    def op(self, eng, method, reads, writes, **kw):
        self.S.op(eng, lambda e, m=method, kw=kw: getattr(e, m)(**kw), self._bl(reads), self._bl(writes))

    def mm(self, out, lhsT, rhs, reads, writes, start=True, stop=True, **kw):
        self.S.op("pe", lambda e: e.matmul(out, lhsT, rhs, start=start, stop=stop, **kw),
                  self._bl(reads), self._bl(writes))

    def tr(self, out, in_, ident, reads, writes):
        self.S.op("pe", lambda e: e.transpose(out, in_, ident), self._bl(reads), self._bl(writes))

    def dma(self, q, out, in_, reads, writes, **kw):
        self.S.dma(q, out, in_, self._bl(reads), self._bl(writes), **kw)

    def q(self):
        self.rr += 1
        return ("sp", "act", "pool")[self.rr % 3]


C_RW = 0
C_Q = 1792
C_CKV = 2304
C_QI = 2432
C_KI = 2688
C_WI = 2752
C_G = 2756
R_RW = 0
R_Q = 1792
R_QI = 2304
R_KI = 2560
R_G = 2624
R_WI = 4672
R_TOT = 4676


def load_cast(K, st, tag, w_ap, kin, n, scale_col=None, dt=BF16, engs=("dve", "pool")):
    nc = K.nc
    kc = kin // 128
    wt = K.sb(st, tag, [128, kc, n], dt)
    src = w_ap.rearrange("(c p) n -> p c n", p=128)
    if True:
        stg = [K.sb(st, "%s_stg%d" % (tag, i), [128, n], F32) for i in range(2)]
        for c in range(kc):
            sg = stg[c % 2]
            nm = "%s_stg%d" % (tag, c % 2)
            K.dma(K.q(), sg[:], src[:, c, :], [], [nm])
            eng = engs[c % len(engs)]
            if scale_col is None:
                K.op(eng, "tensor_copy", [nm], [tag], out=wt[:, c, :], in_=sg[:])
            else:
                K.op(eng, "tensor_scalar", [nm, scale_col[1]], [tag], out=wt[:, c, :], in0=sg[:],
                     scalar1=scale_col[0][:, c:c + 1], scalar2=None, op0=ALU.mult)
    return wt


def norm_rows(K, tag, xt, xt_name, ss, junk, eps_scale=1.0 / D):
    K.op("act", "activation", [xt_name], [tag + "_junk", tag + "_ss"], out=junk[:], in_=xt[:], func=AF.Square,
         accum_out=ss[:])
    K.op("act", "activation", [tag + "_ss"], [tag + "_ss"], out=ss[:], in_=ss[:], func=AF.Sqrt,
         scale=eps_scale, bias=1e-6)
    K.op("dve", "reciprocal", [tag + "_ss"], [tag + "_ss"], out=ss[:], in_=ss[:])


def phase1(K, s, T, X, Wd, SC, CONST):
    nc = K.nc
    NT = T // 128
    NB = T // 512
    with ExitStack() as st:
        xnT = K.sb(st, "p1_xnT", [128, 8, T], BF16)
        gm = K.sb(st, "p1_gm", [128, 8], F32)
        K.dma("sp", gm[:], Wd["norm_mix"].rearrange("o (c p) -> p (o c)", p=128), [], ["p1_gm"], allow_slow_non_contiguous=True)
        bg = K.sb(st, "p1_bg", [128, 16], F32)
        K.dma("sp", bg[:], Wd["b_gate"].rearrange("o (c p) -> p (o c)", p=128), [], ["p1_bg"], allow_slow_non_contiguous=True)
        identb = CONST["identb"]
        pst = K.ps(st, "p1_pst", [128, 8, 128], BF16)
        xts = [K.sb(st, "p1_xt%d" % i, [128, D], F32) for i in range(2)]
        xnb = [K.sb(st, "p1_xn%d" % i, [128, D], BF16) for i in range(2)]
        junk = K.sb(st, "p1_junk", [128, D], F32)
        sss = [K.sb(st, "p1_ss%d" % i, [128, 1], F32) for i in range(2)]
        for tt in range(NT):
            i = tt % 2
            xt, xn, ss = xts[i], xnb[i], sss[i]
            K.dma("sp" if i == 0 else "act", xt[:], X[s * T + tt * 128: s * T + (tt + 1) * 128, :], [], ["p1_xt%d" % i])
            K.op("act", "activation", ["p1_xt%d" % i], ["p1_junk", "p1_ss%d" % i], out=junk[:], in_=xt[:],
                 func=AF.Square, accum_out=ss[:])
            K.op("act", "activation", ["p1_ss%d" % i, "eps6"], ["p1_ss%d" % i], out=ss[:], in_=ss[:], func=AF.Sqrt,
                 scale=1.0 / D, bias=CONST["eps6"][:])
            K.op("dve", "reciprocal", ["p1_ss%d" % i], ["p1_ss%d" % i], out=ss[:], in_=ss[:])
            K.op("dve", "tensor_scalar", ["p1_xt%d" % i, "p1_ss%d" % i], ["p1_xn%d" % i], out=xn[:], in0=xt[:],
                 scalar1=ss[:], scalar2=None, op0=ALU.mult)
            for c in range(8):
                K.tr(pst[:, c, :], xn[:, c * 128:(c + 1) * 128], identb[:], ["p1_xn%d" % i, "identb"], ["p1_pst"])
            K.op("pool" if False else "act", "activation", ["p1_pst"], ["p1_xnT"], out=xnT[:, :, tt * 128:(tt + 1) * 128],
                 in_=pst[:], func=AF.Copy)
        import os
        STOP = int(os.environ.get("STOP", "99"))
        if STOP <= 1:
            return
        chunks = []
        for i in range(14):
            chunks.append((C_RW + i * 128, 128, R_RW + i * 128, "fm", None))
        for i in range(4):
            chunks.append((C_Q + i * 128, 128, R_Q + i * 128, "fm", None))
        for i in range(2):
            chunks.append((C_QI + i * 128, 128, R_QI + i * 128, "fm", None))
        chunks.append((C_KI, 64, R_KI, "fm", None))
        chunks.append((C_WI, 4, R_WI, "fm", None))
        for i in range(16):
            chunks.append((C_G + i * 128, 128, R_G + i * 128, "gate", i))
        wsrc = Wd["w_in"].rearrange("o (c p) n -> p (o c) n", p=128)
        wst = [K.sb(st, "p1_wst%d" % i, [128, 8, 132], F32) for i in range(2)]
        wbf = [K.sb(st, "p1_wbf%d" % i, [128, 8, 132], BF16) for i in range(2)]
        stage = [K.sb(st, "p1_stage%d" % i, [128, T], F32) for i in range(2)]
        pss = [K.ps(st, "p1_ps%d" % i, [128, 512], F32) for i in range(4)]
        gmb = gm[:].unsqueeze(2).to_broadcast([128, 8, 128])
        ZF = SC["ZF"]
        for ci, (c0, ncol, r0, kind, gi) in enumerate(chunks):
            i = ci % 2
            K.dma("sp" if i == 0 else "pool", wst[i][:, :, 0:ncol], wsrc[:, :, c0:c0 + ncol], [], ["p1_wst%d" % i])
            K.op("dve", "tensor_tensor", ["p1_wst%d" % i, "p1_gm"], ["p1_wbf%d" % i], out=wbf[i][:, :, 0:ncol],
                 in0=wst[i][:, :, 0:ncol], in1=gm[:].unsqueeze(2).to_broadcast([128, 8, ncol]), op=ALU.mult)
            for tb in range(NB):
                pj = (ci * NB + tb) % 4
                ps = pss[pj]
                for dc in range(8):
                    K.mm(ps[0:ncol, :], wbf[i][:, dc, 0:ncol], xnT[:, dc, tb * 512:(tb + 1) * 512],
                         ["p1_wbf%d" % i, "p1_xnT"], ["p1_ps%d" % pj], start=(dc == 0), stop=(dc == 7))
                if kind == "gate":
                    K.op("act", "activation", ["p1_ps%d" % pj, "p1_bg"], ["p1_stage%d" % i],
                         out=stage[i][0:ncol, tb * 512:(tb + 1) * 512], in_=ps[0:ncol, :], func=AF.Sigmoid,
                         bias=bg[:, gi:gi + 1])
                else:
                    eng = "dve" if tb % 2 == 0 else "act"
                    if eng == "dve":
                        K.op("dve", "tensor_copy", ["p1_ps%d" % pj], ["p1_stage%d" % i],
                             out=stage[i][0:ncol, tb * 512:(tb + 1) * 512], in_=ps[0:ncol, :])
                    else:
                        K.op("act", "activation", ["p1_ps%d" % pj], ["p1_stage%d" % i],
                             out=stage[i][0:ncol, tb * 512:(tb + 1) * 512], in_=ps[0:ncol, :], func=AF.Copy)
            K.dma("act" if i == 0 else "sp", ZF[s, r0:r0 + ncol, :], stage[i][0:ncol, :], ["p1_stage%d" % i], ["ZF"])
        if STOP <= 2:
            return
        i = len(chunks) % 2
        K.dma("sp", wst[i][:, :, 0:128], wsrc[:, :, C_CKV:C_CKV + 128], [], ["p1_wst%d" % i])
        K.op("dve", "tensor_tensor", ["p1_wst%d" % i, "p1_gm"], ["p1_wbf%d" % i], out=wbf[i][:, :, 0:128],
             in0=wst[i][:, :, 0:128], in1=gm[:].unsqueeze(2).to_broadcast([128, 8, 128]), op=ALU.mult)
        ck = [K.sb(st, "p1_ck%d" % j, [128, 128], F32) for j in range(2)]
        ckb = [K.sb(st, "p1_ckb%d" % j, [128, 128], BF16) for j in range(2)]
        ckT = K.sb(st, "p1_ckT", [128, T], BF16)
        for tt in range(NT):
            j = tt % 2
            pj = tt % 4
            ps = pss[pj]
            for dc in range(8):
                K.mm(ps[:, 0:128], xnT[:, dc, tt * 128:(tt + 1) * 128], wbf[i][:, dc, 0:128],
                     ["p1_wbf%d" % i, "p1_xnT"], ["p1_ps%d" % pj], start=(dc == 0), stop=(dc == 7))
            K.op("dve", "tensor_copy", ["p1_ps%d" % pj], ["p1_ck%d" % j], out=ck[j][:], in_=ps[:, 0:128])
            K.op("act", "activation", ["p1_ck%d" % j], ["p1_junk", "p1_ss%d" % j], out=junk[:, 0:128], in_=ck[j][:],
                 func=AF.Square, accum_out=sss[j][:])
            K.op("act", "activation", ["p1_ss%d" % j, "eps6"], ["p1_ss%d" % j], out=sss[j][:], in_=sss[j][:], func=AF.Sqrt,
                 scale=1.0 / 128, bias=CONST["eps6"][:])
            K.op("dve", "reciprocal", ["p1_ss%d" % j], ["p1_ss%d" % j], out=sss[j][:], in_=sss[j][:])
            K.op("dve", "tensor_scalar", ["p1_ck%d" % j, "p1_ss%d" % j], ["p1_ckb%d" % j], out=ckb[j][:], in0=ck[j][:],
                 scalar1=sss[j][:], scalar2=None, op0=ALU.mult)
            K.tr(pst[:, 0, :], ckb[j][:], identb[:], ["p1_ckb%d" % j, "identb"], ["p1_pst"])
            K.op("act", "activation", ["p1_pst"], ["p1_ckT"], out=ckT[:, tt * 128:(tt + 1) * 128], in_=pst[:, 0, :],
                 func=AF.Copy)
            K.dma("sp", SC["CK"][s, tt * 128:(tt + 1) * 128, :], ckb[j][:], ["p1_ckb%d" % j], ["CK"])
        K.dma("sp", SC["CKT"][s, :, :], ckT[:], ["p1_ckT"], ["CKT"])


NIT = 12


def phase_dsa(K, s, T, Wd, SC, CONST, extra=None):
    nc = K.nc
    NT = T // 128
    ZF = SC["ZF"]
    identb, identf = CONST["identb"], CONST["identf"]
    with ExitStack() as st:
        dps = [K.ps(st, "ds_ps%d" % i, [128, 512], F32) for i in range(4)]
        Ob = [K.ps(st, "ds_o%d" % i, [128, 3, 130], F32) for i in range(3)]
        MT = K.ps(st, "ds_mt", [128, 8, 128], BF16)
        wuk = K.sb(st, "ds_wuk", [128, 512], F32)
        K.dma("sp", wuk[:], Wd["w_uk"].rearrange("o r h d -> r (o h d)"), [], ["ds_wuk"])
        wukT = K.sb(st, "ds_wukT", [64, 8, 128], BF16)
        for h in range(8):
            K.tr(dps[3][0:64, 0:128], wuk[:, h * 64:(h + 1) * 64], identf[:], ["ds_wuk", "identf"], ["ds_ps3"])
            K.op("dve", "tensor_copy", ["ds_ps3"], ["ds_wukT"], out=wukT[:, h, :], in_=dps[3][0:64, 0:128])
        kvn = K.sb(st, "ds_kvn", [128, 1], F32)
        K.dma("sp", kvn[:], Wd["kv_norm"].rearrange("o r -> r o"), [], ["ds_kvn"], allow_slow_non_contiguous=True)
        kvn8 = K.sb(st, "ds_kvn8", [128, 1], F32)
        K.op("dve", "tensor_scalar", ["ds_kvn"], ["ds_kvn8"], out=kvn8[:], in0=kvn[:], scalar1=0.125, scalar2=None,
             op0=ALU.mult)
        wuv = K.sb(st, "ds_wuv", [128, 512], F32)
        K.dma("sp", wuv[:], Wd["w_uv"].rearrange("o r h d -> r (o h d)"), [], ["ds_wuv"])
        wuvb = K.sb(st, "ds_wuvb", [128, 512], BF16)
        K.op("dve", "tensor_scalar", ["ds_wuv", "ds_kvn"], ["ds_wuvb"], out=wuvb[:], in0=wuv[:], scalar1=kvn[:],
             scalar2=None, op0=ALU.mult)
        CKT = K.sb(st, "ds_ckt", [128, T], BF16)
        K.dma("sp", CKT[:], SC["CKT"][s, :, :], ["CKT"], ["ds_ckt"])
        CKA = K.sb(st, "ds_cka", [128, NT, 130], BF16)
        K.op("pool", "memset", [], ["ds_cka"], ap=CKA[:], constant=1.0)
        K.dma("sp", CKA[:, :, 0:128], SC["CK"][s, :, :].rearrange("(k p) r -> p k r", p=128), ["CK"], ["ds_cka"])
        kif = K.sb(st, "ds_kif", [64, T], F32)
        K.dma("act", kif[:], ZF[s, R_KI:R_KI + 64, :], ["ZF"], ["ds_kif"])
        kib = K.sb(st, "ds_kib", [64, T], BF16)
        K.op("dve", "tensor_copy", ["ds_kif"], ["ds_kib"], out=kib[:], in_=kif[:])
        qib = K.sb(st, "ds_qib", [64, 4, 128], BF16)
        zl = K.sb(st, "ds_zl", [128, 128], BF16)
        zb = K.sb(st, "ds_zb", [128, 390], BF16)
        K.op("pool", "memset", [], ["ds_zl"], ap=zl[:], constant=0.0)
        K.op("pool", "memset", [], ["ds_zb"], ap=zb[:], constant=0.0)
        tri01, negtri, pw = CONST["tri01"], CONST["negtri"], CONST["pw"]
        qf = K.sb(st, "ds_qf", [64, 8, 128], F32)
        qb = K.sb(st, "ds_qb", [64, 8, 128], BF16)
        qif = K.sb(st, "ds_qif", [64, 4, 128], F32)
        wif = K.sb(st, "ds_wif", [4, 128], F32)
        wit = K.sb(st, "ds_wit", [128, 4], F32)
        qlat = K.sb(st, "ds_qlat", [128, 1024], BF16)
        isc = K.sb(st, "ds_isc", [128, T], F32)
        junk = K.sb(st, "ds_junk", [128, T], BF16)
        rl = [K.sb(st, "ds_rl%d" % i, [128, 512], F32) for i in range(3)]
        maskb = K.sb(st, "ds_mask", [128, T], BF16)
        col = K.sb(st, "ds_col", [128, 8], F32)
        hk = K.sb(st, "ds_hk", [128, NIT], F32)
        junk2 = K.sb(st, "ds_junk2", [128, T], BF16)
        cola = K.sb(st, "ds_cola", [128, 1], F32)
        mts = [K.sb(st, "ds_mts%d" % i, [128, 128], BF16) for i in range(2)]
        ee = [K.sb(st, "ds_e%d" % i, [128, 4, 128], BF16) for i in range(4)]
        pp = [K.sb(st, "ds_p%d" % i, [128, 4, 128], BF16) for i in range(4)]
        rd = K.sb(st, "ds_rd", [128, 8, 1], F32)
        onb = K.sb(st, "ds_onb", [128, 8, 128], BF16)
        onT = K.sb(st, "ds_onT", [128, 8, 128], BF16)
        ybs = K.sb(st, "ds_ybs", [128, 4, 128], BF16)
        maskbs = [maskb, K.sb(st, "ds_mask1", [128, T], BF16)]
        MN = ["ds_mask", "ds_mask1"]

        def select(qt):
            t0 = qt * 128
            nk = qt + 1
            nkeys = nk * 128
            mb = maskbs[qt % 2]
            mn = MN[qt % 2]
            if qt >= 2:
                K.dma("act", qif[:], ZF[s, R_QI:R_QI + 256, t0:t0 + 128].rearrange("(h p) t -> p h t", p=64), ["ZF"], ["ds_qif"])
                K.op("act", "activation", ["ds_qif"], ["ds_qib"], out=qib[:], in_=qif[:], func=AF.Copy)
                K.dma("act", wif[:], ZF[s, R_WI:R_WI + 4, t0:t0 + 128], ["ZF"], ["ds_wif"])
                K.tr(dps[3][:, 0:4], wif[:], identf[0:4, 0:4], ["ds_wif", "identf"], ["ds_ps3"])
                K.op("dve", "tensor_scalar", ["ds_ps3"], ["ds_wit"], out=wit[:], in0=dps[3][:, 0:4], scalar1=1.0 / 16,
                     scalar2=None, op0=ALU.mult)
                yield
                for kb in range((nkeys + 511) // 512):
                    w = min(512, nkeys - kb * 512)
                    ks = slice(kb * 512, kb * 512 + w)
                    for h in range(4):
                        pb = 2 + (h % 2)
                        K.mm(dps[pb][:, 0:w], qib[:, h, :], kib[:, ks], ["ds_qib", "ds_kib"], ["ds_ps%d" % pb])
                        if h == 0:
                            K.op("dve", "tensor_scalar", ["ds_ps%d" % pb, "ds_wit"], ["ds_isc"], out=isc[:, ks], in0=dps[pb][:, 0:w],
                                 scalar1=0.0, scalar2=wit[:, 0:1], op0=ALU.max, op1=ALU.mult)
                        else:
                            K.op("act", "activation", ["ds_ps%d" % pb], ["ds_rl%d" % (h - 1)], out=rl[h - 1][:, 0:w],
                                 in_=dps[pb][:, 0:w], func=AF.Relu)
                            K.op("dve", "scalar_tensor_tensor", ["ds_rl%d" % (h - 1), "ds_wit", "ds_isc"], ["ds_isc"],
                                 out=isc[:, ks], in0=rl[h - 1][:, 0:w], scalar=wit[:, h:h + 1], in1=isc[:, ks],
                                 op0=ALU.mult, op1=ALU.add)
                    yield
                K.op("dve", "tensor_reduce", ["ds_isc"], ["ds_col"], out=col[:, 0:1], in_=isc[:, 0:nkeys], axis=AX.X, op=ALU.max)
                K.op("dve", "tensor_reduce", ["ds_isc"], ["ds_col"], out=col[:, 1:2], in_=isc[:, 0:nkeys], axis=AX.X, op=ALU.min)
                K.op("dve", "tensor_scalar", ["ds_col"], ["ds_col"], out=col[:, 2:3], in0=col[:, 0:1], scalar1=col[:, 1:2],
                     scalar2=2e-6, op0=ALU.subtract, op1=ALU.add)
                K.op("dve", "tensor_scalar", ["ds_col"], ["ds_col"], out=col[:, 3:4], in0=col[:, 1:2], scalar1=-1e-6,
                     scalar2=None, op0=ALU.add)
                K.op("dve", "tensor_scalar", ["pw", "ds_col"], ["ds_hk"], out=hk[:], in0=pw[:], scalar1=col[:, 2:3],
                     scalar2=None, op0=ALU.mult)
                K.op("dve", "tensor_tensor", ["ds_isc", "negtri"], ["ds_isc"], out=isc[:, t0:t0 + 128], in0=isc[:, t0:t0 + 128],
                     in1=negtri[:], op=ALU.add)
                K.op("dve", "tensor_tensor", ["ds_col", "ds_hk"], ["ds_col", "ds_colm"], out=col[:, 4:5], in0=col[:, 3:4], in1=hk[:, 0:1], op=ALU.add)
                yield
                nd = nkeys
                if nkeys >= 1024 and not os.environ.get("NO_ACTCNT"):
                    nd = ((nkeys * 5 // 8) // 128) * 128
                na = nkeys - nd
                for k in range(NIT):
                    K.op("dve", "tensor_scalar", ["ds_isc", "ds_colm"], ["ds_junk", "ds_col"], out=junk[:, 0:nd],
                         in0=isc[:, 0:nd], scalar1=col[:, 4:5], scalar2=None, op0=ALU.is_ge, op1=ALU.add,
                         accum_out=col[:, 5:6])
                    if na > 0:
                        K.op("act", "activation", ["ds_isc", "ds_colm"], ["ds_junk2", "ds_cola"], out=junk2[:, 0:na], in_=isc[:, nd:nkeys],
                             func=AF.Sign, scale=-1.0, bias=col[:, 4:5], accum_out=cola[:, 0:1])
                        K.op("dve", "scalar_tensor_tensor", ["ds_cola", "ds_col"], ["ds_col"], out=col[:, 5:6], in0=cola[:, 0:1], scalar=-0.5,
                             in1=col[:, 5:6], op0=ALU.mult, op1=ALU.add)
                    K.op("dve", "tensor_scalar", ["ds_col", "ds_hk"], ["ds_col"], out=col[:, 6:7], in0=col[:, 5:6],
                         scalar1=255.5 - 0.5 * na, scalar2=hk[:, k:k + 1], op0=ALU.is_ge, op1=ALU.mult)
                    kn = min(k + 1, NIT - 1)
                    dst = col[:, 4:5] if k < NIT - 1 else col[:, 3:4]
                    K.op("dve", "scalar_tensor_tensor", ["ds_col", "ds_colm", "ds_hk"], ["ds_col", "ds_colm"], out=dst, in0=col[:, 6:7], scalar=col[:, 4:5],
                         in1=hk[:, kn:kn + 1], op0=ALU.add, op1=ALU.subtract)
                    yield
                K.op("dve", "tensor_scalar", ["ds_isc", "ds_col"], [mn], out=mb[:, 0:nkeys], in0=isc[:, 0:nkeys],
                     scalar1=col[:, 3:4], scalar2=None, op0=ALU.is_ge)
            else:
                if qt > 0:
                    K.op("pool", "memset", [], [mn], ap=mb[:, 0:t0], constant=1.0)
                K.op("pool", "tensor_copy", ["tri01"], [mn], out=mb[:, t0:t0 + 128], in_=tri01[:])
            yield

        def attend(qt):
            t0 = qt * 128
            nk = qt + 1
            mb = maskbs[qt % 2]
            mn = MN[qt % 2]
            K.dma("sp", qf[:], ZF[s, R_Q:R_Q + 512, t0:t0 + 128].rearrange("(h p) t -> p h t", p=64), ["ZF"], ["ds_qf"])
            K.op("act", "activation", ["ds_qf"], ["ds_qb"], out=qb[:], in_=qf[:], func=AF.Copy)
            for h in range(8):
                K.mm(dps[h // 4][:, (h % 4) * 128:(h % 4 + 1) * 128], wukT[:, h, :], qb[:, h, :], ["ds_wukT", "ds_qb"],
                     ["ds_ps%d" % (h // 4)])
            for j in range(2):
                K.op("act", "activation", ["ds_ps%d" % j, "ds_kvn8"], ["ds_qlat"], out=qlat[:, j * 512:(j + 1) * 512],
                     in_=dps[j][:], func=AF.Copy, scale=kvn8[:, 0:1])
            for bq in range(3):
                K.mm(Ob[bq][:].rearrange("p a b -> p (a b)"), zl[:], zb[:], ["ds_zl", "ds_zb"], ["ds_o%d" % bq], start=True,
                     stop=False, skip_group_check=True)
            yield
            def front(kt):
                par = kt % 2
                K.tr(MT[:, 0, :], mb[:, kt * 128:(kt + 1) * 128], identb[:], [mn, "identb"], ["ds_mt0", "ds_mt1", "ds_mtall"])
                K.op("act", "activation", ["ds_mt0", "ds_mt1", "ds_mtall"], ["ds_mts%d" % par], out=mts[par][:], in_=MT[:, 0, :], func=AF.Copy)
                for j in range(2):
                    ej = 2 * par + j
                    K.mm(dps[j][:], CKT[:, kt * 128:(kt + 1) * 128], qlat[:, j * 512:(j + 1) * 512], ["ds_ckt", "ds_qlat"],
                         ["ds_ps%d" % j])
                    K.op("act", "activation", ["ds_ps%d" % j], ["ds_e%d" % ej], out=ee[ej][:],
                         in_=dps[j][:].rearrange("p (a b) -> p a b", a=4), func=AF.Exp)
            front(0)
            for kt in range(nk):
                par = kt % 2
                mtb = "ds_mt%d" % par
                if kt + 1 < nk:
                    front(kt + 1)
                for j in range(2):
                    ej = 2 * par + j
                    K.op("dve", "tensor_tensor", ["ds_e%d" % ej, "ds_mts%d" % par], ["ds_p%d" % ej], out=pp[ej][:], in0=ee[ej][:],
                         in1=mts[par][:].unsqueeze(1).to_broadcast([128, 4, 128]), op=ALU.mult)
                for j in range(2):
                    ej = 2 * par + j
                    for hh in range(4):
                        h = 4 * j + hh
                        K.mm(Ob[h // 3][:, h % 3, 0:129], pp[ej][:, hh, :], CKA[:, kt, 0:129], ["ds_p%d" % ej, "ds_cka"],
                             ["ds_o%d" % (h // 3)], start=False, stop=(kt == nk - 1), skip_group_check=True)
                yield
            for bq in range(3):
                nh = 3 if bq < 2 else 2
                K.op("dve", "reciprocal", ["ds_o%d" % bq], ["ds_rd"], out=rd[:, 3 * bq:3 * bq + nh, :], in_=Ob[bq][:, 0:nh, 128:129])
                K.op("dve", "tensor_tensor", ["ds_o%d" % bq, "ds_rd"], ["ds_onb"], out=onb[:, 3 * bq:3 * bq + nh, :],
                     in0=Ob[bq][:, 0:nh, 0:128], in1=rd[:, 3 * bq:3 * bq + nh, :].to_broadcast([128, nh, 128]), op=ALU.mult)
            for h in range(8):
                K.tr(MT[:, h, :], onb[:, h, :], identb[:], ["ds_onb", "identb"], ["ds_mt0", "ds_mt1", "ds_mtall"])
            K.op("act", "activation", ["ds_mt0", "ds_mt1", "ds_mtall"], ["ds_onT"], out=onT[:], in_=MT[:], func=AF.Copy)
            for h in range(8):
                K.mm(dps[0][(h % 2) * 64:(h % 2 + 1) * 64, (h // 2) * 128:(h // 2 + 1) * 128], wuvb[:, h * 64:(h + 1) * 64],
                     onT[:, h, :], ["ds_wuvb", "ds_onT"], ["ds_ps0"])
            K.op("dve", "tensor_copy", ["ds_ps0"], ["ds_ybs"], out=ybs[:], in_=dps[0][:].rearrange("p (a b) -> p a b", a=4))
            K.dma("sp", SC["YB"][s, :, t0:t0 + 128].rearrange("(c p) t -> p c t", p=128), ybs[:], ["ds_ybs"], ["YB"])
            yield

        xg = extra(st) if extra is not None else None
        for step in range(NT + 1):
            gens = []
            if step >= 1:
                gens.append(attend(step - 1))
            if step < NT:
                gens.append(select(step))
            if xg is not None:
                try:
                    next(xg)
                except StopIteration:
                    xg = None
            while gens:
                for g in list(gens):
                    try:
                        next(g)
                    except StopIteration:
                        gens.remove(g)
        if xg is not None:
            for _ in xg:
                pass


class _Stop(Exception):
    pass


def phase_rwkv(K, s, T, Wd, SC, CONST):
    _phase_rwkv(K, s, T, Wd, SC, CONST)
    K.S.muted = False


def _phase_rwkv(K, s, T, Wd, SC, CONST):
    nc = K.nc
    TBK = 256
    NCH = TBK // 64
    NBK = T // TBK
    ZF = SC["ZF"]
    identf = CONST["identf"]
    bo, bo64, maskq, lowm, resetm = CONST["bo"], CONST["bo64"], CONST["maskq"], CONST["lowm"], CONST["resetm"]
    with ExitStack() as st:
        rp = [K.ps(st, "rk_p%d" % i, [128, 512], F32) for i in range(8)]
        RP = ["rk_p%d" % i for i in range(8)]

        def colload(tag, ap512, n=4):
            t = K.sb(st, tag, [128, n], F32)
            K.dma("sp", t[:], ap512.rearrange("o (c p) -> p (o c)", p=128), [], [tag], allow_slow_non_contiguous=True)
            return t
        mu = colload("rk_mu", Wd["shift_mu"], 14)
        w0c = colload("rk_w0c", Wd["rw_w0"])
        a0c = colload("rk_a0c", Wd["rw_a0"])
        kkc = colload("rk_kkc", Wd["rw_k_k"])
        kac = colload("rk_kac", Wd["rw_k_a"])
        rkc = colload("rk_rkc", Wd["rw_r_k"].rearrange("o h d -> o (h d)"))
        lnw = colload("rk_lnw", Wd["rw_ln_w"])
        lnb = colload("rk_lnb", Wd["rw_ln_b"])
        w2a2 = K.sb(st, "rk_w2a2", [128, 512], F32)
        K.dma("sp", w2a2[0:64, :], Wd["rw_w2"][0], [], ["rk_w2a2"])
        K.dma("sp", w2a2[64:128, :], Wd["rw_a2"][0], [], ["rk_w2a2"])
        g2 = K.sb(st, "rk_g2", [128, 512], F32)
        K.dma("sp", g2[:], Wd["rw_g2"][0], [], ["rk_g2"])
        epsg = K.sb(st, "rk_epsg", [128, 1], F32)
        K.op("dve", "memset", [], ["rk_epsg"], ap=epsg[:], constant=64e-5)
        zin = K.sb(st, "rk_zin", [128, 14, TBK + 1], F32)
        zs = K.sb(st, "rk_zs", [128, 14, TBK], F32)
        tw = K.sb(st, "rk_tw", [128, TBK], F32)
        sg = K.sb(st, "rk_sg", [128, TBK], F32)

        def t4(tag):
            return K.sb(st, tag, [128, 4, TBK], F32)
        lw, aa, gg, LL, eL, enL, eLm, kk, t1, kp, bb, bon, Yb = [t4("rk_" + n) for n in
            ("lw", "aa", "gg", "LL", "eL", "enL", "eLm", "kk", "t1", "kp", "bb", "bon", "Yb")]
        QR = K.sb(st, "rk_QR", [128, 4, NCH, 2, 64], F32)
        KB = K.sb(st, "rk_KB", [128, 4, NCH, 2, 64], F32)
        gC = K.sb(st, "rk_gC", [128, 4, NCH], F32)
        M = K.sb(st, "rk_M", [128, 4, 64], F32)
        K.op("dve", "memset", [], ["rk_M"], ap=M[:], constant=0.0)
        KBTs = [K.sb(st, "rk_KBT%d" % i, [128, 4, 128], F32) for i in range(2)]
        VTs = [K.sb(st, "rk_VT%d" % i, [64, 4, 128], F32) for i in range(2)]
        ATs = [K.sb(st, "rk_AT%d" % i, [128, 8, 128], F32) for i in range(2)]
        DDT = BF16 if os.environ.get("RW_BF16", "1") == "1" else F32
        Am = [K.sb(st, "rk_Am%d" % i, [128, 8, 64], DDT) for i in range(2)]
        Bm = [K.sb(st, "rk_Bm%d" % i, [128, 8, 64], DDT) for i in range(2)]
        Pm = [K.sb(st, "rk_Pm%d" % i, [128, 8, 64], DDT) for i in range(2)]
        PmFs = [K.sb(st, "rk_PmF%d" % i, [128, 8, 64], F32) for i in range(2)]
        Rs = K.sb(st, "rk_Rs", [128, 512], F32)
        Us = K.sb(st, "rk_Us", [128, 512], F32)
        yab = K.sb(st, "rk_yab", [128, 4, TBK], BF16)
        H = slice(64, 128)

        def v4(t):
            return t[:].rearrange("p c (n t) -> p c n t", t=64)

        def bc(colt, n=4, w=TBK):
            return colt[:].unsqueeze(2).to_broadcast([128, n, w])

        RS = float(os.environ.get("RSTOP", "99"))

        def chk(k):
            if RS <= k:
                K.S.muted = True

        for tb in range(NBK):
            t0 = tb * TBK
            if tb == 0:
                K.op("dve", "memset", [], ["rk_zin"], ap=zin[:, :, 0:1], constant=0.0)
                K.dma("sp", zin[:, :, 1:TBK + 1], ZF[s, 0:1792, 0:TBK].rearrange("(c p) t -> p c t", p=128), ["ZF"], ["rk_zin"])
            else:
                K.dma("sp", zin[:, :, :], ZF[s, 0:1792, t0 - 1:t0 + TBK].rearrange("(c p) t -> p c t", p=128), ["ZF"], ["rk_zin"])
            K.op("dve", "tensor_tensor", ["rk_zin"], ["rk_zs"], out=zs[:], in0=zin[:, :, 0:TBK], in1=zin[:, :, 1:TBK + 1], op=ALU.subtract)
            for c14 in range(14):
                K.op("dve", "scalar_tensor_tensor", ["rk_zs", "rk_mu", "rk_zin"], ["rk_zs"], out=zs[:, c14, :], in0=zs[:, c14, :], scalar=mu[:, c14:c14 + 1],
                     in1=zin[:, c14, 1:TBK + 1], op0=ALU.mult, op1=ALU.add)
            chk(1)
            r_, k_, v_ = zs[:, 0:4, :], zs[:, 4:8, :], zs[:, 8:12, :]
            K.op("act", "activation", ["rk_zs"], ["rk_tw"], out=tw[0:64, :], in_=zs[0:64, 12, :], func=AF.Tanh)
            K.op("act", "activation", ["rk_zs"], ["rk_sg"], out=sg[:], in_=zs[:, 13, :], func=AF.Sigmoid)
            for cc in range(4):
                cs = slice(cc * 128, (cc + 1) * 128)
                K.mm(rp[0][:, 0:TBK], w2a2[0:64, cs], tw[0:64, :], ["rk_w2a2", "rk_tw"], [RP[0]])
                K.op("act", "activation", [RP[0], "rk_w0c"], ["rk_lw"], out=lw[:, cc, :], in_=rp[0][:, 0:TBK], func=AF.Sigmoid, bias=w0c[:, cc:cc + 1])
                K.mm(rp[1][:, 0:TBK], w2a2[H, cs], zs[H, 12, :], ["rk_w2a2", "rk_zs"], [RP[1]])
                K.op("act", "activation", [RP[1], "rk_a0c"], ["rk_aa"], out=aa[:, cc, :], in_=rp[1][:, 0:TBK], func=AF.Sigmoid, bias=a0c[:, cc:cc + 1])
                K.mm(rp[2][:, 0:TBK], g2[:, cs], sg[:], ["rk_g2", "rk_sg"], [RP[2]])
                K.op("dve", "tensor_copy", [RP[2]], ["rk_gg"], out=gg[:, cc, :], in_=rp[2][:, 0:TBK])
            chk(2)
            K.op("dve", "tensor_scalar", ["rk_lw"], ["rk_lw"], out=lw[:], in0=lw[:], scalar1=-0.6065306597126334, scalar2=None, op0=ALU.mult)
            for cc in range(4):
                K.op("dve", "tensor_tensor_scan", ["rk_lw", "resetm"], ["rk_LL"], out=LL[:, cc, :], data0=resetm[:], data1=lw[:, cc, :],
                     initial=0.0, op0=ALU.mult, op1=ALU.add)
            K.op("act", "activation", ["rk_LL"], ["rk_eL"], out=eL[:], in_=LL[:], func=AF.Exp)
            K.op("act", "activation", ["rk_LL"], ["rk_enL"], out=enL[:], in_=LL[:], func=AF.Exp, scale=-1.0)
            K.op("dve", "tensor_tensor", ["rk_LL", "rk_lw"], ["rk_t1"], out=t1[:], in0=LL[:], in1=lw[:], op=ALU.subtract)
            K.op("act", "activation", ["rk_t1"], ["rk_eLm"], out=eLm[:], in_=t1[:], func=AF.Exp)
            K.op("dve", "tensor_tensor", ["rk_zs", "rk_kkc"], ["rk_kk"], out=kk[:], in0=k_, in1=bc(kkc), op=ALU.mult)
            K.op("pool", "tensor_tensor", ["rk_kk"], ["rk_t1"], out=t1[:], in0=kk[:], in1=kk[:], op=ALU.mult)
            for cc in range(4):
                K.mm(rp[cc % 4][:, 0:TBK], bo[:], t1[:, cc, :], ["bo", "rk_t1"], [RP[cc % 4]])
                K.op("act", "activation", [RP[cc % 4]], ["rk_kp"], out=kp[:, cc, :], in_=rp[cc % 4][:, 0:TBK], func=AF.Sqrt)
            K.op("dve", "tensor_scalar", ["rk_kp"], ["rk_kp"], out=kp[:], in0=kp[:], scalar1=1e-12, scalar2=None, op0=ALU.max)
            K.op("dve", "reciprocal", ["rk_kp"], ["rk_kp"], out=kp[:], in_=kp[:])
            K.op("dve", "tensor_tensor", ["rk_kk", "rk_kp"], ["rk_kk"], out=kk[:], in0=kk[:], in1=kp[:], op=ALU.mult)
            for cc in range(4):
                K.op("dve", "tensor_scalar", ["rk_aa", "rk_kac"], ["rk_t1"], out=t1[:, cc, :], in0=aa[:, cc, :], scalar1=-1.0, scalar2=kac[:, cc:cc + 1],
                     op0=ALU.add, op1=ALU.mult)
            K.op("dve", "scalar_tensor_tensor", ["rk_t1", "rk_zs"], ["rk_kp"], out=kp[:], in0=t1[:], scalar=1.0, in1=k_, op0=ALU.add, op1=ALU.mult)
            K.op("pool", "tensor_tensor", ["rk_kk", "rk_aa"], ["rk_bb"], out=bb[:], in0=kk[:], in1=aa[:], op=ALU.mult)
            K.op("dve", "tensor_tensor", ["rk_zs", "rk_eL"], ["rk_QR"], out=QR[:, :, :, 1, :], in0=r_.rearrange("p c (n t) -> p c n t", t=64), in1=v4(eL), op=ALU.mult)
            K.op("dve", "tensor_tensor", ["rk_kk", "rk_eLm"], ["rk_QR"], out=QR[:, :, :, 0, :], in0=v4(kk), in1=v4(eLm), op=ALU.mult)
            K.op("dve", "tensor_tensor", ["rk_kp", "rk_enL"], ["rk_KB"], out=KB[:, :, :, 0, :], in0=v4(kp), in1=v4(enL), op=ALU.mult)
            K.op("pool", "tensor_tensor", ["rk_bb", "rk_enL"], ["rk_KB"], out=KB[:, :, :, 1, :], in0=v4(bb), in1=v4(enL), op=ALU.mult)
            K.op("dve", "tensor_copy", ["rk_eL"], ["rk_gC"], out=gC[:], in_=v4(eL)[:, :, :, 63])
            K.op("pool", "tensor_tensor", ["rk_zs", "rk_kp"], ["rk_t1"], out=t1[:], in0=r_, in1=kp[:], op=ALU.mult)
            K.op("pool", "tensor_tensor", ["rk_t1", "rk_rkc"], ["rk_t1"], out=t1[:], in0=t1[:], in1=bc(rkc), op=ALU.mult)
            for cc in range(4):
                K.mm(rp[cc % 4][:, 0:TBK], bo[:], t1[:, cc, :], ["bo", "rk_t1"], [RP[cc % 4]])
                K.op("dve", "tensor_tensor", [RP[cc % 4], "rk_zs"], ["rk_bon"], out=bon[:, cc, :], in0=rp[cc % 4][:, 0:TBK], in1=zs[:, 8 + cc, :], op=ALU.mult)
            chk(3)
            def ev(t, par):
                return t.rearrange("p (a two) b -> p a two b", two=2)[:, :, par, :]

            def pre(c):
                q = c % 2
                KBT, VT, AT, PmF = KBTs[q], VTs[q], ATs[q], PmFs[q]
                nKBT, nVT, nAT, nPmF = "rk_KBT%d" % q, "rk_VT%d" % q, "rk_AT%d" % q, "rk_PmF%d" % q
                for cc in range(4):
                    K.tr(rp[0][:, cc * 128:(cc + 1) * 128], KB[:, cc, c, :, :].rearrange("p a b -> p (a b)"), identf[:], ["rk_KB", "identf"], [RP[0]])
                    K.tr(rp[1][0:64, cc * 128:(cc + 1) * 128], zs[:, 8 + cc, c * 64:(c + 1) * 64], identf[:], ["rk_zs", "identf"], [RP[1]])
                K.op("act", "activation", [RP[0]], [nKBT], out=KBT[:].rearrange("p a b -> p (a b)"), in_=rp[0][:], func=AF.Copy)
                K.op("dve", "tensor_copy", [RP[1]], [nVT], out=VT[:].rearrange("p a b -> p (a b)"), in_=rp[1][0:64, :])
                yield
                for h in range(8):
                    cc, h2 = h // 2, h % 2
                    rows = slice(h2 * 64, (h2 + 1) * 64)
                    K.mm(rp[2 + h2][:, cc * 128:(cc + 1) * 128], KB[rows, cc, c, :, :].rearrange("p a b -> p (a b)"),
                         QR[rows, cc, c, :, :].rearrange("p a b -> p (a b)"), ["rk_KB", "rk_QR"], [RP[2 + h2]])
                    K.mm(rp[h2][H, cc * 64:(cc + 1) * 64], QR[rows, cc, c, 0, :], KB[rows, cc, c, 1, :], ["rk_QR", "rk_KB"], [RP[h2]])
                for h2 in range(2):
                    K.op("dve", "tensor_tensor", [RP[2 + h2], "maskq"], [nAT], out=ev(AT[:], h2),
                         in0=rp[2 + h2][:].rearrange("p (a b) -> p a b", a=4), in1=maskq[:].unsqueeze(1).to_broadcast([128, 4, 128]), op=ALU.mult)
                    K.op("dve", "tensor_tensor", [RP[h2], "lowm"], ["rk_Bm0"], out=ev(Bm[0][H, :, :], h2),
                         in0=rp[h2][H, 0:256].rearrange("p (a b) -> p a b", a=4), in1=lowm[H, :].unsqueeze(1).to_broadcast([64, 4, 64]), op=ALU.mult)
                K.op("act", "activation", [nAT], ["rk_Am0"], out=Am[0][H, :, :], in_=AT[H, :, 0:64], func=AF.Copy)
                K.op("dve", "tensor_tensor", ["identf", nAT], ["rk_Pm0"], out=Pm[0][H, :, :],
                     in0=identf[H, 64:128].unsqueeze(1).to_broadcast([64, 8, 64]), in1=AT[H, :, 0:64], op=ALU.subtract)
                yield
                for lvl in range(5):
                    ci, ni = lvl % 2, (lvl + 1) % 2
                    An, Bn, Pn = "rk_Am%d" % ni, "rk_Bm%d" % ni, "rk_Pm%d" % ni
                    Ac, Bc, Pc = "rk_Am%d" % ci, "rk_Bm%d" % ci, "rk_Pm%d" % ci
                    for h in range(8):
                        hs = slice(h * 64, (h + 1) * 64)
                        if lvl < 4:
                            K.mm(rp[2][H, hs], Bm[ci][H, h, :], Am[ci][H, h, :], [Ac, Bc], [RP[2]])
                        K.mm(rp[3][H, hs], Am[ci][H, h, :], Bm[ci][H, h, :], [Ac, Bc], [RP[3]])
                    if lvl < 4:
                        K.op("act", "activation", [RP[2]], [An], out=Am[ni][H, :, :], in_=rp[2][H, :].rearrange("p (a b) -> p a b", a=8), func=AF.Copy)
                    K.op("dve", "tensor_copy", [RP[3]], [Bn], out=Bm[ni][H, :, :], in_=rp[3][H, :].rearrange("p (a b) -> p a b", a=8))
                    yield
                    for h in range(8):
                        hs = slice(h * 64, (h + 1) * 64)
                        K.mm(rp[0][H, hs], Bm[ni][H, h, :], Pm[ci][H, h, :], [Bn, Pc], [RP[0]])
                    if lvl < 4:
                        K.op("dve", "tensor_tensor", [RP[0], Pc], [Pn], out=Pm[ni][H, :, :], in0=rp[0][H, :].rearrange("p (a b) -> p a b", a=8),
                             in1=Pm[ci][H, :, :], op=ALU.add)
                    else:
                        K.op("dve", "tensor_tensor", [RP[0], Pc], [nPmF], out=PmF[H, :, :], in0=rp[0][H, :].rearrange("p (a b) -> p a b", a=8),
                             in1=Pm[ci][H, :, :], op=ALU.add)
                    yield

            def post(c):
                q = c % 2
                KBT, VT, AT, PF = KBTs[q], VTs[q], ATs[q], PmFs[q]
                nKBT, nVT, nAT, PFn = "rk_KBT%d" % q, "rk_VT%d" % q, "rk_AT%d" % q, "rk_PmF%d" % q
                Rs3 = Rs[H, :].rearrange("p (a b) -> p a b", a=8)
                for h in range(8):
                    cc, h2 = h // 2, h % 2
                    rows = slice(h2 * 64, (h2 + 1) * 64)
                    hs = slice(h * 64, (h + 1) * 64)
                    K.mm(rp[6 + h2][H, cc * 64:(cc + 1) * 64], QR[rows, cc, c, 0, :], M[rows, cc, :], ["rk_QR", "rk_M"], [RP[6 + h2]])
                    K.mm(rp[4][H, hs], AT[0:64, h, 0:64], VT[0:64, cc, h2 * 64:(h2 + 1) * 64], [nAT, nVT], [RP[4]])
                for h2 in range(2):
                    K.op("act", "activation", [RP[6 + h2]], ["rk_Rs"], out=ev(Rs3, h2), in_=rp[6 + h2][H, 0:256].rearrange("p (a b) -> p a b", a=4), func=AF.Copy)
                K.op("dve", "tensor_tensor", [RP[4], "rk_Rs"], ["rk_Rs"], out=Rs[H, :], in0=rp[4][H, :], in1=Rs[H, :], op=ALU.add)
                yield
                for h in range(8):
                    hs = slice(h * 64, (h + 1) * 64)
                    K.mm(rp[5][H, hs], PF[H, h, :], Rs[H, hs], [PFn, "rk_Rs"], [RP[5]])
                K.op("act", "activation", [RP[5]], ["rk_Us"], out=Us[H, :], in_=rp[5][H, :], func=AF.Copy, scale=-1.0)
                yield
                for h in range(8):
                    cc, h2 = h // 2, h % 2
                    rows = slice(h2 * 64, (h2 + 1) * 64)
                    hs = slice(h * 64, (h + 1) * 64)
                    ys = slice(cc * 64, (cc + 1) * 64)
                    K.mm(rp[6 + h2][rows, ys], M[rows, cc, :], QR[rows, cc, c, 1, :], ["rk_M", "rk_QR"], [RP[6 + h2]])
                    K.mm(rp[4][rows, ys], VT[0:64, cc, h2 * 64:(h2 + 1) * 64], AT[0:64, h, 64:128], [nVT, nAT], [RP[4]])
                    K.mm(rp[5][rows, ys], Us[H, hs], AT[H, h, 64:128], ["rk_Us", nAT], [RP[5]])
                for h2 in range(2):
                    rows = slice(h2 * 64, (h2 + 1) * 64)
                    K.op("act", "activation", [RP[6 + h2]], ["rk_Yb"], out=Yb[rows, :, c * 64:(c + 1) * 64],
                         in_=rp[6 + h2][rows, 0:256].rearrange("p (a b) -> p a b", a=4), func=AF.Copy)
                yv = Yb[:, :, c * 64:(c + 1) * 64]
                K.op("dve", "tensor_tensor", [RP[4], "rk_Yb"], ["rk_Yb"], out=yv, in0=rp[4][:, 0:256].rearrange("p (a b) -> p a b", a=4), in1=yv, op=ALU.add)
                K.op("dve", "tensor_tensor", [RP[5], "rk_Yb"], ["rk_Yb"], out=yv, in0=rp[5][:, 0:256].rearrange("p (a b) -> p a b", a=4), in1=yv, op=ALU.add)
                yield
                for cc in range(4):
                    for h2 in range(2):
                        h = 2 * cc + h2
                        rows = slice(h2 * 64, (h2 + 1) * 64)
                        hs = slice(h * 64, (h + 1) * 64)
                        K.mm(rp[4][rows, cc * 64:(cc + 1) * 64], KBT[0:64, cc, rows], VT[0:64, cc, rows], [nKBT, nVT], [RP[4]])
                        K.mm(rp[5][rows, cc * 64:(cc + 1) * 64], KBT[H, cc, rows], Us[H, hs], [nKBT, "rk_Us"], [RP[5]])
                K.op("dve", "tensor_tensor", [RP[4], "rk_M"], ["rk_M"], out=M[:], in0=rp[4][:, 0:256].rearrange("p (a b) -> p a b", a=4), in1=M[:], op=ALU.add)
                K.op("dve", "tensor_tensor", [RP[5], "rk_M"], ["rk_M"], out=M[:], in0=rp[5][:, 0:256].rearrange("p (a b) -> p a b", a=4), in1=M[:], op=ALU.add)
                K.op("dve", "tensor_tensor", ["rk_M", "rk_gC"], ["rk_M"], out=M[:], in0=M[:],
                     in1=gC[:, :, c:c + 1].to_broadcast([128, 4, 64]), op=ALU.mult)
                yield

            for step in range(NCH + 1):
                gens = []
                if step >= 1:
                    gens.append(post(step - 1))
                if step < NCH:
                    gens.append(pre(step))
                while gens:
                    for g in list(gens):
                        try:
                            next(g)
                        except StopIteration:
                            gens.remove(g)
            chk(7)
            for cc in range(4):
                K.mm(rp[0][:, 0:TBK], bo64[:], Yb[:, cc, :], ["bo64", "rk_Yb"], [RP[0]])
                K.op("dve", "tensor_tensor", ["rk_Yb", RP[0]], ["rk_t1"], out=t1[:, cc, :], in0=Yb[:, cc, :], in1=rp[0][:, 0:TBK], op=ALU.subtract)
                K.op("pool", "tensor_tensor", ["rk_t1"], ["rk_kk"], out=kk[:, cc, :], in0=t1[:, cc, :], in1=t1[:, cc, :], op=ALU.mult)
                K.mm(rp[1][:, 0:TBK], bo64[:], kk[:, cc, :], ["bo64", "rk_kk"], [RP[1]])
                K.op("act", "activation", [RP[1], "rk_epsg"], ["rk_kp"], out=kp[:, cc, :], in_=rp[1][:, 0:TBK], func=AF.Sqrt, bias=epsg[:])
            K.op("dve", "reciprocal", ["rk_kp"], ["rk_kp"], out=kp[:], in_=kp[:])
            K.op("dve", "tensor_tensor", ["rk_t1", "rk_kp"], ["rk_t1"], out=t1[:], in0=t1[:], in1=kp[:], op=ALU.mult)
            K.op("pool", "tensor_tensor", ["rk_t1", "rk_lnw"], ["rk_t1"], out=t1[:], in0=t1[:], in1=bc(lnw), op=ALU.mult)
            K.op("pool", "tensor_tensor", ["rk_t1", "rk_lnb"], ["rk_t1"], out=t1[:], in0=t1[:], in1=bc(lnb), op=ALU.add)
            K.op("dve", "tensor_tensor", ["rk_t1", "rk_bon"], ["rk_t1"], out=t1[:], in0=t1[:], in1=bon[:], op=ALU.add)
            K.op("dve", "tensor_tensor", ["rk_t1", "rk_gg"], ["rk_yab"], out=yab[:], in0=t1[:], in1=gg[:], op=ALU.mult)
            K.dma("sp", SC["YA"][s, :, t0:t0 + TBK].rearrange("(c p) t -> p c t", p=128), yab[:], ["rk_yab"], ["YA"])


def norm_T(K, tag, src_tile, src_name, xn, ss, junk, pst, dstT, col0, identb, eps):
    K.op("act", "activation", [src_name], [tag + "junk", tag + "ss"], out=junk[:], in_=src_tile, func=AF.Square, accum_out=ss[:])
    K.op("act", "activation", [tag + "ss", "eps6"], [tag + "ss"], out=ss[:], in_=ss[:], func=AF.Sqrt, scale=1.0 / D, bias=eps[:])
    K.op("dve", "reciprocal", [tag + "ss"], [tag + "ss"], out=ss[:], in_=ss[:])
    K.op("dve", "tensor_scalar", [src_name, tag + "ss"], [tag + "xn"], out=xn[:], in0=src_tile, scalar1=ss[:], scalar2=None, op0=ALU.mult)
    for c in range(8):
        K.tr(pst[:, c, :], xn[:, c * 128:(c + 1) * 128], identb[:], [tag + "xn", "identb"], [tag + "pst"])
    K.op("act", "activation", [tag + "pst"], [dstT[1]], out=dstT[0][:, :, col0:col0 + 128], in_=pst[:], func=AF.Copy)


def phase_mix(K, s, T, X, Wd, SC, CONST):
    ZF = SC["ZF"]
    with ExitStack() as st:
        wpa = load_cast(K, st, "mx_wpa", Wd["w_proj_a"][0], 512, 1024)
        wpb = load_cast(K, st, "mx_wpb", Wd["w_proj_b"][0], 512, 1024)
        wout = load_cast(K, st, "mx_wout", Wd["w_out"][0], 1024, 1024)
        ps = [K.ps(st, "mx_ps%d" % i, [128, 512], F32) for i in range(4)]
        ya = K.sb(st, "mx_ya", [128, 4, 512], BF16)
        yb = K.sb(st, "mx_yb", [128, 4, 512], BF16)
        G = K.sb(st, "mx_G", [128, 16, 512], F32)
        ta = K.sb(st, "mx_ta", [128, 512], F32)
        tb_ = K.sb(st, "mx_tb", [128, 512], F32)
        mixT = K.sb(st, "mx_mixT", [128, 8, 512], BF16)
        xt = [K.sb(st, "mx_xt%d" % i, [128, D], F32) for i in range(2)]
        for tb in range(T // 512):
            t0 = tb * 512
            K.dma("sp", ya[:], SC["YA"][s, :, t0:t0 + 512].rearrange("(c p) t -> p c t", p=128), ["YA"], ["mx_ya"])
            K.dma("act", yb[:], SC["YB"][s, :, t0:t0 + 512].rearrange("(c p) t -> p c t", p=128), ["YB"], ["mx_yb"])
            K.dma("sp", G[:], ZF[s, R_G:R_G + 2048, t0:t0 + 512].rearrange("(c p) t -> p c t", p=128), ["ZF"], ["mx_G"])
            for cc in range(8):
                cs = slice(cc * 128, (cc + 1) * 128)
                for k in range(4):
                    K.mm(ps[0][:], wpa[:, k, cs], ya[:, k, :], ["mx_wpa", "mx_ya"], ["mx_ps0"], start=(k == 0), stop=(k == 3))
                for k in range(4):
                    K.mm(ps[1][:], wpb[:, k, cs], yb[:, k, :], ["mx_wpb", "mx_yb"], ["mx_ps1"], start=(k == 0), stop=(k == 3))
                K.op("dve", "tensor_tensor", ["mx_ps0", "mx_G"], ["mx_ta"], out=ta[:], in0=ps[0][:], in1=G[:, cc, :], op=ALU.mult)
                K.op("dve", "tensor_tensor", ["mx_ps1", "mx_G"], ["mx_tb"], out=tb_[:], in0=ps[1][:], in1=G[:, 8 + cc, :], op=ALU.mult)
                K.op("pool", "tensor_tensor", ["mx_ta", "mx_tb"], ["mx_mixT"], out=mixT[:, cc, :], in0=ta[:], in1=tb_[:], op=ALU.add)
            for tt in range(4):
                i = tt % 2
                r0 = s * T + t0 + tt * 128
                K.dma("act", xt[i][:], X[r0:r0 + 128, :], [], ["mx_xt%d" % i])
                for half in range(2):
                    pj = 2 + half
                    for k in range(8):
                        K.mm(ps[pj][:], mixT[:, k, tt * 128:(tt + 1) * 128], wout[:, k, half * 512:(half + 1) * 512], ["mx_mixT", "mx_wout"],
                             ["mx_ps%d" % pj], start=(k == 0), stop=(k == 7))
                    K.op("dve", "tensor_tensor", ["mx_ps%d" % pj, "mx_xt%d" % i], ["mx_xt%d" % i], out=xt[i][:, half * 512:(half + 1) * 512],
                         in0=ps[pj][:], in1=xt[i][:, half * 512:(half + 1) * 512], op=ALU.add)
                K.dma("sp", SC["H1"][r0:r0 + 128, :], xt[i][:], ["mx_xt%d" % i], ["H1"])


def colvec(K, st, tag, ap, n=8):
    t = K.sb(st, tag, [128, n], F32)
    K.dma("sp", t[:], ap.rearrange("o (c p) -> p (o c)", p=128), [], [tag], allow_slow_non_contiguous=True)
    return t


def phase_cross(K, s, T, MEM, Wd, SC, CONST):
    identb = CONST["identb"]
    with ExitStack() as st:
        nrc = colvec(K, st, "cx_nrc", Wd["norm_cross"])
        nrm = colvec(K, st, "cx_nrm", Wd["norm_mem"])
        wcq = load_cast(K, st, "cx_wcq", Wd["w_cq"][0], 1024, 1024, scale_col=(nrc, "cx_nrc"))
        wckv = load_cast(K, st, "cx_wckv", Wd["w_ckv"][0], 1024, 2048, scale_col=(nrm, "cx_nrm"))
        wco = load_cast(K, st, "cx_wco", Wd["w_co"][0], 1024, 1024)
        ps = [K.ps(st, "cx_ps%d" % i, [128, 512], F32) for i in range(6)]
        pst = K.ps(st, "cx_pst", [128, 8, 128], BF16)
        ht = K.sb(st, "cx_ht", [128, 4, D], F32)
        xn = K.sb(st, "cx_xn", [128, D], BF16)
        ss = K.sb(st, "cx_ss", [128, 1], F32)
        junk = K.sb(st, "cx_junk", [128, D], F32)
        memT = K.sb(st, "cx_memT", [128, 8, 256], BF16)
        ones = K.sb(st, "cx_ones", [128, 128], BF16)
        K.op("pool", "memset", [], ["cx_ones"], ap=ones[:], constant=1.0)
        for mt in range(2):
            K.dma("sp", ht[:, 0, :], MEM[s * 256 + mt * 128: s * 256 + (mt + 1) * 128, :], [], ["cx_ht0"])
            norm_T(K, "cx_", ht[:, 0, :], "cx_ht0", xn, ss, junk, pst, (memT, "cx_memT"), mt * 128, identb, CONST["eps6"])
        kTs = K.sb(st, "cx_kTs", [128, 8, 256], BF16)
        vS = K.sb(st, "cx_vS", [128, 2, 1024], BF16)
        for j in range(8):
            for k in range(8):
                K.mm(ps[0][:, 0:256], wckv[:, k, j * 128:(j + 1) * 128], memT[:, k, :], ["cx_wckv", "cx_memT"], ["cx_ps0"], start=(k == 0), stop=(k == 7))
            K.op("dve", "tensor_copy", ["cx_ps0"], ["cx_kTs"], out=kTs[:, j, :], in_=ps[0][:, 0:256])
        for mt in range(2):
            for half in range(2):
                for k in range(8):
                    K.mm(ps[1][:], memT[:, k, mt * 128:(mt + 1) * 128], wckv[:, k, 1024 + half * 512:1024 + (half + 1) * 512], ["cx_wckv", "cx_memT"],
                         ["cx_ps1"], start=(k == 0), stop=(k == 7))
                K.op("dve", "tensor_copy", ["cx_ps1"], ["cx_vS"], out=vS[:, mt, half * 512:(half + 1) * 512], in_=ps[1][:])
        hnT = K.sb(st, "cx_hnT", [128, 8, 512], BF16)
        qTs = K.sb(st, "cx_qTs", [128, 8, 512], BF16)
        pT = [K.sb(st, "cx_pT%d" % i, [128, 512], BF16) for i in range(2)]
        rden = K.sb(st, "cx_rden", [128, 512], F32)
        oT = K.sb(st, "cx_oT", [128, 8, 512], BF16)
        for tb in range(T // 512):
            t0 = tb * 512
            for tt in range(4):
                r0 = s * T + t0 + tt * 128
                K.dma("sp" if tt % 2 == 0 else "act", ht[:, tt, :], SC["H1"][r0:r0 + 128, :], ["H1"], ["cx_ht%d" % tt])
                norm_T(K, "cx_", ht[:, tt, :], "cx_ht%d" % tt, xn, ss, junk, pst, (hnT, "cx_hnT"), tt * 128, identb, CONST["eps6"])
            for j in range(8):
                pj = j % 2
                for k in range(8):
                    K.mm(ps[pj][:], wcq[:, k, j * 128:(j + 1) * 128], hnT[:, k, :], ["cx_wcq", "cx_hnT"], ["cx_ps%d" % pj], start=(k == 0), stop=(k == 7))
                if pj == 0:
                    K.op("dve", "tensor_copy", ["cx_ps0"], ["cx_qTs"], out=qTs[:, j, :], in_=ps[0][:])
                else:
                    K.op("act", "activation", ["cx_ps1"], ["cx_qTs"], out=qTs[:, j, :], in_=ps[1][:], func=AF.Copy)
            for h in range(4):
                for mt in range(2):
                    for dc in range(2):
                        K.mm(ps[2 + mt][:], kTs[:, 2 * h + dc, mt * 128:(mt + 1) * 128], qTs[:, 2 * h + dc, :], ["cx_kTs", "cx_qTs"],
                             ["cx_ps%d" % (2 + mt)], start=(dc == 0), stop=(dc == 1))
                    K.op("act", "activation", ["cx_ps%d" % (2 + mt)], ["cx_pT%d" % mt], out=pT[mt][:], in_=ps[2 + mt][:], func=AF.Exp, scale=1.0 / 16)
                for mt in range(2):
                    K.mm(ps[4][:], ones[:], pT[mt][:], ["cx_ones", "cx_pT%d" % mt], ["cx_ps4"], start=(mt == 0), stop=(mt == 1))
                K.op("dve", "reciprocal", ["cx_ps4"], ["cx_rden"], out=rden[:], in_=ps[4][:])
                for dc in range(2):
                    for mt in range(2):
                        K.mm(ps[5][:], vS[:, mt, h * 256 + dc * 128:h * 256 + (dc + 1) * 128], pT[mt][:], ["cx_vS", "cx_pT%d" % mt], ["cx_ps5"],
                             start=(mt == 0), stop=(mt == 1))
                    K.op("dve", "tensor_tensor", ["cx_ps5", "cx_rden"], ["cx_oT"], out=oT[:, 2 * h + dc, :], in0=ps[5][:], in1=rden[:], op=ALU.mult)
            for tt in range(4):
                r0 = s * T + t0 + tt * 128
                for half in range(2):
                    pj = half
                    for k in range(8):
                        K.mm(ps[pj][:], oT[:, k, tt * 128:(tt + 1) * 128], wco[:, k, half * 512:(half + 1) * 512], ["cx_oT", "cx_wco"],
                             ["cx_ps%d" % pj], start=(k == 0), stop=(k == 7))
                    K.op("dve", "tensor_tensor", ["cx_ps%d" % pj, "cx_ht%d" % tt], ["cx_ht%d" % tt], out=ht[:, tt, half * 512:(half + 1) * 512],
                         in0=ps[pj][:], in1=ht[:, tt, half * 512:(half + 1) * 512], op=ALU.add)
                K.dma("sp", SC["H1"][r0:r0 + 128, :], ht[:, tt, :], ["cx_ht%d" % tt], ["H1"])


def phase_moe(K, s, T, Wd, SC, CONST, OUT):
    identb = CONST["identb"]
    HT = min(T, 1024)
    NTL = HT // 128
    with ExitStack() as st:
        nrf = colvec(K, st, "mo_nrf", Wd["norm_ffn"])
        wrf = K.sb(st, "mo_wrf", [128, 8, 36], F32)
        K.dma("sp", wrf[:, :, 0:4], Wd["w_router_g"][0].rearrange("(c p) n -> p c n", p=128), [], ["mo_wrf"])
        K.dma("sp", wrf[:, :, 4:36], Wd["w_router_e"][0].rearrange("(c p) n -> p c n", p=128), [], ["mo_wrf"])
        wr = K.sb(st, "mo_wr", [128, 8, 36], BF16)
        K.op("dve", "tensor_tensor", ["mo_wrf", "mo_nrf"], ["mo_wr"], out=wr[:], in0=wrf[:], in1=nrf[:].unsqueeze(2).to_broadcast([128, 8, 36]), op=ALU.mult)
        brb = K.sb(st, "mo_brb", [128, 36], F32)
        K.dma("sp", brb[:, 0:4], Wd["b_router_g"].partition_broadcast(128), [], ["mo_brb"])
        K.dma("sp", brb[:, 4:36], Wd["b_router_e"].partition_broadcast(128), [], ["mo_brb"])
        nfb = K.sb(st, "mo_nfb", [128, D], F32)
        K.dma("sp", nfb[:], Wd["norm_final"].partition_broadcast(128), [], ["mo_nfb"])
        ps = [K.ps(st, "mo_ps%d" % i, [128, 512], F32) for i in range(7)]
        pst = K.ps(st, "mo_pst", [128, 8, 128], BF16)
        ht = K.sb(st, "mo_ht", [128, D], F32)
        xn = K.sb(st, "mo_xn", [128, D], BF16)
        ss = K.sb(st, "mo_ss", [128, 1], F32)
        junk = K.sb(st, "mo_junk", [128, D], F32)
        xT = K.sb(st, "mo_xT", [128, 8, HT], BF16)
        G = K.sb(st, "mo_G", [128, NTL, 32], F32)
        acc = K.sb(st, "mo_acc", [128, NTL, D], F32)
        lg = K.sb(st, "mo_lg", [128, 36], F32)
        cl = K.sb(st, "mo_cl", [128, 12], F32)
        lem = K.sb(st, "mo_lem", [128, 4, 8], F32)
        m8 = K.sb(st, "mo_m8", [128, 8], F32)
        sel = K.sb(st, "mo_sel", [128, 32], F32)
        ex = K.sb(st, "mo_ex", [128, 32], F32)
        stg = [K.sb(st, "mo_stg%d" % i, [128, 4096], F32) for i in range(2)]
        wg = [K.sb(st, "mo_wg%d" % i, [128, 8, 512], BF16) for i in range(2)]
        wu = [K.sb(st, "mo_wu%d" % i, [128, 8, 512], BF16) for i in range(2)]
        wd = [K.sb(st, "mo_wd%d" % i, [128, 4, 1024], BF16) for i in range(2)]
        sgts = [K.sb(st, "mo_sgt%d" % i, [128, 512], F32) for i in range(2)]
        hT = K.sb(st, "mo_hT", [128, 4, 512], BF16)
        tmp = [K.sb(st, "mo_tmp%d" % i, [128, 512], F32) for i in range(3)]
        for hf in range(T // HT):
            base = s * T + hf * HT
            for tl in range(NTL):
                r0 = base + tl * 128
                K.dma("sp", ht[:], SC["H1"][r0:r0 + 128, :], ["H1"], ["mo_ht"])
                norm_T(K, "mo_", ht[:], "mo_ht", xn, ss, junk, pst, (xT, "mo_xT"), tl * 128, identb, CONST["eps6"])
                for k in range(8):
                    K.mm(ps[0][:, 0:36], xT[:, k, tl * 128:(tl + 1) * 128], wr[:, k, :], ["mo_xT", "mo_wr"], ["mo_ps0"], start=(k == 0), stop=(k == 7))
                K.op("dve", "tensor_tensor", ["mo_ps0", "mo_brb"], ["mo_lg"], out=lg[:], in0=ps[0][:, 0:36], in1=brb[:], op=ALU.add)
                K.op("dve", "tensor_reduce", ["mo_lg"], ["mo_cl"], out=cl[:, 0:1], in_=lg[:, 0:4], axis=AX.X, op=ALU.max)
                K.op("dve", "tensor_scalar", ["mo_cl"], ["mo_cl"], out=cl[:, 1:2], in0=cl[:, 0:1], scalar1=-1.0, scalar2=None, op0=ALU.mult)
                K.op("act", "activation", ["mo_lg", "mo_cl"], ["mo_ex", "mo_cl"], out=ex[:, 0:4], in_=lg[:, 0:4], func=AF.Exp, bias=cl[:, 1:2], accum_out=cl[:, 2:3])
                K.op("dve", "reciprocal", ["mo_cl"], ["mo_cl"], out=cl[:, 3:4], in_=cl[:, 2:3])
                K.op("dve", "tensor_scalar", ["mo_lg", "mo_cl"], ["mo_sel"], out=sel[:, 0:4], in0=lg[:, 0:4], scalar1=cl[:, 0:1], scalar2=None, op0=ALU.is_ge)
                K.op("dve", "tensor_scalar", ["mo_sel"], ["mo_sel"], out=sel[:, 0:4], in0=sel[:, 0:4], scalar1=-1.0, scalar2=1e30, op0=ALU.add, op1=ALU.mult)
                K.op("dve", "tensor_tensor", ["mo_lg", "mo_sel"], ["mo_lem"], out=lem[:], in0=lg[:, 4:36].rearrange("p (a b) -> p a b", a=4),
                     in1=sel[:, 0:4].unsqueeze(2).to_broadcast([128, 4, 8]), op=ALU.add)
                lemf = lem[:].rearrange("p a b -> p (a b)")
                K.op("dve", "max", ["mo_lem"], ["mo_m8"], out=m8[:], in_=lemf)
                K.op("dve", "tensor_scalar", ["mo_lem", "mo_m8"], ["mo_sel"], out=sel[:], in0=lemf, scalar1=m8[:, 1:2], scalar2=None, op0=ALU.is_ge)
                K.op("dve", "tensor_scalar", ["mo_m8"], ["mo_cl"], out=cl[:, 4:5], in0=m8[:, 0:1], scalar1=-1.0, scalar2=None, op0=ALU.mult)
                K.op("act", "activation", ["mo_lem", "mo_cl"], ["mo_ex"], out=ex[:], in_=lemf, func=AF.Exp, bias=cl[:, 4:5])
                K.op("dve", "tensor_tensor", ["mo_ex", "mo_sel"], ["mo_ex"], out=ex[:], in0=ex[:], in1=sel[:], op=ALU.mult)
                K.op("dve", "tensor_reduce", ["mo_ex"], ["mo_cl"], out=cl[:, 5:6], in_=ex[:], axis=AX.X, op=ALU.add)
                K.op("dve", "reciprocal", ["mo_cl"], ["mo_cl"], out=cl[:, 6:7], in_=cl[:, 5:6])
                K.op("dve", "tensor_tensor", ["mo_cl"], ["mo_cl"], out=cl[:, 7:8], in0=cl[:, 6:7], in1=cl[:, 3:4], op=ALU.mult)
                K.op("dve", "tensor_scalar", ["mo_ex", "mo_cl"], ["mo_G"], out=G[:, tl, :], in0=ex[:], scalar1=cl[:, 7:8], scalar2=None, op0=ALU.mult)
            for e in range(32):
                i = e % 2
                nfb8 = nrf[:].unsqueeze(2).to_broadcast([128, 8, 512])
                K.dma("sp", stg[0][:].rearrange("p (c n) -> p c n", c=8), Wd["w_e_gate"][0, e].rearrange("(c p) n -> p c n", p=128), [], ["mo_stg0"])
                K.op("pool", "tensor_tensor", ["mo_stg0", "mo_nrf"], ["mo_wg%d" % i], out=wg[i][:], in0=stg[0][:].rearrange("p (c n) -> p c n", c=8), in1=nfb8, op=ALU.mult)
                K.dma("act", stg[1][:].rearrange("p (c n) -> p c n", c=8), Wd["w_e_up"][0, e].rearrange("(c p) n -> p c n", p=128), [], ["mo_stg1"])
                K.op("pool", "tensor_tensor", ["mo_stg1", "mo_nrf"], ["mo_wu%d" % i], out=wu[i][:], in0=stg[1][:].rearrange("p (c n) -> p c n", c=8), in1=nfb8, op=ALU.mult)
                K.dma("sp", stg[0][:].rearrange("p (c n) -> p c n", c=4), Wd["w_e_down"][0, e].rearrange("(c p) n -> p c n", p=128), [], ["mo_stg0"])
                K.op("pool", "tensor_copy", ["mo_stg0"], ["mo_wd%d" % i], out=wd[i][:], in_=stg[0][:].rearrange("p (c n) -> p c n", c=4))
                for bk in range(HT // 512):
                    bs = slice(bk * 512, (bk + 1) * 512)
                    for fc in range(4):
                        fs = slice(fc * 128, (fc + 1) * 128)
                        pg, pu = (0, 1) if fc % 2 == 0 else (4, 5)
                        sg_ = sgts[fc % 2]
                        sgn = "mo_sgt%d" % (fc % 2)
                        for k in range(8):
                            K.mm(ps[pg][:], wg[i][:, k, fs], xT[:, k, bs], ["mo_wg%d" % i, "mo_xT"], ["mo_ps%d" % pg], start=(k == 0), stop=(k == 7))
                        for k in range(8):
                            K.mm(ps[pu][:], wu[i][:, k, fs], xT[:, k, bs], ["mo_wu%d" % i, "mo_xT"], ["mo_ps%d" % pu], start=(k == 0), stop=(k == 7))
                        K.op("act", "activation", ["mo_ps%d" % pg], [sgn], out=sg_[:], in_=ps[pg][:], func=AF.Silu)
                        K.op("dve", "tensor_tensor", ["mo_ps%d" % pu, sgn], ["mo_hT%d" % fc], out=hT[:, fc, :], in0=ps[pu][:], in1=sg_[:], op=ALU.mult)
                    for tt in range(4):
                        tl = bk * 4 + tt
                        for half in range(2):
                            pj = (2, 3, 6)[(2 * tt + half) % 3]
                            for fc in range(4):
                                K.mm(ps[pj][:], hT[:, fc, tt * 128:(tt + 1) * 128], wd[i][:, fc, half * 512:(half + 1) * 512], ["mo_hT%d" % fc, "mo_wd%d" % i],
                                     ["mo_ps%d" % pj], start=(fc == 0), stop=(fc == 3))
                            hs = slice(half * 512, (half + 1) * 512)
                            if e == 0:
                                K.op("act", "activation", ["mo_ps%d" % pj, "mo_G"], ["mo_acc%d_%d" % (tl, half)], out=acc[:, tl, hs], in_=ps[pj][:], func=AF.Copy, scale=G[:, tl, e:e + 1])
                            else:
                                ti = (2 * tt + half) % 3
                                accn = "mo_acc%d_%d" % (tl, half)
                                K.op("act", "activation", ["mo_ps%d" % pj, "mo_G"], ["mo_tmp%d" % ti], out=tmp[ti][:], in_=ps[pj][:], func=AF.Copy, scale=G[:, tl, e:e + 1])
                                K.op("pool" if half == 0 else "dve", "tensor_tensor", ["mo_tmp%d" % ti, accn], [accn], out=acc[:, tl, hs], in0=acc[:, tl, hs], in1=tmp[ti][:], op=ALU.add)
            for tl in range(NTL):
                r0 = base + tl * 128
                K.dma("sp", ht[:], SC["H1"][r0:r0 + 128, :], ["H1"], ["mo_ht"])
                K.op("dve", "tensor_tensor", ["mo_ht", "mo_acc%d_0" % tl, "mo_acc%d_1" % tl], ["mo_ht"], out=ht[:], in0=ht[:], in1=acc[:, tl, :], op=ALU.add)
                K.op("act", "activation", ["mo_ht"], ["mo_junk", "mo_ss"], out=junk[:], in_=ht[:], func=AF.Square, accum_out=ss[:])
                K.op("act", "activation", ["mo_ss", "eps6"], ["mo_ss"], out=ss[:], in_=ss[:], func=AF.Sqrt, scale=1.0 / D, bias=CONST["eps6"][:])
                K.op("dve", "reciprocal", ["mo_ss"], ["mo_ss"], out=ss[:], in_=ss[:])
                K.op("dve", "scalar_tensor_tensor", ["mo_ht", "mo_ss", "mo_nfb"], ["mo_junk"], out=junk[:], in0=ht[:], scalar=ss[:], in1=nfb[:], op0=ALU.mult, op1=ALU.mult)
                K.dma("sp", OUT[r0:r0 + 128, :], junk[:], ["mo_junk"], ["OUT"])


I32 = mybir.dt.int32


def prepack_gen(K, st, Wd, SC):
    WGU, WDS = SC["WGU"], SC["WDS"]
    nrf = colvec(K, st, "pk_nrf", Wd["norm_ffn"])
    sg = K.sb(st, "pk_sg", [128, 8, 512], F32)
    su = K.sb(st, "pk_su", [128, 8, 512], F32)
    sd = K.sb(st, "pk_sd", [128, 4, 1024], F32)
    og = K.sb(st, "pk_og", [128, 8, 1024], BF16)
    od = K.sb(st, "pk_od", [128, 4, 1024], BF16)
    nf8 = nrf[:].unsqueeze(2).to_broadcast([128, 8, 512])
    def loads(e):
        K.dma("pool", sg[:], Wd["w_e_gate"][0, e].rearrange("(c p) n -> p c n", p=128), [], ["pk_sg"])
        K.dma("pool", su[:], Wd["w_e_up"][0, e].rearrange("(c p) n -> p c n", p=128), [], ["pk_su"])
        K.dma("pool", sd[:], Wd["w_e_down"][0, e].rearrange("(c p) n -> p c n", p=128), [], ["pk_sd"])

    loads(0)
    yield
    for e in range(32):
        K.op("dve", "tensor_tensor", ["pk_sg", "pk_nrf"], ["pk_og0"], out=og[:, :, 0:512], in0=sg[:], in1=nf8, op=ALU.mult)
        for c in range(8):
            K.op("act", "activation", ["pk_su", "pk_nrf"], ["pk_og1"], out=og[:, c, 512:1024], in_=su[:, c, :], func=AF.Copy, scale=nrf[:, c:c + 1])
        K.op("act", "activation", ["pk_sd"], ["pk_od"], out=od[:], in_=sd[:], func=AF.Copy)
        K.dma("pool", WGU[e * 1024:(e + 1) * 1024, :].rearrange("(c p) n -> p c n", p=128), og[:], ["pk_og0", "pk_og1"], ["WGU"])
        K.dma("pool", WDS[e * 512:(e + 1) * 512, :].rearrange("(c p) n -> p c n", p=128), od[:], ["pk_od"], ["WDS"])
        if e + 1 < 32:
            loads(e + 1)
        yield


def phase_moe_sparse(K, s, T, Wd, SC, CONST, OUT):
    nc = K.nc
    S = K.S
    identb = CONST["identb"]
    NTL = T // 128
    SB = 256
    NBLK = (2 * T) // SB + 32
    XS, YS = SC["XS"], SC["YS"]
    WG = Wd["w_e_gate"].rearrange("o e d f -> (o e d) f")
    WU = Wd["w_e_up"].rearrange("o e d f -> (o e d) f")
    WDN = Wd["w_e_down"].rearrange("o e f d -> (o e f) d")
    base = s * T
    with ExitStack() as st0:
        nrf = colvec(K, st0, "ms_nrf", Wd["norm_ffn"])
        GG = K.sb(st0, "ms_GG", [128, NTL, 2], F32)
        DST = K.sb(st0, "ms_DST", [128, NTL, 2], I32)
        IDXG = K.sb(st0, "ms_IDXG", [128, NBLK, 8], I32)
        IDXD = K.sb(st0, "ms_IDXD", [128, NBLK, 4], I32)
        with ExitStack() as st:
            wrf = K.sb(st, "ms_wrf", [128, 8, 36], F32)
            K.dma("sp", wrf[:, :, 0:4], Wd["w_router_g"][0].rearrange("(c p) n -> p c n", p=128), [], ["ms_wrf"])
            K.dma("sp", wrf[:, :, 4:36], Wd["w_router_e"][0].rearrange("(c p) n -> p c n", p=128), [], ["ms_wrf"])
            wr = K.sb(st, "ms_wr", [128, 8, 36], BF16)
            K.op("dve", "tensor_tensor", ["ms_wrf", "ms_nrf"], ["ms_wr"], out=wr[:], in0=wrf[:], in1=nrf[:].unsqueeze(2).to_broadcast([128, 8, 36]), op=ALU.mult)
            brb = K.sb(st, "ms_brb", [128, 36], F32)
            K.dma("sp", brb[:, 0:4], Wd["b_router_g"].partition_broadcast(128), [], ["ms_brb"])
            K.dma("sp", brb[:, 4:36], Wd["b_router_e"].partition_broadcast(128), [], ["ms_brb"])
            ps = [K.ps(st, "ms_ps%d" % i, [128, 512], F32) for i in range(2)]
            pst = K.ps(st, "ms_pst", [128, 8, 128], BF16)
            ht = K.sb(st, "ms_ht", [128, D], F32)
            ss = K.sb(st, "ms_ss", [128, 1], F32)
            junk = K.sb(st, "ms_junk", [128, D], F32)
            XN = K.sb(st, "ms_XN", [128, NTL, D], BF16)
            xT = K.sb(st, "ms_xT", [128, 8, 128], BF16)
            SEL = K.sb(st, "ms_SEL", [128, NTL, 2, 32], F32)
            RNK = K.sb(st, "ms_RNK", [128, NTL, 2], F32)
            carry = K.sb(st, "ms_carry", [128, 32], F32)
            K.op("dve", "memset", [], ["ms_carry"], ap=carry[:], constant=0.0)
            lg = K.sb(st, "ms_lg", [128, 36], F32)
            cl = K.sb(st, "ms_cl", [128, 12], F32)
            lem = K.sb(st, "ms_lem", [128, 32], F32)
            m8 = K.sb(st, "ms_m8", [128, 8], F32)
            s12 = K.sb(st, "ms_s12", [128, 32], F32)
            ex = K.sb(st, "ms_ex", [128, 32], F32)
            t32 = K.sb(st, "ms_t32", [128, 32], F32)
            utri, ones128, bstart, iotap = CONST["utri"], CONST["ones128"], CONST["bstart"], CONST["iotap"]
            GB = 8
            LG = K.sb(st, "ms_LG", [128, GB, 36], F32)
            LM = K.sb(st, "ms_LM", [128, GB, 32], F32)
            L2 = K.sb(st, "ms_L2", [128, GB, 32], F32)
            EX = K.sb(st, "ms_EX", [128, GB, 32], F32)
            S12 = K.sb(st, "ms_S12", [128, GB, 32], F32)
            RKt = K.sb(st, "ms_RKt", [128, GB, 32], F32)
            T4 = K.sb(st, "ms_T4", [128, GB, 4], F32)
            E4 = K.sb(st, "ms_E4", [128, GB, 4], F32)
            CG = K.sb(st, "ms_CG", [128, 8, GB], F32)
            hts = [ht, K.sb(st, "ms_ht1", [128, D], F32)]

            def b3(colv, n):
                return colv.unsqueeze(2).to_broadcast([128, GB, n])

            for g0 in range(0, NTL, GB):
                for gi in range(GB):
                    tl = g0 + gi
                    r0 = base + tl * 128
                    hh_ = hts[tl % 2]
                    hn = "ms_ht" if tl % 2 == 0 else "ms_ht1"
                    K.dma("sp" if tl % 2 == 0 else "act", hh_[:], SC["H1"][r0:r0 + 128, :], ["H1"], [hn])
                    K.op("act", "activation", [hn], ["ms_junk", "ms_ss"], out=junk[:], in_=hh_[:], func=AF.Square, accum_out=ss[:])
                    K.op("act", "activation", ["ms_ss", "eps6"], ["ms_ss"], out=ss[:], in_=ss[:], func=AF.Sqrt, scale=1.0 / D, bias=CONST["eps6"][:])
                    K.op("dve", "reciprocal", ["ms_ss"], ["ms_ss"], out=ss[:], in_=ss[:])
                    K.op("dve", "tensor_scalar", [hn, "ms_ss"], ["ms_XN%d" % tl], out=XN[:, tl, :], in0=hh_[:], scalar1=ss[:], scalar2=None, op0=ALU.mult)
                    for c in range(8):
                        K.tr(pst[:, c, :], XN[:, tl, c * 128:(c + 1) * 128], identb[:], ["ms_XN%d" % tl, "identb"], ["ms_pst"])
                    K.op("act", "activation", ["ms_pst"], ["ms_xT"], out=xT[:], in_=pst[:], func=AF.Copy)
                    for k in range(8):
                        K.mm(ps[0][:, 0:36], xT[:, k, :], wr[:, k, :], ["ms_xT", "ms_wr"], ["ms_ps0"], start=(k == 0), stop=(k == 7))
                    K.op("dve", "tensor_tensor", ["ms_ps0", "ms_brb"], ["ms_LG"], out=LG[:, gi, :], in0=ps[0][:, 0:36], in1=brb[:], op=ALU.add)
                K.op("dve", "tensor_reduce", ["ms_LG"], ["ms_CG"], out=CG[:, 0, :], in_=LG[:, :, 0:4], axis=AX.X, op=ALU.max)
                K.op("dve", "tensor_tensor", ["ms_LG", "ms_CG"], ["ms_T4"], out=T4[:], in0=LG[:, :, 0:4], in1=b3(CG[:, 0, :], 4), op=ALU.subtract)
                K.op("act", "activation", ["ms_T4"], ["ms_E4"], out=E4[:], in_=T4[:], func=AF.Exp)
                K.op("dve", "tensor_reduce", ["ms_E4"], ["ms_CG"], out=CG[:, 1, :], in_=E4[:], axis=AX.X, op=ALU.add)
                K.op("dve", "reciprocal", ["ms_CG"], ["ms_CG"], out=CG[:, 2, :], in_=CG[:, 1, :])
                K.op("dve", "tensor_scalar", ["ms_T4"], ["ms_T4"], out=T4[:], in0=T4[:], scalar1=0.0, scalar2=None, op0=ALU.is_ge)
                K.op("dve", "tensor_scalar", ["ms_T4"], ["ms_T4"], out=T4[:], in0=T4[:], scalar1=-1.0, scalar2=1e30, op0=ALU.add, op1=ALU.mult)
                K.op("dve", "tensor_tensor", ["ms_LG", "ms_T4"], ["ms_LM"], out=LM[:].rearrange("p g (a b) -> p g a b", a=4),
                     in0=LG[:, :, 4:36].rearrange("p g (a b) -> p g a b", a=4), in1=T4[:].unsqueeze(3).to_broadcast([128, GB, 4, 8]), op=ALU.add)
                K.op("dve", "tensor_reduce", ["ms_LM"], ["ms_CG"], out=CG[:, 3, :], in_=LM[:], axis=AX.X, op=ALU.max)
                sel1 = SEL[:, g0:g0 + GB, 0, :]
                sel2 = SEL[:, g0:g0 + GB, 1, :]
                K.op("dve", "tensor_tensor", ["ms_LM", "ms_CG"], ["ms_SEL"], out=sel1, in0=LM[:], in1=b3(CG[:, 3, :], 32), op=ALU.is_ge)
                K.op("dve", "scalar_tensor_tensor", ["ms_SEL", "ms_LM"], ["ms_L2"], out=L2[:], in0=sel1, scalar=-1e30, in1=LM[:], op0=ALU.mult, op1=ALU.add)
                K.op("dve", "tensor_reduce", ["ms_L2"], ["ms_CG"], out=CG[:, 4, :], in_=L2[:], axis=AX.X, op=ALU.max)
                K.op("dve", "tensor_tensor", ["ms_LM", "ms_CG"], ["ms_S12"], out=S12[:], in0=LM[:], in1=b3(CG[:, 4, :], 32), op=ALU.is_ge)
                K.op("dve", "tensor_tensor", ["ms_S12", "ms_SEL"], ["ms_SEL"], out=sel2, in0=S12[:], in1=sel1, op=ALU.subtract)
                K.op("dve", "tensor_tensor", ["ms_LM", "ms_CG"], ["ms_L2"], out=L2[:], in0=LM[:], in1=b3(CG[:, 3, :], 32), op=ALU.subtract)
                K.op("dve", "tensor_scalar", ["ms_L2"], ["ms_L2"], out=L2[:], in0=L2[:], scalar1=-80.0, scalar2=None, op0=ALU.max)
                K.op("act", "activation", ["ms_L2"], ["ms_EX"], out=EX[:], in_=L2[:], func=AF.Exp)
                K.op("dve", "tensor_tensor", ["ms_EX", "ms_S12"], ["ms_EX"], out=EX[:], in0=EX[:], in1=S12[:], op=ALU.mult)
                K.op("dve", "tensor_reduce", ["ms_EX"], ["ms_CG"], out=CG[:, 5, :], in_=EX[:], axis=AX.X, op=ALU.add)
                K.op("dve", "reciprocal", ["ms_CG"], ["ms_CG"], out=CG[:, 6, :], in_=CG[:, 5, :])
                K.op("dve", "tensor_tensor", ["ms_CG"], ["ms_CG"], out=CG[:, 6, :], in0=CG[:, 6, :], in1=CG[:, 2, :], op=ALU.mult)
                for kk_ in range(2):
                    K.op("dve", "tensor_tensor", ["ms_EX", "ms_SEL"], ["ms_L2"], out=L2[:], in0=EX[:], in1=SEL[:, g0:g0 + GB, kk_, :], op=ALU.mult)
                    K.op("dve", "tensor_reduce", ["ms_L2"], ["ms_CG"], out=CG[:, 7, :], in_=L2[:], axis=AX.X, op=ALU.add)
                    K.op("dve", "tensor_tensor", ["ms_CG"], ["ms_GG"], out=GG[:, g0:g0 + GB, kk_], in0=CG[:, 7, :], in1=CG[:, 6, :], op=ALU.mult)
                for gi in range(GB):
                    K.mm(ps[1][:, gi * 64:gi * 64 + 32], utri[:], S12[:, gi, :], ["utri", "ms_S12"], ["ms_ps1"])
                    K.mm(ps[1][:, gi * 64 + 32:gi * 64 + 64], ones128[:], S12[:, gi, :], ["ones128", "ms_S12"], ["ms_ps1"])
                for gi in range(GB):
                    K.op("dve", "tensor_tensor", ["ms_ps1", "ms_carry"], ["ms_RKt"], out=RKt[:, gi, :], in0=ps[1][:, gi * 64:gi * 64 + 32], in1=carry[:], op=ALU.add)
                    K.op("dve", "tensor_tensor", ["ms_ps1", "ms_carry"], ["ms_carry"], out=carry[:], in0=ps[1][:, gi * 64 + 32:gi * 64 + 64], in1=carry[:], op=ALU.add)
                for kk_ in range(2):
                    K.op("dve", "tensor_tensor", ["ms_RKt", "ms_SEL"], ["ms_L2"], out=L2[:], in0=RKt[:], in1=SEL[:, g0:g0 + GB, kk_, :], op=ALU.mult)
                    K.op("dve", "tensor_reduce", ["ms_L2"], ["ms_RNK"], out=RNK[:, g0:g0 + GB, kk_], in_=L2[:], axis=AX.X, op=ALU.add)
            ci = K.sb(st, "ms_ci", [128, 32], I32)
            pad = K.sb(st, "ms_pad", [128, 32], F32)
            pend = K.sb(st, "ms_pend", [128, 32], F32)
            pstart = K.sb(st, "ms_pstart", [128, 32], F32)
            ones32 = K.sb(st, "ms_ones32", [128, 32], F32)
            K.op("dve", "memset", [], ["ms_ones32"], ap=ones32[:], constant=1.0)
            K.op("dve", "tensor_scalar", ["ms_carry"], ["ms_ci"], out=ci[:], in0=carry[:], scalar1=float(SB - 1), scalar2=None, op0=ALU.add)
            K.op("dve", "tensor_scalar", ["ms_ci"], ["ms_ci"], out=ci[:], in0=ci[:], scalar1=8, scalar2=None, op0=ALU.arith_shift_right)
            K.op("dve", "tensor_scalar", ["ms_ci"], ["ms_ci"], out=ci[:], in0=ci[:], scalar1=8, scalar2=None, op0=ALU.logical_shift_left)
            K.op("dve", "tensor_copy", ["ms_ci"], ["ms_pad"], out=pad[:], in_=ci[:])
            K.op("dve", "tensor_tensor_scan", ["ms_pad", "ms_ones32"], ["ms_pend"], out=pend[:], data0=ones32[:], data1=pad[:], initial=0.0, op0=ALU.mult, op1=ALU.add)
            K.op("dve", "tensor_tensor", ["ms_pend", "ms_pad"], ["ms_pstart"], out=pstart[:], in0=pend[:], in1=pad[:], op=ALU.subtract)
            bst = K.sb(st, "ms_bst", [128, NBLK], F32)
            K.op("dve", "tensor_scalar", ["bstart"], ["ms_bst"], out=bst[:], in0=bstart[:, 0:NBLK], scalar1=float(SB // 128), scalar2=None, op0=ALU.mult)
            be = K.sb(st, "ms_be", [128, NBLK], F32)
            K.op("dve", "tensor_scalar", ["ms_bst", "ms_pend"], ["ms_be"], out=be[:], in0=bst[:], scalar1=pend[:, 0:1], scalar2=None, op0=ALU.is_ge)
            for e in range(1, 32):
                K.op("dve", "scalar_tensor_tensor", ["ms_bst", "ms_pend", "ms_be"], ["ms_be"], out=be[:], in0=bst[:], scalar=pend[:, e:e + 1], in1=be[:],
                     op0=ALU.is_ge, op1=ALU.add)
            K.op("dve", "tensor_scalar", ["ms_be"], ["ms_be"], out=be[:], in0=be[:], scalar1=31.0, scalar2=None, op0=ALU.min)
            bg = K.sb(st, "ms_bg", [128, NBLK], F32)
            bd = K.sb(st, "ms_bd", [128, NBLK], F32)
            K.op("dve", "tensor_scalar", ["ms_be", "iotap"], ["ms_bg"], out=bg[:], in0=be[:], scalar1=1024.0, scalar2=iotap[:, 0:1], op0=ALU.mult, op1=ALU.add)
            K.op("dve", "tensor_scalar", ["ms_be", "iotap"], ["ms_bd"], out=bd[:], in0=be[:], scalar1=512.0, scalar2=iotap[:, 0:1], op0=ALU.mult, op1=ALU.add)
            for c in range(8):
                K.op("dve", "tensor_scalar", ["ms_bg"], ["ms_IDXG"], out=IDXG[:, :, c], in0=bg[:], scalar1=float(c * 128), scalar2=None, op0=ALU.add)
            for c in range(4):
                K.op("dve", "tensor_scalar", ["ms_bd"], ["ms_IDXD"], out=IDXD[:, :, c], in0=bd[:], scalar1=float(c * 128), scalar2=None, op0=ALU.add)
            zt = K.sb(st, "ms_zt", [128, 4, D], BF16)
            K.op("pool", "memset", [], ["ms_zt"], ap=zt[:], constant=0.0)
            XSv = XS.rearrange("(b p) d -> p b d", p=128)
            for b0 in range(0, NBLK * SB // 128, 4):
                K.dma("sp" if (b0 // 4) % 2 == 0 else "act", XSv[:, b0:b0 + 4, :], zt[:], ["ms_zt"], ["XS"])
            TB3 = K.sb(st, "ms_TB3", [128, NTL, 32], F32)
            DF = K.sb(st, "ms_DF", [128, NTL], F32)
            for kk_ in range(2):
                K.op("dve", "tensor_tensor", ["ms_pstart", "ms_SEL"], ["ms_TB3"], out=TB3[:], in0=SEL[:, :, kk_, :],
                     in1=pstart[:].unsqueeze(1).to_broadcast([128, NTL, 32]), op=ALU.mult)
                K.op("dve", "tensor_reduce", ["ms_TB3"], ["ms_DF"], out=DF[:], in_=TB3[:], axis=AX.X, op=ALU.add)
                K.op("dve", "tensor_tensor", ["ms_DF", "ms_RNK"], ["ms_DST"], out=DST[:, :, kk_], in0=DF[:], in1=RNK[:, :, kk_], op=ALU.add)
            for tl in range(NTL):
                for kk_ in range(2):
                    S.dma("pool", None, None, K._bl(["ms_DST", "ms_XN%d" % tl, "XS"]), K._bl(["XSs_%d_%d" % (tl, kk_)]),
                          fn=lambda e, tl=tl, kk_=kk_: e.indirect_dma_start(out=XS, out_offset=bass.IndirectOffsetOnAxis(ap=DST[:, tl, kk_:kk_ + 1], axis=0),
                                                                        in_=XN[:, tl, :], in_offset=None))
        S.barrier()
        with ExitStack() as st:
            ps = [K.ps(st, "mb_ps%d" % i, [128, 512], F32) for i in range(6)]
            pst = K.ps(st, "mb_pst", [128, 8, 128], BF16)
            wgu = [K.sb(st, "mb_wgu%d" % i, [128, 8, 1024], BF16) for i in range(2)]
            wd = [K.sb(st, "mb_wd%d" % i, [128, 4, 1024], BF16) for i in range(2)]
            xb = [K.sb(st, "mb_xb%d" % i, [128, D], BF16) for i in range(2)]
            xT = K.sb(st, "mb_xT", [128, 8, 128], BF16)
            sgt = K.sb(st, "mb_sgt", [128, 512], F32)
            hb = K.sb(st, "mb_hb", [128, 512], BF16)
            hT = K.sb(st, "mb_hT", [128, 4, 128], BF16)
            ysb = [K.sb(st, "mb_ysb%d" % i, [128, D], F32) for i in range(2)]
            WGU, WDS = SC["WGU"], SC["WDS"]
            for b in range(NBLK):
                i = b % 2
                for c in range(8):
                    S.dma("pool", None, None, K._bl(["ms_IDXG"]), K._bl(["mb_wgu%d_%d" % (i, c)]),
                          fn=lambda e, b=b, c=c, i=i: e.indirect_dma_start(out=wgu[i][:, c, :], out_offset=None, in_=WGU,
                                                                         in_offset=bass.IndirectOffsetOnAxis(ap=IDXG[:, b, c:c + 1], axis=0)))
                for c in range(4):
                    S.dma("pool", None, None, K._bl(["ms_IDXD"]), K._bl(["mb_wd%d_%d" % (i, c)]),
                          fn=lambda e, b=b, c=c, i=i: e.indirect_dma_start(out=wd[i][:, c, :], out_offset=None, in_=WDS,
                                                                         in_offset=bass.IndirectOffsetOnAxis(ap=IDXD[:, b, c:c + 1], axis=0)))
                for sub in range(SB // 128):
                    j = sub % 2
                    r0 = b * SB + sub * 128
                    K.dma("sp", xb[j][:], XS[r0:r0 + 128, :], ["XS"], ["mb_xb%d" % j])
                    for c in range(8):
                        K.tr(pst[:, c, :], xb[j][:, c * 128:(c + 1) * 128], identb[:], ["mb_xb%d" % j, "identb"], ["mb_pst"])
                    K.op("act", "activation", ["mb_pst"], ["mb_xT"], out=xT[:], in_=pst[:], func=AF.Copy)
                    for k in range(8):
                        K.mm(ps[0][:], xT[:, k, :], wgu[i][:, k, 0:512], ["mb_xT"] + ["mb_wgu%d_%d" % (i, c) for c in range(8)], ["mb_ps0"], start=(k == 0), stop=(k == 7))
                    for k in range(8):
                        K.mm(ps[1][:], xT[:, k, :], wgu[i][:, k, 512:1024], ["mb_xT"] + ["mb_wgu%d_%d" % (i, c) for c in range(8)], ["mb_ps1"], start=(k == 0), stop=(k == 7))
                    K.op("act", "activation", ["mb_ps0"], ["mb_sgt"], out=sgt[:], in_=ps[0][:], func=AF.Silu)
                    K.op("dve", "tensor_tensor", ["mb_ps1", "mb_sgt"], ["mb_hb"], out=hb[:], in0=ps[1][:], in1=sgt[:], op=ALU.mult)
                    for fc in range(4):
                        K.tr(pst[:, fc, :], hb[:, fc * 128:(fc + 1) * 128], identb[:], ["mb_hb", "identb"], ["mb_pst"])
                    K.op("dve", "tensor_copy", ["mb_pst"], ["mb_hT"], out=hT[:], in_=pst[:, 0:4, :])
                    for half in range(2):
                        pj = 2 + 2 * j + half
                        for fc in range(4):
                            K.mm(ps[pj][:], hT[:, fc, :], wd[i][:, fc, half * 512:(half + 1) * 512], ["mb_hT"] + ["mb_wd%d_%d" % (i, c) for c in range(4)], ["mb_ps%d" % pj], start=(fc == 0), stop=(fc == 3))
                        if half == 0:
                            K.op("act", "activation", ["mb_ps%d" % pj], ["mb_ysb%d" % j], out=ysb[j][:, 0:512], in_=ps[pj][:], func=AF.Copy)
                        else:
                            K.op("dve", "tensor_copy", ["mb_ps%d" % pj], ["mb_ysb%d" % j], out=ysb[j][:, 512:1024], in_=ps[pj][:])
                    K.dma("act", YS[r0:r0 + 128, :], ysb[j][:], ["mb_ysb%d" % j], ["YS"])
        S.barrier()
        with ExitStack() as st:
            nfb = K.sb(st, "mc_nfb", [128, D], F32)
            K.dma("sp", nfb[:], Wd["norm_final"].partition_broadcast(128), [], ["mc_nfb"])
            hts = [K.sb(st, "mc_ht%d" % i, [128, D], F32) for i in range(2)]
            y1 = [K.sb(st, "mc_y1%d" % i, [128, D], F32) for i in range(2)]
            y2 = [K.sb(st, "mc_y2%d" % i, [128, D], F32) for i in range(2)]
            ob = [K.sb(st, "mc_ob%d" % i, [128, D], F32) for i in range(2)]
            junk = K.sb(st, "mc_junk", [128, D], F32)
            sss = [K.sb(st, "mc_ss%d" % i, [128, 1], F32) for i in range(2)]
            for tl in range(NTL):
                i = tl % 2
                r0 = base + tl * 128
                K.dma("sp", hts[i][:], SC["H1"][r0:r0 + 128, :], ["H1"], ["mc_ht%d" % i])
                S.dma("pool", None, None, K._bl(["ms_DST", "YS"]), K._bl(["mc_y1%d" % i]),
                      fn=lambda e, tl=tl, i=i: e.indirect_dma_start(out=y1[i][:], out_offset=None, in_=YS, in_offset=bass.IndirectOffsetOnAxis(ap=DST[:, tl, 0:1], axis=0)))
                S.dma("pool", None, None, K._bl(["ms_DST", "YS"]), K._bl(["mc_y2%d" % i]),
                      fn=lambda e, tl=tl, i=i: e.indirect_dma_start(out=y2[i][:], out_offset=None, in_=YS, in_offset=bass.IndirectOffsetOnAxis(ap=DST[:, tl, 1:2], axis=0)))
                K.op("dve", "scalar_tensor_tensor", ["mc_y1%d" % i, "ms_GG", "mc_ht%d" % i], ["mc_ht%d" % i], out=hts[i][:], in0=y1[i][:], scalar=GG[:, tl, 0:1], in1=hts[i][:],
                     op0=ALU.mult, op1=ALU.add)
                K.op("dve", "scalar_tensor_tensor", ["mc_y2%d" % i, "ms_GG", "mc_ht%d" % i], ["mc_ht%d" % i], out=hts[i][:], in0=y2[i][:], scalar=GG[:, tl, 1:2], in1=hts[i][:],
                     op0=ALU.mult, op1=ALU.add)
                K.op("act", "activation", ["mc_ht%d" % i], ["mc_junk", "mc_ss%d" % i], out=junk[:], in_=hts[i][:], func=AF.Square, accum_out=sss[i][:])
                K.op("act", "activation", ["mc_ss%d" % i, "eps6"], ["mc_ss%d" % i], out=sss[i][:], in_=sss[i][:], func=AF.Sqrt, scale=1.0 / D, bias=CONST["eps6"][:])
                K.op("dve", "reciprocal", ["mc_ss%d" % i], ["mc_ss%d" % i], out=sss[i][:], in_=sss[i][:])
                K.op("dve", "scalar_tensor_tensor", ["mc_ht%d" % i, "mc_ss%d" % i, "mc_nfb"], ["mc_ob%d" % i], out=ob[i][:], in0=hts[i][:], scalar=sss[i][:], in1=nfb[:],
                     op0=ALU.mult, op1=ALU.mult)
                K.dma("act", OUT[r0:r0 + 128, :], ob[i][:], ["mc_ob%d" % i], ["OUT"])


def build(T, NSEQ, stop_after=99, debug=False):
    nc = bass.Bass("TRN2", target_bir_lowering=False)
    NTOK = NSEQ * T

    def din(name, shape, dt=F32):
        return nc.dram_tensor(name, list(shape), dt, kind="ExternalInput").ap()

    X = din("x", [NTOK, D])
    MEM = din("mem", [NSEQ * 256, D])
    Wd = {}
    for name, shape in WSHAPES.items():
        Wd[name] = din(name, shape)
    identb_d = din("c_identb", [128, 128], BF16)
    identf_d = din("c_identf", [128, 128], F32)
    OUT = nc.dram_tensor("out", [NTOK, D], F32, kind="ExternalOutput").ap()
    SC = {}
    SC["ZF"] = nc.dram_tensor("sc_zf", [NSEQ, R_TOT, T], F32, kind="Internal").ap() if not debug else \
        nc.dram_tensor("sc_zf", [NSEQ, R_TOT, T], F32, kind="ExternalOutput").ap()
    kindd = "ExternalOutput" if debug else "Internal"
    SC["CK"] = nc.dram_tensor("sc_ck", [NSEQ, T, 128], BF16, kind=kindd).ap()
    SC["CKT"] = nc.dram_tensor("sc_ckt", [NSEQ, 128, T], BF16, kind=kindd).ap()
    SC["H1"] = nc.dram_tensor("sc_h1", [NTOK, D], F32, kind=kindd).ap()
    NSLOT = ((2 * T) // 256 + 32) * 256
    SC["WGU"] = nc.dram_tensor("sc_wgu", [32 * 1024, 1024], BF16, kind="Internal").ap()
    SC["WDS"] = nc.dram_tensor("sc_wds", [32 * 512, 1024], BF16, kind="Internal").ap()
    SC["XS"] = nc.dram_tensor("sc_xs", [NSLOT, D], BF16, kind="Internal").ap()
    SC["YS"] = nc.dram_tensor("sc_ys", [NSLOT, D], F32, kind="Internal").ap()
    SC["YB"] = nc.dram_tensor("sc_yb", [NSEQ, 512, T], BF16, kind=kindd).ap()
    SC["YA"] = nc.dram_tensor("sc_ya", [NSEQ, 512, T], BF16, kind=kindd).ap()
    cdram = {}
    for nm, arr in consts().items():
        if nm not in ("c_identb", "c_identf"):
            cdram[nm] = din(nm, arr.shape, BF16 if arr.dtype == ml_dtypes.bfloat16 else F32)
    with ExitStack() as st:
        S = Sched(nc, st)
        K = Ctx(nc, S)
        CONST = {}
        CONST["identb"] = K.sb(st, "identb", [128, 128], BF16)
        CONST["identf"] = K.sb(st, "identf", [128, 128], F32)
        CONST["eps6"] = K.sb(st, "eps6", [128, 1], F32)
        K.dma("sp", CONST["identb"][:], identb_d, [], ["identb"])
        K.dma("sp", CONST["identf"][:], identf_d, [], ["identf"])
        K.op("dve", "memset", [], ["eps6"], ap=CONST["eps6"][:], constant=1e-6)
        for nm, ap in cdram.items():
            sh = list(ap.shape)
            CONST[nm[2:]] = K.sb(st, nm[2:], sh, ap.dtype)
            K.dma("sp", CONST[nm[2:]][:], ap, [], [nm[2:]])
        for s in range(NSEQ):
            phase1(K, s, T, X, Wd, SC, CONST)
            S.barrier()
            if stop_after >= 2 and not os.environ.get("SKIP_DSA"):
                ex_ = (lambda st_: prepack_gen(K, st_, Wd, SC)) if (s == 0 and stop_after >= 6 and not os.environ.get("MOE_DENSE")) else None
                phase_dsa(K, s, T, Wd, SC, CONST, extra=ex_)
                S.barrier()
            if stop_after >= 3:
                phase_rwkv(K, s, T, Wd, SC, CONST)
                S.barrier()
            if stop_after >= 4:
                phase_mix(K, s, T, X, Wd, SC, CONST)
                S.barrier()
            if stop_after >= 5:
                phase_cross(K, s, T, MEM, Wd, SC, CONST)
                S.barrier()
            if stop_after >= 6:
                if os.environ.get("MOE_DENSE"):
                    phase_moe(K, s, T, Wd, SC, CONST, OUT)
                else:
                    phase_moe_sparse(K, s, T, Wd, SC, CONST, OUT)
                S.barrier()
        S.finish(list(K.B.values()))
        print("ops", S.nops, "waits", S.nwaits)
        S.emit()
    return nc


WSHAPES = {
    "norm_mix": [1, 1024], "w_in": [1, 1024, 4804], "shift_mu": [1, 1792], "rw_w0": [1, 512],
    "rw_w2": [1, 64, 512], "rw_a0": [1, 512], "rw_a2": [1, 64, 512], "rw_g2": [1, 128, 512],
    "rw_k_k": [1, 512], "rw_k_a": [1, 512], "rw_r_k": [1, 8, 64], "rw_ln_w": [1, 512], "rw_ln_b": [1, 512],
    "kv_norm": [1, 128], "w_uk": [1, 128, 8, 64], "w_uv": [1, 128, 8, 64], "w_proj_a": [1, 512, 1024],
    "w_proj_b": [1, 512, 1024], "b_gate": [1, 2048], "w_out": [1, 1024, 1024], "norm_cross": [1, 1024],
    "norm_mem": [1, 1024], "w_cq": [1, 1024, 1024], "w_ckv": [1, 1024, 2048], "w_co": [1, 1024, 1024],
    "norm_ffn": [1, 1024], "w_router_g": [1, 1024, 4], "b_router_g": [1, 4], "w_router_e": [1, 1024, 32],
    "b_router_e": [1, 32], "w_e_gate": [1, 32, 1024, 512], "w_e_up": [1, 32, 1024, 512],
    "w_e_down": [1, 32, 512, 1024], "norm_final": [1024],
}


def consts():
    return {
        "c_identb": np.eye(128, dtype=np.float32).astype(ml_dtypes.bfloat16),
        "c_identf": np.eye(128, dtype=np.float32),
        "c_tri01": (np.arange(128)[None, :] <= np.arange(128)[:, None]).astype(np.float32).astype(ml_dtypes.bfloat16),
        "c_negtri": np.where(np.arange(128)[None, :] <= np.arange(128)[:, None], 0.0, -1e30).astype(np.float32),
        "c_bo": np.kron(np.eye(2), np.ones((64, 64))).astype(np.float32),
        "c_bo64": (np.kron(np.eye(2), np.ones((64, 64))) / 64.0).astype(np.float32),
        "c_maskq": np.block([[np.triu(np.ones((64, 64)), 1), np.triu(np.ones((64, 64)), 0)],
                             [np.triu(np.ones((64, 64)), 1), np.triu(np.ones((64, 64)), 0)]]).astype(np.float32),
        "c_lowm": np.concatenate([np.zeros((64, 64)), np.tril(np.ones((64, 64)), -1)], 0).astype(np.float32),
        "c_resetm": np.tile((np.arange(256) % 64 != 0).astype(np.float32)[None, :], (128, 1)),
        "c_utri": (np.arange(128)[:, None] < np.arange(128)[None, :]).astype(np.float32),
        "c_ones128": np.ones((128, 128), np.float32),
        "c_bstart": np.tile((np.arange(320) * 128.0)[None, :], (128, 1)).astype(np.float32),
        "c_iotap": np.arange(128, dtype=np.float32)[:, None].copy(),
        "c_pw": np.tile((0.5 ** (np.arange(NIT) + 1))[None, :], (128, 1)).astype(np.float32),
    }


def kernel(**inputs):
    x = np.asarray(inputs["x"], dtype=np.float32)
    mem = np.asarray(inputs["mem"], dtype=np.float32)
    B, T, _ = x.shape
    nseq = B // NCORES
    nc = build(T, nseq)
    cs = consts()
    in_maps = []
    for c in range(NCORES):
        m = {"x": np.ascontiguousarray(x[c * nseq:(c + 1) * nseq].reshape(nseq * T, D)),
             "mem": np.ascontiguousarray(mem[c * nseq:(c + 1) * nseq].reshape(nseq * 256, D))}
        for name in WSHAPES:
            m[name] = np.ascontiguousarray(np.asarray(inputs[name], dtype=np.float32))
        m.update(cs)
        in_maps.append(m)
    res = run_bass_kernel_spmd(nc, in_maps, core_ids=list(range(NCORES)))
    out = np.concatenate([r["out"].reshape(nseq, T, D) for r in res.results], axis=0)
    return out.astype(np.float32)
```

```python
from contextlib import ExitStack
import os
import numpy as np
import ml_dtypes
import concourse.bass as bass
import concourse.mybir as mybir
from concourse.bass_utils import run_bass_kernel_spmd

F32 = mybir.dt.float32
BF16 = mybir.dt.bfloat16
AF = mybir.ActivationFunctionType
ALU = mybir.AluOpType
AX = mybir.AxisListType

D = 1024
NCORES = 8


class Buf:
    __slots__ = ("name", "w", "r")

    def __init__(self, name=""):
        self.name = name
        self.w = None
        self.r = {}


class Sched:
    ENG = ("pe", "act", "dve", "pool", "sp")

    def __init__(self, nc, stack, n_dma_sems=10):
        self.nc = nc
        self.streams = {e: [] for e in self.ENG}
        self.sems = {}
        self.count = {}
        for e in self.ENG:
            self.sems[e] = stack.enter_context(nc.semaphore("s_" + e))
            self.count[e] = 0
        self.dma_sems = {}
        self.dma_rr = {}
        for q in ("sp", "act", "pool"):
            lst = []
            for i in range(n_dma_sems if q != "pool" else 28):
                k = "d_%s_%d" % (q, i)
                self.sems[k] = stack.enter_context(nc.semaphore(k))
                self.count[k] = 0
                lst.append(k)
            self.dma_sems[q] = lst
            self.dma_rr[q] = 0
        self.waited = {}
        self.nwaits = 0
        self.nops = 0

    def _wait(self, eng, key, val):
        if val <= 0 or self.waited.get((eng, key), 0) >= val:
            return
        self.waited[(eng, key)] = val
        self.streams[eng].append(("w", key, val))
        self.nwaits += 1

    def _deps(self, eng, reads, writes, own_key):
        for b in reads:
            if b.w is not None:
                self._dep(eng, b.w, own_key)
        for b in writes:
            if b.w is not None:
                self._dep(eng, b.w, own_key)
            for k, v in b.r.items():
                self._dep(eng, (k, v), own_key)

    def _dep(self, eng, ev, own_key):
        k, v = ev
        if k == "pe" and own_key == "pe":
            return
        self._wait(eng, k, v)

    muted = False

    def op(self, eng, fn, reads=(), writes=()):
        if self.muted:
            return
        self._deps(eng, reads, writes, eng)
        self.count[eng] += 1
        v = self.count[eng]
        self.streams[eng].append(("o", fn, eng, 1))
        for b in writes:
            b.w = (eng, v)
            b.r = {}
        for b in reads:
            if b.r.get(eng, 0) < v:
                b.r[eng] = v
        self.nops += 1

    def dma(self, q, out, in_, reads=(), writes=(), fn=None, **kw):
        if self.muted:
            return
        lst = self.dma_sems[q]
        key = lst[self.dma_rr[q] % len(lst)]
        self.dma_rr[q] += 1
        self._wait(q, key, self.count[key])
        self._deps(q, reads, writes, key)
        self.count[key] += 16
        v = self.count[key]
        if fn is None:
            fn = lambda e, out=out, in_=in_, kw=kw: e.dma_start(out=out, in_=in_, **kw)
        self.streams[q].append(("o", fn, key, 16))
        for b in writes:
            b.w = (key, v)
            b.r = {}
        for b in reads:
            if b.r.get(key, 0) < v:
                b.r[key] = v
        self.nops += 1

    def barrier(self):
        for e in self.ENG:
            for k in self.sems:
                if k != e or True:
                    self._wait(e, k, self.count[k])

    def finish(self, bufs, eng="sp"):
        for b in bufs:
            if b.w is not None:
                self._wait(eng, b.w[0], b.w[1])

    def emit(self):
        nc = self.nc
        sems = self.sems
        streams = self.streams
        with nc.Block() as block:
            def run(engobj, lst):
                for it in lst:
                    if it[0] == "w":
                        engobj.wait_ge(sems[it[1]], it[2])
                    else:
                        it[1](engobj).then_inc(sems[it[2]], it[3])

            @block.tensor
            def _(e):
                run(e, streams["pe"])

            @block.scalar
            def _(e):
                run(e, streams["act"])

            @block.vector
            def _(e):
                run(e, streams["dve"])

            @block.gpsimd
            def _(e):
                run(e, streams["pool"])

            @block.sync
            def _(e):
                run(e, streams["sp"])


class Ctx:
    def __init__(self, nc, S):
        self.nc = nc
        self.S = S
        self.B = {}
        self.rr = 0
        self.uid = 0

    def buf(self, name):
        if name not in self.B:
            self.B[name] = Buf(name)
        return self.B[name]

    def _bl(self, lst):
        return [self.buf(x) if isinstance(x, str) else x for x in lst]

    def sb(self, st, name, shape, dt):
        self.uid += 1
        t = st.enter_context(self.nc.sbuf_tensor("%s_u%d" % (name, self.uid), list(shape), dt))
        self.buf(name)
        return t

    def ps(self, st, name, shape, dt):
        self.uid += 1
        t = st.enter_context(self.nc.psum_tensor("%s_u%d" % (name, self.uid), list(shape), dt))
        self.buf(name)
        return t

    def op(self, eng, method, reads, writes, **kw):
        self.S.op(eng, lambda e, m=method, kw=kw: getattr(e, m)(**kw), self._bl(reads), self._bl(writes))

    def mm(self, out, lhsT, rhs, reads, writes, start=True, stop=True, **kw):
        self.S.op("pe", lambda e: e.matmul(out, lhsT, rhs, start=start, stop=stop, **kw),
                  self._bl(reads), self._bl(writes))

    def tr(self, out, in_, ident, reads, writes):
        self.S.op("pe", lambda e: e.transpose(out, in_, ident), self._bl(reads), self._bl(writes))

    def dma(self, q, out, in_, reads, writes, **kw):
        self.S.dma(q, out, in_, self._bl(reads), self._bl(writes), **kw)

    def q(self):
        self.rr += 1
        return ("sp", "act", "pool")[self.rr % 3]


C_RW = 0
C_Q = 1792
C_CKV = 2304
C_QI = 2432
C_KI = 2688
C_WI = 2752
C_G = 2756
R_RW = 0
R_Q = 1792
R_QI = 2304
R_KI = 2560
R_G = 2624
R_WI = 4672
R_TOT = 4676


def load_cast(K, st, tag, w_ap, kin, n, scale_col=None, dt=BF16, engs=("dve", "pool")):
    nc = K.nc
    kc = kin // 128
    wt = K.sb(st, tag, [128, kc, n], dt)
    src = w_ap.rearrange("(c p) n -> p c n", p=128)
    if True:
        stg = [K.sb(st, "%s_stg%d" % (tag, i), [128, n], F32) for i in range(2)]
        for c in range(kc):
            sg = stg[c % 2]
            nm = "%s_stg%d" % (tag, c % 2)
            K.dma(K.q(), sg[:], src[:, c, :], [], [nm])
            eng = engs[c % len(engs)]
            if scale_col is None:
                K.op(eng, "tensor_copy", [nm], [tag], out=wt[:, c, :], in_=sg[:])
            else:
                K.op(eng, "tensor_scalar", [nm, scale_col[1]], [tag], out=wt[:, c, :], in0=sg[:],
                     scalar1=scale_col[0][:, c:c + 1], scalar2=None, op0=ALU.mult)
    return wt


def norm_rows(K, tag, xt, xt_name, ss, junk, eps_scale=1.0 / D):
    K.op("act", "activation", [xt_name], [tag + "_junk", tag + "_ss"], out=junk[:], in_=xt[:], func=AF.Square,
         accum_out=ss[:])
    K.op("act", "activation", [tag + "_ss"], [tag + "_ss"], out=ss[:], in_=ss[:], func=AF.Sqrt,
         scale=eps_scale, bias=1e-6)
    K.op("dve", "reciprocal", [tag + "_ss"], [tag + "_ss"], out=ss[:], in_=ss[:])


def phase1(K, s, T, X, Wd, SC, CONST):
    nc = K.nc
    NT = T // 128
    NB = T // 512
    with ExitStack() as st:
        xnT = K.sb(st, "p1_xnT", [128, 8, T], BF16)
        gm = K.sb(st, "p1_gm", [128, 8], F32)
        K.dma("sp", gm[:], Wd["norm_mix"].rearrange("o (c p) -> p (o c)", p=128), [], ["p1_gm"], allow_slow_non_contiguous=True)
        bg = K.sb(st, "p1_bg", [128, 16], F32)
        K.dma("sp", bg[:], Wd["b_gate"].rearrange("o (c p) -> p (o c)", p=128), [], ["p1_bg"], allow_slow_non_contiguous=True)
        identb = CONST["identb"]
        pst = K.ps(st, "p1_pst", [128, 8, 128], BF16)
        xts = [K.sb(st, "p1_xt%d" % i, [128, D], F32) for i in range(2)]
        xnb = [K.sb(st, "p1_xn%d" % i, [128, D], BF16) for i in range(2)]
        junk = K.sb(st, "p1_junk", [128, D], F32)
        sss = [K.sb(st, "p1_ss%d" % i, [128, 1], F32) for i in range(2)]
        for tt in range(NT):
            i = tt % 2
            xt, xn, ss = xts[i], xnb[i], sss[i]
            K.dma("sp" if i == 0 else "act", xt[:], X[s * T + tt * 128: s * T + (tt + 1) * 128, :], [], ["p1_xt%d" % i])
            K.op("act", "activation", ["p1_xt%d" % i], ["p1_junk", "p1_ss%d" % i], out=junk[:], in_=xt[:],
                 func=AF.Square, accum_out=ss[:])
            K.op("act", "activation", ["p1_ss%d" % i, "eps6"], ["p1_ss%d" % i], out=ss[:], in_=ss[:], func=AF.Sqrt,
                 scale=1.0 / D, bias=CONST["eps6"][:])
            K.op("dve", "reciprocal", ["p1_ss%d" % i], ["p1_ss%d" % i], out=ss[:], in_=ss[:])
            K.op("dve", "tensor_scalar", ["p1_xt%d" % i, "p1_ss%d" % i], ["p1_xn%d" % i], out=xn[:], in0=xt[:],
                 scalar1=ss[:], scalar2=None, op0=ALU.mult)
            for c in range(8):
                K.tr(pst[:, c, :], xn[:, c * 128:(c + 1) * 128], identb[:], ["p1_xn%d" % i, "identb"], ["p1_pst"])
            K.op("pool" if False else "act", "activation", ["p1_pst"], ["p1_xnT"], out=xnT[:, :, tt * 128:(tt + 1) * 128],
                 in_=pst[:], func=AF.Copy)
        import os
        STOP = int(os.environ.get("STOP", "99"))
        if STOP <= 1:
            return
        chunks = []
        for i in range(14):
            chunks.append((C_RW + i * 128, 128, R_RW + i * 128, "fm", None))
        for i in range(4):
            chunks.append((C_Q + i * 128, 128, R_Q + i * 128, "fm", None))
        for i in range(2):
            chunks.append((C_QI + i * 128, 128, R_QI + i * 128, "fm", None))
        chunks.append((C_KI, 64, R_KI, "fm", None))
        chunks.append((C_WI, 4, R_WI, "fm", None))
        for i in range(16):
            chunks.append((C_G + i * 128, 128, R_G + i * 128, "gate", i))
        wsrc = Wd["w_in"].rearrange("o (c p) n -> p (o c) n", p=128)
        wst = [K.sb(st, "p1_wst%d" % i, [128, 8, 132], F32) for i in range(2)]
        wbf = [K.sb(st, "p1_wbf%d" % i, [128, 8, 132], BF16) for i in range(2)]
        stage = [K.sb(st, "p1_stage%d" % i, [128, T], F32) for i in range(2)]
        pss = [K.ps(st, "p1_ps%d" % i, [128, 512], F32) for i in range(4)]
        gmb = gm[:].unsqueeze(2).to_broadcast([128, 8, 128])
        ZF = SC["ZF"]
        for ci, (c0, ncol, r0, kind, gi) in enumerate(chunks):
            i = ci % 2
            K.dma("sp" if i == 0 else "pool", wst[i][:, :, 0:ncol], wsrc[:, :, c0:c0 + ncol], [], ["p1_wst%d" % i])
            K.op("dve", "tensor_tensor", ["p1_wst%d" % i, "p1_gm"], ["p1_wbf%d" % i], out=wbf[i][:, :, 0:ncol],
                 in0=wst[i][:, :, 0:ncol], in1=gm[:].unsqueeze(2).to_broadcast([128, 8, ncol]), op=ALU.mult)
            for tb in range(NB):
                pj = (ci * NB + tb) % 4
                ps = pss[pj]
                for dc in range(8):
                    K.mm(ps[0:ncol, :], wbf[i][:, dc, 0:ncol], xnT[:, dc, tb * 512:(tb + 1) * 512],
                         ["p1_wbf%d" % i, "p1_xnT"], ["p1_ps%d" % pj], start=(dc == 0), stop=(dc == 7))
                if kind == "gate":
                    K.op("act", "activation", ["p1_ps%d" % pj, "p1_bg"], ["p1_stage%d" % i],
                         out=stage[i][0:ncol, tb * 512:(tb + 1) * 512], in_=ps[0:ncol, :], func=AF.Sigmoid,
                         bias=bg[:, gi:gi + 1])
                else:
                    eng = "dve" if tb % 2 == 0 else "act"
                    if eng == "dve":
                        K.op("dve", "tensor_copy", ["p1_ps%d" % pj], ["p1_stage%d" % i],
                             out=stage[i][0:ncol, tb * 512:(tb + 1) * 512], in_=ps[0:ncol, :])
                    else:
                        K.op("act", "activation", ["p1_ps%d" % pj], ["p1_stage%d" % i],
                             out=stage[i][0:ncol, tb * 512:(tb + 1) * 512], in_=ps[0:ncol, :], func=AF.Copy)
            K.dma("act" if i == 0 else "sp", ZF[s, r0:r0 + ncol, :], stage[i][0:ncol, :], ["p1_stage%d" % i], ["ZF"])
        if STOP <= 2:
            return
        i = len(chunks) % 2
        K.dma("sp", wst[i][:, :, 0:128], wsrc[:, :, C_CKV:C_CKV + 128], [], ["p1_wst%d" % i])
        K.op("dve", "tensor_tensor", ["p1_wst%d" % i, "p1_gm"], ["p1_wbf%d" % i], out=wbf[i][:, :, 0:128],
             in0=wst[i][:, :, 0:128], in1=gm[:].unsqueeze(2).to_broadcast([128, 8, 128]), op=ALU.mult)
        ck = [K.sb(st, "p1_ck%d" % j, [128, 128], F32) for j in range(2)]
        ckb = [K.sb(st, "p1_ckb%d" % j, [128, 128], BF16) for j in range(2)]
        ckT = K.sb(st, "p1_ckT", [128, T], BF16)
        for tt in range(NT):
            j = tt % 2
            pj = tt % 4
            ps = pss[pj]
            for dc in range(8):
                K.mm(ps[:, 0:128], xnT[:, dc, tt * 128:(tt + 1) * 128], wbf[i][:, dc, 0:128],
                     ["p1_wbf%d" % i, "p1_xnT"], ["p1_ps%d" % pj], start=(dc == 0), stop=(dc == 7))
            K.op("dve", "tensor_copy", ["p1_ps%d" % pj], ["p1_ck%d" % j], out=ck[j][:], in_=ps[:, 0:128])
            K.op("act", "activation", ["p1_ck%d" % j], ["p1_junk", "p1_ss%d" % j], out=junk[:, 0:128], in_=ck[j][:],
                 func=AF.Square, accum_out=sss[j][:])
            K.op("act", "activation", ["p1_ss%d" % j, "eps6"], ["p1_ss%d" % j], out=sss[j][:], in_=sss[j][:], func=AF.Sqrt,
                 scale=1.0 / 128, bias=CONST["eps6"][:])
            K.op("dve", "reciprocal", ["p1_ss%d" % j], ["p1_ss%d" % j], out=sss[j][:], in_=sss[j][:])
            K.op("dve", "tensor_scalar", ["p1_ck%d" % j, "p1_ss%d" % j], ["p1_ckb%d" % j], out=ckb[j][:], in0=ck[j][:],
                 scalar1=sss[j][:], scalar2=None, op0=ALU.mult)
            K.tr(pst[:, 0, :], ckb[j][:], identb[:], ["p1_ckb%d" % j, "identb"], ["p1_pst"])
            K.op("act", "activation", ["p1_pst"], ["p1_ckT"], out=ckT[:, tt * 128:(tt + 1) * 128], in_=pst[:, 0, :],
                 func=AF.Copy)
            K.dma("sp", SC["CK"][s, tt * 128:(tt + 1) * 128, :], ckb[j][:], ["p1_ckb%d" % j], ["CK"])
        K.dma("sp", SC["CKT"][s, :, :], ckT[:], ["p1_ckT"], ["CKT"])


NIT = 12


def phase_dsa(K, s, T, Wd, SC, CONST, extra=None):
    nc = K.nc
    NT = T // 128
    ZF = SC["ZF"]
    identb, identf = CONST["identb"], CONST["identf"]
    with ExitStack() as st:
        dps = [K.ps(st, "ds_ps%d" % i, [128, 512], F32) for i in range(4)]
        Ob = [K.ps(st, "ds_o%d" % i, [128, 3, 130], F32) for i in range(3)]
        MT = K.ps(st, "ds_mt", [128, 8, 128], BF16)
        wuk = K.sb(st, "ds_wuk", [128, 512], F32)
        K.dma("sp", wuk[:], Wd["w_uk"].rearrange("o r h d -> r (o h d)"), [], ["ds_wuk"])
        wukT = K.sb(st, "ds_wukT", [64, 8, 128], BF16)
        for h in range(8):
            K.tr(dps[3][0:64, 0:128], wuk[:, h * 64:(h + 1) * 64], identf[:], ["ds_wuk", "identf"], ["ds_ps3"])
            K.op("dve", "tensor_copy", ["ds_ps3"], ["ds_wukT"], out=wukT[:, h, :], in_=dps[3][0:64, 0:128])
        kvn = K.sb(st, "ds_kvn", [128, 1], F32)
        K.dma("sp", kvn[:], Wd["kv_norm"].rearrange("o r -> r o"), [], ["ds_kvn"], allow_slow_non_contiguous=True)
        kvn8 = K.sb(st, "ds_kvn8", [128, 1], F32)
        K.op("dve", "tensor_scalar", ["ds_kvn"], ["ds_kvn8"], out=kvn8[:], in0=kvn[:], scalar1=0.125, scalar2=None,
             op0=ALU.mult)
        wuv = K.sb(st, "ds_wuv", [128, 512], F32)
        K.dma("sp", wuv[:], Wd["w_uv"].rearrange("o r h d -> r (o h d)"), [], ["ds_wuv"])
        wuvb = K.sb(st, "ds_wuvb", [128, 512], BF16)
        K.op("dve", "tensor_scalar", ["ds_wuv", "ds_kvn"], ["ds_wuvb"], out=wuvb[:], in0=wuv[:], scalar1=kvn[:],
             scalar2=None, op0=ALU.mult)
        CKT = K.sb(st, "ds_ckt", [128, T], BF16)
        K.dma("sp", CKT[:], SC["CKT"][s, :, :], ["CKT"], ["ds_ckt"])
        CKA = K.sb(st, "ds_cka", [128, NT, 130], BF16)
        K.op("pool", "memset", [], ["ds_cka"], ap=CKA[:], constant=1.0)
        K.dma("sp", CKA[:, :, 0:128], SC["CK"][s, :, :].rearrange("(k p) r -> p k r", p=128), ["CK"], ["ds_cka"])
        kif = K.sb(st, "ds_kif", [64, T], F32)
        K.dma("act", kif[:], ZF[s, R_KI:R_KI + 64, :], ["ZF"], ["ds_kif"])
        kib = K.sb(st, "ds_kib", [64, T], BF16)
        K.op("dve", "tensor_copy", ["ds_kif"], ["ds_kib"], out=kib[:], in_=kif[:])
        qib = K.sb(st, "ds_qib", [64, 4, 128], BF16)
        zl = K.sb(st, "ds_zl", [128, 128], BF16)
        zb = K.sb(st, "ds_zb", [128, 390], BF16)
        K.op("pool", "memset", [], ["ds_zl"], ap=zl[:], constant=0.0)
        K.op("pool", "memset", [], ["ds_zb"], ap=zb[:], constant=0.0)
        tri01, negtri, pw = CONST["tri01"], CONST["negtri"], CONST["pw"]
        qf = K.sb(st, "ds_qf", [64, 8, 128], F32)
        qb = K.sb(st, "ds_qb", [64, 8, 128], BF16)
        qif = K.sb(st, "ds_qif", [64, 4, 128], F32)
        wif = K.sb(st, "ds_wif", [4, 128], F32)
        wit = K.sb(st, "ds_wit", [128, 4], F32)
        qlat = K.sb(st, "ds_qlat", [128, 1024], BF16)
        isc = K.sb(st, "ds_isc", [128, T], F32)
        junk = K.sb(st, "ds_junk", [128, T], BF16)
        rl = [K.sb(st, "ds_rl%d" % i, [128, 512], F32) for i in range(3)]
        maskb = K.sb(st, "ds_mask", [128, T], BF16)
        col = K.sb(st, "ds_col", [128, 8], F32)
        hk = K.sb(st, "ds_hk", [128, NIT], F32)
        junk2 = K.sb(st, "ds_junk2", [128, T], BF16)
        cola = K.sb(st, "ds_cola", [128, 1], F32)
        mts = [K.sb(st, "ds_mts%d" % i, [128, 128], BF16) for i in range(2)]
        ee = [K.sb(st, "ds_e%d" % i, [128, 4, 128], BF16) for i in range(4)]
        pp = [K.sb(st, "ds_p%d" % i, [128, 4, 128], BF16) for i in range(4)]
        rd = K.sb(st, "ds_rd", [128, 8, 1], F32)
        onb = K.sb(st, "ds_onb", [128, 8, 128], BF16)
        onT = K.sb(st, "ds_onT", [128, 8, 128], BF16)
        ybs = K.sb(st, "ds_ybs", [128, 4, 128], BF16)
        maskbs = [maskb, K.sb(st, "ds_mask1", [128, T], BF16)]
        MN = ["ds_mask", "ds_mask1"]

        def select(qt):
            t0 = qt * 128
            nk = qt + 1
            nkeys = nk * 128
            mb = maskbs[qt % 2]
            mn = MN[qt % 2]
            if qt >= 2:
                K.dma("act", qif[:], ZF[s, R_QI:R_QI + 256, t0:t0 + 128].rearrange("(h p) t -> p h t", p=64), ["ZF"], ["ds_qif"])
                K.op("act", "activation", ["ds_qif"], ["ds_qib"], out=qib[:], in_=qif[:], func=AF.Copy)
                K.dma("act", wif[:], ZF[s, R_WI:R_WI + 4, t0:t0 + 128], ["ZF"], ["ds_wif"])
                K.tr(dps[3][:, 0:4], wif[:], identf[0:4, 0:4], ["ds_wif", "identf"], ["ds_ps3"])
                K.op("dve", "tensor_scalar", ["ds_ps3"], ["ds_wit"], out=wit[:], in0=dps[3][:, 0:4], scalar1=1.0 / 16,
                     scalar2=None, op0=ALU.mult)
                yield
                for kb in range((nkeys + 511) // 512):
                    w = min(512, nkeys - kb * 512)
                    ks = slice(kb * 512, kb * 512 + w)
                    for h in range(4):
                        pb = 2 + (h % 2)
                        K.mm(dps[pb][:, 0:w], qib[:, h, :], kib[:, ks], ["ds_qib", "ds_kib"], ["ds_ps%d" % pb])
                        if h == 0:
                            K.op("dve", "tensor_scalar", ["ds_ps%d" % pb, "ds_wit"], ["ds_isc"], out=isc[:, ks], in0=dps[pb][:, 0:w],
                                 scalar1=0.0, scalar2=wit[:, 0:1], op0=ALU.max, op1=ALU.mult)
                        else:
                            K.op("act", "activation", ["ds_ps%d" % pb], ["ds_rl%d" % (h - 1)], out=rl[h - 1][:, 0:w],
                                 in_=dps[pb][:, 0:w], func=AF.Relu)
                            K.op("dve", "scalar_tensor_tensor", ["ds_rl%d" % (h - 1), "ds_wit", "ds_isc"], ["ds_isc"],
                                 out=isc[:, ks], in0=rl[h - 1][:, 0:w], scalar=wit[:, h:h + 1], in1=isc[:, ks],
                                 op0=ALU.mult, op1=ALU.add)
                    yield
                K.op("dve", "tensor_reduce", ["ds_isc"], ["ds_col"], out=col[:, 0:1], in_=isc[:, 0:nkeys], axis=AX.X, op=ALU.max)
                K.op("dve", "tensor_reduce", ["ds_isc"], ["ds_col"], out=col[:, 1:2], in_=isc[:, 0:nkeys], axis=AX.X, op=ALU.min)
                K.op("dve", "tensor_scalar", ["ds_col"], ["ds_col"], out=col[:, 2:3], in0=col[:, 0:1], scalar1=col[:, 1:2],
                     scalar2=2e-6, op0=ALU.subtract, op1=ALU.add)
                K.op("dve", "tensor_scalar", ["ds_col"], ["ds_col"], out=col[:, 3:4], in0=col[:, 1:2], scalar1=-1e-6,
                     scalar2=None, op0=ALU.add)
                K.op("dve", "tensor_scalar", ["pw", "ds_col"], ["ds_hk"], out=hk[:], in0=pw[:], scalar1=col[:, 2:3],
                     scalar2=None, op0=ALU.mult)
                K.op("dve", "tensor_tensor", ["ds_isc", "negtri"], ["ds_isc"], out=isc[:, t0:t0 + 128], in0=isc[:, t0:t0 + 128],
                     in1=negtri[:], op=ALU.add)
                K.op("dve", "tensor_tensor", ["ds_col", "ds_hk"], ["ds_col", "ds_colm"], out=col[:, 4:5], in0=col[:, 3:4], in1=hk[:, 0:1], op=ALU.add)
                yield
                nd = nkeys
                if nkeys >= 1024 and not os.environ.get("NO_ACTCNT"):
                    nd = ((nkeys * 4 // 8) // 128) * 128
                na = nkeys - nd
                for k in range(NIT):
                    K.op("dve", "tensor_scalar", ["ds_isc", "ds_colm"], ["ds_junk", "ds_col"], out=junk[:, 0:nd],
                         in0=isc[:, 0:nd], scalar1=col[:, 4:5], scalar2=None, op0=ALU.is_ge, op1=ALU.add,
                         accum_out=col[:, 5:6])
                    if na > 0:
                        K.op("act", "activation", ["ds_isc", "ds_colm"], ["ds_junk2", "ds_cola"], out=junk2[:, 0:na], in_=isc[:, nd:nkeys],
                             func=AF.Sign, scale=-1.0, bias=col[:, 4:5], accum_out=cola[:, 0:1])
                        K.op("dve", "scalar_tensor_tensor", ["ds_cola", "ds_col"], ["ds_col"], out=col[:, 5:6], in0=cola[:, 0:1], scalar=-0.5,
                             in1=col[:, 5:6], op0=ALU.mult, op1=ALU.add)
                    K.op("dve", "tensor_scalar", ["ds_col", "ds_hk"], ["ds_col"], out=col[:, 6:7], in0=col[:, 5:6],
                         scalar1=255.5 - 0.5 * na, scalar2=hk[:, k:k + 1], op0=ALU.is_ge, op1=ALU.mult)
                    kn = min(k + 1, NIT - 1)
                    dst = col[:, 4:5] if k < NIT - 1 else col[:, 3:4]
                    K.op("dve", "scalar_tensor_tensor", ["ds_col", "ds_colm", "ds_hk"], ["ds_col", "ds_colm"], out=dst, in0=col[:, 6:7], scalar=col[:, 4:5],
                         in1=hk[:, kn:kn + 1], op0=ALU.add, op1=ALU.subtract)
                    yield
                K.op("dve", "tensor_scalar", ["ds_isc", "ds_col"], [mn], out=mb[:, 0:nkeys], in0=isc[:, 0:nkeys],
                     scalar1=col[:, 3:4], scalar2=None, op0=ALU.is_ge)
            else:
                if qt > 0:
                    K.op("pool", "memset", [], [mn], ap=mb[:, 0:t0], constant=1.0)
                K.op("pool", "tensor_copy", ["tri01"], [mn], out=mb[:, t0:t0 + 128], in_=tri01[:])
            yield

        def attend(qt):
            t0 = qt * 128
            nk = qt + 1
            mb = maskbs[qt % 2]
            mn = MN[qt % 2]
            K.dma("sp", qf[:], ZF[s, R_Q:R_Q + 512, t0:t0 + 128].rearrange("(h p) t -> p h t", p=64), ["ZF"], ["ds_qf"])
            K.op("act", "activation", ["ds_qf"], ["ds_qb"], out=qb[:], in_=qf[:], func=AF.Copy)
            for h in range(8):
                K.mm(dps[h // 4][:, (h % 4) * 128:(h % 4 + 1) * 128], wukT[:, h, :], qb[:, h, :], ["ds_wukT", "ds_qb"],
                     ["ds_ps%d" % (h // 4)])
            for j in range(2):
                K.op("act", "activation", ["ds_ps%d" % j, "ds_kvn8"], ["ds_qlat"], out=qlat[:, j * 512:(j + 1) * 512],
                     in_=dps[j][:], func=AF.Copy, scale=kvn8[:, 0:1])
            for bq in range(3):
                K.mm(Ob[bq][:].rearrange("p a b -> p (a b)"), zl[:], zb[:], ["ds_zl", "ds_zb"], ["ds_o%d" % bq], start=True,
                     stop=False, skip_group_check=True)
            yield
            def front(kt):
                par = kt % 2
                K.tr(MT[:, 0, :], mb[:, kt * 128:(kt + 1) * 128], identb[:], [mn, "identb"], ["ds_mt0", "ds_mt1", "ds_mtall"])
                K.op("act", "activation", ["ds_mt0", "ds_mt1", "ds_mtall"], ["ds_mts%d" % par], out=mts[par][:], in_=MT[:, 0, :], func=AF.Copy)
                for j in range(2):
                    ej = 2 * par + j
                    K.mm(dps[j][:], CKT[:, kt * 128:(kt + 1) * 128], qlat[:, j * 512:(j + 1) * 512], ["ds_ckt", "ds_qlat"],
                         ["ds_ps%d" % j])
                    K.op("act", "activation", ["ds_ps%d" % j], ["ds_e%d" % ej], out=ee[ej][:],
                         in_=dps[j][:].rearrange("p (a b) -> p a b", a=4), func=AF.Exp)
            front(0)
            for kt in range(nk):
                par = kt % 2
                mtb = "ds_mt%d" % par
                if kt + 1 < nk:
                    front(kt + 1)
                for j in range(2):
                    ej = 2 * par + j
                    K.op("dve", "tensor_tensor", ["ds_e%d" % ej, "ds_mts%d" % par], ["ds_p%d" % ej], out=pp[ej][:], in0=ee[ej][:],
                         in1=mts[par][:].unsqueeze(1).to_broadcast([128, 4, 128]), op=ALU.mult)
                for j in range(2):
                    ej = 2 * par + j
                    for hh in range(4):
                        h = 4 * j + hh
                        K.mm(Ob[h // 3][:, h % 3, 0:129], pp[ej][:, hh, :], CKA[:, kt, 0:129], ["ds_p%d" % ej, "ds_cka"],
                             ["ds_o%d" % (h // 3)], start=False, stop=(kt == nk - 1), skip_group_check=True)
                yield
            for bq in range(3):
                nh = 3 if bq < 2 else 2
                K.op("dve", "reciprocal", ["ds_o%d" % bq], ["ds_rd"], out=rd[:, 3 * bq:3 * bq + nh, :], in_=Ob[bq][:, 0:nh, 128:129])
                K.op("dve", "tensor_tensor", ["ds_o%d" % bq, "ds_rd"], ["ds_onb"], out=onb[:, 3 * bq:3 * bq + nh, :],
                     in0=Ob[bq][:, 0:nh, 0:128], in1=rd[:, 3 * bq:3 * bq + nh, :].to_broadcast([128, nh, 128]), op=ALU.mult)
            for h in range(8):
                K.tr(MT[:, h, :], onb[:, h, :], identb[:], ["ds_onb", "identb"], ["ds_mt0", "ds_mt1", "ds_mtall"])
            K.op("act", "activation", ["ds_mt0", "ds_mt1", "ds_mtall"], ["ds_onT"], out=onT[:], in_=MT[:], func=AF.Copy)
            for h in range(8):
                K.mm(dps[0][(h % 2) * 64:(h % 2 + 1) * 64, (h // 2) * 128:(h // 2 + 1) * 128], wuvb[:, h * 64:(h + 1) * 64],
                     onT[:, h, :], ["ds_wuvb", "ds_onT"], ["ds_ps0"])
            K.op("dve", "tensor_copy", ["ds_ps0"], ["ds_ybs"], out=ybs[:], in_=dps[0][:].rearrange("p (a b) -> p a b", a=4))
            K.dma("sp", SC["YB"][s, :, t0:t0 + 128].rearrange("(c p) t -> p c t", p=128), ybs[:], ["ds_ybs"], ["YB"])
            yield

        xg = extra(st) if extra is not None else None
        for step in range(NT + 1):
            gens = []
            if step >= 1:
                gens.append(attend(step - 1))
            if step < NT:
                gens.append(select(step))
            if xg is not None:
                try:
                    next(xg)
                except StopIteration:
                    xg = None
            while gens:
                for g in list(gens):
                    try:
                        next(g)
                    except StopIteration:
                        gens.remove(g)
        if xg is not None:
            for _ in xg:
                pass


class _Stop(Exception):
    pass


def phase_rwkv(K, s, T, Wd, SC, CONST):
    _phase_rwkv(K, s, T, Wd, SC, CONST)
    K.S.muted = False


def _phase_rwkv(K, s, T, Wd, SC, CONST):
    nc = K.nc
    TBK = 256
    NCH = TBK // 64
    NBK = T // TBK
    ZF = SC["ZF"]
    identf = CONST["identf"]
    bo, bo64, maskq, lowm, resetm = CONST["bo"], CONST["bo64"], CONST["maskq"], CONST["lowm"], CONST["resetm"]
    with ExitStack() as st:
        rp = [K.ps(st, "rk_p%d" % i, [128, 512], F32) for i in range(8)]
        RP = ["rk_p%d" % i for i in range(8)]

        def colload(tag, ap512, n=4):
            t = K.sb(st, tag, [128, n], F32)
            K.dma("sp", t[:], ap512.rearrange("o (c p) -> p (o c)", p=128), [], [tag], allow_slow_non_contiguous=True)
            return t
        mu = colload("rk_mu", Wd["shift_mu"], 14)
        w0c = colload("rk_w0c", Wd["rw_w0"])
        a0c = colload("rk_a0c", Wd["rw_a0"])
        kkc = colload("rk_kkc", Wd["rw_k_k"])
        kac = colload("rk_kac", Wd["rw_k_a"])
        rkc = colload("rk_rkc", Wd["rw_r_k"].rearrange("o h d -> o (h d)"))
        lnw = colload("rk_lnw", Wd["rw_ln_w"])
        lnb = colload("rk_lnb", Wd["rw_ln_b"])
        w2a2 = K.sb(st, "rk_w2a2", [128, 512], F32)
        K.dma("sp", w2a2[0:64, :], Wd["rw_w2"][0], [], ["rk_w2a2"])
        K.dma("sp", w2a2[64:128, :], Wd["rw_a2"][0], [], ["rk_w2a2"])
        g2 = K.sb(st, "rk_g2", [128, 512], F32)
        K.dma("sp", g2[:], Wd["rw_g2"][0], [], ["rk_g2"])
        epsg = K.sb(st, "rk_epsg", [128, 1], F32)
        K.op("dve", "memset", [], ["rk_epsg"], ap=epsg[:], constant=64e-5)
        zin = K.sb(st, "rk_zin", [128, 14, TBK + 1], F32)
        zs = K.sb(st, "rk_zs", [128, 14, TBK], F32)
        tw = K.sb(st, "rk_tw", [128, TBK], F32)
        sg = K.sb(st, "rk_sg", [128, TBK], F32)

        def t4(tag):
            return K.sb(st, tag, [128, 4, TBK], F32)
        lw, aa, gg, LL, eL, enL, eLm, kk, t1, kp, bb, bon, Yb = [t4("rk_" + n) for n in
            ("lw", "aa", "gg", "LL", "eL", "enL", "eLm", "kk", "t1", "kp", "bb", "bon", "Yb")]
        QR = K.sb(st, "rk_QR", [128, 4, NCH, 2, 64], F32)
        KB = K.sb(st, "rk_KB", [128, 4, NCH, 2, 64], F32)
        gC = K.sb(st, "rk_gC", [128, 4, NCH], F32)
        M = K.sb(st, "rk_M", [128, 4, 64], F32)
        K.op("dve", "memset", [], ["rk_M"], ap=M[:], constant=0.0)
        KBTs = [K.sb(st, "rk_KBT%d" % i, [128, 4, 128], F32) for i in range(2)]
        VTs = [K.sb(st, "rk_VT%d" % i, [64, 4, 128], F32) for i in range(2)]
        ATs = [K.sb(st, "rk_AT%d" % i, [128, 8, 128], F32) for i in range(2)]
        DDT = BF16 if os.environ.get("RW_BF16", "1") == "1" else F32
        Am = [K.sb(st, "rk_Am%d" % i, [128, 8, 64], DDT) for i in range(2)]
        Bm = [K.sb(st, "rk_Bm%d" % i, [128, 8, 64], DDT) for i in range(2)]
        Pm = [K.sb(st, "rk_Pm%d" % i, [128, 8, 64], DDT) for i in range(2)]
        PmFs = [K.sb(st, "rk_PmF%d" % i, [128, 8, 64], F32) for i in range(2)]
        Rs = K.sb(st, "rk_Rs", [128, 512], F32)
        Us = K.sb(st, "rk_Us", [128, 512], F32)
        yab = K.sb(st, "rk_yab", [128, 4, TBK], BF16)
        H = slice(64, 128)

        def v4(t):
            return t[:].rearrange("p c (n t) -> p c n t", t=64)

        def bc(colt, n=4, w=TBK):
            return colt[:].unsqueeze(2).to_broadcast([128, n, w])

        RS = float(os.environ.get("RSTOP", "99"))

        def chk(k):
            if RS <= k:
                K.S.muted = True

        for tb in range(NBK):
            t0 = tb * TBK
            if tb == 0:
                K.op("dve", "memset", [], ["rk_zin"], ap=zin[:, :, 0:1], constant=0.0)
                K.dma("sp", zin[:, :, 1:TBK + 1], ZF[s, 0:1792, 0:TBK].rearrange("(c p) t -> p c t", p=128), ["ZF"], ["rk_zin"])
            else:
                K.dma("sp", zin[:, :, :], ZF[s, 0:1792, t0 - 1:t0 + TBK].rearrange("(c p) t -> p c t", p=128), ["ZF"], ["rk_zin"])
            K.op("dve", "tensor_tensor", ["rk_zin"], ["rk_zs"], out=zs[:], in0=zin[:, :, 0:TBK], in1=zin[:, :, 1:TBK + 1], op=ALU.subtract)
            for c14 in range(14):
                K.op("dve", "scalar_tensor_tensor", ["rk_zs", "rk_mu", "rk_zin"], ["rk_zs"], out=zs[:, c14, :], in0=zs[:, c14, :], scalar=mu[:, c14:c14 + 1],
                     in1=zin[:, c14, 1:TBK + 1], op0=ALU.mult, op1=ALU.add)
            chk(1)
            r_, k_, v_ = zs[:, 0:4, :], zs[:, 4:8, :], zs[:, 8:12, :]
            K.op("act", "activation", ["rk_zs"], ["rk_tw"], out=tw[0:64, :], in_=zs[0:64, 12, :], func=AF.Tanh)
            K.op("act", "activation", ["rk_zs"], ["rk_sg"], out=sg[:], in_=zs[:, 13, :], func=AF.Sigmoid)
            for cc in range(4):
                cs = slice(cc * 128, (cc + 1) * 128)
                K.mm(rp[0][:, 0:TBK], w2a2[0:64, cs], tw[0:64, :], ["rk_w2a2", "rk_tw"], [RP[0]])
                K.op("act", "activation", [RP[0], "rk_w0c"], ["rk_lw"], out=lw[:, cc, :], in_=rp[0][:, 0:TBK], func=AF.Sigmoid, bias=w0c[:, cc:cc + 1])
                K.mm(rp[1][:, 0:TBK], w2a2[H, cs], zs[H, 12, :], ["rk_w2a2", "rk_zs"], [RP[1]])
                K.op("act", "activation", [RP[1], "rk_a0c"], ["rk_aa"], out=aa[:, cc, :], in_=rp[1][:, 0:TBK], func=AF.Sigmoid, bias=a0c[:, cc:cc + 1])
                K.mm(rp[2][:, 0:TBK], g2[:, cs], sg[:], ["rk_g2", "rk_sg"], [RP[2]])
                K.op("dve", "tensor_copy", [RP[2]], ["rk_gg"], out=gg[:, cc, :], in_=rp[2][:, 0:TBK])
            chk(2)
            K.op("dve", "tensor_scalar", ["rk_lw"], ["rk_lw"], out=lw[:], in0=lw[:], scalar1=-0.6065306597126334, scalar2=None, op0=ALU.mult)
            for cc in range(4):
                K.op("dve", "tensor_tensor_scan", ["rk_lw", "resetm"], ["rk_LL"], out=LL[:, cc, :], data0=resetm[:], data1=lw[:, cc, :],
                     initial=0.0, op0=ALU.mult, op1=ALU.add)
            K.op("act", "activation", ["rk_LL"], ["rk_eL"], out=eL[:], in_=LL[:], func=AF.Exp)
            K.op("act", "activation", ["rk_LL"], ["rk_enL"], out=enL[:], in_=LL[:], func=AF.Exp, scale=-1.0)
            K.op("dve", "tensor_tensor", ["rk_LL", "rk_lw"], ["rk_t1"], out=t1[:], in0=LL[:], in1=lw[:], op=ALU.subtract)
            K.op("act", "activation", ["rk_t1"], ["rk_eLm"], out=eLm[:], in_=t1[:], func=AF.Exp)
            K.op("dve", "tensor_tensor", ["rk_zs", "rk_kkc"], ["rk_kk"], out=kk[:], in0=k_, in1=bc(kkc), op=ALU.mult)
            K.op("pool", "tensor_tensor", ["rk_kk"], ["rk_t1"], out=t1[:], in0=kk[:], in1=kk[:], op=ALU.mult)
            for cc in range(4):
                K.mm(rp[cc % 4][:, 0:TBK], bo[:], t1[:, cc, :], ["bo", "rk_t1"], [RP[cc % 4]])
                K.op("act", "activation", [RP[cc % 4]], ["rk_kp"], out=kp[:, cc, :], in_=rp[cc % 4][:, 0:TBK], func=AF.Sqrt)
            K.op("dve", "tensor_scalar", ["rk_kp"], ["rk_kp"], out=kp[:], in0=kp[:], scalar1=1e-12, scalar2=None, op0=ALU.max)
            K.op("dve", "reciprocal", ["rk_kp"], ["rk_kp"], out=kp[:], in_=kp[:])
            K.op("dve", "tensor_tensor", ["rk_kk", "rk_kp"], ["rk_kk"], out=kk[:], in0=kk[:], in1=kp[:], op=ALU.mult)
            for cc in range(4):
                K.op("dve", "tensor_scalar", ["rk_aa", "rk_kac"], ["rk_t1"], out=t1[:, cc, :], in0=aa[:, cc, :], scalar1=-1.0, scalar2=kac[:, cc:cc + 1],
                     op0=ALU.add, op1=ALU.mult)
            K.op("dve", "scalar_tensor_tensor", ["rk_t1", "rk_zs"], ["rk_kp"], out=kp[:], in0=t1[:], scalar=1.0, in1=k_, op0=ALU.add, op1=ALU.mult)
            K.op("pool", "tensor_tensor", ["rk_kk", "rk_aa"], ["rk_bb"], out=bb[:], in0=kk[:], in1=aa[:], op=ALU.mult)
            K.op("dve", "tensor_tensor", ["rk_zs", "rk_eL"], ["rk_QR"], out=QR[:, :, :, 1, :], in0=r_.rearrange("p c (n t) -> p c n t", t=64), in1=v4(eL), op=ALU.mult)
            K.op("dve", "tensor_tensor", ["rk_kk", "rk_eLm"], ["rk_QR"], out=QR[:, :, :, 0, :], in0=v4(kk), in1=v4(eLm), op=ALU.mult)
            K.op("dve", "tensor_tensor", ["rk_kp", "rk_enL"], ["rk_KB"], out=KB[:, :, :, 0, :], in0=v4(kp), in1=v4(enL), op=ALU.mult)
            K.op("pool", "tensor_tensor", ["rk_bb", "rk_enL"], ["rk_KB"], out=KB[:, :, :, 1, :], in0=v4(bb), in1=v4(enL), op=ALU.mult)
            K.op("dve", "tensor_copy", ["rk_eL"], ["rk_gC"], out=gC[:], in_=v4(eL)[:, :, :, 63])
            K.op("pool", "tensor_tensor", ["rk_zs", "rk_kp"], ["rk_t1"], out=t1[:], in0=r_, in1=kp[:], op=ALU.mult)
            K.op("pool", "tensor_tensor", ["rk_t1", "rk_rkc"], ["rk_t1"], out=t1[:], in0=t1[:], in1=bc(rkc), op=ALU.mult)
            for cc in range(4):
                K.mm(rp[cc % 4][:, 0:TBK], bo[:], t1[:, cc, :], ["bo", "rk_t1"], [RP[cc % 4]])
                K.op("dve", "tensor_tensor", [RP[cc % 4], "rk_zs"], ["rk_bon"], out=bon[:, cc, :], in0=rp[cc % 4][:, 0:TBK], in1=zs[:, 8 + cc, :], op=ALU.mult)
            chk(3)
            def ev(t, par):
                return t.rearrange("p (a two) b -> p a two b", two=2)[:, :, par, :]

            def pre(c):
                q = c % 2
                KBT, VT, AT, PmF = KBTs[q], VTs[q], ATs[q], PmFs[q]
                nKBT, nVT, nAT, nPmF = "rk_KBT%d" % q, "rk_VT%d" % q, "rk_AT%d" % q, "rk_PmF%d" % q
                for cc in range(4):
                    K.tr(rp[0][:, cc * 128:(cc + 1) * 128], KB[:, cc, c, :, :].rearrange("p a b -> p (a b)"), identf[:], ["rk_KB", "identf"], [RP[0]])
                    K.tr(rp[1][0:64, cc * 128:(cc + 1) * 128], zs[:, 8 + cc, c * 64:(c + 1) * 64], identf[:], ["rk_zs", "identf"], [RP[1]])
                K.op("act", "activation", [RP[0]], [nKBT], out=KBT[:].rearrange("p a b -> p (a b)"), in_=rp[0][:], func=AF.Copy)
                K.op("dve", "tensor_copy", [RP[1]], [nVT], out=VT[:].rearrange("p a b -> p (a b)"), in_=rp[1][0:64, :])
                yield
                for h in range(8):
                    cc, h2 = h // 2, h % 2
                    rows = slice(h2 * 64, (h2 + 1) * 64)
                    K.mm(rp[2 + h2][:, cc * 128:(cc + 1) * 128], KB[rows, cc, c, :, :].rearrange("p a b -> p (a b)"),
                         QR[rows, cc, c, :, :].rearrange("p a b -> p (a b)"), ["rk_KB", "rk_QR"], [RP[2 + h2]])
                    K.mm(rp[h2][H, cc * 64:(cc + 1) * 64], QR[rows, cc, c, 0, :], KB[rows, cc, c, 1, :], ["rk_QR", "rk_KB"], [RP[h2]])
                for h2 in range(2):
                    K.op("dve", "tensor_tensor", [RP[2 + h2], "maskq"], [nAT], out=ev(AT[:], h2),
                         in0=rp[2 + h2][:].rearrange("p (a b) -> p a b", a=4), in1=maskq[:].unsqueeze(1).to_broadcast([128, 4, 128]), op=ALU.mult)
                    K.op("dve", "tensor_tensor", [RP[h2], "lowm"], ["rk_Bm0"], out=ev(Bm[0][H, :, :], h2),
                         in0=rp[h2][H, 0:256].rearrange("p (a b) -> p a b", a=4), in1=lowm[H, :].unsqueeze(1).to_broadcast([64, 4, 64]), op=ALU.mult)
                K.op("act", "activation", [nAT], ["rk_Am0"], out=Am[0][H, :, :], in_=AT[H, :, 0:64], func=AF.Copy)
                K.op("dve", "tensor_tensor", ["identf", nAT], ["rk_Pm0"], out=Pm[0][H, :, :],
                     in0=identf[H, 64:128].unsqueeze(1).to_broadcast([64, 8, 64]), in1=AT[H, :, 0:64], op=ALU.subtract)
                yield
                for lvl in range(5):
                    ci, ni = lvl % 2, (lvl + 1) % 2
                    An, Bn, Pn = "rk_Am%d" % ni, "rk_Bm%d" % ni, "rk_Pm%d" % ni
                    Ac, Bc, Pc = "rk_Am%d" % ci, "rk_Bm%d" % ci, "rk_Pm%d" % ci
                    for h in range(8):
                        hs = slice(h * 64, (h + 1) * 64)
                        if lvl < 4:
                            K.mm(rp[2][H, hs], Bm[ci][H, h, :], Am[ci][H, h, :], [Ac, Bc], [RP[2]])
                        K.mm(rp[3][H, hs], Am[ci][H, h, :], Bm[ci][H, h, :], [Ac, Bc], [RP[3]])
                    if lvl < 4:
                        K.op("act", "activation", [RP[2]], [An], out=Am[ni][H, :, :], in_=rp[2][H, :].rearrange("p (a b) -> p a b", a=8), func=AF.Copy)
                    K.op("dve", "tensor_copy", [RP[3]], [Bn], out=Bm[ni][H, :, :], in_=rp[3][H, :].rearrange("p (a b) -> p a b", a=8))
                    yield
                    for h in range(8):
                        hs = slice(h * 64, (h + 1) * 64)
                        K.mm(rp[0][H, hs], Bm[ni][H, h, :], Pm[ci][H, h, :], [Bn, Pc], [RP[0]])
                    if lvl < 4:
                        K.op("dve", "tensor_tensor", [RP[0], Pc], [Pn], out=Pm[ni][H, :, :], in0=rp[0][H, :].rearrange("p (a b) -> p a b", a=8),
                             in1=Pm[ci][H, :, :], op=ALU.add)
                    else:
                        K.op("dve", "tensor_tensor", [RP[0], Pc], [nPmF], out=PmF[H, :, :], in0=rp[0][H, :].rearrange("p (a b) -> p a b", a=8),
                             in1=Pm[ci][H, :, :], op=ALU.add)
                    yield

            def post(c):
                q = c % 2
                KBT, VT, AT, PF = KBTs[q], VTs[q], ATs[q], PmFs[q]
                nKBT, nVT, nAT, PFn = "rk_KBT%d" % q, "rk_VT%d" % q, "rk_AT%d" % q, "rk_PmF%d" % q
                Rs3 = Rs[H, :].rearrange("p (a b) -> p a b", a=8)
                for h in range(8):
                    cc, h2 = h // 2, h % 2
                    rows = slice(h2 * 64, (h2 + 1) * 64)
                    hs = slice(h * 64, (h + 1) * 64)
                    K.mm(rp[6 + h2][H, cc * 64:(cc + 1) * 64], QR[rows, cc, c, 0, :], M[rows, cc, :], ["rk_QR", "rk_M"], [RP[6 + h2]])
                    K.mm(rp[4][H, hs], AT[0:64, h, 0:64], VT[0:64, cc, h2 * 64:(h2 + 1) * 64], [nAT, nVT], [RP[4]])
                for h2 in range(2):
                    K.op("act", "activation", [RP[6 + h2]], ["rk_Rs"], out=ev(Rs3, h2), in_=rp[6 + h2][H, 0:256].rearrange("p (a b) -> p a b", a=4), func=AF.Copy)
                K.op("dve", "tensor_tensor", [RP[4], "rk_Rs"], ["rk_Rs"], out=Rs[H, :], in0=rp[4][H, :], in1=Rs[H, :], op=ALU.add)
                yield
                for h in range(8):
                    hs = slice(h * 64, (h + 1) * 64)
                    K.mm(rp[5][H, hs], PF[H, h, :], Rs[H, hs], [PFn, "rk_Rs"], [RP[5]])
                K.op("act", "activation", [RP[5]], ["rk_Us"], out=Us[H, :], in_=rp[5][H, :], func=AF.Copy, scale=-1.0)
                yield
                for h in range(8):
                    cc, h2 = h // 2, h % 2
                    rows = slice(h2 * 64, (h2 + 1) * 64)
                    hs = slice(h * 64, (h + 1) * 64)
                    ys = slice(cc * 64, (cc + 1) * 64)
                    K.mm(rp[6 + h2][rows, ys], M[rows, cc, :], QR[rows, cc, c, 1, :], ["rk_M", "rk_QR"], [RP[6 + h2]])
                    K.mm(rp[4][rows, ys], VT[0:64, cc, h2 * 64:(h2 + 1) * 64], AT[0:64, h, 64:128], [nVT, nAT], [RP[4]])
                    K.mm(rp[5][rows, ys], Us[H, hs], AT[H, h, 64:128], ["rk_Us", nAT], [RP[5]])
                for h2 in range(2):
                    rows = slice(h2 * 64, (h2 + 1) * 64)
                    K.op("act", "activation", [RP[6 + h2]], ["rk_Yb"], out=Yb[rows, :, c * 64:(c + 1) * 64],
                         in_=rp[6 + h2][rows, 0:256].rearrange("p (a b) -> p a b", a=4), func=AF.Copy)
                yv = Yb[:, :, c * 64:(c + 1) * 64]
                K.op("dve", "tensor_tensor", [RP[4], "rk_Yb"], ["rk_Yb"], out=yv, in0=rp[4][:, 0:256].rearrange("p (a b) -> p a b", a=4), in1=yv, op=ALU.add)
                K.op("dve", "tensor_tensor", [RP[5], "rk_Yb"], ["rk_Yb"], out=yv, in0=rp[5][:, 0:256].rearrange("p (a b) -> p a b", a=4), in1=yv, op=ALU.add)
                yield
                for cc in range(4):
                    for h2 in range(2):
                        h = 2 * cc + h2
                        rows = slice(h2 * 64, (h2 + 1) * 64)
                        hs = slice(h * 64, (h + 1) * 64)
                        K.mm(rp[4][rows, cc * 64:(cc + 1) * 64], KBT[0:64, cc, rows], VT[0:64, cc, rows], [nKBT, nVT], [RP[4]])
                        K.mm(rp[5][rows, cc * 64:(cc + 1) * 64], KBT[H, cc, rows], Us[H, hs], [nKBT, "rk_Us"], [RP[5]])
                K.op("dve", "tensor_tensor", [RP[4], "rk_M"], ["rk_M"], out=M[:], in0=rp[4][:, 0:256].rearrange("p (a b) -> p a b", a=4), in1=M[:], op=ALU.add)
                K.op("dve", "tensor_tensor", [RP[5], "rk_M"], ["rk_M"], out=M[:], in0=rp[5][:, 0:256].rearrange("p (a b) -> p a b", a=4), in1=M[:], op=ALU.add)
                K.op("dve", "tensor_tensor", ["rk_M", "rk_gC"], ["rk_M"], out=M[:], in0=M[:],
                     in1=gC[:, :, c:c + 1].to_broadcast([128, 4, 64]), op=ALU.mult)
                yield

            for step in range(NCH + 1):
                gens = []
                if step >= 1:
                    gens.append(post(step - 1))
                if step < NCH:
                    gens.append(pre(step))
                while gens:
                    for g in list(gens):
                        try:
                            next(g)
                        except StopIteration:
                            gens.remove(g)
            chk(7)
            for cc in range(4):
                K.mm(rp[0][:, 0:TBK], bo64[:], Yb[:, cc, :], ["bo64", "rk_Yb"], [RP[0]])
                K.op("dve", "tensor_tensor", ["rk_Yb", RP[0]], ["rk_t1"], out=t1[:, cc, :], in0=Yb[:, cc, :], in1=rp[0][:, 0:TBK], op=ALU.subtract)
                K.op("pool", "tensor_tensor", ["rk_t1"], ["rk_kk"], out=kk[:, cc, :], in0=t1[:, cc, :], in1=t1[:, cc, :], op=ALU.mult)
                K.mm(rp[1][:, 0:TBK], bo64[:], kk[:, cc, :], ["bo64", "rk_kk"], [RP[1]])
                K.op("act", "activation", [RP[1], "rk_epsg"], ["rk_kp"], out=kp[:, cc, :], in_=rp[1][:, 0:TBK], func=AF.Sqrt, bias=epsg[:])
            K.op("dve", "reciprocal", ["rk_kp"], ["rk_kp"], out=kp[:], in_=kp[:])
            K.op("dve", "tensor_tensor", ["rk_t1", "rk_kp"], ["rk_t1"], out=t1[:], in0=t1[:], in1=kp[:], op=ALU.mult)
            K.op("pool", "tensor_tensor", ["rk_t1", "rk_lnw"], ["rk_t1"], out=t1[:], in0=t1[:], in1=bc(lnw), op=ALU.mult)
            K.op("pool", "tensor_tensor", ["rk_t1", "rk_lnb"], ["rk_t1"], out=t1[:], in0=t1[:], in1=bc(lnb), op=ALU.add)
            K.op("dve", "tensor_tensor", ["rk_t1", "rk_bon"], ["rk_t1"], out=t1[:], in0=t1[:], in1=bon[:], op=ALU.add)
            K.op("dve", "tensor_tensor", ["rk_t1", "rk_gg"], ["rk_yab"], out=yab[:], in0=t1[:], in1=gg[:], op=ALU.mult)
            K.dma("sp", SC["YA"][s, :, t0:t0 + TBK].rearrange("(c p) t -> p c t", p=128), yab[:], ["rk_yab"], ["YA"])


def norm_T(K, tag, src_tile, src_name, xn, ss, junk, pst, dstT, col0, identb, eps):
    K.op("act", "activation", [src_name], [tag + "junk", tag + "ss"], out=junk[:], in_=src_tile, func=AF.Square, accum_out=ss[:])
    K.op("act", "activation", [tag + "ss", "eps6"], [tag + "ss"], out=ss[:], in_=ss[:], func=AF.Sqrt, scale=1.0 / D, bias=eps[:])
    K.op("dve", "reciprocal", [tag + "ss"], [tag + "ss"], out=ss[:], in_=ss[:])
    K.op("dve", "tensor_scalar", [src_name, tag + "ss"], [tag + "xn"], out=xn[:], in0=src_tile, scalar1=ss[:], scalar2=None, op0=ALU.mult)
    for c in range(8):
        K.tr(pst[:, c, :], xn[:, c * 128:(c + 1) * 128], identb[:], [tag + "xn", "identb"], [tag + "pst"])
    K.op("act", "activation", [tag + "pst"], [dstT[1]], out=dstT[0][:, :, col0:col0 + 128], in_=pst[:], func=AF.Copy)


def phase_mix(K, s, T, X, Wd, SC, CONST):
    ZF = SC["ZF"]
    with ExitStack() as st:
        wpa = load_cast(K, st, "mx_wpa", Wd["w_proj_a"][0], 512, 1024)
        wpb = load_cast(K, st, "mx_wpb", Wd["w_proj_b"][0], 512, 1024)
        wout = load_cast(K, st, "mx_wout", Wd["w_out"][0], 1024, 1024)
        ps = [K.ps(st, "mx_ps%d" % i, [128, 512], F32) for i in range(4)]
        ya = K.sb(st, "mx_ya", [128, 4, 512], BF16)
        yb = K.sb(st, "mx_yb", [128, 4, 512], BF16)
        G = K.sb(st, "mx_G", [128, 16, 512], F32)
        ta = K.sb(st, "mx_ta", [128, 512], F32)
        tb_ = K.sb(st, "mx_tb", [128, 512], F32)
        mixT = K.sb(st, "mx_mixT", [128, 8, 512], BF16)
        xt = [K.sb(st, "mx_xt%d" % i, [128, D], F32) for i in range(2)]
        for tb in range(T // 512):
            t0 = tb * 512
            K.dma("sp", ya[:], SC["YA"][s, :, t0:t0 + 512].rearrange("(c p) t -> p c t", p=128), ["YA"], ["mx_ya"])
            K.dma("act", yb[:], SC["YB"][s, :, t0:t0 + 512].rearrange("(c p) t -> p c t", p=128), ["YB"], ["mx_yb"])
            K.dma("sp", G[:], ZF[s, R_G:R_G + 2048, t0:t0 + 512].rearrange("(c p) t -> p c t", p=128), ["ZF"], ["mx_G"])
            for cc in range(8):
                cs = slice(cc * 128, (cc + 1) * 128)
                for k in range(4):
                    K.mm(ps[0][:], wpa[:, k, cs], ya[:, k, :], ["mx_wpa", "mx_ya"], ["mx_ps0"], start=(k == 0), stop=(k == 3))
                for k in range(4):
                    K.mm(ps[1][:], wpb[:, k, cs], yb[:, k, :], ["mx_wpb", "mx_yb"], ["mx_ps1"], start=(k == 0), stop=(k == 3))
                K.op("dve", "tensor_tensor", ["mx_ps0", "mx_G"], ["mx_ta"], out=ta[:], in0=ps[0][:], in1=G[:, cc, :], op=ALU.mult)
                K.op("dve", "tensor_tensor", ["mx_ps1", "mx_G"], ["mx_tb"], out=tb_[:], in0=ps[1][:], in1=G[:, 8 + cc, :], op=ALU.mult)
                K.op("pool", "tensor_tensor", ["mx_ta", "mx_tb"], ["mx_mixT"], out=mixT[:, cc, :], in0=ta[:], in1=tb_[:], op=ALU.add)
            for tt in range(4):
                i = tt % 2
                r0 = s * T + t0 + tt * 128
                K.dma("act", xt[i][:], X[r0:r0 + 128, :], [], ["mx_xt%d" % i])
                for half in range(2):
                    pj = 2 + half
                    for k in range(8):
                        K.mm(ps[pj][:], mixT[:, k, tt * 128:(tt + 1) * 128], wout[:, k, half * 512:(half + 1) * 512], ["mx_mixT", "mx_wout"],
                             ["mx_ps%d" % pj], start=(k == 0), stop=(k == 7))
                    K.op("dve", "tensor_tensor", ["mx_ps%d" % pj, "mx_xt%d" % i], ["mx_xt%d" % i], out=xt[i][:, half * 512:(half + 1) * 512],
                         in0=ps[pj][:], in1=xt[i][:, half * 512:(half + 1) * 512], op=ALU.add)
                K.dma("sp", SC["H1"][r0:r0 + 128, :], xt[i][:], ["mx_xt%d" % i], ["H1"])


def colvec(K, st, tag, ap, n=8):
    t = K.sb(st, tag, [128, n], F32)
    K.dma("sp", t[:], ap.rearrange("o (c p) -> p (o c)", p=128), [], [tag], allow_slow_non_contiguous=True)
    return t


def phase_cross(K, s, T, MEM, Wd, SC, CONST):
    identb = CONST["identb"]
    with ExitStack() as st:
        nrc = colvec(K, st, "cx_nrc", Wd["norm_cross"])
        nrm = colvec(K, st, "cx_nrm", Wd["norm_mem"])
        wcq = load_cast(K, st, "cx_wcq", Wd["w_cq"][0], 1024, 1024, scale_col=(nrc, "cx_nrc"))
        wckv = load_cast(K, st, "cx_wckv", Wd["w_ckv"][0], 1024, 2048, scale_col=(nrm, "cx_nrm"))
        wco = load_cast(K, st, "cx_wco", Wd["w_co"][0], 1024, 1024)
        ps = [K.ps(st, "cx_ps%d" % i, [128, 512], F32) for i in range(6)]
        pst = K.ps(st, "cx_pst", [128, 8, 128], BF16)
        ht = K.sb(st, "cx_ht", [128, 4, D], F32)
        xn = K.sb(st, "cx_xn", [128, D], BF16)
        ss = K.sb(st, "cx_ss", [128, 1], F32)
        junk = K.sb(st, "cx_junk", [128, D], F32)
        memT = K.sb(st, "cx_memT", [128, 8, 256], BF16)
        ones = K.sb(st, "cx_ones", [128, 128], BF16)
        K.op("pool", "memset", [], ["cx_ones"], ap=ones[:], constant=1.0)
        for mt in range(2):
            K.dma("sp", ht[:, 0, :], MEM[s * 256 + mt * 128: s * 256 + (mt + 1) * 128, :], [], ["cx_ht0"])
            norm_T(K, "cx_", ht[:, 0, :], "cx_ht0", xn, ss, junk, pst, (memT, "cx_memT"), mt * 128, identb, CONST["eps6"])
        kTs = K.sb(st, "cx_kTs", [128, 8, 256], BF16)
        vS = K.sb(st, "cx_vS", [128, 2, 1024], BF16)
        for j in range(8):
            for k in range(8):
                K.mm(ps[0][:, 0:256], wckv[:, k, j * 128:(j + 1) * 128], memT[:, k, :], ["cx_wckv", "cx_memT"], ["cx_ps0"], start=(k == 0), stop=(k == 7))
            K.op("dve", "tensor_copy", ["cx_ps0"], ["cx_kTs"], out=kTs[:, j, :], in_=ps[0][:, 0:256])
        for mt in range(2):
            for half in range(2):
                for k in range(8):
                    K.mm(ps[1][:], memT[:, k, mt * 128:(mt + 1) * 128], wckv[:, k, 1024 + half * 512:1024 + (half + 1) * 512], ["cx_wckv", "cx_memT"],
                         ["cx_ps1"], start=(k == 0), stop=(k == 7))
                K.op("dve", "tensor_copy", ["cx_ps1"], ["cx_vS"], out=vS[:, mt, half * 512:(half + 1) * 512], in_=ps[1][:])
        hnT = K.sb(st, "cx_hnT", [128, 8, 512], BF16)
        qTs = K.sb(st, "cx_qTs", [128, 8, 512], BF16)
        pT = [K.sb(st, "cx_pT%d" % i, [128, 512], BF16) for i in range(2)]
        rden = K.sb(st, "cx_rden", [128, 512], F32)
        oT = K.sb(st, "cx_oT", [128, 8, 512], BF16)
        for tb in range(T // 512):
            t0 = tb * 512
            for tt in range(4):
                r0 = s * T + t0 + tt * 128
                K.dma("sp" if tt % 2 == 0 else "act", ht[:, tt, :], SC["H1"][r0:r0 + 128, :], ["H1"], ["cx_ht%d" % tt])
                norm_T(K, "cx_", ht[:, tt, :], "cx_ht%d" % tt, xn, ss, junk, pst, (hnT, "cx_hnT"), tt * 128, identb, CONST["eps6"])
            for j in range(8):
                pj = j % 2
                for k in range(8):
                    K.mm(ps[pj][:], wcq[:, k, j * 128:(j + 1) * 128], hnT[:, k, :], ["cx_wcq", "cx_hnT"], ["cx_ps%d" % pj], start=(k == 0), stop=(k == 7))
                if pj == 0:
                    K.op("dve", "tensor_copy", ["cx_ps0"], ["cx_qTs"], out=qTs[:, j, :], in_=ps[0][:])
                else:
                    K.op("act", "activation", ["cx_ps1"], ["cx_qTs"], out=qTs[:, j, :], in_=ps[1][:], func=AF.Copy)
            for h in range(4):
                for mt in range(2):
                    for dc in range(2):
                        K.mm(ps[2 + mt][:], kTs[:, 2 * h + dc, mt * 128:(mt + 1) * 128], qTs[:, 2 * h + dc, :], ["cx_kTs", "cx_qTs"],
                             ["cx_ps%d" % (2 + mt)], start=(dc == 0), stop=(dc == 1))
                    K.op("act", "activation", ["cx_ps%d" % (2 + mt)], ["cx_pT%d" % mt], out=pT[mt][:], in_=ps[2 + mt][:], func=AF.Exp, scale=1.0 / 16)
                for mt in range(2):
                    K.mm(ps[4][:], ones[:], pT[mt][:], ["cx_ones", "cx_pT%d" % mt], ["cx_ps4"], start=(mt == 0), stop=(mt == 1))
                K.op("dve", "reciprocal", ["cx_ps4"], ["cx_rden"], out=rden[:], in_=ps[4][:])
                for dc in range(2):
                    for mt in range(2):
                        K.mm(ps[5][:], vS[:, mt, h * 256 + dc * 128:h * 256 + (dc + 1) * 128], pT[mt][:], ["cx_vS", "cx_pT%d" % mt], ["cx_ps5"],
                             start=(mt == 0), stop=(mt == 1))
                    K.op("dve", "tensor_tensor", ["cx_ps5", "cx_rden"], ["cx_oT"], out=oT[:, 2 * h + dc, :], in0=ps[5][:], in1=rden[:], op=ALU.mult)
            for tt in range(4):
                r0 = s * T + t0 + tt * 128
                for half in range(2):
                    pj = half
                    for k in range(8):
                        K.mm(ps[pj][:], oT[:, k, tt * 128:(tt + 1) * 128], wco[:, k, half * 512:(half + 1) * 512], ["cx_oT", "cx_wco"],
                             ["cx_ps%d" % pj], start=(k == 0), stop=(k == 7))
                    K.op("dve", "tensor_tensor", ["cx_ps%d" % pj, "cx_ht%d" % tt], ["cx_ht%d" % tt], out=ht[:, tt, half * 512:(half + 1) * 512],
                         in0=ps[pj][:], in1=ht[:, tt, half * 512:(half + 1) * 512], op=ALU.add)
                K.dma("sp", SC["H1"][r0:r0 + 128, :], ht[:, tt, :], ["cx_ht%d" % tt], ["H1"])


def phase_moe(K, s, T, Wd, SC, CONST, OUT):
    identb = CONST["identb"]
    HT = min(T, 1024)
    NTL = HT // 128
    with ExitStack() as st:
        nrf = colvec(K, st, "mo_nrf", Wd["norm_ffn"])
        wrf = K.sb(st, "mo_wrf", [128, 8, 36], F32)
        K.dma("sp", wrf[:, :, 0:4], Wd["w_router_g"][0].rearrange("(c p) n -> p c n", p=128), [], ["mo_wrf"])
        K.dma("sp", wrf[:, :, 4:36], Wd["w_router_e"][0].rearrange("(c p) n -> p c n", p=128), [], ["mo_wrf"])
        wr = K.sb(st, "mo_wr", [128, 8, 36], BF16)
        K.op("dve", "tensor_tensor", ["mo_wrf", "mo_nrf"], ["mo_wr"], out=wr[:], in0=wrf[:], in1=nrf[:].unsqueeze(2).to_broadcast([128, 8, 36]), op=ALU.mult)
        brb = K.sb(st, "mo_brb", [128, 36], F32)
        K.dma("sp", brb[:, 0:4], Wd["b_router_g"].partition_broadcast(128), [], ["mo_brb"])
        K.dma("sp", brb[:, 4:36], Wd["b_router_e"].partition_broadcast(128), [], ["mo_brb"])
        nfb = K.sb(st, "mo_nfb", [128, D], F32)
        K.dma("sp", nfb[:], Wd["norm_final"].partition_broadcast(128), [], ["mo_nfb"])
        ps = [K.ps(st, "mo_ps%d" % i, [128, 512], F32) for i in range(7)]
        pst = K.ps(st, "mo_pst", [128, 8, 128], BF16)
        ht = K.sb(st, "mo_ht", [128, D], F32)
        xn = K.sb(st, "mo_xn", [128, D], BF16)
        ss = K.sb(st, "mo_ss", [128, 1], F32)
        junk = K.sb(st, "mo_junk", [128, D], F32)
        xT = K.sb(st, "mo_xT", [128, 8, HT], BF16)
        G = K.sb(st, "mo_G", [128, NTL, 32], F32)
        acc = K.sb(st, "mo_acc", [128, NTL, D], F32)
        lg = K.sb(st, "mo_lg", [128, 36], F32)
        cl = K.sb(st, "mo_cl", [128, 12], F32)
        lem = K.sb(st, "mo_lem", [128, 4, 8], F32)
        m8 = K.sb(st, "mo_m8", [128, 8], F32)
        sel = K.sb(st, "mo_sel", [128, 32], F32)
        ex = K.sb(st, "mo_ex", [128, 32], F32)
        stg = [K.sb(st, "mo_stg%d" % i, [128, 4096], F32) for i in range(2)]
        wg = [K.sb(st, "mo_wg%d" % i, [128, 8, 512], BF16) for i in range(2)]
        wu = [K.sb(st, "mo_wu%d" % i, [128, 8, 512], BF16) for i in range(2)]
        wd = [K.sb(st, "mo_wd%d" % i, [128, 4, 1024], BF16) for i in range(2)]
        sgts = [K.sb(st, "mo_sgt%d" % i, [128, 512], F32) for i in range(2)]
        hT = K.sb(st, "mo_hT", [128, 4, 512], BF16)
        tmp = [K.sb(st, "mo_tmp%d" % i, [128, 512], F32) for i in range(3)]
        for hf in range(T // HT):
            base = s * T + hf * HT
            for tl in range(NTL):
                r0 = base + tl * 128
                K.dma("sp", ht[:], SC["H1"][r0:r0 + 128, :], ["H1"], ["mo_ht"])
                norm_T(K, "mo_", ht[:], "mo_ht", xn, ss, junk, pst, (xT, "mo_xT"), tl * 128, identb, CONST["eps6"])
                for k in range(8):
                    K.mm(ps[0][:, 0:36], xT[:, k, tl * 128:(tl + 1) * 128], wr[:, k, :], ["mo_xT", "mo_wr"], ["mo_ps0"], start=(k == 0), stop=(k == 7))
                K.op("dve", "tensor_tensor", ["mo_ps0", "mo_brb"], ["mo_lg"], out=lg[:], in0=ps[0][:, 0:36], in1=brb[:], op=ALU.add)
                K.op("dve", "tensor_reduce", ["mo_lg"], ["mo_cl"], out=cl[:, 0:1], in_=lg[:, 0:4], axis=AX.X, op=ALU.max)
                K.op("dve", "tensor_scalar", ["mo_cl"], ["mo_cl"], out=cl[:, 1:2], in0=cl[:, 0:1], scalar1=-1.0, scalar2=None, op0=ALU.mult)
                K.op("act", "activation", ["mo_lg", "mo_cl"], ["mo_ex", "mo_cl"], out=ex[:, 0:4], in_=lg[:, 0:4], func=AF.Exp, bias=cl[:, 1:2], accum_out=cl[:, 2:3])
                K.op("dve", "reciprocal", ["mo_cl"], ["mo_cl"], out=cl[:, 3:4], in_=cl[:, 2:3])
                K.op("dve", "tensor_scalar", ["mo_lg", "mo_cl"], ["mo_sel"], out=sel[:, 0:4], in0=lg[:, 0:4], scalar1=cl[:, 0:1], scalar2=None, op0=ALU.is_ge)
                K.op("dve", "tensor_scalar", ["mo_sel"], ["mo_sel"], out=sel[:, 0:4], in0=sel[:, 0:4], scalar1=-1.0, scalar2=1e30, op0=ALU.add, op1=ALU.mult)
                K.op("dve", "tensor_tensor", ["mo_lg", "mo_sel"], ["mo_lem"], out=lem[:], in0=lg[:, 4:36].rearrange("p (a b) -> p a b", a=4),
                     in1=sel[:, 0:4].unsqueeze(2).to_broadcast([128, 4, 8]), op=ALU.add)
                lemf = lem[:].rearrange("p a b -> p (a b)")
                K.op("dve", "max", ["mo_lem"], ["mo_m8"], out=m8[:], in_=lemf)
                K.op("dve", "tensor_scalar", ["mo_lem", "mo_m8"], ["mo_sel"], out=sel[:], in0=lemf, scalar1=m8[:, 1:2], scalar2=None, op0=ALU.is_ge)
                K.op("dve", "tensor_scalar", ["mo_m8"], ["mo_cl"], out=cl[:, 4:5], in0=m8[:, 0:1], scalar1=-1.0, scalar2=None, op0=ALU.mult)
                K.op("act", "activation", ["mo_lem", "mo_cl"], ["mo_ex"], out=ex[:], in_=lemf, func=AF.Exp, bias=cl[:, 4:5])
                K.op("dve", "tensor_tensor", ["mo_ex", "mo_sel"], ["mo_ex"], out=ex[:], in0=ex[:], in1=sel[:], op=ALU.mult)
                K.op("dve", "tensor_reduce", ["mo_ex"], ["mo_cl"], out=cl[:, 5:6], in_=ex[:], axis=AX.X, op=ALU.add)
                K.op("dve", "reciprocal", ["mo_cl"], ["mo_cl"], out=cl[:, 6:7], in_=cl[:, 5:6])
                K.op("dve", "tensor_tensor", ["mo_cl"], ["mo_cl"], out=cl[:, 7:8], in0=cl[:, 6:7], in1=cl[:, 3:4], op=ALU.mult)
                K.op("dve", "tensor_scalar", ["mo_ex", "mo_cl"], ["mo_G"], out=G[:, tl, :], in0=ex[:], scalar1=cl[:, 7:8], scalar2=None, op0=ALU.mult)
            for e in range(32):
                i = e % 2
                nfb8 = nrf[:].unsqueeze(2).to_broadcast([128, 8, 512])
                K.dma("sp", stg[0][:].rearrange("p (c n) -> p c n", c=8), Wd["w_e_gate"][0, e].rearrange("(c p) n -> p c n", p=128), [], ["mo_stg0"])
                K.op("pool", "tensor_tensor", ["mo_stg0", "mo_nrf"], ["mo_wg%d" % i], out=wg[i][:], in0=stg[0][:].rearrange("p (c n) -> p c n", c=8), in1=nfb8, op=ALU.mult)
                K.dma("act", stg[1][:].rearrange("p (c n) -> p c n", c=8), Wd["w_e_up"][0, e].rearrange("(c p) n -> p c n", p=128), [], ["mo_stg1"])
                K.op("pool", "tensor_tensor", ["mo_stg1", "mo_nrf"], ["mo_wu%d" % i], out=wu[i][:], in0=stg[1][:].rearrange("p (c n) -> p c n", c=8), in1=nfb8, op=ALU.mult)
                K.dma("sp", stg[0][:].rearrange("p (c n) -> p c n", c=4), Wd["w_e_down"][0, e].rearrange("(c p) n -> p c n", p=128), [], ["mo_stg0"])
                K.op("pool", "tensor_copy", ["mo_stg0"], ["mo_wd%d" % i], out=wd[i][:], in_=stg[0][:].rearrange("p (c n) -> p c n", c=4))
                for bk in range(HT // 512):
                    bs = slice(bk * 512, (bk + 1) * 512)
                    for fc in range(4):
                        fs = slice(fc * 128, (fc + 1) * 128)
                        pg, pu = (0, 1) if fc % 2 == 0 else (4, 5)
                        sg_ = sgts[fc % 2]
                        sgn = "mo_sgt%d" % (fc % 2)
                        for k in range(8):
                            K.mm(ps[pg][:], wg[i][:, k, fs], xT[:, k, bs], ["mo_wg%d" % i, "mo_xT"], ["mo_ps%d" % pg], start=(k == 0), stop=(k == 7))
                        for k in range(8):
                            K.mm(ps[pu][:], wu[i][:, k, fs], xT[:, k, bs], ["mo_wu%d" % i, "mo_xT"], ["mo_ps%d" % pu], start=(k == 0), stop=(k == 7))
                        K.op("act", "activation", ["mo_ps%d" % pg], [sgn], out=sg_[:], in_=ps[pg][:], func=AF.Silu)
                        K.op("dve", "tensor_tensor", ["mo_ps%d" % pu, sgn], ["mo_hT%d" % fc], out=hT[:, fc, :], in0=ps[pu][:], in1=sg_[:], op=ALU.mult)
                    for tt in range(4):
                        tl = bk * 4 + tt
                        for half in range(2):
                            pj = (2, 3, 6)[(2 * tt + half) % 3]
                            for fc in range(4):
                                K.mm(ps[pj][:], hT[:, fc, tt * 128:(tt + 1) * 128], wd[i][:, fc, half * 512:(half + 1) * 512], ["mo_hT%d" % fc, "mo_wd%d" % i],
                                     ["mo_ps%d" % pj], start=(fc == 0), stop=(fc == 3))
                            hs = slice(half * 512, (half + 1) * 512)
                            if e == 0:
                                K.op("act", "activation", ["mo_ps%d" % pj, "mo_G"], ["mo_acc%d_%d" % (tl, half)], out=acc[:, tl, hs], in_=ps[pj][:], func=AF.Copy, scale=G[:, tl, e:e + 1])
                            else:
                                ti = (2 * tt + half) % 3
                                accn = "mo_acc%d_%d" % (tl, half)
                                K.op("act", "activation", ["mo_ps%d" % pj, "mo_G"], ["mo_tmp%d" % ti], out=tmp[ti][:], in_=ps[pj][:], func=AF.Copy, scale=G[:, tl, e:e + 1])
                                K.op("pool" if half == 0 else "dve", "tensor_tensor", ["mo_tmp%d" % ti, accn], [accn], out=acc[:, tl, hs], in0=acc[:, tl, hs], in1=tmp[ti][:], op=ALU.add)
            for tl in range(NTL):
                r0 = base + tl * 128
                K.dma("sp", ht[:], SC["H1"][r0:r0 + 128, :], ["H1"], ["mo_ht"])
                K.op("dve", "tensor_tensor", ["mo_ht", "mo_acc%d_0" % tl, "mo_acc%d_1" % tl], ["mo_ht"], out=ht[:], in0=ht[:], in1=acc[:, tl, :], op=ALU.add)
                K.op("act", "activation", ["mo_ht"], ["mo_junk", "mo_ss"], out=junk[:], in_=ht[:], func=AF.Square, accum_out=ss[:])
                K.op("act", "activation", ["mo_ss", "eps6"], ["mo_ss"], out=ss[:], in_=ss[:], func=AF.Sqrt, scale=1.0 / D, bias=CONST["eps6"][:])
                K.op("dve", "reciprocal", ["mo_ss"], ["mo_ss"], out=ss[:], in_=ss[:])
                K.op("dve", "scalar_tensor_tensor", ["mo_ht", "mo_ss", "mo_nfb"], ["mo_junk"], out=junk[:], in0=ht[:], scalar=ss[:], in1=nfb[:], op0=ALU.mult, op1=ALU.mult)
                K.dma("sp", OUT[r0:r0 + 128, :], junk[:], ["mo_junk"], ["OUT"])


I32 = mybir.dt.int32


def prepack_gen(K, st, Wd, SC):
    WGU, WDS = SC["WGU"], SC["WDS"]
    nrf = colvec(K, st, "pk_nrf", Wd["norm_ffn"])
    sg = K.sb(st, "pk_sg", [128, 8, 512], F32)
    su = K.sb(st, "pk_su", [128, 8, 512], F32)
    sd = K.sb(st, "pk_sd", [128, 4, 1024], F32)
    og = K.sb(st, "pk_og", [128, 8, 1024], BF16)
    od = K.sb(st, "pk_od", [128, 4, 1024], BF16)
    nf8 = nrf[:].unsqueeze(2).to_broadcast([128, 8, 512])
    def loads(e):
        K.dma("pool", sg[:], Wd["w_e_gate"][0, e].rearrange("(c p) n -> p c n", p=128), [], ["pk_sg"])
        K.dma("pool", su[:], Wd["w_e_up"][0, e].rearrange("(c p) n -> p c n", p=128), [], ["pk_su"])
        K.dma("pool", sd[:], Wd["w_e_down"][0, e].rearrange("(c p) n -> p c n", p=128), [], ["pk_sd"])

    loads(0)
    yield
    for e in range(32):
        K.op("dve", "tensor_tensor", ["pk_sg", "pk_nrf"], ["pk_og0"], out=og[:, :, 0:512], in0=sg[:], in1=nf8, op=ALU.mult)
        for c in range(8):
            K.op("act", "activation", ["pk_su", "pk_nrf"], ["pk_og1"], out=og[:, c, 512:1024], in_=su[:, c, :], func=AF.Copy, scale=nrf[:, c:c + 1])
        K.op("act", "activation", ["pk_sd"], ["pk_od"], out=od[:], in_=sd[:], func=AF.Copy)
        K.dma("pool", WGU[e * 1024:(e + 1) * 1024, :].rearrange("(c p) n -> p c n", p=128), og[:], ["pk_og0", "pk_og1"], ["WGU"])
        K.dma("pool", WDS[e * 512:(e + 1) * 512, :].rearrange("(c p) n -> p c n", p=128), od[:], ["pk_od"], ["WDS"])
        if e + 1 < 32:
            loads(e + 1)
        yield


def phase_moe_sparse(K, s, T, Wd, SC, CONST, OUT):
    nc = K.nc
    S = K.S
    identb = CONST["identb"]
    NTL = T // 128
    SB = 256
    NBLK = (2 * T) // SB + 32
    XS, YS = SC["XS"], SC["YS"]
    WG = Wd["w_e_gate"].rearrange("o e d f -> (o e d) f")
    WU = Wd["w_e_up"].rearrange("o e d f -> (o e d) f")
    WDN = Wd["w_e_down"].rearrange("o e f d -> (o e f) d")
    base = s * T
    with ExitStack() as st0:
        nrf = colvec(K, st0, "ms_nrf", Wd["norm_ffn"])
        GG = K.sb(st0, "ms_GG", [128, NTL, 2], F32)
        DST = K.sb(st0, "ms_DST", [128, NTL, 2], I32)
        IDXG = K.sb(st0, "ms_IDXG", [128, NBLK, 8], I32)
        IDXD = K.sb(st0, "ms_IDXD", [128, NBLK, 4], I32)
        with ExitStack() as st:
            wrf = K.sb(st, "ms_wrf", [128, 8, 36], F32)
            K.dma("sp", wrf[:, :, 0:4], Wd["w_router_g"][0].rearrange("(c p) n -> p c n", p=128), [], ["ms_wrf"])
            K.dma("sp", wrf[:, :, 4:36], Wd["w_router_e"][0].rearrange("(c p) n -> p c n", p=128), [], ["ms_wrf"])
            wr = K.sb(st, "ms_wr", [128, 8, 36], BF16)
            K.op("dve", "tensor_tensor", ["ms_wrf", "ms_nrf"], ["ms_wr"], out=wr[:], in0=wrf[:], in1=nrf[:].unsqueeze(2).to_broadcast([128, 8, 36]), op=ALU.mult)
            brb = K.sb(st, "ms_brb", [128, 36], F32)
            K.dma("sp", brb[:, 0:4], Wd["b_router_g"].partition_broadcast(128), [], ["ms_brb"])
            K.dma("sp", brb[:, 4:36], Wd["b_router_e"].partition_broadcast(128), [], ["ms_brb"])
            ps = [K.ps(st, "ms_ps%d" % i, [128, 512], F32) for i in range(2)]
            pst = K.ps(st, "ms_pst", [128, 8, 128], BF16)
            ht = K.sb(st, "ms_ht", [128, D], F32)
            ss = K.sb(st, "ms_ss", [128, 1], F32)
            junk = K.sb(st, "ms_junk", [128, D], F32)
            XN = K.sb(st, "ms_XN", [128, NTL, D], BF16)
            xT = K.sb(st, "ms_xT", [128, 8, 128], BF16)
            SEL = K.sb(st, "ms_SEL", [128, NTL, 2, 32], F32)
            RNK = K.sb(st, "ms_RNK", [128, NTL, 2], F32)
            carry = K.sb(st, "ms_carry", [128, 32], F32)
            K.op("dve", "memset", [], ["ms_carry"], ap=carry[:], constant=0.0)
            lg = K.sb(st, "ms_lg", [128, 36], F32)
            cl = K.sb(st, "ms_cl", [128, 12], F32)
            lem = K.sb(st, "ms_lem", [128, 32], F32)
            m8 = K.sb(st, "ms_m8", [128, 8], F32)
            s12 = K.sb(st, "ms_s12", [128, 32], F32)
            ex = K.sb(st, "ms_ex", [128, 32], F32)
            t32 = K.sb(st, "ms_t32", [128, 32], F32)
            utri, ones128, bstart, iotap = CONST["utri"], CONST["ones128"], CONST["bstart"], CONST["iotap"]
            GB = 8
            LG = K.sb(st, "ms_LG", [128, GB, 36], F32)
            LM = K.sb(st, "ms_LM", [128, GB, 32], F32)
            L2 = K.sb(st, "ms_L2", [128, GB, 32], F32)
            EX = K.sb(st, "ms_EX", [128, GB, 32], F32)
            S12 = K.sb(st, "ms_S12", [128, GB, 32], F32)
            RKt = K.sb(st, "ms_RKt", [128, GB, 32], F32)
            T4 = K.sb(st, "ms_T4", [128, GB, 4], F32)
            E4 = K.sb(st, "ms_E4", [128, GB, 4], F32)
            CG = K.sb(st, "ms_CG", [128, 8, GB], F32)
            hts = [ht, K.sb(st, "ms_ht1", [128, D], F32)]

            def b3(colv, n):
                return colv.unsqueeze(2).to_broadcast([128, GB, n])

            for g0 in range(0, NTL, GB):
                for gi in range(GB):
                    tl = g0 + gi
                    r0 = base + tl * 128
                    hh_ = hts[tl % 2]
                    hn = "ms_ht" if tl % 2 == 0 else "ms_ht1"
                    K.dma("sp" if tl % 2 == 0 else "act", hh_[:], SC["H1"][r0:r0 + 128, :], ["H1"], [hn])
                    K.op("act", "activation", [hn], ["ms_junk", "ms_ss"], out=junk[:], in_=hh_[:], func=AF.Square, accum_out=ss[:])
                    K.op("act", "activation", ["ms_ss", "eps6"], ["ms_ss"], out=ss[:], in_=ss[:], func=AF.Sqrt, scale=1.0 / D, bias=CONST["eps6"][:])
                    K.op("dve", "reciprocal", ["ms_ss"], ["ms_ss"], out=ss[:], in_=ss[:])
                    K.op("dve", "tensor_scalar", [hn, "ms_ss"], ["ms_XN%d" % tl], out=XN[:, tl, :], in0=hh_[:], scalar1=ss[:], scalar2=None, op0=ALU.mult)
                    for c in range(8):
                        K.tr(pst[:, c, :], XN[:, tl, c * 128:(c + 1) * 128], identb[:], ["ms_XN%d" % tl, "identb"], ["ms_pst"])
                    K.op("act", "activation", ["ms_pst"], ["ms_xT"], out=xT[:], in_=pst[:], func=AF.Copy)
                    for k in range(8):
                        K.mm(ps[0][:, 0:36], xT[:, k, :], wr[:, k, :], ["ms_xT", "ms_wr"], ["ms_ps0"], start=(k == 0), stop=(k == 7))
                    K.op("dve", "tensor_tensor", ["ms_ps0", "ms_brb"], ["ms_LG"], out=LG[:, gi, :], in0=ps[0][:, 0:36], in1=brb[:], op=ALU.add)
                K.op("dve", "tensor_reduce", ["ms_LG"], ["ms_CG"], out=CG[:, 0, :], in_=LG[:, :, 0:4], axis=AX.X, op=ALU.max)
                K.op("dve", "tensor_tensor", ["ms_LG", "ms_CG"], ["ms_T4"], out=T4[:], in0=LG[:, :, 0:4], in1=b3(CG[:, 0, :], 4), op=ALU.subtract)
                K.op("act", "activation", ["ms_T4"], ["ms_E4"], out=E4[:], in_=T4[:], func=AF.Exp)
                K.op("dve", "tensor_reduce", ["ms_E4"], ["ms_CG"], out=CG[:, 1, :], in_=E4[:], axis=AX.X, op=ALU.add)
                K.op("dve", "reciprocal", ["ms_CG"], ["ms_CG"], out=CG[:, 2, :], in_=CG[:, 1, :])
                K.op("dve", "tensor_scalar", ["ms_T4"], ["ms_T4"], out=T4[:], in0=T4[:], scalar1=0.0, scalar2=None, op0=ALU.is_ge)
                K.op("dve", "tensor_scalar", ["ms_T4"], ["ms_T4"], out=T4[:], in0=T4[:], scalar1=-1.0, scalar2=1e30, op0=ALU.add, op1=ALU.mult)
                K.op("dve", "tensor_tensor", ["ms_LG", "ms_T4"], ["ms_LM"], out=LM[:].rearrange("p g (a b) -> p g a b", a=4),
                     in0=LG[:, :, 4:36].rearrange("p g (a b) -> p g a b", a=4), in1=T4[:].unsqueeze(3).to_broadcast([128, GB, 4, 8]), op=ALU.add)
                K.op("dve", "tensor_reduce", ["ms_LM"], ["ms_CG"], out=CG[:, 3, :], in_=LM[:], axis=AX.X, op=ALU.max)
                sel1 = SEL[:, g0:g0 + GB, 0, :]
                sel2 = SEL[:, g0:g0 + GB, 1, :]
                K.op("dve", "tensor_tensor", ["ms_LM", "ms_CG"], ["ms_SEL"], out=sel1, in0=LM[:], in1=b3(CG[:, 3, :], 32), op=ALU.is_ge)
                K.op("dve", "scalar_tensor_tensor", ["ms_SEL", "ms_LM"], ["ms_L2"], out=L2[:], in0=sel1, scalar=-1e30, in1=LM[:], op0=ALU.mult, op1=ALU.add)
                K.op("dve", "tensor_reduce", ["ms_L2"], ["ms_CG"], out=CG[:, 4, :], in_=L2[:], axis=AX.X, op=ALU.max)
                K.op("dve", "tensor_tensor", ["ms_LM", "ms_CG"], ["ms_S12"], out=S12[:], in0=LM[:], in1=b3(CG[:, 4, :], 32), op=ALU.is_ge)
                K.op("dve", "tensor_tensor", ["ms_S12", "ms_SEL"], ["ms_SEL"], out=sel2, in0=S12[:], in1=sel1, op=ALU.subtract)
                K.op("dve", "tensor_tensor", ["ms_LM", "ms_CG"], ["ms_L2"], out=L2[:], in0=LM[:], in1=b3(CG[:, 3, :], 32), op=ALU.subtract)
                K.op("dve", "tensor_scalar", ["ms_L2"], ["ms_L2"], out=L2[:], in0=L2[:], scalar1=-80.0, scalar2=None, op0=ALU.max)
                K.op("act", "activation", ["ms_L2"], ["ms_EX"], out=EX[:], in_=L2[:], func=AF.Exp)
                K.op("dve", "tensor_tensor", ["ms_EX", "ms_S12"], ["ms_EX"], out=EX[:], in0=EX[:], in1=S12[:], op=ALU.mult)
                K.op("dve", "tensor_reduce", ["ms_EX"], ["ms_CG"], out=CG[:, 5, :], in_=EX[:], axis=AX.X, op=ALU.add)
                K.op("dve", "reciprocal", ["ms_CG"], ["ms_CG"], out=CG[:, 6, :], in_=CG[:, 5, :])
                K.op("dve", "tensor_tensor", ["ms_CG"], ["ms_CG"], out=CG[:, 6, :], in0=CG[:, 6, :], in1=CG[:, 2, :], op=ALU.mult)
                for kk_ in range(2):
                    K.op("dve", "tensor_tensor", ["ms_EX", "ms_SEL"], ["ms_L2"], out=L2[:], in0=EX[:], in1=SEL[:, g0:g0 + GB, kk_, :], op=ALU.mult)
                    K.op("dve", "tensor_reduce", ["ms_L2"], ["ms_CG"], out=CG[:, 7, :], in_=L2[:], axis=AX.X, op=ALU.add)
                    K.op("dve", "tensor_tensor", ["ms_CG"], ["ms_GG"], out=GG[:, g0:g0 + GB, kk_], in0=CG[:, 7, :], in1=CG[:, 6, :], op=ALU.mult)
                for gi in range(GB):
                    K.mm(ps[1][:, gi * 64:gi * 64 + 32], utri[:], S12[:, gi, :], ["utri", "ms_S12"], ["ms_ps1"])
                    K.mm(ps[1][:, gi * 64 + 32:gi * 64 + 64], ones128[:], S12[:, gi, :], ["ones128", "ms_S12"], ["ms_ps1"])
                for gi in range(GB):
                    K.op("dve", "tensor_tensor", ["ms_ps1", "ms_carry"], ["ms_RKt"], out=RKt[:, gi, :], in0=ps[1][:, gi * 64:gi * 64 + 32], in1=carry[:], op=ALU.add)
                    K.op("dve", "tensor_tensor", ["ms_ps1", "ms_carry"], ["ms_carry"], out=carry[:], in0=ps[1][:, gi * 64 + 32:gi * 64 + 64], in1=carry[:], op=ALU.add)
                for kk_ in range(2):
                    K.op("dve", "tensor_tensor", ["ms_RKt", "ms_SEL"], ["ms_L2"], out=L2[:], in0=RKt[:], in1=SEL[:, g0:g0 + GB, kk_, :], op=ALU.mult)
                    K.op("dve", "tensor_reduce", ["ms_L2"], ["ms_RNK"], out=RNK[:, g0:g0 + GB, kk_], in_=L2[:], axis=AX.X, op=ALU.add)
            ci = K.sb(st, "ms_ci", [128, 32], I32)
            pad = K.sb(st, "ms_pad", [128, 32], F32)
            pend = K.sb(st, "ms_pend", [128, 32], F32)
            pstart = K.sb(st, "ms_pstart", [128, 32], F32)
            ones32 = K.sb(st, "ms_ones32", [128, 32], F32)
            K.op("dve", "memset", [], ["ms_ones32"], ap=ones32[:], constant=1.0)
            K.op("dve", "tensor_scalar", ["ms_carry"], ["ms_ci"], out=ci[:], in0=carry[:], scalar1=float(SB - 1), scalar2=None, op0=ALU.add)
            K.op("dve", "tensor_scalar", ["ms_ci"], ["ms_ci"], out=ci[:], in0=ci[:], scalar1=8, scalar2=None, op0=ALU.arith_shift_right)
            K.op("dve", "tensor_scalar", ["ms_ci"], ["ms_ci"], out=ci[:], in0=ci[:], scalar1=8, scalar2=None, op0=ALU.logical_shift_left)
            K.op("dve", "tensor_copy", ["ms_ci"], ["ms_pad"], out=pad[:], in_=ci[:])
            K.op("dve", "tensor_tensor_scan", ["ms_pad", "ms_ones32"], ["ms_pend"], out=pend[:], data0=ones32[:], data1=pad[:], initial=0.0, op0=ALU.mult, op1=ALU.add)
            K.op("dve", "tensor_tensor", ["ms_pend", "ms_pad"], ["ms_pstart"], out=pstart[:], in0=pend[:], in1=pad[:], op=ALU.subtract)
            bst = K.sb(st, "ms_bst", [128, NBLK], F32)
            K.op("dve", "tensor_scalar", ["bstart"], ["ms_bst"], out=bst[:], in0=bstart[:, 0:NBLK], scalar1=float(SB // 128), scalar2=None, op0=ALU.mult)
            be = K.sb(st, "ms_be", [128, NBLK], F32)
            K.op("dve", "tensor_scalar", ["ms_bst", "ms_pend"], ["ms_be"], out=be[:], in0=bst[:], scalar1=pend[:, 0:1], scalar2=None, op0=ALU.is_ge)
            for e in range(1, 32):
                K.op("dve", "scalar_tensor_tensor", ["ms_bst", "ms_pend", "ms_be"], ["ms_be"], out=be[:], in0=bst[:], scalar=pend[:, e:e + 1], in1=be[:],
                     op0=ALU.is_ge, op1=ALU.add)
            K.op("dve", "tensor_scalar", ["ms_be"], ["ms_be"], out=be[:], in0=be[:], scalar1=31.0, scalar2=None, op0=ALU.min)
            bg = K.sb(st, "ms_bg", [128, NBLK], F32)
            bd = K.sb(st, "ms_bd", [128, NBLK], F32)
            K.op("dve", "tensor_scalar", ["ms_be", "iotap"], ["ms_bg"], out=bg[:], in0=be[:], scalar1=1024.0, scalar2=iotap[:, 0:1], op0=ALU.mult, op1=ALU.add)
            K.op("dve", "tensor_scalar", ["ms_be", "iotap"], ["ms_bd"], out=bd[:], in0=be[:], scalar1=512.0, scalar2=iotap[:, 0:1], op0=ALU.mult, op1=ALU.add)
            for c in range(8):
                K.op("dve", "tensor_scalar", ["ms_bg"], ["ms_IDXG"], out=IDXG[:, :, c], in0=bg[:], scalar1=float(c * 128), scalar2=None, op0=ALU.add)
            for c in range(4):
                K.op("dve", "tensor_scalar", ["ms_bd"], ["ms_IDXD"], out=IDXD[:, :, c], in0=bd[:], scalar1=float(c * 128), scalar2=None, op0=ALU.add)
            zt = K.sb(st, "ms_zt", [128, 4, D], BF16)
            K.op("pool", "memset", [], ["ms_zt"], ap=zt[:], constant=0.0)
            XSv = XS.rearrange("(b p) d -> p b d", p=128)
            for b0 in range(0, NBLK * SB // 128, 4):
                K.dma("sp" if (b0 // 4) % 2 == 0 else "act", XSv[:, b0:b0 + 4, :], zt[:], ["ms_zt"], ["XS"])
            TB3 = K.sb(st, "ms_TB3", [128, NTL, 32], F32)
            DF = K.sb(st, "ms_DF", [128, NTL], F32)
            for kk_ in range(2):
                K.op("dve", "tensor_tensor", ["ms_pstart", "ms_SEL"], ["ms_TB3"], out=TB3[:], in0=SEL[:, :, kk_, :],
                     in1=pstart[:].unsqueeze(1).to_broadcast([128, NTL, 32]), op=ALU.mult)
                K.op("dve", "tensor_reduce", ["ms_TB3"], ["ms_DF"], out=DF[:], in_=TB3[:], axis=AX.X, op=ALU.add)
                K.op("dve", "tensor_tensor", ["ms_DF", "ms_RNK"], ["ms_DST"], out=DST[:, :, kk_], in0=DF[:], in1=RNK[:, :, kk_], op=ALU.add)
            for tl in range(NTL):
                for kk_ in range(2):
                    S.dma("pool", None, None, K._bl(["ms_DST", "ms_XN%d" % tl, "XS"]), K._bl(["XSs_%d_%d" % (tl, kk_)]),
                          fn=lambda e, tl=tl, kk_=kk_: e.indirect_dma_start(out=XS, out_offset=bass.IndirectOffsetOnAxis(ap=DST[:, tl, kk_:kk_ + 1], axis=0),
                                                                        in_=XN[:, tl, :], in_offset=None))
        S.barrier()
        with ExitStack() as st:
            ps = [K.ps(st, "mb_ps%d" % i, [128, 512], F32) for i in range(6)]
            pst = K.ps(st, "mb_pst", [128, 8, 128], BF16)
            wgu = [K.sb(st, "mb_wgu%d" % i, [128, 8, 1024], BF16) for i in range(2)]
            wd = [K.sb(st, "mb_wd%d" % i, [128, 4, 1024], BF16) for i in range(2)]
            xb = [K.sb(st, "mb_xb%d" % i, [128, D], BF16) for i in range(2)]
            xT = K.sb(st, "mb_xT", [128, 8, 128], BF16)
            sgt = K.sb(st, "mb_sgt", [128, 512], F32)
            hb = K.sb(st, "mb_hb", [128, 512], BF16)
            hT = K.sb(st, "mb_hT", [128, 4, 128], BF16)
            ysb = [K.sb(st, "mb_ysb%d" % i, [128, D], F32) for i in range(2)]
            WGU, WDS = SC["WGU"], SC["WDS"]
            for b in range(NBLK):
                i = b % 2
                for c in range(8):
                    S.dma("pool", None, None, K._bl(["ms_IDXG"]), K._bl(["mb_wgu%d_%d" % (i, c)]),
                          fn=lambda e, b=b, c=c, i=i: e.indirect_dma_start(out=wgu[i][:, c, :], out_offset=None, in_=WGU,
                                                                         in_offset=bass.IndirectOffsetOnAxis(ap=IDXG[:, b, c:c + 1], axis=0)))
                for c in range(4):
                    S.dma("pool", None, None, K._bl(["ms_IDXD"]), K._bl(["mb_wd%d_%d" % (i, c)]),
                          fn=lambda e, b=b, c=c, i=i: e.indirect_dma_start(out=wd[i][:, c, :], out_offset=None, in_=WDS,
                                                                         in_offset=bass.IndirectOffsetOnAxis(ap=IDXD[:, b, c:c + 1], axis=0)))
                for sub in range(SB // 128):
                    j = sub % 2
                    r0 = b * SB + sub * 128
                    K.dma("sp", xb[j][:], XS[r0:r0 + 128, :], ["XS"], ["mb_xb%d" % j])
                    for c in range(8):
                        K.tr(pst[:, c, :], xb[j][:, c * 128:(c + 1) * 128], identb[:], ["mb_xb%d" % j, "identb"], ["mb_pst"])
                    K.op("act", "activation", ["mb_pst"], ["mb_xT"], out=xT[:], in_=pst[:], func=AF.Copy)
                    for k in range(8):
                        K.mm(ps[0][:], xT[:, k, :], wgu[i][:, k, 0:512], ["mb_xT"] + ["mb_wgu%d_%d" % (i, c) for c in range(8)], ["mb_ps0"], start=(k == 0), stop=(k == 7))
                    for k in range(8):
                        K.mm(ps[1][:], xT[:, k, :], wgu[i][:, k, 512:1024], ["mb_xT"] + ["mb_wgu%d_%d" % (i, c) for c in range(8)], ["mb_ps1"], start=(k == 0), stop=(k == 7))
                    K.op("act", "activation", ["mb_ps0"], ["mb_sgt"], out=sgt[:], in_=ps[0][:], func=AF.Silu)
                    K.op("dve", "tensor_tensor", ["mb_ps1", "mb_sgt"], ["mb_hb"], out=hb[:], in0=ps[1][:], in1=sgt[:], op=ALU.mult)
                    for fc in range(4):
                        K.tr(pst[:, fc, :], hb[:, fc * 128:(fc + 1) * 128], identb[:], ["mb_hb", "identb"], ["mb_pst"])
                    K.op("dve", "tensor_copy", ["mb_pst"], ["mb_hT"], out=hT[:], in_=pst[:, 0:4, :])
                    for half in range(2):
                        pj = 2 + 2 * j + half
                        for fc in range(4):
                            K.mm(ps[pj][:], hT[:, fc, :], wd[i][:, fc, half * 512:(half + 1) * 512], ["mb_hT"] + ["mb_wd%d_%d" % (i, c) for c in range(4)], ["mb_ps%d" % pj], start=(fc == 0), stop=(fc == 3))
                        if half == 0:
                            K.op("act", "activation", ["mb_ps%d" % pj], ["mb_ysb%d" % j], out=ysb[j][:, 0:512], in_=ps[pj][:], func=AF.Copy)
                        else:
                            K.op("dve", "tensor_copy", ["mb_ps%d" % pj], ["mb_ysb%d" % j], out=ysb[j][:, 512:1024], in_=ps[pj][:])
                    K.dma("act", YS[r0:r0 + 128, :], ysb[j][:], ["mb_ysb%d" % j], ["YS"])
        S.barrier()
        with ExitStack() as st:
            nfb = K.sb(st, "mc_nfb", [128, D], F32)
            K.dma("sp", nfb[:], Wd["norm_final"].partition_broadcast(128), [], ["mc_nfb"])
            hts = [K.sb(st, "mc_ht%d" % i, [128, D], F32) for i in range(2)]
            y1 = [K.sb(st, "mc_y1%d" % i, [128, D], F32) for i in range(2)]
            y2 = [K.sb(st, "mc_y2%d" % i, [128, D], F32) for i in range(2)]
            ob = [K.sb(st, "mc_ob%d" % i, [128, D], F32) for i in range(2)]
            junk = K.sb(st, "mc_junk", [128, D], F32)
            sss = [K.sb(st, "mc_ss%d" % i, [128, 1], F32) for i in range(2)]
            for tl in range(NTL):
                i = tl % 2
                r0 = base + tl * 128
                K.dma("sp", hts[i][:], SC["H1"][r0:r0 + 128, :], ["H1"], ["mc_ht%d" % i])
                S.dma("pool", None, None, K._bl(["ms_DST", "YS"]), K._bl(["mc_y1%d" % i]),
                      fn=lambda e, tl=tl, i=i: e.indirect_dma_start(out=y1[i][:], out_offset=None, in_=YS, in_offset=bass.IndirectOffsetOnAxis(ap=DST[:, tl, 0:1], axis=0)))
                S.dma("pool", None, None, K._bl(["ms_DST", "YS"]), K._bl(["mc_y2%d" % i]),
                      fn=lambda e, tl=tl, i=i: e.indirect_dma_start(out=y2[i][:], out_offset=None, in_=YS, in_offset=bass.IndirectOffsetOnAxis(ap=DST[:, tl, 1:2], axis=0)))
                K.op("dve", "scalar_tensor_tensor", ["mc_y1%d" % i, "ms_GG", "mc_ht%d" % i], ["mc_ht%d" % i], out=hts[i][:], in0=y1[i][:], scalar=GG[:, tl, 0:1], in1=hts[i][:],
                     op0=ALU.mult, op1=ALU.add)
                K.op("dve", "scalar_tensor_tensor", ["mc_y2%d" % i, "ms_GG", "mc_ht%d" % i], ["mc_ht%d" % i], out=hts[i][:], in0=y2[i][:], scalar=GG[:, tl, 1:2], in1=hts[i][:],
                     op0=ALU.mult, op1=ALU.add)
                K.op("act", "activation", ["mc_ht%d" % i], ["mc_junk", "mc_ss%d" % i], out=junk[:], in_=hts[i][:], func=AF.Square, accum_out=sss[i][:])
                K.op("act", "activation", ["mc_ss%d" % i, "eps6"], ["mc_ss%d" % i], out=sss[i][:], in_=sss[i][:], func=AF.Sqrt, scale=1.0 / D, bias=CONST["eps6"][:])
                K.op("dve", "reciprocal", ["mc_ss%d" % i], ["mc_ss%d" % i], out=sss[i][:], in_=sss[i][:])
                K.op("dve", "scalar_tensor_tensor", ["mc_ht%d" % i, "mc_ss%d" % i, "mc_nfb"], ["mc_ob%d" % i], out=ob[i][:], in0=hts[i][:], scalar=sss[i][:], in1=nfb[:],
                     op0=ALU.mult, op1=ALU.mult)
                K.dma("act", OUT[r0:r0 + 128, :], ob[i][:], ["mc_ob%d" % i], ["OUT"])


def build(T, NSEQ, stop_after=99, debug=False):
    nc = bass.Bass("TRN2", target_bir_lowering=False)
    NTOK = NSEQ * T

    def din(name, shape, dt=F32):
        return nc.dram_tensor(name, list(shape), dt, kind="ExternalInput").ap()

    X = din("x", [NTOK, D])
    MEM = din("mem", [NSEQ * 256, D])
    Wd = {}
    for name, shape in WSHAPES.items():
        Wd[name] = din(name, shape)
    identb_d = din("c_identb", [128, 128], BF16)
    identf_d = din("c_identf", [128, 128], F32)
    OUT = nc.dram_tensor("out", [NTOK, D], F32, kind="ExternalOutput").ap()
    SC = {}
    SC["ZF"] = nc.dram_tensor("sc_zf", [NSEQ, R_TOT, T], F32, kind="Internal").ap() if not debug else \
        nc.dram_tensor("sc_zf", [NSEQ, R_TOT, T], F32, kind="ExternalOutput").ap()
    kindd = "ExternalOutput" if debug else "Internal"
    SC["CK"] = nc.dram_tensor("sc_ck", [NSEQ, T, 128], BF16, kind=kindd).ap()
    SC["CKT"] = nc.dram_tensor("sc_ckt", [NSEQ, 128, T], BF16, kind=kindd).ap()
    SC["H1"] = nc.dram_tensor("sc_h1", [NTOK, D], F32, kind=kindd).ap()
    NSLOT = ((2 * T) // 256 + 32) * 256
    SC["WGU"] = nc.dram_tensor("sc_wgu", [32 * 1024, 1024], BF16, kind="Internal").ap()
    SC["WDS"] = nc.dram_tensor("sc_wds", [32 * 512, 1024], BF16, kind="Internal").ap()
    SC["XS"] = nc.dram_tensor("sc_xs", [NSLOT, D], BF16, kind="Internal").ap()
    SC["YS"] = nc.dram_tensor("sc_ys", [NSLOT, D], F32, kind="Internal").ap()
    SC["YB"] = nc.dram_tensor("sc_yb", [NSEQ, 512, T], BF16, kind=kindd).ap()
    SC["YA"] = nc.dram_tensor("sc_ya", [NSEQ, 512, T], BF16, kind=kindd).ap()
    cdram = {}
    for nm, arr in consts().items():
        if nm not in ("c_identb", "c_identf"):
            cdram[nm] = din(nm, arr.shape, BF16 if arr.dtype == ml_dtypes.bfloat16 else F32)
    with ExitStack() as st:
        S = Sched(nc, st)
        K = Ctx(nc, S)
        CONST = {}
        CONST["identb"] = K.sb(st, "identb", [128, 128], BF16)
        CONST["identf"] = K.sb(st, "identf", [128, 128], F32)
        CONST["eps6"] = K.sb(st, "eps6", [128, 1], F32)
        K.dma("sp", CONST["identb"][:], identb_d, [], ["identb"])
        K.dma("sp", CONST["identf"][:], identf_d, [], ["identf"])
        K.op("dve", "memset", [], ["eps6"], ap=CONST["eps6"][:], constant=1e-6)
        for nm, ap in cdram.items():
            sh = list(ap.shape)
            CONST[nm[2:]] = K.sb(st, nm[2:], sh, ap.dtype)
            K.dma("sp", CONST[nm[2:]][:], ap, [], [nm[2:]])
        for s in range(NSEQ):
            phase1(K, s, T, X, Wd, SC, CONST)
            S.barrier()
            if stop_after >= 2 and not os.environ.get("SKIP_DSA"):
                ex_ = (lambda st_: prepack_gen(K, st_, Wd, SC)) if (s == 0 and stop_after >= 6 and not os.environ.get("MOE_DENSE")) else None
                phase_dsa(K, s, T, Wd, SC, CONST, extra=ex_)
                S.barrier()
            if stop_after >= 3:
                phase_rwkv(K, s, T, Wd, SC, CONST)
                S.barrier()
            if stop_after >= 4:
                phase_mix(K, s, T, X, Wd, SC, CONST)
                S.barrier()
            if stop_after >= 5:
                phase_cross(K, s, T, MEM, Wd, SC, CONST)
                S.barrier()
            if stop_after >= 6:
                if os.environ.get("MOE_DENSE"):
                    phase_moe(K, s, T, Wd, SC, CONST, OUT)
                else:
                    phase_moe_sparse(K, s, T, Wd, SC, CONST, OUT)
                S.barrier()
        S.finish(list(K.B.values()))
        print("ops", S.nops, "waits", S.nwaits)
        S.emit()
    return nc


WSHAPES = {
    "norm_mix": [1, 1024], "w_in": [1, 1024, 4804], "shift_mu": [1, 1792], "rw_w0": [1, 512],
    "rw_w2": [1, 64, 512], "rw_a0": [1, 512], "rw_a2": [1, 64, 512], "rw_g2": [1, 128, 512],
    "rw_k_k": [1, 512], "rw_k_a": [1, 512], "rw_r_k": [1, 8, 64], "rw_ln_w": [1, 512], "rw_ln_b": [1, 512],
    "kv_norm": [1, 128], "w_uk": [1, 128, 8, 64], "w_uv": [1, 128, 8, 64], "w_proj_a": [1, 512, 1024],
    "w_proj_b": [1, 512, 1024], "b_gate": [1, 2048], "w_out": [1, 1024, 1024], "norm_cross": [1, 1024],
    "norm_mem": [1, 1024], "w_cq": [1, 1024, 1024], "w_ckv": [1, 1024, 2048], "w_co": [1, 1024, 1024],
    "norm_ffn": [1, 1024], "w_router_g": [1, 1024, 4], "b_router_g": [1, 4], "w_router_e": [1, 1024, 32],
    "b_router_e": [1, 32], "w_e_gate": [1, 32, 1024, 512], "w_e_up": [1, 32, 1024, 512],
    "w_e_down": [1, 32, 512, 1024], "norm_final": [1024],
}


def consts():
    return {
        "c_identb": np.eye(128, dtype=np.float32).astype(ml_dtypes.bfloat16),
        "c_identf": np.eye(128, dtype=np.float32),
        "c_tri01": (np.arange(128)[None, :] <= np.arange(128)[:, None]).astype(np.float32).astype(ml_dtypes.bfloat16),
        "c_negtri": np.where(np.arange(128)[None, :] <= np.arange(128)[:, None], 0.0, -1e30).astype(np.float32),
        "c_bo": np.kron(np.eye(2), np.ones((64, 64))).astype(np.float32),
        "c_bo64": (np.kron(np.eye(2), np.ones((64, 64))) / 64.0).astype(np.float32),
        "c_maskq": np.block([[np.triu(np.ones((64, 64)), 1), np.triu(np.ones((64, 64)), 0)],
                             [np.triu(np.ones((64, 64)), 1), np.triu(np.ones((64, 64)), 0)]]).astype(np.float32),
        "c_lowm": np.concatenate([np.zeros((64, 64)), np.tril(np.ones((64, 64)), -1)], 0).astype(np.float32),
        "c_resetm": np.tile((np.arange(256) % 64 != 0).astype(np.float32)[None, :], (128, 1)),
        "c_utri": (np.arange(128)[:, None] < np.arange(128)[None, :]).astype(np.float32),
        "c_ones128": np.ones((128, 128), np.float32),
        "c_bstart": np.tile((np.arange(320) * 128.0)[None, :], (128, 1)).astype(np.float32),
        "c_iotap": np.arange(128, dtype=np.float32)[:, None].copy(),
        "c_pw": np.tile((0.5 ** (np.arange(NIT) + 1))[None, :], (128, 1)).astype(np.float32),
    }


def kernel(**inputs):
    x = np.asarray(inputs["x"], dtype=np.float32)
    mem = np.asarray(inputs["mem"], dtype=np.float32)
    B, T, _ = x.shape
    nseq = B // NCORES
    nc = build(T, nseq)
    cs = consts()
    in_maps = []
    for c in range(NCORES):
        m = {"x": np.ascontiguousarray(x[c * nseq:(c + 1) * nseq].reshape(nseq * T, D)),
             "mem": np.ascontiguousarray(mem[c * nseq:(c + 1) * nseq].reshape(nseq * 256, D))}
        for name in WSHAPES:
            m[name] = np.ascontiguousarray(np.asarray(inputs[name], dtype=np.float32))
        m.update(cs)
        in_maps.append(m)
    res = run_bass_kernel_spmd(nc, in_maps, core_ids=list(range(NCORES)))
    out = np.concatenate([r["out"].reshape(nseq, T, D) for r in res.results], axis=0)
    return out.astype(np.float32)
```
